# Optimizing a Trainium2 kernel written in Bass

```python
import jax, jax.numpy as jnp
from jax import lax
import numpy as np

D_MODEL = 1024
BATCH = 4
SEQ = 4096
DEPTH = 1

PLE_DIM = 256
EPS = 1e-6
NEG = -1e30

NSA_HEADS = 8
NSA_KV = 2
NSA_HPG = NSA_HEADS // NSA_KV
NSA_HD = 64
CMP_LEN = 32
CMP_STRIDE = 16
CMP_HIDDEN = 256
SEL_BLOCK = 64
SEL_TOPK = 16
SEL_FORCE = 1000.0
WINDOW = 512
Q_BLOCK = 128
ROPE_THETA = 500000.0
ROPE_DIM = NSA_HD // 4

ML_HEADS = 4
ML_HD = 128
ML_WIDTH = ML_HEADS * ML_HD
ML_CHUNK = 64
CONV_W = 4

N_GROUPS = 4
EXP_PER_GROUP = 4
N_EXPERTS = N_GROUPS * EXP_PER_GROUP
TOPK_IN_GROUP = 2
D_EXPERT = 256

NSA_QW = NSA_HEADS * NSA_HD
NSA_KVW = NSA_KV * NSA_HD
IN_SIZES = (NSA_QW, NSA_KVW, NSA_KVW, NSA_KVW, NSA_KVW, NSA_KVW, NSA_KVW, 3 * NSA_HEADS, 2 * ML_WIDTH, ML_WIDTH, ML_WIDTH, 2 * ML_HEADS, 2 * D_MODEL)
D_IN = sum(IN_SIZES)

kernel_name = 'hybrid_nsa_mlstm_hmoe'


def rmsnorm(x, g):
    xf = x.astype(jnp.float32)
    y = xf * lax.rsqrt(jnp.mean(xf * xf, axis=-1, keepdims=True) + EPS)
    return (y * g.astype(jnp.float32)).astype(x.dtype)


def rope_tables(positions):
    inv = ROPE_THETA ** (-jnp.arange(0, ROPE_DIM, 2, dtype=jnp.float32) / ROPE_DIM)
    ang = positions.astype(jnp.float32)[:, :, None] * inv
    return jnp.cos(ang)[:, :, None, :], jnp.sin(ang)[:, :, None, :]


def partial_rope(t, cos, sin):
    half = ROPE_DIM // 2
    tf = t.astype(jnp.float32)
    t1, t2, rest = tf[..., :half], tf[..., half:ROPE_DIM], tf[..., ROPE_DIM:]
    out = jnp.concatenate([t1 * cos - t2 * sin, t2 * cos + t1 * sin, rest], axis=-1)
    return out.astype(t.dtype)


def compress_blocks(tok_g, w1, w2, pe):
    b, g, s, d = tok_g.shape
    n_cmp = (s - CMP_LEN) // CMP_STRIDE + 1
    idx = CMP_STRIDE * jnp.arange(n_cmp)[:, None] + jnp.arange(CMP_LEN)[None, :]
    blk = tok_g[:, :, idx] + pe
    flat = blk.reshape(b, g, n_cmp, CMP_LEN * d)
    return jax.nn.gelu(flat @ w1) @ w2


def nsa_attention(q, kc_tok, vc_tok, ks_tok, vs_tok, kw_tok, vw_tok, gates, w_ck1, w_ck2, pe_ck, w_cv1, w_cv2, pe_cv):
    b, s = q.shape[:2]
    dt = q.dtype
    n_sel = s // SEL_BLOCK
    sel_k = min(SEL_TOPK, n_sel)
    scale = NSA_HD ** -0.5
    qg = q.reshape(b, s, NSA_KV, NSA_HPG, NSA_HD).transpose(0, 2, 3, 1, 4)
    gg = gates.reshape(b, s, NSA_KV, NSA_HPG, 3).transpose(0, 2, 3, 1, 4)

    def to_g(t):
        return t.transpose(0, 2, 1, 3)

    kc = compress_blocks(to_g(kc_tok), w_ck1, w_ck2, pe_ck)
    vc = compress_blocks(to_g(vc_tok), w_cv1, w_cv2, pe_cv)
    n_cmp = kc.shape[2]
    ks = to_g(ks_tok).reshape(b, NSA_KV, n_sel, SEL_BLOCK, NSA_HD)
    vs = to_g(vs_tok).reshape(b, NSA_KV, n_sel, SEL_BLOCK, NSA_HD)
    pad = ((0, 0), (0, 0), (WINDOW, 0), (0, 0))
    kw = jnp.pad(to_g(kw_tok), pad)
    vw = jnp.pad(to_g(vw_tok), pad)
    c_start = CMP_STRIDE * jnp.arange(n_cmp)
    c_end = c_start + CMP_LEN - 1
    s_start = SEL_BLOCK * jnp.arange(n_sel)
    overlap = ((c_start[:, None] < s_start[None, :] + SEL_BLOCK) & (c_end[:, None] >= s_start[None, :])).astype(jnp.float32)
    bi = jnp.arange(b)[:, None, None, None]
    gi = jnp.arange(NSA_KV)[None, :, None, None]
    sel_ids = jnp.arange(n_sel)

    def query_block(c):
        t0 = c * Q_BLOCK
        t = t0 + jnp.arange(Q_BLOCK)
        qc = lax.dynamic_slice_in_dim(qg, t0, Q_BLOCK, axis=3)
        gc = lax.dynamic_slice_in_dim(gg, t0, Q_BLOCK, axis=3).astype(jnp.float32)
        cmask = c_end[None, :] <= t[:, None]
        sc = jnp.einsum('bghqd,bgnd->bghqn', qc, kc).astype(jnp.float32) * scale
        p_cmp = jax.nn.softmax(jnp.where(cmask, sc, NEG), axis=-1)
        p_cmp = p_cmp * jnp.any(cmask, axis=-1, keepdims=True).astype(jnp.float32)
        o_cmp = jnp.einsum('bghqn,bgnd->bghqd', p_cmp.astype(dt), vc)
        imp = jnp.einsum('bghqn,ns->bgqs', p_cmp, overlap)
        cur = t // SEL_BLOCK
        forced = (sel_ids[None, :] == 0) | (sel_ids[None, :] == cur[:, None]) | (sel_ids[None, :] == cur[:, None] - 1)
        imp = jnp.where(forced, imp + SEL_FORCE, imp)
        imp = jnp.where(s_start[None, :] <= t[:, None], imp, NEG)
        _, idx = lax.top_k(imp, sel_k)
        k_sel = ks[bi, gi, idx]
        v_sel = vs[bi, gi, idx]
        kpos = idx[..., None] * SEL_BLOCK + jnp.arange(SEL_BLOCK)
        smask = (kpos <= t[:, None, None])[:, :, None]
        ss = jnp.einsum('bghqd,bgqkrd->bghqkr', qc, k_sel).astype(jnp.float32) * scale
        ss = jnp.where(smask, ss, NEG).reshape(b, NSA_KV, NSA_HPG, Q_BLOCK, sel_k * SEL_BLOCK)
        p_sel = jax.nn.softmax(ss, axis=-1).reshape(b, NSA_KV, NSA_HPG, Q_BLOCK, sel_k, SEL_BLOCK)
        o_sel = jnp.einsum('bghqkr,bgqkrd->bghqd', p_sel.astype(dt), v_sel)
        kwc = lax.dynamic_slice_in_dim(kw, t0, Q_BLOCK + WINDOW, axis=2)
        vwc = lax.dynamic_slice_in_dim(vw, t0, Q_BLOCK + WINDOW, axis=2)
        wpos = t0 - WINDOW + jnp.arange(Q_BLOCK + WINDOW)
        wmask = (wpos[None, :] <= t[:, None]) & (wpos[None, :] > t[:, None] - WINDOW) & (wpos[None, :] >= 0)
        sw = jnp.einsum('bghqd,bgkd->bghqk', qc, kwc).astype(jnp.float32) * scale
        p_win = jax.nn.softmax(jnp.where(wmask, sw, NEG), axis=-1)
        o_win = jnp.einsum('bghqk,bgkd->bghqd', p_win.astype(dt), vwc)
        o = gc[..., 0:1] * o_cmp + gc[..., 1:2] * o_sel + gc[..., 2:3] * o_win
        return o.astype(dt)

    out = lax.map(query_block, jnp.arange(s // Q_BLOCK))
    return out.transpose(1, 0, 4, 2, 3, 5).reshape(b, s, NSA_HEADS * NSA_HD)


def causal_conv_silu(u, w, bias):
    s = u.shape[1]
    up = jnp.pad(u, ((0, 0), (CONV_W - 1, 0), (0, 0)))
    y = bias
    for j in range(CONV_W):
        y = y + w[j] * up[:, j:j + s]
    return jax.nn.silu(y)


def mlstm_chunkwise(q, k, v, i_pre, f_pre):
    b, s, nh, d = q.shape
    nc = s // ML_CHUNK
    lc = ML_CHUNK

    def seq_chunks(t):
        return t.astype(jnp.float32).reshape(b, nc, lc, nh, d).transpose(1, 0, 3, 2, 4)

    def gate_chunks(t):
        return t.astype(jnp.float32).reshape(b, nc, lc, nh).transpose(1, 0, 3, 2)

    qc = seq_chunks(q)
    kc = seq_chunks(k) * (d ** -0.5)
    vc = seq_chunks(v)
    ic = gate_chunks(i_pre)
    lfc = gate_chunks(jax.nn.log_sigmoid(f_pre.astype(jnp.float32)))
    causal = jnp.tril(jnp.ones((lc, lc), dtype=bool))

    def step(carry, inp):
        C, n, m = carry
        qt, kt, vt, it, lft = inp
        bcum = jnp.cumsum(lft, axis=-1)
        dmat = jnp.where(causal, bcum[..., :, None] - bcum[..., None, :] + it[..., None, :], -jnp.inf)
        inter = bcum + m[..., None]
        m_t = jnp.maximum(jnp.max(dmat, axis=-1), inter)
        a = jnp.exp(dmat - m_t[..., None]) * jnp.einsum('bhtd,bhsd->bhts', qt, kt)
        dec = jnp.exp(inter - m_t)
        num = jnp.einsum('bhts,bhsd->bhtd', a, vt) + dec[..., None] * jnp.einsum('bhvk,bhtk->bhtv', C, qt)
        den = jnp.sum(a, axis=-1) + dec * jnp.einsum('bhk,bhtk->bht', n, qt)
        h = num / jnp.maximum(jnp.abs(den), jnp.exp(-m_t))[..., None]
        b_last = bcum[..., -1]
        g_s = b_last[..., None] - bcum + it
        m_new = jnp.maximum(b_last + m, jnp.max(g_s, axis=-1))
        w_prev = jnp.exp(b_last + m - m_new)
        w_s = jnp.exp(g_s - m_new[..., None])
        C = w_prev[..., None, None] * C + jnp.einsum('bhs,bhsv,bhsk->bhvk', w_s, vt, kt)
        n = w_prev[..., None] * n + jnp.einsum('bhs,bhsk->bhk', w_s, kt)
        return (C, n, m_new), h

    init = (jnp.zeros((b, nh, d, d), jnp.float32), jnp.zeros((b, nh, d), jnp.float32), jnp.zeros((b, nh), jnp.float32))
    _, hs = lax.scan(step, init, (qc, kc, vc, ic, lfc))
    return hs.transpose(1, 0, 3, 2, 4).reshape(b, s, nh, d)


def hier_moe(h, w_rg, b_rg, w_re, b_re, w_e13, w_e2):
    b, s, dm = h.shape
    t = h.reshape(b * s, dm)
    n_tok = t.shape[0]
    gl = (t @ w_rg).astype(jnp.float32) + b_rg.astype(jnp.float32)
    pg = jax.nn.softmax(gl, axis=-1)
    _, gsel = lax.top_k(gl, 1)
    el = ((t @ w_re).astype(jnp.float32) + b_re.astype(jnp.float32)).reshape(n_tok, N_GROUPS, EXP_PER_GROUP)
    gidx = jnp.broadcast_to(gsel[:, :, None], (n_tok, 1, EXP_PER_GROUP))
    elg = jnp.take_along_axis(el, gidx, axis=1)[:, 0]
    ev, ei = lax.top_k(elg, TOPK_IN_GROUP)
    wts = jnp.take_along_axis(pg, gsel, axis=1) * jax.nn.softmax(ev, axis=-1)
    eid = gsel * EXP_PER_GROUP + ei
    gate = jnp.sum(jax.nn.one_hot(eid, N_EXPERTS, dtype=jnp.float32) * wts[..., None], axis=1)
    a = jnp.einsum('td,edf->tef', t, w_e13)
    g_, u = jnp.split(a, 2, axis=-1)
    act = jax.nn.silu(g_) * u * gate[:, :, None].astype(t.dtype)
    y = jnp.einsum('tef,efd->td', act, w_e2)
    return y.reshape(b, s, dm).astype(h.dtype)


def setup_inputs(seed: int = 0) -> dict:
    key = jax.random.key(seed)
    ks = jax.random.split(key, 32)
    L, D = DEPTH, D_MODEL
    f32 = jnp.float32

    def nrm(k, shape, fan_in):
        return jax.random.normal(k, shape, f32) * (fan_in ** -0.5)

    def gain(k, shape):
        return 1.0 + 0.02 * jax.random.normal(k, shape, f32)

    x = jax.random.normal(ks[0], (BATCH, SEQ, D), f32)
    p = jax.random.normal(ks[1], (DEPTH, BATCH, SEQ, PLE_DIM), f32)
    positions = jax.random.randint(ks[2], (BATCH, 1), 0, 1024, dtype=jnp.int32) + jnp.arange(SEQ, dtype=jnp.int32)[None, :]
    b_i = 0.1 * jax.random.normal(ks[5], (L, ML_HEADS), f32)
    b_f = jnp.linspace(3.0, 6.0, ML_HEADS, dtype=f32)[None, :] + 0.1 * jax.random.normal(ks[6], (L, ML_HEADS), f32)
    return {
        'x': x,
        'p': p,
        'positions': positions,
        'g_mix': gain(ks[3], (L, D)),
        'w_in': nrm(ks[4], (L, D, D_IN), D),
        'b_if': jnp.concatenate([b_i, b_f], axis=-1),
        'w_ck1': nrm(ks[7], (L, CMP_LEN * NSA_HD, CMP_HIDDEN), CMP_LEN * NSA_HD),
        'w_ck2': nrm(ks[8], (L, CMP_HIDDEN, NSA_HD), CMP_HIDDEN),
        'pe_ck': 0.1 * jax.random.normal(ks[9], (L, CMP_LEN, NSA_HD), f32),
        'w_cv1': nrm(ks[10], (L, CMP_LEN * NSA_HD, CMP_HIDDEN), CMP_LEN * NSA_HD),
        'w_cv2': nrm(ks[11], (L, CMP_HIDDEN, NSA_HD), CMP_HIDDEN),
        'pe_cv': 0.1 * jax.random.normal(ks[12], (L, CMP_LEN, NSA_HD), f32),
        'w_conv': nrm(ks[13], (L, CONV_W, 2 * ML_WIDTH), CONV_W),
        'b_conv': 0.02 * jax.random.normal(ks[14], (L, 2 * ML_WIDTH), f32),
        'g_hn': gain(ks[15], (L, ML_WIDTH)),
        'w_pa': nrm(ks[16], (L, NSA_QW, D), NSA_QW),
        'w_pb': nrm(ks[17], (L, ML_WIDTH, D), ML_WIDTH),
        'w_out': nrm(ks[18], (L, D, D), D),
        'g_ffn': gain(ks[19], (L, D)),
        'w_rg': nrm(ks[20], (L, D, N_GROUPS), D),
        'b_rg': 0.01 * jax.random.normal(ks[21], (L, N_GROUPS), f32),
        'w_re': nrm(ks[22], (L, D, N_EXPERTS), D),
        'b_re': 0.01 * jax.random.normal(ks[23], (L, N_EXPERTS), f32),
        'w_e13': nrm(ks[24], (L, N_EXPERTS, D, 2 * D_EXPERT), D),
        'w_e2': nrm(ks[25], (L, N_EXPERTS, D_EXPERT, D), D_EXPERT),
        'g_ple': gain(ks[26], (L, D)),
        'w_pg': nrm(ks[27], (L, D, D), D),
        'w_pp': nrm(ks[28], (L, PLE_DIM, D), PLE_DIM),
        'g_final': gain(ks[29], (D,)),
    }


def reference(x, p, positions, g_mix, w_in, b_if, w_ck1, w_ck2, pe_ck, w_cv1, w_cv2, pe_cv, w_conv, b_conv, g_hn, w_pa, w_pb, w_out, g_ffn, w_rg, b_rg, w_re, b_re, w_e13, w_e2, g_ple, w_pg, w_pp, g_final):
    b, s, _ = x.shape
    cos, sin = rope_tables(positions)
    offs = [int(o) for o in np.cumsum(IN_SIZES)[:-1]]
    for i in range(DEPTH):
        h = rmsnorm(x, g_mix[i])
        z = h @ w_in[i]
        (q_a, kc_a, vc_a, ks_a, vs_a, kw_a, vw_a, gate_a, qk_b, v_b, o_b, if_b, merge) = jnp.split(z, offs, axis=-1)
        nsa_q = partial_rope(q_a.reshape(b, s, NSA_HEADS, NSA_HD), cos, sin)
        k_cmp = partial_rope(kc_a.reshape(b, s, NSA_KV, NSA_HD), cos, sin)
        k_sel = partial_rope(ks_a.reshape(b, s, NSA_KV, NSA_HD), cos, sin)
        k_win = partial_rope(kw_a.reshape(b, s, NSA_KV, NSA_HD), cos, sin)
        nsa_gates = jax.nn.sigmoid(gate_a.astype(jnp.float32)).reshape(b, s, NSA_HEADS, 3)
        y_a = nsa_attention(nsa_q, k_cmp, vc_a.reshape(b, s, NSA_KV, NSA_HD), k_sel, vs_a.reshape(b, s, NSA_KV, NSA_HD), k_win, vw_a.reshape(b, s, NSA_KV, NSA_HD), nsa_gates, w_ck1[i], w_ck2[i], pe_ck[i], w_cv1[i], w_cv2[i], pe_cv[i])
        qk_c = causal_conv_silu(qk_b, w_conv[i], b_conv[i])
        q_b, k_b = jnp.split(qk_c, 2, axis=-1)
        ifp = if_b.astype(jnp.float32) + b_if[i].astype(jnp.float32)
        hm = mlstm_chunkwise(q_b.reshape(b, s, ML_HEADS, ML_HD), k_b.reshape(b, s, ML_HEADS, ML_HD), v_b.reshape(b, s, ML_HEADS, ML_HD), ifp[..., :ML_HEADS], ifp[..., ML_HEADS:])
        hm = hm * jax.nn.sigmoid(o_b.astype(jnp.float32)).reshape(b, s, ML_HEADS, ML_HD)
        hm = hm * lax.rsqrt(jnp.mean(hm * hm, axis=-1, keepdims=True) + EPS) * g_hn[i].astype(jnp.float32).reshape(ML_HEADS, ML_HD)
        y_b = hm.reshape(b, s, ML_WIDTH).astype(x.dtype)
        g_a, g_b = jnp.split(merge, 2, axis=-1)
        mixed = jax.nn.sigmoid(g_a) * (y_a @ w_pa[i]) + jax.nn.sigmoid(g_b) * (y_b @ w_pb[i])
        x = x + mixed @ w_out[i]
        x = x + hier_moe(rmsnorm(x, g_ffn[i]), w_rg[i], b_rg[i], w_re[i], b_re[i], w_e13[i], w_e2[i])
        x = x + jax.nn.sigmoid(rmsnorm(x, g_ple[i]) @ w_pg[i]) * (p[i] @ w_pp[i])
    return rmsnorm(x, g_final)
```

```python
import numpy as np
import concourse.bass as bass
import concourse.mybir as mybir
from concourse.bass_utils import run_bass_kernel_spmd
from contextlib import ExitStack

F32 = mybir.dt.float32
BF16 = mybir.dt.bfloat16
I32 = mybir.dt.int32
AF = mybir.ActivationFunctionType
ALU = mybir.AluOpType
AX = mybir.AxisListType

D = 1024
S_OWN = 2048
S_EXT = 4096
NT_OWN = 16
NT_EXT = 32
EPS = 1e-6
NEGB = -30000.0
DBG = []


class Buf:
    __slots__ = ("t", "lw", "rd", "name", "excl")

    def __init__(self, t, name=""):
        self.t = t
        self.excl = False
        self.lw = None
        self.rd = {}
        self.name = name

    def __getitem__(self, k):
        return self.t[k]


class FW:
    NDMA = 24

    def __init__(self, nc, es):
        self.nc = nc
        self.es = es
        self.eng = {"pe": nc.tensor, "act": nc.scalar, "dve": nc.vector, "pool": nc.gpsimd, "sp": nc.sync}
        self.sem = {k: es.enter_context(nc.semaphore("s_" + k)) for k in self.eng}
        self.cnt = {k: 0 for k in self.eng}
        self.known = {k: {} for k in self.eng}
        self.dsem = [es.enter_context(nc.semaphore(f"s_dma{i}")) for i in range(self.NDMA)]
        self.dval = [0] * self.NDMA
        self.dnext = 0
        self.nbuf = 0
        self.out_waits = []
        self.stopped = False

    def sb(self, shape, dt, name=None, es=None):
        self.nbuf += 1
        name = f"sb{self.nbuf}_" + (name or "t")
        return Buf((es or self.es).enter_context(self.nc.sbuf_tensor(name, list(shape), dt)), name)

    def ps(self, shape, dt, name=None):
        self.nbuf += 1
        name = name or f"ps{self.nbuf}"
        b = Buf(self.es.enter_context(self.nc.psum_tensor(name, list(shape), dt)), name)
        b.excl = True
        return b

    def _wait(self, e, src, idx):
        if self.stopped:
            return
        kn = self.known[e]
        if kn.get(src, 0) >= idx:
            return
        s = self.dsem[src[1]] if isinstance(src, tuple) else self.sem[src]
        self.eng[e].wait_ge(s, idx)
        kn[src] = idx

    def _deps(self, e, reads, writes):
        for b in reads:
            if b.lw is not None:
                self._wait(e, b.lw[0], b.lw[1])
            if b.excl:
                for src, idx in b.rd.items():
                    if src != e:
                        self._wait(e, src, idx)
        for b in writes:
            if b.lw is not None and b.lw[0] != e:
                self._wait(e, b.lw[0], b.lw[1])
            for src, idx in b.rd.items():
                if src != e:
                    self._wait(e, src, idx)

    def op(self, e, fn, reads=(), writes=()):
        if self.stopped:
            return None
        self._deps(e, reads, writes)
        inst = fn(self.eng[e])
        self.cnt[e] += 1
        c = self.cnt[e]
        inst.then_inc(self.sem[e], 1)
        for b in reads:
            if b.rd.get(e, 0) < c:
                b.rd[e] = c
        for b in writes:
            b.lw = (e, c)
            b.rd = {}
        return inst

    def dma(self, out, in_, reads=(), writes=(), q="sp", is_output=False):
        if self.stopped and not is_output:
            return None
        self._deps(q, reads, writes)
        slot = self.dnext
        self.dnext = (self.dnext + 1) % self.NDMA
        key = ("d", slot)
        if self.dval[slot] > 0:
            self._wait(q, key, self.dval[slot])
        inst = self.eng[q].dma_start(out=out, in_=in_)
        self.dval[slot] += 16
        inst.then_inc(self.dsem[slot], 16)
        v = self.dval[slot]
        for b in reads:
            if b.rd.get(key, 0) < v:
                b.rd[key] = v
        for b in writes:
            b.lw = (key, v)
            b.rd = {}
        if is_output:
            self.out_waits.append((key, v))
        return inst

    def barrier(self):
        for e in self.eng:
            for src in ("pe", "act", "dve", "pool"):
                if src != e and self.cnt[src] > 0:
                    self._wait(e, src, self.cnt[src])
            for slot in range(self.NDMA):
                if self.dval[slot] > 0:
                    self._wait(e, ("d", slot), self.dval[slot])

    def scope(self):
        fw = self

        class _Scope(ExitStack):
            def __exit__(self, *a):
                fw.barrier()
                return super().__exit__(*a)
        return _Scope()

    def finish(self):
        for key, v in self.out_waits:
            self._wait("sp", key, v)
        for k in ("pe", "act", "dve", "pool"):
            if self.cnt[k] > 0:
                self._wait("sp", k, self.cnt[k])


class _StopBuild(Exception):
    pass


def build_program(dbg=()):
    nc = bass.Bass("TRN2", target_bir_lowering=False)

    def din(name, shape, dt=F32):
        return nc.dram_tensor(name, list(shape), dt, kind="ExternalInput").ap()

    xe = din("xe", [S_EXT, D])
    pos_d = din("pos", [128, NT_EXT], I32)
    pl_d = din("pl", [S_OWN, 256])
    hv_d = din("hv", [128, 1])
    invf_d = din("invf", [128, 8])
    gvec_d = din("gvec", [4, D])
    w_att_d = din("w_att", [2, D, 652])
    w_qk_d = din("w_qk", [D, 1024])
    w_vo_d = din("w_vo", [D, 1024])
    w_if_d = din("w_if", [D, 8])
    w_mg_d = din("w_mg", [D, 2048])
    b_if_d = din("b_if", [1, 8])
    w_c1_d = din("w_c1", [2, 2048, 256])
    w_c2_d = din("w_c2", [2, 256, 64])
    pe_c_d = din("pe_c", [2, 32, 64])
    wc_d = din("wc", [128, 8, 4])
    bc_d = din("bc", [128, 8])
    g_hn_d = din("g_hn", [1, 512])
    w_pa_d = din("w_pa", [512, D])
    w_pb_d = din("w_pb", [512, D])
    w_out_d = din("w_out", [D, D])
    w_r_d = din("w_r", [D, 20])
    b_r_d = din("b_r", [1, 20])
    w_e13_d = din("w_e13", [16, D, 512])
    w_e2_d = din("w_e2", [16, 256, D])
    w_pg_d = din("w_pg", [D, D])
    w_pp_d = din("w_pp", [256, D])
    out_d = nc.dram_tensor("out", [S_OWN, D], F32, kind="ExternalOutput").ap()
    hT_d = nc.dram_tensor("hT_scr", [128, 8, S_EXT], BF16, kind="Internal").ap()
    dbg_out = {}

    def dbg_t(name, shape, dt=F32):
        dbg_out[name] = nc.dram_tensor("dbg_" + name, list(shape), dt, kind="ExternalOutput").ap()
        return dbg_out[name]

    with ExitStack() as es:
        fw = FW(nc, es)
        op = fw.op
        PS = [fw.ps([128, 512], F32, f"psb{i}") for i in range(8)]

        def psbf(i):
            return PS[i][:].bitcast(BF16)

        ones_f = fw.sb([128, 128], F32, "ones_f")
        op("pool", lambda e: e.memset(ones_f[:], 1.0), writes=[ones_f])
        idf = fw.sb([128, 128], F32, "idf")
        op("pool", lambda e: e.affine_select(out=idf[:], in_=ones_f[:], pattern=[[1, 128]], compare_op=ALU.is_equal,
                                             fill=0.0, base=0, channel_multiplier=-1), reads=[ones_f], writes=[idf])
        idb = fw.sb([128, 128], BF16, "idb")
        op("dve", lambda e: e.tensor_copy(out=idb[:], in_=idf[:]), reads=[idf], writes=[idb])
        U_f = fw.sb([128, 128], F32, "U_f")
        op("pool", lambda e: e.affine_select(out=U_f[:], in_=ones_f[:], pattern=[[1, 128]], compare_op=ALU.is_ge,
                                             fill=0.0, base=0, channel_multiplier=-1), reads=[ones_f], writes=[U_f])
        caus = fw.sb([128, 128], BF16, "caus")
        op("dve", lambda e: e.tensor_copy(out=caus[:], in_=U_f[:]), reads=[U_f], writes=[caus])
        wm0_f = fw.sb([128, 128], F32, "wm0_f")
        op("pool", lambda e: e.affine_select(out=wm0_f[:], in_=ones_f[:], pattern=[[-1, 128]], compare_op=ALU.is_ge,
                                             fill=0.0, base=-1, channel_multiplier=1), reads=[ones_f], writes=[wm0_f])
        wm0 = fw.sb([128, 128], BF16, "wm0")
        op("dve", lambda e: e.tensor_copy(out=wm0[:], in_=wm0_f[:]), reads=[wm0_f], writes=[wm0])
        c_eps = fw.sb([128, 1], F32, "c_eps")
        op("pool", lambda e: e.memset(c_eps[:], EPS), writes=[c_eps])
        c_one = fw.sb([128, 1], F32, "c_one")
        op("pool", lambda e: e.memset(c_one[:], 1.0), writes=[c_one])
        c_zero = fw.sb([128, 1], F32, "c_zero")
        op("pool", lambda e: e.memset(c_zero[:], 0.0), writes=[c_zero])
        acc_junk = fw.sb([128, 2], F32, "acc_junk")
        op("act", lambda e: e.activation(out=acc_junk[:, 0:1], in_=c_one[:], func=AF.Square, accum_out=acc_junk[:, 1:2]),
           reads=[c_one], writes=[acc_junk])
        hv = fw.sb([128, 1], F32, "hv")
        fw.dma(hv[:], hv_d[:, :], writes=[hv])
        hbias = fw.sb([128, 1], F32, "hbias")
        op("dve", lambda e: e.tensor_scalar(out=hbias[:], in0=hv[:], scalar1=-1.0, scalar2=-NEGB, op0=ALU.add, op1=ALU.mult),
           reads=[hv], writes=[hbias])
        gB = fw.sb([128, D], F32, "gB")

        def load_gain(i):
            fw.dma(gB[:], gvec_d[i:i + 1, :].to_broadcast([128, D]), writes=[gB])

        cs = fw.sb([128, NT_EXT, 8], F32, "cs")
        sn = fw.sb([128, NT_EXT, 8], F32, "sn")
        with fw.scope() as es1:
            posi = fw.sb([128, NT_EXT], I32, "posi", es1)
            posf = fw.sb([128, NT_EXT], F32, "posf", es1)
            invf = fw.sb([128, 8], F32, "invf", es1)
            ang = fw.sb([128, NT_EXT, 8], F32, "ang", es1)
            kf = fw.sb([128, NT_EXT, 8], F32, "kf", es1)
            ki = fw.sb([128, NT_EXT, 8], I32, "ki", es1)
            r1 = fw.sb([128, NT_EXT, 8], F32, "r1", es1)
            r2 = fw.sb([128, NT_EXT, 8], F32, "r2", es1)
            fw.dma(posi[:], pos_d[:, :], writes=[posi])
            fw.dma(invf[:], invf_d[:, :], writes=[invf])
            op("dve", lambda e: e.tensor_copy(out=posf[:], in_=posi[:]), reads=[posi], writes=[posf])
            op("dve", lambda e: e.tensor_tensor(out=ang[:], in0=posf[:].unsqueeze(2).to_broadcast([128, NT_EXT, 8]),
                                                in1=invf[:].unsqueeze(1).to_broadcast([128, NT_EXT, 8]), op=ALU.mult),
               reads=[posf, invf], writes=[ang])
            TWO_PI = 6.283185307179586
            C1 = 6.28125
            C2 = TWO_PI - C1
            PI_LO = 3.1415925
            op("dve", lambda e: e.tensor_scalar(out=kf[:], in0=ang[:], scalar1=1.0 / TWO_PI, scalar2=None, op0=ALU.mult),
               reads=[ang], writes=[kf])
            op("dve", lambda e: e.tensor_copy(out=ki[:], in_=kf[:]), reads=[kf], writes=[ki])
            op("dve", lambda e: e.tensor_copy(out=kf[:], in_=ki[:]), reads=[ki], writes=[kf])
            op("dve", lambda e: e.scalar_tensor_tensor(out=r1[:], in0=kf[:], scalar=-C1, in1=ang[:], op0=ALU.mult, op1=ALU.add),
               reads=[kf, ang], writes=[r1])
            op("dve", lambda e: e.scalar_tensor_tensor(out=r1[:], in0=kf[:], scalar=-C2, in1=r1[:], op0=ALU.mult, op1=ALU.add),
               reads=[kf, r1], writes=[r1])
            op("dve", lambda e: e.tensor_scalar(out=r1[:], in0=r1[:], scalar1=PI_LO, scalar2=-PI_LO, op0=ALU.min, op1=ALU.max),
               reads=[r1], writes=[r1])
            op("act", lambda e: e.activation(out=sn[:], in_=r1[:], func=AF.Sin), reads=[r1], writes=[sn])
            op("dve", lambda e: e.tensor_scalar(out=r2[:], in0=r1[:], scalar1=PI_LO / 2 + 0.0, scalar2=None, op0=ALU.add),
               reads=[r1], writes=[r2])
            op("dve", lambda e: e.tensor_scalar(out=kf[:], in0=r2[:], scalar1=PI_LO, scalar2=-TWO_PI, op0=ALU.is_gt, op1=ALU.mult),
               reads=[r2], writes=[kf])
            op("dve", lambda e: e.tensor_tensor(out=r2[:], in0=r2[:], in1=kf[:], op=ALU.add), reads=[r2, kf], writes=[r2])
            op("dve", lambda e: e.tensor_scalar(out=r2[:], in0=r2[:], scalar1=PI_LO, scalar2=-PI_LO, op0=ALU.min, op1=ALU.max),
               reads=[r2], writes=[r2])
            op("act", lambda e: e.activation(out=cs[:], in_=r2[:], func=AF.Sin), reads=[r2], writes=[cs])

        def rms_rstd(src, rstd, n, junk):
            ss = rstd["ss"]
            op("act", lambda e: e.activation(out=junk["ap"], in_=src["ap"], func=AF.Square, accum_out=ss[:]),
               reads=src["bufs"], writes=[junk["buf"], ss])
            op("act", lambda e: e.activation(out=ss[:], in_=ss[:], func=AF.Sqrt, bias=c_eps[:], scale=1.0 / n),
               reads=[ss, c_eps], writes=[ss])
            op("dve", lambda e: e.reciprocal(out=rstd["r"][:], in_=ss[:]), reads=[ss], writes=[rstd["r"]])

        load_gain(0)
        hT_tiles = [Buf(None, f"hT_tile{t}") for t in range(NT_EXT)]
        with fw.scope() as esA:
            xt = [fw.sb([128, D], F32, f"xtA{i}", esA) for i in range(3)]
            xn = [fw.sb([128, D], BF16, f"xnA{i}", esA) for i in range(2)]
            junk = fw.sb([128, D], BF16, "junkA", esA)
            hst = [fw.sb([128, 8, 128], BF16, f"hstA{i}", esA) for i in range(2)]
            ssA = [fw.sb([128, 1], F32, f"ssA{i}", esA) for i in range(2)]
            rrA = [fw.sb([128, 1], F32, f"rrA{i}", esA) for i in range(2)]
            for t in range(NT_EXT):
                x_ = xt[t % 3]
                fw.dma(x_[:], xe[t * 128:(t + 1) * 128, :], writes=[x_])
                rs = {"ss": ssA[t % 2], "r": rrA[t % 2]}
                rms_rstd({"ap": x_[:], "bufs": [x_]}, rs, D, {"ap": junk[:], "buf": junk})
                n_ = xn[t % 2]
                op("dve", lambda e: e.scalar_tensor_tensor(out=n_[:], in0=x_[:], scalar=rs["r"][:], in1=gB[:], op0=ALU.mult, op1=ALU.mult),
                   reads=[x_, rs["r"], gB], writes=[n_])
                pb = t % 2
                for k in range(8):
                    op("pe", lambda e: e.transpose(out=psbf(pb)[:, k * 128:(k + 1) * 128], in_=n_[:, k * 128:(k + 1) * 128], identity=idb[:]),
                       reads=[n_, idb], writes=[PS[pb]])
                h_ = hst[t % 2]
                op("act", lambda e: e.copy(out=h_[:], in_=psbf(pb).rearrange("p (k t) -> p k t", k=8)), reads=[PS[pb]], writes=[h_])
                fw.dma(hT_d[:, :, t * 128:(t + 1) * 128], h_[:], reads=[h_], writes=[hT_tiles[t]])


        if "cs" in dbg:
            o = dbg_t("cs", [128, NT_EXT, 8])
            fw.dma(o[:, :, :], cs[:], reads=[cs], is_output=True)
            o = dbg_t("sn", [128, NT_EXT, 8])
            fw.dma(o[:, :, :], sn[:], reads=[sn], is_output=True)

        def ckpt(name):
            if ("stop_" + name) in dbg:
                fw.stopped = True

        def body():
            def mm(bank, out_ap, lhsT, rhs, start, stop, reads):
                op("pe", lambda e: e.matmul(out_ap, lhsT, rhs, start=start, stop=stop), reads=reads, writes=[bank])

            YT = fw.sb([128, 8, S_OWN], BF16, "YT")
            esBc = fw.scope()
            esBc.__enter__()
            Esel = fw.sb([64, S_EXT], BF16, "Esel", esBc)
            op("pool", lambda e: e.memset(Esel[:], 1.0), writes=[Esel])
            op("pool", lambda e: e.affine_select(out=Esel[:], in_=Esel[:], pattern=[[1, S_EXT]], compare_op=ALU.is_ge, fill=0.0,
                                                 base=0, channel_multiplier=-64), reads=[Esel], writes=[Esel])
            op("pool", lambda e: e.affine_select(out=Esel[:], in_=Esel[:], pattern=[[-1, S_EXT]], compare_op=ALU.is_ge, fill=0.0,
                                                 base=63, channel_multiplier=64), reads=[Esel], writes=[Esel])
            cmask = fw.sb([128, 2, S_OWN], BF16, "cmask", esBc)
            op("pool", lambda e: e.memset(cmask[:], 1.0), writes=[cmask])
            op("pool", lambda e: e.affine_select(out=cmask[:, 0, :], in_=cmask[:, 0, :], pattern=[[1, S_OWN]], compare_op=ALU.is_ge, fill=0.0,
                                                 base=2017, channel_multiplier=-16), reads=[cmask], writes=[cmask])
            op("pool", lambda e: e.affine_select(out=cmask[:, 1, :], in_=cmask[:, 1, :], pattern=[[1, S_OWN]], compare_op=ALU.is_ge, fill=0.0,
                                                 base=-31, channel_multiplier=-16), reads=[cmask], writes=[cmask])
            ovl = fw.sb([128, 2, 64], BF16, "ovl", esBc)
            op("pool", lambda e: e.memset(ovl[:], 1.0), writes=[ovl])
            for j in range(2):
                op("pool", lambda e: e.affine_select(out=ovl[:, j, :], in_=ovl[:, j, :], pattern=[[-4, 64]], compare_op=ALU.is_ge, fill=0.0,
                                                     base=128 * j + 1, channel_multiplier=1), reads=[ovl], writes=[ovl])
                op("pool", lambda e: e.affine_select(out=ovl[:, j, :], in_=ovl[:, j, :], pattern=[[4, 64]], compare_op=ALU.is_ge, fill=0.0,
                                                     base=3 - 128 * j, channel_multiplier=-1), reads=[ovl], writes=[ovl])
            maskadd = fw.sb([128, NT_OWN, 64], F32, "maskadd", esBc)
            Mb = fw.sb([128, 64], F32, "Mb", esBc)
            hm1 = fw.sb([128, 2], F32, "hm1", esBc)
            op("dve", lambda e: e.tensor_scalar(out=hm1[:, 0:1], in0=hv[:], scalar1=-1.0, scalar2=1e30, op0=ALU.add, op1=ALU.mult),
               reads=[hv], writes=[hm1])
            op("dve", lambda e: e.tensor_scalar(out=hm1[:, 1:2], in0=hv[:], scalar1=-1.0, scalar2=-1000.0, op0=ALU.add, op1=ALU.mult),
               reads=[hv, hm1], writes=[hm1])
            op("dve", lambda e: e.memset(Mb[:], 0.0), writes=[Mb])
            op("dve", lambda e: e.tensor_copy(out=Mb[:, 0:32], in_=hm1[:, 0:1].to_broadcast([128, 32])), reads=[hm1, Mb], writes=[Mb])
            op("dve", lambda e: e.scalar_tensor_tensor(out=Mb[:, 0:1], in0=hv[:], scalar=1000.0, in1=Mb[:, 0:1], op0=ALU.mult, op1=ALU.add),
               reads=[hv, Mb], writes=[Mb])
            op("dve", lambda e: e.tensor_copy(out=Mb[:, 32:33], in_=hm1[:, 1:2]), reads=[hm1, Mb], writes=[Mb])
            for c in range(NT_OWN):
                op("pool", lambda e: e.tensor_copy(out=maskadd[:, c, :], in_=Mb[:]), reads=[Mb, maskadd], writes=[maskadd])
                for hf in range(2):
                    lo = 32 + 2 * c + hf + 1
                    if lo < 64:
                        op("pool", lambda e: e.memset(maskadd[hf * 64:(hf + 1) * 64, c, lo:64], -1e30), reads=[maskadd], writes=[maskadd])
                    for col in (32 + 2 * c + hf, 32 + 2 * c + hf - 1):
                        op("pool", lambda e: e.tensor_scalar(out=maskadd[hf * 64:(hf + 1) * 64, c, col:col + 1],
                                                             in0=maskadd[hf * 64:(hf + 1) * 64, c, col:col + 1],
                                                             scalar1=1000.0, scalar2=None, op0=ALU.add), reads=[maskadd], writes=[maskadd])

            ckpt("consts")
            for g in range(2):
                with fw.scope() as esG:
                    qT = fw.sb([64, 4, S_OWN], BF16, f"qT{g}", esG)
                    kkT = fw.sb([64, 2, S_EXT], BF16, f"kkT{g}", esG)
                    vv = fw.sb([128, NT_EXT, 2, 65], BF16, f"vv{g}", esG)
                    gsig = fw.sb([128, NT_OWN, 12], F32, f"gsig{g}", esG)
                    kcmpT = fw.sb([64, 256], BF16, f"kcmpT{g}", esG)
                    vca = fw.sb([128, 2, 65], BF16, f"vca{g}", esG)
                    op("pool", lambda e: e.memset(vv[:, :, :, 64:65], 1.0), writes=[vv])
                    op("pool", lambda e: e.memset(vca[:, :, 64:65], 1.0), writes=[vca])
                    ckpt("B0a")
                    with fw.scope() as esC:
                        ccT = fw.sb([64, 2, S_EXT], BF16, f"ccT{g}", esC)
                        with fw.scope() as esB1:
                            w_att = fw.sb([128, 8, 652], BF16, f"w_att{g}", esB1)
                            fw.dma(w_att[:], w_att_d[g].rearrange("(k p) c -> p k c", p=128), writes=[w_att], q="pool")
                            ckpt("B0b")
                            hblk = [fw.sb([128, 8, 512], BF16, f"hblkB{g}{i}", esB1) for i in range(2)]
                            rp = [fw.sb([128, 8, 64], BF16, f"rp{g}{i}", esB1) for i in range(2)]
                            rpf = [fw.sb([128, 8, 64], F32, f"rpf{g}{i}", esB1) for i in range(2)]
                            ta = fw.sb([128, 7, 8], F32, f"ropa{g}", esB1)
                            tb_ = fw.sb([128, 7, 8], F32, f"ropb{g}", esB1)
                            for t in range(NT_EXT):
                                own = t >= NT_OWN
                                tq = t - NT_OWN
                                hb = hblk[(t // 4) % 2]
                                if t % 4 == 0:
                                    fw.dma(hb[:], hT_d[:, :, t * 128:(t + 4) * 128], reads=hT_tiles[t:t + 4], writes=[hb])
                                tl = t % 4
                                a0 = 0 if own else 256
                                nb = 140 if own else 128
                                bA = 2 + t % 2
                                bB = 4 + t % 2
                                if t == 0:
                                    ckpt("B1x")
                                for k in range(8):
                                    mm(PS[bA], PS[bA][:, a0:512], hb[:, k, tl * 128:(tl + 1) * 128], w_att[:, k, a0:512], k == 0, k == 7, [hb, w_att])
                                if t == 0:
                                    ckpt("B1y")
                                for k in range(8):
                                    mm(PS[bB], PS[bB][:, 0:nb], hb[:, k, tl * 128:(tl + 1) * 128], w_att[:, k, 512:512 + nb], k == 0, k == 7, [hb, w_att])
                                if t == 0:
                                    ckpt("B1a")
                                rp_ = rp[t % 2]
                                h0 = a0 // 64
                                nh = 7 - h0
                                rf = rpf[t % 2]
                                op("act", lambda e: e.copy(out=rf[:, h0:8, :], in_=PS[bA][:, a0:512].rearrange("p (h d) -> p h d", d=64)),
                                   reads=[PS[bA]], writes=[rf])
                                op("pool", lambda e: e.tensor_copy(out=rp_[:, h0:8, :], in_=rf[:, h0:8, :]), reads=[rf], writes=[rp_])
                                if t == 0:
                                    ckpt("B1r0")
                                t1 = rf[:, h0:7, 0:8]
                                t2 = rf[:, h0:7, 8:16]
                                Cb = cs[:, t, :].unsqueeze(1).to_broadcast([128, nh, 8])
                                Sb_ = sn[:, t, :].unsqueeze(1).to_broadcast([128, nh, 8])
                                op("dve", lambda e: e.tensor_tensor(out=ta[:, 0:nh, :], in0=t1, in1=Cb, op=ALU.mult), reads=[rf, cs], writes=[ta])
                                op("dve", lambda e: e.tensor_tensor(out=tb_[:, 0:nh, :], in0=t2, in1=Sb_, op=ALU.mult), reads=[rf, sn], writes=[tb_])
                                if t == 0:
                                    ckpt("B1r1")
                                op("dve", lambda e: e.tensor_tensor(out=rp_[:, h0:7, 0:8], in0=ta[:, 0:nh, :], in1=tb_[:, 0:nh, :], op=ALU.subtract),
                                   reads=[ta, tb_, rp_], writes=[rp_])
                                op("dve", lambda e: e.tensor_tensor(out=ta[:, 0:nh, :], in0=t2, in1=Cb, op=ALU.mult), reads=[rf, cs, ta], writes=[ta])
                                op("dve", lambda e: e.tensor_tensor(out=tb_[:, 0:nh, :], in0=t1, in1=Sb_, op=ALU.mult), reads=[rf, sn, tb_], writes=[tb_])
                                op("dve", lambda e: e.tensor_tensor(out=rp_[:, h0:7, 8:16], in0=ta[:, 0:nh, :], in1=tb_[:, 0:nh, :], op=ALU.add),
                                   reads=[ta, tb_, rp_], writes=[rp_])
                                if t == 0:
                                    ckpt("B1b")
                                bT = t % 2
                                psT = psbf(bT)
                                for j, hh in enumerate(range(h0, 8)):
                                    op("pe", lambda e: e.transpose(out=psT[0:64, j * 128:(j + 1) * 128], in_=rp_[:, hh, :], identity=idb[:]),
                                       reads=[rp_, idb], writes=[PS[bT]])
                                if t == 0:
                                    ckpt("B1c")
                                if own:
                                    op("act", lambda e: e.copy(out=qT[:, :, tq * 128:(tq + 1) * 128], in_=psT[0:64, 0:512].rearrange("p (h t) -> p h t", h=4)),
                                       reads=[PS[bT]], writes=[qT])
                                    o1 = 512
                                else:
                                    o1 = 0
                                op("act", lambda e: e.copy(out=kkT[:, :, t * 128:(t + 1) * 128], in_=psT[0:64, o1:o1 + 256].rearrange("p (h t) -> p h t", h=2)),
                                   reads=[PS[bT]], writes=[kkT])
                                op("act", lambda e: e.copy(out=ccT[:, :, t * 128:(t + 1) * 128], in_=psT[0:64, o1 + 256:o1 + 512].rearrange("p (h t) -> p h t", h=2)),
                                   reads=[PS[bT]], writes=[ccT])
                                op("dve", lambda e: e.tensor_copy(out=vv[:, t, :, 0:64], in_=PS[bB][:, 0:128].rearrange("p (h d) -> p h d", d=64)),
                                   reads=[PS[bB]], writes=[vv])
                                if own:
                                    op("act", lambda e: e.activation(out=gsig[:, tq, :], in_=PS[bB][:, 128:140], func=AF.Sigmoid),
                                       reads=[PS[bB]], writes=[gsig])
                        ckpt("B1")
                        for i in range(2):
                            with fw.scope() as esB2:
                                w1 = fw.sb([64, 32, 256], BF16, f"w1_{g}{i}", esB2)
                                fw.dma(w1[:], w_c1_d[i].rearrange("(l d) h -> d l h", d=64), writes=[w1], q="pool")
                                w2 = fw.sb([128, 2, 64], BF16, f"w2_{g}{i}", esB2)
                                fw.dma(w2[:], w_c2_d[i].rearrange("(c p) d -> p c d", p=128), writes=[w2], q="pool")
                                pe_sb = fw.sb([32, 64], BF16, f"pe_{g}{i}", esB2)
                                fw.dma(pe_sb[:], pe_c_d[i], writes=[pe_sb], q="pool")
                                peT = fw.sb([64, 32], BF16, f"peT_{g}{i}", esB2)
                                op("pe", lambda e: e.transpose(out=psbf(6)[0:64, 0:32], in_=pe_sb[:, :], identity=idb[0:32, 0:32]),
                                   reads=[pe_sb, idb], writes=[PS[6]])
                                op("act", lambda e: e.copy(out=peT[:], in_=psbf(6)[0:64, 0:32]), reads=[PS[6]], writes=[peT])
                                for hc in range(2):
                                    for l in range(32):
                                        mm(PS[7], PS[7][:, hc:hc + 1], w1[:, l, hc * 128:(hc + 1) * 128], peT[:, l:l + 1], l == 0, l == 31, [w1, peT])
                                cbs = fw.sb([128, 2], F32, f"cbs_{g}{i}", esB2)
                                op("act", lambda e: e.copy(out=cbs[:], in_=PS[7][:, 0:2]), reads=[PS[7]], writes=[cbs])
                                G = fw.sb([128, 2, 256], BF16, f"G_{g}{i}", esB2)
                                op("pool", lambda e: e.memset(G[:, :, 255:256], 0.0), writes=[G])
                                u_ = fw.sb([128, 255], F32, f"u_{g}{i}", esB2)
                                u2 = fw.sb([128, 255], F32, f"u2_{g}{i}", esB2)
                                sg_ = fw.sb([128, 255], F32, f"sg_{g}{i}", esB2)
                                for hc in range(2):
                                    for l in range(32):
                                        mm(PS[hc], PS[hc][:, 0:255], w1[:, l, hc * 128:(hc + 1) * 128], ccT[:, i, l:l + 16 * 254 + 1:16], l == 0, l == 31, [w1, ccT])
                                    op("act", lambda e: e.activation(out=u_[:], in_=PS[hc][:, 0:255], func=AF.Identity, bias=cbs[:, hc:hc + 1]),
                                       reads=[PS[hc], cbs], writes=[u_])
                                    op("dve", lambda e: e.tensor_tensor(out=u2[:], in0=u_[:], in1=u_[:], op=ALU.mult), reads=[u_], writes=[u2])
                                    op("dve", lambda e: e.tensor_scalar(out=u2[:], in0=u2[:], scalar1=0.044715, scalar2=1.0, op0=ALU.mult, op1=ALU.add),
                                       reads=[u2], writes=[u2])
                                    op("dve", lambda e: e.tensor_tensor(out=u2[:], in0=u2[:], in1=u_[:], op=ALU.mult), reads=[u2, u_], writes=[u2])
                                    op("act", lambda e: e.activation(out=sg_[:], in_=u2[:], func=AF.Sigmoid, scale=1.5957691216057308),
                                       reads=[u2], writes=[sg_])
                                    op("dve", lambda e: e.tensor_tensor(out=G[:, hc, 0:255], in0=u_[:], in1=sg_[:], op=ALU.mult), reads=[u_, sg_], writes=[G])
                                if i == 0:
                                    for hc in range(2):
                                        mm(PS[6], PS[6][0:64, 0:256], w2[:, hc, :], G[:, hc, :], hc == 0, hc == 1, [w2, G])
                                    op("act", lambda e: e.copy(out=kcmpT[:], in_=PS[6][0:64, 0:256]), reads=[PS[6]], writes=[kcmpT])
                                else:
                                    for nch in range(2):
                                        for hc in range(2):
                                            mm(PS[6], PS[6][:, nch * 64:(nch + 1) * 64], G[:, hc, nch * 128:(nch + 1) * 128], w2[:, hc, :], hc == 0, hc == 1, [w2, G])
                                    op("act", lambda e: e.copy(out=vca[:, :, 0:64], in_=PS[6][:, 0:128].rearrange("p (n d) -> p n d", d=64)),
                                       reads=[PS[6]], writes=[vca])
                    if g == 0 and "B2dump" in dbg:
                        for nm, bf, shp in (("kkT", kkT, [64, 2, S_EXT]), ("qT", qT, [64, 4, S_OWN]), ("vv", vv, [128, NT_EXT, 2, 65]),
                                            ("kcmpT", kcmpT, [64, 256]), ("vca", vca, [128, 2, 65])):
                            o = dbg_t(nm, shp, BF16)
                            fw.dma(o, bf[:], reads=[bf], is_output=True)
                        o = dbg_t("gsig", [128, NT_OWN, 12])
                        fw.dma(o, gsig[:], reads=[gsig], is_output=True)
                    ckpt("B2")
                    with fw.scope() as esB3:
                        Pb = [fw.sb([128, 512], BF16, f"Pb{g}{i}", esB3) for i in range(3)]
                        Sbank = [0, 1, 6]
                        ya = fw.sb([128, 4, 64], F32, f"ya{g}", esB3)
                        yat = fw.sb([128, 4, 64], BF16, f"yat{g}", esB3)
                        sm = fw.sb([128, 16], F32, f"sm{g}", esB3)
                        impv = fw.sb([128, 64], F32, f"impv{g}", esB3)
                        wk = fw.sb([128, 64], F32, f"wk{g}", esB3)
                        m8a = fw.sb([128, 8], F32, f"m8a{g}", esB3)
                        m8b = fw.sb([128, 8], F32, f"m8b{g}", esB3)
                        negm = fw.sb([128, 64], BF16, f"negm{g}", esB3)
                        negmT = fw.sb([64, 128], BF16, f"negmT{g}", esB3)
                        rot = [0]

                        def score(c, lhsT, lreads, extra, bias, mask):
                            r = rot[0] % 3
                            rot[0] += 1
                            sb_i = Sbank[r]
                            P = Pb[r]
                            qrhs = qT[:, :, c * 128:(c + 1) * 128]
                            S3 = PS[sb_i][:, :].rearrange("p (h q) -> p h q", h=4)
                            mm(PS[sb_i], S3, lhsT, qrhs, True, extra is None, lreads + [qT])
                            if extra is not None:
                                mm(PS[sb_i], S3, extra[0], extra[1], False, True, extra[2])
                            op("act", lambda e: e.activation(out=P[:], in_=PS[sb_i][:, :], func=AF.Exp, bias=bias[:], scale=0.125),
                               reads=[PS[sb_i], bias], writes=[P])
                            if mask is not None:
                                op("dve", lambda e: e.tensor_tensor(out=P[:].rearrange("p (h q) -> p h q", h=4), in0=P[:].rearrange("p (h q) -> p h q", h=4),
                                                                    in1=mask[0], op=ALU.mult), reads=[P, mask[1]], writes=[P])
                            return P

                        def pv(P, h, vr, vreads, cc, n, first, last):
                            mm(PS[2 + h], PS[2 + h][:, cc:cc + n], P[:, h * 128:(h + 1) * 128], vr, first, last, [P] + vreads)

                        def chunk(c, lhsT, lreads, extra, bias, mask, vrhs, vreads, col0, first, last):
                            P = score(c, lhsT, lreads, extra, bias, mask)
                            for h in range(4):
                                for (vr, cc, n) in vrhs:
                                    pv(P, h, vr, vreads, cc, n, first, last)

                        def evac(c, h, col0, br, first, final):
                            bank = PS[2 + h]
                            dn = sm[:, 3 * h:3 * h + 1]
                            rd = sm[:, 3 * h + 1:3 * h + 2] if br != 0 else sm[:, 12 + h:13 + h]
                            cf = sm[:, 3 * h + 2:3 * h + 3]
                            op("dve", lambda e: e.tensor_scalar(out=dn, in0=bank[:, col0 + 64:col0 + 65], scalar1=1e-30, scalar2=None, op0=ALU.max),
                               reads=[bank], writes=[sm])
                            op("dve", lambda e: e.reciprocal(out=rd, in_=dn), reads=[sm], writes=[sm])
                            op("dve", lambda e: e.tensor_tensor(out=cf, in0=rd, in1=gsig[:, c, 3 * h + br:3 * h + br + 1], op=ALU.mult),
                               reads=[sm, gsig], writes=[sm])
                            if first:
                                op("dve", lambda e: e.tensor_scalar(out=ya[:, h, :], in0=bank[:, col0:col0 + 64], scalar1=cf, scalar2=None, op0=ALU.mult),
                                   reads=[bank, sm], writes=[ya])
                            else:
                                dst = yat if final else ya
                                op("dve", lambda e: e.scalar_tensor_tensor(out=dst[:, h, :], in0=bank[:, col0:col0 + 64], scalar=cf, in1=ya[:, h, :],
                                                                           op0=ALU.mult, op1=ALU.add), reads=[bank, sm, ya], writes=[dst])

                        for c in range(NT_OWN):
                            Pc = []
                            for nch in range(2):
                                mk = cmask[:, nch, c * 128:(c + 1) * 128].unsqueeze(1).to_broadcast([128, 4, 128])
                                Pc.append(score(c, kcmpT[:, nch * 128:(nch + 1) * 128], [kcmpT], None, hbias if nch == 0 else c_zero, (mk, cmask)))
                            for h in range(4):
                                for nch in range(2):
                                    pv(Pc[nch], h, vca[:, nch, :], [vca], 0, 65, nch == 0, nch == 1)
                                for nch in range(2):
                                    pv(Pc[nch], h, ovl[:, nch, :], [ovl], 65, 64, nch == 0, nch == 1)
                            for h in range(4):
                                evac(c, h, 0, 0, True, False)
                                rdc = sm[:, 12 + h:13 + h]
                                if h == 0:
                                    op("dve", lambda e: e.tensor_scalar(out=impv[:], in0=PS[2][:, 65:129], scalar1=rdc, scalar2=None, op0=ALU.mult),
                                       reads=[PS[2], sm], writes=[impv])
                                else:
                                    op("dve", lambda e: e.scalar_tensor_tensor(out=impv[:], in0=PS[2 + h][:, 65:129], scalar=rdc, in1=impv[:],
                                                                               op0=ALU.mult, op1=ALU.add), reads=[PS[2 + h], sm, impv], writes=[impv])
                            op("dve", lambda e: e.tensor_tensor(out=impv[:], in0=impv[:], in1=maskadd[:, c, :], op=ALU.add), reads=[impv, maskadd], writes=[impv])
                            op("dve", lambda e: e.max(out=m8a[:], in_=impv[:]), reads=[impv], writes=[m8a])
                            op("dve", lambda e: e.match_replace(out=wk[:], in_to_replace=m8a[:], in_values=impv[:], imm_value=-3.0e38),
                               reads=[impv, m8a], writes=[wk])
                            op("dve", lambda e: e.max(out=m8b[:], in_=wk[:]), reads=[wk], writes=[m8b])
                            op("dve", lambda e: e.tensor_scalar(out=negm[:], in0=impv[:], scalar1=m8b[:, 7:8], scalar2=NEGB, op0=ALU.is_lt, op1=ALU.mult),
                               reads=[impv, m8b], writes=[negm])
                            op("pe", lambda e: e.transpose(out=psbf(7)[0:64, 0:128], in_=negm[:, :], identity=idb[:]), reads=[negm, idb], writes=[PS[7]])
                            op("act", lambda e: e.copy(out=negmT[:], in_=psbf(7)[0:64, 0:128]), reads=[PS[7]], writes=[negmT])
                            chs = list(range(NT_OWN)) + [NT_OWN + j for j in range(c + 1)]
                            for i, ch in enumerate(chs):
                                mk = None
                                if ch == NT_OWN + c:
                                    mk = (caus[:].unsqueeze(1).to_broadcast([128, 4, 128]), caus)
                                chunk(c, kkT[:, 0, ch * 128:(ch + 1) * 128], [kkT],
                                      (Esel[:, ch * 128:(ch + 1) * 128], negmT[:].unsqueeze(1).to_broadcast([64, 4, 128]), [Esel, negmT]),
                                      hbias if ch < NT_OWN else c_zero, mk, [(vv[:, ch, 0, :], 129, 65)], [vv], 129, i == 0, i == len(chs) - 1)
                            for h in range(4):
                                evac(c, h, 129, 1, False, False)
                            for j in range(5):
                                ch = NT_OWN + c - 4 + j
                                mk = None
                                if j == 0:
                                    mk = (wm0[:].unsqueeze(1).to_broadcast([128, 4, 128]), wm0)
                                elif j == 4:
                                    mk = (caus[:].unsqueeze(1).to_broadcast([128, 4, 128]), caus)
                                chunk(c, kkT[:, 1, ch * 128:(ch + 1) * 128], [kkT], None, hbias if ch < NT_OWN else c_zero, mk,
                                      [(vv[:, ch, 1, :], 194, 65)], [vv], 194, j == 0, j == 4)
                            for h in range(4):
                                evac(c, h, 194, 2, False, True)
                            if g == 0 and c == 0 and "B3dump" in dbg:
                                hbd = fw.sb([128, 512], F32, "hbd", esB3)
                                op("pool", lambda e: e.memset(hbd[:], 0.0), writes=[hbd])
                                op("act", lambda e: e.copy(out=hbd[:, 0:259], in_=PS[2][:, 0:259]), reads=[PS[2], hbd], writes=[hbd])
                                fw.dma(dbg_t("hb0", [128, 512]), hbd[:], reads=[hbd], is_output=True)
                                fw.dma(dbg_t("impv0", [128, 64]), impv[:], reads=[impv], is_output=True)
                                fw.dma(dbg_t("negm0", [128, 64], BF16), negm[:], reads=[negm], is_output=True)
                                fw.dma(dbg_t("madd", [128, NT_OWN, 64]), maskadd[:], reads=[maskadd], is_output=True)
                                fw.dma(dbg_t("yat0", [128, 4, 64], BF16), yat[:], reads=[yat], is_output=True)
                                ckpt("B3c0")
                            for j in range(2):
                                op("pe", lambda e: e.transpose(out=psbf(7)[:, 128 + j * 128:256 + j * 128],
                                                               in_=yat[:, 2 * j:2 * j + 2, :].rearrange("p h d -> p (h d)"), identity=idb[:]),
                                   reads=[yat, idb], writes=[PS[7]])
                            op("act", lambda e: e.copy(out=YT[:, 2 * g:2 * g + 2, c * 128:(c + 1) * 128],
                                                       in_=psbf(7)[:, 128:384].rearrange("p (j t) -> p j t", j=2)), reads=[PS[7]], writes=[YT])


            esBc.__exit__(None, None, None)
            ckpt("B")
            with fw.scope() as esCg:
                ee = fw.sb([128, NT_EXT, 4], F32, "ee", esCg)
                ff = fw.sb([128, NT_EXT, 4], F32, "ff", esCg)
                fl = fw.sb([128, NT_EXT, 4], F32, "fl", esCg)
                ghn = fw.sb([128, 512], F32, "ghn", esCg)
                fw.dma(ghn[:], g_hn_d[0:1, :].to_broadcast([128, 512]), writes=[ghn])
                wcs = fw.sb([128, 8, 4], F32, "wcs", esCg)
                fw.dma(wcs[:], wc_d[:, :, :], writes=[wcs])
                bcs = fw.sb([128, 8], F32, "bcs", esCg)
                fw.dma(bcs[:], bc_d[:, :], writes=[bcs])
                with fw.scope() as esg:
                    w_if = fw.sb([128, 8, 8], BF16, "w_if", esg)
                    fw.dma(w_if[:], w_if_d.rearrange("(k p) c -> p k c", p=128), writes=[w_if], q="pool")
                    bif = fw.sb([128, 8], F32, "bif", esg)
                    fw.dma(bif[:], b_if_d[0:1, :].to_broadcast([128, 8]), writes=[bif])
                    hblk = [fw.sb([128, 8, 512], BF16, f"hblkG{i}", esg) for i in range(2)]
                    ifp = fw.sb([128, NT_EXT, 8], F32, "ifp", esg)
                    l1 = fw.sb([128, NT_EXT, 4], F32, "l1", esg)
                    tmpg = fw.sb([128, NT_EXT, 4], F32, "tmpg", esg)
                    for t in range(NT_EXT):
                        hb = hblk[(t // 4) % 2]
                        if t % 4 == 0:
                            fw.dma(hb[:], hT_d[:, :, t * 128:(t + 4) * 128], reads=hT_tiles[t:t + 4], writes=[hb])
                        tl = t % 4
                        for k in range(8):
                            mm(PS[0], PS[0][:, t * 8:(t + 1) * 8], hb[:, k, tl * 128:(tl + 1) * 128], w_if[:, k, :], k == 0, k == 7, [hb, w_if])
                    op("act", lambda e: e.copy(out=ifp[:], in_=PS[0][:, 0:256].rearrange("p (t c) -> p t c", c=8)), reads=[PS[0]], writes=[ifp])
                    op("dve", lambda e: e.tensor_tensor(out=ifp[:], in0=ifp[:], in1=bif[:].unsqueeze(1).to_broadcast([128, NT_EXT, 8]), op=ALU.add),
                       reads=[ifp, bif], writes=[ifp])
                    op("act", lambda e: e.activation(out=l1[:], in_=ifp[:, :, 4:8], func=AF.Exp, scale=-1.0), reads=[ifp], writes=[l1])
                    op("act", lambda e: e.activation(out=l1[:], in_=l1[:], func=AF.Ln, bias=c_one[:]), reads=[l1, c_one], writes=[l1])
                    l1f = l1[:].rearrange("p t c -> p (t c)")
                    mm(PS[1], PS[1][:, 0:128], U_f[:], l1f, True, True, [U_f, l1])
                    mm(PS[1], PS[1][:, 128:256], ones_f[:], l1f, True, True, [ones_f, l1])
                    op("act", lambda e: e.copy(out=tmpg[:], in_=PS[1][:, 0:128].rearrange("p (t c) -> p t c", c=4)), reads=[PS[1]], writes=[tmpg])
                    op("act", lambda e: e.activation(out=ff[:], in_=tmpg[:], func=AF.Exp, scale=-1.0), reads=[tmpg], writes=[ff])
                    op("act", lambda e: e.activation(out=fl[:], in_=PS[1][:, 128:256].rearrange("p (t c) -> p t c", c=4), func=AF.Exp, scale=-1.0),
                       reads=[PS[1]], writes=[fl])
                    op("dve", lambda e: e.tensor_tensor(out=tmpg[:], in0=tmpg[:], in1=ifp[:, :, 0:4], op=ALU.add), reads=[tmpg, ifp], writes=[tmpg])
                    op("act", lambda e: e.activation(out=ee[:], in_=tmpg[:], func=AF.Exp), reads=[tmpg], writes=[ee])
                    op("dve", lambda e: e.tensor_scalar(out=ee[:, 0:NT_OWN, :], in0=ee[:, 0:NT_OWN, :], scalar1=hv[:, 0:1], scalar2=None, op0=ALU.mult),
                       reads=[ee, hv], writes=[ee])
                ckpt("Cg")
                for hp in range(2):
                    with fw.scope() as esH:
                        qTb = fw.sb([128, 2, S_OWN], BF16, f"qTb{hp}", esH)
                        kTb = fw.sb([128, 2, S_EXT], BF16, f"kTb{hp}", esH)
                        vaug = fw.sb([128, NT_EXT, 2, 129], BF16, f"vaug{hp}", esH)
                        osig = fw.sb([128, NT_OWN, 256], BF16, f"osig{hp}", esH)
                        CT = fw.sb([128, 2, 129], F32, f"CT{hp}", esH)
                        CTb = fw.sb([128, 2, 129], BF16, f"CTb{hp}", esH)
                        op("pool", lambda e: e.memset(vaug[:, :, :, 128:129], 1.0), writes=[vaug])
                        op("pool", lambda e: e.memset(CT[:], 0.0), writes=[CT])
                        op("pool", lambda e: e.memset(CTb[:], 0.0), writes=[CTb])
                        with fw.scope() as esC1:
                            wq = fw.sb([128, 8, 256], BF16, f"wq{hp}", esC1)
                            wk = fw.sb([128, 8, 256], BF16, f"wk{hp}", esC1)
                            wv = fw.sb([128, 8, 256], BF16, f"wv{hp}", esC1)
                            wo = fw.sb([128, 8, 256], BF16, f"wo{hp}", esC1)
                            fw.dma(wq[:], w_qk_d[:, hp * 256:(hp + 1) * 256].rearrange("(k p) c -> p k c", p=128), writes=[wq], q="pool")
                            fw.dma(wk[:], w_qk_d[:, 512 + hp * 256:512 + (hp + 1) * 256].rearrange("(k p) c -> p k c", p=128), writes=[wk], q="pool")
                            fw.dma(wv[:], w_vo_d[:, hp * 256:(hp + 1) * 256].rearrange("(k p) c -> p k c", p=128), writes=[wv], q="pool")
                            fw.dma(wo[:], w_vo_d[:, 512 + hp * 256:512 + (hp + 1) * 256].rearrange("(k p) c -> p k c", p=128), writes=[wo], q="pool")
                            hblk = [fw.sb([128, 8, 512], BF16, f"hblkC{hp}{i}", esC1) for i in range(2)]
                            uk = [fw.sb([128, 4 + S_EXT], BF16, f"uk{hp}{i}", esC1) for i in range(2)]
                            uq = [fw.sb([128, 4 + 2560], BF16, f"uq{hp}{i}", esC1) for i in range(2)]
                            ycv = [fw.sb([128, 512], F32, f"ycv{hp}{i}", esC1) for i in range(2)]
                            sgm = [fw.sb([128, 512], F32, f"sgm{hp}{i}", esC1) for i in range(2)]
                            for hh in range(2):
                                op("pool", lambda e: e.memset(uk[hh][:, 0:4], 0.0), writes=[uk[hh]])
                                op("pool", lambda e: e.memset(uq[hh][:, 0:4], 0.0), writes=[uq[hh]])
                            for blk in range(8):
                                hb = hblk[blk % 2]
                                fw.dma(hb[:], hT_d[:, :, blk * 512:(blk + 1) * 512], reads=hT_tiles[4 * blk:4 * blk + 4], writes=[hb])
                                for hh in range(2):
                                    for k in range(8):
                                        mm(PS[hh], PS[hh][:, :], wk[:, k, hh * 128:(hh + 1) * 128], hb[:, k, :], k == 0, k == 7, [wk, hb])
                                    op("act", lambda e: e.copy(out=uk[hh][:, 4 + blk * 512:4 + (blk + 1) * 512], in_=PS[hh][:, :]), reads=[PS[hh]], writes=[uk[hh]])
                                if blk >= 3:
                                    for hh in range(2):
                                        for k in range(8):
                                            mm(PS[2 + hh], PS[2 + hh][:, :], wq[:, k, hh * 128:(hh + 1) * 128], hb[:, k, :], k == 0, k == 7, [wq, hb])
                                        op("act", lambda e: e.copy(out=uq[hh][:, 4 + (blk - 3) * 512:4 + (blk - 2) * 512], in_=PS[2 + hh][:, :]),
                                           reads=[PS[2 + hh]], writes=[uq[hh]])
                                for tl in range(4):
                                    t = blk * 4 + tl
                                    bv = 4 + tl % 2
                                    for k in range(8):
                                        mm(PS[bv], PS[bv][:, 0:256], hb[:, k, tl * 128:(tl + 1) * 128], wv[:, k, :], k == 0, k == 7, [wv, hb])
                                    op("dve", lambda e: e.tensor_copy(out=vaug[:, t, :, 0:128], in_=PS[bv][:, 0:256].rearrange("p (h d) -> p h d", d=128)),
                                       reads=[PS[bv]], writes=[vaug])
                                    if blk >= 4:
                                        bo = 6 + tl % 2
                                        for k in range(8):
                                            mm(PS[bo], PS[bo][:, 0:256], hb[:, k, tl * 128:(tl + 1) * 128], wo[:, k, :], k == 0, k == 7, [wo, hb])
                                        op("act", lambda e: e.activation(out=osig[:, t - NT_OWN, :], in_=PS[bo][:, 0:256], func=AF.Sigmoid),
                                           reads=[PS[bo]], writes=[osig])
                            pi = 0
                            for hh in range(2):
                                H = 2 * hp + hh
                                for typ in range(2):
                                    ci = typ * 4 + H
                                    npiece = 4 if typ == 0 else 8
                                    u = uq[hh] if typ == 0 else uk[hh]
                                    for pc in range(npiece):
                                        off = (4 + 512 + pc * 512) if typ == 0 else (4 + pc * 512)
                                        y_ = ycv[pi % 2]
                                        s_ = sgm[pi % 2]
                                        pi += 1
                                        op("dve", lambda e: e.tensor_scalar(out=y_[:], in0=u[:, off - 3:off - 3 + 512], scalar1=wcs[:, ci, 0:1], scalar2=bcs[:, ci:ci + 1],
                                                                            op0=ALU.mult, op1=ALU.add), reads=[u, wcs, bcs], writes=[y_])
                                        for j in range(1, 4):
                                            op("dve", lambda e: e.scalar_tensor_tensor(out=y_[:], in0=u[:, off - 3 + j:off - 3 + j + 512], scalar=wcs[:, ci, j:j + 1], in1=y_[:],
                                                                                       op0=ALU.mult, op1=ALU.add), reads=[u, wcs, y_], writes=[y_])
                                        if typ == 0:
                                            op("act", lambda e: e.activation(out=qTb[:, hh, pc * 512:(pc + 1) * 512], in_=y_[:], func=AF.Silu), reads=[y_], writes=[qTb])
                                        else:
                                            op("act", lambda e: e.activation(out=s_[:], in_=y_[:], func=AF.Sigmoid), reads=[y_], writes=[s_])
                                            op("dve", lambda e: e.scalar_tensor_tensor(out=kTb[:, hh, pc * 512:(pc + 1) * 512], in0=y_[:], scalar=128.0 ** -0.5, in1=s_[:],
                                                                                       op0=ALU.mult, op1=ALU.mult), reads=[y_, s_], writes=[kTb])
                        ckpt("C1")
                        with fw.scope() as esC3:
                            Ve = [[fw.sb([128, 129], BF16, f"Ve{hp}{hh}{i}", esC3) for i in range(2)] for hh in range(2)]
                            Sm = [fw.sb([128, 128], BF16, f"Sm{hp}{hh}", esC3) for hh in range(2)]
                            ktok = [fw.sb([128, 128], BF16, f"ktok{hp}{hh}", esC3) for hh in range(2)]
                            hm_ = [fw.sb([128, 128], F32, f"hm{hp}{hh}", esC3) for hh in range(2)]
                            yb_ = [fw.sb([128, 128], BF16, f"yb{hp}{hh}", esC3) for hh in range(2)]
                            jk = [fw.sb([128, 128], BF16, f"jk{hp}{hh}", esC3) for hh in range(2)]
                            smc = [fw.sb([128, 8], F32, f"smc{hp}{hh}", esC3) for hh in range(2)]
                            tmpC = [fw.sb([128, 129], F32, f"tmpC{hp}{hh}", esC3) for hh in range(2)]
                            for t in range(NT_EXT):
                                for hh in range(2):
                                    H = 2 * hp + hh
                                    bS, bA, bT, bU = 4 * hh, 4 * hh + 1, 4 * hh + 2, 4 * hh + 3
                                    ve = Ve[hh][t % 2]
                                    sc = smc[hh]
                                    op("pool", lambda e: e.tensor_scalar(out=ve[:], in0=vaug[:, t, hh, :], scalar1=ee[:, t, H:H + 1], scalar2=None, op0=ALU.mult),
                                       reads=[vaug, ee], writes=[ve])
                                    if t >= NT_OWN:
                                        tq = t - NT_OWN
                                        mm(PS[bS], PS[bS][:, 0:128], kTb[:, hh, t * 128:(t + 1) * 128], qTb[:, hh, tq * 128:(tq + 1) * 128], True, True, [kTb, qTb])
                                        op("dve", lambda e: e.tensor_tensor(out=Sm[hh][:], in0=PS[bS][:, 0:128], in1=caus[:], op=ALU.mult), reads=[PS[bS], caus], writes=[Sm[hh]])
                                        mm(PS[bA], PS[bA][:, 0:129], Sm[hh][:], ve[:], True, False, [Sm[hh], ve])
                                        mm(PS[bA], PS[bA][:, 0:129], qTb[:, hh, tq * 128:(tq + 1) * 128], CTb[:, hh, :], False, True, [qTb, CTb])
                                        fcol = ff[:, t, H:H + 1]
                                        op("act", lambda e: e.activation(out=sc[:, 6:7], in_=PS[bA][:, 128:129], func=AF.Abs, scale=fcol),
                                           reads=[PS[bA], ff], writes=[sc])
                                        op("dve", lambda e: e.tensor_scalar(out=sc[:, 0:1], in0=sc[:, 6:7], scalar1=1.0, scalar2=None, op0=ALU.max),
                                           reads=[sc], writes=[sc])
                                        op("dve", lambda e: e.reciprocal(out=sc[:, 1:2], in_=sc[:, 0:1]), reads=[sc], writes=[sc])
                                        op("dve", lambda e: e.tensor_tensor(out=sc[:, 2:3], in0=sc[:, 1:2], in1=fcol, op=ALU.mult), reads=[sc, ff], writes=[sc])
                                        op("dve", lambda e: e.scalar_tensor_tensor(out=hm_[hh][:], in0=PS[bA][:, 0:128], scalar=sc[:, 2:3], in1=osig[:, tq, hh * 128:(hh + 1) * 128],
                                                                                   op0=ALU.mult, op1=ALU.mult), reads=[PS[bA], sc, osig], writes=[hm_[hh]])
                                        op("act", lambda e: e.activation(out=jk[hh][:], in_=hm_[hh][:], func=AF.Square, accum_out=sc[:, 3:4]), reads=[hm_[hh]], writes=[jk[hh], sc])
                                        op("act", lambda e: e.activation(out=sc[:, 4:5], in_=sc[:, 3:4], func=AF.Sqrt, bias=c_eps[:], scale=1.0 / 128), reads=[sc, c_eps], writes=[sc])
                                        op("dve", lambda e: e.reciprocal(out=sc[:, 5:6], in_=sc[:, 4:5]), reads=[sc], writes=[sc])
                                        op("dve", lambda e: e.scalar_tensor_tensor(out=yb_[hh][:], in0=hm_[hh][:], scalar=sc[:, 5:6], in1=ghn[:, H * 128:(H + 1) * 128],
                                                                                   op0=ALU.mult, op1=ALU.mult), reads=[hm_[hh], sc, ghn], writes=[yb_[hh]])
                                        op("pe", lambda e: e.transpose(out=psbf(bT)[:, 0:128], in_=yb_[hh][:], identity=idb[:]), reads=[yb_[hh], idb], writes=[PS[bT]])
                                        op("act", lambda e: e.copy(out=YT[:, 4 + H, tq * 128:(tq + 1) * 128], in_=psbf(bT)[:, 0:128]), reads=[PS[bT]], writes=[YT])
                                    if t < NT_EXT - 1:
                                        flcol = fl[:, t, H:H + 1]
                                        op("pe", lambda e: e.transpose(out=psbf(bT)[:, 128:256], in_=kTb[:, hh, t * 128:(t + 1) * 128], identity=idb[:]), reads=[kTb, idb], writes=[PS[bT]])
                                        op("act", lambda e: e.copy(out=ktok[hh][:], in_=psbf(bT)[:, 128:256]), reads=[PS[bT]], writes=[ktok[hh]])
                                        mm(PS[bU], PS[bU][:, 0:129], ktok[hh][:], ve[:], True, True, [ktok[hh], ve])
                                        op("pool", lambda e: e.tensor_scalar(out=tmpC[hh][:], in0=CT[:, hh, :], scalar1=flcol, scalar2=None, op0=ALU.mult), reads=[CT, fl], writes=[tmpC[hh]])
                                        op("dve", lambda e: e.scalar_tensor_tensor(out=CT[:, hh, :], in0=PS[bU][:, 0:129], scalar=flcol, in1=tmpC[hh][:], op0=ALU.mult, op1=ALU.add),
                                           reads=[PS[bU], fl, tmpC[hh]], writes=[CT])
                                        op("act", lambda e: e.copy(out=CTb[:, hh, :], in_=CT[:, hh, :]), reads=[CT], writes=[CTb])
            ckpt("C")
            if "ybT" in dbg:
                o = dbg_t("ybT", [128, 4, S_OWN], BF16)
                fw.dma(o[:, :, :], YT[:, 4:8, :], reads=[YT], is_output=True)


            with fw.scope() as esD:
                x1 = fw.sb([128, NT_OWN, D], F32, "x1", esD)
                with fw.scope() as esD1:
                    mixT = fw.sb([128, 8, S_OWN], BF16, "mixT", esD1)
                    with fw.scope() as esD1a:
                        hTo = fw.sb([128, 8, S_OWN], BF16, "hTo", esD1a)
                        for tb in range(4):
                            fw.dma(hTo[:, :, tb * 512:(tb + 1) * 512], hT_d[:, :, S_OWN + tb * 512:S_OWN + (tb + 1) * 512],
                                   reads=hT_tiles[NT_OWN + 4 * tb:NT_OWN + 4 * tb + 4], writes=[hTo])
                        wga = [fw.sb([128, 8, 128], BF16, f"wga{i}", esD1a) for i in range(2)]
                        wgb = [fw.sb([128, 8, 128], BF16, f"wgb{i}", esD1a) for i in range(2)]
                        wpa = [fw.sb([128, 4, 128], BF16, f"wpa{i}", esD1a) for i in range(2)]
                        wpb = [fw.sb([128, 4, 128], BF16, f"wpb{i}", esD1a) for i in range(2)]
                        sga = [fw.sb([128, 512], BF16, f"sga{i}", esD1a) for i in range(2)]
                        sgb = [fw.sb([128, 512], BF16, f"sgb{i}", esD1a) for i in range(2)]
                        t1 = [fw.sb([128, 512], F32, f"t1_{i}", esD1a) for i in range(2)]
                        t2 = [fw.sb([128, 512], F32, f"t2_{i}", esD1a) for i in range(2)]
                        it = 0
                        for j in range(8):
                            w_ = j % 2
                            fw.dma(wga[w_][:], w_mg_d[:, j * 128:(j + 1) * 128].rearrange("(k p) c -> p k c", p=128), writes=[wga[w_]], q="pool")
                            fw.dma(wgb[w_][:], w_mg_d[:, 1024 + j * 128:1024 + (j + 1) * 128].rearrange("(k p) c -> p k c", p=128), writes=[wgb[w_]], q="pool")
                            fw.dma(wpa[w_][:], w_pa_d[:, j * 128:(j + 1) * 128].rearrange("(k p) c -> p k c", p=128), writes=[wpa[w_]], q="pool")
                            fw.dma(wpb[w_][:], w_pb_d[:, j * 128:(j + 1) * 128].rearrange("(k p) c -> p k c", p=128), writes=[wpb[w_]], q="pool")
                            for tb in range(4):
                                r = it % 2
                                it += 1
                                b0 = 4 * r
                                ts_ = slice(tb * 512, (tb + 1) * 512)
                                for k in range(8):
                                    mm(PS[b0], PS[b0][:, :], wga[w_][:, k, :], hTo[:, k, ts_], k == 0, k == 7, [wga[w_], hTo])
                                op("act", lambda e: e.activation(out=sga[r][:], in_=PS[b0][:, :], func=AF.Sigmoid), reads=[PS[b0]], writes=[sga[r]])
                                for k in range(8):
                                    mm(PS[b0 + 1], PS[b0 + 1][:, :], wgb[w_][:, k, :], hTo[:, k, ts_], k == 0, k == 7, [wgb[w_], hTo])
                                op("act", lambda e: e.activation(out=sgb[r][:], in_=PS[b0 + 1][:, :], func=AF.Sigmoid), reads=[PS[b0 + 1]], writes=[sgb[r]])
                                for k in range(4):
                                    mm(PS[b0 + 2], PS[b0 + 2][:, :], wpa[w_][:, k, :], YT[:, k, ts_], k == 0, k == 3, [wpa[w_], YT])
                                for k in range(4):
                                    mm(PS[b0 + 3], PS[b0 + 3][:, :], wpb[w_][:, k, :], YT[:, 4 + k, ts_], k == 0, k == 3, [wpb[w_], YT])
                                op("dve", lambda e: e.tensor_tensor(out=t1[r][:], in0=PS[b0 + 2][:, :], in1=sga[r][:], op=ALU.mult), reads=[PS[b0 + 2], sga[r]], writes=[t1[r]])
                                op("dve", lambda e: e.tensor_tensor(out=t2[r][:], in0=PS[b0 + 3][:, :], in1=sgb[r][:], op=ALU.mult), reads=[PS[b0 + 3], sgb[r]], writes=[t2[r]])
                                op("pool", lambda e: e.tensor_tensor(out=mixT[:, j, ts_], in0=t1[r][:], in1=t2[r][:], op=ALU.add), reads=[t1[r], t2[r]], writes=[mixT])
                    ckpt("D1a")
                    with fw.scope() as esD1b:
                        w_out = fw.sb([128, 8, D], BF16, "w_out", esD1b)
                        fw.dma(w_out[:], w_out_d.rearrange("(k p) c -> p k c", p=128), writes=[w_out], q="pool")
                        xtl = [fw.sb([128, D], F32, f"xtl{i}", esD1b) for i in range(2)]
                        for t in range(NT_OWN):
                            x_ = xtl[t % 2]
                            fw.dma(x_[:], xe[S_OWN + t * 128:S_OWN + (t + 1) * 128, :], writes=[x_])
                            for half in range(2):
                                b = 2 * (t % 2) + half
                                for j in range(8):
                                    mm(PS[b], PS[b][:, :], mixT[:, j, t * 128:(t + 1) * 128], w_out[:, j, half * 512:(half + 1) * 512], j == 0, j == 7, [mixT, w_out])
                                op("dve", lambda e: e.tensor_tensor(out=x1[:, t, half * 512:(half + 1) * 512], in0=PS[b][:, :], in1=x_[:, half * 512:(half + 1) * 512], op=ALU.add),
                                   reads=[PS[b], x_], writes=[x1])
                ckpt("D1")
                if "x1" in dbg:
                    fw.dma(dbg_t("x1", [128, NT_OWN, D]), x1[:], reads=[x1], is_output=True)
                with fw.scope() as esM:
                    load_gain(1)
                    gateT = fw.sb([16, S_OWN], BF16, "gateT", esM)
                    E16 = fw.sb([16, 16, 128], BF16, "E16", esM)
                    op("pool", lambda e: e.memset(E16[:], 1.0), writes=[E16])
                    op("pool", lambda e: e.affine_select(out=E16[:], in_=E16[:], pattern=[[-1, 16], [0, 128]], compare_op=ALU.is_equal, fill=0.0,
                                                         base=0, channel_multiplier=1), reads=[E16], writes=[E16])
                    with fw.scope() as esR:
                        w_r = fw.sb([128, 8, 20], F32, "w_r", esR)
                        fw.dma(w_r[:], w_r_d.rearrange("(k p) c -> p k c", p=128), writes=[w_r])
                        b_r = fw.sb([128, 20], F32, "b_r", esR)
                        fw.dma(b_r[:], b_r_d[0:1, :].to_broadcast([128, 20]), writes=[b_r])
                        hnf = [fw.sb([128, D], F32, f"hnf{i}", esR) for i in range(2)]
                        hnTf = [fw.sb([128, 8, 128], F32, f"hnTf{i}", esR) for i in range(2)]
                        junkR = fw.sb([128, D], BF16, "junkR", esR)
                        rsm = [fw.sb([128, 24], F32, f"rsm{i}", esR) for i in range(2)]
                        lg = [fw.sb([128, 20], F32, f"lg{i}", esR) for i in range(2)]
                        g1h = fw.sb([128, 4], F32, "g1h", esR)
                        t16 = fw.sb([128, 4, 4], F32, "t16", esR)
                        pad8 = fw.sb([128, 8], F32, "pad8", esR)
                        op("dve", lambda e: e.memset(pad8[:], -1e30), writes=[pad8])
                        m8r = fw.sb([128, 8], F32, "m8r", esR)
                        mk1 = fw.sb([128, 4], F32, "mk1", esR)
                        mk2 = fw.sb([128, 4], F32, "mk2", esR)
                        gig = fw.sb([128, 4], F32, "gig", esR)
                        gate = fw.sb([128, 4, 4], F32, "gate", esR)
                        ssr = [fw.sb([128, 1], F32, f"ssr{i}", esR) for i in range(2)]
                        rrr = [fw.sb([128, 1], F32, f"rrr{i}", esR) for i in range(2)]
                        for t in range(NT_OWN):
                            r = t % 2
                            rs = {"ss": ssr[r], "r": rrr[r]}
                            rms_rstd({"ap": x1[:, t, :], "bufs": [x1]}, rs, D, {"ap": junkR[:], "buf": junkR})
                            op("dve", lambda e: e.scalar_tensor_tensor(out=hnf[r][:], in0=x1[:, t, :], scalar=rs["r"][:], in1=gB[:], op0=ALU.mult, op1=ALU.mult),
                               reads=[x1, rs["r"], gB], writes=[hnf[r]])
                            for k in range(8):
                                b = 0 if k < 4 else 1
                                op("pe", lambda e: e.transpose(out=PS[b][:, (k % 4) * 128:(k % 4 + 1) * 128], in_=hnf[r][:, k * 128:(k + 1) * 128], identity=idf[:]),
                                   reads=[hnf[r], idf], writes=[PS[b]])
                            for b in range(2):
                                op("act", lambda e: e.copy(out=hnTf[r][:, 4 * b:4 * b + 4, :], in_=PS[b][:, :].rearrange("p (k t) -> p k t", k=4)), reads=[PS[b]], writes=[hnTf[r]])
                                op("dve", lambda e: e.tensor_copy(out=YT[:, 4 * b:4 * b + 4, t * 128:(t + 1) * 128], in_=PS[b][:, :].rearrange("p (k t) -> p k t", k=4)),
                                   reads=[PS[b]], writes=[YT])
                            for k in range(8):
                                mm(PS[2], PS[2][:, 0:20], hnTf[r][:, k, :], w_r[:, k, :], k == 0, k == 7, [hnTf[r], w_r])
                            L = lg[r]
                            sm_ = rsm[r]
                            op("dve", lambda e: e.tensor_tensor(out=L[:], in0=PS[2][:, 0:20], in1=b_r[:], op=ALU.add), reads=[PS[2], b_r], writes=[L])
                            op("dve", lambda e: e.tensor_reduce(out=sm_[:, 0:1], in_=L[:, 0:4], axis=AX.X, op=ALU.max), reads=[L], writes=[sm_])
                            op("dve", lambda e: e.tensor_scalar(out=g1h[:], in0=L[:, 0:4], scalar1=sm_[:, 0:1], scalar2=None, op0=ALU.is_equal), reads=[L, sm_], writes=[g1h])
                            op("dve", lambda e: e.tensor_scalar(out=sm_[:, 1:2], in0=sm_[:, 0:1], scalar1=-1.0, scalar2=None, op0=ALU.mult), reads=[sm_], writes=[sm_])
                            op("act", lambda e: e.activation(out=sm_[:, 8:12], in_=L[:, 0:4], func=AF.Exp, bias=sm_[:, 1:2], accum_out=sm_[:, 2:3]), reads=[L, sm_], writes=[sm_])
                            op("dve", lambda e: e.reciprocal(out=sm_[:, 3:4], in_=sm_[:, 2:3]), reads=[sm_], writes=[sm_])
                            op("dve", lambda e: e.tensor_tensor(out=t16[:], in0=L[:, 4:20].rearrange("p (g e) -> p g e", g=4), in1=g1h[:].unsqueeze(2).to_broadcast([128, 4, 4]), op=ALU.mult),
                               reads=[L, g1h], writes=[t16])
                            op("dve", lambda e: e.tensor_reduce(out=pad8[:, 0:4], in_=t16[:].rearrange("p g e -> p e g"), axis=AX.X, op=ALU.add), reads=[t16, pad8], writes=[pad8])
                            op("dve", lambda e: e.max(out=m8r[:], in_=pad8[:]), reads=[pad8], writes=[m8r])
                            op("dve", lambda e: e.tensor_scalar(out=mk1[:], in0=pad8[:, 0:4], scalar1=m8r[:, 0:1], scalar2=None, op0=ALU.is_equal), reads=[pad8, m8r], writes=[mk1])
                            op("dve", lambda e: e.tensor_scalar(out=mk2[:], in0=pad8[:, 0:4], scalar1=m8r[:, 1:2], scalar2=None, op0=ALU.is_equal), reads=[pad8, m8r], writes=[mk2])
                            op("dve", lambda e: e.tensor_tensor(out=sm_[:, 4:5], in0=m8r[:, 0:1], in1=m8r[:, 1:2], op=ALU.subtract), reads=[m8r, sm_], writes=[sm_])
                            op("act", lambda e: e.activation(out=sm_[:, 5:6], in_=sm_[:, 4:5], func=AF.Sigmoid), reads=[sm_], writes=[sm_])
                            op("dve", lambda e: e.tensor_scalar(out=sm_[:, 6:7], in0=sm_[:, 5:6], scalar1=-1.0, scalar2=1.0, op0=ALU.mult, op1=ALU.add), reads=[sm_], writes=[sm_])
                            op("dve", lambda e: e.tensor_scalar(out=sm_[:, 12:14], in0=sm_[:, 5:7], scalar1=sm_[:, 3:4], scalar2=None, op0=ALU.mult), reads=[sm_], writes=[sm_])
                            op("dve", lambda e: e.tensor_scalar(out=gig[:], in0=mk1[:], scalar1=sm_[:, 12:13], scalar2=None, op0=ALU.mult), reads=[mk1, sm_], writes=[gig])
                            op("dve", lambda e: e.scalar_tensor_tensor(out=gig[:], in0=mk2[:], scalar=sm_[:, 13:14], in1=gig[:], op0=ALU.mult, op1=ALU.add), reads=[mk2, sm_, gig], writes=[gig])
                            op("dve", lambda e: e.tensor_tensor(out=gate[:], in0=g1h[:].unsqueeze(2).to_broadcast([128, 4, 4]), in1=gig[:].unsqueeze(1).to_broadcast([128, 4, 4]), op=ALU.mult),
                               reads=[g1h, gig], writes=[gate])
                            op("pe", lambda e: e.transpose(out=PS[3][0:16, 0:128], in_=gate[:].rearrange("p g e -> p (g e)"), identity=idf[:]), reads=[gate, idf], writes=[PS[3]])
                            op("act", lambda e: e.copy(out=gateT[:, t * 128:(t + 1) * 128], in_=PS[3][0:16, 0:128]), reads=[PS[3]], writes=[gateT])
                    ckpt("D2r")
                    if "gateT" in dbg:
                        fw.dma(dbg_t("gateT", [16, S_OWN], BF16), gateT[:], reads=[gateT], is_output=True)
                    with fw.scope() as esE:
                        w13 = [fw.sb([128, 8, 512], BF16, f"w13_{i}", esE) for i in range(2)]
                        w2e = [fw.sb([128, 2, D], BF16, f"w2e_{i}", esE) for i in range(2)]
                        sgE = [fw.sb([128, 512], F32, f"sgE{i}", esE) for i in range(2)]
                        tE = [fw.sb([128, 512], F32, f"tE{i}", esE) for i in range(2)]
                        actT = [[fw.sb([128, 512], BF16, f"actT{i}{fc}", esE) for fc in range(2)] for i in range(2)]
                        ybank = [4, 5, 7]
                        yi = 0
                        it = 0
                        for ex in range(16):
                            wb = ex % 2
                            fw.dma(w13[wb][:], w_e13_d[ex].rearrange("(k p) c -> p k c", p=128), writes=[w13[wb]], q="pool")
                            fw.dma(w2e[wb][:], w_e2_d[ex].rearrange("(k p) c -> p k c", p=128), writes=[w2e[wb]], q="pool")
                            for tb in range(4):
                                r = it % 2
                                it += 1
                                ts_ = slice(tb * 512, (tb + 1) * 512)
                                mm(PS[6], PS[6][:, :], E16[:, ex, :], gateT[:, ts_], True, True, [E16, gateT])
                                for fc in range(2):
                                    for k in range(8):
                                        mm(PS[fc], PS[fc][:, :], w13[wb][:, k, fc * 128:(fc + 1) * 128], YT[:, k, ts_], k == 0, k == 7, [w13[wb], YT])
                                    for k in range(8):
                                        mm(PS[2 + fc], PS[2 + fc][:, :], w13[wb][:, k, 256 + fc * 128:256 + (fc + 1) * 128], YT[:, k, ts_], k == 0, k == 7, [w13[wb], YT])
                                    op("act", lambda e: e.activation(out=sgE[fc][:], in_=PS[fc][:, :], func=AF.Silu), reads=[PS[fc]], writes=[sgE[fc]])
                                    op("dve", lambda e: e.tensor_tensor(out=tE[fc][:], in0=PS[2 + fc][:, :], in1=sgE[fc][:], op=ALU.mult), reads=[PS[2 + fc], sgE[fc]], writes=[tE[fc]])
                                    op("dve", lambda e: e.tensor_tensor(out=actT[r][fc][:], in0=PS[6][:, :], in1=tE[fc][:], op=ALU.mult), reads=[PS[6], tE[fc]], writes=[actT[r][fc]])
                                for tt in range(4):
                                    t = tb * 4 + tt
                                    for half in range(2):
                                        b = ybank[yi % 3]
                                        yi += 1
                                        for fc in range(2):
                                            mm(PS[b], PS[b][:, :], actT[r][fc][:, tt * 128:(tt + 1) * 128], w2e[wb][:, fc, half * 512:(half + 1) * 512], fc == 0, fc == 1, [actT[r][fc], w2e[wb]])
                                        op("dve", lambda e: e.tensor_tensor(out=x1[:, t, half * 512:(half + 1) * 512], in0=PS[b][:, :], in1=x1[:, t, half * 512:(half + 1) * 512], op=ALU.add),
                                           reads=[PS[b], x1], writes=[x1])
                ckpt("D2")
                if "x2" in dbg:
                    fw.dma(dbg_t("x2", [128, NT_OWN, D]), x1[:], reads=[x1], is_output=True)
                with fw.scope() as esP:
                    load_gain(2)
                    gB2 = fw.sb([128, D], F32, "gB2", esP)
                    fw.dma(gB2[:], gvec_d[3:4, :].to_broadcast([128, D]), writes=[gB2])
                    w_pg = fw.sb([128, 8, D], BF16, "w_pg", esP)
                    fw.dma(w_pg[:], w_pg_d.rearrange("(k p) c -> p k c", p=128), writes=[w_pg], q="pool")
                    w_pp = fw.sb([128, 2, D], BF16, "w_pp", esP)
                    fw.dma(w_pp[:], w_pp_d.rearrange("(k p) c -> p k c", p=128), writes=[w_pp], q="pool")
                    hpb = [fw.sb([128, D], BF16, f"hpb{i}", esP) for i in range(2)]
                    hpT = [fw.sb([128, 8, 128], BF16, f"hpT{i}", esP) for i in range(2)]
                    plb = [fw.sb([128, 256], BF16, f"plb{i}", esP) for i in range(2)]
                    plT = [fw.sb([128, 2, 128], BF16, f"plT{i}", esP) for i in range(2)]
                    sgP = [fw.sb([128, 512], F32, f"sgP{i}", esP) for i in range(2)]
                    tP = [fw.sb([128, 512], F32, f"tP{i}", esP) for i in range(2)]
                    outt = [fw.sb([128, D], F32, f"outt{i}", esP) for i in range(2)]
                    junkP = fw.sb([128, D], BF16, "junkP", esP)
                    ssp = [fw.sb([128, 1], F32, f"ssp{i}", esP) for i in range(4)]
                    rrp = [fw.sb([128, 1], F32, f"rrp{i}", esP) for i in range(4)]
                    for t in range(NT_OWN):
                        r = t % 2
                        fw.dma(plb[r][:], pl_d[t * 128:(t + 1) * 128, :], writes=[plb[r]], q="pool")
                        rs = {"ss": ssp[r], "r": rrp[r]}
                        rms_rstd({"ap": x1[:, t, :], "bufs": [x1]}, rs, D, {"ap": junkP[:], "buf": junkP})
                        op("dve", lambda e: e.scalar_tensor_tensor(out=hpb[r][:], in0=x1[:, t, :], scalar=rs["r"][:], in1=gB[:], op0=ALU.mult, op1=ALU.mult),
                           reads=[x1, rs["r"], gB], writes=[hpb[r]])
                        for k in range(8):
                            op("pe", lambda e: e.transpose(out=psbf(0)[:, k * 128:(k + 1) * 128], in_=hpb[r][:, k * 128:(k + 1) * 128], identity=idb[:]), reads=[hpb[r], idb], writes=[PS[0]])
                        op("act", lambda e: e.copy(out=hpT[r][:], in_=psbf(0).rearrange("p (k t) -> p k t", k=8)), reads=[PS[0]], writes=[hpT[r]])
                        for k in range(2):
                            op("pe", lambda e: e.transpose(out=psbf(1)[:, k * 128:(k + 1) * 128], in_=plb[r][:, k * 128:(k + 1) * 128], identity=idb[:]), reads=[plb[r], idb], writes=[PS[1]])
                        op("act", lambda e: e.copy(out=plT[r][:], in_=psbf(1)[:, 0:256].rearrange("p (k t) -> p k t", k=2)), reads=[PS[1]], writes=[plT[r]])
                        for half in range(2):
                            hs = slice(half * 512, (half + 1) * 512)
                            bG = 2 + half
                            bP = 4 + half
                            for k in range(8):
                                mm(PS[bG], PS[bG][:, :], hpT[r][:, k, :], w_pg[:, k, hs], k == 0, k == 7, [hpT[r], w_pg])
                            for k in range(2):
                                mm(PS[bP], PS[bP][:, :], plT[r][:, k, :], w_pp[:, k, hs], k == 0, k == 1, [plT[r], w_pp])
                            op("act", lambda e: e.activation(out=sgP[half][:], in_=PS[bG][:, :], func=AF.Sigmoid), reads=[PS[bG]], writes=[sgP[half]])
                            op("dve", lambda e: e.tensor_tensor(out=tP[half][:], in0=PS[bP][:, :], in1=sgP[half][:], op=ALU.mult), reads=[PS[bP], sgP[half]], writes=[tP[half]])
                            op("dve", lambda e: e.tensor_tensor(out=x1[:, t, hs], in0=x1[:, t, hs], in1=tP[half][:], op=ALU.add), reads=[x1, tP[half]], writes=[x1])
                        rs2 = {"ss": ssp[2 + r], "r": rrp[2 + r]}
                        rms_rstd({"ap": x1[:, t, :], "bufs": [x1]}, rs2, D, {"ap": junkP[:], "buf": junkP})
                        op("dve", lambda e: e.scalar_tensor_tensor(out=outt[r][:], in0=x1[:, t, :], scalar=rs2["r"][:], in1=gB2[:], op0=ALU.mult, op1=ALU.mult),
                           reads=[x1, rs2["r"], gB2], writes=[outt[r]])
                        fw.dma(out_d[t * 128:(t + 1) * 128, :], outt[r][:], reads=[outt[r]], is_output=True)

            if "yaT" in dbg:
                o = dbg_t("yaT", [128, 4, S_OWN], BF16)
                fw.dma(o[:, :, :], YT[:, 0:4, :], reads=[YT], is_output=True)

            if "hT" in dbg:
                o = dbg_t("hT", [128, 8, S_EXT], BF16)
                with fw.scope() as esd:
                    tmp = fw.sb([128, 8, 512], BF16, "dbg_hT", esd)
                    for i in range(8):
                        fw.dma(tmp[:], hT_d[:, :, i * 512:(i + 1) * 512], reads=hT_tiles[4 * i:4 * i + 4], writes=[tmp])
                        fw.dma(o[:, :, i * 512:(i + 1) * 512], tmp[:], reads=[tmp], is_output=True)


        body()
        fw.stopped = False
        fw.finish()
    return nc, dbg_out


_INV = (500000.0 ** (-np.arange(0, 16, 2, dtype=np.float32) / 16.0)).astype(np.float32)


def make_in_maps(inputs):
    f = lambda a: np.ascontiguousarray(np.asarray(a), dtype=np.float32)
    x = f(inputs["x"]); p = f(inputs["p"])
    positions = np.asarray(inputs["positions"]).astype(np.int32)
    w_in = f(inputs["w_in"])[0]
    offs = np.cumsum([0, 512, 128, 128, 128, 128, 128, 128, 24, 1024, 512, 512, 8, 2048])
    seg = {n: (offs[i], offs[i + 1]) for i, n in enumerate(["q", "kc", "vc", "ks", "vs", "kw", "vw", "gate", "qk", "v", "o", "if", "mg"])}
    col = lambda n: w_in[:, seg[n][0]:seg[n][1]]
    w_att = []
    for g in range(2):
        parts = [col("q")[:, g * 256:(g + 1) * 256]]
        for n in ["ks", "kw", "kc", "vc", "vs", "vw"]:
            parts.append(col(n)[:, g * 64:(g + 1) * 64])
        parts.append(col("gate")[:, g * 12:(g + 1) * 12])
        w_att.append(np.concatenate(parts, axis=1))
    w_att = np.ascontiguousarray(np.stack(w_att))
    shared = {
        "invf": np.ascontiguousarray(np.broadcast_to(_INV[None, :], (128, 8))),
        "gvec": np.ascontiguousarray(np.stack([f(inputs["g_mix"])[0], f(inputs["g_ffn"])[0], f(inputs["g_ple"])[0], f(inputs["g_final"])])),
        "w_att": w_att,
        "w_qk": np.ascontiguousarray(col("qk")),
        "w_vo": np.ascontiguousarray(np.concatenate([col("v"), col("o")], axis=1)),
        "w_if": np.ascontiguousarray(col("if")),
        "w_mg": np.ascontiguousarray(col("mg")),
        "b_if": f(inputs["b_if"]).reshape(1, 8),
        "w_c1": np.ascontiguousarray(np.stack([f(inputs["w_ck1"])[0], f(inputs["w_cv1"])[0]])),
        "w_c2": np.ascontiguousarray(np.stack([f(inputs["w_ck2"])[0], f(inputs["w_cv2"])[0]])),
        "pe_c": np.ascontiguousarray(np.stack([f(inputs["pe_ck"])[0], f(inputs["pe_cv"])[0]])),
        "wc": np.ascontiguousarray(f(inputs["w_conv"])[0].reshape(4, 8, 128).transpose(2, 1, 0)),
        "bc": np.ascontiguousarray(f(inputs["b_conv"])[0].reshape(8, 128).T),
        "g_hn": f(inputs["g_hn"]).reshape(1, 512),
        "w_pa": f(inputs["w_pa"])[0], "w_pb": f(inputs["w_pb"])[0], "w_out": f(inputs["w_out"])[0],
        "w_r": np.ascontiguousarray(np.concatenate([f(inputs["w_rg"])[0], f(inputs["w_re"])[0]], axis=1)),
        "b_r": np.ascontiguousarray(np.concatenate([f(inputs["b_rg"])[0], f(inputs["b_re"])[0]])[None, :]),
        "w_e13": f(inputs["w_e13"])[0], "w_e2": f(inputs["w_e2"])[0],
        "w_pg": f(inputs["w_pg"])[0], "w_pp": f(inputs["w_pp"])[0],
    }
    in_maps = []
    for core in range(8):
        b, half = core // 2, core % 2
        if half == 1:
            xe_ = x[b]
            pos_ = positions[b]
        else:
            xe_ = np.concatenate([np.zeros((S_OWN, D), np.float32), x[b, :S_OWN]], axis=0)
            pos_ = np.concatenate([np.zeros(S_OWN, np.int32), positions[b, :S_OWN]])
        m = dict(shared)
        m["xe"] = np.ascontiguousarray(xe_)
        m["pos"] = np.ascontiguousarray(pos_.reshape(NT_EXT, 128).T)
        m["pl"] = np.ascontiguousarray(p[0, b, half * S_OWN:(half + 1) * S_OWN])
        m["hv"] = np.full((128, 1), float(half), np.float32)
        in_maps.append(m)
    return in_maps


def kernel(**inputs):
    nc, _ = build_program()
    in_maps = make_in_maps(inputs)
    res = run_bass_kernel_spmd(nc, in_maps, core_ids=list(range(8)))
    out = np.zeros((4, S_EXT, D), np.float32)
    for core in range(8):
        b, half = core // 2, core % 2
        out[b, half * S_OWN:(half + 1) * S_OWN] = res.results[core]["out"]
    return out
```

```python
import numpy as np
import concourse.bass as bass
import concourse.mybir as mybir
from concourse.bass_utils import run_bass_kernel_spmd
from contextlib import ExitStack

F32 = mybir.dt.float32
BF16 = mybir.dt.bfloat16
I32 = mybir.dt.int32
AF = mybir.ActivationFunctionType
ALU = mybir.AluOpType
AX = mybir.AxisListType

D = 1024
S_OWN = 2048
S_EXT = 4096
NT_OWN = 16
NT_EXT = 32
EPS = 1e-6
NEGB = -30000.0
DBG = []


class Buf:
    __slots__ = ("t", "lw", "rd", "name", "excl")

    def __init__(self, t, name=""):
        self.t = t
        self.excl = False
        self.lw = None
        self.rd = {}
        self.name = name

    def __getitem__(self, k):
        return self.t[k]


class FW:
    NDMA = 24

    def __init__(self, nc, es):
        self.nc = nc
        self.es = es
        self.eng = {"pe": nc.tensor, "act": nc.scalar, "dve": nc.vector, "pool": nc.gpsimd, "sp": nc.sync}
        self.sem = {k: es.enter_context(nc.semaphore("s_" + k)) for k in self.eng}
        self.cnt = {k: 0 for k in self.eng}
        self.known = {k: {} for k in self.eng}
        self.dsem = [es.enter_context(nc.semaphore(f"s_dma{i}")) for i in range(self.NDMA)]
        self.dval = [0] * self.NDMA
        self.dnext = 0
        self.nbuf = 0
        self.out_waits = []
        self.stopped = False

    def sb(self, shape, dt, name=None, es=None):
        self.nbuf += 1
        name = f"sb{self.nbuf}_" + (name or "t")
        return Buf((es or self.es).enter_context(self.nc.sbuf_tensor(name, list(shape), dt)), name)

    def ps(self, shape, dt, name=None):
        self.nbuf += 1
        name = name or f"ps{self.nbuf}"
        b = Buf(self.es.enter_context(self.nc.psum_tensor(name, list(shape), dt)), name)
        b.excl = True
        return b

    def _wait(self, e, src, idx):
        if self.stopped:
            return
        kn = self.known[e]
        if kn.get(src, 0) >= idx:
            return
        s = self.dsem[src[1]] if isinstance(src, tuple) else self.sem[src]
        self.eng[e].wait_ge(s, idx)
        kn[src] = idx

    def _deps(self, e, reads, writes):
        for b in reads:
            if b.lw is not None:
                self._wait(e, b.lw[0], b.lw[1])
            if b.excl:
                for src, idx in b.rd.items():
                    if src != e:
                        self._wait(e, src, idx)
        for b in writes:
            if b.lw is not None and b.lw[0] != e:
                self._wait(e, b.lw[0], b.lw[1])
            for src, idx in b.rd.items():
                if src != e:
                    self._wait(e, src, idx)

    def op(self, e, fn, reads=(), writes=()):
        if self.stopped:
            return None
        self._deps(e, reads, writes)
        inst = fn(self.eng[e])
        self.cnt[e] += 1
        c = self.cnt[e]
        inst.then_inc(self.sem[e], 1)
        for b in reads:
            if b.rd.get(e, 0) < c:
                b.rd[e] = c
        for b in writes:
            b.lw = (e, c)
            b.rd = {}
        return inst

    def dma(self, out, in_, reads=(), writes=(), q="sp", is_output=False):
        if self.stopped and not is_output:
            return None
        self._deps(q, reads, writes)
        slot = self.dnext
        self.dnext = (self.dnext + 1) % self.NDMA
        key = ("d", slot)
        if self.dval[slot] > 0:
            self._wait(q, key, self.dval[slot])
        inst = self.eng[q].dma_start(out=out, in_=in_)
        self.dval[slot] += 16
        inst.then_inc(self.dsem[slot], 16)
        v = self.dval[slot]
        for b in reads:
            if b.rd.get(key, 0) < v:
                b.rd[key] = v
        for b in writes:
            b.lw = (key, v)
            b.rd = {}
        if is_output:
            self.out_waits.append((key, v))
        return inst

    def barrier(self):
        for e in self.eng:
            for src in ("pe", "act", "dve", "pool"):
                if src != e and self.cnt[src] > 0:
                    self._wait(e, src, self.cnt[src])
            for slot in range(self.NDMA):
                if self.dval[slot] > 0:
                    self._wait(e, ("d", slot), self.dval[slot])

    def scope(self):
        fw = self

        class _Scope(ExitStack):
            def __exit__(self, *a):
                fw.barrier()
                return super().__exit__(*a)
        return _Scope()

    def finish(self):
        for key, v in self.out_waits:
            self._wait("sp", key, v)
        for k in ("pe", "act", "dve", "pool"):
            if self.cnt[k] > 0:
                self._wait("sp", k, self.cnt[k])


class _StopBuild(Exception):
    pass


def build_program(dbg=()):
    nc = bass.Bass("TRN2", target_bir_lowering=False)

    def din(name, shape, dt=F32):
        return nc.dram_tensor(name, list(shape), dt, kind="ExternalInput").ap()

    xe = din("xe", [S_EXT, D])
    pos_d = din("pos", [128, NT_EXT], I32)
    pl_d = din("pl", [S_OWN, 256])
    hv_d = din("hv", [128, 1])
    invf_d = din("invf", [128, 8])
    gvec_d = din("gvec", [4, D])
    w_att_d = din("w_att", [2, D, 652])
    w_qk_d = din("w_qk", [D, 1024])
    w_vo_d = din("w_vo", [D, 1024])
    w_if_d = din("w_if", [D, 8])
    w_mg_d = din("w_mg", [D, 2048])
    b_if_d = din("b_if", [1, 8])
    w_c1_d = din("w_c1", [2, 2048, 256])
    w_c2_d = din("w_c2", [2, 256, 64])
    pe_c_d = din("pe_c", [2, 32, 64])
    wc_d = din("wc", [128, 8, 4])
    bc_d = din("bc", [128, 8])
    g_hn_d = din("g_hn", [1, 512])
    w_pa_d = din("w_pa", [512, D])
    w_pb_d = din("w_pb", [512, D])
    w_out_d = din("w_out", [D, D])
    w_r_d = din("w_r", [D, 20])
    b_r_d = din("b_r", [1, 20])
    w_e13_d = din("w_e13", [16, D, 512])
    w_e2_d = din("w_e2", [16, 256, D])
    w_pg_d = din("w_pg", [D, D])
    w_pp_d = din("w_pp", [256, D])
    out_d = nc.dram_tensor("out", [S_OWN, D], F32, kind="ExternalOutput").ap()
    hT_d = nc.dram_tensor("hT_scr", [128, 8, S_EXT], BF16, kind="Internal").ap()
    dbg_out = {}

    def dbg_t(name, shape, dt=F32):
        dbg_out[name] = nc.dram_tensor("dbg_" + name, list(shape), dt, kind="ExternalOutput").ap()
        return dbg_out[name]

    with ExitStack() as es:
        fw = FW(nc, es)
        op = fw.op
        PS = [fw.ps([128, 512], F32, f"psb{i}") for i in range(8)]

        def psbf(i):
            return PS[i][:].bitcast(BF16)

        ones_f = fw.sb([128, 128], F32, "ones_f")
        op("pool", lambda e: e.memset(ones_f[:], 1.0), writes=[ones_f])
        idf = fw.sb([128, 128], F32, "idf")
        op("pool", lambda e: e.affine_select(out=idf[:], in_=ones_f[:], pattern=[[1, 128]], compare_op=ALU.is_equal,
                                             fill=0.0, base=0, channel_multiplier=-1), reads=[ones_f], writes=[idf])
        idb = fw.sb([128, 128], BF16, "idb")
        op("dve", lambda e: e.tensor_copy(out=idb[:], in_=idf[:]), reads=[idf], writes=[idb])
        U_f = fw.sb([128, 128], F32, "U_f")
        op("pool", lambda e: e.affine_select(out=U_f[:], in_=ones_f[:], pattern=[[1, 128]], compare_op=ALU.is_ge,
                                             fill=0.0, base=0, channel_multiplier=-1), reads=[ones_f], writes=[U_f])
        caus = fw.sb([128, 128], BF16, "caus")
        op("dve", lambda e: e.tensor_copy(out=caus[:], in_=U_f[:]), reads=[U_f], writes=[caus])
        wm0_f = fw.sb([128, 128], F32, "wm0_f")
        op("pool", lambda e: e.affine_select(out=wm0_f[:], in_=ones_f[:], pattern=[[-1, 128]], compare_op=ALU.is_ge,
                                             fill=0.0, base=-1, channel_multiplier=1), reads=[ones_f], writes=[wm0_f])
        wm0 = fw.sb([128, 128], BF16, "wm0")
        op("dve", lambda e: e.tensor_copy(out=wm0[:], in_=wm0_f[:]), reads=[wm0_f], writes=[wm0])
        c_eps = fw.sb([128, 1], F32, "c_eps")
        op("pool", lambda e: e.memset(c_eps[:], EPS), writes=[c_eps])
        c_one = fw.sb([128, 1], F32, "c_one")
        op("pool", lambda e: e.memset(c_one[:], 1.0), writes=[c_one])
        c_zero = fw.sb([128, 1], F32, "c_zero")
        op("pool", lambda e: e.memset(c_zero[:], 0.0), writes=[c_zero])
        acc_junk = fw.sb([128, 2], F32, "acc_junk")
        op("act", lambda e: e.activation(out=acc_junk[:, 0:1], in_=c_one[:], func=AF.Square, accum_out=acc_junk[:, 1:2]),
           reads=[c_one], writes=[acc_junk])
        hv = fw.sb([128, 1], F32, "hv")
        fw.dma(hv[:], hv_d[:, :], writes=[hv])
        hbias = fw.sb([128, 1], F32, "hbias")
        op("dve", lambda e: e.tensor_scalar(out=hbias[:], in0=hv[:], scalar1=-1.0, scalar2=-NEGB, op0=ALU.add, op1=ALU.mult),
           reads=[hv], writes=[hbias])
        gB = fw.sb([128, D], F32, "gB")

        def load_gain(i):
            fw.dma(gB[:], gvec_d[i:i + 1, :].to_broadcast([128, D]), writes=[gB])

        cs = fw.sb([128, NT_EXT, 8], F32, "cs")
        sn = fw.sb([128, NT_EXT, 8], F32, "sn")
        with fw.scope() as es1:
            posi = fw.sb([128, NT_EXT], I32, "posi", es1)
            posf = fw.sb([128, NT_EXT], F32, "posf", es1)
            invf = fw.sb([128, 8], F32, "invf", es1)
            ang = fw.sb([128, NT_EXT, 8], F32, "ang", es1)
            kf = fw.sb([128, NT_EXT, 8], F32, "kf", es1)
            ki = fw.sb([128, NT_EXT, 8], I32, "ki", es1)
            r1 = fw.sb([128, NT_EXT, 8], F32, "r1", es1)
            r2 = fw.sb([128, NT_EXT, 8], F32, "r2", es1)
            fw.dma(posi[:], pos_d[:, :], writes=[posi])
            fw.dma(invf[:], invf_d[:, :], writes=[invf])
            op("dve", lambda e: e.tensor_copy(out=posf[:], in_=posi[:]), reads=[posi], writes=[posf])
            op("dve", lambda e: e.tensor_tensor(out=ang[:], in0=posf[:].unsqueeze(2).to_broadcast([128, NT_EXT, 8]),
                                                in1=invf[:].unsqueeze(1).to_broadcast([128, NT_EXT, 8]), op=ALU.mult),
               reads=[posf, invf], writes=[ang])
            TWO_PI = 6.283185307179586
            C1 = 6.28125
            C2 = TWO_PI - C1
            PI_LO = 3.1415925
            op("dve", lambda e: e.tensor_scalar(out=kf[:], in0=ang[:], scalar1=1.0 / TWO_PI, scalar2=None, op0=ALU.mult),
               reads=[ang], writes=[kf])
            op("dve", lambda e: e.tensor_copy(out=ki[:], in_=kf[:]), reads=[kf], writes=[ki])
            op("dve", lambda e: e.tensor_copy(out=kf[:], in_=ki[:]), reads=[ki], writes=[kf])
            op("dve", lambda e: e.scalar_tensor_tensor(out=r1[:], in0=kf[:], scalar=-C1, in1=ang[:], op0=ALU.mult, op1=ALU.add),
               reads=[kf, ang], writes=[r1])
            op("dve", lambda e: e.scalar_tensor_tensor(out=r1[:], in0=kf[:], scalar=-C2, in1=r1[:], op0=ALU.mult, op1=ALU.add),
               reads=[kf, r1], writes=[r1])
            op("dve", lambda e: e.tensor_scalar(out=r1[:], in0=r1[:], scalar1=PI_LO, scalar2=-PI_LO, op0=ALU.min, op1=ALU.max),
               reads=[r1], writes=[r1])
            op("act", lambda e: e.activation(out=sn[:], in_=r1[:], func=AF.Sin), reads=[r1], writes=[sn])
            op("dve", lambda e: e.tensor_scalar(out=r2[:], in0=r1[:], scalar1=PI_LO / 2 + 0.0, scalar2=None, op0=ALU.add),
               reads=[r1], writes=[r2])
            op("dve", lambda e: e.tensor_scalar(out=kf[:], in0=r2[:], scalar1=PI_LO, scalar2=-TWO_PI, op0=ALU.is_gt, op1=ALU.mult),
               reads=[r2], writes=[kf])
            op("dve", lambda e: e.tensor_tensor(out=r2[:], in0=r2[:], in1=kf[:], op=ALU.add), reads=[r2, kf], writes=[r2])
            op("dve", lambda e: e.tensor_scalar(out=r2[:], in0=r2[:], scalar1=PI_LO, scalar2=-PI_LO, op0=ALU.min, op1=ALU.max),
               reads=[r2], writes=[r2])
            op("act", lambda e: e.activation(out=cs[:], in_=r2[:], func=AF.Sin), reads=[r2], writes=[cs])

        def rms_rstd(src, rstd, n, junk):
            ss = rstd["ss"]
            op("act", lambda e: e.activation(out=junk["ap"], in_=src["ap"], func=AF.Square, accum_out=ss[:]),
               reads=src["bufs"], writes=[junk["buf"], ss])
            op("act", lambda e: e.activation(out=ss[:], in_=ss[:], func=AF.Sqrt, bias=c_eps[:], scale=1.0 / n),
               reads=[ss, c_eps], writes=[ss])
            op("dve", lambda e: e.reciprocal(out=rstd["r"][:], in_=ss[:]), reads=[ss], writes=[rstd["r"]])

        load_gain(0)
        hT_tiles = [Buf(None, f"hT_tile{t}") for t in range(NT_EXT)]
        with fw.scope() as esA:
            xt = [fw.sb([128, D], F32, f"xtA{i}", esA) for i in range(3)]
            xn = [fw.sb([128, D], BF16, f"xnA{i}", esA) for i in range(2)]
            junk = fw.sb([128, D], BF16, "junkA", esA)
            hst = [fw.sb([128, 8, 128], BF16, f"hstA{i}", esA) for i in range(2)]
            ssA = [fw.sb([128, 1], F32, f"ssA{i}", esA) for i in range(2)]
            rrA = [fw.sb([128, 1], F32, f"rrA{i}", esA) for i in range(2)]
            for t in range(NT_EXT):
                x_ = xt[t % 3]
                fw.dma(x_[:], xe[t * 128:(t + 1) * 128, :], writes=[x_])
                rs = {"ss": ssA[t % 2], "r": rrA[t % 2]}
                rms_rstd({"ap": x_[:], "bufs": [x_]}, rs, D, {"ap": junk[:], "buf": junk})
                n_ = xn[t % 2]
                op("dve", lambda e: e.scalar_tensor_tensor(out=n_[:], in0=x_[:], scalar=rs["r"][:], in1=gB[:], op0=ALU.mult, op1=ALU.mult),
                   reads=[x_, rs["r"], gB], writes=[n_])
                pb = t % 2
                for k in range(8):
                    op("pe", lambda e: e.transpose(out=psbf(pb)[:, k * 128:(k + 1) * 128], in_=n_[:, k * 128:(k + 1) * 128], identity=idb[:]),
                       reads=[n_, idb], writes=[PS[pb]])
                h_ = hst[t % 2]
                op("act", lambda e: e.copy(out=h_[:], in_=psbf(pb).rearrange("p (k t) -> p k t", k=8)), reads=[PS[pb]], writes=[h_])
                fw.dma(hT_d[:, :, t * 128:(t + 1) * 128], h_[:], reads=[h_], writes=[hT_tiles[t]])


        if "cs" in dbg:
            o = dbg_t("cs", [128, NT_EXT, 8])
            fw.dma(o[:, :, :], cs[:], reads=[cs], is_output=True)
            o = dbg_t("sn", [128, NT_EXT, 8])
            fw.dma(o[:, :, :], sn[:], reads=[sn], is_output=True)

        def ckpt(name):
            if ("stop_" + name) in dbg:
                fw.stopped = True

        def body():
            def mm(bank, out_ap, lhsT, rhs, start, stop, reads):
                op("pe", lambda e: e.matmul(out_ap, lhsT, rhs, start=start, stop=stop), reads=reads, writes=[bank])

            YT = fw.sb([128, 8, S_OWN], BF16, "YT")
            esBc = fw.scope()
            esBc.__enter__()
            cmask = fw.sb([128, 2, S_OWN], BF16, "cmask", esBc)
            op("pool", lambda e: e.memset(cmask[:], 1.0), writes=[cmask])
            op("pool", lambda e: e.affine_select(out=cmask[:, 0, :], in_=cmask[:, 0, :], pattern=[[1, S_OWN]], compare_op=ALU.is_ge, fill=0.0,
                                                 base=2017, channel_multiplier=-16), reads=[cmask], writes=[cmask])
            op("pool", lambda e: e.affine_select(out=cmask[:, 1, :], in_=cmask[:, 1, :], pattern=[[1, S_OWN]], compare_op=ALU.is_ge, fill=0.0,
                                                 base=-31, channel_multiplier=-16), reads=[cmask], writes=[cmask])
            ovl = fw.sb([128, 2, 64], BF16, "ovl", esBc)
            op("pool", lambda e: e.memset(ovl[:], 1.0), writes=[ovl])
            for j in range(2):
                op("pool", lambda e: e.affine_select(out=ovl[:, j, :], in_=ovl[:, j, :], pattern=[[-4, 64]], compare_op=ALU.is_ge, fill=0.0,
                                                     base=128 * j + 1, channel_multiplier=1), reads=[ovl], writes=[ovl])
                op("pool", lambda e: e.affine_select(out=ovl[:, j, :], in_=ovl[:, j, :], pattern=[[4, 64]], compare_op=ALU.is_ge, fill=0.0,
                                                     base=3 - 128 * j, channel_multiplier=-1), reads=[ovl], writes=[ovl])
            maskadd = fw.sb([128, NT_OWN, 64], F32, "maskadd", esBc)
            Mb = fw.sb([128, 64], F32, "Mb", esBc)
            hm1 = fw.sb([128, 2], F32, "hm1", esBc)
            op("dve", lambda e: e.tensor_scalar(out=hm1[:, 0:1], in0=hv[:], scalar1=-1.0, scalar2=1e30, op0=ALU.add, op1=ALU.mult),
               reads=[hv], writes=[hm1])
            op("dve", lambda e: e.tensor_scalar(out=hm1[:, 1:2], in0=hv[:], scalar1=-1.0, scalar2=-1000.0, op0=ALU.add, op1=ALU.mult),
               reads=[hv, hm1], writes=[hm1])
            op("dve", lambda e: e.memset(Mb[:], 0.0), writes=[Mb])
            op("dve", lambda e: e.tensor_copy(out=Mb[:, 0:32], in_=hm1[:, 0:1].to_broadcast([128, 32])), reads=[hm1, Mb], writes=[Mb])
            op("dve", lambda e: e.scalar_tensor_tensor(out=Mb[:, 0:1], in0=hv[:], scalar=1000.0, in1=Mb[:, 0:1], op0=ALU.mult, op1=ALU.add),
               reads=[hv, Mb], writes=[Mb])
            op("dve", lambda e: e.tensor_copy(out=Mb[:, 32:33], in_=hm1[:, 1:2]), reads=[hm1, Mb], writes=[Mb])
            for c in range(NT_OWN):
                op("pool", lambda e: e.tensor_copy(out=maskadd[:, c, :], in_=Mb[:]), reads=[Mb, maskadd], writes=[maskadd])
                for hf in range(2):
                    lo = 32 + 2 * c + hf + 1
                    if lo < 64:
                        op("pool", lambda e: e.memset(maskadd[hf * 64:(hf + 1) * 64, c, lo:64], -1e30), reads=[maskadd], writes=[maskadd])
                    for col in (32 + 2 * c + hf, 32 + 2 * c + hf - 1):
                        op("pool", lambda e: e.tensor_scalar(out=maskadd[hf * 64:(hf + 1) * 64, c, col:col + 1],
                                                             in0=maskadd[hf * 64:(hf + 1) * 64, c, col:col + 1],
                                                             scalar1=1000.0, scalar2=None, op0=ALU.add), reads=[maskadd], writes=[maskadd])

            ckpt("consts")
            for g in range(2):
                with fw.scope() as esG:
                    qT = fw.sb([128, 4, S_OWN], BF16, f"qT{g}", esG)
                    kkT = fw.sb([128, 2, S_EXT], BF16, f"kkT{g}", esG)
                    op("pool", lambda e: e.memset(qT[64:128, :, :], 0.0), writes=[qT])
                    op("pool", lambda e: e.memset(kkT[64:128, 0, :], 1.0), writes=[kkT])
                    op("pool", lambda e: e.memset(kkT[64:128, 1, :], 0.0), writes=[kkT])
                    op("pool", lambda e: e.affine_select(out=kkT[64:128, 0, :], in_=kkT[64:128, 0, :], pattern=[[1, S_EXT]], compare_op=ALU.is_ge, fill=0.0,
                                                         base=0, channel_multiplier=-64), reads=[kkT], writes=[kkT])
                    op("pool", lambda e: e.affine_select(out=kkT[64:128, 0, :], in_=kkT[64:128, 0, :], pattern=[[-1, S_EXT]], compare_op=ALU.is_ge, fill=0.0,
                                                         base=63, channel_multiplier=64), reads=[kkT], writes=[kkT])
                    vv = fw.sb([128, NT_EXT, 2, 65], BF16, f"vv{g}", esG)
                    gsig = fw.sb([128, NT_OWN, 12], F32, f"gsig{g}", esG)
                    kcmpT = fw.sb([128, 256], BF16, f"kcmpT{g}", esG)
                    op("pool", lambda e: e.memset(kcmpT[64:128, :], 0.0), writes=[kcmpT])
                    vca = fw.sb([128, 2, 65], BF16, f"vca{g}", esG)
                    op("pool", lambda e: e.memset(vv[:, :, :, 64:65], 1.0), writes=[vv])
                    op("pool", lambda e: e.memset(vca[:, :, 64:65], 1.0), writes=[vca])
                    ckpt("B0a")
                    with fw.scope() as esC:
                        ccT = fw.sb([64, 2, S_EXT], BF16, f"ccT{g}", esC)
                        with fw.scope() as esB1:
                            w_att = fw.sb([128, 8, 652], BF16, f"w_att{g}", esB1)
                            fw.dma(w_att[:], w_att_d[g].rearrange("(k p) c -> p k c", p=128), writes=[w_att], q="pool")
                            ckpt("B0b")
                            hblk = [fw.sb([128, 8, 512], BF16, f"hblkB{g}{i}", esB1) for i in range(2)]
                            rp = [fw.sb([128, 8, 64], BF16, f"rp{g}{i}", esB1) for i in range(2)]
                            rpf = [fw.sb([128, 8, 64], F32, f"rpf{g}{i}", esB1) for i in range(2)]
                            ta = fw.sb([128, 7, 8], F32, f"ropa{g}", esB1)
                            tb_ = fw.sb([128, 7, 8], F32, f"ropb{g}", esB1)
                            for t in range(NT_EXT):
                                own = t >= NT_OWN
                                tq = t - NT_OWN
                                hb = hblk[(t // 4) % 2]
                                if t % 4 == 0:
                                    fw.dma(hb[:], hT_d[:, :, t * 128:(t + 4) * 128], reads=hT_tiles[t:t + 4], writes=[hb])
                                tl = t % 4
                                a0 = 0 if own else 256
                                nb = 140 if own else 128
                                bA = 2 + t % 2
                                bB = 4 + t % 2
                                if t == 0:
                                    ckpt("B1x")
                                for k in range(8):
                                    mm(PS[bA], PS[bA][:, a0:512], hb[:, k, tl * 128:(tl + 1) * 128], w_att[:, k, a0:512], k == 0, k == 7, [hb, w_att])
                                if t == 0:
                                    ckpt("B1y")
                                for k in range(8):
                                    mm(PS[bB], PS[bB][:, 0:nb], hb[:, k, tl * 128:(tl + 1) * 128], w_att[:, k, 512:512 + nb], k == 0, k == 7, [hb, w_att])
                                if t == 0:
                                    ckpt("B1a")
                                rp_ = rp[t % 2]
                                h0 = a0 // 64
                                nh = 7 - h0
                                rf = rpf[t % 2]
                                op("act", lambda e: e.copy(out=rf[:, h0:8, :], in_=PS[bA][:, a0:512].rearrange("p (h d) -> p h d", d=64)),
                                   reads=[PS[bA]], writes=[rf])
                                op("pool", lambda e: e.tensor_copy(out=rp_[:, h0:8, :], in_=rf[:, h0:8, :]), reads=[rf], writes=[rp_])
                                if t == 0:
                                    ckpt("B1r0")
                                t1 = rf[:, h0:7, 0:8]
                                t2 = rf[:, h0:7, 8:16]
                                Cb = cs[:, t, :].unsqueeze(1).to_broadcast([128, nh, 8])
                                Sb_ = sn[:, t, :].unsqueeze(1).to_broadcast([128, nh, 8])
                                op("dve", lambda e: e.tensor_tensor(out=ta[:, 0:nh, :], in0=t1, in1=Cb, op=ALU.mult), reads=[rf, cs], writes=[ta])
                                op("dve", lambda e: e.tensor_tensor(out=tb_[:, 0:nh, :], in0=t2, in1=Sb_, op=ALU.mult), reads=[rf, sn], writes=[tb_])
                                if t == 0:
                                    ckpt("B1r1")
                                op("dve", lambda e: e.tensor_tensor(out=rp_[:, h0:7, 0:8], in0=ta[:, 0:nh, :], in1=tb_[:, 0:nh, :], op=ALU.subtract),
                                   reads=[ta, tb_, rp_], writes=[rp_])
                                op("dve", lambda e: e.tensor_tensor(out=ta[:, 0:nh, :], in0=t2, in1=Cb, op=ALU.mult), reads=[rf, cs, ta], writes=[ta])
                                op("dve", lambda e: e.tensor_tensor(out=tb_[:, 0:nh, :], in0=t1, in1=Sb_, op=ALU.mult), reads=[rf, sn, tb_], writes=[tb_])
                                op("dve", lambda e: e.tensor_tensor(out=rp_[:, h0:7, 8:16], in0=ta[:, 0:nh, :], in1=tb_[:, 0:nh, :], op=ALU.add),
                                   reads=[ta, tb_, rp_], writes=[rp_])
                                if t == 0:
                                    ckpt("B1b")
                                bT = t % 2
                                psT = psbf(bT)
                                for j, hh in enumerate(range(h0, 8)):
                                    op("pe", lambda e: e.transpose(out=psT[0:64, j * 128:(j + 1) * 128], in_=rp_[:, hh, :], identity=idb[:]),
                                       reads=[rp_, idb], writes=[PS[bT]])
                                if t == 0:
                                    ckpt("B1c")
                                if own:
                                    op("act", lambda e: e.copy(out=qT[0:64, :, tq * 128:(tq + 1) * 128], in_=psT[0:64, 0:512].rearrange("p (h t) -> p h t", h=4)),
                                       reads=[PS[bT]], writes=[qT])
                                    o1 = 512
                                else:
                                    o1 = 0
                                op("act", lambda e: e.copy(out=kkT[0:64, :, t * 128:(t + 1) * 128], in_=psT[0:64, o1:o1 + 256].rearrange("p (h t) -> p h t", h=2)),
                                   reads=[PS[bT]], writes=[kkT])
                                op("act", lambda e: e.copy(out=ccT[:, :, t * 128:(t + 1) * 128], in_=psT[0:64, o1 + 256:o1 + 512].rearrange("p (h t) -> p h t", h=2)),
                                   reads=[PS[bT]], writes=[ccT])
                                op("dve", lambda e: e.tensor_copy(out=vv[:, t, :, 0:64], in_=PS[bB][:, 0:128].rearrange("p (h d) -> p h d", d=64)),
                                   reads=[PS[bB]], writes=[vv])
                                if own:
                                    op("act", lambda e: e.activation(out=gsig[:, tq, :], in_=PS[bB][:, 128:140], func=AF.Sigmoid),
                                       reads=[PS[bB]], writes=[gsig])
                        ckpt("B1")
                        for i in range(2):
                            with fw.scope() as esB2:
                                w1 = fw.sb([64, 32, 256], BF16, f"w1_{g}{i}", esB2)
                                fw.dma(w1[:], w_c1_d[i].rearrange("(l d) h -> d l h", d=64), writes=[w1], q="pool")
                                w2 = fw.sb([128, 2, 64], BF16, f"w2_{g}{i}", esB2)
                                fw.dma(w2[:], w_c2_d[i].rearrange("(c p) d -> p c d", p=128), writes=[w2], q="pool")
                                pe_sb = fw.sb([32, 64], BF16, f"pe_{g}{i}", esB2)
                                fw.dma(pe_sb[:], pe_c_d[i], writes=[pe_sb], q="pool")
                                peT = fw.sb([64, 32], BF16, f"peT_{g}{i}", esB2)
                                op("pe", lambda e: e.transpose(out=psbf(6)[0:64, 0:32], in_=pe_sb[:, :], identity=idb[0:32, 0:32]),
                                   reads=[pe_sb, idb], writes=[PS[6]])
                                op("act", lambda e: e.copy(out=peT[:], in_=psbf(6)[0:64, 0:32]), reads=[PS[6]], writes=[peT])
                                for hc in range(2):
                                    for l in range(32):
                                        mm(PS[7], PS[7][:, hc:hc + 1], w1[:, l, hc * 128:(hc + 1) * 128], peT[:, l:l + 1], l == 0, l == 31, [w1, peT])
                                cbs = fw.sb([128, 2], F32, f"cbs_{g}{i}", esB2)
                                op("act", lambda e: e.copy(out=cbs[:], in_=PS[7][:, 0:2]), reads=[PS[7]], writes=[cbs])
                                G = fw.sb([128, 2, 256], BF16, f"G_{g}{i}", esB2)
                                op("pool", lambda e: e.memset(G[:, :, 255:256], 0.0), writes=[G])
                                u_ = fw.sb([128, 255], F32, f"u_{g}{i}", esB2)
                                u2 = fw.sb([128, 255], F32, f"u2_{g}{i}", esB2)
                                sg_ = fw.sb([128, 255], F32, f"sg_{g}{i}", esB2)
                                for hc in range(2):
                                    for l in range(32):
                                        mm(PS[hc], PS[hc][:, 0:255], w1[:, l, hc * 128:(hc + 1) * 128], ccT[:, i, l:l + 16 * 254 + 1:16], l == 0, l == 31, [w1, ccT])
                                    op("act", lambda e: e.activation(out=u_[:], in_=PS[hc][:, 0:255], func=AF.Identity, bias=cbs[:, hc:hc + 1]),
                                       reads=[PS[hc], cbs], writes=[u_])
                                    op("dve", lambda e: e.tensor_tensor(out=u2[:], in0=u_[:], in1=u_[:], op=ALU.mult), reads=[u_], writes=[u2])
                                    op("dve", lambda e: e.tensor_scalar(out=u2[:], in0=u2[:], scalar1=0.044715, scalar2=1.0, op0=ALU.mult, op1=ALU.add),
                                       reads=[u2], writes=[u2])
                                    op("dve", lambda e: e.tensor_tensor(out=u2[:], in0=u2[:], in1=u_[:], op=ALU.mult), reads=[u2, u_], writes=[u2])
                                    op("act", lambda e: e.activation(out=sg_[:], in_=u2[:], func=AF.Sigmoid, scale=1.5957691216057308),
                                       reads=[u2], writes=[sg_])
                                    op("dve", lambda e: e.tensor_tensor(out=G[:, hc, 0:255], in0=u_[:], in1=sg_[:], op=ALU.mult), reads=[u_, sg_], writes=[G])
                                if i == 0:
                                    for hc in range(2):
                                        mm(PS[6], PS[6][0:64, 0:256], w2[:, hc, :], G[:, hc, :], hc == 0, hc == 1, [w2, G])
                                    op("act", lambda e: e.copy(out=kcmpT[0:64, :], in_=PS[6][0:64, 0:256]), reads=[PS[6]], writes=[kcmpT])
                                else:
                                    for nch in range(2):
                                        for hc in range(2):
                                            mm(PS[6], PS[6][:, nch * 64:(nch + 1) * 64], G[:, hc, nch * 128:(nch + 1) * 128], w2[:, hc, :], hc == 0, hc == 1, [w2, G])
                                    op("act", lambda e: e.copy(out=vca[:, :, 0:64], in_=PS[6][:, 0:128].rearrange("p (n d) -> p n d", d=64)),
                                       reads=[PS[6]], writes=[vca])
                    if g == 0 and "B2dump" in dbg:
                        for nm, bf, shp in (("kkT", kkT, [64, 2, S_EXT]), ("qT", qT, [64, 4, S_OWN]), ("vv", vv, [128, NT_EXT, 2, 65]),
                                            ("kcmpT", kcmpT, [64, 256]), ("vca", vca, [128, 2, 65])):
                            o = dbg_t(nm, shp, BF16)
                            fw.dma(o, bf[0:shp[0]], reads=[bf], is_output=True)
                        o = dbg_t("gsig", [128, NT_OWN, 12])
                        fw.dma(o, gsig[:], reads=[gsig], is_output=True)
                    ckpt("B2")
                    with fw.scope() as esB3:
                        NP = 3
                        Pb = [fw.sb([128, 512], BF16, f"Pb{g}{i}", esB3) for i in range(NP)]
                        Sbank = [0, 1, 6]
                        tpsum = psbf(7)
                        TPB = PS[7]
                        hbS = [fw.sb([128, 4, 132], F32, f"hbS{g}{r}", esB3) for r in range(3)]
                        ya = [fw.sb([128, 4, 64], F32, f"ya{g}{i}", esB3) for i in range(2)]
                        yat = [fw.sb([128, 4, 64], BF16, f"yat{g}{i}", esB3) for i in range(2)]
                        sms = [fw.sb([128, 16], F32, f"sm{g}{i}", esB3) for i in range(3)]
                        rdc = fw.sb([128, 4], F32, f"rdc{g}", esB3)
                        impv = fw.sb([128, 64], F32, f"impv{g}", esB3)
                        wk = fw.sb([128, 64], F32, f"wk{g}", esB3)
                        m8a = fw.sb([128, 8], F32, f"m8a{g}", esB3)
                        m8b = fw.sb([128, 8], F32, f"m8b{g}", esB3)
                        negm2 = fw.sb([128, 128], BF16, f"negm{g}", esB3)
                        op("pool", lambda e: e.memset(negm2[:, 0:64], 0.0), writes=[negm2])
                        rot = [0]
                        REG = {0: (0, 129), 1: (129, 65), 2: (194, 65)}

                        def score(c, lhsT, lreads, extra, bias, mask):
                            r = rot[0] % NP
                            rot[0] += 1
                            sb_i = Sbank[r]
                            P = Pb[r]
                            qrhs = qT[:, :, c * 128:(c + 1) * 128]
                            S3 = PS[sb_i][:, :].rearrange("p (h q) -> p h q", h=4)
                            mm(PS[sb_i], S3, lhsT, qrhs, True, True, lreads + [qT])
                            op("act", lambda e: e.activation(out=P[:], in_=PS[sb_i][:, :], func=AF.Exp, bias=bias[:], scale=0.125),
                               reads=[PS[sb_i], bias], writes=[P])
                            if mask is not None:
                                op("dve", lambda e: e.tensor_tensor(out=P[:].rearrange("p (h q) -> p h q", h=4), in0=P[:].rearrange("p (h q) -> p h q", h=4),
                                                                    in1=mask[0], op=ALU.mult), reads=[P, mask[1]], writes=[P])
                            return P

                        def pv(P, h, reg, vr, vreads, cc, n, first, last):
                            op("pe", lambda e: e.matmul(PS[2 + h][:, cc:cc + n], P[:, h * 128:(h + 1) * 128], vr, start=first, stop=last),
                               reads=[P] + vreads, writes=[PS[2 + h]])

                        def evac_all(c, reg, br, first, final):
                            col0, n = REG[reg]
                            hs = hbS[reg]
                            for h in range(4):
                                op("dve", lambda e: e.tensor_copy(out=hs[:, h, 0:n], in_=PS[2 + h][:, col0:col0 + n]), reads=[PS[2 + h]], writes=[hs])
                            sm = sms[reg]
                            yac = ya[c % 2]
                            dn = sm[:, 0:4]
                            rd = sm[:, 4:8] if br != 0 else rdc[:, 0:4]
                            rdb = sm if br != 0 else rdc
                            cf = sm[:, 8:12]
                            op("dve", lambda e: e.tensor_scalar(out=dn.unsqueeze(2), in0=hs[:, :, 64:65], scalar1=1e-30, scalar2=None, op0=ALU.max),
                               reads=[hs], writes=[sm])
                            op("dve", lambda e: e.reciprocal(out=rd, in_=dn), reads=[sm], writes=[rdb])
                            op("dve", lambda e: e.tensor_tensor(out=cf.unsqueeze(2), in0=rd.unsqueeze(2),
                                                                in1=gsig[:, c, :].rearrange("p (h b) -> p h b", b=3)[:, :, br:br + 1], op=ALU.mult),
                               reads=[sm, rdb, gsig], writes=[sm])
                            cfb = cf.unsqueeze(2).to_broadcast([128, 4, 64])
                            if first:
                                op("dve", lambda e: e.tensor_tensor(out=yac[:], in0=hs[:, :, 0:64], in1=cfb, op=ALU.mult), reads=[hs, sm], writes=[yac])
                            else:
                                op("dve", lambda e: e.tensor_tensor(out=hs[:, :, 0:64], in0=hs[:, :, 0:64], in1=cfb, op=ALU.mult), reads=[hs, sm], writes=[hs])
                                dst = yat[c % 2] if final else yac
                                op("dve", lambda e: e.tensor_tensor(out=dst[:], in0=hs[:, :, 0:64], in1=yac[:], op=ALU.add), reads=[hs, yac], writes=[dst])

                        pend = [None]

                        def flush():
                            if pend[0] is not None:
                                pend[0]()
                                pend[0] = None

                        def pipe(score_fn, pv_fn):
                            P = score_fn()
                            flush()
                            pend[0] = lambda: pv_fn(P)

                        for c in range(NT_OWN):
                            Pc = []
                            for nch in range(2):
                                mk = cmask[:, nch, c * 128:(c + 1) * 128].unsqueeze(1).to_broadcast([128, 4, 128])
                                Pc.append(score(c, kcmpT[:, nch * 128:(nch + 1) * 128], [kcmpT], None, hbias if nch == 0 else c_zero, (mk, cmask)))
                            flush()
                            if c > 0:
                                cp = c - 1
                                evac_all(cp, 1, 1, False, True)
                                for j in range(2):
                                    op("pe", lambda e: e.transpose(out=tpsum[:, j * 128:(j + 1) * 128],
                                                                   in_=yat[cp % 2][:, 2 * j:2 * j + 2, :].rearrange("p h d -> p (h d)"), identity=idb[:]),
                                       reads=[yat[cp % 2], idb], writes=[TPB])
                                op("act", lambda e: e.copy(out=YT[:, 2 * g:2 * g + 2, cp * 128:(cp + 1) * 128],
                                                           in_=tpsum[:, 0:256].rearrange("p (j t) -> p j t", j=2)), reads=[TPB], writes=[YT])
                            if g == 0 and c == 1 and "B3dump" in dbg:
                                for r_ in range(3):
                                    fw.dma(dbg_t(f"hbS{r_}", [128, 4, 132]), hbS[r_][:], reads=[hbS[r_]], is_output=True)
                                fw.dma(dbg_t("yat0", [128, 4, 64], BF16), yat[0][:], reads=[yat[0]], is_output=True)
                                fw.dma(dbg_t("ya0", [128, 4, 64]), ya[0][:], reads=[ya[0]], is_output=True)
                                fw.dma(dbg_t("sm0", [128, 16]), sms[0][:], reads=[sms[0]], is_output=True)
                                fw.dma(dbg_t("sm1", [128, 16]), sms[1][:], reads=[sms[1]], is_output=True)
                                fw.dma(dbg_t("sm2", [128, 16]), sms[2][:], reads=[sms[2]], is_output=True)
                                fw.dma(dbg_t("rdc", [128, 4]), rdc[:], reads=[rdc], is_output=True)
                                ckpt("B3c0")
                            for h in range(4):
                                for nch in range(2):
                                    pv(Pc[nch], h, 0, vca[:, nch, :], [vca], 0, 65, nch == 0, nch == 1)
                                for nch in range(2):
                                    pv(Pc[nch], h, 0, ovl[:, nch, :], [ovl], 65, 64, nch == 0, nch == 1)
                            for j in range(5):
                                ch = NT_OWN + c - 4 + j
                                mk = None
                                if j == 0:
                                    mk = (wm0[:].unsqueeze(1).to_broadcast([128, 4, 128]), wm0)
                                elif j == 4:
                                    mk = (caus[:].unsqueeze(1).to_broadcast([128, 4, 128]), caus)

                                def sfn(ch=ch, mk=mk):
                                    return score(c, kkT[:, 1, ch * 128:(ch + 1) * 128], [kkT], None, hbias if ch < NT_OWN else c_zero, mk)

                                def pfn(P, ch=ch, j=j):
                                    for h in range(4):
                                        pv(P, h, 2, vv[:, ch, 1, :], [vv], 194, 65, j == 0, j == 4)
                                pipe(sfn, pfn)
                                if j == 0:
                                    evac_all(c, 0, 0, True, False)
                                    op("dve", lambda e: e.tensor_tensor(out=hbS[0][:, :, 65:129], in0=hbS[0][:, :, 65:129], in1=rdc[:, 0:4].unsqueeze(2).to_broadcast([128, 4, 64]), op=ALU.mult),
                                       reads=[hbS[0], rdc], writes=[hbS[0]])
                                    op("dve", lambda e: e.tensor_reduce(out=impv[:], in_=hbS[0][:, :, 65:129].rearrange("p h s -> p s h"), axis=AX.X, op=ALU.add),
                                       reads=[hbS[0]], writes=[impv])
                                    op("dve", lambda e: e.tensor_tensor(out=impv[:], in0=impv[:], in1=maskadd[:, c, :], op=ALU.add), reads=[impv, maskadd], writes=[impv])
                                    op("dve", lambda e: e.max(out=m8a[:], in_=impv[:]), reads=[impv], writes=[m8a])
                                    op("dve", lambda e: e.match_replace(out=wk[:], in_to_replace=m8a[:], in_values=impv[:], imm_value=-3.0e38),
                                       reads=[impv, m8a], writes=[wk])
                                    op("dve", lambda e: e.max(out=m8b[:], in_=wk[:]), reads=[wk], writes=[m8b])
                                    op("dve", lambda e: e.tensor_scalar(out=negm2[:, 64:128], in0=impv[:], scalar1=m8b[:, 7:8], scalar2=NEGB, op0=ALU.is_lt, op1=ALU.mult),
                                       reads=[impv, m8b, negm2], writes=[negm2])
                            flush()
                            evac_all(c, 2, 2, False, False)
                            op("pe", lambda e: e.transpose(out=tpsum[:, 256:384], in_=negm2[:, :], identity=idb[:]), reads=[negm2, idb], writes=[TPB])
                            op("act", lambda e: e.copy(out=qT[64:128, :, c * 128:(c + 1) * 128], in_=tpsum[64:128, 256:384].unsqueeze(1).to_broadcast([64, 4, 128])),
                               reads=[TPB], writes=[qT])
                            chs = list(range(NT_OWN)) + [NT_OWN + j for j in range(c + 1)]
                            for i, ch in enumerate(chs):
                                mk = None
                                if ch == NT_OWN + c:
                                    mk = (caus[:].unsqueeze(1).to_broadcast([128, 4, 128]), caus)

                                def sfn(ch=ch, mk=mk):
                                    return score(c, kkT[:, 0, ch * 128:(ch + 1) * 128], [kkT], None, hbias if ch < NT_OWN else c_zero, mk)

                                def pfn(P, ch=ch, i=i, n=len(chs)):
                                    for h in range(4):
                                        pv(P, h, 1, vv[:, ch, 0, :], [vv], 129, 65, i == 0, i == n - 1)
                                pipe(sfn, pfn)
                        flush()
                        cp = NT_OWN - 1
                        evac_all(cp, 1, 1, False, True)
                        for j in range(2):
                            op("pe", lambda e: e.transpose(out=tpsum[:, j * 128:(j + 1) * 128],
                                                           in_=yat[cp % 2][:, 2 * j:2 * j + 2, :].rearrange("p h d -> p (h d)"), identity=idb[:]),
                               reads=[yat[cp % 2], idb], writes=[TPB])
                        op("act", lambda e: e.copy(out=YT[:, 2 * g:2 * g + 2, cp * 128:(cp + 1) * 128],
                                                   in_=tpsum[:, 0:256].rearrange("p (j t) -> p j t", j=2)), reads=[TPB], writes=[YT])
            esBc.__exit__(None, None, None)
            ckpt("B")
            with fw.scope() as esCg:
                ee = fw.sb([128, NT_EXT, 4], F32, "ee", esCg)
                ff = fw.sb([128, NT_EXT, 4], F32, "ff", esCg)
                fl = fw.sb([128, NT_EXT, 4], F32, "fl", esCg)
                ghn = fw.sb([128, 512], F32, "ghn", esCg)
                fw.dma(ghn[:], g_hn_d[0:1, :].to_broadcast([128, 512]), writes=[ghn])
                wcs = fw.sb([128, 8, 4], F32, "wcs", esCg)
                fw.dma(wcs[:], wc_d[:, :, :], writes=[wcs])
                bcs = fw.sb([128, 8], F32, "bcs", esCg)
                fw.dma(bcs[:], bc_d[:, :], writes=[bcs])
                with fw.scope() as esg:
                    w_if = fw.sb([128, 8, 8], BF16, "w_if", esg)
                    fw.dma(w_if[:], w_if_d.rearrange("(k p) c -> p k c", p=128), writes=[w_if], q="pool")
                    bif = fw.sb([128, 8], F32, "bif", esg)
                    fw.dma(bif[:], b_if_d[0:1, :].to_broadcast([128, 8]), writes=[bif])
                    hblk = [fw.sb([128, 8, 512], BF16, f"hblkG{i}", esg) for i in range(2)]
                    ifp = fw.sb([128, NT_EXT, 8], F32, "ifp", esg)
                    l1 = fw.sb([128, NT_EXT, 4], F32, "l1", esg)
                    tmpg = fw.sb([128, NT_EXT, 4], F32, "tmpg", esg)
                    for t in range(NT_EXT):
                        hb = hblk[(t // 4) % 2]
                        if t % 4 == 0:
                            fw.dma(hb[:], hT_d[:, :, t * 128:(t + 4) * 128], reads=hT_tiles[t:t + 4], writes=[hb])
                        tl = t % 4
                        for k in range(8):
                            mm(PS[0], PS[0][:, t * 8:(t + 1) * 8], hb[:, k, tl * 128:(tl + 1) * 128], w_if[:, k, :], k == 0, k == 7, [hb, w_if])
                    op("act", lambda e: e.copy(out=ifp[:], in_=PS[0][:, 0:256].rearrange("p (t c) -> p t c", c=8)), reads=[PS[0]], writes=[ifp])
                    op("dve", lambda e: e.tensor_tensor(out=ifp[:], in0=ifp[:], in1=bif[:].unsqueeze(1).to_broadcast([128, NT_EXT, 8]), op=ALU.add),
                       reads=[ifp, bif], writes=[ifp])
                    op("act", lambda e: e.activation(out=l1[:], in_=ifp[:, :, 4:8], func=AF.Exp, scale=-1.0), reads=[ifp], writes=[l1])
                    op("act", lambda e: e.activation(out=l1[:], in_=l1[:], func=AF.Ln, bias=c_one[:]), reads=[l1, c_one], writes=[l1])
                    l1f = l1[:].rearrange("p t c -> p (t c)")
                    mm(PS[1], PS[1][:, 0:128], U_f[:], l1f, True, True, [U_f, l1])
                    mm(PS[1], PS[1][:, 128:256], ones_f[:], l1f, True, True, [ones_f, l1])
                    op("act", lambda e: e.copy(out=tmpg[:], in_=PS[1][:, 0:128].rearrange("p (t c) -> p t c", c=4)), reads=[PS[1]], writes=[tmpg])
                    op("act", lambda e: e.activation(out=ff[:], in_=tmpg[:], func=AF.Exp, scale=-1.0), reads=[tmpg], writes=[ff])
                    op("act", lambda e: e.activation(out=fl[:], in_=PS[1][:, 128:256].rearrange("p (t c) -> p t c", c=4), func=AF.Exp, scale=-1.0),
                       reads=[PS[1]], writes=[fl])
                    op("dve", lambda e: e.tensor_tensor(out=tmpg[:], in0=tmpg[:], in1=ifp[:, :, 0:4], op=ALU.add), reads=[tmpg, ifp], writes=[tmpg])
                    op("act", lambda e: e.activation(out=ee[:], in_=tmpg[:], func=AF.Exp), reads=[tmpg], writes=[ee])
                    op("dve", lambda e: e.tensor_scalar(out=ee[:, 0:NT_OWN, :], in0=ee[:, 0:NT_OWN, :], scalar1=hv[:, 0:1], scalar2=None, op0=ALU.mult),
                       reads=[ee, hv], writes=[ee])
                ckpt("Cg")
                for hp in range(2):
                    with fw.scope() as esH:
                        qTb = fw.sb([128, 2, S_OWN], BF16, f"qTb{hp}", esH)
                        kTb = fw.sb([128, 2, S_EXT], BF16, f"kTb{hp}", esH)
                        vaug = fw.sb([128, NT_EXT, 2, 129], BF16, f"vaug{hp}", esH)
                        osig = fw.sb([128, NT_OWN, 256], BF16, f"osig{hp}", esH)
                        CT = fw.sb([128, 2, 129], F32, f"CT{hp}", esH)
                        CTb = fw.sb([128, 2, 129], BF16, f"CTb{hp}", esH)
                        op("pool", lambda e: e.memset(vaug[:, :, :, 128:129], 1.0), writes=[vaug])
                        op("pool", lambda e: e.memset(CT[:], 0.0), writes=[CT])
                        op("pool", lambda e: e.memset(CTb[:], 0.0), writes=[CTb])
                        with fw.scope() as esC1:
                            wq = fw.sb([128, 8, 256], BF16, f"wq{hp}", esC1)
                            wk = fw.sb([128, 8, 256], BF16, f"wk{hp}", esC1)
                            wv = fw.sb([128, 8, 256], BF16, f"wv{hp}", esC1)
                            wo = fw.sb([128, 8, 256], BF16, f"wo{hp}", esC1)
                            fw.dma(wq[:], w_qk_d[:, hp * 256:(hp + 1) * 256].rearrange("(k p) c -> p k c", p=128), writes=[wq], q="pool")
                            fw.dma(wk[:], w_qk_d[:, 512 + hp * 256:512 + (hp + 1) * 256].rearrange("(k p) c -> p k c", p=128), writes=[wk], q="pool")
                            fw.dma(wv[:], w_vo_d[:, hp * 256:(hp + 1) * 256].rearrange("(k p) c -> p k c", p=128), writes=[wv], q="pool")
                            fw.dma(wo[:], w_vo_d[:, 512 + hp * 256:512 + (hp + 1) * 256].rearrange("(k p) c -> p k c", p=128), writes=[wo], q="pool")
                            hblk = [fw.sb([128, 8, 512], BF16, f"hblkC{hp}{i}", esC1) for i in range(2)]
                            uk = [fw.sb([128, 4 + S_EXT], BF16, f"uk{hp}{i}", esC1) for i in range(2)]
                            uq = [fw.sb([128, 4 + 2560], BF16, f"uq{hp}{i}", esC1) for i in range(2)]
                            ycv = [fw.sb([128, 512], F32, f"ycv{hp}{i}", esC1) for i in range(2)]
                            sgm = [fw.sb([128, 512], F32, f"sgm{hp}{i}", esC1) for i in range(2)]
                            for hh in range(2):
                                op("pool", lambda e: e.memset(uk[hh][:, 0:4], 0.0), writes=[uk[hh]])
                                op("pool", lambda e: e.memset(uq[hh][:, 0:4], 0.0), writes=[uq[hh]])
                            for blk in range(8):
                                hb = hblk[blk % 2]
                                fw.dma(hb[:], hT_d[:, :, blk * 512:(blk + 1) * 512], reads=hT_tiles[4 * blk:4 * blk + 4], writes=[hb])
                                for hh in range(2):
                                    for k in range(8):
                                        mm(PS[hh], PS[hh][:, :], wk[:, k, hh * 128:(hh + 1) * 128], hb[:, k, :], k == 0, k == 7, [wk, hb])
                                    op("act", lambda e: e.copy(out=uk[hh][:, 4 + blk * 512:4 + (blk + 1) * 512], in_=PS[hh][:, :]), reads=[PS[hh]], writes=[uk[hh]])
                                if blk >= 3:
                                    for hh in range(2):
                                        for k in range(8):
                                            mm(PS[2 + hh], PS[2 + hh][:, :], wq[:, k, hh * 128:(hh + 1) * 128], hb[:, k, :], k == 0, k == 7, [wq, hb])
                                        op("act", lambda e: e.copy(out=uq[hh][:, 4 + (blk - 3) * 512:4 + (blk - 2) * 512], in_=PS[2 + hh][:, :]),
                                           reads=[PS[2 + hh]], writes=[uq[hh]])
                                for tl in range(4):
                                    t = blk * 4 + tl
                                    bv = 4 + tl % 2
                                    for k in range(8):
                                        mm(PS[bv], PS[bv][:, 0:256], hb[:, k, tl * 128:(tl + 1) * 128], wv[:, k, :], k == 0, k == 7, [wv, hb])
                                    op("dve", lambda e: e.tensor_copy(out=vaug[:, t, :, 0:128], in_=PS[bv][:, 0:256].rearrange("p (h d) -> p h d", d=128)),
                                       reads=[PS[bv]], writes=[vaug])
                                    if blk >= 4:
                                        bo = 6 + tl % 2
                                        for k in range(8):
                                            mm(PS[bo], PS[bo][:, 0:256], hb[:, k, tl * 128:(tl + 1) * 128], wo[:, k, :], k == 0, k == 7, [wo, hb])
                                        op("act", lambda e: e.activation(out=osig[:, t - NT_OWN, :], in_=PS[bo][:, 0:256], func=AF.Sigmoid),
                                           reads=[PS[bo]], writes=[osig])
                            pi = 0
                            for hh in range(2):
                                H = 2 * hp + hh
                                for typ in range(2):
                                    ci = typ * 4 + H
                                    npiece = 4 if typ == 0 else 8
                                    u = uq[hh] if typ == 0 else uk[hh]
                                    for pc in range(npiece):
                                        off = (4 + 512 + pc * 512) if typ == 0 else (4 + pc * 512)
                                        y_ = ycv[pi % 2]
                                        s_ = sgm[pi % 2]
                                        pi += 1
                                        op("dve", lambda e: e.tensor_scalar(out=y_[:], in0=u[:, off - 3:off - 3 + 512], scalar1=wcs[:, ci, 0:1], scalar2=bcs[:, ci:ci + 1],
                                                                            op0=ALU.mult, op1=ALU.add), reads=[u, wcs, bcs], writes=[y_])
                                        for j in range(1, 4):
                                            op("dve", lambda e: e.scalar_tensor_tensor(out=y_[:], in0=u[:, off - 3 + j:off - 3 + j + 512], scalar=wcs[:, ci, j:j + 1], in1=y_[:],
                                                                                       op0=ALU.mult, op1=ALU.add), reads=[u, wcs, y_], writes=[y_])
                                        if typ == 0:
                                            op("act", lambda e: e.activation(out=qTb[:, hh, pc * 512:(pc + 1) * 512], in_=y_[:], func=AF.Silu), reads=[y_], writes=[qTb])
                                        else:
                                            op("act", lambda e: e.activation(out=s_[:], in_=y_[:], func=AF.Sigmoid), reads=[y_], writes=[s_])
                                            op("dve", lambda e: e.scalar_tensor_tensor(out=kTb[:, hh, pc * 512:(pc + 1) * 512], in0=y_[:], scalar=128.0 ** -0.5, in1=s_[:],
                                                                                       op0=ALU.mult, op1=ALU.mult), reads=[y_, s_], writes=[kTb])
                        ckpt("C1")
                        with fw.scope() as esC3:
                            Ve = [[fw.sb([128, 129], BF16, f"Ve{hp}{hh}{i}", esC3) for i in range(2)] for hh in range(2)]
                            Sm = [fw.sb([128, 128], BF16, f"Sm{hp}{hh}", esC3) for hh in range(2)]
                            ktok = [fw.sb([128, 128], BF16, f"ktok{hp}{hh}", esC3) for hh in range(2)]
                            hm_ = [fw.sb([128, 128], F32, f"hm{hp}{hh}", esC3) for hh in range(2)]
                            yb_ = [fw.sb([128, 128], BF16, f"yb{hp}{hh}", esC3) for hh in range(2)]
                            jk = [fw.sb([128, 128], BF16, f"jk{hp}{hh}", esC3) for hh in range(2)]
                            smc = [fw.sb([128, 8], F32, f"smc{hp}{hh}", esC3) for hh in range(2)]
                            tmpC = [fw.sb([128, 129], F32, f"tmpC{hp}{hh}", esC3) for hh in range(2)]
                            for t in range(NT_EXT):
                                for hh in range(2):
                                    H = 2 * hp + hh
                                    bS, bA, bT, bU = 4 * hh, 4 * hh + 1, 4 * hh + 2, 4 * hh + 3
                                    ve = Ve[hh][t % 2]
                                    sc = smc[hh]
                                    op("pool", lambda e: e.tensor_scalar(out=ve[:], in0=vaug[:, t, hh, :], scalar1=ee[:, t, H:H + 1], scalar2=None, op0=ALU.mult),
                                       reads=[vaug, ee], writes=[ve])
                                    if t >= NT_OWN:
                                        tq = t - NT_OWN
                                        mm(PS[bS], PS[bS][:, 0:128], kTb[:, hh, t * 128:(t + 1) * 128], qTb[:, hh, tq * 128:(tq + 1) * 128], True, True, [kTb, qTb])
                                        op("dve", lambda e: e.tensor_tensor(out=Sm[hh][:], in0=PS[bS][:, 0:128], in1=caus[:], op=ALU.mult), reads=[PS[bS], caus], writes=[Sm[hh]])
                                        mm(PS[bA], PS[bA][:, 0:129], Sm[hh][:], ve[:], True, False, [Sm[hh], ve])
                                        mm(PS[bA], PS[bA][:, 0:129], qTb[:, hh, tq * 128:(tq + 1) * 128], CTb[:, hh, :], False, True, [qTb, CTb])
                                        fcol = ff[:, t, H:H + 1]
                                        op("act", lambda e: e.activation(out=sc[:, 6:7], in_=PS[bA][:, 128:129], func=AF.Abs, scale=fcol),
                                           reads=[PS[bA], ff], writes=[sc])
                                        op("dve", lambda e: e.tensor_scalar(out=sc[:, 0:1], in0=sc[:, 6:7], scalar1=1.0, scalar2=None, op0=ALU.max),
                                           reads=[sc], writes=[sc])
                                        op("dve", lambda e: e.reciprocal(out=sc[:, 1:2], in_=sc[:, 0:1]), reads=[sc], writes=[sc])
                                        op("dve", lambda e: e.tensor_tensor(out=sc[:, 2:3], in0=sc[:, 1:2], in1=fcol, op=ALU.mult), reads=[sc, ff], writes=[sc])
                                        op("dve", lambda e: e.scalar_tensor_tensor(out=hm_[hh][:], in0=PS[bA][:, 0:128], scalar=sc[:, 2:3], in1=osig[:, tq, hh * 128:(hh + 1) * 128],
                                                                                   op0=ALU.mult, op1=ALU.mult), reads=[PS[bA], sc, osig], writes=[hm_[hh]])
                                        op("act", lambda e: e.activation(out=jk[hh][:], in_=hm_[hh][:], func=AF.Square, accum_out=sc[:, 3:4]), reads=[hm_[hh]], writes=[jk[hh], sc])
                                        op("act", lambda e: e.activation(out=sc[:, 4:5], in_=sc[:, 3:4], func=AF.Sqrt, bias=c_eps[:], scale=1.0 / 128), reads=[sc, c_eps], writes=[sc])
                                        op("dve", lambda e: e.reciprocal(out=sc[:, 5:6], in_=sc[:, 4:5]), reads=[sc], writes=[sc])
                                        op("dve", lambda e: e.scalar_tensor_tensor(out=yb_[hh][:], in0=hm_[hh][:], scalar=sc[:, 5:6], in1=ghn[:, H * 128:(H + 1) * 128],
                                                                                   op0=ALU.mult, op1=ALU.mult), reads=[hm_[hh], sc, ghn], writes=[yb_[hh]])
                                        op("pe", lambda e: e.transpose(out=psbf(bT)[:, 0:128], in_=yb_[hh][:], identity=idb[:]), reads=[yb_[hh], idb], writes=[PS[bT]])
                                        op("act", lambda e: e.copy(out=YT[:, 4 + H, tq * 128:(tq + 1) * 128], in_=psbf(bT)[:, 0:128]), reads=[PS[bT]], writes=[YT])
                                    if t < NT_EXT - 1:
                                        flcol = fl[:, t, H:H + 1]
                                        op("pe", lambda e: e.transpose(out=psbf(bT)[:, 128:256], in_=kTb[:, hh, t * 128:(t + 1) * 128], identity=idb[:]), reads=[kTb, idb], writes=[PS[bT]])
                                        op("act", lambda e: e.copy(out=ktok[hh][:], in_=psbf(bT)[:, 128:256]), reads=[PS[bT]], writes=[ktok[hh]])
                                        mm(PS[bU], PS[bU][:, 0:129], ktok[hh][:], ve[:], True, True, [ktok[hh], ve])
                                        op("pool", lambda e: e.tensor_scalar(out=tmpC[hh][:], in0=CT[:, hh, :], scalar1=flcol, scalar2=None, op0=ALU.mult), reads=[CT, fl], writes=[tmpC[hh]])
                                        op("dve", lambda e: e.scalar_tensor_tensor(out=CT[:, hh, :], in0=PS[bU][:, 0:129], scalar=flcol, in1=tmpC[hh][:], op0=ALU.mult, op1=ALU.add),
                                           reads=[PS[bU], fl, tmpC[hh]], writes=[CT])
                                        op("act", lambda e: e.copy(out=CTb[:, hh, :], in_=CT[:, hh, :]), reads=[CT], writes=[CTb])
            ckpt("C")
            if "ybT" in dbg:
                o = dbg_t("ybT", [128, 4, S_OWN], BF16)
                fw.dma(o[:, :, :], YT[:, 4:8, :], reads=[YT], is_output=True)


            with fw.scope() as esD:
                x1 = fw.sb([128, NT_OWN, D], F32, "x1", esD)
                with fw.scope() as esD1:
                    mixT = fw.sb([128, 8, S_OWN], BF16, "mixT", esD1)
                    with fw.scope() as esD1a:
                        hTo = fw.sb([128, 8, S_OWN], BF16, "hTo", esD1a)
                        for tb in range(4):
                            fw.dma(hTo[:, :, tb * 512:(tb + 1) * 512], hT_d[:, :, S_OWN + tb * 512:S_OWN + (tb + 1) * 512],
                                   reads=hT_tiles[NT_OWN + 4 * tb:NT_OWN + 4 * tb + 4], writes=[hTo])
                        wga = [fw.sb([128, 8, 128], BF16, f"wga{i}", esD1a) for i in range(2)]
                        wgb = [fw.sb([128, 8, 128], BF16, f"wgb{i}", esD1a) for i in range(2)]
                        wpa = [fw.sb([128, 4, 128], BF16, f"wpa{i}", esD1a) for i in range(2)]
                        wpb = [fw.sb([128, 4, 128], BF16, f"wpb{i}", esD1a) for i in range(2)]
                        sga = [fw.sb([128, 512], BF16, f"sga{i}", esD1a) for i in range(2)]
                        sgb = [fw.sb([128, 512], BF16, f"sgb{i}", esD1a) for i in range(2)]
                        t1 = [fw.sb([128, 512], F32, f"t1_{i}", esD1a) for i in range(2)]
                        t2 = [fw.sb([128, 512], F32, f"t2_{i}", esD1a) for i in range(2)]
                        it = 0
                        for j in range(8):
                            w_ = j % 2
                            fw.dma(wga[w_][:], w_mg_d[:, j * 128:(j + 1) * 128].rearrange("(k p) c -> p k c", p=128), writes=[wga[w_]], q="pool")
                            fw.dma(wgb[w_][:], w_mg_d[:, 1024 + j * 128:1024 + (j + 1) * 128].rearrange("(k p) c -> p k c", p=128), writes=[wgb[w_]], q="pool")
                            fw.dma(wpa[w_][:], w_pa_d[:, j * 128:(j + 1) * 128].rearrange("(k p) c -> p k c", p=128), writes=[wpa[w_]], q="pool")
                            fw.dma(wpb[w_][:], w_pb_d[:, j * 128:(j + 1) * 128].rearrange("(k p) c -> p k c", p=128), writes=[wpb[w_]], q="pool")
                            for tb in range(4):
                                r = it % 2
                                it += 1
                                b0 = 4 * r
                                ts_ = slice(tb * 512, (tb + 1) * 512)
                                for k in range(8):
                                    mm(PS[b0], PS[b0][:, :], wga[w_][:, k, :], hTo[:, k, ts_], k == 0, k == 7, [wga[w_], hTo])
                                op("act", lambda e: e.activation(out=sga[r][:], in_=PS[b0][:, :], func=AF.Sigmoid), reads=[PS[b0]], writes=[sga[r]])
                                for k in range(8):
                                    mm(PS[b0 + 1], PS[b0 + 1][:, :], wgb[w_][:, k, :], hTo[:, k, ts_], k == 0, k == 7, [wgb[w_], hTo])
                                op("act", lambda e: e.activation(out=sgb[r][:], in_=PS[b0 + 1][:, :], func=AF.Sigmoid), reads=[PS[b0 + 1]], writes=[sgb[r]])
                                for k in range(4):
                                    mm(PS[b0 + 2], PS[b0 + 2][:, :], wpa[w_][:, k, :], YT[:, k, ts_], k == 0, k == 3, [wpa[w_], YT])
                                for k in range(4):
                                    mm(PS[b0 + 3], PS[b0 + 3][:, :], wpb[w_][:, k, :], YT[:, 4 + k, ts_], k == 0, k == 3, [wpb[w_], YT])
                                op("dve", lambda e: e.tensor_tensor(out=t1[r][:], in0=PS[b0 + 2][:, :], in1=sga[r][:], op=ALU.mult), reads=[PS[b0 + 2], sga[r]], writes=[t1[r]])
                                op("dve", lambda e: e.tensor_tensor(out=t2[r][:], in0=PS[b0 + 3][:, :], in1=sgb[r][:], op=ALU.mult), reads=[PS[b0 + 3], sgb[r]], writes=[t2[r]])
                                op("pool", lambda e: e.tensor_tensor(out=mixT[:, j, ts_], in0=t1[r][:], in1=t2[r][:], op=ALU.add), reads=[t1[r], t2[r]], writes=[mixT])
                    ckpt("D1a")
                    with fw.scope() as esD1b:
                        w_out = fw.sb([128, 8, D], BF16, "w_out", esD1b)
                        fw.dma(w_out[:], w_out_d.rearrange("(k p) c -> p k c", p=128), writes=[w_out], q="pool")
                        xtl = [fw.sb([128, D], F32, f"xtl{i}", esD1b) for i in range(2)]
                        for t in range(NT_OWN):
                            x_ = xtl[t % 2]
                            fw.dma(x_[:], xe[S_OWN + t * 128:S_OWN + (t + 1) * 128, :], writes=[x_])
                            for half in range(2):
                                b = 2 * (t % 2) + half
                                for j in range(8):
                                    mm(PS[b], PS[b][:, :], mixT[:, j, t * 128:(t + 1) * 128], w_out[:, j, half * 512:(half + 1) * 512], j == 0, j == 7, [mixT, w_out])
                                op("dve", lambda e: e.tensor_tensor(out=x1[:, t, half * 512:(half + 1) * 512], in0=PS[b][:, :], in1=x_[:, half * 512:(half + 1) * 512], op=ALU.add),
                                   reads=[PS[b], x_], writes=[x1])
                ckpt("D1")
                if "x1" in dbg:
                    fw.dma(dbg_t("x1", [128, NT_OWN, D]), x1[:], reads=[x1], is_output=True)
                with fw.scope() as esM:
                    load_gain(1)
                    gateT = fw.sb([16, S_OWN], BF16, "gateT", esM)
                    E16 = fw.sb([16, 16, 128], BF16, "E16", esM)
                    op("pool", lambda e: e.memset(E16[:], 1.0), writes=[E16])
                    op("pool", lambda e: e.affine_select(out=E16[:], in_=E16[:], pattern=[[-1, 16], [0, 128]], compare_op=ALU.is_equal, fill=0.0,
                                                         base=0, channel_multiplier=1), reads=[E16], writes=[E16])
                    with fw.scope() as esR:
                        w_r = fw.sb([128, 8, 20], F32, "w_r", esR)
                        fw.dma(w_r[:], w_r_d.rearrange("(k p) c -> p k c", p=128), writes=[w_r])
                        b_r = fw.sb([128, 20], F32, "b_r", esR)
                        fw.dma(b_r[:], b_r_d[0:1, :].to_broadcast([128, 20]), writes=[b_r])
                        hnf = [fw.sb([128, D], F32, f"hnf{i}", esR) for i in range(2)]
                        hnTf = [fw.sb([128, 8, 128], F32, f"hnTf{i}", esR) for i in range(2)]
                        junkR = fw.sb([128, D], BF16, "junkR", esR)
                        rsm = [fw.sb([128, 24], F32, f"rsm{i}", esR) for i in range(2)]
                        lg = [fw.sb([128, 20], F32, f"lg{i}", esR) for i in range(2)]
                        g1h = fw.sb([128, 4], F32, "g1h", esR)
                        t16 = fw.sb([128, 4, 4], F32, "t16", esR)
                        pad8 = fw.sb([128, 8], F32, "pad8", esR)
                        op("dve", lambda e: e.memset(pad8[:], -1e30), writes=[pad8])
                        m8r = fw.sb([128, 8], F32, "m8r", esR)
                        mk1 = fw.sb([128, 4], F32, "mk1", esR)
                        mk2 = fw.sb([128, 4], F32, "mk2", esR)
                        gig = fw.sb([128, 4], F32, "gig", esR)
                        gate = fw.sb([128, 4, 4], F32, "gate", esR)
                        ssr = [fw.sb([128, 1], F32, f"ssr{i}", esR) for i in range(2)]
                        rrr = [fw.sb([128, 1], F32, f"rrr{i}", esR) for i in range(2)]
                        for t in range(NT_OWN):
                            r = t % 2
                            rs = {"ss": ssr[r], "r": rrr[r]}
                            rms_rstd({"ap": x1[:, t, :], "bufs": [x1]}, rs, D, {"ap": junkR[:], "buf": junkR})
                            op("dve", lambda e: e.scalar_tensor_tensor(out=hnf[r][:], in0=x1[:, t, :], scalar=rs["r"][:], in1=gB[:], op0=ALU.mult, op1=ALU.mult),
                               reads=[x1, rs["r"], gB], writes=[hnf[r]])
                            for k in range(8):
                                b = 0 if k < 4 else 1
                                op("pe", lambda e: e.transpose(out=PS[b][:, (k % 4) * 128:(k % 4 + 1) * 128], in_=hnf[r][:, k * 128:(k + 1) * 128], identity=idf[:]),
                                   reads=[hnf[r], idf], writes=[PS[b]])
                            for b in range(2):
                                op("act", lambda e: e.copy(out=hnTf[r][:, 4 * b:4 * b + 4, :], in_=PS[b][:, :].rearrange("p (k t) -> p k t", k=4)), reads=[PS[b]], writes=[hnTf[r]])
                                op("dve", lambda e: e.tensor_copy(out=YT[:, 4 * b:4 * b + 4, t * 128:(t + 1) * 128], in_=PS[b][:, :].rearrange("p (k t) -> p k t", k=4)),
                                   reads=[PS[b]], writes=[YT])
                            for k in range(8):
                                mm(PS[2], PS[2][:, 0:20], hnTf[r][:, k, :], w_r[:, k, :], k == 0, k == 7, [hnTf[r], w_r])
                            L = lg[r]
                            sm_ = rsm[r]
                            op("dve", lambda e: e.tensor_tensor(out=L[:], in0=PS[2][:, 0:20], in1=b_r[:], op=ALU.add), reads=[PS[2], b_r], writes=[L])
                            op("dve", lambda e: e.tensor_reduce(out=sm_[:, 0:1], in_=L[:, 0:4], axis=AX.X, op=ALU.max), reads=[L], writes=[sm_])
                            op("dve", lambda e: e.tensor_scalar(out=g1h[:], in0=L[:, 0:4], scalar1=sm_[:, 0:1], scalar2=None, op0=ALU.is_equal), reads=[L, sm_], writes=[g1h])
                            op("dve", lambda e: e.tensor_scalar(out=sm_[:, 1:2], in0=sm_[:, 0:1], scalar1=-1.0, scalar2=None, op0=ALU.mult), reads=[sm_], writes=[sm_])
                            op("act", lambda e: e.activation(out=sm_[:, 8:12], in_=L[:, 0:4], func=AF.Exp, bias=sm_[:, 1:2], accum_out=sm_[:, 2:3]), reads=[L, sm_], writes=[sm_])
                            op("dve", lambda e: e.reciprocal(out=sm_[:, 3:4], in_=sm_[:, 2:3]), reads=[sm_], writes=[sm_])
                            op("dve", lambda e: e.tensor_tensor(out=t16[:], in0=L[:, 4:20].rearrange("p (g e) -> p g e", g=4), in1=g1h[:].unsqueeze(2).to_broadcast([128, 4, 4]), op=ALU.mult),
                               reads=[L, g1h], writes=[t16])
                            op("dve", lambda e: e.tensor_reduce(out=pad8[:, 0:4], in_=t16[:].rearrange("p g e -> p e g"), axis=AX.X, op=ALU.add), reads=[t16, pad8], writes=[pad8])
                            op("dve", lambda e: e.max(out=m8r[:], in_=pad8[:]), reads=[pad8], writes=[m8r])
                            op("dve", lambda e: e.tensor_scalar(out=mk1[:], in0=pad8[:, 0:4], scalar1=m8r[:, 0:1], scalar2=None, op0=ALU.is_equal), reads=[pad8, m8r], writes=[mk1])
                            op("dve", lambda e: e.tensor_scalar(out=mk2[:], in0=pad8[:, 0:4], scalar1=m8r[:, 1:2], scalar2=None, op0=ALU.is_equal), reads=[pad8, m8r], writes=[mk2])
                            op("dve", lambda e: e.tensor_tensor(out=sm_[:, 4:5], in0=m8r[:, 0:1], in1=m8r[:, 1:2], op=ALU.subtract), reads=[m8r, sm_], writes=[sm_])
                            op("act", lambda e: e.activation(out=sm_[:, 5:6], in_=sm_[:, 4:5], func=AF.Sigmoid), reads=[sm_], writes=[sm_])
                            op("dve", lambda e: e.tensor_scalar(out=sm_[:, 6:7], in0=sm_[:, 5:6], scalar1=-1.0, scalar2=1.0, op0=ALU.mult, op1=ALU.add), reads=[sm_], writes=[sm_])
                            op("dve", lambda e: e.tensor_scalar(out=sm_[:, 12:14], in0=sm_[:, 5:7], scalar1=sm_[:, 3:4], scalar2=None, op0=ALU.mult), reads=[sm_], writes=[sm_])
                            op("dve", lambda e: e.tensor_scalar(out=gig[:], in0=mk1[:], scalar1=sm_[:, 12:13], scalar2=None, op0=ALU.mult), reads=[mk1, sm_], writes=[gig])
                            op("dve", lambda e: e.scalar_tensor_tensor(out=gig[:], in0=mk2[:], scalar=sm_[:, 13:14], in1=gig[:], op0=ALU.mult, op1=ALU.add), reads=[mk2, sm_, gig], writes=[gig])
                            op("dve", lambda e: e.tensor_tensor(out=gate[:], in0=g1h[:].unsqueeze(2).to_broadcast([128, 4, 4]), in1=gig[:].unsqueeze(1).to_broadcast([128, 4, 4]), op=ALU.mult),
                               reads=[g1h, gig], writes=[gate])
                            op("pe", lambda e: e.transpose(out=PS[3][0:16, 0:128], in_=gate[:].rearrange("p g e -> p (g e)"), identity=idf[:]), reads=[gate, idf], writes=[PS[3]])
                            op("act", lambda e: e.copy(out=gateT[:, t * 128:(t + 1) * 128], in_=PS[3][0:16, 0:128]), reads=[PS[3]], writes=[gateT])
                    ckpt("D2r")
                    if "gateT" in dbg:
                        fw.dma(dbg_t("gateT", [16, S_OWN], BF16), gateT[:], reads=[gateT], is_output=True)
                    with fw.scope() as esE:
                        w13 = [fw.sb([128, 8, 512], BF16, f"w13_{i}", esE) for i in range(2)]
                        w2e = [fw.sb([128, 2, D], BF16, f"w2e_{i}", esE) for i in range(2)]
                        sgE = [fw.sb([128, 512], F32, f"sgE{i}", esE) for i in range(2)]
                        tE = [fw.sb([128, 512], F32, f"tE{i}", esE) for i in range(2)]
                        actT = [[fw.sb([128, 512], BF16, f"actT{i}{fc}", esE) for fc in range(2)] for i in range(2)]
                        ybank = [4, 5, 7]
                        yi = 0
                        it = 0
                        for ex in range(16):
                            wb = ex % 2
                            fw.dma(w13[wb][:], w_e13_d[ex].rearrange("(k p) c -> p k c", p=128), writes=[w13[wb]], q="pool")
                            fw.dma(w2e[wb][:], w_e2_d[ex].rearrange("(k p) c -> p k c", p=128), writes=[w2e[wb]], q="pool")
                            for tb in range(4):
                                r = it % 2
                                it += 1
                                ts_ = slice(tb * 512, (tb + 1) * 512)
                                mm(PS[6], PS[6][:, :], E16[:, ex, :], gateT[:, ts_], True, True, [E16, gateT])
                                for fc in range(2):
                                    for k in range(8):
                                        mm(PS[fc], PS[fc][:, :], w13[wb][:, k, fc * 128:(fc + 1) * 128], YT[:, k, ts_], k == 0, k == 7, [w13[wb], YT])
                                    for k in range(8):
                                        mm(PS[2 + fc], PS[2 + fc][:, :], w13[wb][:, k, 256 + fc * 128:256 + (fc + 1) * 128], YT[:, k, ts_], k == 0, k == 7, [w13[wb], YT])
                                    op("act", lambda e: e.activation(out=sgE[fc][:], in_=PS[fc][:, :], func=AF.Silu), reads=[PS[fc]], writes=[sgE[fc]])
                                    op("dve", lambda e: e.tensor_tensor(out=tE[fc][:], in0=PS[2 + fc][:, :], in1=sgE[fc][:], op=ALU.mult), reads=[PS[2 + fc], sgE[fc]], writes=[tE[fc]])
                                    op("dve", lambda e: e.tensor_tensor(out=actT[r][fc][:], in0=PS[6][:, :], in1=tE[fc][:], op=ALU.mult), reads=[PS[6], tE[fc]], writes=[actT[r][fc]])
                                for tt in range(4):
                                    t = tb * 4 + tt
                                    for half in range(2):
                                        b = ybank[yi % 3]
                                        yi += 1
                                        for fc in range(2):
                                            mm(PS[b], PS[b][:, :], actT[r][fc][:, tt * 128:(tt + 1) * 128], w2e[wb][:, fc, half * 512:(half + 1) * 512], fc == 0, fc == 1, [actT[r][fc], w2e[wb]])
                                        op("dve", lambda e: e.tensor_tensor(out=x1[:, t, half * 512:(half + 1) * 512], in0=PS[b][:, :], in1=x1[:, t, half * 512:(half + 1) * 512], op=ALU.add),
                                           reads=[PS[b], x1], writes=[x1])
                ckpt("D2")
                if "x2" in dbg:
                    fw.dma(dbg_t("x2", [128, NT_OWN, D]), x1[:], reads=[x1], is_output=True)
                with fw.scope() as esP:
                    load_gain(2)
                    gB2 = fw.sb([128, D], F32, "gB2", esP)
                    fw.dma(gB2[:], gvec_d[3:4, :].to_broadcast([128, D]), writes=[gB2])
                    w_pg = fw.sb([128, 8, D], BF16, "w_pg", esP)
                    fw.dma(w_pg[:], w_pg_d.rearrange("(k p) c -> p k c", p=128), writes=[w_pg], q="pool")
                    w_pp = fw.sb([128, 2, D], BF16, "w_pp", esP)
                    fw.dma(w_pp[:], w_pp_d.rearrange("(k p) c -> p k c", p=128), writes=[w_pp], q="pool")
                    hpb = [fw.sb([128, D], BF16, f"hpb{i}", esP) for i in range(2)]
                    hpT = [fw.sb([128, 8, 128], BF16, f"hpT{i}", esP) for i in range(2)]
                    plb = [fw.sb([128, 256], BF16, f"plb{i}", esP) for i in range(2)]
                    plT = [fw.sb([128, 2, 128], BF16, f"plT{i}", esP) for i in range(2)]
                    sgP = [fw.sb([128, 512], F32, f"sgP{i}", esP) for i in range(2)]
                    tP = [fw.sb([128, 512], F32, f"tP{i}", esP) for i in range(2)]
                    outt = [fw.sb([128, D], F32, f"outt{i}", esP) for i in range(2)]
                    junkP = fw.sb([128, D], BF16, "junkP", esP)
                    ssp = [fw.sb([128, 1], F32, f"ssp{i}", esP) for i in range(4)]
                    rrp = [fw.sb([128, 1], F32, f"rrp{i}", esP) for i in range(4)]
                    for t in range(NT_OWN):
                        r = t % 2
                        fw.dma(plb[r][:], pl_d[t * 128:(t + 1) * 128, :], writes=[plb[r]], q="pool")
                        rs = {"ss": ssp[r], "r": rrp[r]}
                        rms_rstd({"ap": x1[:, t, :], "bufs": [x1]}, rs, D, {"ap": junkP[:], "buf": junkP})
                        op("dve", lambda e: e.scalar_tensor_tensor(out=hpb[r][:], in0=x1[:, t, :], scalar=rs["r"][:], in1=gB[:], op0=ALU.mult, op1=ALU.mult),
                           reads=[x1, rs["r"], gB], writes=[hpb[r]])
                        for k in range(8):
                            op("pe", lambda e: e.transpose(out=psbf(0)[:, k * 128:(k + 1) * 128], in_=hpb[r][:, k * 128:(k + 1) * 128], identity=idb[:]), reads=[hpb[r], idb], writes=[PS[0]])
                        op("act", lambda e: e.copy(out=hpT[r][:], in_=psbf(0).rearrange("p (k t) -> p k t", k=8)), reads=[PS[0]], writes=[hpT[r]])
                        for k in range(2):
                            op("pe", lambda e: e.transpose(out=psbf(1)[:, k * 128:(k + 1) * 128], in_=plb[r][:, k * 128:(k + 1) * 128], identity=idb[:]), reads=[plb[r], idb], writes=[PS[1]])
                        op("act", lambda e: e.copy(out=plT[r][:], in_=psbf(1)[:, 0:256].rearrange("p (k t) -> p k t", k=2)), reads=[PS[1]], writes=[plT[r]])
                        for half in range(2):
                            hs = slice(half * 512, (half + 1) * 512)
                            bG = 2 + half
                            bP = 4 + half
                            for k in range(8):
                                mm(PS[bG], PS[bG][:, :], hpT[r][:, k, :], w_pg[:, k, hs], k == 0, k == 7, [hpT[r], w_pg])
                            for k in range(2):
                                mm(PS[bP], PS[bP][:, :], plT[r][:, k, :], w_pp[:, k, hs], k == 0, k == 1, [plT[r], w_pp])
                            op("act", lambda e: e.activation(out=sgP[half][:], in_=PS[bG][:, :], func=AF.Sigmoid), reads=[PS[bG]], writes=[sgP[half]])
                            op("dve", lambda e: e.tensor_tensor(out=tP[half][:], in0=PS[bP][:, :], in1=sgP[half][:], op=ALU.mult), reads=[PS[bP], sgP[half]], writes=[tP[half]])
                            op("dve", lambda e: e.tensor_tensor(out=x1[:, t, hs], in0=x1[:, t, hs], in1=tP[half][:], op=ALU.add), reads=[x1, tP[half]], writes=[x1])
                        rs2 = {"ss": ssp[2 + r], "r": rrp[2 + r]}
                        rms_rstd({"ap": x1[:, t, :], "bufs": [x1]}, rs2, D, {"ap": junkP[:], "buf": junkP})
                        op("dve", lambda e: e.scalar_tensor_tensor(out=outt[r][:], in0=x1[:, t, :], scalar=rs2["r"][:], in1=gB2[:], op0=ALU.mult, op1=ALU.mult),
                           reads=[x1, rs2["r"], gB2], writes=[outt[r]])
                        fw.dma(out_d[t * 128:(t + 1) * 128, :], outt[r][:], reads=[outt[r]], is_output=True)

            if "yaT" in dbg:
                o = dbg_t("yaT", [128, 4, S_OWN], BF16)
                fw.dma(o[:, :, :], YT[:, 0:4, :], reads=[YT], is_output=True)

            if "hT" in dbg:
                o = dbg_t("hT", [128, 8, S_EXT], BF16)
                with fw.scope() as esd:
                    tmp = fw.sb([128, 8, 512], BF16, "dbg_hT", esd)
                    for i in range(8):
                        fw.dma(tmp[:], hT_d[:, :, i * 512:(i + 1) * 512], reads=hT_tiles[4 * i:4 * i + 4], writes=[tmp])
                        fw.dma(o[:, :, i * 512:(i + 1) * 512], tmp[:], reads=[tmp], is_output=True)


        body()
        fw.stopped = False
        fw.finish()
    return nc, dbg_out


_INV = (500000.0 ** (-np.arange(0, 16, 2, dtype=np.float32) / 16.0)).astype(np.float32)


def make_in_maps(inputs):
    f = lambda a: np.ascontiguousarray(np.asarray(a), dtype=np.float32)
    x = f(inputs["x"]); p = f(inputs["p"])
    positions = np.asarray(inputs["positions"]).astype(np.int32)
    w_in = f(inputs["w_in"])[0]
    offs = np.cumsum([0, 512, 128, 128, 128, 128, 128, 128, 24, 1024, 512, 512, 8, 2048])
    seg = {n: (offs[i], offs[i + 1]) for i, n in enumerate(["q", "kc", "vc", "ks", "vs", "kw", "vw", "gate", "qk", "v", "o", "if", "mg"])}
    col = lambda n: w_in[:, seg[n][0]:seg[n][1]]
    w_att = []
    for g in range(2):
        parts = [col("q")[:, g * 256:(g + 1) * 256]]
        for n in ["ks", "kw", "kc", "vc", "vs", "vw"]:
            parts.append(col(n)[:, g * 64:(g + 1) * 64])
        parts.append(col("gate")[:, g * 12:(g + 1) * 12])
        w_att.append(np.concatenate(parts, axis=1))
    w_att = np.ascontiguousarray(np.stack(w_att))
    shared = {
        "invf": np.ascontiguousarray(np.broadcast_to(_INV[None, :], (128, 8))),
        "gvec": np.ascontiguousarray(np.stack([f(inputs["g_mix"])[0], f(inputs["g_ffn"])[0], f(inputs["g_ple"])[0], f(inputs["g_final"])])),
        "w_att": w_att,
        "w_qk": np.ascontiguousarray(col("qk")),
        "w_vo": np.ascontiguousarray(np.concatenate([col("v"), col("o")], axis=1)),
        "w_if": np.ascontiguousarray(col("if")),
        "w_mg": np.ascontiguousarray(col("mg")),
        "b_if": f(inputs["b_if"]).reshape(1, 8),
        "w_c1": np.ascontiguousarray(np.stack([f(inputs["w_ck1"])[0], f(inputs["w_cv1"])[0]])),
        "w_c2": np.ascontiguousarray(np.stack([f(inputs["w_ck2"])[0], f(inputs["w_cv2"])[0]])),
        "pe_c": np.ascontiguousarray(np.stack([f(inputs["pe_ck"])[0], f(inputs["pe_cv"])[0]])),
        "wc": np.ascontiguousarray(f(inputs["w_conv"])[0].reshape(4, 8, 128).transpose(2, 1, 0)),
        "bc": np.ascontiguousarray(f(inputs["b_conv"])[0].reshape(8, 128).T),
        "g_hn": f(inputs["g_hn"]).reshape(1, 512),
        "w_pa": f(inputs["w_pa"])[0], "w_pb": f(inputs["w_pb"])[0], "w_out": f(inputs["w_out"])[0],
        "w_r": np.ascontiguousarray(np.concatenate([f(inputs["w_rg"])[0], f(inputs["w_re"])[0]], axis=1)),
        "b_r": np.ascontiguousarray(np.concatenate([f(inputs["b_rg"])[0], f(inputs["b_re"])[0]])[None, :]),
        "w_e13": f(inputs["w_e13"])[0], "w_e2": f(inputs["w_e2"])[0],
        "w_pg": f(inputs["w_pg"])[0], "w_pp": f(inputs["w_pp"])[0],
    }
    in_maps = []
    for core in range(8):
        b, half = core // 2, core % 2
        if half == 1:
            xe_ = x[b]
            pos_ = positions[b]
        else:
            xe_ = np.concatenate([np.zeros((S_OWN, D), np.float32), x[b, :S_OWN]], axis=0)
            pos_ = np.concatenate([np.zeros(S_OWN, np.int32), positions[b, :S_OWN]])
        m = dict(shared)
        m["xe"] = np.ascontiguousarray(xe_)
        m["pos"] = np.ascontiguousarray(pos_.reshape(NT_EXT, 128).T)
        m["pl"] = np.ascontiguousarray(p[0, b, half * S_OWN:(half + 1) * S_OWN])
        m["hv"] = np.full((128, 1), float(half), np.float32)
        in_maps.append(m)
    return in_maps


def kernel(**inputs):
    nc, _ = build_program()
    in_maps = make_in_maps(inputs)
    res = run_bass_kernel_spmd(nc, in_maps, core_ids=list(range(8)))
    out = np.zeros((4, S_EXT, D), np.float32)
    for core in range(8):
        b, half = core // 2, core % 2
        out[b, half * S_OWN:(half + 1) * S_OWN] = res.results[core]["out"]
    return out
```

```python
import numpy as np
import concourse.bass as bass
import concourse.mybir as mybir
from concourse.bass_utils import run_bass_kernel_spmd
from contextlib import ExitStack

F32 = mybir.dt.float32
BF16 = mybir.dt.bfloat16
I32 = mybir.dt.int32
AF = mybir.ActivationFunctionType
ALU = mybir.AluOpType
AX = mybir.AxisListType

D = 1024
S_OWN = 2048
S_EXT = 4096
NT_OWN = 16
NT_EXT = 32
EPS = 1e-6
NEGB = -30000.0
DBG = []


class Buf:
    __slots__ = ("t", "lw", "rd", "name", "excl")

    def __init__(self, t, name=""):
        self.t = t
        self.excl = False
        self.lw = None
        self.rd = {}
        self.name = name

    def __getitem__(self, k):
        return self.t[k]


class FW:
    NDMA = 24

    def __init__(self, nc, es):
        self.nc = nc
        self.es = es
        self.eng = {"pe": nc.tensor, "act": nc.scalar, "dve": nc.vector, "pool": nc.gpsimd, "sp": nc.sync}
        self.sem = {k: es.enter_context(nc.semaphore("s_" + k)) for k in self.eng}
        self.cnt = {k: 0 for k in self.eng}
        self.known = {k: {} for k in self.eng}
        self.dsem = [es.enter_context(nc.semaphore(f"s_dma{i}")) for i in range(self.NDMA)]
        self.dval = [0] * self.NDMA
        self.dnext = 0
        self.nbuf = 0
        self.out_waits = []
        self.stopped = False

    def sb(self, shape, dt, name=None, es=None):
        self.nbuf += 1
        name = f"sb{self.nbuf}_" + (name or "t")
        return Buf((es or self.es).enter_context(self.nc.sbuf_tensor(name, list(shape), dt)), name)

    def ps(self, shape, dt, name=None):
        self.nbuf += 1
        name = name or f"ps{self.nbuf}"
        b = Buf(self.es.enter_context(self.nc.psum_tensor(name, list(shape), dt)), name)
        b.excl = True
        return b

    def _wait(self, e, src, idx):
        if self.stopped:
            return
        kn = self.known[e]
        if kn.get(src, 0) >= idx:
            return
        s = self.dsem[src[1]] if isinstance(src, tuple) else self.sem[src]
        self.eng[e].wait_ge(s, idx)
        kn[src] = idx

    def _deps(self, e, reads, writes):
        for b in reads:
            if b.lw is not None:
                self._wait(e, b.lw[0], b.lw[1])
            if b.excl:
                for src, idx in b.rd.items():
                    if src != e:
                        self._wait(e, src, idx)
        for b in writes:
            if b.lw is not None and b.lw[0] != e:
                self._wait(e, b.lw[0], b.lw[1])
            for src, idx in b.rd.items():
                if src != e:
                    self._wait(e, src, idx)

    def op(self, e, fn, reads=(), writes=()):
        if self.stopped:
            return None
        self._deps(e, reads, writes)
        inst = fn(self.eng[e])
        self.cnt[e] += 1
        c = self.cnt[e]
        inst.then_inc(self.sem[e], 1)
        for b in reads:
            if b.rd.get(e, 0) < c:
                b.rd[e] = c
        for b in writes:
            b.lw = (e, c)
            b.rd = {}
        return inst

    def dma(self, out, in_, reads=(), writes=(), q="sp", is_output=False):
        if self.stopped and not is_output:
            return None
        self._deps(q, reads, writes)
        slot = self.dnext
        self.dnext = (self.dnext + 1) % self.NDMA
        key = ("d", slot)
        if self.dval[slot] > 0:
            self._wait(q, key, self.dval[slot])
        inst = self.eng[q].dma_start(out=out, in_=in_)
        self.dval[slot] += 16
        inst.then_inc(self.dsem[slot], 16)
        v = self.dval[slot]
        for b in reads:
            if b.rd.get(key, 0) < v:
                b.rd[key] = v
        for b in writes:
            b.lw = (key, v)
            b.rd = {}
        if is_output:
            self.out_waits.append((key, v))
        return inst

    def barrier(self):
        for e in self.eng:
            for src in ("pe", "act", "dve", "pool"):
                if src != e and self.cnt[src] > 0:
                    self._wait(e, src, self.cnt[src])
            for slot in range(self.NDMA):
                if self.dval[slot] > 0:
                    self._wait(e, ("d", slot), self.dval[slot])

    def scope(self):
        fw = self

        class _Scope(ExitStack):
            def __exit__(self, *a):
                fw.barrier()
                return super().__exit__(*a)
        return _Scope()

    def finish(self):
        for key, v in self.out_waits:
            self._wait("sp", key, v)
        for k in ("pe", "act", "dve", "pool"):
            if self.cnt[k] > 0:
                self._wait("sp", k, self.cnt[k])


class _StopBuild(Exception):
    pass


def build_program(dbg=()):
    nc = bass.Bass("TRN2", target_bir_lowering=False)

    def din(name, shape, dt=F32):
        return nc.dram_tensor(name, list(shape), dt, kind="ExternalInput").ap()

    xe = din("xe", [S_EXT, D])
    pos_d = din("pos", [128, NT_EXT], I32)
    pl_d = din("pl", [S_OWN, 256])
    hv_d = din("hv", [128, 1])
    invf_d = din("invf", [128, 8])
    gvec_d = din("gvec", [4, D])
    w_att_d = din("w_att", [2, D, 652])
    w_qk_d = din("w_qk", [D, 1024])
    w_vo_d = din("w_vo", [D, 1024])
    w_if_d = din("w_if", [D, 8])
    w_mg_d = din("w_mg", [D, 2048])
    b_if_d = din("b_if", [1, 8])
    w_c1_d = din("w_c1", [2, 2048, 256])
    w_c2_d = din("w_c2", [2, 256, 64])
    pe_c_d = din("pe_c", [2, 32, 64])
    wc_d = din("wc", [128, 8, 4])
    bc_d = din("bc", [128, 8])
    g_hn_d = din("g_hn", [1, 512])
    w_pa_d = din("w_pa", [512, D])
    w_pb_d = din("w_pb", [512, D])
    w_out_d = din("w_out", [D, D])
    w_r_d = din("w_r", [D, 20])
    b_r_d = din("b_r", [1, 20])
    w_e13_d = din("w_e13", [16, D, 512])
    w_e2_d = din("w_e2", [16, 256, D])
    w_pg_d = din("w_pg", [D, D])
    w_pp_d = din("w_pp", [256, D])
    out_d = nc.dram_tensor("out", [S_OWN, D], F32, kind="ExternalOutput").ap()
    hT_d = nc.dram_tensor("hT_scr", [128, 8, S_EXT], BF16, kind="Internal").ap()
    dbg_out = {}

    def dbg_t(name, shape, dt=F32):
        dbg_out[name] = nc.dram_tensor("dbg_" + name, list(shape), dt, kind="ExternalOutput").ap()
        return dbg_out[name]

    with ExitStack() as es:
        fw = FW(nc, es)
        op = fw.op
        PS = [fw.ps([128, 512], F32, f"psb{i}") for i in range(8)]

        def psbf(i):
            return PS[i][:].bitcast(BF16)

        ones_f = fw.sb([128, 128], F32, "ones_f")
        op("pool", lambda e: e.memset(ones_f[:], 1.0), writes=[ones_f])
        idf = fw.sb([128, 128], F32, "idf")
        op("pool", lambda e: e.affine_select(out=idf[:], in_=ones_f[:], pattern=[[1, 128]], compare_op=ALU.is_equal,
                                             fill=0.0, base=0, channel_multiplier=-1), reads=[ones_f], writes=[idf])
        idb = fw.sb([128, 128], BF16, "idb")
        op("dve", lambda e: e.tensor_copy(out=idb[:], in_=idf[:]), reads=[idf], writes=[idb])
        U_f = fw.sb([128, 128], F32, "U_f")
        op("pool", lambda e: e.affine_select(out=U_f[:], in_=ones_f[:], pattern=[[1, 128]], compare_op=ALU.is_ge,
                                             fill=0.0, base=0, channel_multiplier=-1), reads=[ones_f], writes=[U_f])
        caus = fw.sb([128, 128], BF16, "caus")
        op("dve", lambda e: e.tensor_copy(out=caus[:], in_=U_f[:]), reads=[U_f], writes=[caus])
        wm0_f = fw.sb([128, 128], F32, "wm0_f")
        op("pool", lambda e: e.affine_select(out=wm0_f[:], in_=ones_f[:], pattern=[[-1, 128]], compare_op=ALU.is_ge,
                                             fill=0.0, base=-1, channel_multiplier=1), reads=[ones_f], writes=[wm0_f])
        wm0 = fw.sb([128, 128], BF16, "wm0")
        op("dve", lambda e: e.tensor_copy(out=wm0[:], in_=wm0_f[:]), reads=[wm0_f], writes=[wm0])
        c_eps = fw.sb([128, 1], F32, "c_eps")
        op("pool", lambda e: e.memset(c_eps[:], EPS), writes=[c_eps])
        c_one = fw.sb([128, 1], F32, "c_one")
        op("pool", lambda e: e.memset(c_one[:], 1.0), writes=[c_one])
        c_zero = fw.sb([128, 1], F32, "c_zero")
        op("pool", lambda e: e.memset(c_zero[:], 0.0), writes=[c_zero])
        acc_junk = fw.sb([128, 2], F32, "acc_junk")
        op("act", lambda e: e.activation(out=acc_junk[:, 0:1], in_=c_one[:], func=AF.Square, accum_out=acc_junk[:, 1:2]),
           reads=[c_one], writes=[acc_junk])
        hv = fw.sb([128, 1], F32, "hv")
        fw.dma(hv[:], hv_d[:, :], writes=[hv])
        hbias = fw.sb([128, 1], F32, "hbias")
        op("dve", lambda e: e.tensor_scalar(out=hbias[:], in0=hv[:], scalar1=-1.0, scalar2=-NEGB, op0=ALU.add, op1=ALU.mult),
           reads=[hv], writes=[hbias])
        gB = fw.sb([128, D], F32, "gB")

        def load_gain(i):
            fw.dma(gB[:], gvec_d[i:i + 1, :].to_broadcast([128, D]), writes=[gB])

        cs = fw.sb([128, NT_EXT, 8], F32, "cs")
        sn = fw.sb([128, NT_EXT, 8], F32, "sn")
        with fw.scope() as es1:
            posi = fw.sb([128, NT_EXT], I32, "posi", es1)
            posf = fw.sb([128, NT_EXT], F32, "posf", es1)
            invf = fw.sb([128, 8], F32, "invf", es1)
            ang = fw.sb([128, NT_EXT, 8], F32, "ang", es1)
            kf = fw.sb([128, NT_EXT, 8], F32, "kf", es1)
            ki = fw.sb([128, NT_EXT, 8], I32, "ki", es1)
            r1 = fw.sb([128, NT_EXT, 8], F32, "r1", es1)
            r2 = fw.sb([128, NT_EXT, 8], F32, "r2", es1)
            fw.dma(posi[:], pos_d[:, :], writes=[posi])
            fw.dma(invf[:], invf_d[:, :], writes=[invf])
            op("dve", lambda e: e.tensor_copy(out=posf[:], in_=posi[:]), reads=[posi], writes=[posf])
            op("dve", lambda e: e.tensor_tensor(out=ang[:], in0=posf[:].unsqueeze(2).to_broadcast([128, NT_EXT, 8]),
                                                in1=invf[:].unsqueeze(1).to_broadcast([128, NT_EXT, 8]), op=ALU.mult),
               reads=[posf, invf], writes=[ang])
            TWO_PI = 6.283185307179586
            C1 = 6.28125
            C2 = TWO_PI - C1
            PI_LO = 3.1415925
            op("dve", lambda e: e.tensor_scalar(out=kf[:], in0=ang[:], scalar1=1.0 / TWO_PI, scalar2=None, op0=ALU.mult),
               reads=[ang], writes=[kf])
            op("dve", lambda e: e.tensor_copy(out=ki[:], in_=kf[:]), reads=[kf], writes=[ki])
            op("dve", lambda e: e.tensor_copy(out=kf[:], in_=ki[:]), reads=[ki], writes=[kf])
            op("dve", lambda e: e.scalar_tensor_tensor(out=r1[:], in0=kf[:], scalar=-C1, in1=ang[:], op0=ALU.mult, op1=ALU.add),
               reads=[kf, ang], writes=[r1])
            op("dve", lambda e: e.scalar_tensor_tensor(out=r1[:], in0=kf[:], scalar=-C2, in1=r1[:], op0=ALU.mult, op1=ALU.add),
               reads=[kf, r1], writes=[r1])
            op("dve", lambda e: e.tensor_scalar(out=r1[:], in0=r1[:], scalar1=PI_LO, scalar2=-PI_LO, op0=ALU.min, op1=ALU.max),
               reads=[r1], writes=[r1])
            op("act", lambda e: e.activation(out=sn[:], in_=r1[:], func=AF.Sin), reads=[r1], writes=[sn])
            op("dve", lambda e: e.tensor_scalar(out=r2[:], in0=r1[:], scalar1=PI_LO / 2 + 0.0, scalar2=None, op0=ALU.add),
               reads=[r1], writes=[r2])
            op("dve", lambda e: e.tensor_scalar(out=kf[:], in0=r2[:], scalar1=PI_LO, scalar2=-TWO_PI, op0=ALU.is_gt, op1=ALU.mult),
               reads=[r2], writes=[kf])
            op("dve", lambda e: e.tensor_tensor(out=r2[:], in0=r2[:], in1=kf[:], op=ALU.add), reads=[r2, kf], writes=[r2])
            op("dve", lambda e: e.tensor_scalar(out=r2[:], in0=r2[:], scalar1=PI_LO, scalar2=-PI_LO, op0=ALU.min, op1=ALU.max),
               reads=[r2], writes=[r2])
            op("act", lambda e: e.activation(out=cs[:], in_=r2[:], func=AF.Sin), reads=[r2], writes=[cs])

        def rms_rstd(src, rstd, n, junk):
            ss = rstd["ss"]
            op("act", lambda e: e.activation(out=junk["ap"], in_=src["ap"], func=AF.Square, accum_out=ss[:]),
               reads=src["bufs"], writes=[junk["buf"], ss])
            op("act", lambda e: e.activation(out=ss[:], in_=ss[:], func=AF.Sqrt, bias=c_eps[:], scale=1.0 / n),
               reads=[ss, c_eps], writes=[ss])
            op("dve", lambda e: e.reciprocal(out=rstd["r"][:], in_=ss[:]), reads=[ss], writes=[rstd["r"]])

        load_gain(0)
        hT_tiles = [Buf(None, f"hT_tile{t}") for t in range(NT_EXT)]
        with fw.scope() as esA:
            xt = [fw.sb([128, D], F32, f"xtA{i}", esA) for i in range(6)]
            xn = [fw.sb([128, D], BF16, f"xnA{i}", esA) for i in range(2)]
            junk = fw.sb([128, D], BF16, "junkA", esA)
            hst = [fw.sb([128, 8, 128], BF16, f"hstA{i}", esA) for i in range(4)]
            ssA = [fw.sb([128, 1], F32, f"ssA{i}", esA) for i in range(2)]
            rrA = [fw.sb([128, 1], F32, f"rrA{i}", esA) for i in range(2)]
            for t in range(NT_EXT):
                x_ = xt[t % 6]
                if t == 0:
                    for tt in range(5):
                        fw.dma(xt[tt][:], xe[tt * 128:(tt + 1) * 128, :], writes=[xt[tt]])
                if t + 5 < NT_EXT:
                    fw.dma(xt[(t + 5) % 6][:], xe[(t + 5) * 128:(t + 6) * 128, :], writes=[xt[(t + 5) % 6]])
                rs = {"ss": ssA[t % 2], "r": rrA[t % 2]}
                rms_rstd({"ap": x_[:], "bufs": [x_]}, rs, D, {"ap": junk[:], "buf": junk})
                n_ = xn[t % 2]
                op("dve", lambda e: e.scalar_tensor_tensor(out=n_[:], in0=x_[:], scalar=rs["r"][:], in1=gB[:], op0=ALU.mult, op1=ALU.mult),
                   reads=[x_, rs["r"], gB], writes=[n_])
                pb = t % 2
                for k in range(8):
                    op("pe", lambda e: e.transpose(out=psbf(pb)[:, k * 128:(k + 1) * 128], in_=n_[:, k * 128:(k + 1) * 128], identity=idb[:]),
                       reads=[n_, idb], writes=[PS[pb]])
                h_ = hst[t % 4]
                op("act", lambda e: e.copy(out=h_[:], in_=psbf(pb).rearrange("p (k t) -> p k t", k=8)), reads=[PS[pb]], writes=[h_])
                fw.dma(hT_d[:, :, t * 128:(t + 1) * 128], h_[:], reads=[h_], writes=[hT_tiles[t]], q="pool")


        if "cs" in dbg:
            o = dbg_t("cs", [128, NT_EXT, 8])
            fw.dma(o[:, :, :], cs[:], reads=[cs], is_output=True)
            o = dbg_t("sn", [128, NT_EXT, 8])
            fw.dma(o[:, :, :], sn[:], reads=[sn], is_output=True)

        def ckpt(name):
            if ("stop_" + name) in dbg:
                fw.stopped = True

        def body():
            def mm(bank, out_ap, lhsT, rhs, start, stop, reads):
                op("pe", lambda e: e.matmul(out_ap, lhsT, rhs, start=start, stop=stop), reads=reads, writes=[bank])

            YT = fw.sb([128, 8, S_OWN], BF16, "YT")
            esBc = fw.scope()
            esBc.__enter__()
            cmask = fw.sb([128, 2, S_OWN], BF16, "cmask", esBc)
            op("pool", lambda e: e.memset(cmask[:], 1.0), writes=[cmask])
            op("pool", lambda e: e.affine_select(out=cmask[:, 0, :], in_=cmask[:, 0, :], pattern=[[1, S_OWN]], compare_op=ALU.is_ge, fill=0.0,
                                                 base=2017, channel_multiplier=-16), reads=[cmask], writes=[cmask])
            op("pool", lambda e: e.affine_select(out=cmask[:, 1, :], in_=cmask[:, 1, :], pattern=[[1, S_OWN]], compare_op=ALU.is_ge, fill=0.0,
                                                 base=-31, channel_multiplier=-16), reads=[cmask], writes=[cmask])
            ovl = fw.sb([128, 2, 64], BF16, "ovl", esBc)
            op("pool", lambda e: e.memset(ovl[:], 1.0), writes=[ovl])
            for j in range(2):
                op("pool", lambda e: e.affine_select(out=ovl[:, j, :], in_=ovl[:, j, :], pattern=[[-4, 64]], compare_op=ALU.is_ge, fill=0.0,
                                                     base=128 * j + 1, channel_multiplier=1), reads=[ovl], writes=[ovl])
                op("pool", lambda e: e.affine_select(out=ovl[:, j, :], in_=ovl[:, j, :], pattern=[[4, 64]], compare_op=ALU.is_ge, fill=0.0,
                                                     base=3 - 128 * j, channel_multiplier=-1), reads=[ovl], writes=[ovl])
            maskadd = fw.sb([128, NT_OWN, 64], F32, "maskadd", esBc)
            Mb = fw.sb([128, 64], F32, "Mb", esBc)
            hm1 = fw.sb([128, 2], F32, "hm1", esBc)
            op("dve", lambda e: e.tensor_scalar(out=hm1[:, 0:1], in0=hv[:], scalar1=-1.0, scalar2=1e30, op0=ALU.add, op1=ALU.mult),
               reads=[hv], writes=[hm1])
            op("dve", lambda e: e.tensor_scalar(out=hm1[:, 1:2], in0=hv[:], scalar1=-1.0, scalar2=-1000.0, op0=ALU.add, op1=ALU.mult),
               reads=[hv, hm1], writes=[hm1])
            op("dve", lambda e: e.memset(Mb[:], 0.0), writes=[Mb])
            op("dve", lambda e: e.tensor_copy(out=Mb[:, 0:32], in_=hm1[:, 0:1].to_broadcast([128, 32])), reads=[hm1, Mb], writes=[Mb])
            op("dve", lambda e: e.scalar_tensor_tensor(out=Mb[:, 0:1], in0=hv[:], scalar=1000.0, in1=Mb[:, 0:1], op0=ALU.mult, op1=ALU.add),
               reads=[hv, Mb], writes=[Mb])
            op("dve", lambda e: e.tensor_copy(out=Mb[:, 32:33], in_=hm1[:, 1:2]), reads=[hm1, Mb], writes=[Mb])
            for c in range(NT_OWN):
                op("pool", lambda e: e.tensor_copy(out=maskadd[:, c, :], in_=Mb[:]), reads=[Mb, maskadd], writes=[maskadd])
                for hf in range(2):
                    lo = 32 + 2 * c + hf + 1
                    if lo < 64:
                        op("pool", lambda e: e.memset(maskadd[hf * 64:(hf + 1) * 64, c, lo:64], -1e30), reads=[maskadd], writes=[maskadd])
                    for col in (32 + 2 * c + hf, 32 + 2 * c + hf - 1):
                        op("pool", lambda e: e.tensor_scalar(out=maskadd[hf * 64:(hf + 1) * 64, c, col:col + 1],
                                                             in0=maskadd[hf * 64:(hf + 1) * 64, c, col:col + 1],
                                                             scalar1=1000.0, scalar2=None, op0=ALU.add), reads=[maskadd], writes=[maskadd])

            ckpt("consts")
            for g in range(2):
                with fw.scope() as esG:
                    qT = fw.sb([128, 4, S_OWN], BF16, f"qT{g}", esG)
                    kkT = fw.sb([128, 2, S_EXT], BF16, f"kkT{g}", esG)
                    op("pool", lambda e: e.memset(qT[64:128, :, :], 0.0), writes=[qT])
                    op("pool", lambda e: e.memset(kkT[64:128, 0, :], 1.0), writes=[kkT])
                    op("pool", lambda e: e.memset(kkT[64:128, 1, :], 0.0), writes=[kkT])
                    op("pool", lambda e: e.affine_select(out=kkT[64:128, 0, :], in_=kkT[64:128, 0, :], pattern=[[1, S_EXT]], compare_op=ALU.is_ge, fill=0.0,
                                                         base=0, channel_multiplier=-64), reads=[kkT], writes=[kkT])
                    op("pool", lambda e: e.affine_select(out=kkT[64:128, 0, :], in_=kkT[64:128, 0, :], pattern=[[-1, S_EXT]], compare_op=ALU.is_ge, fill=0.0,
                                                         base=63, channel_multiplier=64), reads=[kkT], writes=[kkT])
                    vv = fw.sb([128, NT_EXT, 2, 65], BF16, f"vv{g}", esG)
                    gsig = fw.sb([128, NT_OWN, 12], F32, f"gsig{g}", esG)
                    kcmpT = fw.sb([128, 256], BF16, f"kcmpT{g}", esG)
                    op("pool", lambda e: e.memset(kcmpT[64:128, :], 0.0), writes=[kcmpT])
                    vca = fw.sb([128, 2, 65], BF16, f"vca{g}", esG)
                    op("pool", lambda e: e.memset(vv[:, :, :, 64:65], 1.0), writes=[vv])
                    op("pool", lambda e: e.memset(vca[:, :, 64:65], 1.0), writes=[vca])
                    ckpt("B0a")
                    with fw.scope() as esC:
                        ccT = fw.sb([64, 2, S_EXT], BF16, f"ccT{g}", esC)
                        with fw.scope() as esB1:
                            w_att = fw.sb([128, 8, 652], BF16, f"w_att{g}", esB1)
                            fw.dma(w_att[:], w_att_d[g].rearrange("(k p) c -> p k c", p=128), writes=[w_att], q="pool")
                            ckpt("B0b")
                            hblk = [fw.sb([128, 8, 512], BF16, f"hblkB{g}{i}", esB1) for i in range(2)]
                            rp = [fw.sb([128, 8, 64], BF16, f"rp{g}{i}", esB1) for i in range(2)]
                            rpf = [fw.sb([128, 8, 64], F32, f"rpf{g}{i}", esB1) for i in range(2)]
                            ta = [fw.sb([128, 7, 8], F32, f"ropa{g}{i}", esB1) for i in range(2)]
                            tb_ = [fw.sb([128, 7, 8], F32, f"ropb{g}{i}", esB1) for i in range(2)]
                            tcx = [fw.sb([128, 7, 8], F32, f"ropc{g}{i}", esB1) for i in range(2)]
                            tdx = [fw.sb([128, 7, 8], F32, f"ropd{g}{i}", esB1) for i in range(2)]
                            def b1_front(t):
                                own = t >= NT_OWN
                                tq = t - NT_OWN
                                hb = hblk[(t // 4) % 2]
                                if t % 4 == 0:
                                    fw.dma(hb[:], hT_d[:, :, t * 128:(t + 4) * 128], reads=hT_tiles[t:t + 4], writes=[hb])
                                tl = t % 4
                                a0 = 0 if own else 256
                                nb = 140 if own else 128
                                bA = 2 + t % 2
                                bB = 4 + t % 2
                                for k in range(8):
                                    mm(PS[bA], PS[bA][:, a0:512], hb[:, k, tl * 128:(tl + 1) * 128], w_att[:, k, a0:512], k == 0, k == 7, [hb, w_att])
                                for k in range(8):
                                    mm(PS[bB], PS[bB][:, 0:nb], hb[:, k, tl * 128:(tl + 1) * 128], w_att[:, k, 512:512 + nb], k == 0, k == 7, [hb, w_att])
                                rp_ = rp[t % 2]
                                h0 = a0 // 64
                                nh = 7 - h0
                                rf = rpf[t % 2]
                                op("act", lambda e: e.copy(out=rf[:, h0:8, :], in_=PS[bA][:, a0:512].rearrange("p (h d) -> p h d", d=64)),
                                   reads=[PS[bA]], writes=[rf])
                                op("pool", lambda e: e.tensor_copy(out=rp_[:, h0:8, :], in_=rf[:, h0:8, :]), reads=[rf], writes=[rp_])
                                t1 = rf[:, h0:7, 0:8]
                                t2 = rf[:, h0:7, 8:16]
                                Cb = cs[:, t, :].unsqueeze(1).to_broadcast([128, nh, 8])
                                Sb_ = sn[:, t, :].unsqueeze(1).to_broadcast([128, nh, 8])
                                ta_, tb2 = ta[t % 2], tb_[t % 2]
                                tc_, td_ = tcx[t % 2], tdx[t % 2]
                                op("dve", lambda e: e.tensor_tensor(out=ta_[:, 0:nh, :], in0=t1, in1=Cb, op=ALU.mult), reads=[rf, cs], writes=[ta_])
                                op("dve", lambda e: e.tensor_tensor(out=tb2[:, 0:nh, :], in0=t2, in1=Sb_, op=ALU.mult), reads=[rf, sn], writes=[tb2])
                                op("dve", lambda e: e.tensor_tensor(out=tc_[:, 0:nh, :], in0=t2, in1=Cb, op=ALU.mult), reads=[rf, cs], writes=[tc_])
                                op("dve", lambda e: e.tensor_tensor(out=td_[:, 0:nh, :], in0=t1, in1=Sb_, op=ALU.mult), reads=[rf, sn], writes=[td_])
                                op("dve", lambda e: e.tensor_tensor(out=rp_[:, h0:7, 0:8], in0=ta_[:, 0:nh, :], in1=tb2[:, 0:nh, :], op=ALU.subtract),
                                   reads=[ta_, tb2, rp_], writes=[rp_])
                                op("dve", lambda e: e.tensor_tensor(out=rp_[:, h0:7, 8:16], in0=tc_[:, 0:nh, :], in1=td_[:, 0:nh, :], op=ALU.add),
                                   reads=[tc_, td_, rp_], writes=[rp_])
                                op("dve", lambda e: e.tensor_copy(out=vv[:, t, :, 0:64], in_=PS[bB][:, 0:128].rearrange("p (h d) -> p h d", d=64)),
                                   reads=[PS[bB]], writes=[vv])
                                if own:
                                    op("act", lambda e: e.activation(out=gsig[:, tq, :], in_=PS[bB][:, 128:140], func=AF.Sigmoid),
                                       reads=[PS[bB]], writes=[gsig])

                            def b1_back(t):
                                own = t >= NT_OWN
                                tq = t - NT_OWN
                                rp_ = rp[t % 2]
                                h0 = 0 if own else 4
                                bT = t % 2
                                psT = psbf(bT)
                                for j, hh in enumerate(range(h0, 8)):
                                    op("pe", lambda e: e.transpose(out=psT[0:64, j * 128:(j + 1) * 128], in_=rp_[:, hh, :], identity=idb[:]),
                                       reads=[rp_, idb], writes=[PS[bT]])
                                if own:
                                    op("act", lambda e: e.copy(out=qT[0:64, :, tq * 128:(tq + 1) * 128], in_=psT[0:64, 0:512].rearrange("p (h t) -> p h t", h=4)),
                                       reads=[PS[bT]], writes=[qT])
                                    o1 = 512
                                else:
                                    o1 = 0
                                op("act", lambda e: e.copy(out=kkT[0:64, :, t * 128:(t + 1) * 128], in_=psT[0:64, o1:o1 + 256].rearrange("p (h t) -> p h t", h=2)),
                                   reads=[PS[bT]], writes=[kkT])
                                op("act", lambda e: e.copy(out=ccT[:, :, t * 128:(t + 1) * 128], in_=psT[0:64, o1 + 256:o1 + 512].rearrange("p (h t) -> p h t", h=2)),
                                   reads=[PS[bT]], writes=[ccT])

                            for t in range(NT_EXT + 1):
                                if t < NT_EXT:
                                    b1_front(t)
                                if t >= 1:
                                    b1_back(t - 1)
                        ckpt("B1")
                        for i in range(2):
                            with fw.scope() as esB2:
                                w1 = fw.sb([64, 32, 256], BF16, f"w1_{g}{i}", esB2)
                                fw.dma(w1[:], w_c1_d[i].rearrange("(l d) h -> d l h", d=64), writes=[w1], q="pool")
                                w2 = fw.sb([128, 2, 64], BF16, f"w2_{g}{i}", esB2)
                                fw.dma(w2[:], w_c2_d[i].rearrange("(c p) d -> p c d", p=128), writes=[w2], q="pool")
                                pe_sb = fw.sb([32, 64], BF16, f"pe_{g}{i}", esB2)
                                fw.dma(pe_sb[:], pe_c_d[i], writes=[pe_sb], q="pool")
                                peT = fw.sb([64, 32], BF16, f"peT_{g}{i}", esB2)
                                op("pe", lambda e: e.transpose(out=psbf(6)[0:64, 0:32], in_=pe_sb[:, :], identity=idb[0:32, 0:32]),
                                   reads=[pe_sb, idb], writes=[PS[6]])
                                op("act", lambda e: e.copy(out=peT[:], in_=psbf(6)[0:64, 0:32]), reads=[PS[6]], writes=[peT])
                                for hc in range(2):
                                    for l in range(32):
                                        mm(PS[7], PS[7][:, hc:hc + 1], w1[:, l, hc * 128:(hc + 1) * 128], peT[:, l:l + 1], l == 0, l == 31, [w1, peT])
                                cbs = fw.sb([128, 2], F32, f"cbs_{g}{i}", esB2)
                                op("act", lambda e: e.copy(out=cbs[:], in_=PS[7][:, 0:2]), reads=[PS[7]], writes=[cbs])
                                G = fw.sb([128, 2, 256], BF16, f"G_{g}{i}", esB2)
                                op("pool", lambda e: e.memset(G[:, :, 255:256], 0.0), writes=[G])
                                u_ = fw.sb([128, 255], F32, f"u_{g}{i}", esB2)
                                u2 = fw.sb([128, 255], F32, f"u2_{g}{i}", esB2)
                                sg_ = fw.sb([128, 255], F32, f"sg_{g}{i}", esB2)
                                for hc in range(2):
                                    for l in range(32):
                                        mm(PS[hc], PS[hc][:, 0:255], w1[:, l, hc * 128:(hc + 1) * 128], ccT[:, i, l:l + 16 * 254 + 1:16], l == 0, l == 31, [w1, ccT])
                                    op("act", lambda e: e.activation(out=u_[:], in_=PS[hc][:, 0:255], func=AF.Identity, bias=cbs[:, hc:hc + 1]),
                                       reads=[PS[hc], cbs], writes=[u_])
                                    op("dve", lambda e: e.tensor_tensor(out=u2[:], in0=u_[:], in1=u_[:], op=ALU.mult), reads=[u_], writes=[u2])
                                    op("dve", lambda e: e.tensor_scalar(out=u2[:], in0=u2[:], scalar1=0.044715, scalar2=1.0, op0=ALU.mult, op1=ALU.add),
                                       reads=[u2], writes=[u2])
                                    op("dve", lambda e: e.tensor_tensor(out=u2[:], in0=u2[:], in1=u_[:], op=ALU.mult), reads=[u2, u_], writes=[u2])
                                    op("act", lambda e: e.activation(out=sg_[:], in_=u2[:], func=AF.Sigmoid, scale=1.5957691216057308),
                                       reads=[u2], writes=[sg_])
                                    op("dve", lambda e: e.tensor_tensor(out=G[:, hc, 0:255], in0=u_[:], in1=sg_[:], op=ALU.mult), reads=[u_, sg_], writes=[G])
                                if i == 0:
                                    for hc in range(2):
                                        mm(PS[6], PS[6][0:64, 0:256], w2[:, hc, :], G[:, hc, :], hc == 0, hc == 1, [w2, G])
                                    op("act", lambda e: e.copy(out=kcmpT[0:64, :], in_=PS[6][0:64, 0:256]), reads=[PS[6]], writes=[kcmpT])
                                else:
                                    for nch in range(2):
                                        for hc in range(2):
                                            mm(PS[6], PS[6][:, nch * 64:(nch + 1) * 64], G[:, hc, nch * 128:(nch + 1) * 128], w2[:, hc, :], hc == 0, hc == 1, [w2, G])
                                    op("act", lambda e: e.copy(out=vca[:, :, 0:64], in_=PS[6][:, 0:128].rearrange("p (n d) -> p n d", d=64)),
                                       reads=[PS[6]], writes=[vca])
                    if g == 0 and "B2dump" in dbg:
                        for nm, bf, shp in (("kkT", kkT, [64, 2, S_EXT]), ("qT", qT, [64, 4, S_OWN]), ("vv", vv, [128, NT_EXT, 2, 65]),
                                            ("kcmpT", kcmpT, [64, 256]), ("vca", vca, [128, 2, 65])):
                            o = dbg_t(nm, shp, BF16)
                            fw.dma(o, bf[0:shp[0]], reads=[bf], is_output=True)
                        o = dbg_t("gsig", [128, NT_OWN, 12])
                        fw.dma(o, gsig[:], reads=[gsig], is_output=True)
                    ckpt("B2")
                    with fw.scope() as esB3:
                        NP = 3
                        Pb = [fw.sb([128, 512], BF16, f"Pb{g}{i}", esB3) for i in range(NP)]
                        Sbank = [0, 1, 6]
                        tpsum = psbf(7)
                        TPB = PS[7]
                        hbS = [fw.sb([128, 4, 132], F32, f"hbS{g}{r}", esB3) for r in range(3)]
                        ya = [fw.sb([128, 4, 64], F32, f"ya{g}{i}", esB3) for i in range(2)]
                        yat = [fw.sb([128, 4, 64], BF16, f"yat{g}{i}", esB3) for i in range(2)]
                        sms = [fw.sb([128, 16], F32, f"sm{g}{i}", esB3) for i in range(3)]
                        rdc = fw.sb([128, 4], F32, f"rdc{g}", esB3)
                        impv = fw.sb([128, 64], F32, f"impv{g}", esB3)
                        wk = fw.sb([128, 64], F32, f"wk{g}", esB3)
                        m8a = fw.sb([128, 8], F32, f"m8a{g}", esB3)
                        m8b = fw.sb([128, 8], F32, f"m8b{g}", esB3)
                        negm2 = fw.sb([128, 128], BF16, f"negm{g}", esB3)
                        op("pool", lambda e: e.memset(negm2[:, 0:64], 0.0), writes=[negm2])
                        rot = [0]
                        REG = {0: (0, 129), 1: (129, 65), 2: (194, 65)}

                        def score(c, lhsT, lreads, extra, bias, mask):
                            r = rot[0] % NP
                            rot[0] += 1
                            sb_i = Sbank[r]
                            P = Pb[r]
                            qrhs = qT[:, :, c * 128:(c + 1) * 128]
                            S3 = PS[sb_i][:, :].rearrange("p (h q) -> p h q", h=4)
                            mm(PS[sb_i], S3, lhsT, qrhs, True, True, lreads + [qT])
                            op("act", lambda e: e.activation(out=P[:], in_=PS[sb_i][:, :], func=AF.Exp, bias=bias[:], scale=0.125),
                               reads=[PS[sb_i], bias], writes=[P])
                            if mask is not None:
                                op("dve", lambda e: e.tensor_tensor(out=P[:].rearrange("p (h q) -> p h q", h=4), in0=P[:].rearrange("p (h q) -> p h q", h=4),
                                                                    in1=mask[0], op=ALU.mult), reads=[P, mask[1]], writes=[P])
                            return P

                        def pv(P, h, reg, vr, vreads, cc, n, first, last):
                            op("pe", lambda e: e.matmul(PS[2 + h][:, cc:cc + n], P[:, h * 128:(h + 1) * 128], vr, start=first, stop=last),
                               reads=[P] + vreads, writes=[PS[2 + h]])

                        def evac_all(c, reg, br, first, final):
                            col0, n = REG[reg]
                            hs = hbS[reg]
                            for h in range(4):
                                op("dve", lambda e: e.tensor_copy(out=hs[:, h, 0:n], in_=PS[2 + h][:, col0:col0 + n]), reads=[PS[2 + h]], writes=[hs])
                            sm = sms[reg]
                            yac = ya[c % 2]
                            dn = sm[:, 0:4]
                            rd = sm[:, 4:8] if br != 0 else rdc[:, 0:4]
                            rdb = sm if br != 0 else rdc
                            cf = sm[:, 8:12]
                            op("dve", lambda e: e.tensor_scalar(out=dn.unsqueeze(2), in0=hs[:, :, 64:65], scalar1=1e-30, scalar2=None, op0=ALU.max),
                               reads=[hs], writes=[sm])
                            op("dve", lambda e: e.reciprocal(out=rd, in_=dn), reads=[sm], writes=[rdb])
                            op("dve", lambda e: e.tensor_tensor(out=cf.unsqueeze(2), in0=rd.unsqueeze(2),
                                                                in1=gsig[:, c, :].rearrange("p (h b) -> p h b", b=3)[:, :, br:br + 1], op=ALU.mult),
                               reads=[sm, rdb, gsig], writes=[sm])
                            cfb = cf.unsqueeze(2).to_broadcast([128, 4, 64])
                            if first:
                                op("dve", lambda e: e.tensor_tensor(out=yac[:], in0=hs[:, :, 0:64], in1=cfb, op=ALU.mult), reads=[hs, sm], writes=[yac])
                            else:
                                op("dve", lambda e: e.tensor_tensor(out=hs[:, :, 0:64], in0=hs[:, :, 0:64], in1=cfb, op=ALU.mult), reads=[hs, sm], writes=[hs])
                                dst = yat[c % 2] if final else yac
                                op("dve", lambda e: e.tensor_tensor(out=dst[:], in0=hs[:, :, 0:64], in1=yac[:], op=ALU.add), reads=[hs, yac], writes=[dst])

                        pend = [None]

                        def flush():
                            if pend[0] is not None:
                                pend[0]()
                                pend[0] = None

                        def pipe(score_fn, pv_fn):
                            P = score_fn()
                            flush()
                            pend[0] = lambda: pv_fn(P)

                        for c in range(NT_OWN):
                            Pc = []
                            for nch in range(2):
                                mk = cmask[:, nch, c * 128:(c + 1) * 128].unsqueeze(1).to_broadcast([128, 4, 128])
                                Pc.append(score(c, kcmpT[:, nch * 128:(nch + 1) * 128], [kcmpT], None, hbias if nch == 0 else c_zero, (mk, cmask)))
                            flush()
                            if c > 0:
                                cp = c - 1
                                evac_all(cp, 1, 1, False, True)
                                for j in range(2):
                                    op("pe", lambda e: e.transpose(out=tpsum[:, j * 128:(j + 1) * 128],
                                                                   in_=yat[cp % 2][:, 2 * j:2 * j + 2, :].rearrange("p h d -> p (h d)"), identity=idb[:]),
                                       reads=[yat[cp % 2], idb], writes=[TPB])
                                op("act", lambda e: e.copy(out=YT[:, 2 * g:2 * g + 2, cp * 128:(cp + 1) * 128],
                                                           in_=tpsum[:, 0:256].rearrange("p (j t) -> p j t", j=2)), reads=[TPB], writes=[YT])
                            if g == 0 and c == 1 and "B3dump" in dbg:
                                for r_ in range(3):
                                    fw.dma(dbg_t(f"hbS{r_}", [128, 4, 132]), hbS[r_][:], reads=[hbS[r_]], is_output=True)
                                fw.dma(dbg_t("yat0", [128, 4, 64], BF16), yat[0][:], reads=[yat[0]], is_output=True)
                                fw.dma(dbg_t("ya0", [128, 4, 64]), ya[0][:], reads=[ya[0]], is_output=True)
                                fw.dma(dbg_t("sm0", [128, 16]), sms[0][:], reads=[sms[0]], is_output=True)
                                fw.dma(dbg_t("sm1", [128, 16]), sms[1][:], reads=[sms[1]], is_output=True)
                                fw.dma(dbg_t("sm2", [128, 16]), sms[2][:], reads=[sms[2]], is_output=True)
                                fw.dma(dbg_t("rdc", [128, 4]), rdc[:], reads=[rdc], is_output=True)
                                ckpt("B3c0")
                            for h in range(4):
                                for nch in range(2):
                                    pv(Pc[nch], h, 0, vca[:, nch, :], [vca], 0, 65, nch == 0, nch == 1)
                                for nch in range(2):
                                    pv(Pc[nch], h, 0, ovl[:, nch, :], [ovl], 65, 64, nch == 0, nch == 1)
                            for j in range(5):
                                ch = NT_OWN + c - 4 + j
                                mk = None
                                if j == 0:
                                    mk = (wm0[:].unsqueeze(1).to_broadcast([128, 4, 128]), wm0)
                                elif j == 4:
                                    mk = (caus[:].unsqueeze(1).to_broadcast([128, 4, 128]), caus)

                                def sfn(ch=ch, mk=mk):
                                    return score(c, kkT[:, 1, ch * 128:(ch + 1) * 128], [kkT], None, hbias if ch < NT_OWN else c_zero, mk)

                                def pfn(P, ch=ch, j=j):
                                    for h in range(4):
                                        pv(P, h, 2, vv[:, ch, 1, :], [vv], 194, 65, j == 0, j == 4)
                                pipe(sfn, pfn)
                                if j == 0:
                                    evac_all(c, 0, 0, True, False)
                                    op("dve", lambda e: e.tensor_tensor(out=hbS[0][:, :, 65:129], in0=hbS[0][:, :, 65:129], in1=rdc[:, 0:4].unsqueeze(2).to_broadcast([128, 4, 64]), op=ALU.mult),
                                       reads=[hbS[0], rdc], writes=[hbS[0]])
                                    op("dve", lambda e: e.tensor_reduce(out=impv[:], in_=hbS[0][:, :, 65:129].rearrange("p h s -> p s h"), axis=AX.X, op=ALU.add),
                                       reads=[hbS[0]], writes=[impv])
                                    op("dve", lambda e: e.tensor_tensor(out=impv[:], in0=impv[:], in1=maskadd[:, c, :], op=ALU.add), reads=[impv, maskadd], writes=[impv])
                                    op("dve", lambda e: e.max(out=m8a[:], in_=impv[:]), reads=[impv], writes=[m8a])
                                    op("dve", lambda e: e.match_replace(out=wk[:], in_to_replace=m8a[:], in_values=impv[:], imm_value=-3.0e38),
                                       reads=[impv, m8a], writes=[wk])
                                    op("dve", lambda e: e.max(out=m8b[:], in_=wk[:]), reads=[wk], writes=[m8b])
                                    op("dve", lambda e: e.tensor_scalar(out=negm2[:, 64:128], in0=impv[:], scalar1=m8b[:, 7:8], scalar2=NEGB, op0=ALU.is_lt, op1=ALU.mult),
                                       reads=[impv, m8b, negm2], writes=[negm2])
                            flush()
                            evac_all(c, 2, 2, False, False)
                            op("pe", lambda e: e.transpose(out=tpsum[:, 256:384], in_=negm2[:, :], identity=idb[:]), reads=[negm2, idb], writes=[TPB])
                            op("act", lambda e: e.copy(out=qT[64:128, :, c * 128:(c + 1) * 128], in_=tpsum[64:128, 256:384].unsqueeze(1).to_broadcast([64, 4, 128])),
                               reads=[TPB], writes=[qT])
                            chs = list(range(NT_OWN)) + [NT_OWN + j for j in range(c + 1)]
                            for i, ch in enumerate(chs):
                                mk = None
                                if ch == NT_OWN + c:
                                    mk = (caus[:].unsqueeze(1).to_broadcast([128, 4, 128]), caus)

                                def sfn(ch=ch, mk=mk):
                                    return score(c, kkT[:, 0, ch * 128:(ch + 1) * 128], [kkT], None, hbias if ch < NT_OWN else c_zero, mk)

                                def pfn(P, ch=ch, i=i, n=len(chs)):
                                    for h in range(4):
                                        pv(P, h, 1, vv[:, ch, 0, :], [vv], 129, 65, i == 0, i == n - 1)
                                pipe(sfn, pfn)
                        flush()
                        cp = NT_OWN - 1
                        evac_all(cp, 1, 1, False, True)
                        for j in range(2):
                            op("pe", lambda e: e.transpose(out=tpsum[:, j * 128:(j + 1) * 128],
                                                           in_=yat[cp % 2][:, 2 * j:2 * j + 2, :].rearrange("p h d -> p (h d)"), identity=idb[:]),
                               reads=[yat[cp % 2], idb], writes=[TPB])
                        op("act", lambda e: e.copy(out=YT[:, 2 * g:2 * g + 2, cp * 128:(cp + 1) * 128],
                                                   in_=tpsum[:, 0:256].rearrange("p (j t) -> p j t", j=2)), reads=[TPB], writes=[YT])
            esBc.__exit__(None, None, None)
            ckpt("B")
            with fw.scope() as esCg:
                ee = fw.sb([128, NT_EXT, 4], F32, "ee", esCg)
                ff = fw.sb([128, NT_EXT, 4], F32, "ff", esCg)
                fl = fw.sb([128, NT_EXT, 4], F32, "fl", esCg)
                ghn = fw.sb([128, 512], F32, "ghn", esCg)
                fw.dma(ghn[:], g_hn_d[0:1, :].to_broadcast([128, 512]), writes=[ghn])
                wcs = fw.sb([128, 8, 4], F32, "wcs", esCg)
                fw.dma(wcs[:], wc_d[:, :, :], writes=[wcs])
                bcs = fw.sb([128, 8], F32, "bcs", esCg)
                fw.dma(bcs[:], bc_d[:, :], writes=[bcs])
                with fw.scope() as esg:
                    w_if = fw.sb([128, 8, 8], BF16, "w_if", esg)
                    fw.dma(w_if[:], w_if_d.rearrange("(k p) c -> p k c", p=128), writes=[w_if], q="pool")
                    bif = fw.sb([128, 8], F32, "bif", esg)
                    fw.dma(bif[:], b_if_d[0:1, :].to_broadcast([128, 8]), writes=[bif])
                    hblk = [fw.sb([128, 8, 512], BF16, f"hblkG{i}", esg) for i in range(2)]
                    ifp = fw.sb([128, NT_EXT, 8], F32, "ifp", esg)
                    l1 = fw.sb([128, NT_EXT, 4], F32, "l1", esg)
                    tmpg = fw.sb([128, NT_EXT, 4], F32, "tmpg", esg)
                    for t in range(NT_EXT):
                        hb = hblk[(t // 4) % 2]
                        if t % 4 == 0:
                            fw.dma(hb[:], hT_d[:, :, t * 128:(t + 4) * 128], reads=hT_tiles[t:t + 4], writes=[hb])
                        tl = t % 4
                        for k in range(8):
                            mm(PS[0], PS[0][:, t * 8:(t + 1) * 8], hb[:, k, tl * 128:(tl + 1) * 128], w_if[:, k, :], k == 0, k == 7, [hb, w_if])
                    op("act", lambda e: e.copy(out=ifp[:], in_=PS[0][:, 0:256].rearrange("p (t c) -> p t c", c=8)), reads=[PS[0]], writes=[ifp])
                    op("dve", lambda e: e.tensor_tensor(out=ifp[:], in0=ifp[:], in1=bif[:].unsqueeze(1).to_broadcast([128, NT_EXT, 8]), op=ALU.add),
                       reads=[ifp, bif], writes=[ifp])
                    op("act", lambda e: e.activation(out=l1[:], in_=ifp[:, :, 4:8], func=AF.Exp, scale=-1.0), reads=[ifp], writes=[l1])
                    op("act", lambda e: e.activation(out=l1[:], in_=l1[:], func=AF.Ln, bias=c_one[:]), reads=[l1, c_one], writes=[l1])
                    l1f = l1[:].rearrange("p t c -> p (t c)")
                    mm(PS[1], PS[1][:, 0:128], U_f[:], l1f, True, True, [U_f, l1])
                    mm(PS[1], PS[1][:, 128:256], ones_f[:], l1f, True, True, [ones_f, l1])
                    op("act", lambda e: e.copy(out=tmpg[:], in_=PS[1][:, 0:128].rearrange("p (t c) -> p t c", c=4)), reads=[PS[1]], writes=[tmpg])
                    op("act", lambda e: e.activation(out=ff[:], in_=tmpg[:], func=AF.Exp, scale=-1.0), reads=[tmpg], writes=[ff])
                    op("act", lambda e: e.activation(out=fl[:], in_=PS[1][:, 128:256].rearrange("p (t c) -> p t c", c=4), func=AF.Exp, scale=-1.0),
                       reads=[PS[1]], writes=[fl])
                    op("dve", lambda e: e.tensor_tensor(out=tmpg[:], in0=tmpg[:], in1=ifp[:, :, 0:4], op=ALU.add), reads=[tmpg, ifp], writes=[tmpg])
                    op("act", lambda e: e.activation(out=ee[:], in_=tmpg[:], func=AF.Exp), reads=[tmpg], writes=[ee])
                    op("dve", lambda e: e.tensor_scalar(out=ee[:, 0:NT_OWN, :], in0=ee[:, 0:NT_OWN, :], scalar1=hv[:, 0:1], scalar2=None, op0=ALU.mult),
                       reads=[ee, hv], writes=[ee])
                ckpt("Cg")
                for hp in range(2):
                    with fw.scope() as esH:
                        qTb = fw.sb([128, 2, S_OWN], BF16, f"qTb{hp}", esH)
                        kTb = fw.sb([128, 2, S_EXT], BF16, f"kTb{hp}", esH)
                        vaug = fw.sb([128, NT_EXT, 2, 129], BF16, f"vaug{hp}", esH)
                        osig = fw.sb([128, NT_OWN, 256], BF16, f"osig{hp}", esH)
                        op("pool", lambda e: e.memset(vaug[:, :, :, 128:129], 1.0), writes=[vaug])
                        with fw.scope() as esC1:
                            wq = fw.sb([128, 8, 256], BF16, f"wq{hp}", esC1)
                            wk = fw.sb([128, 8, 256], BF16, f"wk{hp}", esC1)
                            wv = fw.sb([128, 8, 256], BF16, f"wv{hp}", esC1)
                            wo = fw.sb([128, 8, 256], BF16, f"wo{hp}", esC1)
                            fw.dma(wq[:], w_qk_d[:, hp * 256:(hp + 1) * 256].rearrange("(k p) c -> p k c", p=128), writes=[wq], q="pool")
                            fw.dma(wk[:], w_qk_d[:, 512 + hp * 256:512 + (hp + 1) * 256].rearrange("(k p) c -> p k c", p=128), writes=[wk], q="pool")
                            fw.dma(wv[:], w_vo_d[:, hp * 256:(hp + 1) * 256].rearrange("(k p) c -> p k c", p=128), writes=[wv], q="pool")
                            fw.dma(wo[:], w_vo_d[:, 512 + hp * 256:512 + (hp + 1) * 256].rearrange("(k p) c -> p k c", p=128), writes=[wo], q="pool")
                            hblk = [fw.sb([128, 8, 512], BF16, f"hblkC{hp}{i}", esC1) for i in range(2)]
                            uk = [fw.sb([128, 4 + S_EXT], BF16, f"uk{hp}{i}", esC1) for i in range(2)]
                            uq = [fw.sb([128, 4 + 2560], BF16, f"uq{hp}{i}", esC1) for i in range(2)]
                            ycv = [fw.sb([128, 512], F32, f"ycv{hp}{i}", esC1) for i in range(2)]
                            sgm = [fw.sb([128, 512], F32, f"sgm{hp}{i}", esC1) for i in range(2)]
                            for hh in range(2):
                                op("pool", lambda e: e.memset(uk[hh][:, 0:4], 0.0), writes=[uk[hh]])
                                op("pool", lambda e: e.memset(uq[hh][:, 0:4], 0.0), writes=[uq[hh]])
                            for blk in range(8):
                                hb = hblk[blk % 2]
                                fw.dma(hb[:], hT_d[:, :, blk * 512:(blk + 1) * 512], reads=hT_tiles[4 * blk:4 * blk + 4], writes=[hb])
                                for hh in range(2):
                                    for k in range(8):
                                        mm(PS[hh], PS[hh][:, :], wk[:, k, hh * 128:(hh + 1) * 128], hb[:, k, :], k == 0, k == 7, [wk, hb])
                                    op("act", lambda e: e.copy(out=uk[hh][:, 4 + blk * 512:4 + (blk + 1) * 512], in_=PS[hh][:, :]), reads=[PS[hh]], writes=[uk[hh]])
                                if blk >= 3:
                                    for hh in range(2):
                                        for k in range(8):
                                            mm(PS[2 + hh], PS[2 + hh][:, :], wq[:, k, hh * 128:(hh + 1) * 128], hb[:, k, :], k == 0, k == 7, [wq, hb])
                                        op("act", lambda e: e.copy(out=uq[hh][:, 4 + (blk - 3) * 512:4 + (blk - 2) * 512], in_=PS[2 + hh][:, :]),
                                           reads=[PS[2 + hh]], writes=[uq[hh]])
                                for tl in range(4):
                                    t = blk * 4 + tl
                                    bv = 4 + tl % 2
                                    for k in range(8):
                                        mm(PS[bv], PS[bv][:, 0:256], hb[:, k, tl * 128:(tl + 1) * 128], wv[:, k, :], k == 0, k == 7, [wv, hb])
                                    op("dve", lambda e: e.tensor_copy(out=vaug[:, t, :, 0:128], in_=PS[bv][:, 0:256].rearrange("p (h d) -> p h d", d=128)),
                                       reads=[PS[bv]], writes=[vaug])
                                    if blk >= 4:
                                        bo = 6 + tl % 2
                                        for k in range(8):
                                            mm(PS[bo], PS[bo][:, 0:256], hb[:, k, tl * 128:(tl + 1) * 128], wo[:, k, :], k == 0, k == 7, [wo, hb])
                                        op("act", lambda e: e.activation(out=osig[:, t - NT_OWN, :], in_=PS[bo][:, 0:256], func=AF.Sigmoid),
                                           reads=[PS[bo]], writes=[osig])
                            pi = 0
                            for hh in range(2):
                                H = 2 * hp + hh
                                for typ in range(2):
                                    ci = typ * 4 + H
                                    npiece = 4 if typ == 0 else 8
                                    u = uq[hh] if typ == 0 else uk[hh]
                                    for pc in range(npiece):
                                        off = (4 + 512 + pc * 512) if typ == 0 else (4 + pc * 512)
                                        y_ = ycv[pi % 2]
                                        s_ = sgm[pi % 2]
                                        pi += 1
                                        op("dve", lambda e: e.tensor_scalar(out=y_[:], in0=u[:, off - 3:off - 3 + 512], scalar1=wcs[:, ci, 0:1], scalar2=bcs[:, ci:ci + 1],
                                                                            op0=ALU.mult, op1=ALU.add), reads=[u, wcs, bcs], writes=[y_])
                                        for j in range(1, 4):
                                            op("dve", lambda e: e.scalar_tensor_tensor(out=y_[:], in0=u[:, off - 3 + j:off - 3 + j + 512], scalar=wcs[:, ci, j:j + 1], in1=y_[:],
                                                                                       op0=ALU.mult, op1=ALU.add), reads=[u, wcs, y_], writes=[y_])
                                        if typ == 0:
                                            op("act", lambda e: e.activation(out=qTb[:, hh, pc * 512:(pc + 1) * 512], in_=y_[:], func=AF.Silu), reads=[y_], writes=[qTb])
                                        else:
                                            op("act", lambda e: e.activation(out=s_[:], in_=y_[:], func=AF.Sigmoid), reads=[y_], writes=[s_])
                                            op("dve", lambda e: e.scalar_tensor_tensor(out=kTb[:, hh, pc * 512:(pc + 1) * 512], in0=y_[:], scalar=128.0 ** -0.5, in1=s_[:],
                                                                                       op0=ALU.mult, op1=ALU.mult), reads=[y_, s_], writes=[kTb])
                        ckpt("C1")
                        with fw.scope() as esC3:
                            ktokA = fw.sb([128, NT_EXT, 2, 128], BF16, f"ktokA{hp}", esC3)
                            CTall = fw.sb([128, NT_EXT, 2, 129], BF16, f"CTall{hp}", esC3)
                            Xs = [fw.sb([128, 129], F32, f"Xs{hp}{hh}", esC3) for hh in range(2)]
                            Sm = [[fw.sb([128, 128], BF16, f"Sm{hp}{hh}{i}", esC3) for i in range(2)] for hh in range(2)]
                            hm_ = [fw.sb([128, 128], F32, f"hm{hp}{hh}", esC3) for hh in range(2)]
                            yb_ = [fw.sb([128, 128], BF16, f"yb{hp}{hh}", esC3) for hh in range(2)]
                            jk = [fw.sb([128, 128], BF16, f"jk{hp}{hh}", esC3) for hh in range(2)]
                            smc = [fw.sb([128, 8], F32, f"smc{hp}{hh}", esC3) for hh in range(2)]
                            op("pool", lambda e: e.memset(CTall[:, 0, :, :], 0.0), writes=[CTall])
                            for hh in range(2):
                                H = 2 * hp + hh
                                op("dve", lambda e: e.tensor_tensor(out=vaug[:, :, hh, :], in0=vaug[:, :, hh, :],
                                                                    in1=ee[:, :, H:H + 1].to_broadcast([128, NT_EXT, 129]), op=ALU.mult), reads=[vaug, ee], writes=[vaug])
                            grp = 0
                            items = [(t, hh) for t in range(NT_EXT - 1) for hh in range(2)]
                            for i0 in range(0, len(items), 8):
                                bk = grp % 2
                                grp += 1
                                sub = items[i0:i0 + 8]
                                for j, (t, hh) in enumerate(sub):
                                    op("pe", lambda e: e.transpose(out=psbf(bk)[:, j * 128:(j + 1) * 128], in_=kTb[:, hh, t * 128:(t + 1) * 128], identity=idb[:]),
                                       reads=[kTb, idb], writes=[PS[bk]])
                                t0_ = sub[0][0]
                                nt_ = len(sub) // 2
                                op("act", lambda e: e.copy(out=ktokA[:, t0_:t0_ + nt_, :, :].rearrange("p t h d -> p (t h) d"),
                                                           in_=psbf(bk)[:, 0:len(sub) * 128].rearrange("p (j d) -> p j d", d=128)), reads=[PS[bk]], writes=[ktokA])
                            for t in range(NT_EXT - 1):
                                for hh in range(2):
                                    H = 2 * hp + hh
                                    bU = 2 + 2 * hh + (t % 2)
                                    mm(PS[bU], PS[bU][:, 0:129], ktokA[:, t, hh, :], vaug[:, t, hh, :], True, True, [ktokA, vaug])
                                    if t == 0:
                                        op("dve", lambda e: e.tensor_copy(out=Xs[hh][:], in_=PS[bU][:, 0:129]), reads=[PS[bU]], writes=[Xs[hh]])
                                    else:
                                        op("dve", lambda e: e.scalar_tensor_tensor(out=Xs[hh][:], in0=Xs[hh][:], scalar=fl[:, t - 1, H:H + 1], in1=PS[bU][:, 0:129],
                                                                                   op0=ALU.mult, op1=ALU.add), reads=[Xs[hh], fl, PS[bU]], writes=[Xs[hh]])
                                    op("act", lambda e: e.activation(out=CTall[:, t + 1, hh, :], in_=Xs[hh][:], func=AF.Copy, scale=fl[:, t, H:H + 1]),
                                       reads=[Xs[hh], fl], writes=[CTall])
                            pendC = [None]

                            def flushC():
                                if pendC[0] is not None:
                                    pendC[0]()
                                    pendC[0] = None

                            oi = 0
                            for t in range(NT_OWN, NT_EXT):
                                for hh in range(2):
                                    H = 2 * hp + hh
                                    tq = t - NT_OWN
                                    bS = 2 * hh + (oi // 2) % 2
                                    sm_ = Sm[hh][(oi // 2) % 2]
                                    oi += 1
                                    mm(PS[bS], PS[bS][:, 0:128], kTb[:, hh, t * 128:(t + 1) * 128], qTb[:, hh, tq * 128:(tq + 1) * 128], True, True, [kTb, qTb])
                                    op("dve", lambda e: e.tensor_tensor(out=sm_[:], in0=PS[bS][:, 0:128], in1=caus[:], op=ALU.mult), reads=[PS[bS], caus], writes=[sm_])
                                    flushC()

                                    def post(t=t, hh=hh, H=H, tq=tq, sm_=sm_):
                                        bA = 4 + hh
                                        bT = 6 + hh
                                        sc = smc[hh]
                                        mm(PS[bA], PS[bA][:, 0:129], sm_[:], vaug[:, t, hh, :], True, False, [sm_, vaug])
                                        mm(PS[bA], PS[bA][:, 0:129], qTb[:, hh, tq * 128:(tq + 1) * 128], CTall[:, t, hh, :], False, True, [qTb, CTall])
                                        fcol = ff[:, t, H:H + 1]
                                        op("act", lambda e: e.activation(out=sc[:, 6:7], in_=PS[bA][:, 128:129], func=AF.Abs, scale=fcol), reads=[PS[bA], ff], writes=[sc])
                                        op("dve", lambda e: e.tensor_scalar(out=sc[:, 0:1], in0=sc[:, 6:7], scalar1=1.0, scalar2=None, op0=ALU.max), reads=[sc], writes=[sc])
                                        op("dve", lambda e: e.reciprocal(out=sc[:, 1:2], in_=sc[:, 0:1]), reads=[sc], writes=[sc])
                                        op("dve", lambda e: e.tensor_tensor(out=sc[:, 2:3], in0=sc[:, 1:2], in1=fcol, op=ALU.mult), reads=[sc, ff], writes=[sc])
                                        op("dve", lambda e: e.scalar_tensor_tensor(out=hm_[hh][:], in0=PS[bA][:, 0:128], scalar=sc[:, 2:3], in1=osig[:, tq, hh * 128:(hh + 1) * 128],
                                                                                   op0=ALU.mult, op1=ALU.mult), reads=[PS[bA], sc, osig], writes=[hm_[hh]])
                                        op("act", lambda e: e.activation(out=jk[hh][:], in_=hm_[hh][:], func=AF.Square, accum_out=sc[:, 3:4]), reads=[hm_[hh]], writes=[jk[hh], sc])
                                        op("act", lambda e: e.activation(out=sc[:, 4:5], in_=sc[:, 3:4], func=AF.Sqrt, bias=c_eps[:], scale=1.0 / 128), reads=[sc, c_eps], writes=[sc])
                                        op("dve", lambda e: e.reciprocal(out=sc[:, 5:6], in_=sc[:, 4:5]), reads=[sc], writes=[sc])
                                        op("dve", lambda e: e.scalar_tensor_tensor(out=yb_[hh][:], in0=hm_[hh][:], scalar=sc[:, 5:6], in1=ghn[:, H * 128:(H + 1) * 128],
                                                                                   op0=ALU.mult, op1=ALU.mult), reads=[hm_[hh], sc, ghn], writes=[yb_[hh]])
                                        op("pe", lambda e: e.transpose(out=psbf(bT)[:, 0:128], in_=yb_[hh][:], identity=idb[:]), reads=[yb_[hh], idb], writes=[PS[bT]])
                                        op("act", lambda e: e.copy(out=YT[:, 4 + H, tq * 128:(tq + 1) * 128], in_=psbf(bT)[:, 0:128]), reads=[PS[bT]], writes=[YT])
                                    pendC[0] = post
                            flushC()
            ckpt("C")
            if "ybT" in dbg:
                o = dbg_t("ybT", [128, 4, S_OWN], BF16)
                fw.dma(o[:, :, :], YT[:, 4:8, :], reads=[YT], is_output=True)


            with fw.scope() as esD:
                x1 = fw.sb([128, NT_OWN, D], F32, "x1", esD)
                with fw.scope() as esD1:
                    mixT = fw.sb([128, 8, S_OWN], BF16, "mixT", esD1)
                    with fw.scope() as esD1a:
                        hTo = fw.sb([128, 8, S_OWN], BF16, "hTo", esD1a)
                        for tb in range(4):
                            fw.dma(hTo[:, :, tb * 512:(tb + 1) * 512], hT_d[:, :, S_OWN + tb * 512:S_OWN + (tb + 1) * 512],
                                   reads=hT_tiles[NT_OWN + 4 * tb:NT_OWN + 4 * tb + 4], writes=[hTo])
                        wga = [fw.sb([128, 8, 128], BF16, f"wga{i}", esD1a) for i in range(2)]
                        wgb = [fw.sb([128, 8, 128], BF16, f"wgb{i}", esD1a) for i in range(2)]
                        wpa = [fw.sb([128, 4, 128], BF16, f"wpa{i}", esD1a) for i in range(2)]
                        wpb = [fw.sb([128, 4, 128], BF16, f"wpb{i}", esD1a) for i in range(2)]
                        sga = [fw.sb([128, 512], BF16, f"sga{i}", esD1a) for i in range(2)]
                        sgb = [fw.sb([128, 512], BF16, f"sgb{i}", esD1a) for i in range(2)]
                        t1 = [fw.sb([128, 512], F32, f"t1_{i}", esD1a) for i in range(2)]
                        t2 = [fw.sb([128, 512], F32, f"t2_{i}", esD1a) for i in range(2)]
                        it = 0
                        for j in range(8):
                            w_ = j % 2
                            fw.dma(wga[w_][:], w_mg_d[:, j * 128:(j + 1) * 128].rearrange("(k p) c -> p k c", p=128), writes=[wga[w_]], q="pool")
                            fw.dma(wgb[w_][:], w_mg_d[:, 1024 + j * 128:1024 + (j + 1) * 128].rearrange("(k p) c -> p k c", p=128), writes=[wgb[w_]], q="pool")
                            fw.dma(wpa[w_][:], w_pa_d[:, j * 128:(j + 1) * 128].rearrange("(k p) c -> p k c", p=128), writes=[wpa[w_]], q="pool")
                            fw.dma(wpb[w_][:], w_pb_d[:, j * 128:(j + 1) * 128].rearrange("(k p) c -> p k c", p=128), writes=[wpb[w_]], q="pool")
                            for tb in range(4):
                                r = it % 2
                                it += 1
                                b0 = 4 * r
                                ts_ = slice(tb * 512, (tb + 1) * 512)
                                for k in range(8):
                                    mm(PS[b0], PS[b0][:, :], wga[w_][:, k, :], hTo[:, k, ts_], k == 0, k == 7, [wga[w_], hTo])
                                op("act", lambda e: e.activation(out=sga[r][:], in_=PS[b0][:, :], func=AF.Sigmoid), reads=[PS[b0]], writes=[sga[r]])
                                for k in range(8):
                                    mm(PS[b0 + 1], PS[b0 + 1][:, :], wgb[w_][:, k, :], hTo[:, k, ts_], k == 0, k == 7, [wgb[w_], hTo])
                                op("act", lambda e: e.activation(out=sgb[r][:], in_=PS[b0 + 1][:, :], func=AF.Sigmoid), reads=[PS[b0 + 1]], writes=[sgb[r]])
                                for k in range(4):
                                    mm(PS[b0 + 2], PS[b0 + 2][:, :], wpa[w_][:, k, :], YT[:, k, ts_], k == 0, k == 3, [wpa[w_], YT])
                                for k in range(4):
                                    mm(PS[b0 + 3], PS[b0 + 3][:, :], wpb[w_][:, k, :], YT[:, 4 + k, ts_], k == 0, k == 3, [wpb[w_], YT])
                                op("dve", lambda e: e.tensor_tensor(out=t1[r][:], in0=PS[b0 + 2][:, :], in1=sga[r][:], op=ALU.mult), reads=[PS[b0 + 2], sga[r]], writes=[t1[r]])
                                op("dve", lambda e: e.tensor_tensor(out=t2[r][:], in0=PS[b0 + 3][:, :], in1=sgb[r][:], op=ALU.mult), reads=[PS[b0 + 3], sgb[r]], writes=[t2[r]])
                                op("pool", lambda e: e.tensor_tensor(out=mixT[:, j, ts_], in0=t1[r][:], in1=t2[r][:], op=ALU.add), reads=[t1[r], t2[r]], writes=[mixT])
                    ckpt("D1a")
                    with fw.scope() as esD1b:
                        w_out = fw.sb([128, 8, D], BF16, "w_out", esD1b)
                        fw.dma(w_out[:], w_out_d.rearrange("(k p) c -> p k c", p=128), writes=[w_out], q="pool")
                        xtl = [fw.sb([128, D], F32, f"xtl{i}", esD1b) for i in range(2)]
                        for t in range(NT_OWN):
                            x_ = xtl[t % 2]
                            fw.dma(x_[:], xe[S_OWN + t * 128:S_OWN + (t + 1) * 128, :], writes=[x_])
                            for half in range(2):
                                b = 2 * (t % 2) + half
                                for j in range(8):
                                    mm(PS[b], PS[b][:, :], mixT[:, j, t * 128:(t + 1) * 128], w_out[:, j, half * 512:(half + 1) * 512], j == 0, j == 7, [mixT, w_out])
                                op("dve", lambda e: e.tensor_tensor(out=x1[:, t, half * 512:(half + 1) * 512], in0=PS[b][:, :], in1=x_[:, half * 512:(half + 1) * 512], op=ALU.add),
                                   reads=[PS[b], x_], writes=[x1])
                ckpt("D1")
                if "x1" in dbg:
                    fw.dma(dbg_t("x1", [128, NT_OWN, D]), x1[:], reads=[x1], is_output=True)
                with fw.scope() as esM:
                    load_gain(1)
                    gateT = fw.sb([16, S_OWN], BF16, "gateT", esM)
                    E16 = fw.sb([16, 16, 128], BF16, "E16", esM)
                    op("pool", lambda e: e.memset(E16[:], 1.0), writes=[E16])
                    op("pool", lambda e: e.affine_select(out=E16[:], in_=E16[:], pattern=[[-1, 16], [0, 128]], compare_op=ALU.is_equal, fill=0.0,
                                                         base=0, channel_multiplier=1), reads=[E16], writes=[E16])
                    with fw.scope() as esR:
                        w_r = fw.sb([128, 8, 20], F32, "w_r", esR)
                        fw.dma(w_r[:], w_r_d.rearrange("(k p) c -> p k c", p=128), writes=[w_r])
                        b_r = fw.sb([128, 20], F32, "b_r", esR)
                        fw.dma(b_r[:], b_r_d[0:1, :].to_broadcast([128, 20]), writes=[b_r])
                        hnf = [fw.sb([128, D], F32, f"hnf{i}", esR) for i in range(2)]
                        hnTf = [fw.sb([128, 8, 128], F32, f"hnTf{i}", esR) for i in range(2)]
                        junkR = fw.sb([128, D], BF16, "junkR", esR)
                        ssr = [fw.sb([128, 1], F32, f"ssr{i}", esR) for i in range(2)]
                        rrr = [fw.sb([128, 1], F32, f"rrr{i}", esR) for i in range(2)]
                        T_ = NT_OWN
                        lgA = fw.sb([128, T_, 20], F32, "lgA", esR)

                        def r_front(t):
                            r = t % 2
                            rs = {"ss": ssr[r], "r": rrr[r]}
                            rms_rstd({"ap": x1[:, t, :], "bufs": [x1]}, rs, D, {"ap": junkR[:], "buf": junkR})
                            op("dve", lambda e: e.scalar_tensor_tensor(out=hnf[r][:], in0=x1[:, t, :], scalar=rs["r"][:], in1=gB[:], op0=ALU.mult, op1=ALU.mult),
                               reads=[x1, rs["r"], gB], writes=[hnf[r]])
                            for k in range(8):
                                b = 2 * r + (0 if k < 4 else 1)
                                op("pe", lambda e: e.transpose(out=PS[b][:, (k % 4) * 128:(k % 4 + 1) * 128], in_=hnf[r][:, k * 128:(k + 1) * 128], identity=idf[:]),
                                   reads=[hnf[r], idf], writes=[PS[b]])
                            for bb in range(2):
                                b = 2 * r + bb
                                op("act", lambda e: e.copy(out=hnTf[r][:, 4 * bb:4 * bb + 4, :], in_=PS[b][:, :].rearrange("p (k t) -> p k t", k=4)), reads=[PS[b]], writes=[hnTf[r]])
                                op("dve", lambda e: e.tensor_copy(out=YT[:, 4 * bb:4 * bb + 4, t * 128:(t + 1) * 128], in_=PS[b][:, :].rearrange("p (k t) -> p k t", k=4)),
                                   reads=[PS[b]], writes=[YT])

                        def r_back(t):
                            r = t % 2
                            bl = 4 + r
                            for k in range(8):
                                mm(PS[bl], PS[bl][:, 0:20], hnTf[r][:, k, :], w_r[:, k, :], k == 0, k == 7, [hnTf[r], w_r])
                            op("dve", lambda e: e.tensor_tensor(out=lgA[:, t, :], in0=PS[bl][:, 0:20], in1=b_r[:], op=ALU.add), reads=[PS[bl], b_r], writes=[lgA])

                        for t in range(T_ + 1):
                            if t < T_:
                                r_front(t)
                            if t >= 1:
                                r_back(t - 1)
                        gl = lgA[:, :, 0:4]
                        el = lgA[:, :, 4:20].rearrange("p t (g e) -> p t g e", g=4)
                        gmax = fw.sb([128, T_], F32, "gmax", esR)
                        g1h = fw.sb([128, T_, 4], F32, "g1h", esR)
                        exg = fw.sb([128, T_, 4], F32, "exg", esR)
                        pgs = fw.sb([128, T_], F32, "pgs", esR)
                        t16 = fw.sb([128, T_, 4, 4], F32, "t16", esR)
                        elg = fw.sb([128, T_, 4], F32, "elg", esR)
                        elg2 = fw.sb([128, T_, 4], F32, "elg2", esR)
                        ev1 = fw.sb([128, T_], F32, "ev1", esR)
                        ev2 = fw.sb([128, T_], F32, "ev2", esR)
                        mk1 = fw.sb([128, T_, 4], F32, "mk1", esR)
                        mk2 = fw.sb([128, T_, 4], F32, "mk2", esR)
                        w12 = fw.sb([128, 2, T_], F32, "w12", esR)
                        gig = fw.sb([128, T_, 4], F32, "gig", esR)
                        gate = fw.sb([128, T_, 4, 4], F32, "gate", esR)
                        B3 = [128, T_, 4]
                        op("dve", lambda e: e.tensor_reduce(out=gmax[:], in_=gl, axis=AX.X, op=ALU.max), reads=[lgA], writes=[gmax])
                        op("dve", lambda e: e.tensor_tensor(out=g1h[:], in0=gl, in1=gmax[:].unsqueeze(2).to_broadcast(B3), op=ALU.is_equal), reads=[lgA, gmax], writes=[g1h])
                        op("dve", lambda e: e.tensor_tensor(out=exg[:], in0=gl, in1=gmax[:].unsqueeze(2).to_broadcast(B3), op=ALU.subtract), reads=[lgA, gmax], writes=[exg])
                        op("act", lambda e: e.activation(out=exg[:], in_=exg[:], func=AF.Exp), reads=[exg], writes=[exg])
                        op("dve", lambda e: e.tensor_reduce(out=pgs[:], in_=exg[:], axis=AX.X, op=ALU.add), reads=[exg], writes=[pgs])
                        op("dve", lambda e: e.reciprocal(out=pgs[:], in_=pgs[:]), reads=[pgs], writes=[pgs])
                        op("dve", lambda e: e.tensor_tensor(out=t16[:], in0=el, in1=g1h[:].unsqueeze(3).to_broadcast([128, T_, 4, 4]), op=ALU.mult), reads=[lgA, g1h], writes=[t16])
                        op("dve", lambda e: e.tensor_reduce(out=elg[:], in_=t16[:].rearrange("p t g e -> p t e g"), axis=AX.X, op=ALU.add), reads=[t16], writes=[elg])
                        op("dve", lambda e: e.tensor_reduce(out=ev1[:], in_=elg[:], axis=AX.X, op=ALU.max), reads=[elg], writes=[ev1])
                        op("dve", lambda e: e.tensor_tensor(out=mk1[:], in0=elg[:], in1=ev1[:].unsqueeze(2).to_broadcast(B3), op=ALU.is_equal), reads=[elg, ev1], writes=[mk1])
                        op("dve", lambda e: e.scalar_tensor_tensor(out=elg2[:], in0=mk1[:], scalar=-1e30, in1=elg[:], op0=ALU.mult, op1=ALU.add), reads=[mk1, elg], writes=[elg2])
                        op("dve", lambda e: e.tensor_reduce(out=ev2[:], in_=elg2[:], axis=AX.X, op=ALU.max), reads=[elg2], writes=[ev2])
                        op("dve", lambda e: e.tensor_tensor(out=mk2[:], in0=elg2[:], in1=ev2[:].unsqueeze(2).to_broadcast(B3), op=ALU.is_equal), reads=[elg2, ev2], writes=[mk2])
                        op("dve", lambda e: e.tensor_tensor(out=w12[:, 0, :], in0=ev1[:], in1=ev2[:], op=ALU.subtract), reads=[ev1, ev2], writes=[w12])
                        op("act", lambda e: e.activation(out=w12[:, 0, :], in_=w12[:, 0, :], func=AF.Sigmoid), reads=[w12], writes=[w12])
                        op("dve", lambda e: e.tensor_scalar(out=w12[:, 1, :], in0=w12[:, 0, :], scalar1=-1.0, scalar2=1.0, op0=ALU.mult, op1=ALU.add), reads=[w12], writes=[w12])
                        op("dve", lambda e: e.tensor_tensor(out=w12[:], in0=w12[:], in1=pgs[:].unsqueeze(1).to_broadcast([128, 2, T_]), op=ALU.mult), reads=[w12, pgs], writes=[w12])
                        op("dve", lambda e: e.tensor_tensor(out=gig[:], in0=mk1[:], in1=w12[:, 0, :].unsqueeze(2).to_broadcast(B3), op=ALU.mult), reads=[mk1, w12], writes=[gig])
                        op("dve", lambda e: e.tensor_tensor(out=mk2[:], in0=mk2[:], in1=w12[:, 1, :].unsqueeze(2).to_broadcast(B3), op=ALU.mult), reads=[mk2, w12], writes=[mk2])
                        op("dve", lambda e: e.tensor_tensor(out=gig[:], in0=gig[:], in1=mk2[:], op=ALU.add), reads=[gig, mk2], writes=[gig])
                        op("dve", lambda e: e.tensor_tensor(out=gate[:], in0=g1h[:].unsqueeze(3).to_broadcast([128, T_, 4, 4]),
                                                            in1=gig[:].unsqueeze(2).to_broadcast([128, T_, 4, 4]), op=ALU.mult), reads=[g1h, gig], writes=[gate])
                        for t4 in range(T_ // 4):
                            bk = 6 + t4 % 2
                            for j in range(4):
                                t = t4 * 4 + j
                                op("pe", lambda e: e.transpose(out=PS[bk][0:16, j * 128:(j + 1) * 128], in_=gate[:, t, :, :].rearrange("p g e -> p (g e)"), identity=idf[:]),
                                   reads=[gate, idf], writes=[PS[bk]])
                            op("act", lambda e: e.copy(out=gateT[:, t4 * 512:(t4 + 1) * 512], in_=PS[bk][0:16, :]), reads=[PS[bk]], writes=[gateT])
                    ckpt("D2r")
                    if "gateT" in dbg:
                        fw.dma(dbg_t("gateT", [16, S_OWN], BF16), gateT[:], reads=[gateT], is_output=True)
                    with fw.scope() as esE:
                        w13 = [fw.sb([128, 8, 512], BF16, f"w13_{i}", esE) for i in range(2)]
                        w2e = [fw.sb([128, 2, D], BF16, f"w2e_{i}", esE) for i in range(2)]
                        sgE = [fw.sb([128, 512], F32, f"sgE{i}", esE) for i in range(2)]
                        tE = [fw.sb([128, 512], F32, f"tE{i}", esE) for i in range(2)]
                        actT = [[fw.sb([128, 512], BF16, f"actT{i}{fc}", esE) for fc in range(2)] for i in range(2)]
                        ybank = [4, 5, 7]
                        yi = 0
                        it = 0
                        for ex in range(16):
                            wb = ex % 2
                            fw.dma(w13[wb][:], w_e13_d[ex].rearrange("(k p) c -> p k c", p=128), writes=[w13[wb]], q="pool")
                            fw.dma(w2e[wb][:], w_e2_d[ex].rearrange("(k p) c -> p k c", p=128), writes=[w2e[wb]], q="pool")
                            for tb in range(4):
                                r = it % 2
                                it += 1
                                ts_ = slice(tb * 512, (tb + 1) * 512)
                                mm(PS[6], PS[6][:, :], E16[:, ex, :], gateT[:, ts_], True, True, [E16, gateT])
                                for fc in range(2):
                                    for k in range(8):
                                        mm(PS[fc], PS[fc][:, :], w13[wb][:, k, fc * 128:(fc + 1) * 128], YT[:, k, ts_], k == 0, k == 7, [w13[wb], YT])
                                    for k in range(8):
                                        mm(PS[2 + fc], PS[2 + fc][:, :], w13[wb][:, k, 256 + fc * 128:256 + (fc + 1) * 128], YT[:, k, ts_], k == 0, k == 7, [w13[wb], YT])
                                    op("act", lambda e: e.activation(out=sgE[fc][:], in_=PS[fc][:, :], func=AF.Silu), reads=[PS[fc]], writes=[sgE[fc]])
                                    op("dve", lambda e: e.tensor_tensor(out=tE[fc][:], in0=PS[2 + fc][:, :], in1=sgE[fc][:], op=ALU.mult), reads=[PS[2 + fc], sgE[fc]], writes=[tE[fc]])
                                    op("dve", lambda e: e.tensor_tensor(out=actT[r][fc][:], in0=PS[6][:, :], in1=tE[fc][:], op=ALU.mult), reads=[PS[6], tE[fc]], writes=[actT[r][fc]])
                                for tt in range(4):
                                    t = tb * 4 + tt
                                    for half in range(2):
                                        b = ybank[yi % 3]
                                        yi += 1
                                        for fc in range(2):
                                            mm(PS[b], PS[b][:, :], actT[r][fc][:, tt * 128:(tt + 1) * 128], w2e[wb][:, fc, half * 512:(half + 1) * 512], fc == 0, fc == 1, [actT[r][fc], w2e[wb]])
                                        op("dve", lambda e: e.tensor_tensor(out=x1[:, t, half * 512:(half + 1) * 512], in0=PS[b][:, :], in1=x1[:, t, half * 512:(half + 1) * 512], op=ALU.add),
                                           reads=[PS[b], x1], writes=[x1])
                ckpt("D2")
                if "x2" in dbg:
                    fw.dma(dbg_t("x2", [128, NT_OWN, D]), x1[:], reads=[x1], is_output=True)
                with fw.scope() as esP:
                    load_gain(2)
                    gB2 = fw.sb([128, D], F32, "gB2", esP)
                    fw.dma(gB2[:], gvec_d[3:4, :].to_broadcast([128, D]), writes=[gB2])
                    w_pg = fw.sb([128, 8, D], BF16, "w_pg", esP)
                    fw.dma(w_pg[:], w_pg_d.rearrange("(k p) c -> p k c", p=128), writes=[w_pg], q="pool")
                    w_pp = fw.sb([128, 2, D], BF16, "w_pp", esP)
                    fw.dma(w_pp[:], w_pp_d.rearrange("(k p) c -> p k c", p=128), writes=[w_pp], q="pool")
                    hpb = [fw.sb([128, D], BF16, f"hpb{i}", esP) for i in range(2)]
                    hpT = [fw.sb([128, 8, 128], BF16, f"hpT{i}", esP) for i in range(2)]
                    plb = [fw.sb([128, 256], BF16, f"plb{i}", esP) for i in range(2)]
                    plT = [fw.sb([128, 2, 128], BF16, f"plT{i}", esP) for i in range(2)]
                    sgP = [fw.sb([128, 512], F32, f"sgP{i}", esP) for i in range(2)]
                    tP = [fw.sb([128, 512], F32, f"tP{i}", esP) for i in range(2)]
                    outt = [fw.sb([128, D], F32, f"outt{i}", esP) for i in range(2)]
                    junkP = fw.sb([128, D], BF16, "junkP", esP)
                    ssp = [fw.sb([128, 1], F32, f"ssp{i}", esP) for i in range(4)]
                    rrp = [fw.sb([128, 1], F32, f"rrp{i}", esP) for i in range(4)]
                    for t in range(NT_OWN):
                        r = t % 2
                        fw.dma(plb[r][:], pl_d[t * 128:(t + 1) * 128, :], writes=[plb[r]], q="pool")
                        rs = {"ss": ssp[r], "r": rrp[r]}
                        rms_rstd({"ap": x1[:, t, :], "bufs": [x1]}, rs, D, {"ap": junkP[:], "buf": junkP})
                        op("dve", lambda e: e.scalar_tensor_tensor(out=hpb[r][:], in0=x1[:, t, :], scalar=rs["r"][:], in1=gB[:], op0=ALU.mult, op1=ALU.mult),
                           reads=[x1, rs["r"], gB], writes=[hpb[r]])
                        for k in range(8):
                            op("pe", lambda e: e.transpose(out=psbf(0)[:, k * 128:(k + 1) * 128], in_=hpb[r][:, k * 128:(k + 1) * 128], identity=idb[:]), reads=[hpb[r], idb], writes=[PS[0]])
                        op("act", lambda e: e.copy(out=hpT[r][:], in_=psbf(0).rearrange("p (k t) -> p k t", k=8)), reads=[PS[0]], writes=[hpT[r]])
                        for k in range(2):
                            op("pe", lambda e: e.transpose(out=psbf(1)[:, k * 128:(k + 1) * 128], in_=plb[r][:, k * 128:(k + 1) * 128], identity=idb[:]), reads=[plb[r], idb], writes=[PS[1]])
                        op("act", lambda e: e.copy(out=plT[r][:], in_=psbf(1)[:, 0:256].rearrange("p (k t) -> p k t", k=2)), reads=[PS[1]], writes=[plT[r]])
                        for half in range(2):
                            hs = slice(half * 512, (half + 1) * 512)
                            bG = 2 + half
                            bP = 4 + half
                            for k in range(8):
                                mm(PS[bG], PS[bG][:, :], hpT[r][:, k, :], w_pg[:, k, hs], k == 0, k == 7, [hpT[r], w_pg])
                            for k in range(2):
                                mm(PS[bP], PS[bP][:, :], plT[r][:, k, :], w_pp[:, k, hs], k == 0, k == 1, [plT[r], w_pp])
                            op("act", lambda e: e.activation(out=sgP[half][:], in_=PS[bG][:, :], func=AF.Sigmoid), reads=[PS[bG]], writes=[sgP[half]])
                            op("dve", lambda e: e.tensor_tensor(out=tP[half][:], in0=PS[bP][:, :], in1=sgP[half][:], op=ALU.mult), reads=[PS[bP], sgP[half]], writes=[tP[half]])
                            op("dve", lambda e: e.tensor_tensor(out=x1[:, t, hs], in0=x1[:, t, hs], in1=tP[half][:], op=ALU.add), reads=[x1, tP[half]], writes=[x1])
                        rs2 = {"ss": ssp[2 + r], "r": rrp[2 + r]}
                        rms_rstd({"ap": x1[:, t, :], "bufs": [x1]}, rs2, D, {"ap": junkP[:], "buf": junkP})
                        op("dve", lambda e: e.scalar_tensor_tensor(out=outt[r][:], in0=x1[:, t, :], scalar=rs2["r"][:], in1=gB2[:], op0=ALU.mult, op1=ALU.mult),
                           reads=[x1, rs2["r"], gB2], writes=[outt[r]])
                        fw.dma(out_d[t * 128:(t + 1) * 128, :], outt[r][:], reads=[outt[r]], is_output=True)

            if "yaT" in dbg:
                o = dbg_t("yaT", [128, 4, S_OWN], BF16)
                fw.dma(o[:, :, :], YT[:, 0:4, :], reads=[YT], is_output=True)

            if "hT" in dbg:
                o = dbg_t("hT", [128, 8, S_EXT], BF16)
                with fw.scope() as esd:
                    tmp = fw.sb([128, 8, 512], BF16, "dbg_hT", esd)
                    for i in range(8):
                        fw.dma(tmp[:], hT_d[:, :, i * 512:(i + 1) * 512], reads=hT_tiles[4 * i:4 * i + 4], writes=[tmp])
                        fw.dma(o[:, :, i * 512:(i + 1) * 512], tmp[:], reads=[tmp], is_output=True)


        body()
        fw.stopped = False
        fw.finish()
    return nc, dbg_out


_INV = (500000.0 ** (-np.arange(0, 16, 2, dtype=np.float32) / 16.0)).astype(np.float32)


def make_in_maps(inputs):
    f = lambda a: np.ascontiguousarray(np.asarray(a), dtype=np.float32)
    x = f(inputs["x"]); p = f(inputs["p"])
    positions = np.asarray(inputs["positions"]).astype(np.int32)
    w_in = f(inputs["w_in"])[0]
    offs = np.cumsum([0, 512, 128, 128, 128, 128, 128, 128, 24, 1024, 512, 512, 8, 2048])
    seg = {n: (offs[i], offs[i + 1]) for i, n in enumerate(["q", "kc", "vc", "ks", "vs", "kw", "vw", "gate", "qk", "v", "o", "if", "mg"])}
    col = lambda n: w_in[:, seg[n][0]:seg[n][1]]
    w_att = []
    for g in range(2):
        parts = [col("q")[:, g * 256:(g + 1) * 256]]
        for n in ["ks", "kw", "kc", "vc", "vs", "vw"]:
            parts.append(col(n)[:, g * 64:(g + 1) * 64])
        parts.append(col("gate")[:, g * 12:(g + 1) * 12])
        w_att.append(np.concatenate(parts, axis=1))
    w_att = np.ascontiguousarray(np.stack(w_att))
    shared = {
        "invf": np.ascontiguousarray(np.broadcast_to(_INV[None, :], (128, 8))),
        "gvec": np.ascontiguousarray(np.stack([f(inputs["g_mix"])[0], f(inputs["g_ffn"])[0], f(inputs["g_ple"])[0], f(inputs["g_final"])])),
        "w_att": w_att,
        "w_qk": np.ascontiguousarray(col("qk")),
        "w_vo": np.ascontiguousarray(np.concatenate([col("v"), col("o")], axis=1)),
        "w_if": np.ascontiguousarray(col("if")),
        "w_mg": np.ascontiguousarray(col("mg")),
        "b_if": f(inputs["b_if"]).reshape(1, 8),
        "w_c1": np.ascontiguousarray(np.stack([f(inputs["w_ck1"])[0], f(inputs["w_cv1"])[0]])),
        "w_c2": np.ascontiguousarray(np.stack([f(inputs["w_ck2"])[0], f(inputs["w_cv2"])[0]])),
        "pe_c": np.ascontiguousarray(np.stack([f(inputs["pe_ck"])[0], f(inputs["pe_cv"])[0]])),
        "wc": np.ascontiguousarray(f(inputs["w_conv"])[0].reshape(4, 8, 128).transpose(2, 1, 0)),
        "bc": np.ascontiguousarray(f(inputs["b_conv"])[0].reshape(8, 128).T),
        "g_hn": f(inputs["g_hn"]).reshape(1, 512),
        "w_pa": f(inputs["w_pa"])[0], "w_pb": f(inputs["w_pb"])[0], "w_out": f(inputs["w_out"])[0],
        "w_r": np.ascontiguousarray(np.concatenate([f(inputs["w_rg"])[0], f(inputs["w_re"])[0]], axis=1)),
        "b_r": np.ascontiguousarray(np.concatenate([f(inputs["b_rg"])[0], f(inputs["b_re"])[0]])[None, :]),
        "w_e13": f(inputs["w_e13"])[0], "w_e2": f(inputs["w_e2"])[0],
        "w_pg": f(inputs["w_pg"])[0], "w_pp": f(inputs["w_pp"])[0],
    }
    in_maps = []
    for core in range(8):
        b, half = core // 2, core % 2
        if half == 1:
            xe_ = x[b]
            pos_ = positions[b]
        else:
            xe_ = np.concatenate([np.zeros((S_OWN, D), np.float32), x[b, :S_OWN]], axis=0)
            pos_ = np.concatenate([np.zeros(S_OWN, np.int32), positions[b, :S_OWN]])
        m = dict(shared)
        m["xe"] = np.ascontiguousarray(xe_)
        m["pos"] = np.ascontiguousarray(pos_.reshape(NT_EXT, 128).T)
        m["pl"] = np.ascontiguousarray(p[0, b, half * S_OWN:(half + 1) * S_OWN])
        m["hv"] = np.full((128, 1), float(half), np.float32)
        in_maps.append(m)
    return in_maps


def kernel(**inputs):
    nc, _ = build_program()
    in_maps = make_in_maps(inputs)
    res = run_bass_kernel_spmd(nc, in_maps, core_ids=list(range(8)))
    out = np.zeros((4, S_EXT, D), np.float32)
    for core in range(8):
        b, half = core // 2, core % 2
        out[b, half * S_OWN:(half + 1) * S_OWN] = res.results[core]["out"]
    return out
```

```python
import numpy as np
import concourse.bass as bass
import concourse.mybir as mybir
from concourse.bass_utils import run_bass_kernel_spmd
from contextlib import ExitStack

F32 = mybir.dt.float32
BF16 = mybir.dt.bfloat16
I32 = mybir.dt.int32
AF = mybir.ActivationFunctionType
ALU = mybir.AluOpType
AX = mybir.AxisListType

D = 1024
S_OWN = 2048
S_EXT = 4096
NT_OWN = 16
NT_EXT = 32
EPS = 1e-6
NEGB = -30000.0
DBG = []


class Buf:
    __slots__ = ("t", "lw", "rd", "name", "excl")

    def __init__(self, t, name=""):
        self.t = t
        self.excl = False
        self.lw = None
        self.rd = {}
        self.name = name

    def __getitem__(self, k):
        return self.t[k]


class FW:
    NDMA = 24

    def __init__(self, nc, es):
        self.nc = nc
        self.es = es
        self.eng = {"pe": nc.tensor, "act": nc.scalar, "dve": nc.vector, "pool": nc.gpsimd, "sp": nc.sync}
        self.sem = {k: es.enter_context(nc.semaphore("s_" + k)) for k in self.eng}
        self.cnt = {k: 0 for k in self.eng}
        self.known = {k: {} for k in self.eng}
        self.dsem = [es.enter_context(nc.semaphore(f"s_dma{i}")) for i in range(self.NDMA)]
        self.dval = [0] * self.NDMA
        self.dnext = 0
        self.nbuf = 0
        self.out_waits = []
        self.stopped = False

    def sb(self, shape, dt, name=None, es=None):
        self.nbuf += 1
        name = f"sb{self.nbuf}_" + (name or "t")
        return Buf((es or self.es).enter_context(self.nc.sbuf_tensor(name, list(shape), dt)), name)

    def ps(self, shape, dt, name=None):
        self.nbuf += 1
        name = name or f"ps{self.nbuf}"
        b = Buf(self.es.enter_context(self.nc.psum_tensor(name, list(shape), dt)), name)
        b.excl = True
        return b

    def _wait(self, e, src, idx):
        if self.stopped:
            return
        kn = self.known[e]
        if kn.get(src, 0) >= idx:
            return
        s = self.dsem[src[1]] if isinstance(src, tuple) else self.sem[src]
        self.eng[e].wait_ge(s, idx)
        kn[src] = idx

    def _deps(self, e, reads, writes):
        for b in reads:
            if b.lw is not None:
                self._wait(e, b.lw[0], b.lw[1])
            if b.excl:
                for src, idx in b.rd.items():
                    if src != e:
                        self._wait(e, src, idx)
        for b in writes:
            if b.lw is not None and b.lw[0] != e:
                self._wait(e, b.lw[0], b.lw[1])
            for src, idx in b.rd.items():
                if src != e:
                    self._wait(e, src, idx)

    def op(self, e, fn, reads=(), writes=()):
        if self.stopped:
            return None
        self._deps(e, reads, writes)
        inst = fn(self.eng[e])
        self.cnt[e] += 1
        c = self.cnt[e]
        inst.then_inc(self.sem[e], 1)
        for b in reads:
            if b.rd.get(e, 0) < c:
                b.rd[e] = c
        for b in writes:
            b.lw = (e, c)
            b.rd = {}
        return inst

    def dma(self, out, in_, reads=(), writes=(), q="sp", is_output=False):
        if self.stopped and not is_output:
            return None
        self._deps(q, reads, writes)
        slot = self.dnext
        self.dnext = (self.dnext + 1) % self.NDMA
        key = ("d", slot)
        if self.dval[slot] > 0:
            self._wait(q, key, self.dval[slot])
        inst = self.eng[q].dma_start(out=out, in_=in_)
        self.dval[slot] += 16
        inst.then_inc(self.dsem[slot], 16)
        v = self.dval[slot]
        for b in reads:
            if b.rd.get(key, 0) < v:
                b.rd[key] = v
        for b in writes:
            b.lw = (key, v)
            b.rd = {}
        if is_output:
            self.out_waits.append((key, v))
        return inst

    def barrier(self):
        for e in self.eng:
            for src in ("pe", "act", "dve", "pool"):
                if src != e and self.cnt[src] > 0:
                    self._wait(e, src, self.cnt[src])
            for slot in range(self.NDMA):
                if self.dval[slot] > 0:
                    self._wait(e, ("d", slot), self.dval[slot])

    def scope(self):
        fw = self

        class _Scope(ExitStack):
            def __exit__(self, *a):
                fw.barrier()
                return super().__exit__(*a)
        return _Scope()

    def finish(self):
        for key, v in self.out_waits:
            self._wait("sp", key, v)
        for k in ("pe", "act", "dve", "pool"):
            if self.cnt[k] > 0:
                self._wait("sp", k, self.cnt[k])


class _StopBuild(Exception):
    pass


def build_program(dbg=()):
    nc = bass.Bass("TRN2", target_bir_lowering=False)

    def din(name, shape, dt=F32):
        return nc.dram_tensor(name, list(shape), dt, kind="ExternalInput").ap()

    xe = din("xe", [S_EXT, D])
    pos_d = din("pos", [128, NT_EXT], I32)
    pl_d = din("pl", [S_OWN, 256])
    hv_d = din("hv", [128, 1])
    invf_d = din("invf", [128, 8])
    gvec_d = din("gvec", [4, D])
    w_att_d = din("w_att", [2, D, 652])
    w_qk_d = din("w_qk", [D, 1024])
    w_vo_d = din("w_vo", [D, 1024])
    w_if_d = din("w_if", [D, 8])
    w_mg_d = din("w_mg", [D, 2048])
    b_if_d = din("b_if", [1, 8])
    w_c1_d = din("w_c1", [2, 2048, 256])
    w_c2_d = din("w_c2", [2, 256, 64])
    pe_c_d = din("pe_c", [2, 32, 64])
    wc_d = din("wc", [128, 8, 4])
    bc_d = din("bc", [128, 8])
    g_hn_d = din("g_hn", [1, 512])
    w_pa_d = din("w_pa", [512, D])
    w_pb_d = din("w_pb", [512, D])
    w_out_d = din("w_out", [D, D])
    w_r_d = din("w_r", [D, 20])
    b_r_d = din("b_r", [1, 20])
    w_e13_d = din("w_e13", [16, D, 512])
    w_e2_d = din("w_e2", [16, 256, D])
    w_pg_d = din("w_pg", [D, D])
    w_pp_d = din("w_pp", [256, D])
    out_d = nc.dram_tensor("out", [S_OWN, D], F32, kind="ExternalOutput").ap()
    hT_d = nc.dram_tensor("hT_scr", [128, 8, S_EXT], BF16, kind="Internal").ap()
    dbg_out = {}

    def dbg_t(name, shape, dt=F32):
        dbg_out[name] = nc.dram_tensor("dbg_" + name, list(shape), dt, kind="ExternalOutput").ap()
        return dbg_out[name]

    with ExitStack() as es:
        fw = FW(nc, es)
        op = fw.op
        PS = [fw.ps([128, 512], F32, f"psb{i}") for i in range(8)]

        def psbf(i):
            return PS[i][:].bitcast(BF16)

        ones_f = fw.sb([128, 128], F32, "ones_f")
        op("pool", lambda e: e.memset(ones_f[:], 1.0), writes=[ones_f])
        idf = fw.sb([128, 128], F32, "idf")
        op("pool", lambda e: e.affine_select(out=idf[:], in_=ones_f[:], pattern=[[1, 128]], compare_op=ALU.is_equal,
                                             fill=0.0, base=0, channel_multiplier=-1), reads=[ones_f], writes=[idf])
        idb = fw.sb([128, 128], BF16, "idb")
        op("dve", lambda e: e.tensor_copy(out=idb[:], in_=idf[:]), reads=[idf], writes=[idb])
        U_f = fw.sb([128, 128], F32, "U_f")
        op("pool", lambda e: e.affine_select(out=U_f[:], in_=ones_f[:], pattern=[[1, 128]], compare_op=ALU.is_ge,
                                             fill=0.0, base=0, channel_multiplier=-1), reads=[ones_f], writes=[U_f])
        caus = fw.sb([128, 128], BF16, "caus")
        op("dve", lambda e: e.tensor_copy(out=caus[:], in_=U_f[:]), reads=[U_f], writes=[caus])
        wm0_f = fw.sb([128, 128], F32, "wm0_f")
        op("pool", lambda e: e.affine_select(out=wm0_f[:], in_=ones_f[:], pattern=[[-1, 128]], compare_op=ALU.is_ge,
                                             fill=0.0, base=-1, channel_multiplier=1), reads=[ones_f], writes=[wm0_f])
        wm0 = fw.sb([128, 128], BF16, "wm0")
        op("dve", lambda e: e.tensor_copy(out=wm0[:], in_=wm0_f[:]), reads=[wm0_f], writes=[wm0])
        c_eps = fw.sb([128, 1], F32, "c_eps")
        op("pool", lambda e: e.memset(c_eps[:], EPS), writes=[c_eps])
        c_one = fw.sb([128, 1], F32, "c_one")
        op("pool", lambda e: e.memset(c_one[:], 1.0), writes=[c_one])
        c_zero = fw.sb([128, 1], F32, "c_zero")
        op("pool", lambda e: e.memset(c_zero[:], 0.0), writes=[c_zero])
        acc_junk = fw.sb([128, 2], F32, "acc_junk")
        op("act", lambda e: e.activation(out=acc_junk[:, 0:1], in_=c_one[:], func=AF.Square, accum_out=acc_junk[:, 1:2]),
           reads=[c_one], writes=[acc_junk])
        hv = fw.sb([128, 1], F32, "hv")
        fw.dma(hv[:], hv_d[:, :], writes=[hv])
        hbias = fw.sb([128, 1], F32, "hbias")
        op("dve", lambda e: e.tensor_scalar(out=hbias[:], in0=hv[:], scalar1=-1.0, scalar2=-NEGB, op0=ALU.add, op1=ALU.mult),
           reads=[hv], writes=[hbias])
        gB = fw.sb([128, D], F32, "gB")

        def load_gain(i):
            fw.dma(gB[:], gvec_d[i:i + 1, :].to_broadcast([128, D]), writes=[gB])

        cs = fw.sb([128, NT_EXT, 8], F32, "cs")
        sn = fw.sb([128, NT_EXT, 8], F32, "sn")
        with fw.scope() as es1:
            posi = fw.sb([128, NT_EXT], I32, "posi", es1)
            posf = fw.sb([128, NT_EXT], F32, "posf", es1)
            invf = fw.sb([128, 8], F32, "invf", es1)
            ang = fw.sb([128, NT_EXT, 8], F32, "ang", es1)
            kf = fw.sb([128, NT_EXT, 8], F32, "kf", es1)
            ki = fw.sb([128, NT_EXT, 8], I32, "ki", es1)
            r1 = fw.sb([128, NT_EXT, 8], F32, "r1", es1)
            r2 = fw.sb([128, NT_EXT, 8], F32, "r2", es1)
            fw.dma(posi[:], pos_d[:, :], writes=[posi])
            fw.dma(invf[:], invf_d[:, :], writes=[invf])
            op("dve", lambda e: e.tensor_copy(out=posf[:], in_=posi[:]), reads=[posi], writes=[posf])
            op("dve", lambda e: e.tensor_tensor(out=ang[:], in0=posf[:].unsqueeze(2).to_broadcast([128, NT_EXT, 8]),
                                                in1=invf[:].unsqueeze(1).to_broadcast([128, NT_EXT, 8]), op=ALU.mult),
               reads=[posf, invf], writes=[ang])
            TWO_PI = 6.283185307179586
            C1 = 6.28125
            C2 = TWO_PI - C1
            PI_LO = 3.1415925
            op("dve", lambda e: e.tensor_scalar(out=kf[:], in0=ang[:], scalar1=1.0 / TWO_PI, scalar2=None, op0=ALU.mult),
               reads=[ang], writes=[kf])
            op("dve", lambda e: e.tensor_copy(out=ki[:], in_=kf[:]), reads=[kf], writes=[ki])
            op("dve", lambda e: e.tensor_copy(out=kf[:], in_=ki[:]), reads=[ki], writes=[kf])
            op("dve", lambda e: e.scalar_tensor_tensor(out=r1[:], in0=kf[:], scalar=-C1, in1=ang[:], op0=ALU.mult, op1=ALU.add),
               reads=[kf, ang], writes=[r1])
            op("dve", lambda e: e.scalar_tensor_tensor(out=r1[:], in0=kf[:], scalar=-C2, in1=r1[:], op0=ALU.mult, op1=ALU.add),
               reads=[kf, r1], writes=[r1])
            op("dve", lambda e: e.tensor_scalar(out=r1[:], in0=r1[:], scalar1=PI_LO, scalar2=-PI_LO, op0=ALU.min, op1=ALU.max),
               reads=[r1], writes=[r1])
            op("act", lambda e: e.activation(out=sn[:], in_=r1[:], func=AF.Sin), reads=[r1], writes=[sn])
            op("dve", lambda e: e.tensor_scalar(out=r2[:], in0=r1[:], scalar1=PI_LO / 2 + 0.0, scalar2=None, op0=ALU.add),
               reads=[r1], writes=[r2])
            op("dve", lambda e: e.tensor_scalar(out=kf[:], in0=r2[:], scalar1=PI_LO, scalar2=-TWO_PI, op0=ALU.is_gt, op1=ALU.mult),
               reads=[r2], writes=[kf])
            op("dve", lambda e: e.tensor_tensor(out=r2[:], in0=r2[:], in1=kf[:], op=ALU.add), reads=[r2, kf], writes=[r2])
            op("dve", lambda e: e.tensor_scalar(out=r2[:], in0=r2[:], scalar1=PI_LO, scalar2=-PI_LO, op0=ALU.min, op1=ALU.max),
               reads=[r2], writes=[r2])
            op("act", lambda e: e.activation(out=cs[:], in_=r2[:], func=AF.Sin), reads=[r2], writes=[cs])

        def rms_rstd(src, rstd, n, junk):
            ss = rstd["ss"]
            op("act", lambda e: e.activation(out=junk["ap"], in_=src["ap"], func=AF.Square, accum_out=ss[:]),
               reads=src["bufs"], writes=[junk["buf"], ss])
            op("act", lambda e: e.activation(out=ss[:], in_=ss[:], func=AF.Sqrt, bias=c_eps[:], scale=1.0 / n),
               reads=[ss, c_eps], writes=[ss])
            op("dve", lambda e: e.reciprocal(out=rstd["r"][:], in_=ss[:]), reads=[ss], writes=[rstd["r"]])

        load_gain(0)
        hT_tiles = [Buf(None, f"hT_tile{t}") for t in range(NT_EXT)]
        with fw.scope() as esA:
            xt = [fw.sb([128, D], F32, f"xtA{i}", esA) for i in range(6)]
            xn = [fw.sb([128, D], BF16, f"xnA{i}", esA) for i in range(2)]
            junk = fw.sb([128, D], BF16, "junkA", esA)
            hst = [fw.sb([128, 8, 128], BF16, f"hstA{i}", esA) for i in range(4)]
            ssA = [fw.sb([128, 1], F32, f"ssA{i}", esA) for i in range(2)]
            rrA = [fw.sb([128, 1], F32, f"rrA{i}", esA) for i in range(2)]
            for t in range(NT_EXT):
                x_ = xt[t % 6]
                if t == 0:
                    for tt in range(5):
                        fw.dma(xt[tt][:], xe[tt * 128:(tt + 1) * 128, :], writes=[xt[tt]])
                if t + 5 < NT_EXT:
                    fw.dma(xt[(t + 5) % 6][:], xe[(t + 5) * 128:(t + 6) * 128, :], writes=[xt[(t + 5) % 6]])
                rs = {"ss": ssA[t % 2], "r": rrA[t % 2]}
                rms_rstd({"ap": x_[:], "bufs": [x_]}, rs, D, {"ap": junk[:], "buf": junk})
                n_ = xn[t % 2]
                op("dve", lambda e: e.scalar_tensor_tensor(out=n_[:], in0=x_[:], scalar=rs["r"][:], in1=gB[:], op0=ALU.mult, op1=ALU.mult),
                   reads=[x_, rs["r"], gB], writes=[n_])
                pb = t % 2
                for k in range(8):
                    op("pe", lambda e: e.transpose(out=psbf(pb)[:, k * 128:(k + 1) * 128], in_=n_[:, k * 128:(k + 1) * 128], identity=idb[:]),
                       reads=[n_, idb], writes=[PS[pb]])
                h_ = hst[t % 4]
                op("act", lambda e: e.copy(out=h_[:], in_=psbf(pb).rearrange("p (k t) -> p k t", k=8)), reads=[PS[pb]], writes=[h_])
                fw.dma(hT_d[:, :, t * 128:(t + 1) * 128], h_[:], reads=[h_], writes=[hT_tiles[t]], q="pool")


        if "cs" in dbg:
            o = dbg_t("cs", [128, NT_EXT, 8])
            fw.dma(o[:, :, :], cs[:], reads=[cs], is_output=True)
            o = dbg_t("sn", [128, NT_EXT, 8])
            fw.dma(o[:, :, :], sn[:], reads=[sn], is_output=True)

        def ckpt(name):
            if ("stop_" + name) in dbg:
                fw.stopped = True

        def body():
            def mm(bank, out_ap, lhsT, rhs, start, stop, reads):
                op("pe", lambda e: e.matmul(out_ap, lhsT, rhs, start=start, stop=stop), reads=reads, writes=[bank])

            YT = fw.sb([128, 8, S_OWN], BF16, "YT")
            esBc = fw.scope()
            esBc.__enter__()
            cmask = fw.sb([128, 2, S_OWN], BF16, "cmask", esBc)
            op("pool", lambda e: e.memset(cmask[:], 1.0), writes=[cmask])
            op("pool", lambda e: e.affine_select(out=cmask[:, 0, :], in_=cmask[:, 0, :], pattern=[[1, S_OWN]], compare_op=ALU.is_ge, fill=0.0,
                                                 base=2017, channel_multiplier=-16), reads=[cmask], writes=[cmask])
            op("pool", lambda e: e.affine_select(out=cmask[:, 1, :], in_=cmask[:, 1, :], pattern=[[1, S_OWN]], compare_op=ALU.is_ge, fill=0.0,
                                                 base=-31, channel_multiplier=-16), reads=[cmask], writes=[cmask])
            ovl = fw.sb([128, 2, 64], BF16, "ovl", esBc)
            op("pool", lambda e: e.memset(ovl[:], 1.0), writes=[ovl])
            for j in range(2):
                op("pool", lambda e: e.affine_select(out=ovl[:, j, :], in_=ovl[:, j, :], pattern=[[-4, 64]], compare_op=ALU.is_ge, fill=0.0,
                                                     base=128 * j + 1, channel_multiplier=1), reads=[ovl], writes=[ovl])
                op("pool", lambda e: e.affine_select(out=ovl[:, j, :], in_=ovl[:, j, :], pattern=[[4, 64]], compare_op=ALU.is_ge, fill=0.0,
                                                     base=3 - 128 * j, channel_multiplier=-1), reads=[ovl], writes=[ovl])
            maskadd = fw.sb([128, NT_OWN, 64], F32, "maskadd", esBc)
            Mb = fw.sb([128, 64], F32, "Mb", esBc)
            hm1 = fw.sb([128, 2], F32, "hm1", esBc)
            op("dve", lambda e: e.tensor_scalar(out=hm1[:, 0:1], in0=hv[:], scalar1=-1.0, scalar2=1e30, op0=ALU.add, op1=ALU.mult),
               reads=[hv], writes=[hm1])
            op("dve", lambda e: e.tensor_scalar(out=hm1[:, 1:2], in0=hv[:], scalar1=-1.0, scalar2=-1000.0, op0=ALU.add, op1=ALU.mult),
               reads=[hv, hm1], writes=[hm1])
            op("dve", lambda e: e.memset(Mb[:], 0.0), writes=[Mb])
            op("dve", lambda e: e.tensor_copy(out=Mb[:, 0:32], in_=hm1[:, 0:1].to_broadcast([128, 32])), reads=[hm1, Mb], writes=[Mb])
            op("dve", lambda e: e.scalar_tensor_tensor(out=Mb[:, 0:1], in0=hv[:], scalar=1000.0, in1=Mb[:, 0:1], op0=ALU.mult, op1=ALU.add),
               reads=[hv, Mb], writes=[Mb])
            op("dve", lambda e: e.tensor_copy(out=Mb[:, 32:33], in_=hm1[:, 1:2]), reads=[hm1, Mb], writes=[Mb])
            for c in range(NT_OWN):
                op("pool", lambda e: e.tensor_copy(out=maskadd[:, c, :], in_=Mb[:]), reads=[Mb, maskadd], writes=[maskadd])
                for hf in range(2):
                    lo = 32 + 2 * c + hf + 1
                    if lo < 64:
                        op("pool", lambda e: e.memset(maskadd[hf * 64:(hf + 1) * 64, c, lo:64], -1e30), reads=[maskadd], writes=[maskadd])
                    for col in (32 + 2 * c + hf, 32 + 2 * c + hf - 1):
                        op("pool", lambda e: e.tensor_scalar(out=maskadd[hf * 64:(hf + 1) * 64, c, col:col + 1],
                                                             in0=maskadd[hf * 64:(hf + 1) * 64, c, col:col + 1],
                                                             scalar1=1000.0, scalar2=None, op0=ALU.add), reads=[maskadd], writes=[maskadd])

            ckpt("consts")
            for g in range(2):
                with fw.scope() as esG:
                    qT = fw.sb([128, 4, S_OWN], BF16, f"qT{g}", esG)
                    kkT = fw.sb([128, 2, S_EXT], BF16, f"kkT{g}", esG)
                    op("pool", lambda e: e.memset(qT[64:128, :, :], 0.0), writes=[qT])
                    op("pool", lambda e: e.memset(kkT[64:128, 0, :], 1.0), writes=[kkT])
                    op("pool", lambda e: e.memset(kkT[64:128, 1, :], 0.0), writes=[kkT])
                    op("pool", lambda e: e.affine_select(out=kkT[64:128, 0, :], in_=kkT[64:128, 0, :], pattern=[[1, S_EXT]], compare_op=ALU.is_ge, fill=0.0,
                                                         base=0, channel_multiplier=-64), reads=[kkT], writes=[kkT])
                    op("pool", lambda e: e.affine_select(out=kkT[64:128, 0, :], in_=kkT[64:128, 0, :], pattern=[[-1, S_EXT]], compare_op=ALU.is_ge, fill=0.0,
                                                         base=63, channel_multiplier=64), reads=[kkT], writes=[kkT])
                    vv = fw.sb([128, NT_EXT, 2, 65], BF16, f"vv{g}", esG)
                    gsig = fw.sb([128, NT_OWN, 12], F32, f"gsig{g}", esG)
                    kcmpT = fw.sb([128, 256], BF16, f"kcmpT{g}", esG)
                    op("pool", lambda e: e.memset(kcmpT[64:128, :], 0.0), writes=[kcmpT])
                    vca = fw.sb([128, 2, 65], BF16, f"vca{g}", esG)
                    op("pool", lambda e: e.memset(vv[:, :, :, 64:65], 1.0), writes=[vv])
                    op("pool", lambda e: e.memset(vca[:, :, 64:65], 1.0), writes=[vca])
                    ckpt("B0a")
                    with fw.scope() as esC:
                        ccT = fw.sb([64, 2, S_EXT], BF16, f"ccT{g}", esC)
                        with fw.scope() as esB1:
                            w_att = fw.sb([128, 8, 652], BF16, f"w_att{g}", esB1)
                            fw.dma(w_att[:], w_att_d[g].rearrange("(k p) c -> p k c", p=128), writes=[w_att], q="pool")
                            ckpt("B0b")
                            hblk = [fw.sb([128, 8, 512], BF16, f"hblkB{g}{i}", esB1) for i in range(2)]
                            rp = [fw.sb([128, 8, 64], BF16, f"rp{g}{i}", esB1) for i in range(2)]
                            rpf = [fw.sb([128, 8, 64], F32, f"rpf{g}{i}", esB1) for i in range(2)]
                            ta = [fw.sb([128, 7, 8], F32, f"ropa{g}{i}", esB1) for i in range(2)]
                            tb_ = [fw.sb([128, 7, 8], F32, f"ropb{g}{i}", esB1) for i in range(2)]
                            tcx = [fw.sb([128, 7, 8], F32, f"ropc{g}{i}", esB1) for i in range(2)]
                            tdx = [fw.sb([128, 7, 8], F32, f"ropd{g}{i}", esB1) for i in range(2)]
                            def b1_front(t):
                                own = t >= NT_OWN
                                tq = t - NT_OWN
                                hb = hblk[(t // 4) % 2]
                                if t % 4 == 0:
                                    fw.dma(hb[:], hT_d[:, :, t * 128:(t + 4) * 128], reads=hT_tiles[t:t + 4], writes=[hb])
                                tl = t % 4
                                a0 = 0 if own else 256
                                nb = 140 if own else 128
                                bA = 2 + t % 2
                                bB = 4 + t % 2
                                for k in range(8):
                                    mm(PS[bA], PS[bA][:, a0:512], hb[:, k, tl * 128:(tl + 1) * 128], w_att[:, k, a0:512], k == 0, k == 7, [hb, w_att])
                                for k in range(8):
                                    mm(PS[bB], PS[bB][:, 0:nb], hb[:, k, tl * 128:(tl + 1) * 128], w_att[:, k, 512:512 + nb], k == 0, k == 7, [hb, w_att])
                                rp_ = rp[t % 2]
                                h0 = a0 // 64
                                nh = 7 - h0
                                rf = rpf[t % 2]
                                op("act", lambda e: e.copy(out=rf[:, h0:8, :], in_=PS[bA][:, a0:512].rearrange("p (h d) -> p h d", d=64)),
                                   reads=[PS[bA]], writes=[rf])
                                op("pool", lambda e: e.tensor_copy(out=rp_[:, h0:8, :], in_=rf[:, h0:8, :]), reads=[rf], writes=[rp_])
                                t1 = rf[:, h0:7, 0:8]
                                t2 = rf[:, h0:7, 8:16]
                                Cb = cs[:, t, :].unsqueeze(1).to_broadcast([128, nh, 8])
                                Sb_ = sn[:, t, :].unsqueeze(1).to_broadcast([128, nh, 8])
                                ta_, tb2 = ta[t % 2], tb_[t % 2]
                                tc_, td_ = tcx[t % 2], tdx[t % 2]
                                op("dve", lambda e: e.tensor_tensor(out=ta_[:, 0:nh, :], in0=t1, in1=Cb, op=ALU.mult), reads=[rf, cs], writes=[ta_])
                                op("dve", lambda e: e.tensor_tensor(out=tb2[:, 0:nh, :], in0=t2, in1=Sb_, op=ALU.mult), reads=[rf, sn], writes=[tb2])
                                op("dve", lambda e: e.tensor_tensor(out=tc_[:, 0:nh, :], in0=t2, in1=Cb, op=ALU.mult), reads=[rf, cs], writes=[tc_])
                                op("dve", lambda e: e.tensor_tensor(out=td_[:, 0:nh, :], in0=t1, in1=Sb_, op=ALU.mult), reads=[rf, sn], writes=[td_])
                                op("dve", lambda e: e.tensor_tensor(out=rp_[:, h0:7, 0:8], in0=ta_[:, 0:nh, :], in1=tb2[:, 0:nh, :], op=ALU.subtract),
                                   reads=[ta_, tb2, rp_], writes=[rp_])
                                op("dve", lambda e: e.tensor_tensor(out=rp_[:, h0:7, 8:16], in0=tc_[:, 0:nh, :], in1=td_[:, 0:nh, :], op=ALU.add),
                                   reads=[tc_, td_, rp_], writes=[rp_])
                                op("dve", lambda e: e.tensor_copy(out=vv[:, t, :, 0:64], in_=PS[bB][:, 0:128].rearrange("p (h d) -> p h d", d=64)),
                                   reads=[PS[bB]], writes=[vv])
                                if own:
                                    op("act", lambda e: e.activation(out=gsig[:, tq, :], in_=PS[bB][:, 128:140], func=AF.Sigmoid),
                                       reads=[PS[bB]], writes=[gsig])

                            def b1_back(t):
                                own = t >= NT_OWN
                                tq = t - NT_OWN
                                rp_ = rp[t % 2]
                                h0 = 0 if own else 4
                                bT = t % 2
                                psT = psbf(bT)
                                for j, hh in enumerate(range(h0, 8)):
                                    op("pe", lambda e: e.transpose(out=psT[0:64, j * 128:(j + 1) * 128], in_=rp_[:, hh, :], identity=idb[:]),
                                       reads=[rp_, idb], writes=[PS[bT]])
                                if own:
                                    op("act", lambda e: e.copy(out=qT[0:64, :, tq * 128:(tq + 1) * 128], in_=psT[0:64, 0:512].rearrange("p (h t) -> p h t", h=4)),
                                       reads=[PS[bT]], writes=[qT])
                                    o1 = 512
                                else:
                                    o1 = 0
                                op("act", lambda e: e.copy(out=kkT[0:64, :, t * 128:(t + 1) * 128], in_=psT[0:64, o1:o1 + 256].rearrange("p (h t) -> p h t", h=2)),
                                   reads=[PS[bT]], writes=[kkT])
                                op("act", lambda e: e.copy(out=ccT[:, :, t * 128:(t + 1) * 128], in_=psT[0:64, o1 + 256:o1 + 512].rearrange("p (h t) -> p h t", h=2)),
                                   reads=[PS[bT]], writes=[ccT])

                            for t in range(NT_EXT + 1):
                                if t < NT_EXT:
                                    b1_front(t)
                                if t >= 1:
                                    b1_back(t - 1)
                        ckpt("B1")
                        for i in range(2):
                            with fw.scope() as esB2:
                                w1 = fw.sb([64, 32, 256], BF16, f"w1_{g}{i}", esB2)
                                fw.dma(w1[:], w_c1_d[i].rearrange("(l d) h -> d l h", d=64), writes=[w1], q="pool")
                                w2 = fw.sb([128, 2, 64], BF16, f"w2_{g}{i}", esB2)
                                fw.dma(w2[:], w_c2_d[i].rearrange("(c p) d -> p c d", p=128), writes=[w2], q="pool")
                                pe_sb = fw.sb([32, 64], BF16, f"pe_{g}{i}", esB2)
                                fw.dma(pe_sb[:], pe_c_d[i], writes=[pe_sb], q="pool")
                                peT = fw.sb([64, 32], BF16, f"peT_{g}{i}", esB2)
                                op("pe", lambda e: e.transpose(out=psbf(6)[0:64, 0:32], in_=pe_sb[:, :], identity=idb[0:32, 0:32]),
                                   reads=[pe_sb, idb], writes=[PS[6]])
                                op("act", lambda e: e.copy(out=peT[:], in_=psbf(6)[0:64, 0:32]), reads=[PS[6]], writes=[peT])
                                for hc in range(2):
                                    for l in range(32):
                                        mm(PS[7], PS[7][:, hc:hc + 1], w1[:, l, hc * 128:(hc + 1) * 128], peT[:, l:l + 1], l == 0, l == 31, [w1, peT])
                                cbs = fw.sb([128, 2], F32, f"cbs_{g}{i}", esB2)
                                op("act", lambda e: e.copy(out=cbs[:], in_=PS[7][:, 0:2]), reads=[PS[7]], writes=[cbs])
                                G = fw.sb([128, 2, 256], BF16, f"G_{g}{i}", esB2)
                                op("pool", lambda e: e.memset(G[:, :, 255:256], 0.0), writes=[G])
                                u_ = fw.sb([128, 255], F32, f"u_{g}{i}", esB2)
                                u2 = fw.sb([128, 255], F32, f"u2_{g}{i}", esB2)
                                sg_ = fw.sb([128, 255], F32, f"sg_{g}{i}", esB2)
                                for hc in range(2):
                                    for l in range(32):
                                        mm(PS[hc], PS[hc][:, 0:255], w1[:, l, hc * 128:(hc + 1) * 128], ccT[:, i, l:l + 16 * 254 + 1:16], l == 0, l == 31, [w1, ccT])
                                    op("act", lambda e: e.activation(out=u_[:], in_=PS[hc][:, 0:255], func=AF.Identity, bias=cbs[:, hc:hc + 1]),
                                       reads=[PS[hc], cbs], writes=[u_])
                                    op("dve", lambda e: e.tensor_tensor(out=u2[:], in0=u_[:], in1=u_[:], op=ALU.mult), reads=[u_], writes=[u2])
                                    op("dve", lambda e: e.tensor_scalar(out=u2[:], in0=u2[:], scalar1=0.044715, scalar2=1.0, op0=ALU.mult, op1=ALU.add),
                                       reads=[u2], writes=[u2])
                                    op("dve", lambda e: e.tensor_tensor(out=u2[:], in0=u2[:], in1=u_[:], op=ALU.mult), reads=[u2, u_], writes=[u2])
                                    op("act", lambda e: e.activation(out=sg_[:], in_=u2[:], func=AF.Sigmoid, scale=1.5957691216057308),
                                       reads=[u2], writes=[sg_])
                                    op("dve", lambda e: e.tensor_tensor(out=G[:, hc, 0:255], in0=u_[:], in1=sg_[:], op=ALU.mult), reads=[u_, sg_], writes=[G])
                                if i == 0:
                                    for hc in range(2):
                                        mm(PS[6], PS[6][0:64, 0:256], w2[:, hc, :], G[:, hc, :], hc == 0, hc == 1, [w2, G])
                                    op("act", lambda e: e.copy(out=kcmpT[0:64, :], in_=PS[6][0:64, 0:256]), reads=[PS[6]], writes=[kcmpT])
                                else:
                                    for nch in range(2):
                                        for hc in range(2):
                                            mm(PS[6], PS[6][:, nch * 64:(nch + 1) * 64], G[:, hc, nch * 128:(nch + 1) * 128], w2[:, hc, :], hc == 0, hc == 1, [w2, G])
                                    op("act", lambda e: e.copy(out=vca[:, :, 0:64], in_=PS[6][:, 0:128].rearrange("p (n d) -> p n d", d=64)),
                                       reads=[PS[6]], writes=[vca])
                    if g == 0 and "B2dump" in dbg:
                        for nm, bf, shp in (("kkT", kkT, [64, 2, S_EXT]), ("qT", qT, [64, 4, S_OWN]), ("vv", vv, [128, NT_EXT, 2, 65]),
                                            ("kcmpT", kcmpT, [64, 256]), ("vca", vca, [128, 2, 65])):
                            o = dbg_t(nm, shp, BF16)
                            fw.dma(o, bf[0:shp[0]], reads=[bf], is_output=True)
                        o = dbg_t("gsig", [128, NT_OWN, 12])
                        fw.dma(o, gsig[:], reads=[gsig], is_output=True)
                    ckpt("B2")
                    with fw.scope() as esB3:
                        NP = 4
                        LA = 2
                        Pb = [fw.sb([128, 512], BF16, f"Pb{g}{i}", esB3) for i in range(NP)]
                        Sbank = [0, 1, 6, 7]
                        tpsum = psbf(2)[:, 520:1024]
                        TPB = PS[2]
                        hbS = [fw.sb([128, 4, 132], F32, f"hbS{g}{r}", esB3) for r in range(3)]
                        ya = [fw.sb([128, 4, 64], F32, f"ya{g}{i}", esB3) for i in range(2)]
                        yat = [fw.sb([128, 4, 64], BF16, f"yat{g}{i}", esB3) for i in range(2)]
                        sms = [fw.sb([128, 16], F32, f"sm{g}{i}", esB3) for i in range(3)]
                        rdc = fw.sb([128, 4], F32, f"rdc{g}", esB3)
                        impv = fw.sb([128, 64], F32, f"impv{g}", esB3)
                        wk = fw.sb([128, 64], F32, f"wk{g}", esB3)
                        m8a = fw.sb([128, 8], F32, f"m8a{g}", esB3)
                        m8b = fw.sb([128, 8], F32, f"m8b{g}", esB3)
                        negm2 = fw.sb([128, 128], BF16, f"negm{g}", esB3)
                        op("pool", lambda e: e.memset(negm2[:, 0:64], 0.0), writes=[negm2])
                        rot = [0]
                        REG = {0: (0, 129), 1: (129, 65), 2: (194, 65)}

                        def score(c, lhsT, lreads, extra, bias, mask):
                            r = rot[0] % NP
                            rot[0] += 1
                            sb_i = Sbank[r]
                            P = Pb[r]
                            qrhs = qT[:, :, c * 128:(c + 1) * 128]
                            S3 = PS[sb_i][:, :].rearrange("p (h q) -> p h q", h=4)
                            mm(PS[sb_i], S3, lhsT, qrhs, True, True, lreads + [qT])
                            op("act", lambda e: e.activation(out=P[:], in_=PS[sb_i][:, :], func=AF.Exp, bias=bias[:], scale=0.125),
                               reads=[PS[sb_i], bias], writes=[P])
                            if mask is not None:
                                op("dve", lambda e: e.tensor_tensor(out=P[:].rearrange("p (h q) -> p h q", h=4), in0=P[:].rearrange("p (h q) -> p h q", h=4),
                                                                    in1=mask[0], op=ALU.mult), reads=[P, mask[1]], writes=[P])
                            return P

                        def pv(P, h, reg, vr, vreads, cc, n, first, last):
                            op("pe", lambda e: e.matmul(PS[2 + h][:, cc:cc + n], P[:, h * 128:(h + 1) * 128], vr, start=first, stop=last),
                               reads=[P] + vreads, writes=[PS[2 + h]])

                        def evac_all(c, reg, br, first, final):
                            col0, n = REG[reg]
                            hs = hbS[reg]
                            for h in range(4):
                                op("dve", lambda e: e.tensor_copy(out=hs[:, h, 0:n], in_=PS[2 + h][:, col0:col0 + n]), reads=[PS[2 + h]], writes=[hs])
                            sm = sms[reg]
                            yac = ya[c % 2]
                            dn = sm[:, 0:4]
                            rd = sm[:, 4:8] if br != 0 else rdc[:, 0:4]
                            rdb = sm if br != 0 else rdc
                            cf = sm[:, 8:12]
                            op("dve", lambda e: e.tensor_scalar(out=dn.unsqueeze(2), in0=hs[:, :, 64:65], scalar1=1e-30, scalar2=None, op0=ALU.max),
                               reads=[hs], writes=[sm])
                            op("dve", lambda e: e.reciprocal(out=rd, in_=dn), reads=[sm], writes=[rdb])
                            op("dve", lambda e: e.tensor_tensor(out=cf.unsqueeze(2), in0=rd.unsqueeze(2),
                                                                in1=gsig[:, c, :].rearrange("p (h b) -> p h b", b=3)[:, :, br:br + 1], op=ALU.mult),
                               reads=[sm, rdb, gsig], writes=[sm])
                            cfb = cf.unsqueeze(2).to_broadcast([128, 4, 64])
                            if first:
                                op("dve", lambda e: e.tensor_tensor(out=yac[:], in0=hs[:, :, 0:64], in1=cfb, op=ALU.mult), reads=[hs, sm], writes=[yac])
                            else:
                                op("dve", lambda e: e.tensor_tensor(out=hs[:, :, 0:64], in0=hs[:, :, 0:64], in1=cfb, op=ALU.mult), reads=[hs, sm], writes=[hs])
                                dst = yat[c % 2] if final else yac
                                op("dve", lambda e: e.tensor_tensor(out=dst[:], in0=hs[:, :, 0:64], in1=yac[:], op=ALU.add), reads=[hs, yac], writes=[dst])

                        pend = []

                        def flush():
                            while pend:
                                pend.pop(0)()

                        def pipe(score_fn, pv_fn):
                            P = score_fn()
                            while len(pend) >= LA:
                                pend.pop(0)()
                            pend.append(lambda: pv_fn(P))

                        for c in range(NT_OWN):
                            Pc = []
                            for nch in range(2):
                                mk = cmask[:, nch, c * 128:(c + 1) * 128].unsqueeze(1).to_broadcast([128, 4, 128])
                                Pc.append(score(c, kcmpT[:, nch * 128:(nch + 1) * 128], [kcmpT], None, hbias if nch == 0 else c_zero, (mk, cmask)))
                            flush()
                            if c > 0:
                                cp = c - 1
                                evac_all(cp, 1, 1, False, True)
                                for j in range(2):
                                    op("pe", lambda e: e.transpose(out=tpsum[:, j * 128:(j + 1) * 128],
                                                                   in_=yat[cp % 2][:, 2 * j:2 * j + 2, :].rearrange("p h d -> p (h d)"), identity=idb[:]),
                                       reads=[yat[cp % 2], idb], writes=[TPB])
                                op("act", lambda e: e.copy(out=YT[:, 2 * g:2 * g + 2, cp * 128:(cp + 1) * 128],
                                                           in_=tpsum[:, 0:256].rearrange("p (j t) -> p j t", j=2)), reads=[TPB], writes=[YT])
                            if g == 0 and c == 1 and "B3dump" in dbg:
                                for r_ in range(3):
                                    fw.dma(dbg_t(f"hbS{r_}", [128, 4, 132]), hbS[r_][:], reads=[hbS[r_]], is_output=True)
                                fw.dma(dbg_t("yat0", [128, 4, 64], BF16), yat[0][:], reads=[yat[0]], is_output=True)
                                fw.dma(dbg_t("ya0", [128, 4, 64]), ya[0][:], reads=[ya[0]], is_output=True)
                                fw.dma(dbg_t("sm0", [128, 16]), sms[0][:], reads=[sms[0]], is_output=True)
                                fw.dma(dbg_t("sm1", [128, 16]), sms[1][:], reads=[sms[1]], is_output=True)
                                fw.dma(dbg_t("sm2", [128, 16]), sms[2][:], reads=[sms[2]], is_output=True)
                                fw.dma(dbg_t("rdc", [128, 4]), rdc[:], reads=[rdc], is_output=True)
                                ckpt("B3c0")
                            for h in range(4):
                                for nch in range(2):
                                    pv(Pc[nch], h, 0, vca[:, nch, :], [vca], 0, 65, nch == 0, nch == 1)
                                for nch in range(2):
                                    pv(Pc[nch], h, 0, ovl[:, nch, :], [ovl], 65, 64, nch == 0, nch == 1)
                            for j in range(5):
                                ch = NT_OWN + c - 4 + j
                                mk = None
                                if j == 0:
                                    mk = (wm0[:].unsqueeze(1).to_broadcast([128, 4, 128]), wm0)
                                elif j == 4:
                                    mk = (caus[:].unsqueeze(1).to_broadcast([128, 4, 128]), caus)

                                def sfn(ch=ch, mk=mk):
                                    return score(c, kkT[:, 1, ch * 128:(ch + 1) * 128], [kkT], None, hbias if ch < NT_OWN else c_zero, mk)

                                def pfn(P, ch=ch, j=j):
                                    for h in range(4):
                                        pv(P, h, 2, vv[:, ch, 1, :], [vv], 194, 65, j == 0, j == 4)
                                pipe(sfn, pfn)
                                if j == 0:
                                    evac_all(c, 0, 0, True, False)
                                    op("dve", lambda e: e.tensor_tensor(out=hbS[0][:, :, 65:129], in0=hbS[0][:, :, 65:129], in1=rdc[:, 0:4].unsqueeze(2).to_broadcast([128, 4, 64]), op=ALU.mult),
                                       reads=[hbS[0], rdc], writes=[hbS[0]])
                                    op("dve", lambda e: e.tensor_reduce(out=impv[:], in_=hbS[0][:, :, 65:129].rearrange("p h s -> p s h"), axis=AX.X, op=ALU.add),
                                       reads=[hbS[0]], writes=[impv])
                                    op("dve", lambda e: e.tensor_tensor(out=impv[:], in0=impv[:], in1=maskadd[:, c, :], op=ALU.add), reads=[impv, maskadd], writes=[impv])
                                    op("dve", lambda e: e.max(out=m8a[:], in_=impv[:]), reads=[impv], writes=[m8a])
                                    op("dve", lambda e: e.match_replace(out=wk[:], in_to_replace=m8a[:], in_values=impv[:], imm_value=-3.0e38),
                                       reads=[impv, m8a], writes=[wk])
                                    op("dve", lambda e: e.max(out=m8b[:], in_=wk[:]), reads=[wk], writes=[m8b])
                                    op("dve", lambda e: e.tensor_scalar(out=negm2[:, 64:128], in0=impv[:], scalar1=m8b[:, 7:8], scalar2=NEGB, op0=ALU.is_lt, op1=ALU.mult),
                                       reads=[impv, m8b, negm2], writes=[negm2])
                            flush()
                            evac_all(c, 2, 2, False, False)
                            op("pe", lambda e: e.transpose(out=tpsum[:, 256:384], in_=negm2[:, :], identity=idb[:]), reads=[negm2, idb], writes=[TPB])
                            op("act", lambda e: e.copy(out=qT[64:128, :, c * 128:(c + 1) * 128], in_=tpsum[64:128, 256:384].unsqueeze(1).to_broadcast([64, 4, 128])),
                               reads=[TPB], writes=[qT])
                            chs = list(range(NT_OWN)) + [NT_OWN + j for j in range(c + 1)]
                            for i, ch in enumerate(chs):
                                mk = None
                                if ch == NT_OWN + c:
                                    mk = (caus[:].unsqueeze(1).to_broadcast([128, 4, 128]), caus)

                                def sfn(ch=ch, mk=mk):
                                    return score(c, kkT[:, 0, ch * 128:(ch + 1) * 128], [kkT], None, hbias if ch < NT_OWN else c_zero, mk)

                                def pfn(P, ch=ch, i=i, n=len(chs)):
                                    for h in range(4):
                                        pv(P, h, 1, vv[:, ch, 0, :], [vv], 129, 65, i == 0, i == n - 1)
                                pipe(sfn, pfn)
                        flush()
                        cp = NT_OWN - 1
                        evac_all(cp, 1, 1, False, True)
                        for j in range(2):
                            op("pe", lambda e: e.transpose(out=tpsum[:, j * 128:(j + 1) * 128],
                                                           in_=yat[cp % 2][:, 2 * j:2 * j + 2, :].rearrange("p h d -> p (h d)"), identity=idb[:]),
                               reads=[yat[cp % 2], idb], writes=[TPB])
                        op("act", lambda e: e.copy(out=YT[:, 2 * g:2 * g + 2, cp * 128:(cp + 1) * 128],
                                                   in_=tpsum[:, 0:256].rearrange("p (j t) -> p j t", j=2)), reads=[TPB], writes=[YT])
            esBc.__exit__(None, None, None)
            ckpt("B")
            with fw.scope() as esCg:
                ee = fw.sb([128, NT_EXT, 4], F32, "ee", esCg)
                ff = fw.sb([128, NT_EXT, 4], F32, "ff", esCg)
                fl = fw.sb([128, NT_EXT, 4], F32, "fl", esCg)
                ghn = fw.sb([128, 512], F32, "ghn", esCg)
                fw.dma(ghn[:], g_hn_d[0:1, :].to_broadcast([128, 512]), writes=[ghn])
                wcs = fw.sb([128, 8, 4], F32, "wcs", esCg)
                fw.dma(wcs[:], wc_d[:, :, :], writes=[wcs])
                bcs = fw.sb([128, 8], F32, "bcs", esCg)
                fw.dma(bcs[:], bc_d[:, :], writes=[bcs])
                with fw.scope() as esg:
                    w_if = fw.sb([128, 8, 8], BF16, "w_if", esg)
                    fw.dma(w_if[:], w_if_d.rearrange("(k p) c -> p k c", p=128), writes=[w_if], q="pool")
                    bif = fw.sb([128, 8], F32, "bif", esg)
                    fw.dma(bif[:], b_if_d[0:1, :].to_broadcast([128, 8]), writes=[bif])
                    hblk = [fw.sb([128, 8, 512], BF16, f"hblkG{i}", esg) for i in range(2)]
                    ifp = fw.sb([128, NT_EXT, 8], F32, "ifp", esg)
                    l1 = fw.sb([128, NT_EXT, 4], F32, "l1", esg)
                    tmpg = fw.sb([128, NT_EXT, 4], F32, "tmpg", esg)
                    for t in range(NT_EXT):
                        hb = hblk[(t // 4) % 2]
                        if t % 4 == 0:
                            fw.dma(hb[:], hT_d[:, :, t * 128:(t + 4) * 128], reads=hT_tiles[t:t + 4], writes=[hb])
                        tl = t % 4
                        for k in range(8):
                            mm(PS[0], PS[0][:, t * 8:(t + 1) * 8], hb[:, k, tl * 128:(tl + 1) * 128], w_if[:, k, :], k == 0, k == 7, [hb, w_if])
                    op("act", lambda e: e.copy(out=ifp[:], in_=PS[0][:, 0:256].rearrange("p (t c) -> p t c", c=8)), reads=[PS[0]], writes=[ifp])
                    op("dve", lambda e: e.tensor_tensor(out=ifp[:], in0=ifp[:], in1=bif[:].unsqueeze(1).to_broadcast([128, NT_EXT, 8]), op=ALU.add),
                       reads=[ifp, bif], writes=[ifp])
                    op("act", lambda e: e.activation(out=l1[:], in_=ifp[:, :, 4:8], func=AF.Exp, scale=-1.0), reads=[ifp], writes=[l1])
                    op("act", lambda e: e.activation(out=l1[:], in_=l1[:], func=AF.Ln, bias=c_one[:]), reads=[l1, c_one], writes=[l1])
                    l1f = l1[:].rearrange("p t c -> p (t c)")
                    mm(PS[1], PS[1][:, 0:128], U_f[:], l1f, True, True, [U_f, l1])
                    mm(PS[1], PS[1][:, 128:256], ones_f[:], l1f, True, True, [ones_f, l1])
                    op("act", lambda e: e.copy(out=tmpg[:], in_=PS[1][:, 0:128].rearrange("p (t c) -> p t c", c=4)), reads=[PS[1]], writes=[tmpg])
                    op("act", lambda e: e.activation(out=ff[:], in_=tmpg[:], func=AF.Exp, scale=-1.0), reads=[tmpg], writes=[ff])
                    op("act", lambda e: e.activation(out=fl[:], in_=PS[1][:, 128:256].rearrange("p (t c) -> p t c", c=4), func=AF.Exp, scale=-1.0),
                       reads=[PS[1]], writes=[fl])
                    op("dve", lambda e: e.tensor_tensor(out=tmpg[:], in0=tmpg[:], in1=ifp[:, :, 0:4], op=ALU.add), reads=[tmpg, ifp], writes=[tmpg])
                    op("act", lambda e: e.activation(out=ee[:], in_=tmpg[:], func=AF.Exp), reads=[tmpg], writes=[ee])
                    op("dve", lambda e: e.tensor_scalar(out=ee[:, 0:NT_OWN, :], in0=ee[:, 0:NT_OWN, :], scalar1=hv[:, 0:1], scalar2=None, op0=ALU.mult),
                       reads=[ee, hv], writes=[ee])
                ckpt("Cg")
                for hp in range(2):
                    with fw.scope() as esH:
                        qTb = fw.sb([128, 2, S_OWN], BF16, f"qTb{hp}", esH)
                        kTb = fw.sb([128, 2, S_EXT], BF16, f"kTb{hp}", esH)
                        vaug = fw.sb([128, NT_EXT, 2, 129], BF16, f"vaug{hp}", esH)
                        osig = fw.sb([128, NT_OWN, 256], BF16, f"osig{hp}", esH)
                        op("pool", lambda e: e.memset(vaug[:, :, :, 128:129], 1.0), writes=[vaug])
                        with fw.scope() as esC1:
                            wq = fw.sb([128, 8, 256], BF16, f"wq{hp}", esC1)
                            wk = fw.sb([128, 8, 256], BF16, f"wk{hp}", esC1)
                            wv = fw.sb([128, 8, 256], BF16, f"wv{hp}", esC1)
                            wo = fw.sb([128, 8, 256], BF16, f"wo{hp}", esC1)
                            fw.dma(wq[:], w_qk_d[:, hp * 256:(hp + 1) * 256].rearrange("(k p) c -> p k c", p=128), writes=[wq], q="pool")
                            fw.dma(wk[:], w_qk_d[:, 512 + hp * 256:512 + (hp + 1) * 256].rearrange("(k p) c -> p k c", p=128), writes=[wk], q="pool")
                            fw.dma(wv[:], w_vo_d[:, hp * 256:(hp + 1) * 256].rearrange("(k p) c -> p k c", p=128), writes=[wv], q="pool")
                            fw.dma(wo[:], w_vo_d[:, 512 + hp * 256:512 + (hp + 1) * 256].rearrange("(k p) c -> p k c", p=128), writes=[wo], q="pool")
                            hblk = [fw.sb([128, 8, 512], BF16, f"hblkC{hp}{i}", esC1) for i in range(2)]
                            uk = [fw.sb([128, 4 + S_EXT], BF16, f"uk{hp}{i}", esC1) for i in range(2)]
                            uq = [fw.sb([128, 4 + 2560], BF16, f"uq{hp}{i}", esC1) for i in range(2)]
                            ycv = [fw.sb([128, 512], F32, f"ycv{hp}{i}", esC1) for i in range(2)]
                            sgm = [fw.sb([128, 512], F32, f"sgm{hp}{i}", esC1) for i in range(2)]
                            for hh in range(2):
                                op("pool", lambda e: e.memset(uk[hh][:, 0:4], 0.0), writes=[uk[hh]])
                                op("pool", lambda e: e.memset(uq[hh][:, 0:4], 0.0), writes=[uq[hh]])
                            for blk in range(8):
                                hb = hblk[blk % 2]
                                fw.dma(hb[:], hT_d[:, :, blk * 512:(blk + 1) * 512], reads=hT_tiles[4 * blk:4 * blk + 4], writes=[hb])
                                for hh in range(2):
                                    for k in range(8):
                                        mm(PS[hh], PS[hh][:, :], wk[:, k, hh * 128:(hh + 1) * 128], hb[:, k, :], k == 0, k == 7, [wk, hb])
                                    op("act", lambda e: e.copy(out=uk[hh][:, 4 + blk * 512:4 + (blk + 1) * 512], in_=PS[hh][:, :]), reads=[PS[hh]], writes=[uk[hh]])
                                if blk >= 3:
                                    for hh in range(2):
                                        for k in range(8):
                                            mm(PS[2 + hh], PS[2 + hh][:, :], wq[:, k, hh * 128:(hh + 1) * 128], hb[:, k, :], k == 0, k == 7, [wq, hb])
                                        op("act", lambda e: e.copy(out=uq[hh][:, 4 + (blk - 3) * 512:4 + (blk - 2) * 512], in_=PS[2 + hh][:, :]),
                                           reads=[PS[2 + hh]], writes=[uq[hh]])
                                for tl in range(4):
                                    t = blk * 4 + tl
                                    bv = 4 + tl % 2
                                    for k in range(8):
                                        mm(PS[bv], PS[bv][:, 0:256], hb[:, k, tl * 128:(tl + 1) * 128], wv[:, k, :], k == 0, k == 7, [wv, hb])
                                    op("dve", lambda e: e.tensor_copy(out=vaug[:, t, :, 0:128], in_=PS[bv][:, 0:256].rearrange("p (h d) -> p h d", d=128)),
                                       reads=[PS[bv]], writes=[vaug])
                                    if blk >= 4:
                                        bo = 6 + tl % 2
                                        for k in range(8):
                                            mm(PS[bo], PS[bo][:, 0:256], hb[:, k, tl * 128:(tl + 1) * 128], wo[:, k, :], k == 0, k == 7, [wo, hb])
                                        op("act", lambda e: e.activation(out=osig[:, t - NT_OWN, :], in_=PS[bo][:, 0:256], func=AF.Sigmoid),
                                           reads=[PS[bo]], writes=[osig])
                            pi = 0
                            for hh in range(2):
                                H = 2 * hp + hh
                                for typ in range(2):
                                    ci = typ * 4 + H
                                    npiece = 4 if typ == 0 else 8
                                    u = uq[hh] if typ == 0 else uk[hh]
                                    for pc in range(npiece):
                                        off = (4 + 512 + pc * 512) if typ == 0 else (4 + pc * 512)
                                        y_ = ycv[pi % 2]
                                        s_ = sgm[pi % 2]
                                        pi += 1
                                        op("dve", lambda e: e.tensor_scalar(out=y_[:], in0=u[:, off - 3:off - 3 + 512], scalar1=wcs[:, ci, 0:1], scalar2=bcs[:, ci:ci + 1],
                                                                            op0=ALU.mult, op1=ALU.add), reads=[u, wcs, bcs], writes=[y_])
                                        for j in range(1, 4):
                                            op("dve", lambda e: e.scalar_tensor_tensor(out=y_[:], in0=u[:, off - 3 + j:off - 3 + j + 512], scalar=wcs[:, ci, j:j + 1], in1=y_[:],
                                                                                       op0=ALU.mult, op1=ALU.add), reads=[u, wcs, y_], writes=[y_])
                                        if typ == 0:
                                            op("act", lambda e: e.activation(out=qTb[:, hh, pc * 512:(pc + 1) * 512], in_=y_[:], func=AF.Silu), reads=[y_], writes=[qTb])
                                        else:
                                            op("act", lambda e: e.activation(out=s_[:], in_=y_[:], func=AF.Sigmoid), reads=[y_], writes=[s_])
                                            op("dve", lambda e: e.scalar_tensor_tensor(out=kTb[:, hh, pc * 512:(pc + 1) * 512], in0=y_[:], scalar=128.0 ** -0.5, in1=s_[:],
                                                                                       op0=ALU.mult, op1=ALU.mult), reads=[y_, s_], writes=[kTb])
                        ckpt("C1")
                        with fw.scope() as esC3:
                            ktokA = fw.sb([128, NT_EXT, 2, 128], BF16, f"ktokA{hp}", esC3)
                            CTall = fw.sb([128, NT_EXT, 2, 129], BF16, f"CTall{hp}", esC3)
                            Xs = [fw.sb([128, 129], F32, f"Xs{hp}{hh}", esC3) for hh in range(2)]
                            Sm = [[fw.sb([128, 128], BF16, f"Sm{hp}{hh}{i}", esC3) for i in range(2)] for hh in range(2)]
                            hm_ = [fw.sb([128, 128], F32, f"hm{hp}{hh}", esC3) for hh in range(2)]
                            yb_ = [fw.sb([128, 128], BF16, f"yb{hp}{hh}", esC3) for hh in range(2)]
                            jk = [fw.sb([128, 128], BF16, f"jk{hp}{hh}", esC3) for hh in range(2)]
                            smc = [fw.sb([128, 8], F32, f"smc{hp}{hh}", esC3) for hh in range(2)]
                            op("pool", lambda e: e.memset(CTall[:, 0, :, :], 0.0), writes=[CTall])
                            for hh in range(2):
                                H = 2 * hp + hh
                                op("dve", lambda e: e.tensor_tensor(out=vaug[:, :, hh, :], in0=vaug[:, :, hh, :],
                                                                    in1=ee[:, :, H:H + 1].to_broadcast([128, NT_EXT, 129]), op=ALU.mult), reads=[vaug, ee], writes=[vaug])
                            grp = 0
                            items = [(t, hh) for t in range(NT_EXT - 1) for hh in range(2)]
                            for i0 in range(0, len(items), 8):
                                bk = grp % 2
                                grp += 1
                                sub = items[i0:i0 + 8]
                                for j, (t, hh) in enumerate(sub):
                                    op("pe", lambda e: e.transpose(out=psbf(bk)[:, j * 128:(j + 1) * 128], in_=kTb[:, hh, t * 128:(t + 1) * 128], identity=idb[:]),
                                       reads=[kTb, idb], writes=[PS[bk]])
                                t0_ = sub[0][0]
                                nt_ = len(sub) // 2
                                op("act", lambda e: e.copy(out=ktokA[:, t0_:t0_ + nt_, :, :].rearrange("p t h d -> p (t h) d"),
                                                           in_=psbf(bk)[:, 0:len(sub) * 128].rearrange("p (j d) -> p j d", d=128)), reads=[PS[bk]], writes=[ktokA])
                            for t in range(NT_EXT - 1):
                                for hh in range(2):
                                    H = 2 * hp + hh
                                    bU = 2 + 2 * hh + (t % 2)
                                    mm(PS[bU], PS[bU][:, 0:129], ktokA[:, t, hh, :], vaug[:, t, hh, :], True, True, [ktokA, vaug])
                                    if t == 0:
                                        op("dve", lambda e: e.tensor_copy(out=Xs[hh][:], in_=PS[bU][:, 0:129]), reads=[PS[bU]], writes=[Xs[hh]])
                                    else:
                                        op("dve", lambda e: e.scalar_tensor_tensor(out=Xs[hh][:], in0=Xs[hh][:], scalar=fl[:, t - 1, H:H + 1], in1=PS[bU][:, 0:129],
                                                                                   op0=ALU.mult, op1=ALU.add), reads=[Xs[hh], fl, PS[bU]], writes=[Xs[hh]])
                                    op("act", lambda e: e.activation(out=CTall[:, t + 1, hh, :], in_=Xs[hh][:], func=AF.Copy, scale=fl[:, t, H:H + 1]),
                                       reads=[Xs[hh], fl], writes=[CTall])
                            pendC = [None]

                            def flushC():
                                if pendC[0] is not None:
                                    pendC[0]()
                                    pendC[0] = None

                            oi = 0
                            for t in range(NT_OWN, NT_EXT):
                                for hh in range(2):
                                    H = 2 * hp + hh
                                    tq = t - NT_OWN
                                    bS = 2 * hh + (oi // 2) % 2
                                    sm_ = Sm[hh][(oi // 2) % 2]
                                    oi += 1
                                    mm(PS[bS], PS[bS][:, 0:128], kTb[:, hh, t * 128:(t + 1) * 128], qTb[:, hh, tq * 128:(tq + 1) * 128], True, True, [kTb, qTb])
                                    op("dve", lambda e: e.tensor_tensor(out=sm_[:], in0=PS[bS][:, 0:128], in1=caus[:], op=ALU.mult), reads=[PS[bS], caus], writes=[sm_])
                                    flushC()

                                    def post(t=t, hh=hh, H=H, tq=tq, sm_=sm_):
                                        bA = 4 + hh
                                        bT = 6 + hh
                                        sc = smc[hh]
                                        mm(PS[bA], PS[bA][:, 0:129], sm_[:], vaug[:, t, hh, :], True, False, [sm_, vaug])
                                        mm(PS[bA], PS[bA][:, 0:129], qTb[:, hh, tq * 128:(tq + 1) * 128], CTall[:, t, hh, :], False, True, [qTb, CTall])
                                        fcol = ff[:, t, H:H + 1]
                                        op("act", lambda e: e.activation(out=sc[:, 6:7], in_=PS[bA][:, 128:129], func=AF.Abs, scale=fcol), reads=[PS[bA], ff], writes=[sc])
                                        op("dve", lambda e: e.tensor_scalar(out=sc[:, 0:1], in0=sc[:, 6:7], scalar1=1.0, scalar2=None, op0=ALU.max), reads=[sc], writes=[sc])
                                        op("dve", lambda e: e.reciprocal(out=sc[:, 1:2], in_=sc[:, 0:1]), reads=[sc], writes=[sc])
                                        op("dve", lambda e: e.tensor_tensor(out=sc[:, 2:3], in0=sc[:, 1:2], in1=fcol, op=ALU.mult), reads=[sc, ff], writes=[sc])
                                        op("dve", lambda e: e.scalar_tensor_tensor(out=hm_[hh][:], in0=PS[bA][:, 0:128], scalar=sc[:, 2:3], in1=osig[:, tq, hh * 128:(hh + 1) * 128],
                                                                                   op0=ALU.mult, op1=ALU.mult), reads=[PS[bA], sc, osig], writes=[hm_[hh]])
                                        op("act", lambda e: e.activation(out=jk[hh][:], in_=hm_[hh][:], func=AF.Square, accum_out=sc[:, 3:4]), reads=[hm_[hh]], writes=[jk[hh], sc])
                                        op("act", lambda e: e.activation(out=sc[:, 4:5], in_=sc[:, 3:4], func=AF.Sqrt, bias=c_eps[:], scale=1.0 / 128), reads=[sc, c_eps], writes=[sc])
                                        op("dve", lambda e: e.reciprocal(out=sc[:, 5:6], in_=sc[:, 4:5]), reads=[sc], writes=[sc])
                                        op("dve", lambda e: e.scalar_tensor_tensor(out=yb_[hh][:], in0=hm_[hh][:], scalar=sc[:, 5:6], in1=ghn[:, H * 128:(H + 1) * 128],
                                                                                   op0=ALU.mult, op1=ALU.mult), reads=[hm_[hh], sc, ghn], writes=[yb_[hh]])
                                        op("pe", lambda e: e.transpose(out=psbf(bT)[:, 0:128], in_=yb_[hh][:], identity=idb[:]), reads=[yb_[hh], idb], writes=[PS[bT]])
                                        op("act", lambda e: e.copy(out=YT[:, 4 + H, tq * 128:(tq + 1) * 128], in_=psbf(bT)[:, 0:128]), reads=[PS[bT]], writes=[YT])
                                    pendC[0] = post
                            flushC()
            ckpt("C")
            if "ybT" in dbg:
                o = dbg_t("ybT", [128, 4, S_OWN], BF16)
                fw.dma(o[:, :, :], YT[:, 4:8, :], reads=[YT], is_output=True)


            with fw.scope() as esD:
                x1 = fw.sb([128, NT_OWN, D], F32, "x1", esD)
                with fw.scope() as esD1:
                    mixT = fw.sb([128, 8, S_OWN], BF16, "mixT", esD1)
                    with fw.scope() as esD1a:
                        hTo = fw.sb([128, 8, S_OWN], BF16, "hTo", esD1a)
                        for tb in range(4):
                            fw.dma(hTo[:, :, tb * 512:(tb + 1) * 512], hT_d[:, :, S_OWN + tb * 512:S_OWN + (tb + 1) * 512],
                                   reads=hT_tiles[NT_OWN + 4 * tb:NT_OWN + 4 * tb + 4], writes=[hTo])
                        wga = [fw.sb([128, 8, 128], BF16, f"wga{i}", esD1a) for i in range(2)]
                        wgb = [fw.sb([128, 8, 128], BF16, f"wgb{i}", esD1a) for i in range(2)]
                        wpa = [fw.sb([128, 4, 128], BF16, f"wpa{i}", esD1a) for i in range(2)]
                        wpb = [fw.sb([128, 4, 128], BF16, f"wpb{i}", esD1a) for i in range(2)]
                        sga = [fw.sb([128, 512], BF16, f"sga{i}", esD1a) for i in range(2)]
                        sgb = [fw.sb([128, 512], BF16, f"sgb{i}", esD1a) for i in range(2)]
                        t1 = [fw.sb([128, 512], F32, f"t1_{i}", esD1a) for i in range(2)]
                        t2 = [fw.sb([128, 512], F32, f"t2_{i}", esD1a) for i in range(2)]
                        it = 0
                        for j in range(8):
                            w_ = j % 2
                            fw.dma(wga[w_][:], w_mg_d[:, j * 128:(j + 1) * 128].rearrange("(k p) c -> p k c", p=128), writes=[wga[w_]], q="pool")
                            fw.dma(wgb[w_][:], w_mg_d[:, 1024 + j * 128:1024 + (j + 1) * 128].rearrange("(k p) c -> p k c", p=128), writes=[wgb[w_]], q="pool")
                            fw.dma(wpa[w_][:], w_pa_d[:, j * 128:(j + 1) * 128].rearrange("(k p) c -> p k c", p=128), writes=[wpa[w_]], q="pool")
                            fw.dma(wpb[w_][:], w_pb_d[:, j * 128:(j + 1) * 128].rearrange("(k p) c -> p k c", p=128), writes=[wpb[w_]], q="pool")
                            for tb in range(4):
                                r = it % 2
                                it += 1
                                b0 = 4 * r
                                ts_ = slice(tb * 512, (tb + 1) * 512)
                                for k in range(8):
                                    mm(PS[b0], PS[b0][:, :], wga[w_][:, k, :], hTo[:, k, ts_], k == 0, k == 7, [wga[w_], hTo])
                                op("act", lambda e: e.activation(out=sga[r][:], in_=PS[b0][:, :], func=AF.Sigmoid), reads=[PS[b0]], writes=[sga[r]])
                                for k in range(8):
                                    mm(PS[b0 + 1], PS[b0 + 1][:, :], wgb[w_][:, k, :], hTo[:, k, ts_], k == 0, k == 7, [wgb[w_], hTo])
                                op("act", lambda e: e.activation(out=sgb[r][:], in_=PS[b0 + 1][:, :], func=AF.Sigmoid), reads=[PS[b0 + 1]], writes=[sgb[r]])
                                for k in range(4):
                                    mm(PS[b0 + 2], PS[b0 + 2][:, :], wpa[w_][:, k, :], YT[:, k, ts_], k == 0, k == 3, [wpa[w_], YT])
                                for k in range(4):
                                    mm(PS[b0 + 3], PS[b0 + 3][:, :], wpb[w_][:, k, :], YT[:, 4 + k, ts_], k == 0, k == 3, [wpb[w_], YT])
                                op("dve", lambda e: e.tensor_tensor(out=t1[r][:], in0=PS[b0 + 2][:, :], in1=sga[r][:], op=ALU.mult), reads=[PS[b0 + 2], sga[r]], writes=[t1[r]])
                                op("dve", lambda e: e.tensor_tensor(out=t2[r][:], in0=PS[b0 + 3][:, :], in1=sgb[r][:], op=ALU.mult), reads=[PS[b0 + 3], sgb[r]], writes=[t2[r]])
                                op("pool", lambda e: e.tensor_tensor(out=mixT[:, j, ts_], in0=t1[r][:], in1=t2[r][:], op=ALU.add), reads=[t1[r], t2[r]], writes=[mixT])
                    ckpt("D1a")
                    with fw.scope() as esD1b:
                        w_out = fw.sb([128, 8, D], BF16, "w_out", esD1b)
                        fw.dma(w_out[:], w_out_d.rearrange("(k p) c -> p k c", p=128), writes=[w_out], q="pool")
                        xtl = [fw.sb([128, D], F32, f"xtl{i}", esD1b) for i in range(2)]
                        for t in range(NT_OWN):
                            x_ = xtl[t % 2]
                            fw.dma(x_[:], xe[S_OWN + t * 128:S_OWN + (t + 1) * 128, :], writes=[x_])
                            for half in range(2):
                                b = 2 * (t % 2) + half
                                for j in range(8):
                                    mm(PS[b], PS[b][:, :], mixT[:, j, t * 128:(t + 1) * 128], w_out[:, j, half * 512:(half + 1) * 512], j == 0, j == 7, [mixT, w_out])
                                op("dve", lambda e: e.tensor_tensor(out=x1[:, t, half * 512:(half + 1) * 512], in0=PS[b][:, :], in1=x_[:, half * 512:(half + 1) * 512], op=ALU.add),
                                   reads=[PS[b], x_], writes=[x1])
                ckpt("D1")
                if "x1" in dbg:
                    fw.dma(dbg_t("x1", [128, NT_OWN, D]), x1[:], reads=[x1], is_output=True)
                with fw.scope() as esM:
                    load_gain(1)
                    gateT = fw.sb([16, S_OWN], BF16, "gateT", esM)
                    E16 = fw.sb([16, 16, 128], BF16, "E16", esM)
                    op("pool", lambda e: e.memset(E16[:], 1.0), writes=[E16])
                    op("pool", lambda e: e.affine_select(out=E16[:], in_=E16[:], pattern=[[-1, 16], [0, 128]], compare_op=ALU.is_equal, fill=0.0,
                                                         base=0, channel_multiplier=1), reads=[E16], writes=[E16])
                    with fw.scope() as esR:
                        w_r = fw.sb([128, 8, 20], F32, "w_r", esR)
                        fw.dma(w_r[:], w_r_d.rearrange("(k p) c -> p k c", p=128), writes=[w_r])
                        b_r = fw.sb([128, 20], F32, "b_r", esR)
                        fw.dma(b_r[:], b_r_d[0:1, :].to_broadcast([128, 20]), writes=[b_r])
                        hnf = [fw.sb([128, D], F32, f"hnf{i}", esR) for i in range(2)]
                        hnTf = [fw.sb([128, 8, 128], F32, f"hnTf{i}", esR) for i in range(2)]
                        junkR = fw.sb([128, D], BF16, "junkR", esR)
                        ssr = [fw.sb([128, 1], F32, f"ssr{i}", esR) for i in range(2)]
                        rrr = [fw.sb([128, 1], F32, f"rrr{i}", esR) for i in range(2)]
                        T_ = NT_OWN
                        lgA = fw.sb([128, T_, 20], F32, "lgA", esR)

                        def r_front(t):
                            r = t % 2
                            rs = {"ss": ssr[r], "r": rrr[r]}
                            rms_rstd({"ap": x1[:, t, :], "bufs": [x1]}, rs, D, {"ap": junkR[:], "buf": junkR})
                            op("dve", lambda e: e.scalar_tensor_tensor(out=hnf[r][:], in0=x1[:, t, :], scalar=rs["r"][:], in1=gB[:], op0=ALU.mult, op1=ALU.mult),
                               reads=[x1, rs["r"], gB], writes=[hnf[r]])
                            for k in range(8):
                                b = 2 * r + (0 if k < 4 else 1)
                                op("pe", lambda e: e.transpose(out=PS[b][:, (k % 4) * 128:(k % 4 + 1) * 128], in_=hnf[r][:, k * 128:(k + 1) * 128], identity=idf[:]),
                                   reads=[hnf[r], idf], writes=[PS[b]])
                            for bb in range(2):
                                b = 2 * r + bb
                                op("act", lambda e: e.copy(out=hnTf[r][:, 4 * bb:4 * bb + 4, :], in_=PS[b][:, :].rearrange("p (k t) -> p k t", k=4)), reads=[PS[b]], writes=[hnTf[r]])
                                op("dve", lambda e: e.tensor_copy(out=YT[:, 4 * bb:4 * bb + 4, t * 128:(t + 1) * 128], in_=PS[b][:, :].rearrange("p (k t) -> p k t", k=4)),
                                   reads=[PS[b]], writes=[YT])

                        def r_back(t):
                            r = t % 2
                            bl = 4 + r
                            for k in range(8):
                                mm(PS[bl], PS[bl][:, 0:20], hnTf[r][:, k, :], w_r[:, k, :], k == 0, k == 7, [hnTf[r], w_r])
                            op("dve", lambda e: e.tensor_tensor(out=lgA[:, t, :], in0=PS[bl][:, 0:20], in1=b_r[:], op=ALU.add), reads=[PS[bl], b_r], writes=[lgA])

                        for t in range(T_ + 1):
                            if t < T_:
                                r_front(t)
                            if t >= 1:
                                r_back(t - 1)
                        gl = lgA[:, :, 0:4]
                        el = lgA[:, :, 4:20].rearrange("p t (g e) -> p t g e", g=4)
                        gmax = fw.sb([128, T_], F32, "gmax", esR)
                        g1h = fw.sb([128, T_, 4], F32, "g1h", esR)
                        exg = fw.sb([128, T_, 4], F32, "exg", esR)
                        pgs = fw.sb([128, T_], F32, "pgs", esR)
                        t16 = fw.sb([128, T_, 4, 4], F32, "t16", esR)
                        elg = fw.sb([128, T_, 4], F32, "elg", esR)
                        elg2 = fw.sb([128, T_, 4], F32, "elg2", esR)
                        ev1 = fw.sb([128, T_], F32, "ev1", esR)
                        ev2 = fw.sb([128, T_], F32, "ev2", esR)
                        mk1 = fw.sb([128, T_, 4], F32, "mk1", esR)
                        mk2 = fw.sb([128, T_, 4], F32, "mk2", esR)
                        w12 = fw.sb([128, 2, T_], F32, "w12", esR)
                        gig = fw.sb([128, T_, 4], F32, "gig", esR)
                        gate = fw.sb([128, T_, 4, 4], F32, "gate", esR)
                        B3 = [128, T_, 4]
                        op("dve", lambda e: e.tensor_reduce(out=gmax[:], in_=gl, axis=AX.X, op=ALU.max), reads=[lgA], writes=[gmax])
                        op("dve", lambda e: e.tensor_tensor(out=g1h[:], in0=gl, in1=gmax[:].unsqueeze(2).to_broadcast(B3), op=ALU.is_equal), reads=[lgA, gmax], writes=[g1h])
                        op("dve", lambda e: e.tensor_tensor(out=exg[:], in0=gl, in1=gmax[:].unsqueeze(2).to_broadcast(B3), op=ALU.subtract), reads=[lgA, gmax], writes=[exg])
                        op("act", lambda e: e.activation(out=exg[:], in_=exg[:], func=AF.Exp), reads=[exg], writes=[exg])
                        op("dve", lambda e: e.tensor_reduce(out=pgs[:], in_=exg[:], axis=AX.X, op=ALU.add), reads=[exg], writes=[pgs])
                        op("dve", lambda e: e.reciprocal(out=pgs[:], in_=pgs[:]), reads=[pgs], writes=[pgs])
                        op("dve", lambda e: e.tensor_tensor(out=t16[:], in0=el, in1=g1h[:].unsqueeze(3).to_broadcast([128, T_, 4, 4]), op=ALU.mult), reads=[lgA, g1h], writes=[t16])
                        op("dve", lambda e: e.tensor_reduce(out=elg[:], in_=t16[:].rearrange("p t g e -> p t e g"), axis=AX.X, op=ALU.add), reads=[t16], writes=[elg])
                        op("dve", lambda e: e.tensor_reduce(out=ev1[:], in_=elg[:], axis=AX.X, op=ALU.max), reads=[elg], writes=[ev1])
                        op("dve", lambda e: e.tensor_tensor(out=mk1[:], in0=elg[:], in1=ev1[:].unsqueeze(2).to_broadcast(B3), op=ALU.is_equal), reads=[elg, ev1], writes=[mk1])
                        op("dve", lambda e: e.scalar_tensor_tensor(out=elg2[:], in0=mk1[:], scalar=-1e30, in1=elg[:], op0=ALU.mult, op1=ALU.add), reads=[mk1, elg], writes=[elg2])
                        op("dve", lambda e: e.tensor_reduce(out=ev2[:], in_=elg2[:], axis=AX.X, op=ALU.max), reads=[elg2], writes=[ev2])
                        op("dve", lambda e: e.tensor_tensor(out=mk2[:], in0=elg2[:], in1=ev2[:].unsqueeze(2).to_broadcast(B3), op=ALU.is_equal), reads=[elg2, ev2], writes=[mk2])
                        op("dve", lambda e: e.tensor_tensor(out=w12[:, 0, :], in0=ev1[:], in1=ev2[:], op=ALU.subtract), reads=[ev1, ev2], writes=[w12])
                        op("act", lambda e: e.activation(out=w12[:, 0, :], in_=w12[:, 0, :], func=AF.Sigmoid), reads=[w12], writes=[w12])
                        op("dve", lambda e: e.tensor_scalar(out=w12[:, 1, :], in0=w12[:, 0, :], scalar1=-1.0, scalar2=1.0, op0=ALU.mult, op1=ALU.add), reads=[w12], writes=[w12])
                        op("dve", lambda e: e.tensor_tensor(out=w12[:], in0=w12[:], in1=pgs[:].unsqueeze(1).to_broadcast([128, 2, T_]), op=ALU.mult), reads=[w12, pgs], writes=[w12])
                        op("dve", lambda e: e.tensor_tensor(out=gig[:], in0=mk1[:], in1=w12[:, 0, :].unsqueeze(2).to_broadcast(B3), op=ALU.mult), reads=[mk1, w12], writes=[gig])
                        op("dve", lambda e: e.tensor_tensor(out=mk2[:], in0=mk2[:], in1=w12[:, 1, :].unsqueeze(2).to_broadcast(B3), op=ALU.mult), reads=[mk2, w12], writes=[mk2])
                        op("dve", lambda e: e.tensor_tensor(out=gig[:], in0=gig[:], in1=mk2[:], op=ALU.add), reads=[gig, mk2], writes=[gig])
                        op("dve", lambda e: e.tensor_tensor(out=gate[:], in0=g1h[:].unsqueeze(3).to_broadcast([128, T_, 4, 4]),
                                                            in1=gig[:].unsqueeze(2).to_broadcast([128, T_, 4, 4]), op=ALU.mult), reads=[g1h, gig], writes=[gate])
                        for t4 in range(T_ // 4):
                            bk = 6 + t4 % 2
                            for j in range(4):
                                t = t4 * 4 + j
                                op("pe", lambda e: e.transpose(out=PS[bk][0:16, j * 128:(j + 1) * 128], in_=gate[:, t, :, :].rearrange("p g e -> p (g e)"), identity=idf[:]),
                                   reads=[gate, idf], writes=[PS[bk]])
                            op("act", lambda e: e.copy(out=gateT[:, t4 * 512:(t4 + 1) * 512], in_=PS[bk][0:16, :]), reads=[PS[bk]], writes=[gateT])
                    ckpt("D2r")
                    if "gateT" in dbg:
                        fw.dma(dbg_t("gateT", [16, S_OWN], BF16), gateT[:], reads=[gateT], is_output=True)
                    with fw.scope() as esE:
                        w13 = [fw.sb([128, 8, 512], BF16, f"w13_{i}", esE) for i in range(2)]
                        w2e = [fw.sb([128, 2, D], BF16, f"w2e_{i}", esE) for i in range(2)]
                        sgE = [fw.sb([128, 512], F32, f"sgE{i}", esE) for i in range(2)]
                        tE = [fw.sb([128, 512], F32, f"tE{i}", esE) for i in range(2)]
                        actT = [[fw.sb([128, 512], BF16, f"actT{i}{fc}", esE) for fc in range(2)] for i in range(2)]
                        ybank = [4, 5, 7]
                        yi = 0
                        it = 0
                        for ex in range(16):
                            wb = ex % 2
                            fw.dma(w13[wb][:], w_e13_d[ex].rearrange("(k p) c -> p k c", p=128), writes=[w13[wb]], q="pool")
                            fw.dma(w2e[wb][:], w_e2_d[ex].rearrange("(k p) c -> p k c", p=128), writes=[w2e[wb]], q="pool")
                            for tb in range(4):
                                r = it % 2
                                it += 1
                                ts_ = slice(tb * 512, (tb + 1) * 512)
                                mm(PS[6], PS[6][:, :], E16[:, ex, :], gateT[:, ts_], True, True, [E16, gateT])
                                for fc in range(2):
                                    for k in range(8):
                                        mm(PS[fc], PS[fc][:, :], w13[wb][:, k, fc * 128:(fc + 1) * 128], YT[:, k, ts_], k == 0, k == 7, [w13[wb], YT])
                                    for k in range(8):
                                        mm(PS[2 + fc], PS[2 + fc][:, :], w13[wb][:, k, 256 + fc * 128:256 + (fc + 1) * 128], YT[:, k, ts_], k == 0, k == 7, [w13[wb], YT])
                                    op("act", lambda e: e.activation(out=sgE[fc][:], in_=PS[fc][:, :], func=AF.Silu), reads=[PS[fc]], writes=[sgE[fc]])
                                    op("dve", lambda e: e.tensor_tensor(out=tE[fc][:], in0=PS[2 + fc][:, :], in1=sgE[fc][:], op=ALU.mult), reads=[PS[2 + fc], sgE[fc]], writes=[tE[fc]])
                                    op("dve", lambda e: e.tensor_tensor(out=actT[r][fc][:], in0=PS[6][:, :], in1=tE[fc][:], op=ALU.mult), reads=[PS[6], tE[fc]], writes=[actT[r][fc]])
                                for tt in range(4):
                                    t = tb * 4 + tt
                                    for half in range(2):
                                        b = ybank[yi % 3]
                                        yi += 1
                                        for fc in range(2):
                                            mm(PS[b], PS[b][:, :], actT[r][fc][:, tt * 128:(tt + 1) * 128], w2e[wb][:, fc, half * 512:(half + 1) * 512], fc == 0, fc == 1, [actT[r][fc], w2e[wb]])
                                        op("dve", lambda e: e.tensor_tensor(out=x1[:, t, half * 512:(half + 1) * 512], in0=PS[b][:, :], in1=x1[:, t, half * 512:(half + 1) * 512], op=ALU.add),
                                           reads=[PS[b], x1], writes=[x1])
                ckpt("D2")
                if "x2" in dbg:
                    fw.dma(dbg_t("x2", [128, NT_OWN, D]), x1[:], reads=[x1], is_output=True)
                with fw.scope() as esP:
                    load_gain(2)
                    gB2 = fw.sb([128, D], F32, "gB2", esP)
                    fw.dma(gB2[:], gvec_d[3:4, :].to_broadcast([128, D]), writes=[gB2])
                    w_pg = fw.sb([128, 8, D], BF16, "w_pg", esP)
                    fw.dma(w_pg[:], w_pg_d.rearrange("(k p) c -> p k c", p=128), writes=[w_pg], q="pool")
                    w_pp = fw.sb([128, 2, D], BF16, "w_pp", esP)
                    fw.dma(w_pp[:], w_pp_d.rearrange("(k p) c -> p k c", p=128), writes=[w_pp], q="pool")
                    hpb = [fw.sb([128, D], BF16, f"hpb{i}", esP) for i in range(2)]
                    hpT = [fw.sb([128, 8, 128], BF16, f"hpT{i}", esP) for i in range(2)]
                    plb = [fw.sb([128, 256], BF16, f"plb{i}", esP) for i in range(2)]
                    plT = [fw.sb([128, 2, 128], BF16, f"plT{i}", esP) for i in range(2)]
                    sgP = [fw.sb([128, 512], F32, f"sgP{i}", esP) for i in range(2)]
                    tP = [fw.sb([128, 512], F32, f"tP{i}", esP) for i in range(2)]
                    outt = [fw.sb([128, D], F32, f"outt{i}", esP) for i in range(2)]
                    junkP = fw.sb([128, D], BF16, "junkP", esP)
                    ssp = [fw.sb([128, 1], F32, f"ssp{i}", esP) for i in range(4)]
                    rrp = [fw.sb([128, 1], F32, f"rrp{i}", esP) for i in range(4)]
                    for t in range(NT_OWN):
                        r = t % 2
                        fw.dma(plb[r][:], pl_d[t * 128:(t + 1) * 128, :], writes=[plb[r]], q="pool")
                        rs = {"ss": ssp[r], "r": rrp[r]}
                        rms_rstd({"ap": x1[:, t, :], "bufs": [x1]}, rs, D, {"ap": junkP[:], "buf": junkP})
                        op("dve", lambda e: e.scalar_tensor_tensor(out=hpb[r][:], in0=x1[:, t, :], scalar=rs["r"][:], in1=gB[:], op0=ALU.mult, op1=ALU.mult),
                           reads=[x1, rs["r"], gB], writes=[hpb[r]])
                        for k in range(8):
                            op("pe", lambda e: e.transpose(out=psbf(0)[:, k * 128:(k + 1) * 128], in_=hpb[r][:, k * 128:(k + 1) * 128], identity=idb[:]), reads=[hpb[r], idb], writes=[PS[0]])
                        op("act", lambda e: e.copy(out=hpT[r][:], in_=psbf(0).rearrange("p (k t) -> p k t", k=8)), reads=[PS[0]], writes=[hpT[r]])
                        for k in range(2):
                            op("pe", lambda e: e.transpose(out=psbf(1)[:, k * 128:(k + 1) * 128], in_=plb[r][:, k * 128:(k + 1) * 128], identity=idb[:]), reads=[plb[r], idb], writes=[PS[1]])
                        op("act", lambda e: e.copy(out=plT[r][:], in_=psbf(1)[:, 0:256].rearrange("p (k t) -> p k t", k=2)), reads=[PS[1]], writes=[plT[r]])
                        for half in range(2):
                            hs = slice(half * 512, (half + 1) * 512)
                            bG = 2 + half
                            bP = 4 + half
                            for k in range(8):
                                mm(PS[bG], PS[bG][:, :], hpT[r][:, k, :], w_pg[:, k, hs], k == 0, k == 7, [hpT[r], w_pg])
                            for k in range(2):
                                mm(PS[bP], PS[bP][:, :], plT[r][:, k, :], w_pp[:, k, hs], k == 0, k == 1, [plT[r], w_pp])
                            op("act", lambda e: e.activation(out=sgP[half][:], in_=PS[bG][:, :], func=AF.Sigmoid), reads=[PS[bG]], writes=[sgP[half]])
                            op("dve", lambda e: e.tensor_tensor(out=tP[half][:], in0=PS[bP][:, :], in1=sgP[half][:], op=ALU.mult), reads=[PS[bP], sgP[half]], writes=[tP[half]])
                            op("dve", lambda e: e.tensor_tensor(out=x1[:, t, hs], in0=x1[:, t, hs], in1=tP[half][:], op=ALU.add), reads=[x1, tP[half]], writes=[x1])
                        rs2 = {"ss": ssp[2 + r], "r": rrp[2 + r]}
                        rms_rstd({"ap": x1[:, t, :], "bufs": [x1]}, rs2, D, {"ap": junkP[:], "buf": junkP})
                        op("dve", lambda e: e.scalar_tensor_tensor(out=outt[r][:], in0=x1[:, t, :], scalar=rs2["r"][:], in1=gB2[:], op0=ALU.mult, op1=ALU.mult),
                           reads=[x1, rs2["r"], gB2], writes=[outt[r]])
                        fw.dma(out_d[t * 128:(t + 1) * 128, :], outt[r][:], reads=[outt[r]], is_output=True)

            if "yaT" in dbg:
                o = dbg_t("yaT", [128, 4, S_OWN], BF16)
                fw.dma(o[:, :, :], YT[:, 0:4, :], reads=[YT], is_output=True)

            if "hT" in dbg:
                o = dbg_t("hT", [128, 8, S_EXT], BF16)
                with fw.scope() as esd:
                    tmp = fw.sb([128, 8, 512], BF16, "dbg_hT", esd)
                    for i in range(8):
                        fw.dma(tmp[:], hT_d[:, :, i * 512:(i + 1) * 512], reads=hT_tiles[4 * i:4 * i + 4], writes=[tmp])
                        fw.dma(o[:, :, i * 512:(i + 1) * 512], tmp[:], reads=[tmp], is_output=True)


        body()
        fw.stopped = False
        fw.finish()
    return nc, dbg_out


_INV = (500000.0 ** (-np.arange(0, 16, 2, dtype=np.float32) / 16.0)).astype(np.float32)


def make_in_maps(inputs):
    f = lambda a: np.ascontiguousarray(np.asarray(a), dtype=np.float32)
    x = f(inputs["x"]); p = f(inputs["p"])
    positions = np.asarray(inputs["positions"]).astype(np.int32)
    w_in = f(inputs["w_in"])[0]
    offs = np.cumsum([0, 512, 128, 128, 128, 128, 128, 128, 24, 1024, 512, 512, 8, 2048])
    seg = {n: (offs[i], offs[i + 1]) for i, n in enumerate(["q", "kc", "vc", "ks", "vs", "kw", "vw", "gate", "qk", "v", "o", "if", "mg"])}
    col = lambda n: w_in[:, seg[n][0]:seg[n][1]]
    w_att = []
    for g in range(2):
        parts = [col("q")[:, g * 256:(g + 1) * 256]]
        for n in ["ks", "kw", "kc", "vc", "vs", "vw"]:
            parts.append(col(n)[:, g * 64:(g + 1) * 64])
        parts.append(col("gate")[:, g * 12:(g + 1) * 12])
        w_att.append(np.concatenate(parts, axis=1))
    w_att = np.ascontiguousarray(np.stack(w_att))
    shared = {
        "invf": np.ascontiguousarray(np.broadcast_to(_INV[None, :], (128, 8))),
        "gvec": np.ascontiguousarray(np.stack([f(inputs["g_mix"])[0], f(inputs["g_ffn"])[0], f(inputs["g_ple"])[0], f(inputs["g_final"])])),
        "w_att": w_att,
        "w_qk": np.ascontiguousarray(col("qk")),
        "w_vo": np.ascontiguousarray(np.concatenate([col("v"), col("o")], axis=1)),
        "w_if": np.ascontiguousarray(col("if")),
        "w_mg": np.ascontiguousarray(col("mg")),
        "b_if": f(inputs["b_if"]).reshape(1, 8),
        "w_c1": np.ascontiguousarray(np.stack([f(inputs["w_ck1"])[0], f(inputs["w_cv1"])[0]])),
        "w_c2": np.ascontiguousarray(np.stack([f(inputs["w_ck2"])[0], f(inputs["w_cv2"])[0]])),
        "pe_c": np.ascontiguousarray(np.stack([f(inputs["pe_ck"])[0], f(inputs["pe_cv"])[0]])),
        "wc": np.ascontiguousarray(f(inputs["w_conv"])[0].reshape(4, 8, 128).transpose(2, 1, 0)),
        "bc": np.ascontiguousarray(f(inputs["b_conv"])[0].reshape(8, 128).T),
        "g_hn": f(inputs["g_hn"]).reshape(1, 512),
        "w_pa": f(inputs["w_pa"])[0], "w_pb": f(inputs["w_pb"])[0], "w_out": f(inputs["w_out"])[0],
        "w_r": np.ascontiguousarray(np.concatenate([f(inputs["w_rg"])[0], f(inputs["w_re"])[0]], axis=1)),
        "b_r": np.ascontiguousarray(np.concatenate([f(inputs["b_rg"])[0], f(inputs["b_re"])[0]])[None, :]),
        "w_e13": f(inputs["w_e13"])[0], "w_e2": f(inputs["w_e2"])[0],
        "w_pg": f(inputs["w_pg"])[0], "w_pp": f(inputs["w_pp"])[0],
    }
    in_maps = []
    for core in range(8):
        b, half = core // 2, core % 2
        if half == 1:
            xe_ = x[b]
            pos_ = positions[b]
        else:
            xe_ = np.concatenate([np.zeros((S_OWN, D), np.float32), x[b, :S_OWN]], axis=0)
            pos_ = np.concatenate([np.zeros(S_OWN, np.int32), positions[b, :S_OWN]])
        m = dict(shared)
        m["xe"] = np.ascontiguousarray(xe_)
        m["pos"] = np.ascontiguousarray(pos_.reshape(NT_EXT, 128).T)
        m["pl"] = np.ascontiguousarray(p[0, b, half * S_OWN:(half + 1) * S_OWN])
        m["hv"] = np.full((128, 1), float(half), np.float32)
        in_maps.append(m)
    return in_maps


def kernel(**inputs):
    nc, _ = build_program()
    in_maps = make_in_maps(inputs)
    res = run_bass_kernel_spmd(nc, in_maps, core_ids=list(range(8)))
    out = np.zeros((4, S_EXT, D), np.float32)
    for core in range(8):
        b, half = core // 2, core % 2
        out[b, half * S_OWN:(half + 1) * S_OWN] = res.results[core]["out"]
    return out
```

```python
import numpy as np
import concourse.bass as bass
import concourse.mybir as mybir
from concourse.bass_utils import run_bass_kernel_spmd
from contextlib import ExitStack

F32 = mybir.dt.float32
BF16 = mybir.dt.bfloat16
I32 = mybir.dt.int32
AF = mybir.ActivationFunctionType
ALU = mybir.AluOpType
AX = mybir.AxisListType

D = 1024
S_OWN = 2048
S_EXT = 4096
NT_OWN = 16
NT_EXT = 32
EPS = 1e-6
NEGB = -30000.0
DBG = []


class Buf:
    __slots__ = ("t", "lw", "rd", "name", "excl")

    def __init__(self, t, name=""):
        self.t = t
        self.excl = False
        self.lw = None
        self.rd = {}
        self.name = name

    def __getitem__(self, k):
        return self.t[k]


class FW:
    NDMA = 24

    def __init__(self, nc, es):
        self.nc = nc
        self.es = es
        self.eng = {"pe": nc.tensor, "act": nc.scalar, "dve": nc.vector, "pool": nc.gpsimd, "sp": nc.sync}
        self.sem = {k: es.enter_context(nc.semaphore("s_" + k)) for k in self.eng}
        self.cnt = {k: 0 for k in self.eng}
        self.known = {k: {} for k in self.eng}
        self.dsem = [es.enter_context(nc.semaphore(f"s_dma{i}")) for i in range(self.NDMA)]
        self.dval = [0] * self.NDMA
        self.dnext = 0
        self.nbuf = 0
        self.out_waits = []
        self.stopped = False

    def sb(self, shape, dt, name=None, es=None):
        self.nbuf += 1
        name = f"sb{self.nbuf}_" + (name or "t")
        return Buf((es or self.es).enter_context(self.nc.sbuf_tensor(name, list(shape), dt)), name)

    def ps(self, shape, dt, name=None):
        self.nbuf += 1
        name = name or f"ps{self.nbuf}"
        b = Buf(self.es.enter_context(self.nc.psum_tensor(name, list(shape), dt)), name)
        b.excl = True
        return b

    def _wait(self, e, src, idx):
        if self.stopped:
            return
        kn = self.known[e]
        if kn.get(src, 0) >= idx:
            return
        s = self.dsem[src[1]] if isinstance(src, tuple) else self.sem[src]
        self.eng[e].wait_ge(s, idx)
        kn[src] = idx

    def _deps(self, e, reads, writes):
        for b in reads:
            if b.lw is not None:
                self._wait(e, b.lw[0], b.lw[1])
            if b.excl:
                for src, idx in b.rd.items():
                    if src != e:
                        self._wait(e, src, idx)
        for b in writes:
            if b.lw is not None and b.lw[0] != e:
                self._wait(e, b.lw[0], b.lw[1])
            for src, idx in b.rd.items():
                if src != e:
                    self._wait(e, src, idx)

    def op(self, e, fn, reads=(), writes=()):
        if self.stopped:
            return None
        self._deps(e, reads, writes)
        inst = fn(self.eng[e])
        self.cnt[e] += 1
        c = self.cnt[e]
        inst.then_inc(self.sem[e], 1)
        for b in reads:
            if b.rd.get(e, 0) < c:
                b.rd[e] = c
        for b in writes:
            b.lw = (e, c)
            b.rd = {}
        return inst

    def dma(self, out, in_, reads=(), writes=(), q="sp", is_output=False):
        if self.stopped and not is_output:
            return None
        self._deps(q, reads, writes)
        slot = self.dnext
        self.dnext = (self.dnext + 1) % self.NDMA
        key = ("d", slot)
        if self.dval[slot] > 0:
            self._wait(q, key, self.dval[slot])
        inst = self.eng[q].dma_start(out=out, in_=in_)
        self.dval[slot] += 16
        inst.then_inc(self.dsem[slot], 16)
        v = self.dval[slot]
        for b in reads:
            if b.rd.get(key, 0) < v:
                b.rd[key] = v
        for b in writes:
            b.lw = (key, v)
            b.rd = {}
        if is_output:
            self.out_waits.append((key, v))
        return inst

    def barrier(self):
        for e in self.eng:
            for src in ("pe", "act", "dve", "pool"):
                if src != e and self.cnt[src] > 0:
                    self._wait(e, src, self.cnt[src])
            for slot in range(self.NDMA):
                if self.dval[slot] > 0:
                    self._wait(e, ("d", slot), self.dval[slot])

    def scope(self):
        fw = self

        class _Scope(ExitStack):
            def __exit__(self, *a):
                fw.barrier()
                return super().__exit__(*a)
        return _Scope()

    def finish(self):
        for key, v in self.out_waits:
            self._wait("sp", key, v)
        for k in ("pe", "act", "dve", "pool"):
            if self.cnt[k] > 0:
                self._wait("sp", k, self.cnt[k])


class _StopBuild(Exception):
    pass


def build_program(dbg=()):
    nc = bass.Bass("TRN2", target_bir_lowering=False)

    def din(name, shape, dt=F32):
        return nc.dram_tensor(name, list(shape), dt, kind="ExternalInput").ap()

    xe = din("xe", [S_EXT, D])
    pos_d = din("pos", [128, NT_EXT], I32)
    pl_d = din("pl", [S_OWN, 256])
    hv_d = din("hv", [128, 1])
    invf_d = din("invf", [128, 8])
    gvec_d = din("gvec", [4, D])
    w_att_d = din("w_att", [2, D, 652])
    w_qk_d = din("w_qk", [D, 1024])
    w_vo_d = din("w_vo", [D, 1024])
    w_if_d = din("w_if", [D, 8])
    w_mg_d = din("w_mg", [D, 2048])
    b_if_d = din("b_if", [1, 8])
    w_c1_d = din("w_c1", [2, 2048, 256])
    w_c2_d = din("w_c2", [2, 256, 64])
    pe_c_d = din("pe_c", [2, 32, 64])
    wc_d = din("wc", [128, 8, 4])
    bc_d = din("bc", [128, 8])
    g_hn_d = din("g_hn", [1, 512])
    w_pa_d = din("w_pa", [512, D])
    w_pb_d = din("w_pb", [512, D])
    w_out_d = din("w_out", [D, D])
    w_r_d = din("w_r", [D, 20])
    b_r_d = din("b_r", [1, 20])
    w_e13_d = din("w_e13", [16, D, 512])
    w_e2_d = din("w_e2", [16, 256, D])
    w_pg_d = din("w_pg", [D, D])
    w_pp_d = din("w_pp", [256, D])
    out_d = nc.dram_tensor("out", [S_OWN, D], F32, kind="ExternalOutput").ap()
    hT_d = nc.dram_tensor("hT_scr", [128, 8, S_EXT], BF16, kind="Internal").ap()
    dbg_out = {}

    def dbg_t(name, shape, dt=F32):
        dbg_out[name] = nc.dram_tensor("dbg_" + name, list(shape), dt, kind="ExternalOutput").ap()
        return dbg_out[name]

    with ExitStack() as es:
        fw = FW(nc, es)
        op = fw.op
        PS = [fw.ps([128, 512], F32, f"psb{i}") for i in range(8)]

        def psbf(i):
            return PS[i][:].bitcast(BF16)

        ones_f = fw.sb([128, 128], F32, "ones_f")
        op("pool", lambda e: e.memset(ones_f[:], 1.0), writes=[ones_f])
        idf = fw.sb([128, 128], F32, "idf")
        op("pool", lambda e: e.affine_select(out=idf[:], in_=ones_f[:], pattern=[[1, 128]], compare_op=ALU.is_equal,
                                             fill=0.0, base=0, channel_multiplier=-1), reads=[ones_f], writes=[idf])
        idb = fw.sb([128, 128], BF16, "idb")
        op("dve", lambda e: e.tensor_copy(out=idb[:], in_=idf[:]), reads=[idf], writes=[idb])
        U_f = fw.sb([128, 128], F32, "U_f")
        op("pool", lambda e: e.affine_select(out=U_f[:], in_=ones_f[:], pattern=[[1, 128]], compare_op=ALU.is_ge,
                                             fill=0.0, base=0, channel_multiplier=-1), reads=[ones_f], writes=[U_f])
        caus = fw.sb([128, 128], BF16, "caus")
        op("dve", lambda e: e.tensor_copy(out=caus[:], in_=U_f[:]), reads=[U_f], writes=[caus])
        wm0_f = fw.sb([128, 128], F32, "wm0_f")
        op("pool", lambda e: e.affine_select(out=wm0_f[:], in_=ones_f[:], pattern=[[-1, 128]], compare_op=ALU.is_ge,
                                             fill=0.0, base=-1, channel_multiplier=1), reads=[ones_f], writes=[wm0_f])
        wm0 = fw.sb([128, 128], BF16, "wm0")
        op("dve", lambda e: e.tensor_copy(out=wm0[:], in_=wm0_f[:]), reads=[wm0_f], writes=[wm0])
        c_eps = fw.sb([128, 1], F32, "c_eps")
        op("pool", lambda e: e.memset(c_eps[:], EPS), writes=[c_eps])
        c_one = fw.sb([128, 1], F32, "c_one")
        op("pool", lambda e: e.memset(c_one[:], 1.0), writes=[c_one])
        c_zero = fw.sb([128, 1], F32, "c_zero")
        op("pool", lambda e: e.memset(c_zero[:], 0.0), writes=[c_zero])
        acc_junk = fw.sb([128, 2], F32, "acc_junk")
        op("act", lambda e: e.activation(out=acc_junk[:, 0:1], in_=c_one[:], func=AF.Square, accum_out=acc_junk[:, 1:2]),
           reads=[c_one], writes=[acc_junk])
        hv = fw.sb([128, 1], F32, "hv")
        fw.dma(hv[:], hv_d[:, :], writes=[hv])
        hbias = fw.sb([128, 1], F32, "hbias")
        op("dve", lambda e: e.tensor_scalar(out=hbias[:], in0=hv[:], scalar1=-1.0, scalar2=-NEGB, op0=ALU.add, op1=ALU.mult),
           reads=[hv], writes=[hbias])
        gB = fw.sb([128, D], F32, "gB")

        def load_gain(i):
            fw.dma(gB[:], gvec_d[i:i + 1, :].to_broadcast([128, D]), writes=[gB])

        cs = fw.sb([128, NT_EXT, 8], F32, "cs")
        sn = fw.sb([128, NT_EXT, 8], F32, "sn")
        with fw.scope() as es1:
            posi = fw.sb([128, NT_EXT], I32, "posi", es1)
            posf = fw.sb([128, NT_EXT], F32, "posf", es1)
            invf = fw.sb([128, 8], F32, "invf", es1)
            ang = fw.sb([128, NT_EXT, 8], F32, "ang", es1)
            kf = fw.sb([128, NT_EXT, 8], F32, "kf", es1)
            ki = fw.sb([128, NT_EXT, 8], I32, "ki", es1)
            r1 = fw.sb([128, NT_EXT, 8], F32, "r1", es1)
            r2 = fw.sb([128, NT_EXT, 8], F32, "r2", es1)
            fw.dma(posi[:], pos_d[:, :], writes=[posi])
            fw.dma(invf[:], invf_d[:, :], writes=[invf])
            op("dve", lambda e: e.tensor_copy(out=posf[:], in_=posi[:]), reads=[posi], writes=[posf])
            op("dve", lambda e: e.tensor_tensor(out=ang[:], in0=posf[:].unsqueeze(2).to_broadcast([128, NT_EXT, 8]),
                                                in1=invf[:].unsqueeze(1).to_broadcast([128, NT_EXT, 8]), op=ALU.mult),
               reads=[posf, invf], writes=[ang])
            TWO_PI = 6.283185307179586
            C1 = 6.28125
            C2 = TWO_PI - C1
            PI_LO = 3.1415925
            op("dve", lambda e: e.tensor_scalar(out=kf[:], in0=ang[:], scalar1=1.0 / TWO_PI, scalar2=None, op0=ALU.mult),
               reads=[ang], writes=[kf])
            op("dve", lambda e: e.tensor_copy(out=ki[:], in_=kf[:]), reads=[kf], writes=[ki])
            op("dve", lambda e: e.tensor_copy(out=kf[:], in_=ki[:]), reads=[ki], writes=[kf])
            op("dve", lambda e: e.scalar_tensor_tensor(out=r1[:], in0=kf[:], scalar=-C1, in1=ang[:], op0=ALU.mult, op1=ALU.add),
               reads=[kf, ang], writes=[r1])
            op("dve", lambda e: e.scalar_tensor_tensor(out=r1[:], in0=kf[:], scalar=-C2, in1=r1[:], op0=ALU.mult, op1=ALU.add),
               reads=[kf, r1], writes=[r1])
            op("dve", lambda e: e.tensor_scalar(out=r1[:], in0=r1[:], scalar1=PI_LO, scalar2=-PI_LO, op0=ALU.min, op1=ALU.max),
               reads=[r1], writes=[r1])
            op("act", lambda e: e.activation(out=sn[:], in_=r1[:], func=AF.Sin), reads=[r1], writes=[sn])
            op("dve", lambda e: e.tensor_scalar(out=r2[:], in0=r1[:], scalar1=PI_LO / 2 + 0.0, scalar2=None, op0=ALU.add),
               reads=[r1], writes=[r2])
            op("dve", lambda e: e.tensor_scalar(out=kf[:], in0=r2[:], scalar1=PI_LO, scalar2=-TWO_PI, op0=ALU.is_gt, op1=ALU.mult),
               reads=[r2], writes=[kf])
            op("dve", lambda e: e.tensor_tensor(out=r2[:], in0=r2[:], in1=kf[:], op=ALU.add), reads=[r2, kf], writes=[r2])
            op("dve", lambda e: e.tensor_scalar(out=r2[:], in0=r2[:], scalar1=PI_LO, scalar2=-PI_LO, op0=ALU.min, op1=ALU.max),
               reads=[r2], writes=[r2])
            op("act", lambda e: e.activation(out=cs[:], in_=r2[:], func=AF.Sin), reads=[r2], writes=[cs])

        def rms_rstd(src, rstd, n, junk):
            ss = rstd["ss"]
            op("act", lambda e: e.activation(out=junk["ap"], in_=src["ap"], func=AF.Square, accum_out=ss[:]),
               reads=src["bufs"], writes=[junk["buf"], ss])
            op("act", lambda e: e.activation(out=ss[:], in_=ss[:], func=AF.Sqrt, bias=c_eps[:], scale=1.0 / n),
               reads=[ss, c_eps], writes=[ss])
            op("dve", lambda e: e.reciprocal(out=rstd["r"][:], in_=ss[:]), reads=[ss], writes=[rstd["r"]])

        load_gain(0)
        hT_tiles = [Buf(None, f"hT_tile{t}") for t in range(NT_EXT)]
        with fw.scope() as esA:
            xt = [fw.sb([128, D], F32, f"xtA{i}", esA) for i in range(6)]
            xn = [fw.sb([128, D], BF16, f"xnA{i}", esA) for i in range(2)]
            junk = fw.sb([128, D], BF16, "junkA", esA)
            hst = [fw.sb([128, 8, 128], BF16, f"hstA{i}", esA) for i in range(4)]
            ssA = [fw.sb([128, 1], F32, f"ssA{i}", esA) for i in range(2)]
            rrA = [fw.sb([128, 1], F32, f"rrA{i}", esA) for i in range(2)]
            for t in range(NT_EXT):
                x_ = xt[t % 6]
                if t == 0:
                    for tt in range(5):
                        fw.dma(xt[tt][:], xe[tt * 128:(tt + 1) * 128, :], writes=[xt[tt]])
                if t + 5 < NT_EXT:
                    fw.dma(xt[(t + 5) % 6][:], xe[(t + 5) * 128:(t + 6) * 128, :], writes=[xt[(t + 5) % 6]])
                rs = {"ss": ssA[t % 2], "r": rrA[t % 2]}
                rms_rstd({"ap": x_[:], "bufs": [x_]}, rs, D, {"ap": junk[:], "buf": junk})
                n_ = xn[t % 2]
                op("dve", lambda e: e.scalar_tensor_tensor(out=n_[:], in0=x_[:], scalar=rs["r"][:], in1=gB[:], op0=ALU.mult, op1=ALU.mult),
                   reads=[x_, rs["r"], gB], writes=[n_])
                pb = t % 2
                for k in range(8):
                    op("pe", lambda e: e.transpose(out=psbf(pb)[:, k * 128:(k + 1) * 128], in_=n_[:, k * 128:(k + 1) * 128], identity=idb[:]),
                       reads=[n_, idb], writes=[PS[pb]])
                h_ = hst[t % 4]
                op("act", lambda e: e.copy(out=h_[:], in_=psbf(pb).rearrange("p (k t) -> p k t", k=8)), reads=[PS[pb]], writes=[h_])
                fw.dma(hT_d[:, :, t * 128:(t + 1) * 128], h_[:], reads=[h_], writes=[hT_tiles[t]], q="pool")


        if "cs" in dbg:
            o = dbg_t("cs", [128, NT_EXT, 8])
            fw.dma(o[:, :, :], cs[:], reads=[cs], is_output=True)
            o = dbg_t("sn", [128, NT_EXT, 8])
            fw.dma(o[:, :, :], sn[:], reads=[sn], is_output=True)

        def ckpt(name):
            if ("stop_" + name) in dbg:
                fw.stopped = True

        def body():
            def mm(bank, out_ap, lhsT, rhs, start, stop, reads):
                op("pe", lambda e: e.matmul(out_ap, lhsT, rhs, start=start, stop=stop), reads=reads, writes=[bank])

            YT = fw.sb([128, 8, S_OWN], BF16, "YT")
            esBc = fw.scope()
            esBc.__enter__()
            cmask = fw.sb([128, 2, S_OWN], BF16, "cmask", esBc)
            op("pool", lambda e: e.memset(cmask[:], 1.0), writes=[cmask])
            op("pool", lambda e: e.affine_select(out=cmask[:, 0, :], in_=cmask[:, 0, :], pattern=[[1, S_OWN]], compare_op=ALU.is_ge, fill=0.0,
                                                 base=2017, channel_multiplier=-16), reads=[cmask], writes=[cmask])
            op("pool", lambda e: e.affine_select(out=cmask[:, 1, :], in_=cmask[:, 1, :], pattern=[[1, S_OWN]], compare_op=ALU.is_ge, fill=0.0,
                                                 base=-31, channel_multiplier=-16), reads=[cmask], writes=[cmask])
            ovl = fw.sb([128, 2, 64], BF16, "ovl", esBc)
            op("pool", lambda e: e.memset(ovl[:], 1.0), writes=[ovl])
            for j in range(2):
                op("pool", lambda e: e.affine_select(out=ovl[:, j, :], in_=ovl[:, j, :], pattern=[[-4, 64]], compare_op=ALU.is_ge, fill=0.0,
                                                     base=128 * j + 1, channel_multiplier=1), reads=[ovl], writes=[ovl])
                op("pool", lambda e: e.affine_select(out=ovl[:, j, :], in_=ovl[:, j, :], pattern=[[4, 64]], compare_op=ALU.is_ge, fill=0.0,
                                                     base=3 - 128 * j, channel_multiplier=-1), reads=[ovl], writes=[ovl])
            maskadd = fw.sb([128, NT_OWN, 64], F32, "maskadd", esBc)
            Mb = fw.sb([128, 64], F32, "Mb", esBc)
            hm1 = fw.sb([128, 2], F32, "hm1", esBc)
            op("dve", lambda e: e.tensor_scalar(out=hm1[:, 0:1], in0=hv[:], scalar1=-1.0, scalar2=1e30, op0=ALU.add, op1=ALU.mult),
               reads=[hv], writes=[hm1])
            op("dve", lambda e: e.tensor_scalar(out=hm1[:, 1:2], in0=hv[:], scalar1=-1.0, scalar2=-1000.0, op0=ALU.add, op1=ALU.mult),
               reads=[hv, hm1], writes=[hm1])
            op("dve", lambda e: e.memset(Mb[:], 0.0), writes=[Mb])
            op("dve", lambda e: e.tensor_copy(out=Mb[:, 0:32], in_=hm1[:, 0:1].to_broadcast([128, 32])), reads=[hm1, Mb], writes=[Mb])
            op("dve", lambda e: e.scalar_tensor_tensor(out=Mb[:, 0:1], in0=hv[:], scalar=1000.0, in1=Mb[:, 0:1], op0=ALU.mult, op1=ALU.add),
               reads=[hv, Mb], writes=[Mb])
            op("dve", lambda e: e.tensor_copy(out=Mb[:, 32:33], in_=hm1[:, 1:2]), reads=[hm1, Mb], writes=[Mb])
            for c in range(NT_OWN):
                op("pool", lambda e: e.tensor_copy(out=maskadd[:, c, :], in_=Mb[:]), reads=[Mb, maskadd], writes=[maskadd])
                for hf in range(2):
                    lo = 32 + 2 * c + hf + 1
                    if lo < 64:
                        op("pool", lambda e: e.memset(maskadd[hf * 64:(hf + 1) * 64, c, lo:64], -1e30), reads=[maskadd], writes=[maskadd])
                    for col in (32 + 2 * c + hf, 32 + 2 * c + hf - 1):
                        op("pool", lambda e: e.tensor_scalar(out=maskadd[hf * 64:(hf + 1) * 64, c, col:col + 1],
                                                             in0=maskadd[hf * 64:(hf + 1) * 64, c, col:col + 1],
                                                             scalar1=1000.0, scalar2=None, op0=ALU.add), reads=[maskadd], writes=[maskadd])

            ckpt("consts")
            for g in range(2):
                with fw.scope() as esG:
                    qT = fw.sb([128, 4, S_OWN], BF16, f"qT{g}", esG)
                    kkT = fw.sb([128, 2, S_EXT], BF16, f"kkT{g}", esG)
                    op("pool", lambda e: e.memset(qT[64:128, :, :], 0.0), writes=[qT])
                    op("pool", lambda e: e.memset(kkT[64:128, 0, :], 1.0), writes=[kkT])
                    op("pool", lambda e: e.memset(kkT[64:128, 1, :], 0.0), writes=[kkT])
                    op("pool", lambda e: e.affine_select(out=kkT[64:128, 0, :], in_=kkT[64:128, 0, :], pattern=[[1, S_EXT]], compare_op=ALU.is_ge, fill=0.0,
                                                         base=0, channel_multiplier=-64), reads=[kkT], writes=[kkT])
                    op("pool", lambda e: e.affine_select(out=kkT[64:128, 0, :], in_=kkT[64:128, 0, :], pattern=[[-1, S_EXT]], compare_op=ALU.is_ge, fill=0.0,
                                                         base=63, channel_multiplier=64), reads=[kkT], writes=[kkT])
                    vv = fw.sb([128, NT_EXT, 2, 65], BF16, f"vv{g}", esG)
                    gsig = fw.sb([128, NT_OWN, 12], F32, f"gsig{g}", esG)
                    kcmpT = fw.sb([128, 256], BF16, f"kcmpT{g}", esG)
                    op("pool", lambda e: e.memset(kcmpT[64:128, :], 0.0), writes=[kcmpT])
                    vca = fw.sb([128, 2, 65], BF16, f"vca{g}", esG)
                    op("pool", lambda e: e.memset(vv[:, :, :, 64:65], 1.0), writes=[vv])
                    op("pool", lambda e: e.memset(vca[:, :, 64:65], 1.0), writes=[vca])
                    ckpt("B0a")
                    with fw.scope() as esC:
                        ccT = fw.sb([64, 2, S_EXT], BF16, f"ccT{g}", esC)
                        with fw.scope() as esB1:
                            w_att = fw.sb([128, 8, 652], BF16, f"w_att{g}", esB1)
                            fw.dma(w_att[:], w_att_d[g].rearrange("(k p) c -> p k c", p=128), writes=[w_att], q="pool")
                            ckpt("B0b")
                            hblk = [fw.sb([128, 8, 512], BF16, f"hblkB{g}{i}", esB1) for i in range(2)]
                            rp = [fw.sb([128, 8, 64], BF16, f"rp{g}{i}", esB1) for i in range(2)]
                            rpf = [fw.sb([128, 8, 64], F32, f"rpf{g}{i}", esB1) for i in range(2)]
                            ta = [fw.sb([128, 7, 8], F32, f"ropa{g}{i}", esB1) for i in range(2)]
                            tb_ = [fw.sb([128, 7, 8], F32, f"ropb{g}{i}", esB1) for i in range(2)]
                            tcx = [fw.sb([128, 7, 8], F32, f"ropc{g}{i}", esB1) for i in range(2)]
                            tdx = [fw.sb([128, 7, 8], F32, f"ropd{g}{i}", esB1) for i in range(2)]
                            def b1_front(t):
                                own = t >= NT_OWN
                                tq = t - NT_OWN
                                hb = hblk[(t // 4) % 2]
                                if t % 4 == 0:
                                    fw.dma(hb[:], hT_d[:, :, t * 128:(t + 4) * 128], reads=hT_tiles[t:t + 4], writes=[hb])
                                tl = t % 4
                                a0 = 0 if own else 256
                                nb = 140 if own else 128
                                bA = 2 + t % 2
                                bB = 4 + t % 2
                                for k in range(8):
                                    mm(PS[bA], PS[bA][:, a0:512], hb[:, k, tl * 128:(tl + 1) * 128], w_att[:, k, a0:512], k == 0, k == 7, [hb, w_att])
                                for k in range(8):
                                    mm(PS[bB], PS[bB][:, 0:nb], hb[:, k, tl * 128:(tl + 1) * 128], w_att[:, k, 512:512 + nb], k == 0, k == 7, [hb, w_att])
                                rp_ = rp[t % 2]
                                h0 = a0 // 64
                                nh = 7 - h0
                                rf = rpf[t % 2]
                                op("act", lambda e: e.copy(out=rf[:, h0:8, :], in_=PS[bA][:, a0:512].rearrange("p (h d) -> p h d", d=64)),
                                   reads=[PS[bA]], writes=[rf])
                                op("pool", lambda e: e.tensor_copy(out=rp_[:, h0:8, :], in_=rf[:, h0:8, :]), reads=[rf], writes=[rp_])
                                t1 = rf[:, h0:7, 0:8]
                                t2 = rf[:, h0:7, 8:16]
                                Cb = cs[:, t, :].unsqueeze(1).to_broadcast([128, nh, 8])
                                Sb_ = sn[:, t, :].unsqueeze(1).to_broadcast([128, nh, 8])
                                ta_, tb2 = ta[t % 2], tb_[t % 2]
                                tc_, td_ = tcx[t % 2], tdx[t % 2]
                                op("dve", lambda e: e.tensor_tensor(out=ta_[:, 0:nh, :], in0=t1, in1=Cb, op=ALU.mult), reads=[rf, cs], writes=[ta_])
                                op("dve", lambda e: e.tensor_tensor(out=tb2[:, 0:nh, :], in0=t2, in1=Sb_, op=ALU.mult), reads=[rf, sn], writes=[tb2])
                                op("dve", lambda e: e.tensor_tensor(out=tc_[:, 0:nh, :], in0=t2, in1=Cb, op=ALU.mult), reads=[rf, cs], writes=[tc_])
                                op("dve", lambda e: e.tensor_tensor(out=td_[:, 0:nh, :], in0=t1, in1=Sb_, op=ALU.mult), reads=[rf, sn], writes=[td_])
                                op("dve", lambda e: e.tensor_tensor(out=rp_[:, h0:7, 0:8], in0=ta_[:, 0:nh, :], in1=tb2[:, 0:nh, :], op=ALU.subtract),
                                   reads=[ta_, tb2, rp_], writes=[rp_])
                                op("dve", lambda e: e.tensor_tensor(out=rp_[:, h0:7, 8:16], in0=tc_[:, 0:nh, :], in1=td_[:, 0:nh, :], op=ALU.add),
                                   reads=[tc_, td_, rp_], writes=[rp_])
                                op("dve", lambda e: e.tensor_copy(out=vv[:, t, :, 0:64], in_=PS[bB][:, 0:128].rearrange("p (h d) -> p h d", d=64)),
                                   reads=[PS[bB]], writes=[vv])
                                if own:
                                    op("act", lambda e: e.activation(out=gsig[:, tq, :], in_=PS[bB][:, 128:140], func=AF.Sigmoid),
                                       reads=[PS[bB]], writes=[gsig])

                            def b1_back(t):
                                own = t >= NT_OWN
                                tq = t - NT_OWN
                                rp_ = rp[t % 2]
                                h0 = 0 if own else 4
                                bT = t % 2
                                psT = psbf(bT)
                                for j, hh in enumerate(range(h0, 8)):
                                    op("pe", lambda e: e.transpose(out=psT[0:64, j * 128:(j + 1) * 128], in_=rp_[:, hh, :], identity=idb[:]),
                                       reads=[rp_, idb], writes=[PS[bT]])
                                if own:
                                    op("act", lambda e: e.copy(out=qT[0:64, :, tq * 128:(tq + 1) * 128], in_=psT[0:64, 0:512].rearrange("p (h t) -> p h t", h=4)),
                                       reads=[PS[bT]], writes=[qT])
                                    o1 = 512
                                else:
                                    o1 = 0
                                op("act", lambda e: e.copy(out=kkT[0:64, :, t * 128:(t + 1) * 128], in_=psT[0:64, o1:o1 + 256].rearrange("p (h t) -> p h t", h=2)),
                                   reads=[PS[bT]], writes=[kkT])
                                op("act", lambda e: e.copy(out=ccT[:, :, t * 128:(t + 1) * 128], in_=psT[0:64, o1 + 256:o1 + 512].rearrange("p (h t) -> p h t", h=2)),
                                   reads=[PS[bT]], writes=[ccT])

                            for t in range(NT_EXT + 1):
                                if t < NT_EXT:
                                    b1_front(t)
                                if t >= 1:
                                    b1_back(t - 1)
                        ckpt("B1")
                        for i in range(2):
                            with fw.scope() as esB2:
                                w1 = fw.sb([64, 32, 256], BF16, f"w1_{g}{i}", esB2)
                                fw.dma(w1[:], w_c1_d[i].rearrange("(l d) h -> d l h", d=64), writes=[w1], q="pool")
                                w2 = fw.sb([128, 2, 64], BF16, f"w2_{g}{i}", esB2)
                                fw.dma(w2[:], w_c2_d[i].rearrange("(c p) d -> p c d", p=128), writes=[w2], q="pool")
                                pe_sb = fw.sb([32, 64], BF16, f"pe_{g}{i}", esB2)
                                fw.dma(pe_sb[:], pe_c_d[i], writes=[pe_sb], q="pool")
                                peT = fw.sb([64, 32], BF16, f"peT_{g}{i}", esB2)
                                op("pe", lambda e: e.transpose(out=psbf(6)[0:64, 0:32], in_=pe_sb[:, :], identity=idb[0:32, 0:32]),
                                   reads=[pe_sb, idb], writes=[PS[6]])
                                op("act", lambda e: e.copy(out=peT[:], in_=psbf(6)[0:64, 0:32]), reads=[PS[6]], writes=[peT])
                                for hc in range(2):
                                    for l in range(32):
                                        mm(PS[7], PS[7][:, hc:hc + 1], w1[:, l, hc * 128:(hc + 1) * 128], peT[:, l:l + 1], l == 0, l == 31, [w1, peT])
                                cbs = fw.sb([128, 2], F32, f"cbs_{g}{i}", esB2)
                                op("act", lambda e: e.copy(out=cbs[:], in_=PS[7][:, 0:2]), reads=[PS[7]], writes=[cbs])
                                G = fw.sb([128, 2, 256], BF16, f"G_{g}{i}", esB2)
                                op("pool", lambda e: e.memset(G[:, :, 255:256], 0.0), writes=[G])
                                u_ = fw.sb([128, 255], F32, f"u_{g}{i}", esB2)
                                u2 = fw.sb([128, 255], F32, f"u2_{g}{i}", esB2)
                                sg_ = fw.sb([128, 255], F32, f"sg_{g}{i}", esB2)
                                for hc in range(2):
                                    for l in range(32):
                                        mm(PS[hc], PS[hc][:, 0:255], w1[:, l, hc * 128:(hc + 1) * 128], ccT[:, i, l:l + 16 * 254 + 1:16], l == 0, l == 31, [w1, ccT])
                                    op("act", lambda e: e.activation(out=u_[:], in_=PS[hc][:, 0:255], func=AF.Identity, bias=cbs[:, hc:hc + 1]),
                                       reads=[PS[hc], cbs], writes=[u_])
                                    op("dve", lambda e: e.tensor_tensor(out=u2[:], in0=u_[:], in1=u_[:], op=ALU.mult), reads=[u_], writes=[u2])
                                    op("dve", lambda e: e.tensor_scalar(out=u2[:], in0=u2[:], scalar1=0.044715, scalar2=1.0, op0=ALU.mult, op1=ALU.add),
                                       reads=[u2], writes=[u2])
                                    op("dve", lambda e: e.tensor_tensor(out=u2[:], in0=u2[:], in1=u_[:], op=ALU.mult), reads=[u2, u_], writes=[u2])
                                    op("act", lambda e: e.activation(out=sg_[:], in_=u2[:], func=AF.Sigmoid, scale=1.5957691216057308),
                                       reads=[u2], writes=[sg_])
                                    op("dve", lambda e: e.tensor_tensor(out=G[:, hc, 0:255], in0=u_[:], in1=sg_[:], op=ALU.mult), reads=[u_, sg_], writes=[G])
                                if i == 0:
                                    for hc in range(2):
                                        mm(PS[6], PS[6][0:64, 0:256], w2[:, hc, :], G[:, hc, :], hc == 0, hc == 1, [w2, G])
                                    op("act", lambda e: e.copy(out=kcmpT[0:64, :], in_=PS[6][0:64, 0:256]), reads=[PS[6]], writes=[kcmpT])
                                else:
                                    for nch in range(2):
                                        for hc in range(2):
                                            mm(PS[6], PS[6][:, nch * 64:(nch + 1) * 64], G[:, hc, nch * 128:(nch + 1) * 128], w2[:, hc, :], hc == 0, hc == 1, [w2, G])
                                    op("act", lambda e: e.copy(out=vca[:, :, 0:64], in_=PS[6][:, 0:128].rearrange("p (n d) -> p n d", d=64)),
                                       reads=[PS[6]], writes=[vca])
                    if g == 0 and "B2dump" in dbg:
                        for nm, bf, shp in (("kkT", kkT, [64, 2, S_EXT]), ("qT", qT, [64, 4, S_OWN]), ("vv", vv, [128, NT_EXT, 2, 65]),
                                            ("kcmpT", kcmpT, [64, 256]), ("vca", vca, [128, 2, 65])):
                            o = dbg_t(nm, shp, BF16)
                            fw.dma(o, bf[0:shp[0]], reads=[bf], is_output=True)
                        o = dbg_t("gsig", [128, NT_OWN, 12])
                        fw.dma(o, gsig[:], reads=[gsig], is_output=True)
                    ckpt("B2")
                    with fw.scope() as esB3:
                        NP = 4
                        LA = 2
                        Pb = [fw.sb([128, 512], BF16, f"Pb{g}{i}", esB3) for i in range(NP)]
                        Sbank = [0, 1, 6, 7]
                        tpsum = psbf(2)[:, 520:1024]
                        TPB = PS[2]
                        hbS = [fw.sb([128, 4, 132], F32, f"hbS{g}{r}", esB3) for r in range(3)]
                        ya = [fw.sb([128, 4, 64], F32, f"ya{g}{i}", esB3) for i in range(2)]
                        yat = [fw.sb([128, 4, 64], BF16, f"yat{g}{i}", esB3) for i in range(2)]
                        sms = [fw.sb([128, 16], F32, f"sm{g}{i}", esB3) for i in range(3)]
                        rdc = fw.sb([128, 4], F32, f"rdc{g}", esB3)
                        impv = fw.sb([128, 64], F32, f"impv{g}", esB3)
                        wk = fw.sb([128, 64], F32, f"wk{g}", esB3)
                        m8a = fw.sb([128, 8], F32, f"m8a{g}", esB3)
                        m8b = fw.sb([128, 8], F32, f"m8b{g}", esB3)
                        negm2 = fw.sb([128, 128], BF16, f"negm{g}", esB3)
                        op("pool", lambda e: e.memset(negm2[:, 0:64], 0.0), writes=[negm2])
                        rot = [0]
                        REG = {0: (0, 129), 1: (129, 65), 2: (194, 65)}

                        def score(c, lhsT, lreads, extra, bias, mask):
                            r = rot[0] % NP
                            rot[0] += 1
                            sb_i = Sbank[r]
                            P = Pb[r]
                            qrhs = qT[:, :, c * 128:(c + 1) * 128]
                            S3 = PS[sb_i][:, :].rearrange("p (h q) -> p h q", h=4)
                            mm(PS[sb_i], S3, lhsT, qrhs, True, True, lreads + [qT])
                            op("act", lambda e: e.activation(out=P[:], in_=PS[sb_i][:, :], func=AF.Exp, bias=bias[:], scale=0.125),
                               reads=[PS[sb_i], bias], writes=[P])
                            if mask is not None:
                                op("dve", lambda e: e.tensor_tensor(out=P[:].rearrange("p (h q) -> p h q", h=4), in0=P[:].rearrange("p (h q) -> p h q", h=4),
                                                                    in1=mask[0], op=ALU.mult), reads=[P, mask[1]], writes=[P])
                            return P

                        def pv(P, h, reg, vr, vreads, cc, n, first, last):
                            op("pe", lambda e: e.matmul(PS[2 + h][:, cc:cc + n], P[:, h * 128:(h + 1) * 128], vr, start=first, stop=last),
                               reads=[P] + vreads, writes=[PS[2 + h]])

                        def evac_all(c, reg, br, first, final):
                            col0, n = REG[reg]
                            hs = hbS[reg]
                            for h in range(4):
                                op("dve", lambda e: e.tensor_copy(out=hs[:, h, 0:n], in_=PS[2 + h][:, col0:col0 + n]), reads=[PS[2 + h]], writes=[hs])
                            sm = sms[reg]
                            yac = ya[c % 2]
                            dn = sm[:, 0:4]
                            rd = sm[:, 4:8] if br != 0 else rdc[:, 0:4]
                            rdb = sm if br != 0 else rdc
                            cf = sm[:, 8:12]
                            op("dve", lambda e: e.tensor_scalar(out=dn.unsqueeze(2), in0=hs[:, :, 64:65], scalar1=1e-30, scalar2=None, op0=ALU.max),
                               reads=[hs], writes=[sm])
                            op("dve", lambda e: e.reciprocal(out=rd, in_=dn), reads=[sm], writes=[rdb])
                            op("dve", lambda e: e.tensor_tensor(out=cf.unsqueeze(2), in0=rd.unsqueeze(2),
                                                                in1=gsig[:, c, :].rearrange("p (h b) -> p h b", b=3)[:, :, br:br + 1], op=ALU.mult),
                               reads=[sm, rdb, gsig], writes=[sm])
                            cfb = cf.unsqueeze(2).to_broadcast([128, 4, 64])
                            if first:
                                op("dve", lambda e: e.tensor_tensor(out=yac[:], in0=hs[:, :, 0:64], in1=cfb, op=ALU.mult), reads=[hs, sm], writes=[yac])
                            else:
                                op("dve", lambda e: e.tensor_tensor(out=hs[:, :, 0:64], in0=hs[:, :, 0:64], in1=cfb, op=ALU.mult), reads=[hs, sm], writes=[hs])
                                dst = yat[c % 2] if final else yac
                                op("dve", lambda e: e.tensor_tensor(out=dst[:], in0=hs[:, :, 0:64], in1=yac[:], op=ALU.add), reads=[hs, yac], writes=[dst])

                        pend = []

                        def flush():
                            while pend:
                                pend.pop(0)()

                        def pipe(score_fn, pv_fn):
                            P = score_fn()
                            while len(pend) >= LA:
                                pend.pop(0)()
                            pend.append(lambda: pv_fn(P))

                        for c in range(NT_OWN):
                            Pc = []
                            for nch in range(2):
                                mk = cmask[:, nch, c * 128:(c + 1) * 128].unsqueeze(1).to_broadcast([128, 4, 128])
                                Pc.append(score(c, kcmpT[:, nch * 128:(nch + 1) * 128], [kcmpT], None, hbias if nch == 0 else c_zero, (mk, cmask)))
                            flush()
                            if c > 0:
                                cp = c - 1
                                evac_all(cp, 1, 1, False, True)
                                for j in range(2):
                                    op("pe", lambda e: e.transpose(out=tpsum[:, j * 128:(j + 1) * 128],
                                                                   in_=yat[cp % 2][:, 2 * j:2 * j + 2, :].rearrange("p h d -> p (h d)"), identity=idb[:]),
                                       reads=[yat[cp % 2], idb], writes=[TPB])
                                op("act", lambda e: e.copy(out=YT[:, 2 * g:2 * g + 2, cp * 128:(cp + 1) * 128],
                                                           in_=tpsum[:, 0:256].rearrange("p (j t) -> p j t", j=2)), reads=[TPB], writes=[YT])
                            if g == 0 and c == 1 and "B3dump" in dbg:
                                for r_ in range(3):
                                    fw.dma(dbg_t(f"hbS{r_}", [128, 4, 132]), hbS[r_][:], reads=[hbS[r_]], is_output=True)
                                fw.dma(dbg_t("yat0", [128, 4, 64], BF16), yat[0][:], reads=[yat[0]], is_output=True)
                                fw.dma(dbg_t("ya0", [128, 4, 64]), ya[0][:], reads=[ya[0]], is_output=True)
                                fw.dma(dbg_t("sm0", [128, 16]), sms[0][:], reads=[sms[0]], is_output=True)
                                fw.dma(dbg_t("sm1", [128, 16]), sms[1][:], reads=[sms[1]], is_output=True)
                                fw.dma(dbg_t("sm2", [128, 16]), sms[2][:], reads=[sms[2]], is_output=True)
                                fw.dma(dbg_t("rdc", [128, 4]), rdc[:], reads=[rdc], is_output=True)
                                ckpt("B3c0")
                            for h in range(4):
                                for nch in range(2):
                                    pv(Pc[nch], h, 0, vca[:, nch, :], [vca], 0, 65, nch == 0, nch == 1)
                                for nch in range(2):
                                    pv(Pc[nch], h, 0, ovl[:, nch, :], [ovl], 65, 64, nch == 0, nch == 1)
                            for j in range(5):
                                ch = NT_OWN + c - 4 + j
                                mk = None
                                if j == 0:
                                    mk = (wm0[:].unsqueeze(1).to_broadcast([128, 4, 128]), wm0)
                                elif j == 4:
                                    mk = (caus[:].unsqueeze(1).to_broadcast([128, 4, 128]), caus)

                                def sfn(ch=ch, mk=mk):
                                    return score(c, kkT[:, 1, ch * 128:(ch + 1) * 128], [kkT], None, hbias if ch < NT_OWN else c_zero, mk)

                                def pfn(P, ch=ch, j=j):
                                    for h in range(4):
                                        pv(P, h, 2, vv[:, ch, 1, :], [vv], 194, 65, j == 0, j == 4)
                                pipe(sfn, pfn)
                                if j == 0:
                                    evac_all(c, 0, 0, True, False)
                                    op("dve", lambda e: e.tensor_tensor(out=hbS[0][:, :, 65:129], in0=hbS[0][:, :, 65:129], in1=rdc[:, 0:4].unsqueeze(2).to_broadcast([128, 4, 64]), op=ALU.mult),
                                       reads=[hbS[0], rdc], writes=[hbS[0]])
                                    op("dve", lambda e: e.tensor_reduce(out=impv[:], in_=hbS[0][:, :, 65:129].rearrange("p h s -> p s h"), axis=AX.X, op=ALU.add),
                                       reads=[hbS[0]], writes=[impv])
                                    op("dve", lambda e: e.tensor_tensor(out=impv[:], in0=impv[:], in1=maskadd[:, c, :], op=ALU.add), reads=[impv, maskadd], writes=[impv])
                                    op("dve", lambda e: e.max(out=m8a[:], in_=impv[:]), reads=[impv], writes=[m8a])
                                    op("dve", lambda e: e.match_replace(out=wk[:], in_to_replace=m8a[:], in_values=impv[:], imm_value=-3.0e38),
                                       reads=[impv, m8a], writes=[wk])
                                    op("dve", lambda e: e.max(out=m8b[:], in_=wk[:]), reads=[wk], writes=[m8b])
                                    op("dve", lambda e: e.tensor_scalar(out=negm2[:, 64:128], in0=impv[:], scalar1=m8b[:, 7:8], scalar2=NEGB, op0=ALU.is_lt, op1=ALU.mult),
                                       reads=[impv, m8b, negm2], writes=[negm2])
                            flush()
                            evac_all(c, 2, 2, False, False)
                            op("pe", lambda e: e.transpose(out=tpsum[:, 256:384], in_=negm2[:, :], identity=idb[:]), reads=[negm2, idb], writes=[TPB])
                            op("act", lambda e: e.copy(out=qT[64:128, :, c * 128:(c + 1) * 128], in_=tpsum[64:128, 256:384].unsqueeze(1).to_broadcast([64, 4, 128])),
                               reads=[TPB], writes=[qT])
                            chs = list(range(NT_OWN)) + [NT_OWN + j for j in range(c + 1)]
                            for i, ch in enumerate(chs):
                                mk = None
                                if ch == NT_OWN + c:
                                    mk = (caus[:].unsqueeze(1).to_broadcast([128, 4, 128]), caus)

                                def sfn(ch=ch, mk=mk):
                                    return score(c, kkT[:, 0, ch * 128:(ch + 1) * 128], [kkT], None, hbias if ch < NT_OWN else c_zero, mk)

                                def pfn(P, ch=ch, i=i, n=len(chs)):
                                    for h in range(4):
                                        pv(P, h, 1, vv[:, ch, 0, :], [vv], 129, 65, i == 0, i == n - 1)
                                pipe(sfn, pfn)
                        flush()
                        cp = NT_OWN - 1
                        evac_all(cp, 1, 1, False, True)
                        for j in range(2):
                            op("pe", lambda e: e.transpose(out=tpsum[:, j * 128:(j + 1) * 128],
                                                           in_=yat[cp % 2][:, 2 * j:2 * j + 2, :].rearrange("p h d -> p (h d)"), identity=idb[:]),
                               reads=[yat[cp % 2], idb], writes=[TPB])
                        op("act", lambda e: e.copy(out=YT[:, 2 * g:2 * g + 2, cp * 128:(cp + 1) * 128],
                                                   in_=tpsum[:, 0:256].rearrange("p (j t) -> p j t", j=2)), reads=[TPB], writes=[YT])
            esBc.__exit__(None, None, None)
            ckpt("B")
            with fw.scope() as esCg:
                ee = fw.sb([128, NT_EXT, 4], F32, "ee", esCg)
                ff = fw.sb([128, NT_EXT, 4], F32, "ff", esCg)
                fl = fw.sb([128, NT_EXT, 4], F32, "fl", esCg)
                ghn = fw.sb([128, 512], F32, "ghn", esCg)
                fw.dma(ghn[:], g_hn_d[0:1, :].to_broadcast([128, 512]), writes=[ghn])
                wcs = fw.sb([128, 8, 4], F32, "wcs", esCg)
                fw.dma(wcs[:], wc_d[:, :, :], writes=[wcs])
                bcs = fw.sb([128, 8], F32, "bcs", esCg)
                fw.dma(bcs[:], bc_d[:, :], writes=[bcs])
                with fw.scope() as esg:
                    w_if = fw.sb([128, 8, 8], BF16, "w_if", esg)
                    fw.dma(w_if[:], w_if_d.rearrange("(k p) c -> p k c", p=128), writes=[w_if], q="pool")
                    bif = fw.sb([128, 8], F32, "bif", esg)
                    fw.dma(bif[:], b_if_d[0:1, :].to_broadcast([128, 8]), writes=[bif])
                    hblk = [fw.sb([128, 8, 512], BF16, f"hblkG{i}", esg) for i in range(2)]
                    ifp = fw.sb([128, NT_EXT, 8], F32, "ifp", esg)
                    l1 = fw.sb([128, NT_EXT, 4], F32, "l1", esg)
                    tmpg = fw.sb([128, NT_EXT, 4], F32, "tmpg", esg)
                    for t in range(NT_EXT):
                        hb = hblk[(t // 4) % 2]
                        if t % 4 == 0:
                            fw.dma(hb[:], hT_d[:, :, t * 128:(t + 4) * 128], reads=hT_tiles[t:t + 4], writes=[hb])
                        tl = t % 4
                        for k in range(8):
                            mm(PS[0], PS[0][:, t * 8:(t + 1) * 8], hb[:, k, tl * 128:(tl + 1) * 128], w_if[:, k, :], k == 0, k == 7, [hb, w_if])
                    op("act", lambda e: e.copy(out=ifp[:], in_=PS[0][:, 0:256].rearrange("p (t c) -> p t c", c=8)), reads=[PS[0]], writes=[ifp])
                    op("dve", lambda e: e.tensor_tensor(out=ifp[:], in0=ifp[:], in1=bif[:].unsqueeze(1).to_broadcast([128, NT_EXT, 8]), op=ALU.add),
                       reads=[ifp, bif], writes=[ifp])
                    op("act", lambda e: e.activation(out=l1[:], in_=ifp[:, :, 4:8], func=AF.Exp, scale=-1.0), reads=[ifp], writes=[l1])
                    op("act", lambda e: e.activation(out=l1[:], in_=l1[:], func=AF.Ln, bias=c_one[:]), reads=[l1, c_one], writes=[l1])
                    l1f = l1[:].rearrange("p t c -> p (t c)")
                    mm(PS[1], PS[1][:, 0:128], U_f[:], l1f, True, True, [U_f, l1])
                    mm(PS[1], PS[1][:, 128:256], ones_f[:], l1f, True, True, [ones_f, l1])
                    op("act", lambda e: e.copy(out=tmpg[:], in_=PS[1][:, 0:128].rearrange("p (t c) -> p t c", c=4)), reads=[PS[1]], writes=[tmpg])
                    op("act", lambda e: e.activation(out=ff[:], in_=tmpg[:], func=AF.Exp, scale=-1.0), reads=[tmpg], writes=[ff])
                    op("act", lambda e: e.activation(out=fl[:], in_=PS[1][:, 128:256].rearrange("p (t c) -> p t c", c=4), func=AF.Exp, scale=-1.0),
                       reads=[PS[1]], writes=[fl])
                    op("dve", lambda e: e.tensor_tensor(out=tmpg[:], in0=tmpg[:], in1=ifp[:, :, 0:4], op=ALU.add), reads=[tmpg, ifp], writes=[tmpg])
                    op("act", lambda e: e.activation(out=ee[:], in_=tmpg[:], func=AF.Exp), reads=[tmpg], writes=[ee])
                    op("dve", lambda e: e.tensor_scalar(out=ee[:, 0:NT_OWN, :], in0=ee[:, 0:NT_OWN, :], scalar1=hv[:, 0:1], scalar2=None, op0=ALU.mult),
                       reads=[ee, hv], writes=[ee])
                ckpt("Cg")
                qTb = fw.sb([128, 4, S_OWN], BF16, "qTb", esCg)
                kTb = fw.sb([128, 4, S_EXT], BF16, "kTb", esCg)
                vaug = fw.sb([128, NT_EXT, 4, 129], BF16, "vaug", esCg)
                osig = fw.sb([128, NT_OWN, 512], BF16, "osig", esCg)
                op("pool", lambda e: e.memset(vaug[:, :, :, 128:129], 1.0), writes=[vaug])
                for hp in range(2):
                    with fw.scope() as esC1:
                        wq = fw.sb([128, 8, 256], BF16, f"wq{hp}", esC1)
                        wk = fw.sb([128, 8, 256], BF16, f"wk{hp}", esC1)
                        wv = fw.sb([128, 8, 256], BF16, f"wv{hp}", esC1)
                        wo = fw.sb([128, 8, 256], BF16, f"wo{hp}", esC1)
                        fw.dma(wq[:], w_qk_d[:, hp * 256:(hp + 1) * 256].rearrange("(k p) c -> p k c", p=128), writes=[wq], q="pool")
                        fw.dma(wk[:], w_qk_d[:, 512 + hp * 256:512 + (hp + 1) * 256].rearrange("(k p) c -> p k c", p=128), writes=[wk], q="pool")
                        fw.dma(wv[:], w_vo_d[:, hp * 256:(hp + 1) * 256].rearrange("(k p) c -> p k c", p=128), writes=[wv], q="pool")
                        fw.dma(wo[:], w_vo_d[:, 512 + hp * 256:512 + (hp + 1) * 256].rearrange("(k p) c -> p k c", p=128), writes=[wo], q="pool")
                        hblk = [fw.sb([128, 8, 512], BF16, f"hblkC{hp}{i}", esC1) for i in range(2)]
                        uk = [fw.sb([128, 4 + S_EXT], BF16, f"uk{hp}{i}", esC1) for i in range(2)]
                        uq = [fw.sb([128, 4 + 2560], BF16, f"uq{hp}{i}", esC1) for i in range(2)]
                        ycv = [fw.sb([128, 512], F32, f"ycv{hp}{i}", esC1) for i in range(2)]
                        sgm = [fw.sb([128, 512], F32, f"sgm{hp}{i}", esC1) for i in range(2)]
                        for hh in range(2):
                            op("pool", lambda e: e.memset(uk[hh][:, 0:4], 0.0), writes=[uk[hh]])
                            op("pool", lambda e: e.memset(uq[hh][:, 0:4], 0.0), writes=[uq[hh]])
                        for blk in range(8):
                            hb = hblk[blk % 2]
                            fw.dma(hb[:], hT_d[:, :, blk * 512:(blk + 1) * 512], reads=hT_tiles[4 * blk:4 * blk + 4], writes=[hb])
                            for hh in range(2):
                                for k in range(8):
                                    mm(PS[hh], PS[hh][:, :], wk[:, k, hh * 128:(hh + 1) * 128], hb[:, k, :], k == 0, k == 7, [wk, hb])
                                op("act", lambda e: e.copy(out=uk[hh][:, 4 + blk * 512:4 + (blk + 1) * 512], in_=PS[hh][:, :]), reads=[PS[hh]], writes=[uk[hh]])
                            if blk >= 3:
                                for hh in range(2):
                                    for k in range(8):
                                        mm(PS[2 + hh], PS[2 + hh][:, :], wq[:, k, hh * 128:(hh + 1) * 128], hb[:, k, :], k == 0, k == 7, [wq, hb])
                                    op("act", lambda e: e.copy(out=uq[hh][:, 4 + (blk - 3) * 512:4 + (blk - 2) * 512], in_=PS[2 + hh][:, :]),
                                       reads=[PS[2 + hh]], writes=[uq[hh]])
                            for tl in range(4):
                                t = blk * 4 + tl
                                bv = 4 + tl % 2
                                for k in range(8):
                                    mm(PS[bv], PS[bv][:, 0:256], hb[:, k, tl * 128:(tl + 1) * 128], wv[:, k, :], k == 0, k == 7, [wv, hb])
                                op("dve", lambda e: e.tensor_copy(out=vaug[:, t, 2 * hp:2 * hp + 2, 0:128], in_=PS[bv][:, 0:256].rearrange("p (h d) -> p h d", d=128)),
                                   reads=[PS[bv]], writes=[vaug])
                                if blk >= 4:
                                    bo = 6 + tl % 2
                                    for k in range(8):
                                        mm(PS[bo], PS[bo][:, 0:256], hb[:, k, tl * 128:(tl + 1) * 128], wo[:, k, :], k == 0, k == 7, [wo, hb])
                                    op("act", lambda e: e.activation(out=osig[:, t - NT_OWN, hp * 256:(hp + 1) * 256], in_=PS[bo][:, 0:256], func=AF.Sigmoid),
                                       reads=[PS[bo]], writes=[osig])
                        pi = 0
                        for hh in range(2):
                            H = 2 * hp + hh
                            for typ in range(2):
                                ci = typ * 4 + H
                                npiece = 4 if typ == 0 else 8
                                u = uq[hh] if typ == 0 else uk[hh]
                                for pc in range(npiece):
                                    off = (4 + 512 + pc * 512) if typ == 0 else (4 + pc * 512)
                                    y_ = ycv[pi % 2]
                                    s_ = sgm[pi % 2]
                                    pi += 1
                                    op("dve", lambda e: e.tensor_scalar(out=y_[:], in0=u[:, off - 3:off - 3 + 512], scalar1=wcs[:, ci, 0:1], scalar2=bcs[:, ci:ci + 1],
                                                                        op0=ALU.mult, op1=ALU.add), reads=[u, wcs, bcs], writes=[y_])
                                    for j in range(1, 4):
                                        op("dve", lambda e: e.scalar_tensor_tensor(out=y_[:], in0=u[:, off - 3 + j:off - 3 + j + 512], scalar=wcs[:, ci, j:j + 1], in1=y_[:],
                                                                                   op0=ALU.mult, op1=ALU.add), reads=[u, wcs, y_], writes=[y_])
                                    if typ == 0:
                                        op("act", lambda e: e.activation(out=qTb[:, 2 * hp + hh, pc * 512:(pc + 1) * 512], in_=y_[:], func=AF.Silu), reads=[y_], writes=[qTb])
                                    else:
                                        op("act", lambda e: e.activation(out=s_[:], in_=y_[:], func=AF.Sigmoid), reads=[y_], writes=[s_])
                                        op("dve", lambda e: e.scalar_tensor_tensor(out=kTb[:, 2 * hp + hh, pc * 512:(pc + 1) * 512], in0=y_[:], scalar=128.0 ** -0.5, in1=s_[:],
                                                                                   op0=ALU.mult, op1=ALU.mult), reads=[y_, s_], writes=[kTb])
                ckpt("C1")
                with fw.scope() as esC3:
                    ktokR = [fw.sb([128, 4, 128], BF16, f"ktokR{i}", esC3) for i in range(3)]
                    CTall = fw.sb([128, NT_OWN, 4, 129], BF16, "CTall", esC3)
                    Xs = [fw.sb([128, 129], F32, f"Xs{H}", esC3) for H in range(4)]
                    Sm = [[fw.sb([128, 128], BF16, f"Sm{H}{i}", esC3) for i in range(2)] for H in range(4)]
                    hm_ = [fw.sb([128, 128], F32, f"hm{H}", esC3) for H in range(4)]
                    yb_ = [fw.sb([128, 128], BF16, f"yb{H}", esC3) for H in range(4)]
                    jk = [fw.sb([128, 128], BF16, f"jk{H}", esC3) for H in range(4)]
                    smc = [fw.sb([128, 8], F32, f"smc{H}", esC3) for H in range(4)]
                    for H in range(4):
                        op("dve", lambda e: e.tensor_tensor(out=vaug[:, :, H, :], in0=vaug[:, :, H, :],
                                                            in1=ee[:, :, H:H + 1].to_broadcast([128, NT_EXT, 129]), op=ALU.mult), reads=[vaug, ee], writes=[vaug])

                    def k_tr(t):
                        bk = t % 2
                        for H in range(4):
                            op("pe", lambda e: e.transpose(out=psbf(bk)[:, H * 128:(H + 1) * 128], in_=kTb[:, H, t * 128:(t + 1) * 128], identity=idb[:]),
                               reads=[kTb, idb], writes=[PS[bk]])
                        op("act", lambda e: e.copy(out=ktokR[t % 3][:], in_=psbf(bk)[:, 0:512].rearrange("p (h d) -> p h d", d=128)), reads=[PS[bk]], writes=[ktokR[t % 3]])

                    k_tr(0)
                    for t in range(NT_EXT - 1):
                        if t + 1 < NT_EXT - 1:
                            k_tr(t + 1)
                        for H in range(4):
                            bU = 2 + H
                            mm(PS[bU], PS[bU][:, 0:129], ktokR[t % 3][:, H, :], vaug[:, t, H, :], True, True, [ktokR[t % 3], vaug])
                            if t == 0:
                                op("dve", lambda e: e.tensor_copy(out=Xs[H][:], in_=PS[bU][:, 0:129]), reads=[PS[bU]], writes=[Xs[H]])
                            else:
                                op("dve", lambda e: e.scalar_tensor_tensor(out=Xs[H][:], in0=Xs[H][:], scalar=fl[:, t - 1, H:H + 1], in1=PS[bU][:, 0:129],
                                                                           op0=ALU.mult, op1=ALU.add), reads=[Xs[H], fl, PS[bU]], writes=[Xs[H]])
                            if t + 1 >= NT_OWN:
                                op("act", lambda e: e.activation(out=CTall[:, t + 1 - NT_OWN, H, :], in_=Xs[H][:], func=AF.Copy, scale=fl[:, t, H:H + 1]),
                                   reads=[Xs[H], fl], writes=[CTall])
                    sc4 = fw.sb([128, 4, 8], F32, "sc4", esC3)

                    def stA(t):
                        tq = t - NT_OWN
                        for H in range(4):
                            sm_ = Sm[H][tq % 2]
                            mm(PS[H], PS[H][:, 0:128], kTb[:, H, t * 128:(t + 1) * 128], qTb[:, H, tq * 128:(tq + 1) * 128], True, True, [kTb, qTb])
                            op("dve", lambda e: e.tensor_tensor(out=sm_[:], in0=PS[H][:, 0:128], in1=caus[:], op=ALU.mult), reads=[PS[H], caus], writes=[sm_])

                    def stRest(t):
                        tq = t - NT_OWN
                        for H in range(4):
                            sm_ = Sm[H][tq % 2]
                            bA = 4 + H
                            mm(PS[bA], PS[bA][:, 0:129], sm_[:], vaug[:, t, H, :], True, False, [sm_, vaug])
                            mm(PS[bA], PS[bA][:, 0:129], qTb[:, H, tq * 128:(tq + 1) * 128], CTall[:, tq, H, :], False, True, [qTb, CTall])
                        for H in range(4):
                            op("act", lambda e: e.activation(out=sc4[:, H, 6:7], in_=PS[4 + H][:, 128:129], func=AF.Abs, scale=ff[:, t, H:H + 1]),
                               reads=[PS[4 + H], ff], writes=[sc4])
                        op("dve", lambda e: e.tensor_scalar(out=sc4[:, :, 0:1], in0=sc4[:, :, 6:7], scalar1=1.0, scalar2=None, op0=ALU.max), reads=[sc4], writes=[sc4])
                        op("dve", lambda e: e.reciprocal(out=sc4[:, :, 1:2], in_=sc4[:, :, 0:1]), reads=[sc4], writes=[sc4])
                        op("dve", lambda e: e.tensor_tensor(out=sc4[:, :, 2:3], in0=sc4[:, :, 1:2], in1=ff[:, t, :].unsqueeze(2), op=ALU.mult), reads=[sc4, ff], writes=[sc4])
                        for H in range(4):
                            op("dve", lambda e: e.scalar_tensor_tensor(out=hm_[H][:], in0=PS[4 + H][:, 0:128], scalar=sc4[:, H, 2:3], in1=osig[:, tq, H * 128:(H + 1) * 128],
                                                                       op0=ALU.mult, op1=ALU.mult), reads=[PS[4 + H], sc4, osig], writes=[hm_[H]])
                        for H in range(4):
                            op("act", lambda e: e.activation(out=jk[H][:], in_=hm_[H][:], func=AF.Square, accum_out=sc4[:, H, 3:4]), reads=[hm_[H]], writes=[jk[H], sc4])
                        op("act", lambda e: e.activation(out=sc4[:, :, 4:5], in_=sc4[:, :, 3:4], func=AF.Sqrt, bias=c_eps[:], scale=1.0 / 128), reads=[sc4, c_eps], writes=[sc4])
                        op("dve", lambda e: e.reciprocal(out=sc4[:, :, 5:6], in_=sc4[:, :, 4:5]), reads=[sc4], writes=[sc4])
                        for H in range(4):
                            op("dve", lambda e: e.scalar_tensor_tensor(out=yb_[H][:], in0=hm_[H][:], scalar=sc4[:, H, 5:6], in1=ghn[:, H * 128:(H + 1) * 128],
                                                                       op0=ALU.mult, op1=ALU.mult), reads=[hm_[H], sc4, ghn], writes=[yb_[H]])
                        for H in range(4):
                            op("pe", lambda e: e.transpose(out=psbf(4 + H)[:, 512:640], in_=yb_[H][:], identity=idb[:]), reads=[yb_[H], idb], writes=[PS[4 + H]])
                        for H in range(4):
                            op("act", lambda e: e.copy(out=YT[:, 4 + H, tq * 128:(tq + 1) * 128], in_=psbf(4 + H)[:, 512:640]), reads=[PS[4 + H]], writes=[YT])

                    stA(NT_OWN)
                    for t in range(NT_OWN, NT_EXT):
                        if t + 1 < NT_EXT:
                            stA(t + 1)
                        stRest(t)
            ckpt("C")
            if "ybT" in dbg:
                o = dbg_t("ybT", [128, 4, S_OWN], BF16)
                fw.dma(o[:, :, :], YT[:, 4:8, :], reads=[YT], is_output=True)


            with fw.scope() as esD:
                x1 = fw.sb([128, NT_OWN, D], F32, "x1", esD)
                with fw.scope() as esD1:
                    mixT = fw.sb([128, 8, S_OWN], BF16, "mixT", esD1)
                    with fw.scope() as esD1a:
                        hTo = fw.sb([128, 8, S_OWN], BF16, "hTo", esD1a)
                        for tb in range(4):
                            fw.dma(hTo[:, :, tb * 512:(tb + 1) * 512], hT_d[:, :, S_OWN + tb * 512:S_OWN + (tb + 1) * 512],
                                   reads=hT_tiles[NT_OWN + 4 * tb:NT_OWN + 4 * tb + 4], writes=[hTo])
                        wga = [fw.sb([128, 8, 128], BF16, f"wga{i}", esD1a) for i in range(2)]
                        wgb = [fw.sb([128, 8, 128], BF16, f"wgb{i}", esD1a) for i in range(2)]
                        wpa = [fw.sb([128, 4, 128], BF16, f"wpa{i}", esD1a) for i in range(2)]
                        wpb = [fw.sb([128, 4, 128], BF16, f"wpb{i}", esD1a) for i in range(2)]
                        sga = [fw.sb([128, 512], BF16, f"sga{i}", esD1a) for i in range(2)]
                        sgb = [fw.sb([128, 512], BF16, f"sgb{i}", esD1a) for i in range(2)]
                        t1 = [fw.sb([128, 512], F32, f"t1_{i}", esD1a) for i in range(2)]
                        t2 = [fw.sb([128, 512], F32, f"t2_{i}", esD1a) for i in range(2)]
                        it = 0
                        for j in range(8):
                            w_ = j % 2
                            fw.dma(wga[w_][:], w_mg_d[:, j * 128:(j + 1) * 128].rearrange("(k p) c -> p k c", p=128), writes=[wga[w_]], q="pool")
                            fw.dma(wgb[w_][:], w_mg_d[:, 1024 + j * 128:1024 + (j + 1) * 128].rearrange("(k p) c -> p k c", p=128), writes=[wgb[w_]], q="pool")
                            fw.dma(wpa[w_][:], w_pa_d[:, j * 128:(j + 1) * 128].rearrange("(k p) c -> p k c", p=128), writes=[wpa[w_]], q="pool")
                            fw.dma(wpb[w_][:], w_pb_d[:, j * 128:(j + 1) * 128].rearrange("(k p) c -> p k c", p=128), writes=[wpb[w_]], q="pool")
                            for tb in range(4):
                                r = it % 2
                                it += 1
                                b0 = 4 * r
                                ts_ = slice(tb * 512, (tb + 1) * 512)
                                for k in range(8):
                                    mm(PS[b0], PS[b0][:, :], wga[w_][:, k, :], hTo[:, k, ts_], k == 0, k == 7, [wga[w_], hTo])
                                op("act", lambda e: e.activation(out=sga[r][:], in_=PS[b0][:, :], func=AF.Sigmoid), reads=[PS[b0]], writes=[sga[r]])
                                for k in range(8):
                                    mm(PS[b0 + 1], PS[b0 + 1][:, :], wgb[w_][:, k, :], hTo[:, k, ts_], k == 0, k == 7, [wgb[w_], hTo])
                                op("act", lambda e: e.activation(out=sgb[r][:], in_=PS[b0 + 1][:, :], func=AF.Sigmoid), reads=[PS[b0 + 1]], writes=[sgb[r]])
                                for k in range(4):
                                    mm(PS[b0 + 2], PS[b0 + 2][:, :], wpa[w_][:, k, :], YT[:, k, ts_], k == 0, k == 3, [wpa[w_], YT])
                                for k in range(4):
                                    mm(PS[b0 + 3], PS[b0 + 3][:, :], wpb[w_][:, k, :], YT[:, 4 + k, ts_], k == 0, k == 3, [wpb[w_], YT])
                                op("dve", lambda e: e.tensor_tensor(out=t1[r][:], in0=PS[b0 + 2][:, :], in1=sga[r][:], op=ALU.mult), reads=[PS[b0 + 2], sga[r]], writes=[t1[r]])
                                op("dve", lambda e: e.tensor_tensor(out=t2[r][:], in0=PS[b0 + 3][:, :], in1=sgb[r][:], op=ALU.mult), reads=[PS[b0 + 3], sgb[r]], writes=[t2[r]])
                                op("pool", lambda e: e.tensor_tensor(out=mixT[:, j, ts_], in0=t1[r][:], in1=t2[r][:], op=ALU.add), reads=[t1[r], t2[r]], writes=[mixT])
                    ckpt("D1a")
                    with fw.scope() as esD1b:
                        w_out = fw.sb([128, 8, D], BF16, "w_out", esD1b)
                        fw.dma(w_out[:], w_out_d.rearrange("(k p) c -> p k c", p=128), writes=[w_out], q="pool")
                        xtl = [fw.sb([128, D], F32, f"xtl{i}", esD1b) for i in range(2)]
                        for t in range(NT_OWN):
                            x_ = xtl[t % 2]
                            fw.dma(x_[:], xe[S_OWN + t * 128:S_OWN + (t + 1) * 128, :], writes=[x_])
                            for half in range(2):
                                b = 2 * (t % 2) + half
                                for j in range(8):
                                    mm(PS[b], PS[b][:, :], mixT[:, j, t * 128:(t + 1) * 128], w_out[:, j, half * 512:(half + 1) * 512], j == 0, j == 7, [mixT, w_out])
                                op("dve", lambda e: e.tensor_tensor(out=x1[:, t, half * 512:(half + 1) * 512], in0=PS[b][:, :], in1=x_[:, half * 512:(half + 1) * 512], op=ALU.add),
                                   reads=[PS[b], x_], writes=[x1])
                ckpt("D1")
                if "x1" in dbg:
                    fw.dma(dbg_t("x1", [128, NT_OWN, D]), x1[:], reads=[x1], is_output=True)
                with fw.scope() as esM:
                    load_gain(1)
                    gateT = fw.sb([16, S_OWN], BF16, "gateT", esM)
                    E16 = fw.sb([16, 16, 128], BF16, "E16", esM)
                    op("pool", lambda e: e.memset(E16[:], 1.0), writes=[E16])
                    op("pool", lambda e: e.affine_select(out=E16[:], in_=E16[:], pattern=[[-1, 16], [0, 128]], compare_op=ALU.is_equal, fill=0.0,
                                                         base=0, channel_multiplier=1), reads=[E16], writes=[E16])
                    with fw.scope() as esR:
                        w_r = fw.sb([128, 8, 20], F32, "w_r", esR)
                        fw.dma(w_r[:], w_r_d.rearrange("(k p) c -> p k c", p=128), writes=[w_r])
                        b_r = fw.sb([128, 20], F32, "b_r", esR)
                        fw.dma(b_r[:], b_r_d[0:1, :].to_broadcast([128, 20]), writes=[b_r])
                        hnf = [fw.sb([128, D], F32, f"hnf{i}", esR) for i in range(2)]
                        hnTf = [fw.sb([128, 8, 128], F32, f"hnTf{i}", esR) for i in range(2)]
                        junkR = fw.sb([128, D], BF16, "junkR", esR)
                        ssr = [fw.sb([128, 1], F32, f"ssr{i}", esR) for i in range(2)]
                        rrr = [fw.sb([128, 1], F32, f"rrr{i}", esR) for i in range(2)]
                        T_ = NT_OWN
                        lgA = fw.sb([128, T_, 20], F32, "lgA", esR)

                        def r_front(t):
                            r = t % 2
                            rs = {"ss": ssr[r], "r": rrr[r]}
                            rms_rstd({"ap": x1[:, t, :], "bufs": [x1]}, rs, D, {"ap": junkR[:], "buf": junkR})
                            op("dve", lambda e: e.scalar_tensor_tensor(out=hnf[r][:], in0=x1[:, t, :], scalar=rs["r"][:], in1=gB[:], op0=ALU.mult, op1=ALU.mult),
                               reads=[x1, rs["r"], gB], writes=[hnf[r]])
                            for k in range(8):
                                b = 2 * r + (0 if k < 4 else 1)
                                op("pe", lambda e: e.transpose(out=PS[b][:, (k % 4) * 128:(k % 4 + 1) * 128], in_=hnf[r][:, k * 128:(k + 1) * 128], identity=idf[:]),
                                   reads=[hnf[r], idf], writes=[PS[b]])
                            for bb in range(2):
                                b = 2 * r + bb
                                op("act", lambda e: e.copy(out=hnTf[r][:, 4 * bb:4 * bb + 4, :], in_=PS[b][:, :].rearrange("p (k t) -> p k t", k=4)), reads=[PS[b]], writes=[hnTf[r]])
                                op("dve", lambda e: e.tensor_copy(out=YT[:, 4 * bb:4 * bb + 4, t * 128:(t + 1) * 128], in_=PS[b][:, :].rearrange("p (k t) -> p k t", k=4)),
                                   reads=[PS[b]], writes=[YT])

                        def r_back(t):
                            r = t % 2
                            bl = 4 + r
                            for k in range(8):
                                mm(PS[bl], PS[bl][:, 0:20], hnTf[r][:, k, :], w_r[:, k, :], k == 0, k == 7, [hnTf[r], w_r])
                            op("dve", lambda e: e.tensor_tensor(out=lgA[:, t, :], in0=PS[bl][:, 0:20], in1=b_r[:], op=ALU.add), reads=[PS[bl], b_r], writes=[lgA])

                        for t in range(T_ + 1):
                            if t < T_:
                                r_front(t)
                            if t >= 1:
                                r_back(t - 1)
                        gl = lgA[:, :, 0:4]
                        el = lgA[:, :, 4:20].rearrange("p t (g e) -> p t g e", g=4)
                        gmax = fw.sb([128, T_], F32, "gmax", esR)
                        g1h = fw.sb([128, T_, 4], F32, "g1h", esR)
                        exg = fw.sb([128, T_, 4], F32, "exg", esR)
                        pgs = fw.sb([128, T_], F32, "pgs", esR)
                        t16 = fw.sb([128, T_, 4, 4], F32, "t16", esR)
                        elg = fw.sb([128, T_, 4], F32, "elg", esR)
                        elg2 = fw.sb([128, T_, 4], F32, "elg2", esR)
                        ev1 = fw.sb([128, T_], F32, "ev1", esR)
                        ev2 = fw.sb([128, T_], F32, "ev2", esR)
                        mk1 = fw.sb([128, T_, 4], F32, "mk1", esR)
                        mk2 = fw.sb([128, T_, 4], F32, "mk2", esR)
                        w12 = fw.sb([128, 2, T_], F32, "w12", esR)
                        gig = fw.sb([128, T_, 4], F32, "gig", esR)
                        gate = fw.sb([128, T_, 4, 4], F32, "gate", esR)
                        B3 = [128, T_, 4]
                        op("dve", lambda e: e.tensor_reduce(out=gmax[:], in_=gl, axis=AX.X, op=ALU.max), reads=[lgA], writes=[gmax])
                        op("dve", lambda e: e.tensor_tensor(out=g1h[:], in0=gl, in1=gmax[:].unsqueeze(2).to_broadcast(B3), op=ALU.is_equal), reads=[lgA, gmax], writes=[g1h])
                        op("dve", lambda e: e.tensor_tensor(out=exg[:], in0=gl, in1=gmax[:].unsqueeze(2).to_broadcast(B3), op=ALU.subtract), reads=[lgA, gmax], writes=[exg])
                        op("act", lambda e: e.activation(out=exg[:], in_=exg[:], func=AF.Exp), reads=[exg], writes=[exg])
                        op("dve", lambda e: e.tensor_reduce(out=pgs[:], in_=exg[:], axis=AX.X, op=ALU.add), reads=[exg], writes=[pgs])
                        op("dve", lambda e: e.reciprocal(out=pgs[:], in_=pgs[:]), reads=[pgs], writes=[pgs])
                        op("dve", lambda e: e.tensor_tensor(out=t16[:], in0=el, in1=g1h[:].unsqueeze(3).to_broadcast([128, T_, 4, 4]), op=ALU.mult), reads=[lgA, g1h], writes=[t16])
                        op("dve", lambda e: e.tensor_reduce(out=elg[:], in_=t16[:].rearrange("p t g e -> p t e g"), axis=AX.X, op=ALU.add), reads=[t16], writes=[elg])
                        op("dve", lambda e: e.tensor_reduce(out=ev1[:], in_=elg[:], axis=AX.X, op=ALU.max), reads=[elg], writes=[ev1])
                        op("dve", lambda e: e.tensor_tensor(out=mk1[:], in0=elg[:], in1=ev1[:].unsqueeze(2).to_broadcast(B3), op=ALU.is_equal), reads=[elg, ev1], writes=[mk1])
                        op("dve", lambda e: e.scalar_tensor_tensor(out=elg2[:], in0=mk1[:], scalar=-1e30, in1=elg[:], op0=ALU.mult, op1=ALU.add), reads=[mk1, elg], writes=[elg2])
                        op("dve", lambda e: e.tensor_reduce(out=ev2[:], in_=elg2[:], axis=AX.X, op=ALU.max), reads=[elg2], writes=[ev2])
                        op("dve", lambda e: e.tensor_tensor(out=mk2[:], in0=elg2[:], in1=ev2[:].unsqueeze(2).to_broadcast(B3), op=ALU.is_equal), reads=[elg2, ev2], writes=[mk2])
                        op("dve", lambda e: e.tensor_tensor(out=w12[:, 0, :], in0=ev1[:], in1=ev2[:], op=ALU.subtract), reads=[ev1, ev2], writes=[w12])
                        op("act", lambda e: e.activation(out=w12[:, 0, :], in_=w12[:, 0, :], func=AF.Sigmoid), reads=[w12], writes=[w12])
                        op("dve", lambda e: e.tensor_scalar(out=w12[:, 1, :], in0=w12[:, 0, :], scalar1=-1.0, scalar2=1.0, op0=ALU.mult, op1=ALU.add), reads=[w12], writes=[w12])
                        op("dve", lambda e: e.tensor_tensor(out=w12[:], in0=w12[:], in1=pgs[:].unsqueeze(1).to_broadcast([128, 2, T_]), op=ALU.mult), reads=[w12, pgs], writes=[w12])
                        op("dve", lambda e: e.tensor_tensor(out=gig[:], in0=mk1[:], in1=w12[:, 0, :].unsqueeze(2).to_broadcast(B3), op=ALU.mult), reads=[mk1, w12], writes=[gig])
                        op("dve", lambda e: e.tensor_tensor(out=mk2[:], in0=mk2[:], in1=w12[:, 1, :].unsqueeze(2).to_broadcast(B3), op=ALU.mult), reads=[mk2, w12], writes=[mk2])
                        op("dve", lambda e: e.tensor_tensor(out=gig[:], in0=gig[:], in1=mk2[:], op=ALU.add), reads=[gig, mk2], writes=[gig])
                        op("dve", lambda e: e.tensor_tensor(out=gate[:], in0=g1h[:].unsqueeze(3).to_broadcast([128, T_, 4, 4]),
                                                            in1=gig[:].unsqueeze(2).to_broadcast([128, T_, 4, 4]), op=ALU.mult), reads=[g1h, gig], writes=[gate])
                        for t4 in range(T_ // 4):
                            bk = 6 + t4 % 2
                            for j in range(4):
                                t = t4 * 4 + j
                                op("pe", lambda e: e.transpose(out=PS[bk][0:16, j * 128:(j + 1) * 128], in_=gate[:, t, :, :].rearrange("p g e -> p (g e)"), identity=idf[:]),
                                   reads=[gate, idf], writes=[PS[bk]])
                            op("act", lambda e: e.copy(out=gateT[:, t4 * 512:(t4 + 1) * 512], in_=PS[bk][0:16, :]), reads=[PS[bk]], writes=[gateT])
                    ckpt("D2r")
                    if "gateT" in dbg:
                        fw.dma(dbg_t("gateT", [16, S_OWN], BF16), gateT[:], reads=[gateT], is_output=True)
                    with fw.scope() as esE:
                        w13 = [fw.sb([128, 8, 512], BF16, f"w13_{i}", esE) for i in range(2)]
                        w2e = [fw.sb([128, 2, D], BF16, f"w2e_{i}", esE) for i in range(2)]
                        sgE = [fw.sb([128, 512], F32, f"sgE{i}", esE) for i in range(2)]
                        tE = [fw.sb([128, 512], F32, f"tE{i}", esE) for i in range(2)]
                        actT = [[fw.sb([128, 512], BF16, f"actT{i}{fc}", esE) for fc in range(2)] for i in range(2)]
                        ybank = [4, 5, 7]
                        yi = 0
                        it = 0
                        for ex in range(16):
                            wb = ex % 2
                            fw.dma(w13[wb][:], w_e13_d[ex].rearrange("(k p) c -> p k c", p=128), writes=[w13[wb]], q="pool")
                            fw.dma(w2e[wb][:], w_e2_d[ex].rearrange("(k p) c -> p k c", p=128), writes=[w2e[wb]], q="pool")
                            for tb in range(4):
                                r = it % 2
                                it += 1
                                ts_ = slice(tb * 512, (tb + 1) * 512)
                                mm(PS[6], PS[6][:, :], E16[:, ex, :], gateT[:, ts_], True, True, [E16, gateT])
                                for fc in range(2):
                                    for k in range(8):
                                        mm(PS[fc], PS[fc][:, :], w13[wb][:, k, fc * 128:(fc + 1) * 128], YT[:, k, ts_], k == 0, k == 7, [w13[wb], YT])
                                    for k in range(8):
                                        mm(PS[2 + fc], PS[2 + fc][:, :], w13[wb][:, k, 256 + fc * 128:256 + (fc + 1) * 128], YT[:, k, ts_], k == 0, k == 7, [w13[wb], YT])
                                    op("act", lambda e: e.activation(out=sgE[fc][:], in_=PS[fc][:, :], func=AF.Silu), reads=[PS[fc]], writes=[sgE[fc]])
                                    op("dve", lambda e: e.tensor_tensor(out=tE[fc][:], in0=PS[2 + fc][:, :], in1=sgE[fc][:], op=ALU.mult), reads=[PS[2 + fc], sgE[fc]], writes=[tE[fc]])
                                    op("dve", lambda e: e.tensor_tensor(out=actT[r][fc][:], in0=PS[6][:, :], in1=tE[fc][:], op=ALU.mult), reads=[PS[6], tE[fc]], writes=[actT[r][fc]])
                                for tt in range(4):
                                    t = tb * 4 + tt
                                    for half in range(2):
                                        b = ybank[yi % 3]
                                        yi += 1
                                        for fc in range(2):
                                            mm(PS[b], PS[b][:, :], actT[r][fc][:, tt * 128:(tt + 1) * 128], w2e[wb][:, fc, half * 512:(half + 1) * 512], fc == 0, fc == 1, [actT[r][fc], w2e[wb]])
                                        op("dve", lambda e: e.tensor_tensor(out=x1[:, t, half * 512:(half + 1) * 512], in0=PS[b][:, :], in1=x1[:, t, half * 512:(half + 1) * 512], op=ALU.add),
                                           reads=[PS[b], x1], writes=[x1])
                ckpt("D2")
                if "x2" in dbg:
                    fw.dma(dbg_t("x2", [128, NT_OWN, D]), x1[:], reads=[x1], is_output=True)
                with fw.scope() as esP:
                    load_gain(2)
                    gB2 = fw.sb([128, D], F32, "gB2", esP)
                    fw.dma(gB2[:], gvec_d[3:4, :].to_broadcast([128, D]), writes=[gB2])
                    w_pg = fw.sb([128, 8, D], BF16, "w_pg", esP)
                    fw.dma(w_pg[:], w_pg_d.rearrange("(k p) c -> p k c", p=128), writes=[w_pg], q="pool")
                    w_pp = fw.sb([128, 2, D], BF16, "w_pp", esP)
                    fw.dma(w_pp[:], w_pp_d.rearrange("(k p) c -> p k c", p=128), writes=[w_pp], q="pool")
                    hpb = [fw.sb([128, D], BF16, f"hpb{i}", esP) for i in range(3)]
                    hpT = [fw.sb([128, 8, 128], BF16, f"hpT{i}", esP) for i in range(3)]
                    plb = [fw.sb([128, 256], BF16, f"plb{i}", esP) for i in range(3)]
                    plT = [fw.sb([128, 2, 128], BF16, f"plT{i}", esP) for i in range(3)]
                    junkP2 = fw.sb([128, D], BF16, "junkP2", esP)
                    sgP = [fw.sb([128, 512], F32, f"sgP{i}", esP) for i in range(2)]
                    tP = [fw.sb([128, 512], F32, f"tP{i}", esP) for i in range(2)]
                    outt = [fw.sb([128, D], F32, f"outt{i}", esP) for i in range(2)]
                    junkP = fw.sb([128, D], BF16, "junkP", esP)
                    ssp = [fw.sb([128, 1], F32, f"ssp{i}", esP) for i in range(5)]
                    rrp = [fw.sb([128, 1], F32, f"rrp{i}", esP) for i in range(5)]
                    x1T = [Buf(x1.t, f"x1_{t}") for t in range(NT_OWN)]
                    for b_ in x1T:
                        b_.lw = x1.lw
                        b_.rd = dict(x1.rd)

                    def p_s1(t):
                        r = t % 3
                        fw.dma(plb[r][:], pl_d[t * 128:(t + 1) * 128, :], writes=[plb[r]], q="pool")
                        rs = {"ss": ssp[r], "r": rrp[r]}
                        rms_rstd({"ap": x1[:, t, :], "bufs": [x1T[t]]}, rs, D, {"ap": junkP[:], "buf": junkP})
                        op("dve", lambda e: e.scalar_tensor_tensor(out=hpb[r][:], in0=x1[:, t, :], scalar=rs["r"][:], in1=gB[:], op0=ALU.mult, op1=ALU.mult),
                           reads=[x1T[t], rs["r"], gB], writes=[hpb[r]])

                    def p_s2(t):
                        r = t % 3
                        b0 = 2 * (t % 2)
                        for k in range(8):
                            op("pe", lambda e: e.transpose(out=psbf(b0)[:, k * 128:(k + 1) * 128], in_=hpb[r][:, k * 128:(k + 1) * 128], identity=idb[:]), reads=[hpb[r], idb], writes=[PS[b0]])
                        op("act", lambda e: e.copy(out=hpT[r][:], in_=psbf(b0).rearrange("p (k t) -> p k t", k=8)), reads=[PS[b0]], writes=[hpT[r]])
                        for k in range(2):
                            op("pe", lambda e: e.transpose(out=psbf(b0 + 1)[:, k * 128:(k + 1) * 128], in_=plb[r][:, k * 128:(k + 1) * 128], identity=idb[:]), reads=[plb[r], idb], writes=[PS[b0 + 1]])
                        op("act", lambda e: e.copy(out=plT[r][:], in_=psbf(b0 + 1)[:, 0:256].rearrange("p (k t) -> p k t", k=2)), reads=[PS[b0 + 1]], writes=[plT[r]])

                    def p_s3(t):
                        r = t % 3
                        for half in range(2):
                            hs = slice(half * 512, (half + 1) * 512)
                            bG = 4 + half
                            bP = 6 + half
                            for k in range(8):
                                mm(PS[bG], PS[bG][:, :], hpT[r][:, k, :], w_pg[:, k, hs], k == 0, k == 7, [hpT[r], w_pg])
                            for k in range(2):
                                mm(PS[bP], PS[bP][:, :], plT[r][:, k, :], w_pp[:, k, hs], k == 0, k == 1, [plT[r], w_pp])
                            op("act", lambda e: e.activation(out=sgP[half][:], in_=PS[bG][:, :], func=AF.Sigmoid), reads=[PS[bG]], writes=[sgP[half]])
                            op("dve", lambda e: e.tensor_tensor(out=tP[half][:], in0=PS[bP][:, :], in1=sgP[half][:], op=ALU.mult), reads=[PS[bP], sgP[half]], writes=[tP[half]])
                            op("dve", lambda e: e.tensor_tensor(out=x1[:, t, hs], in0=x1[:, t, hs], in1=tP[half][:], op=ALU.add), reads=[x1T[t], tP[half]], writes=[x1T[t]])
                        rs2 = {"ss": ssp[3 + t % 2], "r": rrp[3 + t % 2]}
                        rms_rstd({"ap": x1[:, t, :], "bufs": [x1T[t]]}, rs2, D, {"ap": junkP2[:], "buf": junkP2})
                        o_ = outt[t % 2]
                        op("dve", lambda e: e.scalar_tensor_tensor(out=o_[:], in0=x1[:, t, :], scalar=rs2["r"][:], in1=gB2[:], op0=ALU.mult, op1=ALU.mult),
                           reads=[x1T[t], rs2["r"], gB2], writes=[o_])
                        fw.dma(out_d[t * 128:(t + 1) * 128, :], o_[:], reads=[o_], is_output=True)

                    for i in range(NT_OWN + 2):
                        if i < NT_OWN:
                            p_s1(i)
                        if 1 <= i <= NT_OWN:
                            p_s2(i - 1)
                        if i >= 2:
                            p_s3(i - 2)

            if "yaT" in dbg:
                o = dbg_t("yaT", [128, 4, S_OWN], BF16)
                fw.dma(o[:, :, :], YT[:, 0:4, :], reads=[YT], is_output=True)

            if "hT" in dbg:
                o = dbg_t("hT", [128, 8, S_EXT], BF16)
                with fw.scope() as esd:
                    tmp = fw.sb([128, 8, 512], BF16, "dbg_hT", esd)
                    for i in range(8):
                        fw.dma(tmp[:], hT_d[:, :, i * 512:(i + 1) * 512], reads=hT_tiles[4 * i:4 * i + 4], writes=[tmp])
                        fw.dma(o[:, :, i * 512:(i + 1) * 512], tmp[:], reads=[tmp], is_output=True)


        body()
        fw.stopped = False
        fw.finish()
    return nc, dbg_out


_INV = (500000.0 ** (-np.arange(0, 16, 2, dtype=np.float32) / 16.0)).astype(np.float32)


def make_in_maps(inputs):
    f = lambda a: np.ascontiguousarray(np.asarray(a), dtype=np.float32)
    x = f(inputs["x"]); p = f(inputs["p"])
    positions = np.asarray(inputs["positions"]).astype(np.int32)
    w_in = f(inputs["w_in"])[0]
    offs = np.cumsum([0, 512, 128, 128, 128, 128, 128, 128, 24, 1024, 512, 512, 8, 2048])
    seg = {n: (offs[i], offs[i + 1]) for i, n in enumerate(["q", "kc", "vc", "ks", "vs", "kw", "vw", "gate", "qk", "v", "o", "if", "mg"])}
    col = lambda n: w_in[:, seg[n][0]:seg[n][1]]
    w_att = []
    for g in range(2):
        parts = [col("q")[:, g * 256:(g + 1) * 256]]
        for n in ["ks", "kw", "kc", "vc", "vs", "vw"]:
            parts.append(col(n)[:, g * 64:(g + 1) * 64])
        parts.append(col("gate")[:, g * 12:(g + 1) * 12])
        w_att.append(np.concatenate(parts, axis=1))
    w_att = np.ascontiguousarray(np.stack(w_att))
    shared = {
        "invf": np.ascontiguousarray(np.broadcast_to(_INV[None, :], (128, 8))),
        "gvec": np.ascontiguousarray(np.stack([f(inputs["g_mix"])[0], f(inputs["g_ffn"])[0], f(inputs["g_ple"])[0], f(inputs["g_final"])])),
        "w_att": w_att,
        "w_qk": np.ascontiguousarray(col("qk")),
        "w_vo": np.ascontiguousarray(np.concatenate([col("v"), col("o")], axis=1)),
        "w_if": np.ascontiguousarray(col("if")),
        "w_mg": np.ascontiguousarray(col("mg")),
        "b_if": f(inputs["b_if"]).reshape(1, 8),
        "w_c1": np.ascontiguousarray(np.stack([f(inputs["w_ck1"])[0], f(inputs["w_cv1"])[0]])),
        "w_c2": np.ascontiguousarray(np.stack([f(inputs["w_ck2"])[0], f(inputs["w_cv2"])[0]])),
        "pe_c": np.ascontiguousarray(np.stack([f(inputs["pe_ck"])[0], f(inputs["pe_cv"])[0]])),
        "wc": np.ascontiguousarray(f(inputs["w_conv"])[0].reshape(4, 8, 128).transpose(2, 1, 0)),
        "bc": np.ascontiguousarray(f(inputs["b_conv"])[0].reshape(8, 128).T),
        "g_hn": f(inputs["g_hn"]).reshape(1, 512),
        "w_pa": f(inputs["w_pa"])[0], "w_pb": f(inputs["w_pb"])[0], "w_out": f(inputs["w_out"])[0],
        "w_r": np.ascontiguousarray(np.concatenate([f(inputs["w_rg"])[0], f(inputs["w_re"])[0]], axis=1)),
        "b_r": np.ascontiguousarray(np.concatenate([f(inputs["b_rg"])[0], f(inputs["b_re"])[0]])[None, :]),
        "w_e13": f(inputs["w_e13"])[0], "w_e2": f(inputs["w_e2"])[0],
        "w_pg": f(inputs["w_pg"])[0], "w_pp": f(inputs["w_pp"])[0],
    }
    in_maps = []
    for core in range(8):
        b, half = core // 2, core % 2
        if half == 1:
            xe_ = x[b]
            pos_ = positions[b]
        else:
            xe_ = np.concatenate([np.zeros((S_OWN, D), np.float32), x[b, :S_OWN]], axis=0)
            pos_ = np.concatenate([np.zeros(S_OWN, np.int32), positions[b, :S_OWN]])
        m = dict(shared)
        m["xe"] = np.ascontiguousarray(xe_)
        m["pos"] = np.ascontiguousarray(pos_.reshape(NT_EXT, 128).T)
        m["pl"] = np.ascontiguousarray(p[0, b, half * S_OWN:(half + 1) * S_OWN])
        m["hv"] = np.full((128, 1), float(half), np.float32)
        in_maps.append(m)
    return in_maps


def kernel(**inputs):
    nc, _ = build_program()
    in_maps = make_in_maps(inputs)
    res = run_bass_kernel_spmd(nc, in_maps, core_ids=list(range(8)))
    out = np.zeros((4, S_EXT, D), np.float32)
    for core in range(8):
        b, half = core // 2, core % 2
        out[b, half * S_OWN:(half + 1) * S_OWN] = res.results[core]["out"]
    return out
```

```python
import numpy as np
import concourse.bass as bass
import concourse.mybir as mybir
from concourse.bass_utils import run_bass_kernel_spmd
from contextlib import ExitStack

F32 = mybir.dt.float32
BF16 = mybir.dt.bfloat16
I32 = mybir.dt.int32
AF = mybir.ActivationFunctionType
ALU = mybir.AluOpType
AX = mybir.AxisListType

D = 1024
S_OWN = 2048
S_EXT = 4096
NT_OWN = 16
NT_EXT = 32
EPS = 1e-6
NEGB = -30000.0
DBG = []


class Buf:
    __slots__ = ("t", "lw", "rd", "name", "excl")

    def __init__(self, t, name=""):
        self.t = t
        self.excl = False
        self.lw = None
        self.rd = {}
        self.name = name

    def __getitem__(self, k):
        return self.t[k]


class FW:
    NDMA = 24

    def __init__(self, nc, es):
        self.nc = nc
        self.es = es
        self.eng = {"pe": nc.tensor, "act": nc.scalar, "dve": nc.vector, "pool": nc.gpsimd, "sp": nc.sync}
        self.sem = {k: es.enter_context(nc.semaphore("s_" + k)) for k in self.eng}
        self.cnt = {k: 0 for k in self.eng}
        self.known = {k: {} for k in self.eng}
        self.dsem = [es.enter_context(nc.semaphore(f"s_dma{i}")) for i in range(self.NDMA)]
        self.dval = [0] * self.NDMA
        self.dnext = 0
        self.nbuf = 0
        self.out_waits = []
        self.stopped = False

    def sb(self, shape, dt, name=None, es=None):
        self.nbuf += 1
        name = f"sb{self.nbuf}_" + (name or "t")
        return Buf((es or self.es).enter_context(self.nc.sbuf_tensor(name, list(shape), dt)), name)

    def ps(self, shape, dt, name=None):
        self.nbuf += 1
        name = name or f"ps{self.nbuf}"
        b = Buf(self.es.enter_context(self.nc.psum_tensor(name, list(shape), dt)), name)
        b.excl = True
        return b

    def _wait(self, e, src, idx):
        if self.stopped:
            return
        kn = self.known[e]
        if kn.get(src, 0) >= idx:
            return
        s = self.dsem[src[1]] if isinstance(src, tuple) else self.sem[src]
        self.eng[e].wait_ge(s, idx)
        kn[src] = idx

    def _deps(self, e, reads, writes):
        for b in reads:
            if b.lw is not None:
                self._wait(e, b.lw[0], b.lw[1])
            if b.excl:
                for src, idx in b.rd.items():
                    if src != e:
                        self._wait(e, src, idx)
        for b in writes:
            if b.lw is not None and b.lw[0] != e:
                self._wait(e, b.lw[0], b.lw[1])
            for src, idx in b.rd.items():
                if src != e:
                    self._wait(e, src, idx)

    def op(self, e, fn, reads=(), writes=()):
        if self.stopped:
            return None
        self._deps(e, reads, writes)
        inst = fn(self.eng[e])
        self.cnt[e] += 1
        c = self.cnt[e]
        inst.then_inc(self.sem[e], 1)
        for b in reads:
            if b.rd.get(e, 0) < c:
                b.rd[e] = c
        for b in writes:
            b.lw = (e, c)
            b.rd = {}
        return inst

    def dma(self, out, in_, reads=(), writes=(), q="sp", is_output=False):
        if self.stopped and not is_output:
            return None
        self._deps(q, reads, writes)
        slot = self.dnext
        self.dnext = (self.dnext + 1) % self.NDMA
        key = ("d", slot)
        if self.dval[slot] > 0:
            self._wait(q, key, self.dval[slot])
        inst = self.eng[q].dma_start(out=out, in_=in_)
        self.dval[slot] += 16
        inst.then_inc(self.dsem[slot], 16)
        v = self.dval[slot]
        for b in reads:
            if b.rd.get(key, 0) < v:
                b.rd[key] = v
        for b in writes:
            b.lw = (key, v)
            b.rd = {}
        if is_output:
            self.out_waits.append((key, v))
        return inst

    def barrier(self):
        for e in self.eng:
            for src in ("pe", "act", "dve", "pool"):
                if src != e and self.cnt[src] > 0:
                    self._wait(e, src, self.cnt[src])
            for slot in range(self.NDMA):
                if self.dval[slot] > 0:
                    self._wait(e, ("d", slot), self.dval[slot])

    def scope(self):
        fw = self

        class _Scope(ExitStack):
            def __exit__(self, *a):
                fw.barrier()
                return super().__exit__(*a)
        return _Scope()

    def finish(self):
        for key, v in self.out_waits:
            self._wait("sp", key, v)
        for k in ("pe", "act", "dve", "pool"):
            if self.cnt[k] > 0:
                self._wait("sp", k, self.cnt[k])


class _StopBuild(Exception):
    pass


def build_program(dbg=()):
    nc = bass.Bass("TRN2", target_bir_lowering=False)

    def din(name, shape, dt=F32):
        return nc.dram_tensor(name, list(shape), dt, kind="ExternalInput").ap()

    xe = din("xe", [S_EXT, D])
    pos_d = din("pos", [128, NT_EXT], I32)
    pl_d = din("pl", [S_OWN, 256])
    hv_d = din("hv", [128, 1])
    invf_d = din("invf", [128, 8])
    gvec_d = din("gvec", [4, D])
    w_att_d = din("w_att", [2, D, 652])
    w_qk_d = din("w_qk", [D, 1024])
    w_vo_d = din("w_vo", [D, 1024])
    w_if_d = din("w_if", [D, 8])
    w_mg_d = din("w_mg", [D, 2048])
    b_if_d = din("b_if", [1, 8])
    w_c1_d = din("w_c1", [2, 2048, 256])
    w_c2_d = din("w_c2", [2, 256, 64])
    pe_c_d = din("pe_c", [2, 32, 64])
    wc_d = din("wc", [128, 8, 4])
    bc_d = din("bc", [128, 8])
    g_hn_d = din("g_hn", [1, 512])
    w_pa_d = din("w_pa", [512, D])
    w_pb_d = din("w_pb", [512, D])
    w_out_d = din("w_out", [D, D])
    w_r_d = din("w_r", [D, 20])
    b_r_d = din("b_r", [1, 20])
    w_e13_d = din("w_e13", [16, D, 512])
    w_e2_d = din("w_e2", [16, 256, D])
    w_pg_d = din("w_pg", [D, D])
    w_pp_d = din("w_pp", [256, D])
    out_d = nc.dram_tensor("out", [S_OWN, D], F32, kind="ExternalOutput").ap()
    hT_d = nc.dram_tensor("hT_scr", [128, 8, S_EXT], BF16, kind="Internal").ap()
    dbg_out = {}

    def dbg_t(name, shape, dt=F32):
        dbg_out[name] = nc.dram_tensor("dbg_" + name, list(shape), dt, kind="ExternalOutput").ap()
        return dbg_out[name]

    with ExitStack() as es:
        fw = FW(nc, es)
        op = fw.op
        PS = [fw.ps([128, 512], F32, f"psb{i}") for i in range(8)]

        def psbf(i):
            return PS[i][:].bitcast(BF16)

        ones_f = fw.sb([128, 128], F32, "ones_f")
        op("pool", lambda e: e.memset(ones_f[:], 1.0), writes=[ones_f])
        idf = fw.sb([128, 128], F32, "idf")
        op("pool", lambda e: e.affine_select(out=idf[:], in_=ones_f[:], pattern=[[1, 128]], compare_op=ALU.is_equal,
                                             fill=0.0, base=0, channel_multiplier=-1), reads=[ones_f], writes=[idf])
        idb = fw.sb([128, 128], BF16, "idb")
        op("dve", lambda e: e.tensor_copy(out=idb[:], in_=idf[:]), reads=[idf], writes=[idb])
        U_f = fw.sb([128, 128], F32, "U_f")
        op("pool", lambda e: e.affine_select(out=U_f[:], in_=ones_f[:], pattern=[[1, 128]], compare_op=ALU.is_ge,
                                             fill=0.0, base=0, channel_multiplier=-1), reads=[ones_f], writes=[U_f])
        caus = fw.sb([128, 128], BF16, "caus")
        op("dve", lambda e: e.tensor_copy(out=caus[:], in_=U_f[:]), reads=[U_f], writes=[caus])
        wm0_f = fw.sb([128, 128], F32, "wm0_f")
        op("pool", lambda e: e.affine_select(out=wm0_f[:], in_=ones_f[:], pattern=[[-1, 128]], compare_op=ALU.is_ge,
                                             fill=0.0, base=-1, channel_multiplier=1), reads=[ones_f], writes=[wm0_f])
        wm0 = fw.sb([128, 128], BF16, "wm0")
        op("dve", lambda e: e.tensor_copy(out=wm0[:], in_=wm0_f[:]), reads=[wm0_f], writes=[wm0])
        c_eps = fw.sb([128, 1], F32, "c_eps")
        op("pool", lambda e: e.memset(c_eps[:], EPS), writes=[c_eps])
        c_one = fw.sb([128, 1], F32, "c_one")
        op("pool", lambda e: e.memset(c_one[:], 1.0), writes=[c_one])
        c_zero = fw.sb([128, 1], F32, "c_zero")
        op("pool", lambda e: e.memset(c_zero[:], 0.0), writes=[c_zero])
        acc_junk = fw.sb([128, 2], F32, "acc_junk")
        op("act", lambda e: e.activation(out=acc_junk[:, 0:1], in_=c_one[:], func=AF.Square, accum_out=acc_junk[:, 1:2]),
           reads=[c_one], writes=[acc_junk])
        hv = fw.sb([128, 1], F32, "hv")
        fw.dma(hv[:], hv_d[:, :], writes=[hv])
        hbias = fw.sb([128, 1], F32, "hbias")
        op("dve", lambda e: e.tensor_scalar(out=hbias[:], in0=hv[:], scalar1=-1.0, scalar2=-NEGB, op0=ALU.add, op1=ALU.mult),
           reads=[hv], writes=[hbias])
        gB = fw.sb([128, D], F32, "gB")

        def load_gain(i):
            fw.dma(gB[:], gvec_d[i:i + 1, :].to_broadcast([128, D]), writes=[gB])

        cs = fw.sb([128, NT_EXT, 8], F32, "cs")
        sn = fw.sb([128, NT_EXT, 8], F32, "sn")
        with fw.scope() as es1:
            posi = fw.sb([128, NT_EXT], I32, "posi", es1)
            posf = fw.sb([128, NT_EXT], F32, "posf", es1)
            invf = fw.sb([128, 8], F32, "invf", es1)
            ang = fw.sb([128, NT_EXT, 8], F32, "ang", es1)
            kf = fw.sb([128, NT_EXT, 8], F32, "kf", es1)
            ki = fw.sb([128, NT_EXT, 8], I32, "ki", es1)
            r1 = fw.sb([128, NT_EXT, 8], F32, "r1", es1)
            r2 = fw.sb([128, NT_EXT, 8], F32, "r2", es1)
            fw.dma(posi[:], pos_d[:, :], writes=[posi])
            fw.dma(invf[:], invf_d[:, :], writes=[invf])
            op("dve", lambda e: e.tensor_copy(out=posf[:], in_=posi[:]), reads=[posi], writes=[posf])
            op("dve", lambda e: e.tensor_tensor(out=ang[:], in0=posf[:].unsqueeze(2).to_broadcast([128, NT_EXT, 8]),
                                                in1=invf[:].unsqueeze(1).to_broadcast([128, NT_EXT, 8]), op=ALU.mult),
               reads=[posf, invf], writes=[ang])
            TWO_PI = 6.283185307179586
            C1 = 6.28125
            C2 = TWO_PI - C1
            PI_LO = 3.1415925
            op("dve", lambda e: e.tensor_scalar(out=kf[:], in0=ang[:], scalar1=1.0 / TWO_PI, scalar2=None, op0=ALU.mult),
               reads=[ang], writes=[kf])
            op("dve", lambda e: e.tensor_copy(out=ki[:], in_=kf[:]), reads=[kf], writes=[ki])
            op("dve", lambda e: e.tensor_copy(out=kf[:], in_=ki[:]), reads=[ki], writes=[kf])
            op("dve", lambda e: e.scalar_tensor_tensor(out=r1[:], in0=kf[:], scalar=-C1, in1=ang[:], op0=ALU.mult, op1=ALU.add),
               reads=[kf, ang], writes=[r1])
            op("dve", lambda e: e.scalar_tensor_tensor(out=r1[:], in0=kf[:], scalar=-C2, in1=r1[:], op0=ALU.mult, op1=ALU.add),
               reads=[kf, r1], writes=[r1])
            op("dve", lambda e: e.tensor_scalar(out=r1[:], in0=r1[:], scalar1=PI_LO, scalar2=-PI_LO, op0=ALU.min, op1=ALU.max),
               reads=[r1], writes=[r1])
            op("act", lambda e: e.activation(out=sn[:], in_=r1[:], func=AF.Sin), reads=[r1], writes=[sn])
            op("dve", lambda e: e.tensor_scalar(out=r2[:], in0=r1[:], scalar1=PI_LO / 2 + 0.0, scalar2=None, op0=ALU.add),
               reads=[r1], writes=[r2])
            op("dve", lambda e: e.tensor_scalar(out=kf[:], in0=r2[:], scalar1=PI_LO, scalar2=-TWO_PI, op0=ALU.is_gt, op1=ALU.mult),
               reads=[r2], writes=[kf])
            op("dve", lambda e: e.tensor_tensor(out=r2[:], in0=r2[:], in1=kf[:], op=ALU.add), reads=[r2, kf], writes=[r2])
            op("dve", lambda e: e.tensor_scalar(out=r2[:], in0=r2[:], scalar1=PI_LO, scalar2=-PI_LO, op0=ALU.min, op1=ALU.max),
               reads=[r2], writes=[r2])
            op("act", lambda e: e.activation(out=cs[:], in_=r2[:], func=AF.Sin), reads=[r2], writes=[cs])

        def rms_rstd(src, rstd, n, junk):
            ss = rstd["ss"]
            op("act", lambda e: e.activation(out=junk["ap"], in_=src["ap"], func=AF.Square, accum_out=ss[:]),
               reads=src["bufs"], writes=[junk["buf"], ss])
            op("act", lambda e: e.activation(out=ss[:], in_=ss[:], func=AF.Sqrt, bias=c_eps[:], scale=1.0 / n),
               reads=[ss, c_eps], writes=[ss])
            op("dve", lambda e: e.reciprocal(out=rstd["r"][:], in_=ss[:]), reads=[ss], writes=[rstd["r"]])

        load_gain(0)
        hT_tiles = [Buf(None, f"hT_tile{t}") for t in range(NT_EXT)]
        with fw.scope() as esA:
            xt = [fw.sb([128, D], F32, f"xtA{i}", esA) for i in range(6)]
            xn = [fw.sb([128, D], BF16, f"xnA{i}", esA) for i in range(3)]
            junk = fw.sb([128, D], BF16, "junkA", esA)
            hst = [fw.sb([128, 8, 128], BF16, f"hstA{i}", esA) for i in range(4)]
            ssA = [fw.sb([128, 1], F32, f"ssA{i}", esA) for i in range(3)]
            rrA = [fw.sb([128, 1], F32, f"rrA{i}", esA) for i in range(3)]
            def a_s1(t):
                x_ = xt[t % 6]
                if t == 0:
                    for tt in range(5):
                        fw.dma(xt[tt][:], xe[tt * 128:(tt + 1) * 128, :], writes=[xt[tt]])
                if t + 5 < NT_EXT:
                    fw.dma(xt[(t + 5) % 6][:], xe[(t + 5) * 128:(t + 6) * 128, :], writes=[xt[(t + 5) % 6]])
                rs = {"ss": ssA[t % 3], "r": rrA[t % 3]}
                rms_rstd({"ap": x_[:], "bufs": [x_]}, rs, D, {"ap": junk[:], "buf": junk})
                n_ = xn[t % 3]
                op("dve", lambda e: e.scalar_tensor_tensor(out=n_[:], in0=x_[:], scalar=rs["r"][:], in1=gB[:], op0=ALU.mult, op1=ALU.mult),
                   reads=[x_, rs["r"], gB], writes=[n_])

            def a_s2(t):
                n_ = xn[t % 3]
                pb = t % 2
                for k in range(8):
                    op("pe", lambda e: e.transpose(out=psbf(pb)[:, k * 128:(k + 1) * 128], in_=n_[:, k * 128:(k + 1) * 128], identity=idb[:]),
                       reads=[n_, idb], writes=[PS[pb]])
                h_ = hst[t % 4]
                op("act", lambda e: e.copy(out=h_[:], in_=psbf(pb).rearrange("p (k t) -> p k t", k=8)), reads=[PS[pb]], writes=[h_])
                fw.dma(hT_d[:, :, t * 128:(t + 1) * 128], h_[:], reads=[h_], writes=[hT_tiles[t]], q="pool")

            for t in range(NT_EXT + 1):
                if t < NT_EXT:
                    a_s1(t)
                if t >= 1:
                    a_s2(t - 1)

        if "cs" in dbg:
            o = dbg_t("cs", [128, NT_EXT, 8])
            fw.dma(o[:, :, :], cs[:], reads=[cs], is_output=True)
            o = dbg_t("sn", [128, NT_EXT, 8])
            fw.dma(o[:, :, :], sn[:], reads=[sn], is_output=True)

        def ckpt(name):
            if ("stop_" + name) in dbg:
                fw.stopped = True

        def body():
            def mm(bank, out_ap, lhsT, rhs, start, stop, reads):
                op("pe", lambda e: e.matmul(out_ap, lhsT, rhs, start=start, stop=stop), reads=reads, writes=[bank])

            YT = fw.sb([128, 8, S_OWN], BF16, "YT")
            esBc = fw.scope()
            esBc.__enter__()
            cmask = fw.sb([128, 2, S_OWN], BF16, "cmask", esBc)
            op("pool", lambda e: e.memset(cmask[:], 1.0), writes=[cmask])
            op("pool", lambda e: e.affine_select(out=cmask[:, 0, :], in_=cmask[:, 0, :], pattern=[[1, S_OWN]], compare_op=ALU.is_ge, fill=0.0,
                                                 base=2017, channel_multiplier=-16), reads=[cmask], writes=[cmask])
            op("pool", lambda e: e.affine_select(out=cmask[:, 1, :], in_=cmask[:, 1, :], pattern=[[1, S_OWN]], compare_op=ALU.is_ge, fill=0.0,
                                                 base=-31, channel_multiplier=-16), reads=[cmask], writes=[cmask])
            ovl = fw.sb([128, 2, 64], BF16, "ovl", esBc)
            op("pool", lambda e: e.memset(ovl[:], 1.0), writes=[ovl])
            for j in range(2):
                op("pool", lambda e: e.affine_select(out=ovl[:, j, :], in_=ovl[:, j, :], pattern=[[-4, 64]], compare_op=ALU.is_ge, fill=0.0,
                                                     base=128 * j + 1, channel_multiplier=1), reads=[ovl], writes=[ovl])
                op("pool", lambda e: e.affine_select(out=ovl[:, j, :], in_=ovl[:, j, :], pattern=[[4, 64]], compare_op=ALU.is_ge, fill=0.0,
                                                     base=3 - 128 * j, channel_multiplier=-1), reads=[ovl], writes=[ovl])
            maskadd = fw.sb([128, NT_OWN, 64], F32, "maskadd", esBc)
            Mb = fw.sb([128, 64], F32, "Mb", esBc)
            hm1 = fw.sb([128, 2], F32, "hm1", esBc)
            op("dve", lambda e: e.tensor_scalar(out=hm1[:, 0:1], in0=hv[:], scalar1=-1.0, scalar2=1e30, op0=ALU.add, op1=ALU.mult),
               reads=[hv], writes=[hm1])
            op("dve", lambda e: e.tensor_scalar(out=hm1[:, 1:2], in0=hv[:], scalar1=-1.0, scalar2=-1000.0, op0=ALU.add, op1=ALU.mult),
               reads=[hv, hm1], writes=[hm1])
            op("dve", lambda e: e.memset(Mb[:], 0.0), writes=[Mb])
            op("dve", lambda e: e.tensor_copy(out=Mb[:, 0:32], in_=hm1[:, 0:1].to_broadcast([128, 32])), reads=[hm1, Mb], writes=[Mb])
            op("dve", lambda e: e.scalar_tensor_tensor(out=Mb[:, 0:1], in0=hv[:], scalar=1000.0, in1=Mb[:, 0:1], op0=ALU.mult, op1=ALU.add),
               reads=[hv, Mb], writes=[Mb])
            op("dve", lambda e: e.tensor_copy(out=Mb[:, 32:33], in_=hm1[:, 1:2]), reads=[hm1, Mb], writes=[Mb])
            for c in range(NT_OWN):
                op("pool", lambda e: e.tensor_copy(out=maskadd[:, c, :], in_=Mb[:]), reads=[Mb, maskadd], writes=[maskadd])
                for hf in range(2):
                    lo = 32 + 2 * c + hf + 1
                    if lo < 64:
                        op("pool", lambda e: e.memset(maskadd[hf * 64:(hf + 1) * 64, c, lo:64], -1e30), reads=[maskadd], writes=[maskadd])
                    for col in (32 + 2 * c + hf, 32 + 2 * c + hf - 1):
                        op("pool", lambda e: e.tensor_scalar(out=maskadd[hf * 64:(hf + 1) * 64, c, col:col + 1],
                                                             in0=maskadd[hf * 64:(hf + 1) * 64, c, col:col + 1],
                                                             scalar1=1000.0, scalar2=None, op0=ALU.add), reads=[maskadd], writes=[maskadd])

            ckpt("consts")
            for g in range(2):
                with fw.scope() as esG:
                    qT = fw.sb([128, 4, S_OWN], BF16, f"qT{g}", esG)
                    kkT = fw.sb([128, 2, S_EXT], BF16, f"kkT{g}", esG)
                    op("pool", lambda e: e.memset(qT[64:128, :, :], 0.0), writes=[qT])
                    op("pool", lambda e: e.memset(kkT[64:128, 0, :], 1.0), writes=[kkT])
                    op("pool", lambda e: e.memset(kkT[64:128, 1, :], 0.0), writes=[kkT])
                    op("pool", lambda e: e.affine_select(out=kkT[64:128, 0, :], in_=kkT[64:128, 0, :], pattern=[[1, S_EXT]], compare_op=ALU.is_ge, fill=0.0,
                                                         base=0, channel_multiplier=-64), reads=[kkT], writes=[kkT])
                    op("pool", lambda e: e.affine_select(out=kkT[64:128, 0, :], in_=kkT[64:128, 0, :], pattern=[[-1, S_EXT]], compare_op=ALU.is_ge, fill=0.0,
                                                         base=63, channel_multiplier=64), reads=[kkT], writes=[kkT])
                    vv = fw.sb([128, NT_EXT, 2, 65], BF16, f"vv{g}", esG)
                    gsig = fw.sb([128, NT_OWN, 12], F32, f"gsig{g}", esG)
                    kcmpT = fw.sb([128, 256], BF16, f"kcmpT{g}", esG)
                    op("pool", lambda e: e.memset(kcmpT[64:128, :], 0.0), writes=[kcmpT])
                    vca = fw.sb([128, 2, 65], BF16, f"vca{g}", esG)
                    op("pool", lambda e: e.memset(vv[:, :, :, 64:65], 1.0), writes=[vv])
                    op("pool", lambda e: e.memset(vca[:, :, 64:65], 1.0), writes=[vca])
                    ckpt("B0a")
                    with fw.scope() as esC:
                        ccT = fw.sb([64, 2, S_EXT], BF16, f"ccT{g}", esC)
                        with fw.scope() as esB1:
                            w_att = fw.sb([128, 8, 652], BF16, f"w_att{g}", esB1)
                            fw.dma(w_att[:], w_att_d[g].rearrange("(k p) c -> p k c", p=128), writes=[w_att], q="pool")
                            ckpt("B0b")
                            hblk = [fw.sb([128, 8, 512], BF16, f"hblkB{g}{i}", esB1) for i in range(2)]
                            rp = [fw.sb([128, 8, 64], BF16, f"rp{g}{i}", esB1) for i in range(2)]
                            rpf = [fw.sb([128, 8, 64], F32, f"rpf{g}{i}", esB1) for i in range(2)]
                            ta = [fw.sb([128, 7, 8], F32, f"ropa{g}{i}", esB1) for i in range(2)]
                            tb_ = [fw.sb([128, 7, 8], F32, f"ropb{g}{i}", esB1) for i in range(2)]
                            tcx = [fw.sb([128, 7, 8], F32, f"ropc{g}{i}", esB1) for i in range(2)]
                            tdx = [fw.sb([128, 7, 8], F32, f"ropd{g}{i}", esB1) for i in range(2)]
                            def b1_front(t):
                                own = t >= NT_OWN
                                tq = t - NT_OWN
                                hb = hblk[(t // 4) % 2]
                                if t % 4 == 0:
                                    fw.dma(hb[:], hT_d[:, :, t * 128:(t + 4) * 128], reads=hT_tiles[t:t + 4], writes=[hb])
                                tl = t % 4
                                a0 = 0 if own else 256
                                nb = 140 if own else 128
                                bA = 2 + t % 2
                                bB = 4 + t % 2
                                for k in range(8):
                                    mm(PS[bA], PS[bA][:, a0:512], hb[:, k, tl * 128:(tl + 1) * 128], w_att[:, k, a0:512], k == 0, k == 7, [hb, w_att])
                                for k in range(8):
                                    mm(PS[bB], PS[bB][:, 0:nb], hb[:, k, tl * 128:(tl + 1) * 128], w_att[:, k, 512:512 + nb], k == 0, k == 7, [hb, w_att])
                                rp_ = rp[t % 2]
                                h0 = a0 // 64
                                nh = 7 - h0
                                rf = rpf[t % 2]
                                op("act", lambda e: e.copy(out=rf[:, h0:8, :], in_=PS[bA][:, a0:512].rearrange("p (h d) -> p h d", d=64)),
                                   reads=[PS[bA]], writes=[rf])
                                op("pool", lambda e: e.tensor_copy(out=rp_[:, h0:8, :], in_=rf[:, h0:8, :]), reads=[rf], writes=[rp_])
                                t1 = rf[:, h0:7, 0:8]
                                t2 = rf[:, h0:7, 8:16]
                                Cb = cs[:, t, :].unsqueeze(1).to_broadcast([128, nh, 8])
                                Sb_ = sn[:, t, :].unsqueeze(1).to_broadcast([128, nh, 8])
                                ta_, tb2 = ta[t % 2], tb_[t % 2]
                                tc_, td_ = tcx[t % 2], tdx[t % 2]
                                op("dve", lambda e: e.tensor_tensor(out=ta_[:, 0:nh, :], in0=t1, in1=Cb, op=ALU.mult), reads=[rf, cs], writes=[ta_])
                                op("dve", lambda e: e.tensor_tensor(out=tb2[:, 0:nh, :], in0=t2, in1=Sb_, op=ALU.mult), reads=[rf, sn], writes=[tb2])
                                op("dve", lambda e: e.tensor_tensor(out=tc_[:, 0:nh, :], in0=t2, in1=Cb, op=ALU.mult), reads=[rf, cs], writes=[tc_])
                                op("dve", lambda e: e.tensor_tensor(out=td_[:, 0:nh, :], in0=t1, in1=Sb_, op=ALU.mult), reads=[rf, sn], writes=[td_])
                                op("dve", lambda e: e.tensor_tensor(out=rp_[:, h0:7, 0:8], in0=ta_[:, 0:nh, :], in1=tb2[:, 0:nh, :], op=ALU.subtract),
                                   reads=[ta_, tb2, rp_], writes=[rp_])
                                op("dve", lambda e: e.tensor_tensor(out=rp_[:, h0:7, 8:16], in0=tc_[:, 0:nh, :], in1=td_[:, 0:nh, :], op=ALU.add),
                                   reads=[tc_, td_, rp_], writes=[rp_])
                                op("dve", lambda e: e.tensor_copy(out=vv[:, t, :, 0:64], in_=PS[bB][:, 0:128].rearrange("p (h d) -> p h d", d=64)),
                                   reads=[PS[bB]], writes=[vv])
                                if own:
                                    op("act", lambda e: e.activation(out=gsig[:, tq, :], in_=PS[bB][:, 128:140], func=AF.Sigmoid),
                                       reads=[PS[bB]], writes=[gsig])

                            def b1_back(t):
                                own = t >= NT_OWN
                                tq = t - NT_OWN
                                rp_ = rp[t % 2]
                                h0 = 0 if own else 4
                                bT = t % 2
                                psT = psbf(bT)
                                for j, hh in enumerate(range(h0, 8)):
                                    op("pe", lambda e: e.transpose(out=psT[0:64, j * 128:(j + 1) * 128], in_=rp_[:, hh, :], identity=idb[:]),
                                       reads=[rp_, idb], writes=[PS[bT]])
                                if own:
                                    op("act", lambda e: e.copy(out=qT[0:64, :, tq * 128:(tq + 1) * 128], in_=psT[0:64, 0:512].rearrange("p (h t) -> p h t", h=4)),
                                       reads=[PS[bT]], writes=[qT])
                                    o1 = 512
                                else:
                                    o1 = 0
                                op("act", lambda e: e.copy(out=kkT[0:64, :, t * 128:(t + 1) * 128], in_=psT[0:64, o1:o1 + 256].rearrange("p (h t) -> p h t", h=2)),
                                   reads=[PS[bT]], writes=[kkT])
                                op("act", lambda e: e.copy(out=ccT[:, :, t * 128:(t + 1) * 128], in_=psT[0:64, o1 + 256:o1 + 512].rearrange("p (h t) -> p h t", h=2)),
                                   reads=[PS[bT]], writes=[ccT])

                            for t in range(NT_EXT + 1):
                                if t < NT_EXT:
                                    b1_front(t)
                                if t >= 1:
                                    b1_back(t - 1)
                        ckpt("B1")
                        for i in range(2):
                            with fw.scope() as esB2:
                                w1 = fw.sb([64, 32, 256], BF16, f"w1_{g}{i}", esB2)
                                fw.dma(w1[:], w_c1_d[i].rearrange("(l d) h -> d l h", d=64), writes=[w1], q="pool")
                                w2 = fw.sb([128, 2, 64], BF16, f"w2_{g}{i}", esB2)
                                fw.dma(w2[:], w_c2_d[i].rearrange("(c p) d -> p c d", p=128), writes=[w2], q="pool")
                                pe_sb = fw.sb([32, 64], BF16, f"pe_{g}{i}", esB2)
                                fw.dma(pe_sb[:], pe_c_d[i], writes=[pe_sb], q="pool")
                                peT = fw.sb([64, 32], BF16, f"peT_{g}{i}", esB2)
                                op("pe", lambda e: e.transpose(out=psbf(6)[0:64, 0:32], in_=pe_sb[:, :], identity=idb[0:32, 0:32]),
                                   reads=[pe_sb, idb], writes=[PS[6]])
                                op("act", lambda e: e.copy(out=peT[:], in_=psbf(6)[0:64, 0:32]), reads=[PS[6]], writes=[peT])
                                for hc in range(2):
                                    for l in range(32):
                                        mm(PS[7], PS[7][:, hc:hc + 1], w1[:, l, hc * 128:(hc + 1) * 128], peT[:, l:l + 1], l == 0, l == 31, [w1, peT])
                                cbs = fw.sb([128, 2], F32, f"cbs_{g}{i}", esB2)
                                op("act", lambda e: e.copy(out=cbs[:], in_=PS[7][:, 0:2]), reads=[PS[7]], writes=[cbs])
                                G = fw.sb([128, 2, 256], BF16, f"G_{g}{i}", esB2)
                                op("pool", lambda e: e.memset(G[:, :, 255:256], 0.0), writes=[G])
                                u_ = fw.sb([128, 255], F32, f"u_{g}{i}", esB2)
                                u2 = fw.sb([128, 255], F32, f"u2_{g}{i}", esB2)
                                sg_ = fw.sb([128, 255], F32, f"sg_{g}{i}", esB2)
                                for hc in range(2):
                                    for l in range(32):
                                        mm(PS[hc], PS[hc][:, 0:255], w1[:, l, hc * 128:(hc + 1) * 128], ccT[:, i, l:l + 16 * 254 + 1:16], l == 0, l == 31, [w1, ccT])
                                    op("act", lambda e: e.activation(out=u_[:], in_=PS[hc][:, 0:255], func=AF.Identity, bias=cbs[:, hc:hc + 1]),
                                       reads=[PS[hc], cbs], writes=[u_])
                                    op("dve", lambda e: e.tensor_tensor(out=u2[:], in0=u_[:], in1=u_[:], op=ALU.mult), reads=[u_], writes=[u2])
                                    op("dve", lambda e: e.tensor_scalar(out=u2[:], in0=u2[:], scalar1=0.044715, scalar2=1.0, op0=ALU.mult, op1=ALU.add),
                                       reads=[u2], writes=[u2])
                                    op("dve", lambda e: e.tensor_tensor(out=u2[:], in0=u2[:], in1=u_[:], op=ALU.mult), reads=[u2, u_], writes=[u2])
                                    op("act", lambda e: e.activation(out=sg_[:], in_=u2[:], func=AF.Sigmoid, scale=1.5957691216057308),
                                       reads=[u2], writes=[sg_])
                                    op("dve", lambda e: e.tensor_tensor(out=G[:, hc, 0:255], in0=u_[:], in1=sg_[:], op=ALU.mult), reads=[u_, sg_], writes=[G])
                                if i == 0:
                                    for hc in range(2):
                                        mm(PS[6], PS[6][0:64, 0:256], w2[:, hc, :], G[:, hc, :], hc == 0, hc == 1, [w2, G])
                                    op("act", lambda e: e.copy(out=kcmpT[0:64, :], in_=PS[6][0:64, 0:256]), reads=[PS[6]], writes=[kcmpT])
                                else:
                                    for nch in range(2):
                                        for hc in range(2):
                                            mm(PS[6], PS[6][:, nch * 64:(nch + 1) * 64], G[:, hc, nch * 128:(nch + 1) * 128], w2[:, hc, :], hc == 0, hc == 1, [w2, G])
                                    op("act", lambda e: e.copy(out=vca[:, :, 0:64], in_=PS[6][:, 0:128].rearrange("p (n d) -> p n d", d=64)),
                                       reads=[PS[6]], writes=[vca])
                    if g == 0 and "B2dump" in dbg:
                        for nm, bf, shp in (("kkT", kkT, [64, 2, S_EXT]), ("qT", qT, [64, 4, S_OWN]), ("vv", vv, [128, NT_EXT, 2, 65]),
                                            ("kcmpT", kcmpT, [64, 256]), ("vca", vca, [128, 2, 65])):
                            o = dbg_t(nm, shp, BF16)
                            fw.dma(o, bf[0:shp[0]], reads=[bf], is_output=True)
                        o = dbg_t("gsig", [128, NT_OWN, 12])
                        fw.dma(o, gsig[:], reads=[gsig], is_output=True)
                    ckpt("B2")
                    with fw.scope() as esB3:
                        NP = 4
                        LA = 2
                        Pb = [fw.sb([128, 512], BF16, f"Pb{g}{i}", esB3) for i in range(NP)]
                        Sbank = [0, 1, 6, 7]
                        tpsum = psbf(2)[:, 520:1024]
                        TPB = PS[2]
                        hbS = [fw.sb([128, 4, 132], F32, f"hbS{g}{r}", esB3) for r in range(3)]
                        ya = [fw.sb([128, 4, 64], F32, f"ya{g}{i}", esB3) for i in range(2)]
                        yat = [fw.sb([128, 4, 64], BF16, f"yat{g}{i}", esB3) for i in range(2)]
                        sms = [fw.sb([128, 16], F32, f"sm{g}{i}", esB3) for i in range(3)]
                        rdc = fw.sb([128, 4], F32, f"rdc{g}", esB3)
                        impv = fw.sb([128, 64], F32, f"impv{g}", esB3)
                        wk = fw.sb([128, 64], F32, f"wk{g}", esB3)
                        m8a = fw.sb([128, 8], F32, f"m8a{g}", esB3)
                        m8b = fw.sb([128, 8], F32, f"m8b{g}", esB3)
                        negm2 = fw.sb([128, 128], BF16, f"negm{g}", esB3)
                        op("pool", lambda e: e.memset(negm2[:, 0:64], 0.0), writes=[negm2])
                        rot = [0]
                        REG = {0: (0, 129), 1: (129, 65), 2: (194, 65)}

                        def score(c, lhsT, lreads, extra, bias, mask):
                            r = rot[0] % NP
                            rot[0] += 1
                            sb_i = Sbank[r]
                            P = Pb[r]
                            qrhs = qT[:, :, c * 128:(c + 1) * 128]
                            S3 = PS[sb_i][:, :].rearrange("p (h q) -> p h q", h=4)
                            mm(PS[sb_i], S3, lhsT, qrhs, True, True, lreads + [qT])
                            op("act", lambda e: e.activation(out=P[:], in_=PS[sb_i][:, :], func=AF.Exp, bias=bias[:], scale=0.125),
                               reads=[PS[sb_i], bias], writes=[P])
                            if mask is not None:
                                op("dve", lambda e: e.tensor_tensor(out=P[:].rearrange("p (h q) -> p h q", h=4), in0=P[:].rearrange("p (h q) -> p h q", h=4),
                                                                    in1=mask[0], op=ALU.mult), reads=[P, mask[1]], writes=[P])
                            return P

                        def pv(P, h, reg, vr, vreads, cc, n, first, last):
                            op("pe", lambda e: e.matmul(PS[2 + h][:, cc:cc + n], P[:, h * 128:(h + 1) * 128], vr, start=first, stop=last),
                               reads=[P] + vreads, writes=[PS[2 + h]])

                        def evac_all(c, reg, br, first, final, mid=None):
                            col0, n = REG[reg]
                            hs = hbS[reg]
                            for h in range(4):
                                op("dve", lambda e: e.tensor_copy(out=hs[:, h, 0:n], in_=PS[2 + h][:, col0:col0 + n]), reads=[PS[2 + h]], writes=[hs])
                            sm = sms[reg]
                            yac = ya[c % 2]
                            dn = sm[:, 0:4]
                            rd = sm[:, 4:8] if br != 0 else rdc[:, 0:4]
                            rdb = sm if br != 0 else rdc
                            cf = sm[:, 8:12]
                            op("dve", lambda e: e.tensor_scalar(out=dn.unsqueeze(2), in0=hs[:, :, 64:65], scalar1=1e-30, scalar2=None, op0=ALU.max),
                               reads=[hs], writes=[sm])
                            op("dve", lambda e: e.reciprocal(out=rd, in_=dn), reads=[sm], writes=[rdb])
                            if mid is not None:
                                mid()
                            op("dve", lambda e: e.tensor_tensor(out=cf.unsqueeze(2), in0=rd.unsqueeze(2),
                                                                in1=gsig[:, c, :].rearrange("p (h b) -> p h b", b=3)[:, :, br:br + 1], op=ALU.mult),
                               reads=[sm, rdb, gsig], writes=[sm])
                            cfb = cf.unsqueeze(2).to_broadcast([128, 4, 64])
                            if first:
                                op("dve", lambda e: e.tensor_tensor(out=yac[:], in0=hs[:, :, 0:64], in1=cfb, op=ALU.mult), reads=[hs, sm], writes=[yac])
                            else:
                                op("dve", lambda e: e.tensor_tensor(out=hs[:, :, 0:64], in0=hs[:, :, 0:64], in1=cfb, op=ALU.mult), reads=[hs, sm], writes=[hs])
                                dst = yat[c % 2] if final else yac
                                op("dve", lambda e: e.tensor_tensor(out=dst[:], in0=hs[:, :, 0:64], in1=yac[:], op=ALU.add), reads=[hs, yac], writes=[dst])

                        pend = []

                        def flush():
                            while pend:
                                pend.pop(0)()

                        def pipe(score_fn, pv_fn):
                            P = score_fn()
                            while len(pend) >= LA:
                                pend.pop(0)()
                            pend.append(lambda: pv_fn(P))

                        for c in range(NT_OWN):
                            Pc = []
                            for nch in range(2):
                                mk = cmask[:, nch, c * 128:(c + 1) * 128].unsqueeze(1).to_broadcast([128, 4, 128])
                                Pc.append(score(c, kcmpT[:, nch * 128:(nch + 1) * 128], [kcmpT], None, hbias if nch == 0 else c_zero, (mk, cmask)))
                            flush()
                            if c > 0:
                                cp = c - 1
                                evac_all(cp, 1, 1, False, True)
                                for j in range(2):
                                    op("pe", lambda e: e.transpose(out=tpsum[:, j * 128:(j + 1) * 128],
                                                                   in_=yat[cp % 2][:, 2 * j:2 * j + 2, :].rearrange("p h d -> p (h d)"), identity=idb[:]),
                                       reads=[yat[cp % 2], idb], writes=[TPB])
                                op("act", lambda e: e.copy(out=YT[:, 2 * g:2 * g + 2, cp * 128:(cp + 1) * 128],
                                                           in_=tpsum[:, 0:256].rearrange("p (j t) -> p j t", j=2)), reads=[TPB], writes=[YT])
                            if g == 0 and c == 1 and "B3dump" in dbg:
                                for r_ in range(3):
                                    fw.dma(dbg_t(f"hbS{r_}", [128, 4, 132]), hbS[r_][:], reads=[hbS[r_]], is_output=True)
                                fw.dma(dbg_t("yat0", [128, 4, 64], BF16), yat[0][:], reads=[yat[0]], is_output=True)
                                fw.dma(dbg_t("ya0", [128, 4, 64]), ya[0][:], reads=[ya[0]], is_output=True)
                                fw.dma(dbg_t("sm0", [128, 16]), sms[0][:], reads=[sms[0]], is_output=True)
                                fw.dma(dbg_t("sm1", [128, 16]), sms[1][:], reads=[sms[1]], is_output=True)
                                fw.dma(dbg_t("sm2", [128, 16]), sms[2][:], reads=[sms[2]], is_output=True)
                                fw.dma(dbg_t("rdc", [128, 4]), rdc[:], reads=[rdc], is_output=True)
                                ckpt("B3c0")
                            for h in range(4):
                                for nch in range(2):
                                    pv(Pc[nch], h, 0, vca[:, nch, :], [vca], 0, 65, nch == 0, nch == 1)
                                for nch in range(2):
                                    pv(Pc[nch], h, 0, ovl[:, nch, :], [ovl], 65, 64, nch == 0, nch == 1)
                            for j in range(5):
                                ch = NT_OWN + c - 4 + j
                                mk = None
                                if j == 0:
                                    mk = (wm0[:].unsqueeze(1).to_broadcast([128, 4, 128]), wm0)
                                elif j == 4:
                                    mk = (caus[:].unsqueeze(1).to_broadcast([128, 4, 128]), caus)

                                def sfn(ch=ch, mk=mk):
                                    return score(c, kkT[:, 1, ch * 128:(ch + 1) * 128], [kkT], None, hbias if ch < NT_OWN else c_zero, mk)

                                def pfn(P, ch=ch, j=j):
                                    for h in range(4):
                                        pv(P, h, 2, vv[:, ch, 1, :], [vv], 194, 65, j == 0, j == 4)
                                pipe(sfn, pfn)
                                if j == 0:
                                    def topk_chain(c=c):
                                        op("dve", lambda e: e.tensor_tensor(out=hbS[0][:, :, 65:129], in0=hbS[0][:, :, 65:129], in1=rdc[:, 0:4].unsqueeze(2).to_broadcast([128, 4, 64]), op=ALU.mult),
                                           reads=[hbS[0], rdc], writes=[hbS[0]])
                                        op("dve", lambda e: e.tensor_reduce(out=impv[:], in_=hbS[0][:, :, 65:129].rearrange("p h s -> p s h"), axis=AX.X, op=ALU.add),
                                           reads=[hbS[0]], writes=[impv])
                                        op("dve", lambda e: e.tensor_tensor(out=impv[:], in0=impv[:], in1=maskadd[:, c, :], op=ALU.add), reads=[impv, maskadd], writes=[impv])
                                        op("dve", lambda e: e.max(out=m8a[:], in_=impv[:]), reads=[impv], writes=[m8a])
                                        op("dve", lambda e: e.match_replace(out=wk[:], in_to_replace=m8a[:], in_values=impv[:], imm_value=-3.0e38),
                                           reads=[impv, m8a], writes=[wk])
                                        op("dve", lambda e: e.max(out=m8b[:], in_=wk[:]), reads=[wk], writes=[m8b])
                                        op("dve", lambda e: e.tensor_scalar(out=negm2[:, 64:128], in0=impv[:], scalar1=m8b[:, 7:8], scalar2=NEGB, op0=ALU.is_lt, op1=ALU.mult),
                                           reads=[impv, m8b, negm2], writes=[negm2])
                                    evac_all(c, 0, 0, True, False, mid=topk_chain)
                            flush()
                            evac_all(c, 2, 2, False, False)
                            op("pe", lambda e: e.transpose(out=tpsum[:, 256:384], in_=negm2[:, :], identity=idb[:]), reads=[negm2, idb], writes=[TPB])
                            op("act", lambda e: e.copy(out=qT[64:128, :, c * 128:(c + 1) * 128], in_=tpsum[64:128, 256:384].unsqueeze(1).to_broadcast([64, 4, 128])),
                               reads=[TPB], writes=[qT])
                            chs = list(range(NT_OWN)) + [NT_OWN + j for j in range(c + 1)]
                            for i, ch in enumerate(chs):
                                mk = None
                                if ch == NT_OWN + c:
                                    mk = (caus[:].unsqueeze(1).to_broadcast([128, 4, 128]), caus)

                                def sfn(ch=ch, mk=mk):
                                    return score(c, kkT[:, 0, ch * 128:(ch + 1) * 128], [kkT], None, hbias if ch < NT_OWN else c_zero, mk)

                                def pfn(P, ch=ch, i=i, n=len(chs)):
                                    for h in range(4):
                                        pv(P, h, 1, vv[:, ch, 0, :], [vv], 129, 65, i == 0, i == n - 1)
                                pipe(sfn, pfn)
                        flush()
                        cp = NT_OWN - 1
                        evac_all(cp, 1, 1, False, True)
                        for j in range(2):
                            op("pe", lambda e: e.transpose(out=tpsum[:, j * 128:(j + 1) * 128],
                                                           in_=yat[cp % 2][:, 2 * j:2 * j + 2, :].rearrange("p h d -> p (h d)"), identity=idb[:]),
                               reads=[yat[cp % 2], idb], writes=[TPB])
                        op("act", lambda e: e.copy(out=YT[:, 2 * g:2 * g + 2, cp * 128:(cp + 1) * 128],
                                                   in_=tpsum[:, 0:256].rearrange("p (j t) -> p j t", j=2)), reads=[TPB], writes=[YT])
            esBc.__exit__(None, None, None)
            ckpt("B")
            with fw.scope() as esCg:
                ee = fw.sb([128, NT_EXT, 4], F32, "ee", esCg)
                ff = fw.sb([128, NT_EXT, 4], F32, "ff", esCg)
                fl = fw.sb([128, NT_EXT, 4], F32, "fl", esCg)
                ghn = fw.sb([128, 512], F32, "ghn", esCg)
                fw.dma(ghn[:], g_hn_d[0:1, :].to_broadcast([128, 512]), writes=[ghn])
                wcs = fw.sb([128, 8, 4], F32, "wcs", esCg)
                fw.dma(wcs[:], wc_d[:, :, :], writes=[wcs])
                bcs = fw.sb([128, 8], F32, "bcs", esCg)
                fw.dma(bcs[:], bc_d[:, :], writes=[bcs])
                with fw.scope() as esg:
                    w_if = fw.sb([128, 8, 8], BF16, "w_if", esg)
                    fw.dma(w_if[:], w_if_d.rearrange("(k p) c -> p k c", p=128), writes=[w_if], q="pool")
                    bif = fw.sb([128, 8], F32, "bif", esg)
                    fw.dma(bif[:], b_if_d[0:1, :].to_broadcast([128, 8]), writes=[bif])
                    hblk = [fw.sb([128, 8, 512], BF16, f"hblkG{i}", esg) for i in range(2)]
                    ifp = fw.sb([128, NT_EXT, 8], F32, "ifp", esg)
                    l1 = fw.sb([128, NT_EXT, 4], F32, "l1", esg)
                    tmpg = fw.sb([128, NT_EXT, 4], F32, "tmpg", esg)
                    for t in range(NT_EXT):
                        hb = hblk[(t // 4) % 2]
                        if t % 4 == 0:
                            fw.dma(hb[:], hT_d[:, :, t * 128:(t + 4) * 128], reads=hT_tiles[t:t + 4], writes=[hb])
                        tl = t % 4
                        for k in range(8):
                            mm(PS[0], PS[0][:, t * 8:(t + 1) * 8], hb[:, k, tl * 128:(tl + 1) * 128], w_if[:, k, :], k == 0, k == 7, [hb, w_if])
                    op("act", lambda e: e.copy(out=ifp[:], in_=PS[0][:, 0:256].rearrange("p (t c) -> p t c", c=8)), reads=[PS[0]], writes=[ifp])
                    op("dve", lambda e: e.tensor_tensor(out=ifp[:], in0=ifp[:], in1=bif[:].unsqueeze(1).to_broadcast([128, NT_EXT, 8]), op=ALU.add),
                       reads=[ifp, bif], writes=[ifp])
                    op("act", lambda e: e.activation(out=l1[:], in_=ifp[:, :, 4:8], func=AF.Exp, scale=-1.0), reads=[ifp], writes=[l1])
                    op("act", lambda e: e.activation(out=l1[:], in_=l1[:], func=AF.Ln, bias=c_one[:]), reads=[l1, c_one], writes=[l1])
                    l1f = l1[:].rearrange("p t c -> p (t c)")
                    mm(PS[1], PS[1][:, 0:128], U_f[:], l1f, True, True, [U_f, l1])
                    mm(PS[1], PS[1][:, 128:256], ones_f[:], l1f, True, True, [ones_f, l1])
                    op("act", lambda e: e.copy(out=tmpg[:], in_=PS[1][:, 0:128].rearrange("p (t c) -> p t c", c=4)), reads=[PS[1]], writes=[tmpg])
                    op("act", lambda e: e.activation(out=ff[:], in_=tmpg[:], func=AF.Exp, scale=-1.0), reads=[tmpg], writes=[ff])
                    op("act", lambda e: e.activation(out=fl[:], in_=PS[1][:, 128:256].rearrange("p (t c) -> p t c", c=4), func=AF.Exp, scale=-1.0),
                       reads=[PS[1]], writes=[fl])
                    op("dve", lambda e: e.tensor_tensor(out=tmpg[:], in0=tmpg[:], in1=ifp[:, :, 0:4], op=ALU.add), reads=[tmpg, ifp], writes=[tmpg])
                    op("act", lambda e: e.activation(out=ee[:], in_=tmpg[:], func=AF.Exp), reads=[tmpg], writes=[ee])
                    op("dve", lambda e: e.tensor_scalar(out=ee[:, 0:NT_OWN, :], in0=ee[:, 0:NT_OWN, :], scalar1=hv[:, 0:1], scalar2=None, op0=ALU.mult),
                       reads=[ee, hv], writes=[ee])
                ckpt("Cg")
                qTb = fw.sb([128, 4, S_OWN], BF16, "qTb", esCg)
                kTb = fw.sb([128, 4, S_EXT], BF16, "kTb", esCg)
                vaug = fw.sb([128, NT_EXT, 4, 129], BF16, "vaug", esCg)
                osig = fw.sb([128, NT_OWN, 512], BF16, "osig", esCg)
                op("pool", lambda e: e.memset(vaug[:, :, :, 128:129], 1.0), writes=[vaug])
                for hp in range(2):
                    with fw.scope() as esC1:
                        wq = fw.sb([128, 8, 256], BF16, f"wq{hp}", esC1)
                        wk = fw.sb([128, 8, 256], BF16, f"wk{hp}", esC1)
                        wv = fw.sb([128, 8, 256], BF16, f"wv{hp}", esC1)
                        wo = fw.sb([128, 8, 256], BF16, f"wo{hp}", esC1)
                        fw.dma(wq[:], w_qk_d[:, hp * 256:(hp + 1) * 256].rearrange("(k p) c -> p k c", p=128), writes=[wq], q="pool")
                        fw.dma(wk[:], w_qk_d[:, 512 + hp * 256:512 + (hp + 1) * 256].rearrange("(k p) c -> p k c", p=128), writes=[wk], q="pool")
                        fw.dma(wv[:], w_vo_d[:, hp * 256:(hp + 1) * 256].rearrange("(k p) c -> p k c", p=128), writes=[wv], q="pool")
                        fw.dma(wo[:], w_vo_d[:, 512 + hp * 256:512 + (hp + 1) * 256].rearrange("(k p) c -> p k c", p=128), writes=[wo], q="pool")
                        hblk = [fw.sb([128, 8, 512], BF16, f"hblkC{hp}{i}", esC1) for i in range(2)]
                        uk = [fw.sb([128, 4 + S_EXT], BF16, f"uk{hp}{i}", esC1) for i in range(2)]
                        uq = [fw.sb([128, 4 + 2560], BF16, f"uq{hp}{i}", esC1) for i in range(2)]
                        ycv = [fw.sb([128, 512], F32, f"ycv{hp}{i}", esC1) for i in range(2)]
                        sgm = [fw.sb([128, 512], F32, f"sgm{hp}{i}", esC1) for i in range(2)]
                        for hh in range(2):
                            op("pool", lambda e: e.memset(uk[hh][:, 0:4], 0.0), writes=[uk[hh]])
                            op("pool", lambda e: e.memset(uq[hh][:, 0:4], 0.0), writes=[uq[hh]])
                        for blk in range(8):
                            hb = hblk[blk % 2]
                            fw.dma(hb[:], hT_d[:, :, blk * 512:(blk + 1) * 512], reads=hT_tiles[4 * blk:4 * blk + 4], writes=[hb])
                            for hh in range(2):
                                for k in range(8):
                                    mm(PS[hh], PS[hh][:, :], wk[:, k, hh * 128:(hh + 1) * 128], hb[:, k, :], k == 0, k == 7, [wk, hb])
                                op("act", lambda e: e.copy(out=uk[hh][:, 4 + blk * 512:4 + (blk + 1) * 512], in_=PS[hh][:, :]), reads=[PS[hh]], writes=[uk[hh]])
                            if blk >= 3:
                                for hh in range(2):
                                    for k in range(8):
                                        mm(PS[2 + hh], PS[2 + hh][:, :], wq[:, k, hh * 128:(hh + 1) * 128], hb[:, k, :], k == 0, k == 7, [wq, hb])
                                    op("act", lambda e: e.copy(out=uq[hh][:, 4 + (blk - 3) * 512:4 + (blk - 2) * 512], in_=PS[2 + hh][:, :]),
                                       reads=[PS[2 + hh]], writes=[uq[hh]])
                            for tl in range(4):
                                t = blk * 4 + tl
                                bv = 4 + tl % 2
                                for k in range(8):
                                    mm(PS[bv], PS[bv][:, 0:256], hb[:, k, tl * 128:(tl + 1) * 128], wv[:, k, :], k == 0, k == 7, [wv, hb])
                                op("dve", lambda e: e.tensor_copy(out=vaug[:, t, 2 * hp:2 * hp + 2, 0:128], in_=PS[bv][:, 0:256].rearrange("p (h d) -> p h d", d=128)),
                                   reads=[PS[bv]], writes=[vaug])
                                if blk >= 4:
                                    bo = 6 + tl % 2
                                    for k in range(8):
                                        mm(PS[bo], PS[bo][:, 0:256], hb[:, k, tl * 128:(tl + 1) * 128], wo[:, k, :], k == 0, k == 7, [wo, hb])
                                    op("act", lambda e: e.activation(out=osig[:, t - NT_OWN, hp * 256:(hp + 1) * 256], in_=PS[bo][:, 0:256], func=AF.Sigmoid),
                                       reads=[PS[bo]], writes=[osig])
                        pi = 0
                        for hh in range(2):
                            H = 2 * hp + hh
                            for typ in range(2):
                                ci = typ * 4 + H
                                npiece = 4 if typ == 0 else 8
                                u = uq[hh] if typ == 0 else uk[hh]
                                for pc in range(npiece):
                                    off = (4 + 512 + pc * 512) if typ == 0 else (4 + pc * 512)
                                    y_ = ycv[pi % 2]
                                    s_ = sgm[pi % 2]
                                    pi += 1
                                    op("dve", lambda e: e.tensor_scalar(out=y_[:], in0=u[:, off - 3:off - 3 + 512], scalar1=wcs[:, ci, 0:1], scalar2=bcs[:, ci:ci + 1],
                                                                        op0=ALU.mult, op1=ALU.add), reads=[u, wcs, bcs], writes=[y_])
                                    for j in range(1, 4):
                                        op("dve", lambda e: e.scalar_tensor_tensor(out=y_[:], in0=u[:, off - 3 + j:off - 3 + j + 512], scalar=wcs[:, ci, j:j + 1], in1=y_[:],
                                                                                   op0=ALU.mult, op1=ALU.add), reads=[u, wcs, y_], writes=[y_])
                                    if typ == 0:
                                        op("act", lambda e: e.activation(out=qTb[:, 2 * hp + hh, pc * 512:(pc + 1) * 512], in_=y_[:], func=AF.Silu), reads=[y_], writes=[qTb])
                                    else:
                                        op("act", lambda e: e.activation(out=s_[:], in_=y_[:], func=AF.Sigmoid), reads=[y_], writes=[s_])
                                        op("dve", lambda e: e.scalar_tensor_tensor(out=kTb[:, 2 * hp + hh, pc * 512:(pc + 1) * 512], in0=y_[:], scalar=128.0 ** -0.5, in1=s_[:],
                                                                                   op0=ALU.mult, op1=ALU.mult), reads=[y_, s_], writes=[kTb])
                ckpt("C1")
                with fw.scope() as esC3:
                    ktokR = [fw.sb([128, 4, 128], BF16, f"ktokR{i}", esC3) for i in range(3)]
                    CTall = fw.sb([128, NT_OWN, 4, 129], BF16, "CTall", esC3)
                    Xs = [fw.sb([128, 129], F32, f"Xs{H}", esC3) for H in range(4)]
                    Sm = [[fw.sb([128, 128], BF16, f"Sm{H}{i}", esC3) for i in range(2)] for H in range(4)]
                    hm_ = [fw.sb([128, 128], F32, f"hm{H}", esC3) for H in range(4)]
                    yb_ = [fw.sb([128, 128], BF16, f"yb{H}", esC3) for H in range(4)]
                    jk = [fw.sb([128, 128], BF16, f"jk{H}", esC3) for H in range(4)]
                    smc = [fw.sb([128, 8], F32, f"smc{H}", esC3) for H in range(4)]
                    for H in range(4):
                        op("dve", lambda e: e.tensor_tensor(out=vaug[:, :, H, :], in0=vaug[:, :, H, :],
                                                            in1=ee[:, :, H:H + 1].to_broadcast([128, NT_EXT, 129]), op=ALU.mult), reads=[vaug, ee], writes=[vaug])

                    def k_tr(t):
                        bk = t % 2
                        for H in range(4):
                            op("pe", lambda e: e.transpose(out=psbf(bk)[:, H * 128:(H + 1) * 128], in_=kTb[:, H, t * 128:(t + 1) * 128], identity=idb[:]),
                               reads=[kTb, idb], writes=[PS[bk]])
                        op("act", lambda e: e.copy(out=ktokR[t % 3][:], in_=psbf(bk)[:, 0:512].rearrange("p (h d) -> p h d", d=128)), reads=[PS[bk]], writes=[ktokR[t % 3]])

                    k_tr(0)
                    for t in range(NT_EXT - 1):
                        if t + 1 < NT_EXT - 1:
                            k_tr(t + 1)
                        for H in range(4):
                            bU = 2 + H
                            mm(PS[bU], PS[bU][:, 0:129], ktokR[t % 3][:, H, :], vaug[:, t, H, :], True, True, [ktokR[t % 3], vaug])
                            if t == 0:
                                op("dve", lambda e: e.tensor_copy(out=Xs[H][:], in_=PS[bU][:, 0:129]), reads=[PS[bU]], writes=[Xs[H]])
                            else:
                                op("dve", lambda e: e.scalar_tensor_tensor(out=Xs[H][:], in0=Xs[H][:], scalar=fl[:, t - 1, H:H + 1], in1=PS[bU][:, 0:129],
                                                                           op0=ALU.mult, op1=ALU.add), reads=[Xs[H], fl, PS[bU]], writes=[Xs[H]])
                            if t + 1 >= NT_OWN:
                                op("act", lambda e: e.activation(out=CTall[:, t + 1 - NT_OWN, H, :], in_=Xs[H][:], func=AF.Copy, scale=fl[:, t, H:H + 1]),
                                   reads=[Xs[H], fl], writes=[CTall])
                    sc4 = fw.sb([128, 4, 8], F32, "sc4", esC3)

                    def stA(t):
                        tq = t - NT_OWN
                        for H in range(4):
                            sm_ = Sm[H][tq % 2]
                            mm(PS[H], PS[H][:, 0:128], kTb[:, H, t * 128:(t + 1) * 128], qTb[:, H, tq * 128:(tq + 1) * 128], True, True, [kTb, qTb])
                            op("dve", lambda e: e.tensor_tensor(out=sm_[:], in0=PS[H][:, 0:128], in1=caus[:], op=ALU.mult), reads=[PS[H], caus], writes=[sm_])

                    def stRest(t):
                        tq = t - NT_OWN
                        for H in range(4):
                            sm_ = Sm[H][tq % 2]
                            bA = 4 + H
                            mm(PS[bA], PS[bA][:, 0:129], sm_[:], vaug[:, t, H, :], True, False, [sm_, vaug])
                            mm(PS[bA], PS[bA][:, 0:129], qTb[:, H, tq * 128:(tq + 1) * 128], CTall[:, tq, H, :], False, True, [qTb, CTall])
                        for H in range(4):
                            op("act", lambda e: e.activation(out=sc4[:, H, 6:7], in_=PS[4 + H][:, 128:129], func=AF.Abs, scale=ff[:, t, H:H + 1]),
                               reads=[PS[4 + H], ff], writes=[sc4])
                        op("dve", lambda e: e.tensor_scalar(out=sc4[:, :, 0:1], in0=sc4[:, :, 6:7], scalar1=1.0, scalar2=None, op0=ALU.max), reads=[sc4], writes=[sc4])
                        op("dve", lambda e: e.reciprocal(out=sc4[:, :, 1:2], in_=sc4[:, :, 0:1]), reads=[sc4], writes=[sc4])
                        op("dve", lambda e: e.tensor_tensor(out=sc4[:, :, 2:3], in0=sc4[:, :, 1:2], in1=ff[:, t, :].unsqueeze(2), op=ALU.mult), reads=[sc4, ff], writes=[sc4])
                        for H in range(4):
                            op("dve", lambda e: e.scalar_tensor_tensor(out=hm_[H][:], in0=PS[4 + H][:, 0:128], scalar=sc4[:, H, 2:3], in1=osig[:, tq, H * 128:(H + 1) * 128],
                                                                       op0=ALU.mult, op1=ALU.mult), reads=[PS[4 + H], sc4, osig], writes=[hm_[H]])
                        for H in range(4):
                            op("act", lambda e: e.activation(out=jk[H][:], in_=hm_[H][:], func=AF.Square, accum_out=sc4[:, H, 3:4]), reads=[hm_[H]], writes=[jk[H], sc4])
                        op("act", lambda e: e.activation(out=sc4[:, :, 4:5], in_=sc4[:, :, 3:4], func=AF.Sqrt, bias=c_eps[:], scale=1.0 / 128), reads=[sc4, c_eps], writes=[sc4])
                        op("dve", lambda e: e.reciprocal(out=sc4[:, :, 5:6], in_=sc4[:, :, 4:5]), reads=[sc4], writes=[sc4])
                        for H in range(4):
                            op("dve", lambda e: e.scalar_tensor_tensor(out=yb_[H][:], in0=hm_[H][:], scalar=sc4[:, H, 5:6], in1=ghn[:, H * 128:(H + 1) * 128],
                                                                       op0=ALU.mult, op1=ALU.mult), reads=[hm_[H], sc4, ghn], writes=[yb_[H]])
                        for H in range(4):
                            op("pe", lambda e: e.transpose(out=psbf(4 + H)[:, 512:640], in_=yb_[H][:], identity=idb[:]), reads=[yb_[H], idb], writes=[PS[4 + H]])
                        for H in range(4):
                            op("act", lambda e: e.copy(out=YT[:, 4 + H, tq * 128:(tq + 1) * 128], in_=psbf(4 + H)[:, 512:640]), reads=[PS[4 + H]], writes=[YT])

                    stA(NT_OWN)
                    for t in range(NT_OWN, NT_EXT):
                        if t + 1 < NT_EXT:
                            stA(t + 1)
                        stRest(t)
            ckpt("C")
            if "ybT" in dbg:
                o = dbg_t("ybT", [128, 4, S_OWN], BF16)
                fw.dma(o[:, :, :], YT[:, 4:8, :], reads=[YT], is_output=True)


            with fw.scope() as esD:
                x1 = fw.sb([128, NT_OWN, D], F32, "x1", esD)
                with fw.scope() as esD1:
                    mixT = fw.sb([128, 8, S_OWN], BF16, "mixT", esD1)
                    with fw.scope() as esD1a:
                        hTo = fw.sb([128, 8, S_OWN], BF16, "hTo", esD1a)
                        for tb in range(4):
                            fw.dma(hTo[:, :, tb * 512:(tb + 1) * 512], hT_d[:, :, S_OWN + tb * 512:S_OWN + (tb + 1) * 512],
                                   reads=hT_tiles[NT_OWN + 4 * tb:NT_OWN + 4 * tb + 4], writes=[hTo])
                        wga = [fw.sb([128, 8, 128], BF16, f"wga{i}", esD1a) for i in range(2)]
                        wgb = [fw.sb([128, 8, 128], BF16, f"wgb{i}", esD1a) for i in range(2)]
                        wpa = [fw.sb([128, 4, 128], BF16, f"wpa{i}", esD1a) for i in range(2)]
                        wpb = [fw.sb([128, 4, 128], BF16, f"wpb{i}", esD1a) for i in range(2)]
                        sga = [fw.sb([128, 512], BF16, f"sga{i}", esD1a) for i in range(2)]
                        sgb = [fw.sb([128, 512], BF16, f"sgb{i}", esD1a) for i in range(2)]
                        t1 = [fw.sb([128, 512], F32, f"t1_{i}", esD1a) for i in range(2)]
                        t2 = [fw.sb([128, 512], F32, f"t2_{i}", esD1a) for i in range(2)]
                        it = 0
                        for j in range(8):
                            w_ = j % 2
                            fw.dma(wga[w_][:], w_mg_d[:, j * 128:(j + 1) * 128].rearrange("(k p) c -> p k c", p=128), writes=[wga[w_]], q="pool")
                            fw.dma(wgb[w_][:], w_mg_d[:, 1024 + j * 128:1024 + (j + 1) * 128].rearrange("(k p) c -> p k c", p=128), writes=[wgb[w_]], q="pool")
                            fw.dma(wpa[w_][:], w_pa_d[:, j * 128:(j + 1) * 128].rearrange("(k p) c -> p k c", p=128), writes=[wpa[w_]], q="pool")
                            fw.dma(wpb[w_][:], w_pb_d[:, j * 128:(j + 1) * 128].rearrange("(k p) c -> p k c", p=128), writes=[wpb[w_]], q="pool")
                            for tb in range(4):
                                r = it % 2
                                it += 1
                                b0 = 4 * r
                                ts_ = slice(tb * 512, (tb + 1) * 512)
                                for k in range(8):
                                    mm(PS[b0], PS[b0][:, :], wga[w_][:, k, :], hTo[:, k, ts_], k == 0, k == 7, [wga[w_], hTo])
                                op("act", lambda e: e.activation(out=sga[r][:], in_=PS[b0][:, :], func=AF.Sigmoid), reads=[PS[b0]], writes=[sga[r]])
                                for k in range(8):
                                    mm(PS[b0 + 1], PS[b0 + 1][:, :], wgb[w_][:, k, :], hTo[:, k, ts_], k == 0, k == 7, [wgb[w_], hTo])
                                op("act", lambda e: e.activation(out=sgb[r][:], in_=PS[b0 + 1][:, :], func=AF.Sigmoid), reads=[PS[b0 + 1]], writes=[sgb[r]])
                                for k in range(4):
                                    mm(PS[b0 + 2], PS[b0 + 2][:, :], wpa[w_][:, k, :], YT[:, k, ts_], k == 0, k == 3, [wpa[w_], YT])
                                for k in range(4):
                                    mm(PS[b0 + 3], PS[b0 + 3][:, :], wpb[w_][:, k, :], YT[:, 4 + k, ts_], k == 0, k == 3, [wpb[w_], YT])
                                op("dve", lambda e: e.tensor_tensor(out=t1[r][:], in0=PS[b0 + 2][:, :], in1=sga[r][:], op=ALU.mult), reads=[PS[b0 + 2], sga[r]], writes=[t1[r]])
                                op("dve", lambda e: e.tensor_tensor(out=t2[r][:], in0=PS[b0 + 3][:, :], in1=sgb[r][:], op=ALU.mult), reads=[PS[b0 + 3], sgb[r]], writes=[t2[r]])
                                op("pool", lambda e: e.tensor_tensor(out=mixT[:, j, ts_], in0=t1[r][:], in1=t2[r][:], op=ALU.add), reads=[t1[r], t2[r]], writes=[mixT])
                    ckpt("D1a")
                    with fw.scope() as esD1b:
                        w_out = fw.sb([128, 8, D], BF16, "w_out", esD1b)
                        fw.dma(w_out[:], w_out_d.rearrange("(k p) c -> p k c", p=128), writes=[w_out], q="pool")
                        xtl = [fw.sb([128, D], F32, f"xtl{i}", esD1b) for i in range(2)]
                        for t in range(NT_OWN):
                            x_ = xtl[t % 2]
                            fw.dma(x_[:], xe[S_OWN + t * 128:S_OWN + (t + 1) * 128, :], writes=[x_])
                            for half in range(2):
                                b = 2 * (t % 2) + half
                                for j in range(8):
                                    mm(PS[b], PS[b][:, :], mixT[:, j, t * 128:(t + 1) * 128], w_out[:, j, half * 512:(half + 1) * 512], j == 0, j == 7, [mixT, w_out])
                                op("dve", lambda e: e.tensor_tensor(out=x1[:, t, half * 512:(half + 1) * 512], in0=PS[b][:, :], in1=x_[:, half * 512:(half + 1) * 512], op=ALU.add),
                                   reads=[PS[b], x_], writes=[x1])
                ckpt("D1")
                if "x1" in dbg:
                    fw.dma(dbg_t("x1", [128, NT_OWN, D]), x1[:], reads=[x1], is_output=True)
                with fw.scope() as esM:
                    load_gain(1)
                    gateT = fw.sb([16, S_OWN], BF16, "gateT", esM)
                    E16 = fw.sb([16, 16, 128], BF16, "E16", esM)
                    op("pool", lambda e: e.memset(E16[:], 1.0), writes=[E16])
                    op("pool", lambda e: e.affine_select(out=E16[:], in_=E16[:], pattern=[[-1, 16], [0, 128]], compare_op=ALU.is_equal, fill=0.0,
                                                         base=0, channel_multiplier=1), reads=[E16], writes=[E16])
                    with fw.scope() as esR:
                        w_r = fw.sb([128, 8, 20], F32, "w_r", esR)
                        fw.dma(w_r[:], w_r_d.rearrange("(k p) c -> p k c", p=128), writes=[w_r])
                        b_r = fw.sb([128, 20], F32, "b_r", esR)
                        fw.dma(b_r[:], b_r_d[0:1, :].to_broadcast([128, 20]), writes=[b_r])
                        hnf = [fw.sb([128, D], F32, f"hnf{i}", esR) for i in range(2)]
                        hnTf = [fw.sb([128, 8, 128], F32, f"hnTf{i}", esR) for i in range(2)]
                        junkR = fw.sb([128, D], BF16, "junkR", esR)
                        ssr = [fw.sb([128, 1], F32, f"ssr{i}", esR) for i in range(2)]
                        rrr = [fw.sb([128, 1], F32, f"rrr{i}", esR) for i in range(2)]
                        T_ = NT_OWN
                        lgA = fw.sb([128, T_, 20], F32, "lgA", esR)

                        def r_front(t):
                            r = t % 2
                            rs = {"ss": ssr[r], "r": rrr[r]}
                            rms_rstd({"ap": x1[:, t, :], "bufs": [x1]}, rs, D, {"ap": junkR[:], "buf": junkR})
                            op("dve", lambda e: e.scalar_tensor_tensor(out=hnf[r][:], in0=x1[:, t, :], scalar=rs["r"][:], in1=gB[:], op0=ALU.mult, op1=ALU.mult),
                               reads=[x1, rs["r"], gB], writes=[hnf[r]])
                            for k in range(8):
                                b = 2 * r + (0 if k < 4 else 1)
                                op("pe", lambda e: e.transpose(out=PS[b][:, (k % 4) * 128:(k % 4 + 1) * 128], in_=hnf[r][:, k * 128:(k + 1) * 128], identity=idf[:]),
                                   reads=[hnf[r], idf], writes=[PS[b]])
                            for bb in range(2):
                                b = 2 * r + bb
                                op("act", lambda e: e.copy(out=hnTf[r][:, 4 * bb:4 * bb + 4, :], in_=PS[b][:, :].rearrange("p (k t) -> p k t", k=4)), reads=[PS[b]], writes=[hnTf[r]])
                                op("dve", lambda e: e.tensor_copy(out=YT[:, 4 * bb:4 * bb + 4, t * 128:(t + 1) * 128], in_=PS[b][:, :].rearrange("p (k t) -> p k t", k=4)),
                                   reads=[PS[b]], writes=[YT])

                        def r_back(t):
                            r = t % 2
                            bl = 4 + r
                            for k in range(8):
                                mm(PS[bl], PS[bl][:, 0:20], hnTf[r][:, k, :], w_r[:, k, :], k == 0, k == 7, [hnTf[r], w_r])
                            op("dve", lambda e: e.tensor_tensor(out=lgA[:, t, :], in0=PS[bl][:, 0:20], in1=b_r[:], op=ALU.add), reads=[PS[bl], b_r], writes=[lgA])

                        for t in range(T_ + 1):
                            if t < T_:
                                r_front(t)
                            if t >= 1:
                                r_back(t - 1)
                        gl = lgA[:, :, 0:4]
                        el = lgA[:, :, 4:20].rearrange("p t (g e) -> p t g e", g=4)
                        gmax = fw.sb([128, T_], F32, "gmax", esR)
                        g1h = fw.sb([128, T_, 4], F32, "g1h", esR)
                        exg = fw.sb([128, T_, 4], F32, "exg", esR)
                        pgs = fw.sb([128, T_], F32, "pgs", esR)
                        t16 = fw.sb([128, T_, 4, 4], F32, "t16", esR)
                        elg = fw.sb([128, T_, 4], F32, "elg", esR)
                        elg2 = fw.sb([128, T_, 4], F32, "elg2", esR)
                        ev1 = fw.sb([128, T_], F32, "ev1", esR)
                        ev2 = fw.sb([128, T_], F32, "ev2", esR)
                        mk1 = fw.sb([128, T_, 4], F32, "mk1", esR)
                        mk2 = fw.sb([128, T_, 4], F32, "mk2", esR)
                        w12 = fw.sb([128, 2, T_], F32, "w12", esR)
                        gig = fw.sb([128, T_, 4], F32, "gig", esR)
                        gate = fw.sb([128, T_, 4, 4], F32, "gate", esR)
                        B3 = [128, T_, 4]
                        op("dve", lambda e: e.tensor_reduce(out=gmax[:], in_=gl, axis=AX.X, op=ALU.max), reads=[lgA], writes=[gmax])
                        op("dve", lambda e: e.tensor_tensor(out=g1h[:], in0=gl, in1=gmax[:].unsqueeze(2).to_broadcast(B3), op=ALU.is_equal), reads=[lgA, gmax], writes=[g1h])
                        op("dve", lambda e: e.tensor_tensor(out=exg[:], in0=gl, in1=gmax[:].unsqueeze(2).to_broadcast(B3), op=ALU.subtract), reads=[lgA, gmax], writes=[exg])
                        op("act", lambda e: e.activation(out=exg[:], in_=exg[:], func=AF.Exp), reads=[exg], writes=[exg])
                        op("dve", lambda e: e.tensor_reduce(out=pgs[:], in_=exg[:], axis=AX.X, op=ALU.add), reads=[exg], writes=[pgs])
                        op("dve", lambda e: e.reciprocal(out=pgs[:], in_=pgs[:]), reads=[pgs], writes=[pgs])
                        op("dve", lambda e: e.tensor_tensor(out=t16[:], in0=el, in1=g1h[:].unsqueeze(3).to_broadcast([128, T_, 4, 4]), op=ALU.mult), reads=[lgA, g1h], writes=[t16])
                        op("dve", lambda e: e.tensor_reduce(out=elg[:], in_=t16[:].rearrange("p t g e -> p t e g"), axis=AX.X, op=ALU.add), reads=[t16], writes=[elg])
                        op("dve", lambda e: e.tensor_reduce(out=ev1[:], in_=elg[:], axis=AX.X, op=ALU.max), reads=[elg], writes=[ev1])
                        op("dve", lambda e: e.tensor_tensor(out=mk1[:], in0=elg[:], in1=ev1[:].unsqueeze(2).to_broadcast(B3), op=ALU.is_equal), reads=[elg, ev1], writes=[mk1])
                        op("dve", lambda e: e.scalar_tensor_tensor(out=elg2[:], in0=mk1[:], scalar=-1e30, in1=elg[:], op0=ALU.mult, op1=ALU.add), reads=[mk1, elg], writes=[elg2])
                        op("dve", lambda e: e.tensor_reduce(out=ev2[:], in_=elg2[:], axis=AX.X, op=ALU.max), reads=[elg2], writes=[ev2])
                        op("dve", lambda e: e.tensor_tensor(out=mk2[:], in0=elg2[:], in1=ev2[:].unsqueeze(2).to_broadcast(B3), op=ALU.is_equal), reads=[elg2, ev2], writes=[mk2])
                        op("dve", lambda e: e.tensor_tensor(out=w12[:, 0, :], in0=ev1[:], in1=ev2[:], op=ALU.subtract), reads=[ev1, ev2], writes=[w12])
                        op("act", lambda e: e.activation(out=w12[:, 0, :], in_=w12[:, 0, :], func=AF.Sigmoid), reads=[w12], writes=[w12])
                        op("dve", lambda e: e.tensor_scalar(out=w12[:, 1, :], in0=w12[:, 0, :], scalar1=-1.0, scalar2=1.0, op0=ALU.mult, op1=ALU.add), reads=[w12], writes=[w12])
                        op("dve", lambda e: e.tensor_tensor(out=w12[:], in0=w12[:], in1=pgs[:].unsqueeze(1).to_broadcast([128, 2, T_]), op=ALU.mult), reads=[w12, pgs], writes=[w12])
                        op("dve", lambda e: e.tensor_tensor(out=gig[:], in0=mk1[:], in1=w12[:, 0, :].unsqueeze(2).to_broadcast(B3), op=ALU.mult), reads=[mk1, w12], writes=[gig])
                        op("dve", lambda e: e.tensor_tensor(out=mk2[:], in0=mk2[:], in1=w12[:, 1, :].unsqueeze(2).to_broadcast(B3), op=ALU.mult), reads=[mk2, w12], writes=[mk2])
                        op("dve", lambda e: e.tensor_tensor(out=gig[:], in0=gig[:], in1=mk2[:], op=ALU.add), reads=[gig, mk2], writes=[gig])
                        op("dve", lambda e: e.tensor_tensor(out=gate[:], in0=g1h[:].unsqueeze(3).to_broadcast([128, T_, 4, 4]),
                                                            in1=gig[:].unsqueeze(2).to_broadcast([128, T_, 4, 4]), op=ALU.mult), reads=[g1h, gig], writes=[gate])
                        for t4 in range(T_ // 4):
                            bk = 6 + t4 % 2
                            for j in range(4):
                                t = t4 * 4 + j
                                op("pe", lambda e: e.transpose(out=PS[bk][0:16, j * 128:(j + 1) * 128], in_=gate[:, t, :, :].rearrange("p g e -> p (g e)"), identity=idf[:]),
                                   reads=[gate, idf], writes=[PS[bk]])
                            op("act", lambda e: e.copy(out=gateT[:, t4 * 512:(t4 + 1) * 512], in_=PS[bk][0:16, :]), reads=[PS[bk]], writes=[gateT])
                    ckpt("D2r")
                    if "gateT" in dbg:
                        fw.dma(dbg_t("gateT", [16, S_OWN], BF16), gateT[:], reads=[gateT], is_output=True)
                    with fw.scope() as esE:
                        NW = 3
                        w13 = [fw.sb([128, 8, 512], BF16, f"w13_{i}", esE) for i in range(NW)]
                        w2e = [fw.sb([128, 2, D], BF16, f"w2e_{i}", esE) for i in range(NW)]
                        sgE = [fw.sb([128, 512], F32, f"sgE{i}", esE) for i in range(2)]
                        tE = [fw.sb([128, 512], F32, f"tE{i}", esE) for i in range(2)]
                        actT = [[fw.sb([128, 512], BF16, f"actT{i}{fc}", esE) for fc in range(2)] for i in range(2)]
                        x1M = [Buf(x1.t, f"x1m_{t}") for t in range(NT_OWN)]
                        for b_ in x1M:
                            b_.lw = x1.lw
                            b_.rd = dict(x1.rd)
                        ybank = [4, 5, 7]
                        yi = [0]

                        def load_w(ex):
                            wb = ex % NW
                            fw.dma(w13[wb][:], w_e13_d[ex].rearrange("(k p) c -> p k c", p=128), writes=[w13[wb]], q="pool")
                            fw.dma(w2e[wb][:], w_e2_d[ex].rearrange("(k p) c -> p k c", p=128), writes=[w2e[wb]], q="pool")

                        def e_front_pe(it):
                            ex, tb = it // 4, it % 4
                            wb = ex % NW
                            ts_ = slice(tb * 512, (tb + 1) * 512)
                            mm(PS[6], PS[6][:, :], E16[:, ex, :], gateT[:, ts_], True, True, [E16, gateT])
                            for fc in range(2):
                                for k in range(8):
                                    mm(PS[fc], PS[fc][:, :], w13[wb][:, k, fc * 128:(fc + 1) * 128], YT[:, k, ts_], k == 0, k == 7, [w13[wb], YT])
                                for k in range(8):
                                    mm(PS[2 + fc], PS[2 + fc][:, :], w13[wb][:, k, 256 + fc * 128:256 + (fc + 1) * 128], YT[:, k, ts_], k == 0, k == 7, [w13[wb], YT])

                        def e_front_post(it):
                            r = it % 2
                            for fc in range(2):
                                op("act", lambda e: e.activation(out=sgE[fc][:], in_=PS[fc][:, :], func=AF.Silu), reads=[PS[fc]], writes=[sgE[fc]])
                                op("dve", lambda e: e.tensor_tensor(out=tE[fc][:], in0=PS[2 + fc][:, :], in1=sgE[fc][:], op=ALU.mult), reads=[PS[2 + fc], sgE[fc]], writes=[tE[fc]])
                                op("dve", lambda e: e.tensor_tensor(out=actT[r][fc][:], in0=PS[6][:, :], in1=tE[fc][:], op=ALU.mult), reads=[PS[6], tE[fc]], writes=[actT[r][fc]])

                        def e_back(it):
                            ex, tb = it // 4, it % 4
                            wb = ex % NW
                            r = it % 2
                            for tt in range(4):
                                t = tb * 4 + tt
                                for half in range(2):
                                    b = ybank[yi[0] % 3]
                                    yi[0] += 1
                                    for fc in range(2):
                                        mm(PS[b], PS[b][:, :], actT[r][fc][:, tt * 128:(tt + 1) * 128], w2e[wb][:, fc, half * 512:(half + 1) * 512], fc == 0, fc == 1, [actT[r][fc], w2e[wb]])
                                    op("dve", lambda e: e.tensor_tensor(out=x1[:, t, half * 512:(half + 1) * 512], in0=PS[b][:, :], in1=x1[:, t, half * 512:(half + 1) * 512], op=ALU.add),
                                       reads=[PS[b], x1M[t]], writes=[x1M[t]])

                        load_w(0)
                        load_w(1)
                        NIT = 64
                        for it in range(NIT + 1):
                            if it < NIT:
                                e_front_pe(it)
                                e_front_post(it)
                            if it >= 1:
                                e_back(it - 1)
                            if it < NIT and it % 4 == 0 and it // 4 + 2 < 16:
                                load_w(it // 4 + 2)
                        for b_ in x1M:
                            if b_.lw is not None and (x1.lw is None or True):
                                pass
                        x1.lw = None
                        x1.rd = {}
                        fw.barrier()
                ckpt("D2")
                if "x2" in dbg:
                    fw.dma(dbg_t("x2", [128, NT_OWN, D]), x1[:], reads=[x1], is_output=True)
                with fw.scope() as esP:
                    load_gain(2)
                    gB2 = fw.sb([128, D], F32, "gB2", esP)
                    fw.dma(gB2[:], gvec_d[3:4, :].to_broadcast([128, D]), writes=[gB2])
                    w_pg = fw.sb([128, 8, D], BF16, "w_pg", esP)
                    fw.dma(w_pg[:], w_pg_d.rearrange("(k p) c -> p k c", p=128), writes=[w_pg], q="pool")
                    w_pp = fw.sb([128, 2, D], BF16, "w_pp", esP)
                    fw.dma(w_pp[:], w_pp_d.rearrange("(k p) c -> p k c", p=128), writes=[w_pp], q="pool")
                    hpb = [fw.sb([128, D], BF16, f"hpb{i}", esP) for i in range(3)]
                    hpT = [fw.sb([128, 8, 128], BF16, f"hpT{i}", esP) for i in range(3)]
                    plb = [fw.sb([128, 256], BF16, f"plb{i}", esP) for i in range(3)]
                    plT = [fw.sb([128, 2, 128], BF16, f"plT{i}", esP) for i in range(3)]
                    junkP2 = fw.sb([128, D], BF16, "junkP2", esP)
                    sgP = [fw.sb([128, 512], F32, f"sgP{i}", esP) for i in range(2)]
                    tP = [fw.sb([128, 512], F32, f"tP{i}", esP) for i in range(2)]
                    outt = [fw.sb([128, D], F32, f"outt{i}", esP) for i in range(2)]
                    junkP = fw.sb([128, D], BF16, "junkP", esP)
                    ssp = [fw.sb([128, 1], F32, f"ssp{i}", esP) for i in range(5)]
                    rrp = [fw.sb([128, 1], F32, f"rrp{i}", esP) for i in range(5)]
                    x1T = [Buf(x1.t, f"x1_{t}") for t in range(NT_OWN)]
                    for b_ in x1T:
                        b_.lw = x1.lw
                        b_.rd = dict(x1.rd)

                    def p_s1(t):
                        r = t % 3
                        fw.dma(plb[r][:], pl_d[t * 128:(t + 1) * 128, :], writes=[plb[r]], q="pool")
                        rs = {"ss": ssp[r], "r": rrp[r]}
                        rms_rstd({"ap": x1[:, t, :], "bufs": [x1T[t]]}, rs, D, {"ap": junkP[:], "buf": junkP})
                        op("dve", lambda e: e.scalar_tensor_tensor(out=hpb[r][:], in0=x1[:, t, :], scalar=rs["r"][:], in1=gB[:], op0=ALU.mult, op1=ALU.mult),
                           reads=[x1T[t], rs["r"], gB], writes=[hpb[r]])

                    def p_s2(t):
                        r = t % 3
                        b0 = 2 * (t % 2)
                        for k in range(8):
                            op("pe", lambda e: e.transpose(out=psbf(b0)[:, k * 128:(k + 1) * 128], in_=hpb[r][:, k * 128:(k + 1) * 128], identity=idb[:]), reads=[hpb[r], idb], writes=[PS[b0]])
                        op("act", lambda e: e.copy(out=hpT[r][:], in_=psbf(b0).rearrange("p (k t) -> p k t", k=8)), reads=[PS[b0]], writes=[hpT[r]])
                        for k in range(2):
                            op("pe", lambda e: e.transpose(out=psbf(b0 + 1)[:, k * 128:(k + 1) * 128], in_=plb[r][:, k * 128:(k + 1) * 128], identity=idb[:]), reads=[plb[r], idb], writes=[PS[b0 + 1]])
                        op("act", lambda e: e.copy(out=plT[r][:], in_=psbf(b0 + 1)[:, 0:256].rearrange("p (k t) -> p k t", k=2)), reads=[PS[b0 + 1]], writes=[plT[r]])

                    def p_s3(t):
                        r = t % 3
                        for half in range(2):
                            hs = slice(half * 512, (half + 1) * 512)
                            bG = 4 + half
                            bP = 6 + half
                            for k in range(8):
                                mm(PS[bG], PS[bG][:, :], hpT[r][:, k, :], w_pg[:, k, hs], k == 0, k == 7, [hpT[r], w_pg])
                            for k in range(2):
                                mm(PS[bP], PS[bP][:, :], plT[r][:, k, :], w_pp[:, k, hs], k == 0, k == 1, [plT[r], w_pp])
                            op("act", lambda e: e.activation(out=sgP[half][:], in_=PS[bG][:, :], func=AF.Sigmoid), reads=[PS[bG]], writes=[sgP[half]])
                            op("dve", lambda e: e.tensor_tensor(out=tP[half][:], in0=PS[bP][:, :], in1=sgP[half][:], op=ALU.mult), reads=[PS[bP], sgP[half]], writes=[tP[half]])
                            op("dve", lambda e: e.tensor_tensor(out=x1[:, t, hs], in0=x1[:, t, hs], in1=tP[half][:], op=ALU.add), reads=[x1T[t], tP[half]], writes=[x1T[t]])
                        rs2 = {"ss": ssp[3 + t % 2], "r": rrp[3 + t % 2]}
                        rms_rstd({"ap": x1[:, t, :], "bufs": [x1T[t]]}, rs2, D, {"ap": junkP2[:], "buf": junkP2})
                        o_ = outt[t % 2]
                        op("dve", lambda e: e.scalar_tensor_tensor(out=o_[:], in0=x1[:, t, :], scalar=rs2["r"][:], in1=gB2[:], op0=ALU.mult, op1=ALU.mult),
                           reads=[x1T[t], rs2["r"], gB2], writes=[o_])
                        fw.dma(out_d[t * 128:(t + 1) * 128, :], o_[:], reads=[o_], is_output=True)

                    for i in range(NT_OWN + 2):
                        if i < NT_OWN:
                            p_s1(i)
                        if 1 <= i <= NT_OWN:
                            p_s2(i - 1)
                        if i >= 2:
                            p_s3(i - 2)

            if "yaT" in dbg:
                o = dbg_t("yaT", [128, 4, S_OWN], BF16)
                fw.dma(o[:, :, :], YT[:, 0:4, :], reads=[YT], is_output=True)

            if "hT" in dbg:
                o = dbg_t("hT", [128, 8, S_EXT], BF16)
                with fw.scope() as esd:
                    tmp = fw.sb([128, 8, 512], BF16, "dbg_hT", esd)
                    for i in range(8):
                        fw.dma(tmp[:], hT_d[:, :, i * 512:(i + 1) * 512], reads=hT_tiles[4 * i:4 * i + 4], writes=[tmp])
                        fw.dma(o[:, :, i * 512:(i + 1) * 512], tmp[:], reads=[tmp], is_output=True)


        body()
        fw.stopped = False
        fw.finish()
    return nc, dbg_out


_INV = (500000.0 ** (-np.arange(0, 16, 2, dtype=np.float32) / 16.0)).astype(np.float32)


def make_in_maps(inputs):
    f = lambda a: np.ascontiguousarray(np.asarray(a), dtype=np.float32)
    x = f(inputs["x"]); p = f(inputs["p"])
    positions = np.asarray(inputs["positions"]).astype(np.int32)
    w_in = f(inputs["w_in"])[0]
    offs = np.cumsum([0, 512, 128, 128, 128, 128, 128, 128, 24, 1024, 512, 512, 8, 2048])
    seg = {n: (offs[i], offs[i + 1]) for i, n in enumerate(["q", "kc", "vc", "ks", "vs", "kw", "vw", "gate", "qk", "v", "o", "if", "mg"])}
    col = lambda n: w_in[:, seg[n][0]:seg[n][1]]
    w_att = []
    for g in range(2):
        parts = [col("q")[:, g * 256:(g + 1) * 256]]
        for n in ["ks", "kw", "kc", "vc", "vs", "vw"]:
            parts.append(col(n)[:, g * 64:(g + 1) * 64])
        parts.append(col("gate")[:, g * 12:(g + 1) * 12])
        w_att.append(np.concatenate(parts, axis=1))
    w_att = np.ascontiguousarray(np.stack(w_att))
    shared = {
        "invf": np.ascontiguousarray(np.broadcast_to(_INV[None, :], (128, 8))),
        "gvec": np.ascontiguousarray(np.stack([f(inputs["g_mix"])[0], f(inputs["g_ffn"])[0], f(inputs["g_ple"])[0], f(inputs["g_final"])])),
        "w_att": w_att,
        "w_qk": np.ascontiguousarray(col("qk")),
        "w_vo": np.ascontiguousarray(np.concatenate([col("v"), col("o")], axis=1)),
        "w_if": np.ascontiguousarray(col("if")),
        "w_mg": np.ascontiguousarray(col("mg")),
        "b_if": f(inputs["b_if"]).reshape(1, 8),
        "w_c1": np.ascontiguousarray(np.stack([f(inputs["w_ck1"])[0], f(inputs["w_cv1"])[0]])),
        "w_c2": np.ascontiguousarray(np.stack([f(inputs["w_ck2"])[0], f(inputs["w_cv2"])[0]])),
        "pe_c": np.ascontiguousarray(np.stack([f(inputs["pe_ck"])[0], f(inputs["pe_cv"])[0]])),
        "wc": np.ascontiguousarray(f(inputs["w_conv"])[0].reshape(4, 8, 128).transpose(2, 1, 0)),
        "bc": np.ascontiguousarray(f(inputs["b_conv"])[0].reshape(8, 128).T),
        "g_hn": f(inputs["g_hn"]).reshape(1, 512),
        "w_pa": f(inputs["w_pa"])[0], "w_pb": f(inputs["w_pb"])[0], "w_out": f(inputs["w_out"])[0],
        "w_r": np.ascontiguousarray(np.concatenate([f(inputs["w_rg"])[0], f(inputs["w_re"])[0]], axis=1)),
        "b_r": np.ascontiguousarray(np.concatenate([f(inputs["b_rg"])[0], f(inputs["b_re"])[0]])[None, :]),
        "w_e13": f(inputs["w_e13"])[0], "w_e2": f(inputs["w_e2"])[0],
        "w_pg": f(inputs["w_pg"])[0], "w_pp": f(inputs["w_pp"])[0],
    }
    in_maps = []
    for core in range(8):
        b, half = core // 2, core % 2
        if half == 1:
            xe_ = x[b]
            pos_ = positions[b]
        else:
            xe_ = np.concatenate([np.zeros((S_OWN, D), np.float32), x[b, :S_OWN]], axis=0)
            pos_ = np.concatenate([np.zeros(S_OWN, np.int32), positions[b, :S_OWN]])
        m = dict(shared)
        m["xe"] = np.ascontiguousarray(xe_)
        m["pos"] = np.ascontiguousarray(pos_.reshape(NT_EXT, 128).T)
        m["pl"] = np.ascontiguousarray(p[0, b, half * S_OWN:(half + 1) * S_OWN])
        m["hv"] = np.full((128, 1), float(half), np.float32)
        in_maps.append(m)
    return in_maps


def kernel(**inputs):
    nc, _ = build_program()
    in_maps = make_in_maps(inputs)
    res = run_bass_kernel_spmd(nc, in_maps, core_ids=list(range(8)))
    out = np.zeros((4, S_EXT, D), np.float32)
    for core in range(8):
        b, half = core // 2, core % 2
        out[b, half * S_OWN:(half + 1) * S_OWN] = res.results[core]["out"]
    return out
```

```python
import numpy as np
import concourse.bass as bass
import concourse.mybir as mybir
from concourse.bass_utils import run_bass_kernel_spmd
from contextlib import ExitStack

F32 = mybir.dt.float32
BF16 = mybir.dt.bfloat16
I32 = mybir.dt.int32
AF = mybir.ActivationFunctionType
ALU = mybir.AluOpType
AX = mybir.AxisListType

D = 1024
S_OWN = 2048
S_EXT = 4096
NT_OWN = 16
NT_EXT = 32
EPS = 1e-6
NEGB = -30000.0
DBG = []


class Buf:
    __slots__ = ("t", "lw", "rd", "name", "excl")

    def __init__(self, t, name=""):
        self.t = t
        self.excl = False
        self.lw = None
        self.rd = {}
        self.name = name

    def __getitem__(self, k):
        return self.t[k]


class FW:
    NDMA = 24

    def __init__(self, nc, es):
        self.nc = nc
        self.es = es
        self.eng = {"pe": nc.tensor, "act": nc.scalar, "dve": nc.vector, "pool": nc.gpsimd, "sp": nc.sync}
        self.sem = {k: es.enter_context(nc.semaphore("s_" + k)) for k in self.eng}
        self.cnt = {k: 0 for k in self.eng}
        self.known = {k: {} for k in self.eng}
        self.dsem = [es.enter_context(nc.semaphore(f"s_dma{i}")) for i in range(self.NDMA)]
        self.dval = [0] * self.NDMA
        self.dnext = 0
        self.nbuf = 0
        self.out_waits = []
        self.stopped = False

    def sb(self, shape, dt, name=None, es=None):
        self.nbuf += 1
        name = f"sb{self.nbuf}_" + (name or "t")
        return Buf((es or self.es).enter_context(self.nc.sbuf_tensor(name, list(shape), dt)), name)

    def ps(self, shape, dt, name=None):
        self.nbuf += 1
        name = name or f"ps{self.nbuf}"
        b = Buf(self.es.enter_context(self.nc.psum_tensor(name, list(shape), dt)), name)
        b.excl = True
        return b

    def _wait(self, e, src, idx):
        if self.stopped:
            return
        kn = self.known[e]
        if kn.get(src, 0) >= idx:
            return
        s = self.dsem[src[1]] if isinstance(src, tuple) else self.sem[src]
        self.eng[e].wait_ge(s, idx)
        kn[src] = idx

    def _deps(self, e, reads, writes):
        for b in reads:
            if b.lw is not None:
                self._wait(e, b.lw[0], b.lw[1])
            if b.excl:
                for src, idx in b.rd.items():
                    if src != e:
                        self._wait(e, src, idx)
        for b in writes:
            if b.lw is not None and b.lw[0] != e:
                self._wait(e, b.lw[0], b.lw[1])
            for src, idx in b.rd.items():
                if src != e:
                    self._wait(e, src, idx)

    def op(self, e, fn, reads=(), writes=()):
        if self.stopped:
            return None
        self._deps(e, reads, writes)
        inst = fn(self.eng[e])
        self.cnt[e] += 1
        c = self.cnt[e]
        inst.then_inc(self.sem[e], 1)
        for b in reads:
            if b.rd.get(e, 0) < c:
                b.rd[e] = c
        for b in writes:
            b.lw = (e, c)
            b.rd = {}
        return inst

    def dma(self, out, in_, reads=(), writes=(), q="sp", is_output=False):
        if self.stopped and not is_output:
            return None
        self._deps(q, reads, writes)
        slot = self.dnext
        self.dnext = (self.dnext + 1) % self.NDMA
        key = ("d", slot)
        if self.dval[slot] > 0:
            self._wait(q, key, self.dval[slot])
        inst = self.eng[q].dma_start(out=out, in_=in_)
        self.dval[slot] += 16
        inst.then_inc(self.dsem[slot], 16)
        v = self.dval[slot]
        for b in reads:
            if b.rd.get(key, 0) < v:
                b.rd[key] = v
        for b in writes:
            b.lw = (key, v)
            b.rd = {}
        if is_output:
            self.out_waits.append((key, v))
        return inst

    def barrier(self):
        for e in self.eng:
            for src in ("pe", "act", "dve", "pool"):
                if src != e and self.cnt[src] > 0:
                    self._wait(e, src, self.cnt[src])
            for slot in range(self.NDMA):
                if self.dval[slot] > 0:
                    self._wait(e, ("d", slot), self.dval[slot])

    def scope(self):
        fw = self

        class _Scope(ExitStack):
            def __exit__(self, *a):
                fw.barrier()
                return super().__exit__(*a)
        return _Scope()

    def finish(self):
        for key, v in self.out_waits:
            self._wait("sp", key, v)
        for k in ("pe", "act", "dve", "pool"):
            if self.cnt[k] > 0:
                self._wait("sp", k, self.cnt[k])


class _StopBuild(Exception):
    pass


def build_program(dbg=()):
    nc = bass.Bass("TRN2", target_bir_lowering=False)

    def din(name, shape, dt=F32):
        return nc.dram_tensor(name, list(shape), dt, kind="ExternalInput").ap()

    xe = din("xe", [S_EXT, D])
    pos_d = din("pos", [128, NT_EXT], I32)
    pl_d = din("pl", [S_OWN, 256])
    hv_d = din("hv", [128, 1])
    invf_d = din("invf", [128, 8])
    gvec_d = din("gvec", [4, D])
    w_att_d = din("w_att", [2, D, 652])
    w_qk_d = din("w_qk", [D, 1024])
    w_vo_d = din("w_vo", [D, 1024])
    w_if_d = din("w_if", [D, 8])
    w_mg_d = din("w_mg", [D, 2048])
    b_if_d = din("b_if", [1, 8])
    w_c1_d = din("w_c1", [2, 2048, 256])
    w_c2_d = din("w_c2", [2, 256, 64])
    pe_c_d = din("pe_c", [2, 32, 64])
    wc_d = din("wc", [128, 8, 4])
    bc_d = din("bc", [128, 8])
    g_hn_d = din("g_hn", [1, 512])
    w_pa_d = din("w_pa", [512, D])
    w_pb_d = din("w_pb", [512, D])
    w_out_d = din("w_out", [D, D])
    w_r_d = din("w_r", [D, 20])
    b_r_d = din("b_r", [1, 20])
    w_e13_d = din("w_e13", [16, D, 512])
    w_e2_d = din("w_e2", [16, 256, D])
    w_pg_d = din("w_pg", [D, D])
    w_pp_d = din("w_pp", [256, D])
    out_d = nc.dram_tensor("out", [S_OWN, D], F32, kind="ExternalOutput").ap()
    hT_d = nc.dram_tensor("hT_scr", [128, 8, S_EXT], BF16, kind="Internal").ap()
    dbg_out = {}

    def dbg_t(name, shape, dt=F32):
        dbg_out[name] = nc.dram_tensor("dbg_" + name, list(shape), dt, kind="ExternalOutput").ap()
        return dbg_out[name]

    with ExitStack() as es:
        fw = FW(nc, es)
        op = fw.op
        PS = [fw.ps([128, 512], F32, f"psb{i}") for i in range(8)]

        def psbf(i):
            return PS[i][:].bitcast(BF16)

        ones_f = fw.sb([128, 128], F32, "ones_f")
        op("pool", lambda e: e.memset(ones_f[:], 1.0), writes=[ones_f])
        idf = fw.sb([128, 128], F32, "idf")
        op("pool", lambda e: e.affine_select(out=idf[:], in_=ones_f[:], pattern=[[1, 128]], compare_op=ALU.is_equal,
                                             fill=0.0, base=0, channel_multiplier=-1), reads=[ones_f], writes=[idf])
        idb = fw.sb([128, 128], BF16, "idb")
        op("dve", lambda e: e.tensor_copy(out=idb[:], in_=idf[:]), reads=[idf], writes=[idb])
        U_f = fw.sb([128, 128], F32, "U_f")
        op("pool", lambda e: e.affine_select(out=U_f[:], in_=ones_f[:], pattern=[[1, 128]], compare_op=ALU.is_ge,
                                             fill=0.0, base=0, channel_multiplier=-1), reads=[ones_f], writes=[U_f])
        caus = fw.sb([128, 128], BF16, "caus")
        op("dve", lambda e: e.tensor_copy(out=caus[:], in_=U_f[:]), reads=[U_f], writes=[caus])
        wm0_f = fw.sb([128, 128], F32, "wm0_f")
        op("pool", lambda e: e.affine_select(out=wm0_f[:], in_=ones_f[:], pattern=[[-1, 128]], compare_op=ALU.is_ge,
                                             fill=0.0, base=-1, channel_multiplier=1), reads=[ones_f], writes=[wm0_f])
        wm0 = fw.sb([128, 128], BF16, "wm0")
        op("dve", lambda e: e.tensor_copy(out=wm0[:], in_=wm0_f[:]), reads=[wm0_f], writes=[wm0])
        c_eps = fw.sb([128, 1], F32, "c_eps")
        op("pool", lambda e: e.memset(c_eps[:], EPS), writes=[c_eps])
        c_one = fw.sb([128, 1], F32, "c_one")
        op("pool", lambda e: e.memset(c_one[:], 1.0), writes=[c_one])
        c_zero = fw.sb([128, 1], F32, "c_zero")
        op("pool", lambda e: e.memset(c_zero[:], 0.0), writes=[c_zero])
        acc_junk = fw.sb([128, 2], F32, "acc_junk")
        op("act", lambda e: e.activation(out=acc_junk[:, 0:1], in_=c_one[:], func=AF.Square, accum_out=acc_junk[:, 1:2]),
           reads=[c_one], writes=[acc_junk])
        hv = fw.sb([128, 1], F32, "hv")
        fw.dma(hv[:], hv_d[:, :], writes=[hv])
        hbias = fw.sb([128, 1], F32, "hbias")
        op("dve", lambda e: e.tensor_scalar(out=hbias[:], in0=hv[:], scalar1=-1.0, scalar2=-NEGB, op0=ALU.add, op1=ALU.mult),
           reads=[hv], writes=[hbias])
        gB = fw.sb([128, D], F32, "gB")

        def load_gain(i):
            fw.dma(gB[:], gvec_d[i:i + 1, :].to_broadcast([128, D]), writes=[gB])

        cs = fw.sb([128, NT_EXT, 8], F32, "cs")
        sn = fw.sb([128, NT_EXT, 8], F32, "sn")
        with fw.scope() as es1:
            posi = fw.sb([128, NT_EXT], I32, "posi", es1)
            posf = fw.sb([128, NT_EXT], F32, "posf", es1)
            invf = fw.sb([128, 8], F32, "invf", es1)
            ang = fw.sb([128, NT_EXT, 8], F32, "ang", es1)
            kf = fw.sb([128, NT_EXT, 8], F32, "kf", es1)
            ki = fw.sb([128, NT_EXT, 8], I32, "ki", es1)
            r1 = fw.sb([128, NT_EXT, 8], F32, "r1", es1)
            r2 = fw.sb([128, NT_EXT, 8], F32, "r2", es1)
            fw.dma(posi[:], pos_d[:, :], writes=[posi])
            fw.dma(invf[:], invf_d[:, :], writes=[invf])
            op("dve", lambda e: e.tensor_copy(out=posf[:], in_=posi[:]), reads=[posi], writes=[posf])
            op("dve", lambda e: e.tensor_tensor(out=ang[:], in0=posf[:].unsqueeze(2).to_broadcast([128, NT_EXT, 8]),
                                                in1=invf[:].unsqueeze(1).to_broadcast([128, NT_EXT, 8]), op=ALU.mult),
               reads=[posf, invf], writes=[ang])
            TWO_PI = 6.283185307179586
            C1 = 6.28125
            C2 = TWO_PI - C1
            PI_LO = 3.1415925
            op("dve", lambda e: e.tensor_scalar(out=kf[:], in0=ang[:], scalar1=1.0 / TWO_PI, scalar2=None, op0=ALU.mult),
               reads=[ang], writes=[kf])
            op("dve", lambda e: e.tensor_copy(out=ki[:], in_=kf[:]), reads=[kf], writes=[ki])
            op("dve", lambda e: e.tensor_copy(out=kf[:], in_=ki[:]), reads=[ki], writes=[kf])
            op("dve", lambda e: e.scalar_tensor_tensor(out=r1[:], in0=kf[:], scalar=-C1, in1=ang[:], op0=ALU.mult, op1=ALU.add),
               reads=[kf, ang], writes=[r1])
            op("dve", lambda e: e.scalar_tensor_tensor(out=r1[:], in0=kf[:], scalar=-C2, in1=r1[:], op0=ALU.mult, op1=ALU.add),
               reads=[kf, r1], writes=[r1])
            op("dve", lambda e: e.tensor_scalar(out=r1[:], in0=r1[:], scalar1=PI_LO, scalar2=-PI_LO, op0=ALU.min, op1=ALU.max),
               reads=[r1], writes=[r1])
            op("act", lambda e: e.activation(out=sn[:], in_=r1[:], func=AF.Sin), reads=[r1], writes=[sn])
            op("dve", lambda e: e.tensor_scalar(out=r2[:], in0=r1[:], scalar1=PI_LO / 2 + 0.0, scalar2=None, op0=ALU.add),
               reads=[r1], writes=[r2])
            op("dve", lambda e: e.tensor_scalar(out=kf[:], in0=r2[:], scalar1=PI_LO, scalar2=-TWO_PI, op0=ALU.is_gt, op1=ALU.mult),
               reads=[r2], writes=[kf])
            op("dve", lambda e: e.tensor_tensor(out=r2[:], in0=r2[:], in1=kf[:], op=ALU.add), reads=[r2, kf], writes=[r2])
            op("dve", lambda e: e.tensor_scalar(out=r2[:], in0=r2[:], scalar1=PI_LO, scalar2=-PI_LO, op0=ALU.min, op1=ALU.max),
               reads=[r2], writes=[r2])
            op("act", lambda e: e.activation(out=cs[:], in_=r2[:], func=AF.Sin), reads=[r2], writes=[cs])

        def rms_rstd(src, rstd, n, junk):
            ss = rstd["ss"]
            op("act", lambda e: e.activation(out=junk["ap"], in_=src["ap"], func=AF.Square, accum_out=ss[:]),
               reads=src["bufs"], writes=[junk["buf"], ss])
            op("act", lambda e: e.activation(out=ss[:], in_=ss[:], func=AF.Sqrt, bias=c_eps[:], scale=1.0 / n),
               reads=[ss, c_eps], writes=[ss])
            op("dve", lambda e: e.reciprocal(out=rstd["r"][:], in_=ss[:]), reads=[ss], writes=[rstd["r"]])

        load_gain(0)
        hT_tiles = [Buf(None, f"hT_tile{t}") for t in range(NT_EXT)]
        with fw.scope() as esA:
            xt = [fw.sb([128, D], F32, f"xtA{i}", esA) for i in range(6)]
            xn = [fw.sb([128, D], BF16, f"xnA{i}", esA) for i in range(3)]
            junk = fw.sb([128, D], BF16, "junkA", esA)
            hst = [fw.sb([128, 8, 128], BF16, f"hstA{i}", esA) for i in range(4)]
            ssA = [fw.sb([128, 1], F32, f"ssA{i}", esA) for i in range(3)]
            rrA = [fw.sb([128, 1], F32, f"rrA{i}", esA) for i in range(3)]
            def a_s1(t):
                x_ = xt[t % 6]
                if t == 0:
                    for tt in range(5):
                        fw.dma(xt[tt][:], xe[tt * 128:(tt + 1) * 128, :], writes=[xt[tt]])
                if t + 5 < NT_EXT:
                    fw.dma(xt[(t + 5) % 6][:], xe[(t + 5) * 128:(t + 6) * 128, :], writes=[xt[(t + 5) % 6]])
                rs = {"ss": ssA[t % 3], "r": rrA[t % 3]}
                rms_rstd({"ap": x_[:], "bufs": [x_]}, rs, D, {"ap": junk[:], "buf": junk})
                n_ = xn[t % 3]
                op("dve", lambda e: e.scalar_tensor_tensor(out=n_[:], in0=x_[:], scalar=rs["r"][:], in1=gB[:], op0=ALU.mult, op1=ALU.mult),
                   reads=[x_, rs["r"], gB], writes=[n_])

            def a_s2(t):
                n_ = xn[t % 3]
                pb = t % 2
                for k in range(8):
                    op("pe", lambda e: e.transpose(out=psbf(pb)[:, k * 128:(k + 1) * 128], in_=n_[:, k * 128:(k + 1) * 128], identity=idb[:]),
                       reads=[n_, idb], writes=[PS[pb]])
                h_ = hst[t % 4]
                op("act", lambda e: e.copy(out=h_[:], in_=psbf(pb).rearrange("p (k t) -> p k t", k=8)), reads=[PS[pb]], writes=[h_])
                fw.dma(hT_d[:, :, t * 128:(t + 1) * 128], h_[:], reads=[h_], writes=[hT_tiles[t]], q="pool")

            for t in range(NT_EXT + 1):
                if t < NT_EXT:
                    a_s1(t)
                if t >= 1:
                    a_s2(t - 1)

        if "cs" in dbg:
            o = dbg_t("cs", [128, NT_EXT, 8])
            fw.dma(o[:, :, :], cs[:], reads=[cs], is_output=True)
            o = dbg_t("sn", [128, NT_EXT, 8])
            fw.dma(o[:, :, :], sn[:], reads=[sn], is_output=True)

        def ckpt(name):
            if ("stop_" + name) in dbg:
                fw.stopped = True

        def body():
            def mm(bank, out_ap, lhsT, rhs, start, stop, reads):
                op("pe", lambda e: e.matmul(out_ap, lhsT, rhs, start=start, stop=stop), reads=reads, writes=[bank])

            YT = fw.sb([128, 8, S_OWN], BF16, "YT")
            esBc = fw.scope()
            esBc.__enter__()
            cmask = fw.sb([128, 2, S_OWN], BF16, "cmask", esBc)
            op("pool", lambda e: e.memset(cmask[:], 1.0), writes=[cmask])
            op("pool", lambda e: e.affine_select(out=cmask[:, 0, :], in_=cmask[:, 0, :], pattern=[[1, S_OWN]], compare_op=ALU.is_ge, fill=0.0,
                                                 base=2017, channel_multiplier=-16), reads=[cmask], writes=[cmask])
            op("pool", lambda e: e.affine_select(out=cmask[:, 1, :], in_=cmask[:, 1, :], pattern=[[1, S_OWN]], compare_op=ALU.is_ge, fill=0.0,
                                                 base=-31, channel_multiplier=-16), reads=[cmask], writes=[cmask])
            ovl = fw.sb([128, 2, 64], BF16, "ovl", esBc)
            op("pool", lambda e: e.memset(ovl[:], 1.0), writes=[ovl])
            for j in range(2):
                op("pool", lambda e: e.affine_select(out=ovl[:, j, :], in_=ovl[:, j, :], pattern=[[-4, 64]], compare_op=ALU.is_ge, fill=0.0,
                                                     base=128 * j + 1, channel_multiplier=1), reads=[ovl], writes=[ovl])
                op("pool", lambda e: e.affine_select(out=ovl[:, j, :], in_=ovl[:, j, :], pattern=[[4, 64]], compare_op=ALU.is_ge, fill=0.0,
                                                     base=3 - 128 * j, channel_multiplier=-1), reads=[ovl], writes=[ovl])
            maskadd = fw.sb([128, NT_OWN, 64], F32, "maskadd", esBc)
            Mb = fw.sb([128, 64], F32, "Mb", esBc)
            hm1 = fw.sb([128, 2], F32, "hm1", esBc)
            op("dve", lambda e: e.tensor_scalar(out=hm1[:, 0:1], in0=hv[:], scalar1=-1.0, scalar2=1e30, op0=ALU.add, op1=ALU.mult),
               reads=[hv], writes=[hm1])
            op("dve", lambda e: e.tensor_scalar(out=hm1[:, 1:2], in0=hv[:], scalar1=-1.0, scalar2=-1000.0, op0=ALU.add, op1=ALU.mult),
               reads=[hv, hm1], writes=[hm1])
            op("dve", lambda e: e.memset(Mb[:], 0.0), writes=[Mb])
            op("dve", lambda e: e.tensor_copy(out=Mb[:, 0:32], in_=hm1[:, 0:1].to_broadcast([128, 32])), reads=[hm1, Mb], writes=[Mb])
            op("dve", lambda e: e.scalar_tensor_tensor(out=Mb[:, 0:1], in0=hv[:], scalar=1000.0, in1=Mb[:, 0:1], op0=ALU.mult, op1=ALU.add),
               reads=[hv, Mb], writes=[Mb])
            op("dve", lambda e: e.tensor_copy(out=Mb[:, 32:33], in_=hm1[:, 1:2]), reads=[hm1, Mb], writes=[Mb])
            for c in range(NT_OWN):
                op("pool", lambda e: e.tensor_copy(out=maskadd[:, c, :], in_=Mb[:]), reads=[Mb, maskadd], writes=[maskadd])
                for hf in range(2):
                    lo = 32 + 2 * c + hf + 1
                    if lo < 64:
                        op("pool", lambda e: e.memset(maskadd[hf * 64:(hf + 1) * 64, c, lo:64], -1e30), reads=[maskadd], writes=[maskadd])
                    for col in (32 + 2 * c + hf, 32 + 2 * c + hf - 1):
                        op("pool", lambda e: e.tensor_scalar(out=maskadd[hf * 64:(hf + 1) * 64, c, col:col + 1],
                                                             in0=maskadd[hf * 64:(hf + 1) * 64, c, col:col + 1],
                                                             scalar1=1000.0, scalar2=None, op0=ALU.add), reads=[maskadd], writes=[maskadd])

            ckpt("consts")
            for g in range(2):
                with fw.scope() as esG:
                    qT = fw.sb([128, 4, S_OWN], BF16, f"qT{g}", esG)
                    kkT = fw.sb([128, 2, S_EXT], BF16, f"kkT{g}", esG)
                    op("pool", lambda e: e.memset(qT[64:128, :, :], 0.0), writes=[qT])
                    op("pool", lambda e: e.memset(kkT[64:128, 0, :], 1.0), writes=[kkT])
                    op("pool", lambda e: e.memset(kkT[64:128, 1, :], 0.0), writes=[kkT])
                    op("pool", lambda e: e.affine_select(out=kkT[64:128, 0, :], in_=kkT[64:128, 0, :], pattern=[[1, S_EXT]], compare_op=ALU.is_ge, fill=0.0,
                                                         base=0, channel_multiplier=-64), reads=[kkT], writes=[kkT])
                    op("pool", lambda e: e.affine_select(out=kkT[64:128, 0, :], in_=kkT[64:128, 0, :], pattern=[[-1, S_EXT]], compare_op=ALU.is_ge, fill=0.0,
                                                         base=63, channel_multiplier=64), reads=[kkT], writes=[kkT])
                    vv = fw.sb([128, NT_EXT, 2, 65], BF16, f"vv{g}", esG)
                    gsig = fw.sb([128, NT_OWN, 12], F32, f"gsig{g}", esG)
                    kcmpT = fw.sb([128, 256], BF16, f"kcmpT{g}", esG)
                    op("pool", lambda e: e.memset(kcmpT[64:128, :], 0.0), writes=[kcmpT])
                    vca = fw.sb([128, 2, 65], BF16, f"vca{g}", esG)
                    op("pool", lambda e: e.memset(vv[:, :, :, 64:65], 1.0), writes=[vv])
                    op("pool", lambda e: e.memset(vca[:, :, 64:65], 1.0), writes=[vca])
                    ckpt("B0a")
                    with fw.scope() as esC:
                        ccT = fw.sb([64, 2, S_EXT], BF16, f"ccT{g}", esC)
                        with fw.scope() as esB1:
                            w_att = fw.sb([128, 8, 652], BF16, f"w_att{g}", esB1)
                            fw.dma(w_att[:], w_att_d[g].rearrange("(k p) c -> p k c", p=128), writes=[w_att], q="pool")
                            ckpt("B0b")
                            hblk = [fw.sb([128, 8, 512], BF16, f"hblkB{g}{i}", esB1) for i in range(2)]
                            rp = [fw.sb([128, 8, 64], BF16, f"rp{g}{i}", esB1) for i in range(2)]
                            rpf = [fw.sb([128, 8, 64], F32, f"rpf{g}{i}", esB1) for i in range(2)]
                            ta = [fw.sb([128, 7, 8], F32, f"ropa{g}{i}", esB1) for i in range(2)]
                            tb_ = [fw.sb([128, 7, 8], F32, f"ropb{g}{i}", esB1) for i in range(2)]
                            tcx = [fw.sb([128, 7, 8], F32, f"ropc{g}{i}", esB1) for i in range(2)]
                            tdx = [fw.sb([128, 7, 8], F32, f"ropd{g}{i}", esB1) for i in range(2)]
                            def b1_front(t):
                                own = t >= NT_OWN
                                tq = t - NT_OWN
                                hb = hblk[(t // 4) % 2]
                                if t % 4 == 0:
                                    fw.dma(hb[:], hT_d[:, :, t * 128:(t + 4) * 128], reads=hT_tiles[t:t + 4], writes=[hb])
                                tl = t % 4
                                a0 = 0 if own else 256
                                nb = 140 if own else 128
                                bA = 2 + t % 2
                                bB = 4 + t % 2
                                for k in range(8):
                                    mm(PS[bA], PS[bA][:, a0:512], hb[:, k, tl * 128:(tl + 1) * 128], w_att[:, k, a0:512], k == 0, k == 7, [hb, w_att])
                                for k in range(8):
                                    mm(PS[bB], PS[bB][:, 0:nb], hb[:, k, tl * 128:(tl + 1) * 128], w_att[:, k, 512:512 + nb], k == 0, k == 7, [hb, w_att])
                                rp_ = rp[t % 2]
                                h0 = a0 // 64
                                nh = 7 - h0
                                rf = rpf[t % 2]
                                op("act", lambda e: e.copy(out=rf[:, h0:8, :], in_=PS[bA][:, a0:512].rearrange("p (h d) -> p h d", d=64)),
                                   reads=[PS[bA]], writes=[rf])
                                op("pool", lambda e: e.tensor_copy(out=rp_[:, h0:8, :], in_=rf[:, h0:8, :]), reads=[rf], writes=[rp_])
                                t1 = rf[:, h0:7, 0:8]
                                t2 = rf[:, h0:7, 8:16]
                                Cb = cs[:, t, :].unsqueeze(1).to_broadcast([128, nh, 8])
                                Sb_ = sn[:, t, :].unsqueeze(1).to_broadcast([128, nh, 8])
                                ta_, tb2 = ta[t % 2], tb_[t % 2]
                                tc_, td_ = tcx[t % 2], tdx[t % 2]
                                op("dve", lambda e: e.tensor_tensor(out=ta_[:, 0:nh, :], in0=t1, in1=Cb, op=ALU.mult), reads=[rf, cs], writes=[ta_])
                                op("dve", lambda e: e.tensor_tensor(out=tb2[:, 0:nh, :], in0=t2, in1=Sb_, op=ALU.mult), reads=[rf, sn], writes=[tb2])
                                op("dve", lambda e: e.tensor_tensor(out=tc_[:, 0:nh, :], in0=t2, in1=Cb, op=ALU.mult), reads=[rf, cs], writes=[tc_])
                                op("dve", lambda e: e.tensor_tensor(out=td_[:, 0:nh, :], in0=t1, in1=Sb_, op=ALU.mult), reads=[rf, sn], writes=[td_])
                                op("dve", lambda e: e.tensor_tensor(out=rp_[:, h0:7, 0:8], in0=ta_[:, 0:nh, :], in1=tb2[:, 0:nh, :], op=ALU.subtract),
                                   reads=[ta_, tb2, rp_], writes=[rp_])
                                op("dve", lambda e: e.tensor_tensor(out=rp_[:, h0:7, 8:16], in0=tc_[:, 0:nh, :], in1=td_[:, 0:nh, :], op=ALU.add),
                                   reads=[tc_, td_, rp_], writes=[rp_])
                                op("dve", lambda e: e.tensor_copy(out=vv[:, t, :, 0:64], in_=PS[bB][:, 0:128].rearrange("p (h d) -> p h d", d=64)),
                                   reads=[PS[bB]], writes=[vv])
                                if own:
                                    op("act", lambda e: e.activation(out=gsig[:, tq, :], in_=PS[bB][:, 128:140], func=AF.Sigmoid),
                                       reads=[PS[bB]], writes=[gsig])

                            def b1_back(t):
                                own = t >= NT_OWN
                                tq = t - NT_OWN
                                rp_ = rp[t % 2]
                                h0 = 0 if own else 4
                                bT = t % 2
                                psT = psbf(bT)
                                for j, hh in enumerate(range(h0, 8)):
                                    op("pe", lambda e: e.transpose(out=psT[0:64, j * 128:(j + 1) * 128], in_=rp_[:, hh, :], identity=idb[:]),
                                       reads=[rp_, idb], writes=[PS[bT]])
                                if own:
                                    op("act", lambda e: e.copy(out=qT[0:64, :, tq * 128:(tq + 1) * 128], in_=psT[0:64, 0:512].rearrange("p (h t) -> p h t", h=4)),
                                       reads=[PS[bT]], writes=[qT])
                                    o1 = 512
                                else:
                                    o1 = 0
                                op("act", lambda e: e.copy(out=kkT[0:64, :, t * 128:(t + 1) * 128], in_=psT[0:64, o1:o1 + 256].rearrange("p (h t) -> p h t", h=2)),
                                   reads=[PS[bT]], writes=[kkT])
                                op("act", lambda e: e.copy(out=ccT[:, :, t * 128:(t + 1) * 128], in_=psT[0:64, o1 + 256:o1 + 512].rearrange("p (h t) -> p h t", h=2)),
                                   reads=[PS[bT]], writes=[ccT])

                            for t in range(NT_EXT + 1):
                                if t < NT_EXT:
                                    b1_front(t)
                                if t >= 1:
                                    b1_back(t - 1)
                        ckpt("B1")
                        for i in range(2):
                            with fw.scope() as esB2:
                                w1 = fw.sb([64, 32, 256], BF16, f"w1_{g}{i}", esB2)
                                fw.dma(w1[:], w_c1_d[i].rearrange("(l d) h -> d l h", d=64), writes=[w1], q="pool")
                                w2 = fw.sb([128, 2, 64], BF16, f"w2_{g}{i}", esB2)
                                fw.dma(w2[:], w_c2_d[i].rearrange("(c p) d -> p c d", p=128), writes=[w2], q="pool")
                                pe_sb = fw.sb([32, 64], BF16, f"pe_{g}{i}", esB2)
                                fw.dma(pe_sb[:], pe_c_d[i], writes=[pe_sb], q="pool")
                                peT = fw.sb([64, 32], BF16, f"peT_{g}{i}", esB2)
                                op("pe", lambda e: e.transpose(out=psbf(6)[0:64, 0:32], in_=pe_sb[:, :], identity=idb[0:32, 0:32]),
                                   reads=[pe_sb, idb], writes=[PS[6]])
                                op("act", lambda e: e.copy(out=peT[:], in_=psbf(6)[0:64, 0:32]), reads=[PS[6]], writes=[peT])
                                for hc in range(2):
                                    for l in range(32):
                                        mm(PS[7], PS[7][:, hc:hc + 1], w1[:, l, hc * 128:(hc + 1) * 128], peT[:, l:l + 1], l == 0, l == 31, [w1, peT])
                                cbs = fw.sb([128, 2], F32, f"cbs_{g}{i}", esB2)
                                op("act", lambda e: e.copy(out=cbs[:], in_=PS[7][:, 0:2]), reads=[PS[7]], writes=[cbs])
                                G = fw.sb([128, 2, 256], BF16, f"G_{g}{i}", esB2)
                                op("pool", lambda e: e.memset(G[:, :, 255:256], 0.0), writes=[G])
                                u_ = fw.sb([128, 255], F32, f"u_{g}{i}", esB2)
                                u2 = fw.sb([128, 255], F32, f"u2_{g}{i}", esB2)
                                sg_ = fw.sb([128, 255], F32, f"sg_{g}{i}", esB2)
                                for hc in range(2):
                                    for l in range(32):
                                        mm(PS[hc], PS[hc][:, 0:255], w1[:, l, hc * 128:(hc + 1) * 128], ccT[:, i, l:l + 16 * 254 + 1:16], l == 0, l == 31, [w1, ccT])
                                    op("act", lambda e: e.activation(out=u_[:], in_=PS[hc][:, 0:255], func=AF.Identity, bias=cbs[:, hc:hc + 1]),
                                       reads=[PS[hc], cbs], writes=[u_])
                                    op("dve", lambda e: e.tensor_tensor(out=u2[:], in0=u_[:], in1=u_[:], op=ALU.mult), reads=[u_], writes=[u2])
                                    op("dve", lambda e: e.tensor_scalar(out=u2[:], in0=u2[:], scalar1=0.044715, scalar2=1.0, op0=ALU.mult, op1=ALU.add),
                                       reads=[u2], writes=[u2])
                                    op("dve", lambda e: e.tensor_tensor(out=u2[:], in0=u2[:], in1=u_[:], op=ALU.mult), reads=[u2, u_], writes=[u2])
                                    op("act", lambda e: e.activation(out=sg_[:], in_=u2[:], func=AF.Sigmoid, scale=1.5957691216057308),
                                       reads=[u2], writes=[sg_])
                                    op("dve", lambda e: e.tensor_tensor(out=G[:, hc, 0:255], in0=u_[:], in1=sg_[:], op=ALU.mult), reads=[u_, sg_], writes=[G])
                                if i == 0:
                                    for hc in range(2):
                                        mm(PS[6], PS[6][0:64, 0:256], w2[:, hc, :], G[:, hc, :], hc == 0, hc == 1, [w2, G])
                                    op("act", lambda e: e.copy(out=kcmpT[0:64, :], in_=PS[6][0:64, 0:256]), reads=[PS[6]], writes=[kcmpT])
                                else:
                                    for nch in range(2):
                                        for hc in range(2):
                                            mm(PS[6], PS[6][:, nch * 64:(nch + 1) * 64], G[:, hc, nch * 128:(nch + 1) * 128], w2[:, hc, :], hc == 0, hc == 1, [w2, G])
                                    op("act", lambda e: e.copy(out=vca[:, :, 0:64], in_=PS[6][:, 0:128].rearrange("p (n d) -> p n d", d=64)),
                                       reads=[PS[6]], writes=[vca])
                    if g == 0 and "B2dump" in dbg:
                        for nm, bf, shp in (("kkT", kkT, [64, 2, S_EXT]), ("qT", qT, [64, 4, S_OWN]), ("vv", vv, [128, NT_EXT, 2, 65]),
                                            ("kcmpT", kcmpT, [64, 256]), ("vca", vca, [128, 2, 65])):
                            o = dbg_t(nm, shp, BF16)
                            fw.dma(o, bf[0:shp[0]], reads=[bf], is_output=True)
                        o = dbg_t("gsig", [128, NT_OWN, 12])
                        fw.dma(o, gsig[:], reads=[gsig], is_output=True)
                    ckpt("B2")
                    with fw.scope() as esB3:
                        NP = 4
                        LA = 2
                        Pb = [fw.sb([128, 512], BF16, f"Pb{g}{i}", esB3) for i in range(NP)]
                        Sbank = [0, 1, 6, 7]
                        hbS = [fw.sb([128, 4, 132], F32, f"hbS{g}{r}", esB3) for r in range(3)]
                        ya = [fw.sb([128, 4, 64], F32, f"ya{g}{i}", esB3) for i in range(2)]
                        yat = [fw.sb([128, 4, 64], BF16, f"yat{g}{i}", esB3) for i in range(2)]
                        sms = [fw.sb([128, 16], F32, f"sm{g}{i}", esB3) for i in range(3)]
                        rdc = fw.sb([128, 4], F32, f"rdc{g}", esB3)
                        impv = fw.sb([128, 64], F32, f"impv{g}", esB3)
                        wk = fw.sb([128, 64], F32, f"wk{g}", esB3)
                        m8a = fw.sb([128, 8], F32, f"m8a{g}", esB3)
                        m8b = fw.sb([128, 8], F32, f"m8b{g}", esB3)
                        negm2 = fw.sb([128, 128], BF16, f"negm{g}", esB3)
                        op("pool", lambda e: e.memset(negm2[:, 0:64], 0.0), writes=[negm2])
                        rot = [0]
                        REG = {0: (0, 129), 1: (129, 65), 2: (194, 65)}

                        def score(c, lhsT, lreads, extra, bias, mask):
                            r = rot[0] % NP
                            rot[0] += 1
                            sb_i = Sbank[r]
                            P = Pb[r]
                            qrhs = qT[:, :, c * 128:(c + 1) * 128]
                            S3 = PS[sb_i][:, :].rearrange("p (h q) -> p h q", h=4)
                            mm(PS[sb_i], S3, lhsT, qrhs, True, True, lreads + [qT])
                            op("act", lambda e: e.activation(out=P[:], in_=PS[sb_i][:, :], func=AF.Exp, bias=bias[:], scale=0.125),
                               reads=[PS[sb_i], bias], writes=[P])
                            if mask is not None:
                                op("dve", lambda e: e.tensor_tensor(out=P[:].rearrange("p (h q) -> p h q", h=4), in0=P[:].rearrange("p (h q) -> p h q", h=4),
                                                                    in1=mask[0], op=ALU.mult), reads=[P, mask[1]], writes=[P])
                            return P

                        def pv(P, h, reg, vr, vreads, cc, n, first, last):
                            op("pe", lambda e: e.matmul(PS[2 + h][:, cc:cc + n], P[:, h * 128:(h + 1) * 128], vr, start=first, stop=last),
                               reads=[P] + vreads, writes=[PS[2 + h]])

                        def evac_all(c, reg, br, first, final, mid=None):
                            col0, n = REG[reg]
                            hs = hbS[reg]
                            for h in range(4):
                                op("dve", lambda e: e.tensor_copy(out=hs[:, h, 0:n], in_=PS[2 + h][:, col0:col0 + n]), reads=[PS[2 + h]], writes=[hs])
                            sm = sms[reg]
                            yac = ya[c % 2]
                            dn = sm[:, 0:4]
                            rd = sm[:, 4:8] if br != 0 else rdc[:, 0:4]
                            rdb = sm if br != 0 else rdc
                            cf = sm[:, 8:12]
                            op("dve", lambda e: e.tensor_scalar(out=dn.unsqueeze(2), in0=hs[:, :, 64:65], scalar1=1e-30, scalar2=None, op0=ALU.max),
                               reads=[hs], writes=[sm])
                            op("dve", lambda e: e.reciprocal(out=rd, in_=dn), reads=[sm], writes=[rdb])
                            if mid is not None:
                                mid()
                            op("dve", lambda e: e.tensor_tensor(out=cf.unsqueeze(2), in0=rd.unsqueeze(2),
                                                                in1=gsig[:, c, :].rearrange("p (h b) -> p h b", b=3)[:, :, br:br + 1], op=ALU.mult),
                               reads=[sm, rdb, gsig], writes=[sm])
                            cfb = cf.unsqueeze(2).to_broadcast([128, 4, 64])
                            if first:
                                op("dve", lambda e: e.tensor_tensor(out=yac[:], in0=hs[:, :, 0:64], in1=cfb, op=ALU.mult), reads=[hs, sm], writes=[yac])
                            else:
                                op("dve", lambda e: e.tensor_tensor(out=hs[:, :, 0:64], in0=hs[:, :, 0:64], in1=cfb, op=ALU.mult), reads=[hs, sm], writes=[hs])
                                dst = yat[c % 2] if final else yac
                                op("dve", lambda e: e.tensor_tensor(out=dst[:], in0=hs[:, :, 0:64], in1=yac[:], op=ALU.add), reads=[hs, yac], writes=[dst])

                        pend = []

                        def flush():
                            while pend:
                                pend.pop(0)()

                        def pipe(score_fn, pv_fn):
                            P = score_fn()
                            while len(pend) >= LA:
                                pend.pop(0)()
                            pend.append(lambda: pv_fn(P))

                        def tr_slot():
                            r = rot[0] % NP
                            rot[0] += 1
                            return Sbank[r]

                        def cmp_scores_pv(c):
                            Pc = []
                            for nch in range(2):
                                mk = cmask[:, nch, c * 128:(c + 1) * 128].unsqueeze(1).to_broadcast([128, 4, 128])
                                Pc.append(score(c, kcmpT[:, nch * 128:(nch + 1) * 128], [kcmpT], None, hbias if nch == 0 else c_zero, (mk, cmask)))
                            flush()
                            for h in range(4):
                                for nch in range(2):
                                    pv(Pc[nch], h, 0, vca[:, nch, :], [vca], 0, 65, nch == 0, nch == 1)
                                for nch in range(2):
                                    pv(Pc[nch], h, 0, ovl[:, nch, :], [ovl], 65, 64, nch == 0, nch == 1)

                        def cmp_evac_topk(c):
                            def topk_chain():
                                op("dve", lambda e: e.tensor_tensor(out=hbS[0][:, :, 65:129], in0=hbS[0][:, :, 65:129], in1=rdc[:, 0:4].unsqueeze(2).to_broadcast([128, 4, 64]), op=ALU.mult),
                                   reads=[hbS[0], rdc], writes=[hbS[0]])
                                op("dve", lambda e: e.tensor_reduce(out=impv[:], in_=hbS[0][:, :, 65:129].rearrange("p h s -> p s h"), axis=AX.X, op=ALU.add),
                                   reads=[hbS[0]], writes=[impv])
                                op("dve", lambda e: e.tensor_tensor(out=impv[:], in0=impv[:], in1=maskadd[:, c, :], op=ALU.add), reads=[impv, maskadd], writes=[impv])
                                op("dve", lambda e: e.max(out=m8a[:], in_=impv[:]), reads=[impv], writes=[m8a])
                                op("dve", lambda e: e.match_replace(out=wk[:], in_to_replace=m8a[:], in_values=impv[:], imm_value=-3.0e38),
                                   reads=[impv, m8a], writes=[wk])
                                op("dve", lambda e: e.max(out=m8b[:], in_=wk[:]), reads=[wk], writes=[m8b])
                                op("dve", lambda e: e.tensor_scalar(out=negm2[:, 64:128], in0=impv[:], scalar1=m8b[:, 7:8], scalar2=NEGB, op0=ALU.is_lt, op1=ALU.mult),
                                   reads=[impv, m8b, negm2], writes=[negm2])
                            evac_all(c, 0, 0, True, False, mid=topk_chain)

                        def negm_to_q(c):
                            bk = tr_slot()
                            op("pe", lambda e: e.transpose(out=psbf(bk)[:, 0:128], in_=negm2[:, :], identity=idb[:]), reads=[negm2, idb], writes=[PS[bk]])
                            op("act", lambda e: e.copy(out=qT[64:128, :, c * 128:(c + 1) * 128], in_=psbf(bk)[64:128, 0:128].unsqueeze(1).to_broadcast([64, 4, 128])),
                               reads=[PS[bk]], writes=[qT])

                        def finish_tile(cp):
                            evac_all(cp, 1, 1, False, True)
                            bk = tr_slot()
                            for j in range(2):
                                op("pe", lambda e: e.transpose(out=psbf(bk)[:, j * 128:(j + 1) * 128],
                                                               in_=yat[cp % 2][:, 2 * j:2 * j + 2, :].rearrange("p h d -> p (h d)"), identity=idb[:]),
                                   reads=[yat[cp % 2], idb], writes=[PS[bk]])
                            op("act", lambda e: e.copy(out=YT[:, 2 * g:2 * g + 2, cp * 128:(cp + 1) * 128],
                                                       in_=psbf(bk)[:, 0:256].rearrange("p (j t) -> p j t", j=2)), reads=[PS[bk]], writes=[YT])

                        cmp_scores_pv(0)
                        cmp_evac_topk(0)
                        negm_to_q(0)
                        for c in range(NT_OWN):
                            for j in range(5):
                                ch = NT_OWN + c - 4 + j
                                mk = None
                                if j == 0:
                                    mk = (wm0[:].unsqueeze(1).to_broadcast([128, 4, 128]), wm0)
                                elif j == 4:
                                    mk = (caus[:].unsqueeze(1).to_broadcast([128, 4, 128]), caus)

                                def sfn(ch=ch, mk=mk):
                                    return score(c, kkT[:, 1, ch * 128:(ch + 1) * 128], [kkT], None, hbias if ch < NT_OWN else c_zero, mk)

                                def pfn(P, ch=ch, j=j):
                                    for h in range(4):
                                        pv(P, h, 2, vv[:, ch, 1, :], [vv], 194, 65, j == 0, j == 4)
                                pipe(sfn, pfn)
                                if j == 1 and c > 0:
                                    finish_tile(c - 1)
                            flush()
                            evac_all(c, 2, 2, False, False)
                            if c + 1 < NT_OWN:
                                cmp_scores_pv(c + 1)
                            chs = list(range(NT_OWN)) + [NT_OWN + j for j in range(c + 1)]
                            for i, ch in enumerate(chs):
                                mk = None
                                if ch == NT_OWN + c:
                                    mk = (caus[:].unsqueeze(1).to_broadcast([128, 4, 128]), caus)

                                def sfn(ch=ch, mk=mk):
                                    return score(c, kkT[:, 0, ch * 128:(ch + 1) * 128], [kkT], None, hbias if ch < NT_OWN else c_zero, mk)

                                def pfn(P, ch=ch, i=i, n=len(chs)):
                                    for h in range(4):
                                        pv(P, h, 1, vv[:, ch, 0, :], [vv], 129, 65, i == 0, i == n - 1)
                                pipe(sfn, pfn)
                                if c + 1 < NT_OWN:
                                    if i == 2:
                                        cmp_evac_topk(c + 1)
                                    elif i == 10:
                                        negm_to_q(c + 1)
                        flush()
                        finish_tile(NT_OWN - 1)
            esBc.__exit__(None, None, None)
            ckpt("B")
            with fw.scope() as esCg:
                ee = fw.sb([128, NT_EXT, 4], F32, "ee", esCg)
                ff = fw.sb([128, NT_EXT, 4], F32, "ff", esCg)
                fl = fw.sb([128, NT_EXT, 4], F32, "fl", esCg)
                ghn = fw.sb([128, 512], F32, "ghn", esCg)
                fw.dma(ghn[:], g_hn_d[0:1, :].to_broadcast([128, 512]), writes=[ghn])
                wcs = fw.sb([128, 8, 4], F32, "wcs", esCg)
                fw.dma(wcs[:], wc_d[:, :, :], writes=[wcs])
                bcs = fw.sb([128, 8], F32, "bcs", esCg)
                fw.dma(bcs[:], bc_d[:, :], writes=[bcs])
                with fw.scope() as esg:
                    w_if = fw.sb([128, 8, 8], BF16, "w_if", esg)
                    fw.dma(w_if[:], w_if_d.rearrange("(k p) c -> p k c", p=128), writes=[w_if], q="pool")
                    bif = fw.sb([128, 8], F32, "bif", esg)
                    fw.dma(bif[:], b_if_d[0:1, :].to_broadcast([128, 8]), writes=[bif])
                    hblk = [fw.sb([128, 8, 512], BF16, f"hblkG{i}", esg) for i in range(2)]
                    ifp = fw.sb([128, NT_EXT, 8], F32, "ifp", esg)
                    l1 = fw.sb([128, NT_EXT, 4], F32, "l1", esg)
                    tmpg = fw.sb([128, NT_EXT, 4], F32, "tmpg", esg)
                    for t in range(NT_EXT):
                        hb = hblk[(t // 4) % 2]
                        if t % 4 == 0:
                            fw.dma(hb[:], hT_d[:, :, t * 128:(t + 4) * 128], reads=hT_tiles[t:t + 4], writes=[hb])
                        tl = t % 4
                        for k in range(8):
                            mm(PS[0], PS[0][:, t * 8:(t + 1) * 8], hb[:, k, tl * 128:(tl + 1) * 128], w_if[:, k, :], k == 0, k == 7, [hb, w_if])
                    op("act", lambda e: e.copy(out=ifp[:], in_=PS[0][:, 0:256].rearrange("p (t c) -> p t c", c=8)), reads=[PS[0]], writes=[ifp])
                    op("dve", lambda e: e.tensor_tensor(out=ifp[:], in0=ifp[:], in1=bif[:].unsqueeze(1).to_broadcast([128, NT_EXT, 8]), op=ALU.add),
                       reads=[ifp, bif], writes=[ifp])
                    op("act", lambda e: e.activation(out=l1[:], in_=ifp[:, :, 4:8], func=AF.Exp, scale=-1.0), reads=[ifp], writes=[l1])
                    op("act", lambda e: e.activation(out=l1[:], in_=l1[:], func=AF.Ln, bias=c_one[:]), reads=[l1, c_one], writes=[l1])
                    l1f = l1[:].rearrange("p t c -> p (t c)")
                    mm(PS[1], PS[1][:, 0:128], U_f[:], l1f, True, True, [U_f, l1])
                    mm(PS[1], PS[1][:, 128:256], ones_f[:], l1f, True, True, [ones_f, l1])
                    op("act", lambda e: e.copy(out=tmpg[:], in_=PS[1][:, 0:128].rearrange("p (t c) -> p t c", c=4)), reads=[PS[1]], writes=[tmpg])
                    op("act", lambda e: e.activation(out=ff[:], in_=tmpg[:], func=AF.Exp, scale=-1.0), reads=[tmpg], writes=[ff])
                    op("act", lambda e: e.activation(out=fl[:], in_=PS[1][:, 128:256].rearrange("p (t c) -> p t c", c=4), func=AF.Exp, scale=-1.0),
                       reads=[PS[1]], writes=[fl])
                    op("dve", lambda e: e.tensor_tensor(out=tmpg[:], in0=tmpg[:], in1=ifp[:, :, 0:4], op=ALU.add), reads=[tmpg, ifp], writes=[tmpg])
                    op("act", lambda e: e.activation(out=ee[:], in_=tmpg[:], func=AF.Exp), reads=[tmpg], writes=[ee])
                    op("dve", lambda e: e.tensor_scalar(out=ee[:, 0:NT_OWN, :], in0=ee[:, 0:NT_OWN, :], scalar1=hv[:, 0:1], scalar2=None, op0=ALU.mult),
                       reads=[ee, hv], writes=[ee])
                ckpt("Cg")
                qTb = fw.sb([128, 4, S_OWN], BF16, "qTb", esCg)
                kTb = fw.sb([128, 4, S_EXT], BF16, "kTb", esCg)
                vaug = fw.sb([128, NT_EXT, 4, 129], BF16, "vaug", esCg)
                osig = fw.sb([128, NT_OWN, 512], BF16, "osig", esCg)
                op("pool", lambda e: e.memset(vaug[:, :, :, 128:129], 1.0), writes=[vaug])
                for hp in range(2):
                    with fw.scope() as esC1:
                        wq = fw.sb([128, 8, 256], BF16, f"wq{hp}", esC1)
                        wk = fw.sb([128, 8, 256], BF16, f"wk{hp}", esC1)
                        wv = fw.sb([128, 8, 256], BF16, f"wv{hp}", esC1)
                        wo = fw.sb([128, 8, 256], BF16, f"wo{hp}", esC1)
                        fw.dma(wq[:], w_qk_d[:, hp * 256:(hp + 1) * 256].rearrange("(k p) c -> p k c", p=128), writes=[wq], q="pool")
                        fw.dma(wk[:], w_qk_d[:, 512 + hp * 256:512 + (hp + 1) * 256].rearrange("(k p) c -> p k c", p=128), writes=[wk], q="pool")
                        fw.dma(wv[:], w_vo_d[:, hp * 256:(hp + 1) * 256].rearrange("(k p) c -> p k c", p=128), writes=[wv], q="pool")
                        fw.dma(wo[:], w_vo_d[:, 512 + hp * 256:512 + (hp + 1) * 256].rearrange("(k p) c -> p k c", p=128), writes=[wo], q="pool")
                        hblk = [fw.sb([128, 8, 512], BF16, f"hblkC{hp}{i}", esC1) for i in range(2)]
                        uk = [fw.sb([128, 4 + S_EXT], BF16, f"uk{hp}{i}", esC1) for i in range(2)]
                        uq = [fw.sb([128, 4 + 2560], BF16, f"uq{hp}{i}", esC1) for i in range(2)]
                        ycv = [fw.sb([128, 512], F32, f"ycv{hp}{i}", esC1) for i in range(2)]
                        sgm = [fw.sb([128, 512], F32, f"sgm{hp}{i}", esC1) for i in range(2)]
                        for hh in range(2):
                            op("pool", lambda e: e.memset(uk[hh][:, 0:4], 0.0), writes=[uk[hh]])
                            op("pool", lambda e: e.memset(uq[hh][:, 0:4], 0.0), writes=[uq[hh]])
                        for blk in range(8):
                            hb = hblk[blk % 2]
                            fw.dma(hb[:], hT_d[:, :, blk * 512:(blk + 1) * 512], reads=hT_tiles[4 * blk:4 * blk + 4], writes=[hb])
                            for hh in range(2):
                                for k in range(8):
                                    mm(PS[hh], PS[hh][:, :], wk[:, k, hh * 128:(hh + 1) * 128], hb[:, k, :], k == 0, k == 7, [wk, hb])
                                op("act", lambda e: e.copy(out=uk[hh][:, 4 + blk * 512:4 + (blk + 1) * 512], in_=PS[hh][:, :]), reads=[PS[hh]], writes=[uk[hh]])
                            if blk >= 3:
                                for hh in range(2):
                                    for k in range(8):
                                        mm(PS[2 + hh], PS[2 + hh][:, :], wq[:, k, hh * 128:(hh + 1) * 128], hb[:, k, :], k == 0, k == 7, [wq, hb])
                                    op("act", lambda e: e.copy(out=uq[hh][:, 4 + (blk - 3) * 512:4 + (blk - 2) * 512], in_=PS[2 + hh][:, :]),
                                       reads=[PS[2 + hh]], writes=[uq[hh]])
                            for tl in range(4):
                                t = blk * 4 + tl
                                bv = 4 + tl % 2
                                for k in range(8):
                                    mm(PS[bv], PS[bv][:, 0:256], hb[:, k, tl * 128:(tl + 1) * 128], wv[:, k, :], k == 0, k == 7, [wv, hb])
                                op("dve", lambda e: e.tensor_copy(out=vaug[:, t, 2 * hp:2 * hp + 2, 0:128], in_=PS[bv][:, 0:256].rearrange("p (h d) -> p h d", d=128)),
                                   reads=[PS[bv]], writes=[vaug])
                                if blk >= 4:
                                    bo = 6 + tl % 2
                                    for k in range(8):
                                        mm(PS[bo], PS[bo][:, 0:256], hb[:, k, tl * 128:(tl + 1) * 128], wo[:, k, :], k == 0, k == 7, [wo, hb])
                                    op("act", lambda e: e.activation(out=osig[:, t - NT_OWN, hp * 256:(hp + 1) * 256], in_=PS[bo][:, 0:256], func=AF.Sigmoid),
                                       reads=[PS[bo]], writes=[osig])
                        pi = 0
                        for hh in range(2):
                            H = 2 * hp + hh
                            for typ in range(2):
                                ci = typ * 4 + H
                                npiece = 4 if typ == 0 else 8
                                u = uq[hh] if typ == 0 else uk[hh]
                                for pc in range(npiece):
                                    off = (4 + 512 + pc * 512) if typ == 0 else (4 + pc * 512)
                                    y_ = ycv[pi % 2]
                                    s_ = sgm[pi % 2]
                                    pi += 1
                                    op("dve", lambda e: e.tensor_scalar(out=y_[:], in0=u[:, off - 3:off - 3 + 512], scalar1=wcs[:, ci, 0:1], scalar2=bcs[:, ci:ci + 1],
                                                                        op0=ALU.mult, op1=ALU.add), reads=[u, wcs, bcs], writes=[y_])
                                    for j in range(1, 4):
                                        op("dve", lambda e: e.scalar_tensor_tensor(out=y_[:], in0=u[:, off - 3 + j:off - 3 + j + 512], scalar=wcs[:, ci, j:j + 1], in1=y_[:],
                                                                                   op0=ALU.mult, op1=ALU.add), reads=[u, wcs, y_], writes=[y_])
                                    if typ == 0:
                                        op("act", lambda e: e.activation(out=qTb[:, 2 * hp + hh, pc * 512:(pc + 1) * 512], in_=y_[:], func=AF.Silu), reads=[y_], writes=[qTb])
                                    else:
                                        op("act", lambda e: e.activation(out=s_[:], in_=y_[:], func=AF.Sigmoid), reads=[y_], writes=[s_])
                                        op("dve", lambda e: e.scalar_tensor_tensor(out=kTb[:, 2 * hp + hh, pc * 512:(pc + 1) * 512], in0=y_[:], scalar=128.0 ** -0.5, in1=s_[:],
                                                                                   op0=ALU.mult, op1=ALU.mult), reads=[y_, s_], writes=[kTb])
                ckpt("C1")
                with fw.scope() as esC3:
                    ktokR = [fw.sb([128, 4, 128], BF16, f"ktokR{i}", esC3) for i in range(3)]
                    CTall = fw.sb([128, NT_OWN, 4, 129], BF16, "CTall", esC3)
                    Xs = [fw.sb([128, 129], F32, f"Xs{H}", esC3) for H in range(4)]
                    Sm = [[fw.sb([128, 128], BF16, f"Sm{H}{i}", esC3) for i in range(2)] for H in range(4)]
                    hm_ = [fw.sb([128, 128], F32, f"hm{H}", esC3) for H in range(4)]
                    yb_ = [fw.sb([128, 128], BF16, f"yb{H}", esC3) for H in range(4)]
                    jk = [fw.sb([128, 128], BF16, f"jk{H}", esC3) for H in range(4)]
                    smc = [fw.sb([128, 8], F32, f"smc{H}", esC3) for H in range(4)]
                    for H in range(4):
                        op("dve", lambda e: e.tensor_tensor(out=vaug[:, :, H, :], in0=vaug[:, :, H, :],
                                                            in1=ee[:, :, H:H + 1].to_broadcast([128, NT_EXT, 129]), op=ALU.mult), reads=[vaug, ee], writes=[vaug])

                    def k_tr(t):
                        bk = t % 2
                        for H in range(4):
                            op("pe", lambda e: e.transpose(out=psbf(bk)[:, H * 128:(H + 1) * 128], in_=kTb[:, H, t * 128:(t + 1) * 128], identity=idb[:]),
                               reads=[kTb, idb], writes=[PS[bk]])
                        op("act", lambda e: e.copy(out=ktokR[t % 3][:], in_=psbf(bk)[:, 0:512].rearrange("p (h d) -> p h d", d=128)), reads=[PS[bk]], writes=[ktokR[t % 3]])

                    k_tr(0)
                    for t in range(NT_EXT - 1):
                        if t + 1 < NT_EXT - 1:
                            k_tr(t + 1)
                        for H in range(4):
                            bU = 2 + H
                            mm(PS[bU], PS[bU][:, 0:129], ktokR[t % 3][:, H, :], vaug[:, t, H, :], True, True, [ktokR[t % 3], vaug])
                            if t == 0:
                                op("dve", lambda e: e.tensor_copy(out=Xs[H][:], in_=PS[bU][:, 0:129]), reads=[PS[bU]], writes=[Xs[H]])
                            else:
                                op("dve", lambda e: e.scalar_tensor_tensor(out=Xs[H][:], in0=Xs[H][:], scalar=fl[:, t - 1, H:H + 1], in1=PS[bU][:, 0:129],
                                                                           op0=ALU.mult, op1=ALU.add), reads=[Xs[H], fl, PS[bU]], writes=[Xs[H]])
                            if t + 1 >= NT_OWN:
                                op("act", lambda e: e.activation(out=CTall[:, t + 1 - NT_OWN, H, :], in_=Xs[H][:], func=AF.Copy, scale=fl[:, t, H:H + 1]),
                                   reads=[Xs[H], fl], writes=[CTall])
                    sc4 = fw.sb([128, 4, 8], F32, "sc4", esC3)

                    def stA(t):
                        tq = t - NT_OWN
                        for H in range(4):
                            sm_ = Sm[H][tq % 2]
                            mm(PS[H], PS[H][:, 0:128], kTb[:, H, t * 128:(t + 1) * 128], qTb[:, H, tq * 128:(tq + 1) * 128], True, True, [kTb, qTb])
                            op("dve", lambda e: e.tensor_tensor(out=sm_[:], in0=PS[H][:, 0:128], in1=caus[:], op=ALU.mult), reads=[PS[H], caus], writes=[sm_])

                    def stRest(t):
                        tq = t - NT_OWN
                        for H in range(4):
                            sm_ = Sm[H][tq % 2]
                            bA = 4 + H
                            mm(PS[bA], PS[bA][:, 0:129], sm_[:], vaug[:, t, H, :], True, False, [sm_, vaug])
                            mm(PS[bA], PS[bA][:, 0:129], qTb[:, H, tq * 128:(tq + 1) * 128], CTall[:, tq, H, :], False, True, [qTb, CTall])
                        for H in range(4):
                            op("act", lambda e: e.activation(out=sc4[:, H, 6:7], in_=PS[4 + H][:, 128:129], func=AF.Abs, scale=ff[:, t, H:H + 1]),
                               reads=[PS[4 + H], ff], writes=[sc4])
                        op("dve", lambda e: e.tensor_scalar(out=sc4[:, :, 0:1], in0=sc4[:, :, 6:7], scalar1=1.0, scalar2=None, op0=ALU.max), reads=[sc4], writes=[sc4])
                        op("dve", lambda e: e.reciprocal(out=sc4[:, :, 1:2], in_=sc4[:, :, 0:1]), reads=[sc4], writes=[sc4])
                        op("dve", lambda e: e.tensor_tensor(out=sc4[:, :, 2:3], in0=sc4[:, :, 1:2], in1=ff[:, t, :].unsqueeze(2), op=ALU.mult), reads=[sc4, ff], writes=[sc4])
                        for H in range(4):
                            op("dve", lambda e: e.scalar_tensor_tensor(out=hm_[H][:], in0=PS[4 + H][:, 0:128], scalar=sc4[:, H, 2:3], in1=osig[:, tq, H * 128:(H + 1) * 128],
                                                                       op0=ALU.mult, op1=ALU.mult), reads=[PS[4 + H], sc4, osig], writes=[hm_[H]])
                        for H in range(4):
                            op("act", lambda e: e.activation(out=jk[H][:], in_=hm_[H][:], func=AF.Square, accum_out=sc4[:, H, 3:4]), reads=[hm_[H]], writes=[jk[H], sc4])
                        op("act", lambda e: e.activation(out=sc4[:, :, 4:5], in_=sc4[:, :, 3:4], func=AF.Sqrt, bias=c_eps[:], scale=1.0 / 128), reads=[sc4, c_eps], writes=[sc4])
                        op("dve", lambda e: e.reciprocal(out=sc4[:, :, 5:6], in_=sc4[:, :, 4:5]), reads=[sc4], writes=[sc4])
                        for H in range(4):
                            op("dve", lambda e: e.scalar_tensor_tensor(out=yb_[H][:], in0=hm_[H][:], scalar=sc4[:, H, 5:6], in1=ghn[:, H * 128:(H + 1) * 128],
                                                                       op0=ALU.mult, op1=ALU.mult), reads=[hm_[H], sc4, ghn], writes=[yb_[H]])
                        for H in range(4):
                            op("pe", lambda e: e.transpose(out=psbf(4 + H)[:, 512:640], in_=yb_[H][:], identity=idb[:]), reads=[yb_[H], idb], writes=[PS[4 + H]])
                        for H in range(4):
                            op("act", lambda e: e.copy(out=YT[:, 4 + H, tq * 128:(tq + 1) * 128], in_=psbf(4 + H)[:, 512:640]), reads=[PS[4 + H]], writes=[YT])

                    stA(NT_OWN)
                    for t in range(NT_OWN, NT_EXT):
                        if t + 1 < NT_EXT:
                            stA(t + 1)
                        stRest(t)
            ckpt("C")
            if "ybT" in dbg:
                o = dbg_t("ybT", [128, 4, S_OWN], BF16)
                fw.dma(o[:, :, :], YT[:, 4:8, :], reads=[YT], is_output=True)


            with fw.scope() as esD:
                x1 = fw.sb([128, NT_OWN, D], F32, "x1", esD)
                with fw.scope() as esD1:
                    mixT = fw.sb([128, 8, S_OWN], BF16, "mixT", esD1)
                    with fw.scope() as esD1a:
                        hTo = fw.sb([128, 8, S_OWN], BF16, "hTo", esD1a)
                        for tb in range(4):
                            fw.dma(hTo[:, :, tb * 512:(tb + 1) * 512], hT_d[:, :, S_OWN + tb * 512:S_OWN + (tb + 1) * 512],
                                   reads=hT_tiles[NT_OWN + 4 * tb:NT_OWN + 4 * tb + 4], writes=[hTo])
                        wga = [fw.sb([128, 8, 128], BF16, f"wga{i}", esD1a) for i in range(2)]
                        wgb = [fw.sb([128, 8, 128], BF16, f"wgb{i}", esD1a) for i in range(2)]
                        wpa = [fw.sb([128, 4, 128], BF16, f"wpa{i}", esD1a) for i in range(2)]
                        wpb = [fw.sb([128, 4, 128], BF16, f"wpb{i}", esD1a) for i in range(2)]
                        sga = [fw.sb([128, 512], BF16, f"sga{i}", esD1a) for i in range(2)]
                        sgb = [fw.sb([128, 512], BF16, f"sgb{i}", esD1a) for i in range(2)]
                        t1 = [fw.sb([128, 512], F32, f"t1_{i}", esD1a) for i in range(2)]
                        t2 = [fw.sb([128, 512], F32, f"t2_{i}", esD1a) for i in range(2)]
                        it = 0
                        for j in range(8):
                            w_ = j % 2
                            fw.dma(wga[w_][:], w_mg_d[:, j * 128:(j + 1) * 128].rearrange("(k p) c -> p k c", p=128), writes=[wga[w_]], q="pool")
                            fw.dma(wgb[w_][:], w_mg_d[:, 1024 + j * 128:1024 + (j + 1) * 128].rearrange("(k p) c -> p k c", p=128), writes=[wgb[w_]], q="pool")
                            fw.dma(wpa[w_][:], w_pa_d[:, j * 128:(j + 1) * 128].rearrange("(k p) c -> p k c", p=128), writes=[wpa[w_]], q="pool")
                            fw.dma(wpb[w_][:], w_pb_d[:, j * 128:(j + 1) * 128].rearrange("(k p) c -> p k c", p=128), writes=[wpb[w_]], q="pool")
                            for tb in range(4):
                                r = it % 2
                                it += 1
                                b0 = 4 * r
                                ts_ = slice(tb * 512, (tb + 1) * 512)
                                for k in range(8):
                                    mm(PS[b0], PS[b0][:, :], wga[w_][:, k, :], hTo[:, k, ts_], k == 0, k == 7, [wga[w_], hTo])
                                op("act", lambda e: e.activation(out=sga[r][:], in_=PS[b0][:, :], func=AF.Sigmoid), reads=[PS[b0]], writes=[sga[r]])
                                for k in range(8):
                                    mm(PS[b0 + 1], PS[b0 + 1][:, :], wgb[w_][:, k, :], hTo[:, k, ts_], k == 0, k == 7, [wgb[w_], hTo])
                                op("act", lambda e: e.activation(out=sgb[r][:], in_=PS[b0 + 1][:, :], func=AF.Sigmoid), reads=[PS[b0 + 1]], writes=[sgb[r]])
                                for k in range(4):
                                    mm(PS[b0 + 2], PS[b0 + 2][:, :], wpa[w_][:, k, :], YT[:, k, ts_], k == 0, k == 3, [wpa[w_], YT])
                                for k in range(4):
                                    mm(PS[b0 + 3], PS[b0 + 3][:, :], wpb[w_][:, k, :], YT[:, 4 + k, ts_], k == 0, k == 3, [wpb[w_], YT])
                                op("dve", lambda e: e.tensor_tensor(out=t1[r][:], in0=PS[b0 + 2][:, :], in1=sga[r][:], op=ALU.mult), reads=[PS[b0 + 2], sga[r]], writes=[t1[r]])
                                op("dve", lambda e: e.tensor_tensor(out=t2[r][:], in0=PS[b0 + 3][:, :], in1=sgb[r][:], op=ALU.mult), reads=[PS[b0 + 3], sgb[r]], writes=[t2[r]])
                                op("pool", lambda e: e.tensor_tensor(out=mixT[:, j, ts_], in0=t1[r][:], in1=t2[r][:], op=ALU.add), reads=[t1[r], t2[r]], writes=[mixT])
                    ckpt("D1a")
                    with fw.scope() as esD1b:
                        w_out = fw.sb([128, 8, D], BF16, "w_out", esD1b)
                        fw.dma(w_out[:], w_out_d.rearrange("(k p) c -> p k c", p=128), writes=[w_out], q="pool")
                        xtl = [fw.sb([128, D], F32, f"xtl{i}", esD1b) for i in range(2)]
                        for t in range(NT_OWN):
                            x_ = xtl[t % 2]
                            fw.dma(x_[:], xe[S_OWN + t * 128:S_OWN + (t + 1) * 128, :], writes=[x_])
                            for half in range(2):
                                b = 2 * (t % 2) + half
                                for j in range(8):
                                    mm(PS[b], PS[b][:, :], mixT[:, j, t * 128:(t + 1) * 128], w_out[:, j, half * 512:(half + 1) * 512], j == 0, j == 7, [mixT, w_out])
                                op("dve", lambda e: e.tensor_tensor(out=x1[:, t, half * 512:(half + 1) * 512], in0=PS[b][:, :], in1=x_[:, half * 512:(half + 1) * 512], op=ALU.add),
                                   reads=[PS[b], x_], writes=[x1])
                ckpt("D1")
                if "x1" in dbg:
                    fw.dma(dbg_t("x1", [128, NT_OWN, D]), x1[:], reads=[x1], is_output=True)
                with fw.scope() as esM:
                    load_gain(1)
                    gateT = fw.sb([16, S_OWN], BF16, "gateT", esM)
                    E16 = fw.sb([16, 16, 128], BF16, "E16", esM)
                    op("pool", lambda e: e.memset(E16[:], 1.0), writes=[E16])
                    op("pool", lambda e: e.affine_select(out=E16[:], in_=E16[:], pattern=[[-1, 16], [0, 128]], compare_op=ALU.is_equal, fill=0.0,
                                                         base=0, channel_multiplier=1), reads=[E16], writes=[E16])
                    with fw.scope() as esR:
                        w_r = fw.sb([128, 8, 20], F32, "w_r", esR)
                        fw.dma(w_r[:], w_r_d.rearrange("(k p) c -> p k c", p=128), writes=[w_r])
                        b_r = fw.sb([128, 20], F32, "b_r", esR)
                        fw.dma(b_r[:], b_r_d[0:1, :].to_broadcast([128, 20]), writes=[b_r])
                        hnf = [fw.sb([128, D], F32, f"hnf{i}", esR) for i in range(2)]
                        hnTf = [fw.sb([128, 8, 128], F32, f"hnTf{i}", esR) for i in range(2)]
                        junkR = fw.sb([128, D], BF16, "junkR", esR)
                        ssr = [fw.sb([128, 1], F32, f"ssr{i}", esR) for i in range(2)]
                        rrr = [fw.sb([128, 1], F32, f"rrr{i}", esR) for i in range(2)]
                        T_ = NT_OWN
                        lgA = fw.sb([128, T_, 20], F32, "lgA", esR)

                        def r_front(t):
                            r = t % 2
                            rs = {"ss": ssr[r], "r": rrr[r]}
                            rms_rstd({"ap": x1[:, t, :], "bufs": [x1]}, rs, D, {"ap": junkR[:], "buf": junkR})
                            op("dve", lambda e: e.scalar_tensor_tensor(out=hnf[r][:], in0=x1[:, t, :], scalar=rs["r"][:], in1=gB[:], op0=ALU.mult, op1=ALU.mult),
                               reads=[x1, rs["r"], gB], writes=[hnf[r]])
                            for k in range(8):
                                b = 2 * r + (0 if k < 4 else 1)
                                op("pe", lambda e: e.transpose(out=PS[b][:, (k % 4) * 128:(k % 4 + 1) * 128], in_=hnf[r][:, k * 128:(k + 1) * 128], identity=idf[:]),
                                   reads=[hnf[r], idf], writes=[PS[b]])
                            for bb in range(2):
                                b = 2 * r + bb
                                op("act", lambda e: e.copy(out=hnTf[r][:, 4 * bb:4 * bb + 4, :], in_=PS[b][:, :].rearrange("p (k t) -> p k t", k=4)), reads=[PS[b]], writes=[hnTf[r]])
                                op("dve", lambda e: e.tensor_copy(out=YT[:, 4 * bb:4 * bb + 4, t * 128:(t + 1) * 128], in_=PS[b][:, :].rearrange("p (k t) -> p k t", k=4)),
                                   reads=[PS[b]], writes=[YT])

                        def r_back(t):
                            r = t % 2
                            bl = 4 + r
                            for k in range(8):
                                mm(PS[bl], PS[bl][:, 0:20], hnTf[r][:, k, :], w_r[:, k, :], k == 0, k == 7, [hnTf[r], w_r])
                            op("dve", lambda e: e.tensor_tensor(out=lgA[:, t, :], in0=PS[bl][:, 0:20], in1=b_r[:], op=ALU.add), reads=[PS[bl], b_r], writes=[lgA])

                        for t in range(T_ + 1):
                            if t < T_:
                                r_front(t)
                            if t >= 1:
                                r_back(t - 1)
                        gl = lgA[:, :, 0:4]
                        el = lgA[:, :, 4:20].rearrange("p t (g e) -> p t g e", g=4)
                        gmax = fw.sb([128, T_], F32, "gmax", esR)
                        g1h = fw.sb([128, T_, 4], F32, "g1h", esR)
                        exg = fw.sb([128, T_, 4], F32, "exg", esR)
                        pgs = fw.sb([128, T_], F32, "pgs", esR)
                        t16 = fw.sb([128, T_, 4, 4], F32, "t16", esR)
                        elg = fw.sb([128, T_, 4], F32, "elg", esR)
                        elg2 = fw.sb([128, T_, 4], F32, "elg2", esR)
                        ev1 = fw.sb([128, T_], F32, "ev1", esR)
                        ev2 = fw.sb([128, T_], F32, "ev2", esR)
                        mk1 = fw.sb([128, T_, 4], F32, "mk1", esR)
                        mk2 = fw.sb([128, T_, 4], F32, "mk2", esR)
                        w12 = fw.sb([128, 2, T_], F32, "w12", esR)
                        gig = fw.sb([128, T_, 4], F32, "gig", esR)
                        gate = fw.sb([128, T_, 4, 4], F32, "gate", esR)
                        B3 = [128, T_, 4]
                        op("dve", lambda e: e.tensor_reduce(out=gmax[:], in_=gl, axis=AX.X, op=ALU.max), reads=[lgA], writes=[gmax])
                        op("dve", lambda e: e.tensor_tensor(out=g1h[:], in0=gl, in1=gmax[:].unsqueeze(2).to_broadcast(B3), op=ALU.is_equal), reads=[lgA, gmax], writes=[g1h])
                        op("dve", lambda e: e.tensor_tensor(out=exg[:], in0=gl, in1=gmax[:].unsqueeze(2).to_broadcast(B3), op=ALU.subtract), reads=[lgA, gmax], writes=[exg])
                        op("act", lambda e: e.activation(out=exg[:], in_=exg[:], func=AF.Exp), reads=[exg], writes=[exg])
                        op("dve", lambda e: e.tensor_reduce(out=pgs[:], in_=exg[:], axis=AX.X, op=ALU.add), reads=[exg], writes=[pgs])
                        op("dve", lambda e: e.reciprocal(out=pgs[:], in_=pgs[:]), reads=[pgs], writes=[pgs])
                        op("dve", lambda e: e.tensor_tensor(out=t16[:], in0=el, in1=g1h[:].unsqueeze(3).to_broadcast([128, T_, 4, 4]), op=ALU.mult), reads=[lgA, g1h], writes=[t16])
                        op("dve", lambda e: e.tensor_reduce(out=elg[:], in_=t16[:].rearrange("p t g e -> p t e g"), axis=AX.X, op=ALU.add), reads=[t16], writes=[elg])
                        op("dve", lambda e: e.tensor_reduce(out=ev1[:], in_=elg[:], axis=AX.X, op=ALU.max), reads=[elg], writes=[ev1])
                        op("dve", lambda e: e.tensor_tensor(out=mk1[:], in0=elg[:], in1=ev1[:].unsqueeze(2).to_broadcast(B3), op=ALU.is_equal), reads=[elg, ev1], writes=[mk1])
                        op("dve", lambda e: e.scalar_tensor_tensor(out=elg2[:], in0=mk1[:], scalar=-1e30, in1=elg[:], op0=ALU.mult, op1=ALU.add), reads=[mk1, elg], writes=[elg2])
                        op("dve", lambda e: e.tensor_reduce(out=ev2[:], in_=elg2[:], axis=AX.X, op=ALU.max), reads=[elg2], writes=[ev2])
                        op("dve", lambda e: e.tensor_tensor(out=mk2[:], in0=elg2[:], in1=ev2[:].unsqueeze(2).to_broadcast(B3), op=ALU.is_equal), reads=[elg2, ev2], writes=[mk2])
                        op("dve", lambda e: e.tensor_tensor(out=w12[:, 0, :], in0=ev1[:], in1=ev2[:], op=ALU.subtract), reads=[ev1, ev2], writes=[w12])
                        op("act", lambda e: e.activation(out=w12[:, 0, :], in_=w12[:, 0, :], func=AF.Sigmoid), reads=[w12], writes=[w12])
                        op("dve", lambda e: e.tensor_scalar(out=w12[:, 1, :], in0=w12[:, 0, :], scalar1=-1.0, scalar2=1.0, op0=ALU.mult, op1=ALU.add), reads=[w12], writes=[w12])
                        op("dve", lambda e: e.tensor_tensor(out=w12[:], in0=w12[:], in1=pgs[:].unsqueeze(1).to_broadcast([128, 2, T_]), op=ALU.mult), reads=[w12, pgs], writes=[w12])
                        op("dve", lambda e: e.tensor_tensor(out=gig[:], in0=mk1[:], in1=w12[:, 0, :].unsqueeze(2).to_broadcast(B3), op=ALU.mult), reads=[mk1, w12], writes=[gig])
                        op("dve", lambda e: e.tensor_tensor(out=mk2[:], in0=mk2[:], in1=w12[:, 1, :].unsqueeze(2).to_broadcast(B3), op=ALU.mult), reads=[mk2, w12], writes=[mk2])
                        op("dve", lambda e: e.tensor_tensor(out=gig[:], in0=gig[:], in1=mk2[:], op=ALU.add), reads=[gig, mk2], writes=[gig])
                        op("dve", lambda e: e.tensor_tensor(out=gate[:], in0=g1h[:].unsqueeze(3).to_broadcast([128, T_, 4, 4]),
                                                            in1=gig[:].unsqueeze(2).to_broadcast([128, T_, 4, 4]), op=ALU.mult), reads=[g1h, gig], writes=[gate])
                        for t4 in range(T_ // 4):
                            bk = 6 + t4 % 2
                            for j in range(4):
                                t = t4 * 4 + j
                                op("pe", lambda e: e.transpose(out=PS[bk][0:16, j * 128:(j + 1) * 128], in_=gate[:, t, :, :].rearrange("p g e -> p (g e)"), identity=idf[:]),
                                   reads=[gate, idf], writes=[PS[bk]])
                            op("act", lambda e: e.copy(out=gateT[:, t4 * 512:(t4 + 1) * 512], in_=PS[bk][0:16, :]), reads=[PS[bk]], writes=[gateT])
                    ckpt("D2r")
                    if "gateT" in dbg:
                        fw.dma(dbg_t("gateT", [16, S_OWN], BF16), gateT[:], reads=[gateT], is_output=True)
                    with fw.scope() as esE:
                        NW = 3
                        w13 = [fw.sb([128, 8, 512], BF16, f"w13_{i}", esE) for i in range(NW)]
                        w2e = [fw.sb([128, 2, D], BF16, f"w2e_{i}", esE) for i in range(NW)]
                        sgE = [fw.sb([128, 512], F32, f"sgE{i}", esE) for i in range(2)]
                        tE = [fw.sb([128, 512], F32, f"tE{i}", esE) for i in range(2)]
                        actT = [[fw.sb([128, 512], BF16, f"actT{i}{fc}", esE) for fc in range(2)] for i in range(2)]
                        x1M = [Buf(x1.t, f"x1m_{t}") for t in range(NT_OWN)]
                        for b_ in x1M:
                            b_.lw = x1.lw
                            b_.rd = dict(x1.rd)
                        ybank = [4, 5, 7]
                        yi = [0]

                        def load_w(ex):
                            wb = ex % NW
                            fw.dma(w13[wb][:], w_e13_d[ex].rearrange("(k p) c -> p k c", p=128), writes=[w13[wb]], q="pool")
                            fw.dma(w2e[wb][:], w_e2_d[ex].rearrange("(k p) c -> p k c", p=128), writes=[w2e[wb]], q="pool")

                        def e_front_pe(it):
                            ex, tb = it // 4, it % 4
                            wb = ex % NW
                            ts_ = slice(tb * 512, (tb + 1) * 512)
                            mm(PS[6], PS[6][:, :], E16[:, ex, :], gateT[:, ts_], True, True, [E16, gateT])
                            for fc in range(2):
                                for k in range(8):
                                    mm(PS[fc], PS[fc][:, :], w13[wb][:, k, fc * 128:(fc + 1) * 128], YT[:, k, ts_], k == 0, k == 7, [w13[wb], YT])
                                for k in range(8):
                                    mm(PS[2 + fc], PS[2 + fc][:, :], w13[wb][:, k, 256 + fc * 128:256 + (fc + 1) * 128], YT[:, k, ts_], k == 0, k == 7, [w13[wb], YT])

                        def e_front_post(it):
                            r = it % 2
                            for fc in range(2):
                                op("act", lambda e: e.activation(out=sgE[fc][:], in_=PS[fc][:, :], func=AF.Silu), reads=[PS[fc]], writes=[sgE[fc]])
                                op("dve", lambda e: e.tensor_tensor(out=tE[fc][:], in0=PS[2 + fc][:, :], in1=sgE[fc][:], op=ALU.mult), reads=[PS[2 + fc], sgE[fc]], writes=[tE[fc]])
                                op("dve", lambda e: e.tensor_tensor(out=actT[r][fc][:], in0=PS[6][:, :], in1=tE[fc][:], op=ALU.mult), reads=[PS[6], tE[fc]], writes=[actT[r][fc]])

                        def e_back(it):
                            ex, tb = it // 4, it % 4
                            wb = ex % NW
                            r = it % 2
                            for tt in range(4):
                                t = tb * 4 + tt
                                for half in range(2):
                                    b = ybank[yi[0] % 3]
                                    yi[0] += 1
                                    for fc in range(2):
                                        mm(PS[b], PS[b][:, :], actT[r][fc][:, tt * 128:(tt + 1) * 128], w2e[wb][:, fc, half * 512:(half + 1) * 512], fc == 0, fc == 1, [actT[r][fc], w2e[wb]])
                                    op("dve", lambda e: e.tensor_tensor(out=x1[:, t, half * 512:(half + 1) * 512], in0=PS[b][:, :], in1=x1[:, t, half * 512:(half + 1) * 512], op=ALU.add),
                                       reads=[PS[b], x1M[t]], writes=[x1M[t]])

                        load_w(0)
                        load_w(1)
                        NIT = 64
                        for it in range(NIT + 1):
                            if it < NIT:
                                e_front_pe(it)
                                e_front_post(it)
                            if it >= 1:
                                e_back(it - 1)
                            if it < NIT and it % 4 == 0 and it // 4 + 2 < 16:
                                load_w(it // 4 + 2)
                        for b_ in x1M:
                            if b_.lw is not None and (x1.lw is None or True):
                                pass
                        x1.lw = None
                        x1.rd = {}
                        fw.barrier()
                ckpt("D2")
                if "x2" in dbg:
                    fw.dma(dbg_t("x2", [128, NT_OWN, D]), x1[:], reads=[x1], is_output=True)
                with fw.scope() as esP:
                    load_gain(2)
                    gB2 = fw.sb([128, D], F32, "gB2", esP)
                    fw.dma(gB2[:], gvec_d[3:4, :].to_broadcast([128, D]), writes=[gB2])
                    w_pg = fw.sb([128, 8, D], BF16, "w_pg", esP)
                    fw.dma(w_pg[:], w_pg_d.rearrange("(k p) c -> p k c", p=128), writes=[w_pg], q="pool")
                    w_pp = fw.sb([128, 2, D], BF16, "w_pp", esP)
                    fw.dma(w_pp[:], w_pp_d.rearrange("(k p) c -> p k c", p=128), writes=[w_pp], q="pool")
                    hpb = [fw.sb([128, D], BF16, f"hpb{i}", esP) for i in range(3)]
                    hpT = [fw.sb([128, 8, 128], BF16, f"hpT{i}", esP) for i in range(3)]
                    plb = [fw.sb([128, 256], BF16, f"plb{i}", esP) for i in range(3)]
                    plT = [fw.sb([128, 2, 128], BF16, f"plT{i}", esP) for i in range(3)]
                    junkP2 = fw.sb([128, D], BF16, "junkP2", esP)
                    sgP = [fw.sb([128, 512], F32, f"sgP{i}", esP) for i in range(2)]
                    tP = [fw.sb([128, 512], F32, f"tP{i}", esP) for i in range(2)]
                    outt = [fw.sb([128, D], F32, f"outt{i}", esP) for i in range(2)]
                    junkP = fw.sb([128, D], BF16, "junkP", esP)
                    ssp = [fw.sb([128, 1], F32, f"ssp{i}", esP) for i in range(5)]
                    rrp = [fw.sb([128, 1], F32, f"rrp{i}", esP) for i in range(5)]
                    x1T = [Buf(x1.t, f"x1_{t}") for t in range(NT_OWN)]
                    for b_ in x1T:
                        b_.lw = x1.lw
                        b_.rd = dict(x1.rd)

                    def p_s1(t):
                        r = t % 3
                        fw.dma(plb[r][:], pl_d[t * 128:(t + 1) * 128, :], writes=[plb[r]], q="pool")
                        rs = {"ss": ssp[r], "r": rrp[r]}
                        rms_rstd({"ap": x1[:, t, :], "bufs": [x1T[t]]}, rs, D, {"ap": junkP[:], "buf": junkP})
                        op("dve", lambda e: e.scalar_tensor_tensor(out=hpb[r][:], in0=x1[:, t, :], scalar=rs["r"][:], in1=gB[:], op0=ALU.mult, op1=ALU.mult),
                           reads=[x1T[t], rs["r"], gB], writes=[hpb[r]])

                    def p_s2(t):
                        r = t % 3
                        b0 = 2 * (t % 2)
                        for k in range(8):
                            op("pe", lambda e: e.transpose(out=psbf(b0)[:, k * 128:(k + 1) * 128], in_=hpb[r][:, k * 128:(k + 1) * 128], identity=idb[:]), reads=[hpb[r], idb], writes=[PS[b0]])
                        op("act", lambda e: e.copy(out=hpT[r][:], in_=psbf(b0).rearrange("p (k t) -> p k t", k=8)), reads=[PS[b0]], writes=[hpT[r]])
                        for k in range(2):
                            op("pe", lambda e: e.transpose(out=psbf(b0 + 1)[:, k * 128:(k + 1) * 128], in_=plb[r][:, k * 128:(k + 1) * 128], identity=idb[:]), reads=[plb[r], idb], writes=[PS[b0 + 1]])
                        op("act", lambda e: e.copy(out=plT[r][:], in_=psbf(b0 + 1)[:, 0:256].rearrange("p (k t) -> p k t", k=2)), reads=[PS[b0 + 1]], writes=[plT[r]])

                    def p_s3(t):
                        r = t % 3
                        for half in range(2):
                            hs = slice(half * 512, (half + 1) * 512)
                            bG = 4 + half
                            bP = 6 + half
                            for k in range(8):
                                mm(PS[bG], PS[bG][:, :], hpT[r][:, k, :], w_pg[:, k, hs], k == 0, k == 7, [hpT[r], w_pg])
                            for k in range(2):
                                mm(PS[bP], PS[bP][:, :], plT[r][:, k, :], w_pp[:, k, hs], k == 0, k == 1, [plT[r], w_pp])
                            op("act", lambda e: e.activation(out=sgP[half][:], in_=PS[bG][:, :], func=AF.Sigmoid), reads=[PS[bG]], writes=[sgP[half]])
                            op("dve", lambda e: e.tensor_tensor(out=tP[half][:], in0=PS[bP][:, :], in1=sgP[half][:], op=ALU.mult), reads=[PS[bP], sgP[half]], writes=[tP[half]])
                            op("dve", lambda e: e.tensor_tensor(out=x1[:, t, hs], in0=x1[:, t, hs], in1=tP[half][:], op=ALU.add), reads=[x1T[t], tP[half]], writes=[x1T[t]])
                        rs2 = {"ss": ssp[3 + t % 2], "r": rrp[3 + t % 2]}
                        rms_rstd({"ap": x1[:, t, :], "bufs": [x1T[t]]}, rs2, D, {"ap": junkP2[:], "buf": junkP2})
                        o_ = outt[t % 2]
                        op("dve", lambda e: e.scalar_tensor_tensor(out=o_[:], in0=x1[:, t, :], scalar=rs2["r"][:], in1=gB2[:], op0=ALU.mult, op1=ALU.mult),
                           reads=[x1T[t], rs2["r"], gB2], writes=[o_])
                        fw.dma(out_d[t * 128:(t + 1) * 128, :], o_[:], reads=[o_], is_output=True)

                    for i in range(NT_OWN + 2):
                        if i < NT_OWN:
                            p_s1(i)
                        if 1 <= i <= NT_OWN:
                            p_s2(i - 1)
                        if i >= 2:
                            p_s3(i - 2)

            if "yaT" in dbg:
                o = dbg_t("yaT", [128, 4, S_OWN], BF16)
                fw.dma(o[:, :, :], YT[:, 0:4, :], reads=[YT], is_output=True)

            if "hT" in dbg:
                o = dbg_t("hT", [128, 8, S_EXT], BF16)
                with fw.scope() as esd:
                    tmp = fw.sb([128, 8, 512], BF16, "dbg_hT", esd)
                    for i in range(8):
                        fw.dma(tmp[:], hT_d[:, :, i * 512:(i + 1) * 512], reads=hT_tiles[4 * i:4 * i + 4], writes=[tmp])
                        fw.dma(o[:, :, i * 512:(i + 1) * 512], tmp[:], reads=[tmp], is_output=True)


        body()
        fw.stopped = False
        fw.finish()
    return nc, dbg_out


_INV = (500000.0 ** (-np.arange(0, 16, 2, dtype=np.float32) / 16.0)).astype(np.float32)


def make_in_maps(inputs):
    f = lambda a: np.ascontiguousarray(np.asarray(a), dtype=np.float32)
    x = f(inputs["x"]); p = f(inputs["p"])
    positions = np.asarray(inputs["positions"]).astype(np.int32)
    w_in = f(inputs["w_in"])[0]
    offs = np.cumsum([0, 512, 128, 128, 128, 128, 128, 128, 24, 1024, 512, 512, 8, 2048])
    seg = {n: (offs[i], offs[i + 1]) for i, n in enumerate(["q", "kc", "vc", "ks", "vs", "kw", "vw", "gate", "qk", "v", "o", "if", "mg"])}
    col = lambda n: w_in[:, seg[n][0]:seg[n][1]]
    w_att = []
    for g in range(2):
        parts = [col("q")[:, g * 256:(g + 1) * 256]]
        for n in ["ks", "kw", "kc", "vc", "vs", "vw"]:
            parts.append(col(n)[:, g * 64:(g + 1) * 64])
        parts.append(col("gate")[:, g * 12:(g + 1) * 12])
        w_att.append(np.concatenate(parts, axis=1))
    w_att = np.ascontiguousarray(np.stack(w_att))
    shared = {
        "invf": np.ascontiguousarray(np.broadcast_to(_INV[None, :], (128, 8))),
        "gvec": np.ascontiguousarray(np.stack([f(inputs["g_mix"])[0], f(inputs["g_ffn"])[0], f(inputs["g_ple"])[0], f(inputs["g_final"])])),
        "w_att": w_att,
        "w_qk": np.ascontiguousarray(col("qk")),
        "w_vo": np.ascontiguousarray(np.concatenate([col("v"), col("o")], axis=1)),
        "w_if": np.ascontiguousarray(col("if")),
        "w_mg": np.ascontiguousarray(col("mg")),
        "b_if": f(inputs["b_if"]).reshape(1, 8),
        "w_c1": np.ascontiguousarray(np.stack([f(inputs["w_ck1"])[0], f(inputs["w_cv1"])[0]])),
        "w_c2": np.ascontiguousarray(np.stack([f(inputs["w_ck2"])[0], f(inputs["w_cv2"])[0]])),
        "pe_c": np.ascontiguousarray(np.stack([f(inputs["pe_ck"])[0], f(inputs["pe_cv"])[0]])),
        "wc": np.ascontiguousarray(f(inputs["w_conv"])[0].reshape(4, 8, 128).transpose(2, 1, 0)),
        "bc": np.ascontiguousarray(f(inputs["b_conv"])[0].reshape(8, 128).T),
        "g_hn": f(inputs["g_hn"]).reshape(1, 512),
        "w_pa": f(inputs["w_pa"])[0], "w_pb": f(inputs["w_pb"])[0], "w_out": f(inputs["w_out"])[0],
        "w_r": np.ascontiguousarray(np.concatenate([f(inputs["w_rg"])[0], f(inputs["w_re"])[0]], axis=1)),
        "b_r": np.ascontiguousarray(np.concatenate([f(inputs["b_rg"])[0], f(inputs["b_re"])[0]])[None, :]),
        "w_e13": f(inputs["w_e13"])[0], "w_e2": f(inputs["w_e2"])[0],
        "w_pg": f(inputs["w_pg"])[0], "w_pp": f(inputs["w_pp"])[0],
    }
    in_maps = []
    for core in range(8):
        b, half = core // 2, core % 2
        if half == 1:
            xe_ = x[b]
            pos_ = positions[b]
        else:
            xe_ = np.concatenate([np.zeros((S_OWN, D), np.float32), x[b, :S_OWN]], axis=0)
            pos_ = np.concatenate([np.zeros(S_OWN, np.int32), positions[b, :S_OWN]])
        m = dict(shared)
        m["xe"] = np.ascontiguousarray(xe_)
        m["pos"] = np.ascontiguousarray(pos_.reshape(NT_EXT, 128).T)
        m["pl"] = np.ascontiguousarray(p[0, b, half * S_OWN:(half + 1) * S_OWN])
        m["hv"] = np.full((128, 1), float(half), np.float32)
        in_maps.append(m)
    return in_maps


def kernel(**inputs):
    nc, _ = build_program()
    in_maps = make_in_maps(inputs)
    res = run_bass_kernel_spmd(nc, in_maps, core_ids=list(range(8)))
    out = np.zeros((4, S_EXT, D), np.float32)
    for core in range(8):
        b, half = core // 2, core % 2
        out[b, half * S_OWN:(half + 1) * S_OWN] = res.results[core]["out"]
    return out
```

```python
import numpy as np
import concourse.bass as bass
import concourse.mybir as mybir
from concourse.bass_utils import run_bass_kernel_spmd
from contextlib import ExitStack

F32 = mybir.dt.float32
BF16 = mybir.dt.bfloat16
I32 = mybir.dt.int32
AF = mybir.ActivationFunctionType
ALU = mybir.AluOpType
AX = mybir.AxisListType

D = 1024
S_OWN = 2048
S_EXT = 4096
NT_OWN = 16
NT_EXT = 32
EPS = 1e-6
NEGB = -30000.0
DBG = []


class Buf:
    __slots__ = ("t", "lw", "rd", "name", "excl")

    def __init__(self, t, name=""):
        self.t = t
        self.excl = False
        self.lw = None
        self.rd = {}
        self.name = name

    def __getitem__(self, k):
        return self.t[k]


class FW:
    NDMA = 24

    def __init__(self, nc, es):
        self.nc = nc
        self.es = es
        self.eng = {"pe": nc.tensor, "act": nc.scalar, "dve": nc.vector, "pool": nc.gpsimd, "sp": nc.sync}
        self.sem = {k: es.enter_context(nc.semaphore("s_" + k)) for k in self.eng}
        self.cnt = {k: 0 for k in self.eng}
        self.known = {k: {} for k in self.eng}
        self.dsem = [es.enter_context(nc.semaphore(f"s_dma{i}")) for i in range(self.NDMA)]
        self.dval = [0] * self.NDMA
        self.dnext = 0
        self.nbuf = 0
        self.out_waits = []
        self.stopped = False

    def sb(self, shape, dt, name=None, es=None):
        self.nbuf += 1
        name = f"sb{self.nbuf}_" + (name or "t")
        return Buf((es or self.es).enter_context(self.nc.sbuf_tensor(name, list(shape), dt)), name)

    def ps(self, shape, dt, name=None):
        self.nbuf += 1
        name = name or f"ps{self.nbuf}"
        b = Buf(self.es.enter_context(self.nc.psum_tensor(name, list(shape), dt)), name)
        b.excl = True
        return b

    def _wait(self, e, src, idx):
        if self.stopped:
            return
        kn = self.known[e]
        if kn.get(src, 0) >= idx:
            return
        s = self.dsem[src[1]] if isinstance(src, tuple) else self.sem[src]
        self.eng[e].wait_ge(s, idx)
        kn[src] = idx

    def _deps(self, e, reads, writes):
        for b in reads:
            if b.lw is not None:
                self._wait(e, b.lw[0], b.lw[1])
            if b.excl:
                for src, idx in b.rd.items():
                    if src != e:
                        self._wait(e, src, idx)
        for b in writes:
            if b.lw is not None and b.lw[0] != e:
                self._wait(e, b.lw[0], b.lw[1])
            for src, idx in b.rd.items():
                if src != e:
                    self._wait(e, src, idx)

    def op(self, e, fn, reads=(), writes=()):
        if self.stopped:
            return None
        self._deps(e, reads, writes)
        inst = fn(self.eng[e])
        self.cnt[e] += 1
        c = self.cnt[e]
        inst.then_inc(self.sem[e], 1)
        for b in reads:
            if b.rd.get(e, 0) < c:
                b.rd[e] = c
        for b in writes:
            b.lw = (e, c)
            b.rd = {}
        return inst

    def dma(self, out, in_, reads=(), writes=(), q="sp", is_output=False):
        if self.stopped and not is_output:
            return None
        self._deps(q, reads, writes)
        slot = self.dnext
        self.dnext = (self.dnext + 1) % self.NDMA
        key = ("d", slot)
        if self.dval[slot] > 0:
            self._wait(q, key, self.dval[slot])
        inst = self.eng[q].dma_start(out=out, in_=in_)
        self.dval[slot] += 16
        inst.then_inc(self.dsem[slot], 16)
        v = self.dval[slot]
        for b in reads:
            if b.rd.get(key, 0) < v:
                b.rd[key] = v
        for b in writes:
            b.lw = (key, v)
            b.rd = {}
        if is_output:
            self.out_waits.append((key, v))
        return inst

    def barrier(self):
        for e in self.eng:
            for src in ("pe", "act", "dve", "pool"):
                if src != e and self.cnt[src] > 0:
                    self._wait(e, src, self.cnt[src])
            for slot in range(self.NDMA):
                if self.dval[slot] > 0:
                    self._wait(e, ("d", slot), self.dval[slot])

    def scope(self):
        fw = self

        class _Scope(ExitStack):
            def __exit__(self, *a):
                fw.barrier()
                return super().__exit__(*a)
        return _Scope()

    def finish(self):
        for key, v in self.out_waits:
            self._wait("sp", key, v)
        for k in ("pe", "act", "dve", "pool"):
            if self.cnt[k] > 0:
                self._wait("sp", k, self.cnt[k])


class _StopBuild(Exception):
    pass


def build_program(dbg=()):
    nc = bass.Bass("TRN2", target_bir_lowering=False)

    def din(name, shape, dt=F32):
        return nc.dram_tensor(name, list(shape), dt, kind="ExternalInput").ap()

    xe = din("xe", [S_EXT, D])
    pos_d = din("pos", [128, NT_EXT], I32)
    pl_d = din("pl", [S_OWN, 256])
    hv_d = din("hv", [128, 1])
    invf_d = din("invf", [128, 8])
    gvec_d = din("gvec", [4, D])
    w_att_d = din("w_att", [2, D, 652])
    w_qk_d = din("w_qk", [D, 1024])
    w_vo_d = din("w_vo", [D, 1024])
    w_if_d = din("w_if", [D, 8])
    w_mg_d = din("w_mg", [D, 2048])
    b_if_d = din("b_if", [1, 8])
    w_c1_d = din("w_c1", [2, 2048, 256])
    w_c2_d = din("w_c2", [2, 256, 64])
    pe_c_d = din("pe_c", [2, 32, 64])
    wc_d = din("wc", [128, 8, 4])
    bc_d = din("bc", [128, 8])
    g_hn_d = din("g_hn", [1, 512])
    w_pa_d = din("w_pa", [512, D])
    w_pb_d = din("w_pb", [512, D])
    w_out_d = din("w_out", [D, D])
    w_r_d = din("w_r", [D, 20])
    b_r_d = din("b_r", [1, 20])
    w_e13_d = din("w_e13", [16, D, 512])
    w_e2_d = din("w_e2", [16, 256, D])
    w_pg_d = din("w_pg", [D, D])
    w_pp_d = din("w_pp", [256, D])
    out_d = nc.dram_tensor("out", [S_OWN, D], F32, kind="ExternalOutput").ap()
    hT_d = nc.dram_tensor("hT_scr", [128, 8, S_EXT], BF16, kind="Internal").ap()
    dbg_out = {}

    def dbg_t(name, shape, dt=F32):
        dbg_out[name] = nc.dram_tensor("dbg_" + name, list(shape), dt, kind="ExternalOutput").ap()
        return dbg_out[name]

    with ExitStack() as es:
        fw = FW(nc, es)
        op = fw.op
        PS = [fw.ps([128, 512], F32, f"psb{i}") for i in range(8)]

        def psbf(i):
            return PS[i][:].bitcast(BF16)

        ones_f = fw.sb([128, 128], F32, "ones_f")
        op("pool", lambda e: e.memset(ones_f[:], 1.0), writes=[ones_f])
        idf = fw.sb([128, 128], F32, "idf")
        op("pool", lambda e: e.affine_select(out=idf[:], in_=ones_f[:], pattern=[[1, 128]], compare_op=ALU.is_equal,
                                             fill=0.0, base=0, channel_multiplier=-1), reads=[ones_f], writes=[idf])
        idb = fw.sb([128, 128], BF16, "idb")
        op("dve", lambda e: e.tensor_copy(out=idb[:], in_=idf[:]), reads=[idf], writes=[idb])
        U_f = fw.sb([128, 128], F32, "U_f")
        op("pool", lambda e: e.affine_select(out=U_f[:], in_=ones_f[:], pattern=[[1, 128]], compare_op=ALU.is_ge,
                                             fill=0.0, base=0, channel_multiplier=-1), reads=[ones_f], writes=[U_f])
        caus = fw.sb([128, 128], BF16, "caus")
        op("dve", lambda e: e.tensor_copy(out=caus[:], in_=U_f[:]), reads=[U_f], writes=[caus])
        wm0_f = fw.sb([128, 128], F32, "wm0_f")
        op("pool", lambda e: e.affine_select(out=wm0_f[:], in_=ones_f[:], pattern=[[-1, 128]], compare_op=ALU.is_ge,
                                             fill=0.0, base=-1, channel_multiplier=1), reads=[ones_f], writes=[wm0_f])
        wm0 = fw.sb([128, 128], BF16, "wm0")
        op("dve", lambda e: e.tensor_copy(out=wm0[:], in_=wm0_f[:]), reads=[wm0_f], writes=[wm0])
        c_eps = fw.sb([128, 1], F32, "c_eps")
        op("pool", lambda e: e.memset(c_eps[:], EPS), writes=[c_eps])
        c_one = fw.sb([128, 1], F32, "c_one")
        op("pool", lambda e: e.memset(c_one[:], 1.0), writes=[c_one])
        c_zero = fw.sb([128, 1], F32, "c_zero")
        op("pool", lambda e: e.memset(c_zero[:], 0.0), writes=[c_zero])
        acc_junk = fw.sb([128, 2], F32, "acc_junk")
        op("act", lambda e: e.activation(out=acc_junk[:, 0:1], in_=c_one[:], func=AF.Square, accum_out=acc_junk[:, 1:2]),
           reads=[c_one], writes=[acc_junk])
        hv = fw.sb([128, 1], F32, "hv")
        fw.dma(hv[:], hv_d[:, :], writes=[hv])
        hbias = fw.sb([128, 1], F32, "hbias")
        op("dve", lambda e: e.tensor_scalar(out=hbias[:], in0=hv[:], scalar1=-1.0, scalar2=-NEGB, op0=ALU.add, op1=ALU.mult),
           reads=[hv], writes=[hbias])
        gB = fw.sb([128, D], F32, "gB")

        def load_gain(i):
            fw.dma(gB[:], gvec_d[i:i + 1, :].to_broadcast([128, D]), writes=[gB])

        cs = fw.sb([128, NT_EXT, 8], F32, "cs")
        sn = fw.sb([128, NT_EXT, 8], F32, "sn")
        with fw.scope() as es1:
            posi = fw.sb([128, NT_EXT], I32, "posi", es1)
            posf = fw.sb([128, NT_EXT], F32, "posf", es1)
            invf = fw.sb([128, 8], F32, "invf", es1)
            ang = fw.sb([128, NT_EXT, 8], F32, "ang", es1)
            kf = fw.sb([128, NT_EXT, 8], F32, "kf", es1)
            ki = fw.sb([128, NT_EXT, 8], I32, "ki", es1)
            r1 = fw.sb([128, NT_EXT, 8], F32, "r1", es1)
            r2 = fw.sb([128, NT_EXT, 8], F32, "r2", es1)
            fw.dma(posi[:], pos_d[:, :], writes=[posi])
            fw.dma(invf[:], invf_d[:, :], writes=[invf])
            op("dve", lambda e: e.tensor_copy(out=posf[:], in_=posi[:]), reads=[posi], writes=[posf])
            op("dve", lambda e: e.tensor_tensor(out=ang[:], in0=posf[:].unsqueeze(2).to_broadcast([128, NT_EXT, 8]),
                                                in1=invf[:].unsqueeze(1).to_broadcast([128, NT_EXT, 8]), op=ALU.mult),
               reads=[posf, invf], writes=[ang])
            TWO_PI = 6.283185307179586
            C1 = 6.28125
            C2 = TWO_PI - C1
            PI_LO = 3.1415925
            op("dve", lambda e: e.tensor_scalar(out=kf[:], in0=ang[:], scalar1=1.0 / TWO_PI, scalar2=None, op0=ALU.mult),
               reads=[ang], writes=[kf])
            op("dve", lambda e: e.tensor_copy(out=ki[:], in_=kf[:]), reads=[kf], writes=[ki])
            op("dve", lambda e: e.tensor_copy(out=kf[:], in_=ki[:]), reads=[ki], writes=[kf])
            op("dve", lambda e: e.scalar_tensor_tensor(out=r1[:], in0=kf[:], scalar=-C1, in1=ang[:], op0=ALU.mult, op1=ALU.add),
               reads=[kf, ang], writes=[r1])
            op("dve", lambda e: e.scalar_tensor_tensor(out=r1[:], in0=kf[:], scalar=-C2, in1=r1[:], op0=ALU.mult, op1=ALU.add),
               reads=[kf, r1], writes=[r1])
            op("dve", lambda e: e.tensor_scalar(out=r1[:], in0=r1[:], scalar1=PI_LO, scalar2=-PI_LO, op0=ALU.min, op1=ALU.max),
               reads=[r1], writes=[r1])
            op("act", lambda e: e.activation(out=sn[:], in_=r1[:], func=AF.Sin), reads=[r1], writes=[sn])
            op("dve", lambda e: e.tensor_scalar(out=r2[:], in0=r1[:], scalar1=PI_LO / 2 + 0.0, scalar2=None, op0=ALU.add),
               reads=[r1], writes=[r2])
            op("dve", lambda e: e.tensor_scalar(out=kf[:], in0=r2[:], scalar1=PI_LO, scalar2=-TWO_PI, op0=ALU.is_gt, op1=ALU.mult),
               reads=[r2], writes=[kf])
            op("dve", lambda e: e.tensor_tensor(out=r2[:], in0=r2[:], in1=kf[:], op=ALU.add), reads=[r2, kf], writes=[r2])
            op("dve", lambda e: e.tensor_scalar(out=r2[:], in0=r2[:], scalar1=PI_LO, scalar2=-PI_LO, op0=ALU.min, op1=ALU.max),
               reads=[r2], writes=[r2])
            op("act", lambda e: e.activation(out=cs[:], in_=r2[:], func=AF.Sin), reads=[r2], writes=[cs])

        def rms_rstd(src, rstd, n, junk):
            ss = rstd["ss"]
            op("act", lambda e: e.activation(out=junk["ap"], in_=src["ap"], func=AF.Square, accum_out=ss[:]),
               reads=src["bufs"], writes=[junk["buf"], ss])
            op("act", lambda e: e.activation(out=ss[:], in_=ss[:], func=AF.Sqrt, bias=c_eps[:], scale=1.0 / n),
               reads=[ss, c_eps], writes=[ss])
            op("dve", lambda e: e.reciprocal(out=rstd["r"][:], in_=ss[:]), reads=[ss], writes=[rstd["r"]])

        load_gain(0)
        hT_tiles = [Buf(None, f"hT_tile{t}") for t in range(NT_EXT)]
        with fw.scope() as esA:
            xt = [fw.sb([128, D], F32, f"xtA{i}", esA) for i in range(6)]
            xn = [fw.sb([128, D], BF16, f"xnA{i}", esA) for i in range(3)]
            junk = fw.sb([128, D], BF16, "junkA", esA)
            hst = [fw.sb([128, 8, 128], BF16, f"hstA{i}", esA) for i in range(4)]
            ssA = [fw.sb([128, 1], F32, f"ssA{i}", esA) for i in range(3)]
            rrA = [fw.sb([128, 1], F32, f"rrA{i}", esA) for i in range(3)]
            def a_s1(t):
                x_ = xt[t % 6]
                if t == 0:
                    for tt in range(5):
                        fw.dma(xt[tt][:], xe[tt * 128:(tt + 1) * 128, :], writes=[xt[tt]])
                if t + 5 < NT_EXT:
                    fw.dma(xt[(t + 5) % 6][:], xe[(t + 5) * 128:(t + 6) * 128, :], writes=[xt[(t + 5) % 6]])
                rs = {"ss": ssA[t % 3], "r": rrA[t % 3]}
                rms_rstd({"ap": x_[:], "bufs": [x_]}, rs, D, {"ap": junk[:], "buf": junk})
                n_ = xn[t % 3]
                op("dve", lambda e: e.scalar_tensor_tensor(out=n_[:], in0=x_[:], scalar=rs["r"][:], in1=gB[:], op0=ALU.mult, op1=ALU.mult),
                   reads=[x_, rs["r"], gB], writes=[n_])

            def a_s2(t):
                n_ = xn[t % 3]
                pb = t % 2
                for k in range(8):
                    op("pe", lambda e: e.transpose(out=psbf(pb)[:, k * 128:(k + 1) * 128], in_=n_[:, k * 128:(k + 1) * 128], identity=idb[:]),
                       reads=[n_, idb], writes=[PS[pb]])
                h_ = hst[t % 4]
                op("act", lambda e: e.copy(out=h_[:], in_=psbf(pb).rearrange("p (k t) -> p k t", k=8)), reads=[PS[pb]], writes=[h_])
                fw.dma(hT_d[:, :, t * 128:(t + 1) * 128], h_[:], reads=[h_], writes=[hT_tiles[t]], q="pool")

            for t in range(NT_EXT + 1):
                if t < NT_EXT:
                    a_s1(t)
                if t >= 1:
                    a_s2(t - 1)

        if "cs" in dbg:
            o = dbg_t("cs", [128, NT_EXT, 8])
            fw.dma(o[:, :, :], cs[:], reads=[cs], is_output=True)
            o = dbg_t("sn", [128, NT_EXT, 8])
            fw.dma(o[:, :, :], sn[:], reads=[sn], is_output=True)

        def ckpt(name):
            if ("stop_" + name) in dbg:
                fw.stopped = True

        def body():
            def mm(bank, out_ap, lhsT, rhs, start, stop, reads):
                op("pe", lambda e: e.matmul(out_ap, lhsT, rhs, start=start, stop=stop), reads=reads, writes=[bank])

            YT = fw.sb([128, 8, S_OWN], BF16, "YT")
            esBc = fw.scope()
            esBc.__enter__()
            cmask = fw.sb([128, 2, S_OWN], BF16, "cmask", esBc)
            op("pool", lambda e: e.memset(cmask[:], 1.0), writes=[cmask])
            op("pool", lambda e: e.affine_select(out=cmask[:, 0, :], in_=cmask[:, 0, :], pattern=[[1, S_OWN]], compare_op=ALU.is_ge, fill=0.0,
                                                 base=2017, channel_multiplier=-16), reads=[cmask], writes=[cmask])
            op("pool", lambda e: e.affine_select(out=cmask[:, 1, :], in_=cmask[:, 1, :], pattern=[[1, S_OWN]], compare_op=ALU.is_ge, fill=0.0,
                                                 base=-31, channel_multiplier=-16), reads=[cmask], writes=[cmask])
            ovl = fw.sb([128, 2, 64], BF16, "ovl", esBc)
            op("pool", lambda e: e.memset(ovl[:], 1.0), writes=[ovl])
            for j in range(2):
                op("pool", lambda e: e.affine_select(out=ovl[:, j, :], in_=ovl[:, j, :], pattern=[[-4, 64]], compare_op=ALU.is_ge, fill=0.0,
                                                     base=128 * j + 1, channel_multiplier=1), reads=[ovl], writes=[ovl])
                op("pool", lambda e: e.affine_select(out=ovl[:, j, :], in_=ovl[:, j, :], pattern=[[4, 64]], compare_op=ALU.is_ge, fill=0.0,
                                                     base=3 - 128 * j, channel_multiplier=-1), reads=[ovl], writes=[ovl])
            maskadd = fw.sb([128, NT_OWN, 64], F32, "maskadd", esBc)
            Mb = fw.sb([128, 64], F32, "Mb", esBc)
            hm1 = fw.sb([128, 2], F32, "hm1", esBc)
            op("dve", lambda e: e.tensor_scalar(out=hm1[:, 0:1], in0=hv[:], scalar1=-1.0, scalar2=1e30, op0=ALU.add, op1=ALU.mult),
               reads=[hv], writes=[hm1])
            op("dve", lambda e: e.tensor_scalar(out=hm1[:, 1:2], in0=hv[:], scalar1=-1.0, scalar2=-1000.0, op0=ALU.add, op1=ALU.mult),
               reads=[hv, hm1], writes=[hm1])
            op("dve", lambda e: e.memset(Mb[:], 0.0), writes=[Mb])
            op("dve", lambda e: e.tensor_copy(out=Mb[:, 0:32], in_=hm1[:, 0:1].to_broadcast([128, 32])), reads=[hm1, Mb], writes=[Mb])
            op("dve", lambda e: e.scalar_tensor_tensor(out=Mb[:, 0:1], in0=hv[:], scalar=1000.0, in1=Mb[:, 0:1], op0=ALU.mult, op1=ALU.add),
               reads=[hv, Mb], writes=[Mb])
            op("dve", lambda e: e.tensor_copy(out=Mb[:, 32:33], in_=hm1[:, 1:2]), reads=[hm1, Mb], writes=[Mb])
            for c in range(NT_OWN):
                op("pool", lambda e: e.tensor_copy(out=maskadd[:, c, :], in_=Mb[:]), reads=[Mb, maskadd], writes=[maskadd])
                for hf in range(2):
                    lo = 32 + 2 * c + hf + 1
                    if lo < 64:
                        op("pool", lambda e: e.memset(maskadd[hf * 64:(hf + 1) * 64, c, lo:64], -1e30), reads=[maskadd], writes=[maskadd])
                    for col in (32 + 2 * c + hf, 32 + 2 * c + hf - 1):
                        op("pool", lambda e: e.tensor_scalar(out=maskadd[hf * 64:(hf + 1) * 64, c, col:col + 1],
                                                             in0=maskadd[hf * 64:(hf + 1) * 64, c, col:col + 1],
                                                             scalar1=1000.0, scalar2=None, op0=ALU.add), reads=[maskadd], writes=[maskadd])

            ckpt("consts")
            for g in range(2):
                with fw.scope() as esG:
                    qT = fw.sb([128, 4, S_OWN], BF16, f"qT{g}", esG)
                    kkT = fw.sb([128, 2, S_EXT], BF16, f"kkT{g}", esG)
                    op("pool", lambda e: e.memset(qT[64:128, :, :], 0.0), writes=[qT])
                    op("pool", lambda e: e.memset(kkT[64:128, 0, :], 1.0), writes=[kkT])
                    op("pool", lambda e: e.memset(kkT[64:128, 1, :], 0.0), writes=[kkT])
                    op("pool", lambda e: e.affine_select(out=kkT[64:128, 0, :], in_=kkT[64:128, 0, :], pattern=[[1, S_EXT]], compare_op=ALU.is_ge, fill=0.0,
                                                         base=0, channel_multiplier=-64), reads=[kkT], writes=[kkT])
                    op("pool", lambda e: e.affine_select(out=kkT[64:128, 0, :], in_=kkT[64:128, 0, :], pattern=[[-1, S_EXT]], compare_op=ALU.is_ge, fill=0.0,
                                                         base=63, channel_multiplier=64), reads=[kkT], writes=[kkT])
                    vv = fw.sb([128, NT_EXT, 2, 65], BF16, f"vv{g}", esG)
                    gsig = fw.sb([128, NT_OWN, 12], F32, f"gsig{g}", esG)
                    kcmpT = fw.sb([128, 256], BF16, f"kcmpT{g}", esG)
                    op("pool", lambda e: e.memset(kcmpT[64:128, :], 0.0), writes=[kcmpT])
                    vca = fw.sb([128, 2, 65], BF16, f"vca{g}", esG)
                    op("pool", lambda e: e.memset(vv[:, :, :, 64:65], 1.0), writes=[vv])
                    op("pool", lambda e: e.memset(vca[:, :, 64:65], 1.0), writes=[vca])
                    ckpt("B0a")
                    with fw.scope() as esC:
                        ccT = fw.sb([64, 2, S_EXT], BF16, f"ccT{g}", esC)
                        with fw.scope() as esB1:
                            w_att = fw.sb([128, 8, 652], BF16, f"w_att{g}", esB1)
                            fw.dma(w_att[:], w_att_d[g].rearrange("(k p) c -> p k c", p=128), writes=[w_att], q="pool")
                            ckpt("B0b")
                            hblk = [fw.sb([128, 8, 512], BF16, f"hblkB{g}{i}", esB1) for i in range(2)]
                            rp = [fw.sb([128, 8, 64], BF16, f"rp{g}{i}", esB1) for i in range(2)]
                            rpf = [fw.sb([128, 8, 64], F32, f"rpf{g}{i}", esB1) for i in range(2)]
                            ta = [fw.sb([128, 7, 8], F32, f"ropa{g}{i}", esB1) for i in range(2)]
                            tb_ = [fw.sb([128, 7, 8], F32, f"ropb{g}{i}", esB1) for i in range(2)]
                            tcx = [fw.sb([128, 7, 8], F32, f"ropc{g}{i}", esB1) for i in range(2)]
                            tdx = [fw.sb([128, 7, 8], F32, f"ropd{g}{i}", esB1) for i in range(2)]
                            def b1_front(t):
                                own = t >= NT_OWN
                                tq = t - NT_OWN
                                hb = hblk[(t // 4) % 2]
                                if t % 4 == 0:
                                    fw.dma(hb[:], hT_d[:, :, t * 128:(t + 4) * 128], reads=hT_tiles[t:t + 4], writes=[hb])
                                tl = t % 4
                                a0 = 0 if own else 256
                                nb = 140 if own else 128
                                bA = 2 + t % 2
                                bB = 4 + t % 2
                                for k in range(8):
                                    mm(PS[bA], PS[bA][:, a0:512], hb[:, k, tl * 128:(tl + 1) * 128], w_att[:, k, a0:512], k == 0, k == 7, [hb, w_att])
                                for k in range(8):
                                    mm(PS[bB], PS[bB][:, 0:nb], hb[:, k, tl * 128:(tl + 1) * 128], w_att[:, k, 512:512 + nb], k == 0, k == 7, [hb, w_att])
                                rp_ = rp[t % 2]
                                h0 = a0 // 64
                                nh = 7 - h0
                                rf = rpf[t % 2]
                                op("act", lambda e: e.copy(out=rf[:, h0:8, :], in_=PS[bA][:, a0:512].rearrange("p (h d) -> p h d", d=64)),
                                   reads=[PS[bA]], writes=[rf])
                                op("pool", lambda e: e.tensor_copy(out=rp_[:, h0:8, :], in_=rf[:, h0:8, :]), reads=[rf], writes=[rp_])
                                t1 = rf[:, h0:7, 0:8]
                                t2 = rf[:, h0:7, 8:16]
                                Cb = cs[:, t, :].unsqueeze(1).to_broadcast([128, nh, 8])
                                Sb_ = sn[:, t, :].unsqueeze(1).to_broadcast([128, nh, 8])
                                ta_, tb2 = ta[t % 2], tb_[t % 2]
                                tc_, td_ = tcx[t % 2], tdx[t % 2]
                                op("dve", lambda e: e.tensor_tensor(out=ta_[:, 0:nh, :], in0=t1, in1=Cb, op=ALU.mult), reads=[rf, cs], writes=[ta_])
                                op("dve", lambda e: e.tensor_tensor(out=tb2[:, 0:nh, :], in0=t2, in1=Sb_, op=ALU.mult), reads=[rf, sn], writes=[tb2])
                                op("dve", lambda e: e.tensor_tensor(out=tc_[:, 0:nh, :], in0=t2, in1=Cb, op=ALU.mult), reads=[rf, cs], writes=[tc_])
                                op("dve", lambda e: e.tensor_tensor(out=td_[:, 0:nh, :], in0=t1, in1=Sb_, op=ALU.mult), reads=[rf, sn], writes=[td_])
                                op("dve", lambda e: e.tensor_tensor(out=rp_[:, h0:7, 0:8], in0=ta_[:, 0:nh, :], in1=tb2[:, 0:nh, :], op=ALU.subtract),
                                   reads=[ta_, tb2, rp_], writes=[rp_])
                                op("dve", lambda e: e.tensor_tensor(out=rp_[:, h0:7, 8:16], in0=tc_[:, 0:nh, :], in1=td_[:, 0:nh, :], op=ALU.add),
                                   reads=[tc_, td_, rp_], writes=[rp_])
                                op("dve", lambda e: e.tensor_copy(out=vv[:, t, :, 0:64], in_=PS[bB][:, 0:128].rearrange("p (h d) -> p h d", d=64)),
                                   reads=[PS[bB]], writes=[vv])
                                if own:
                                    op("act", lambda e: e.activation(out=gsig[:, tq, :], in_=PS[bB][:, 128:140], func=AF.Sigmoid),
                                       reads=[PS[bB]], writes=[gsig])

                            def b1_back(t):
                                own = t >= NT_OWN
                                tq = t - NT_OWN
                                rp_ = rp[t % 2]
                                h0 = 0 if own else 4
                                bT = t % 2
                                psT = psbf(bT)
                                for j, hh in enumerate(range(h0, 8)):
                                    op("pe", lambda e: e.transpose(out=psT[0:64, j * 128:(j + 1) * 128], in_=rp_[:, hh, :], identity=idb[:]),
                                       reads=[rp_, idb], writes=[PS[bT]])
                                if own:
                                    op("act", lambda e: e.copy(out=qT[0:64, :, tq * 128:(tq + 1) * 128], in_=psT[0:64, 0:512].rearrange("p (h t) -> p h t", h=4)),
                                       reads=[PS[bT]], writes=[qT])
                                    o1 = 512
                                else:
                                    o1 = 0
                                op("act", lambda e: e.copy(out=kkT[0:64, :, t * 128:(t + 1) * 128], in_=psT[0:64, o1:o1 + 256].rearrange("p (h t) -> p h t", h=2)),
                                   reads=[PS[bT]], writes=[kkT])
                                op("act", lambda e: e.copy(out=ccT[:, :, t * 128:(t + 1) * 128], in_=psT[0:64, o1 + 256:o1 + 512].rearrange("p (h t) -> p h t", h=2)),
                                   reads=[PS[bT]], writes=[ccT])

                            for t in range(NT_EXT + 1):
                                if t < NT_EXT:
                                    b1_front(t)
                                if t >= 1:
                                    b1_back(t - 1)
                        ckpt("B1")
                        for i in range(2):
                            with fw.scope() as esB2:
                                w1 = fw.sb([64, 32, 256], BF16, f"w1_{g}{i}", esB2)
                                fw.dma(w1[:], w_c1_d[i].rearrange("(l d) h -> d l h", d=64), writes=[w1], q="pool")
                                w2 = fw.sb([128, 2, 64], BF16, f"w2_{g}{i}", esB2)
                                fw.dma(w2[:], w_c2_d[i].rearrange("(c p) d -> p c d", p=128), writes=[w2], q="pool")
                                pe_sb = fw.sb([32, 64], BF16, f"pe_{g}{i}", esB2)
                                fw.dma(pe_sb[:], pe_c_d[i], writes=[pe_sb], q="pool")
                                peT = fw.sb([64, 32], BF16, f"peT_{g}{i}", esB2)
                                op("pe", lambda e: e.transpose(out=psbf(6)[0:64, 0:32], in_=pe_sb[:, :], identity=idb[0:32, 0:32]),
                                   reads=[pe_sb, idb], writes=[PS[6]])
                                op("act", lambda e: e.copy(out=peT[:], in_=psbf(6)[0:64, 0:32]), reads=[PS[6]], writes=[peT])
                                for hc in range(2):
                                    for l in range(32):
                                        mm(PS[7], PS[7][:, hc:hc + 1], w1[:, l, hc * 128:(hc + 1) * 128], peT[:, l:l + 1], l == 0, l == 31, [w1, peT])
                                cbs = fw.sb([128, 2], F32, f"cbs_{g}{i}", esB2)
                                op("act", lambda e: e.copy(out=cbs[:], in_=PS[7][:, 0:2]), reads=[PS[7]], writes=[cbs])
                                G = fw.sb([128, 2, 256], BF16, f"G_{g}{i}", esB2)
                                op("pool", lambda e: e.memset(G[:, :, 255:256], 0.0), writes=[G])
                                u_ = fw.sb([128, 255], F32, f"u_{g}{i}", esB2)
                                u2 = fw.sb([128, 255], F32, f"u2_{g}{i}", esB2)
                                sg_ = fw.sb([128, 255], F32, f"sg_{g}{i}", esB2)
                                for hc in range(2):
                                    for l in range(32):
                                        mm(PS[hc], PS[hc][:, 0:255], w1[:, l, hc * 128:(hc + 1) * 128], ccT[:, i, l:l + 16 * 254 + 1:16], l == 0, l == 31, [w1, ccT])
                                    op("act", lambda e: e.activation(out=u_[:], in_=PS[hc][:, 0:255], func=AF.Identity, bias=cbs[:, hc:hc + 1]),
                                       reads=[PS[hc], cbs], writes=[u_])
                                    op("dve", lambda e: e.tensor_tensor(out=u2[:], in0=u_[:], in1=u_[:], op=ALU.mult), reads=[u_], writes=[u2])
                                    op("dve", lambda e: e.tensor_scalar(out=u2[:], in0=u2[:], scalar1=0.044715, scalar2=1.0, op0=ALU.mult, op1=ALU.add),
                                       reads=[u2], writes=[u2])
                                    op("dve", lambda e: e.tensor_tensor(out=u2[:], in0=u2[:], in1=u_[:], op=ALU.mult), reads=[u2, u_], writes=[u2])
                                    op("act", lambda e: e.activation(out=sg_[:], in_=u2[:], func=AF.Sigmoid, scale=1.5957691216057308),
                                       reads=[u2], writes=[sg_])
                                    op("dve", lambda e: e.tensor_tensor(out=G[:, hc, 0:255], in0=u_[:], in1=sg_[:], op=ALU.mult), reads=[u_, sg_], writes=[G])
                                if i == 0:
                                    for hc in range(2):
                                        mm(PS[6], PS[6][0:64, 0:256], w2[:, hc, :], G[:, hc, :], hc == 0, hc == 1, [w2, G])
                                    op("act", lambda e: e.copy(out=kcmpT[0:64, :], in_=PS[6][0:64, 0:256]), reads=[PS[6]], writes=[kcmpT])
                                else:
                                    for nch in range(2):
                                        for hc in range(2):
                                            mm(PS[6], PS[6][:, nch * 64:(nch + 1) * 64], G[:, hc, nch * 128:(nch + 1) * 128], w2[:, hc, :], hc == 0, hc == 1, [w2, G])
                                    op("act", lambda e: e.copy(out=vca[:, :, 0:64], in_=PS[6][:, 0:128].rearrange("p (n d) -> p n d", d=64)),
                                       reads=[PS[6]], writes=[vca])
                    if g == 0 and "B2dump" in dbg:
                        for nm, bf, shp in (("kkT", kkT, [64, 2, S_EXT]), ("qT", qT, [64, 4, S_OWN]), ("vv", vv, [128, NT_EXT, 2, 65]),
                                            ("kcmpT", kcmpT, [64, 256]), ("vca", vca, [128, 2, 65])):
                            o = dbg_t(nm, shp, BF16)
                            fw.dma(o, bf[0:shp[0]], reads=[bf], is_output=True)
                        o = dbg_t("gsig", [128, NT_OWN, 12])
                        fw.dma(o, gsig[:], reads=[gsig], is_output=True)
                    ckpt("B2")
                    with fw.scope() as esB3:
                        NP = 4
                        LA = 2
                        Pb = [fw.sb([128, 512], BF16, f"Pb{g}{i}", esB3) for i in range(NP)]
                        Sbank = [0, 1, 6, 7]
                        hbS = [fw.sb([128, 4, 132], F32, f"hbS{g}{r}", esB3) for r in range(3)]
                        ya = [fw.sb([128, 4, 64], F32, f"ya{g}{i}", esB3) for i in range(2)]
                        yat = [fw.sb([128, 4, 64], BF16, f"yat{g}{i}", esB3) for i in range(2)]
                        sms = [fw.sb([128, 16], F32, f"sm{g}{i}", esB3) for i in range(3)]
                        rdc = fw.sb([128, 4], F32, f"rdc{g}", esB3)
                        impv = fw.sb([128, 64], F32, f"impv{g}", esB3)
                        wk = fw.sb([128, 64], F32, f"wk{g}", esB3)
                        m8a = fw.sb([128, 8], F32, f"m8a{g}", esB3)
                        m8b = fw.sb([128, 8], F32, f"m8b{g}", esB3)
                        negm2 = fw.sb([128, 128], BF16, f"negm{g}", esB3)
                        op("pool", lambda e: e.memset(negm2[:, 0:64], 0.0), writes=[negm2])
                        rot = [0]
                        REG = {0: (0, 129), 1: (129, 65), 2: (194, 65)}

                        def score(c, lhsT, lreads, extra, bias, mask):
                            r = rot[0] % NP
                            rot[0] += 1
                            sb_i = Sbank[r]
                            P = Pb[r]
                            qrhs = qT[:, :, c * 128:(c + 1) * 128]
                            S3 = PS[sb_i][:, :].rearrange("p (h q) -> p h q", h=4)
                            mm(PS[sb_i], S3, lhsT, qrhs, True, True, lreads + [qT])
                            op("act", lambda e: e.activation(out=P[:], in_=PS[sb_i][:, :], func=AF.Exp, bias=bias[:], scale=0.125),
                               reads=[PS[sb_i], bias], writes=[P])
                            if mask is not None:
                                op("dve", lambda e: e.tensor_tensor(out=P[:].rearrange("p (h q) -> p h q", h=4), in0=P[:].rearrange("p (h q) -> p h q", h=4),
                                                                    in1=mask[0], op=ALU.mult), reads=[P, mask[1]], writes=[P])
                            return P

                        def pv(P, h, reg, vr, vreads, cc, n, first, last):
                            op("pe", lambda e: e.matmul(PS[2 + h][:, cc:cc + n], P[:, h * 128:(h + 1) * 128], vr, start=first, stop=last),
                               reads=[P] + vreads, writes=[PS[2 + h]])

                        def evac_all(c, reg, br, first, final, mid=None):
                            col0, n = REG[reg]
                            hs = hbS[reg]
                            for h in range(4):
                                op("dve", lambda e: e.tensor_copy(out=hs[:, h, 0:n], in_=PS[2 + h][:, col0:col0 + n]), reads=[PS[2 + h]], writes=[hs])
                            sm = sms[reg]
                            yac = ya[c % 2]
                            dn = sm[:, 0:4]
                            rd = sm[:, 4:8] if br != 0 else rdc[:, 0:4]
                            rdb = sm if br != 0 else rdc
                            cf = sm[:, 8:12]
                            op("dve", lambda e: e.tensor_scalar(out=dn.unsqueeze(2), in0=hs[:, :, 64:65], scalar1=1e-30, scalar2=None, op0=ALU.max),
                               reads=[hs], writes=[sm])
                            op("dve", lambda e: e.reciprocal(out=rd, in_=dn), reads=[sm], writes=[rdb])
                            if mid is not None:
                                mid()
                            op("dve", lambda e: e.tensor_tensor(out=cf.unsqueeze(2), in0=rd.unsqueeze(2),
                                                                in1=gsig[:, c, :].rearrange("p (h b) -> p h b", b=3)[:, :, br:br + 1], op=ALU.mult),
                               reads=[sm, rdb, gsig], writes=[sm])
                            cfb = cf.unsqueeze(2).to_broadcast([128, 4, 64])
                            if first:
                                op("dve", lambda e: e.tensor_tensor(out=yac[:], in0=hs[:, :, 0:64], in1=cfb, op=ALU.mult), reads=[hs, sm], writes=[yac])
                            else:
                                op("dve", lambda e: e.tensor_tensor(out=hs[:, :, 0:64], in0=hs[:, :, 0:64], in1=cfb, op=ALU.mult), reads=[hs, sm], writes=[hs])
                                dst = yat[c % 2] if final else yac
                                op("dve", lambda e: e.tensor_tensor(out=dst[:], in0=hs[:, :, 0:64], in1=yac[:], op=ALU.add), reads=[hs, yac], writes=[dst])

                        pend = []

                        def flush():
                            while pend:
                                pend.pop(0)()

                        def pipe(score_fn, pv_fn):
                            P = score_fn()
                            while len(pend) >= LA:
                                pend.pop(0)()
                            pend.append(lambda: pv_fn(P))

                        def tr_slot():
                            r = rot[0] % NP
                            rot[0] += 1
                            return Sbank[r]

                        def cmp_scores_pv(c):
                            Pc = []
                            for nch in range(2):
                                mk = cmask[:, nch, c * 128:(c + 1) * 128].unsqueeze(1).to_broadcast([128, 4, 128])
                                Pc.append(score(c, kcmpT[:, nch * 128:(nch + 1) * 128], [kcmpT], None, hbias if nch == 0 else c_zero, (mk, cmask)))
                            flush()
                            for h in range(4):
                                for nch in range(2):
                                    pv(Pc[nch], h, 0, vca[:, nch, :], [vca], 0, 65, nch == 0, nch == 1)
                                for nch in range(2):
                                    pv(Pc[nch], h, 0, ovl[:, nch, :], [ovl], 65, 64, nch == 0, nch == 1)

                        def cmp_evac_topk(c):
                            def topk_chain():
                                op("dve", lambda e: e.tensor_tensor(out=hbS[0][:, :, 65:129], in0=hbS[0][:, :, 65:129], in1=rdc[:, 0:4].unsqueeze(2).to_broadcast([128, 4, 64]), op=ALU.mult),
                                   reads=[hbS[0], rdc], writes=[hbS[0]])
                                op("dve", lambda e: e.tensor_reduce(out=impv[:], in_=hbS[0][:, :, 65:129].rearrange("p h s -> p s h"), axis=AX.X, op=ALU.add),
                                   reads=[hbS[0]], writes=[impv])
                                op("dve", lambda e: e.tensor_tensor(out=impv[:], in0=impv[:], in1=maskadd[:, c, :], op=ALU.add), reads=[impv, maskadd], writes=[impv])
                                op("dve", lambda e: e.max(out=m8a[:], in_=impv[:]), reads=[impv], writes=[m8a])
                                op("dve", lambda e: e.match_replace(out=wk[:], in_to_replace=m8a[:], in_values=impv[:], imm_value=-3.0e38),
                                   reads=[impv, m8a], writes=[wk])
                                op("dve", lambda e: e.max(out=m8b[:], in_=wk[:]), reads=[wk], writes=[m8b])
                                op("dve", lambda e: e.tensor_scalar(out=negm2[:, 64:128], in0=impv[:], scalar1=m8b[:, 7:8], scalar2=NEGB, op0=ALU.is_lt, op1=ALU.mult),
                                   reads=[impv, m8b, negm2], writes=[negm2])
                            evac_all(c, 0, 0, True, False, mid=topk_chain)

                        def negm_to_q(c):
                            bk = tr_slot()
                            op("pe", lambda e: e.transpose(out=psbf(bk)[:, 0:128], in_=negm2[:, :], identity=idb[:]), reads=[negm2, idb], writes=[PS[bk]])
                            op("act", lambda e: e.copy(out=qT[64:128, :, c * 128:(c + 1) * 128], in_=psbf(bk)[64:128, 0:128].unsqueeze(1).to_broadcast([64, 4, 128])),
                               reads=[PS[bk]], writes=[qT])

                        def finish_tile(cp):
                            evac_all(cp, 1, 1, False, True)
                            bk = tr_slot()
                            for j in range(2):
                                op("pe", lambda e: e.transpose(out=psbf(bk)[:, j * 128:(j + 1) * 128],
                                                               in_=yat[cp % 2][:, 2 * j:2 * j + 2, :].rearrange("p h d -> p (h d)"), identity=idb[:]),
                                   reads=[yat[cp % 2], idb], writes=[PS[bk]])
                            op("act", lambda e: e.copy(out=YT[:, 2 * g:2 * g + 2, cp * 128:(cp + 1) * 128],
                                                       in_=psbf(bk)[:, 0:256].rearrange("p (j t) -> p j t", j=2)), reads=[PS[bk]], writes=[YT])

                        cmp_scores_pv(0)
                        cmp_evac_topk(0)
                        negm_to_q(0)
                        for c in range(NT_OWN):
                            for j in range(5):
                                ch = NT_OWN + c - 4 + j
                                mk = None
                                if j == 0:
                                    mk = (wm0[:].unsqueeze(1).to_broadcast([128, 4, 128]), wm0)
                                elif j == 4:
                                    mk = (caus[:].unsqueeze(1).to_broadcast([128, 4, 128]), caus)

                                def sfn(ch=ch, mk=mk):
                                    return score(c, kkT[:, 1, ch * 128:(ch + 1) * 128], [kkT], None, hbias if ch < NT_OWN else c_zero, mk)

                                def pfn(P, ch=ch, j=j):
                                    for h in range(4):
                                        pv(P, h, 2, vv[:, ch, 1, :], [vv], 194, 65, j == 0, j == 4)
                                pipe(sfn, pfn)
                                if j == 1 and c > 0:
                                    finish_tile(c - 1)
                            flush()
                            evac_all(c, 2, 2, False, False)
                            if c + 1 < NT_OWN:
                                cmp_scores_pv(c + 1)
                            chs = list(range(NT_OWN)) + [NT_OWN + j for j in range(c + 1)]
                            for i, ch in enumerate(chs):
                                mk = None
                                if ch == NT_OWN + c:
                                    mk = (caus[:].unsqueeze(1).to_broadcast([128, 4, 128]), caus)

                                def sfn(ch=ch, mk=mk):
                                    return score(c, kkT[:, 0, ch * 128:(ch + 1) * 128], [kkT], None, hbias if ch < NT_OWN else c_zero, mk)

                                def pfn(P, ch=ch, i=i, n=len(chs)):
                                    for h in range(4):
                                        pv(P, h, 1, vv[:, ch, 0, :], [vv], 129, 65, i == 0, i == n - 1)
                                pipe(sfn, pfn)
                                if c + 1 < NT_OWN:
                                    if i == 2:
                                        cmp_evac_topk(c + 1)
                                    elif i == 10:
                                        negm_to_q(c + 1)
                        flush()
                        finish_tile(NT_OWN - 1)
            esBc.__exit__(None, None, None)
            ckpt("B")
            with fw.scope() as esCg:
                ee = fw.sb([128, NT_EXT, 4], F32, "ee", esCg)
                ff = fw.sb([128, NT_EXT, 4], F32, "ff", esCg)
                fl = fw.sb([128, NT_EXT, 4], F32, "fl", esCg)
                ghn = fw.sb([128, 512], F32, "ghn", esCg)
                fw.dma(ghn[:], g_hn_d[0:1, :].to_broadcast([128, 512]), writes=[ghn])
                wcs = fw.sb([128, 8, 4], F32, "wcs", esCg)
                fw.dma(wcs[:], wc_d[:, :, :], writes=[wcs])
                bcs = fw.sb([128, 8], F32, "bcs", esCg)
                fw.dma(bcs[:], bc_d[:, :], writes=[bcs])
                with fw.scope() as esg:
                    w_if = fw.sb([128, 8, 8], BF16, "w_if", esg)
                    fw.dma(w_if[:], w_if_d.rearrange("(k p) c -> p k c", p=128), writes=[w_if], q="pool")
                    bif = fw.sb([128, 8], F32, "bif", esg)
                    fw.dma(bif[:], b_if_d[0:1, :].to_broadcast([128, 8]), writes=[bif])
                    hblk = [fw.sb([128, 8, 512], BF16, f"hblkG{i}", esg) for i in range(2)]
                    ifp = fw.sb([128, NT_EXT, 8], F32, "ifp", esg)
                    l1 = fw.sb([128, NT_EXT, 4], F32, "l1", esg)
                    tmpg = fw.sb([128, NT_EXT, 4], F32, "tmpg", esg)
                    for t in range(NT_EXT):
                        hb = hblk[(t // 4) % 2]
                        if t % 4 == 0:
                            fw.dma(hb[:], hT_d[:, :, t * 128:(t + 4) * 128], reads=hT_tiles[t:t + 4], writes=[hb])
                        tl = t % 4
                        for k in range(8):
                            mm(PS[0], PS[0][:, t * 8:(t + 1) * 8], hb[:, k, tl * 128:(tl + 1) * 128], w_if[:, k, :], k == 0, k == 7, [hb, w_if])
                    op("act", lambda e: e.copy(out=ifp[:], in_=PS[0][:, 0:256].rearrange("p (t c) -> p t c", c=8)), reads=[PS[0]], writes=[ifp])
                    op("dve", lambda e: e.tensor_tensor(out=ifp[:], in0=ifp[:], in1=bif[:].unsqueeze(1).to_broadcast([128, NT_EXT, 8]), op=ALU.add),
                       reads=[ifp, bif], writes=[ifp])
                    op("act", lambda e: e.activation(out=l1[:], in_=ifp[:, :, 4:8], func=AF.Exp, scale=-1.0), reads=[ifp], writes=[l1])
                    op("act", lambda e: e.activation(out=l1[:], in_=l1[:], func=AF.Ln, bias=c_one[:]), reads=[l1, c_one], writes=[l1])
                    l1f = l1[:].rearrange("p t c -> p (t c)")
                    mm(PS[1], PS[1][:, 0:128], U_f[:], l1f, True, True, [U_f, l1])
                    mm(PS[1], PS[1][:, 128:256], ones_f[:], l1f, True, True, [ones_f, l1])
                    op("act", lambda e: e.copy(out=tmpg[:], in_=PS[1][:, 0:128].rearrange("p (t c) -> p t c", c=4)), reads=[PS[1]], writes=[tmpg])
                    op("act", lambda e: e.activation(out=ff[:], in_=tmpg[:], func=AF.Exp, scale=-1.0), reads=[tmpg], writes=[ff])
                    op("act", lambda e: e.activation(out=fl[:], in_=PS[1][:, 128:256].rearrange("p (t c) -> p t c", c=4), func=AF.Exp, scale=-1.0),
                       reads=[PS[1]], writes=[fl])
                    op("dve", lambda e: e.tensor_tensor(out=tmpg[:], in0=tmpg[:], in1=ifp[:, :, 0:4], op=ALU.add), reads=[tmpg, ifp], writes=[tmpg])
                    op("act", lambda e: e.activation(out=ee[:], in_=tmpg[:], func=AF.Exp), reads=[tmpg], writes=[ee])
                    op("dve", lambda e: e.tensor_scalar(out=ee[:, 0:NT_OWN, :], in0=ee[:, 0:NT_OWN, :], scalar1=hv[:, 0:1], scalar2=None, op0=ALU.mult),
                       reads=[ee, hv], writes=[ee])
                ckpt("Cg")
                qTb = fw.sb([128, 4, S_OWN], BF16, "qTb", esCg)
                kTb = fw.sb([128, 4, S_EXT], BF16, "kTb", esCg)
                vaug = fw.sb([128, NT_EXT, 4, 129], BF16, "vaug", esCg)
                osig = fw.sb([128, NT_OWN, 512], BF16, "osig", esCg)
                op("pool", lambda e: e.memset(vaug[:, :, :, 128:129], 1.0), writes=[vaug])
                for hp in range(2):
                    with fw.scope() as esC1:
                        wq = fw.sb([128, 8, 256], BF16, f"wq{hp}", esC1)
                        wk = fw.sb([128, 8, 256], BF16, f"wk{hp}", esC1)
                        wv = fw.sb([128, 8, 256], BF16, f"wv{hp}", esC1)
                        wo = fw.sb([128, 8, 256], BF16, f"wo{hp}", esC1)
                        fw.dma(wq[:], w_qk_d[:, hp * 256:(hp + 1) * 256].rearrange("(k p) c -> p k c", p=128), writes=[wq], q="pool")
                        fw.dma(wk[:], w_qk_d[:, 512 + hp * 256:512 + (hp + 1) * 256].rearrange("(k p) c -> p k c", p=128), writes=[wk], q="pool")
                        fw.dma(wv[:], w_vo_d[:, hp * 256:(hp + 1) * 256].rearrange("(k p) c -> p k c", p=128), writes=[wv], q="pool")
                        fw.dma(wo[:], w_vo_d[:, 512 + hp * 256:512 + (hp + 1) * 256].rearrange("(k p) c -> p k c", p=128), writes=[wo], q="pool")
                        hblk = [fw.sb([128, 8, 512], BF16, f"hblkC{hp}{i}", esC1) for i in range(2)]
                        uk = [fw.sb([128, 4 + S_EXT], BF16, f"uk{hp}{i}", esC1) for i in range(2)]
                        uq = [fw.sb([128, 4 + 2560], BF16, f"uq{hp}{i}", esC1) for i in range(2)]
                        ycv = [fw.sb([128, 512], F32, f"ycv{hp}{i}", esC1) for i in range(2)]
                        sgm = [fw.sb([128, 512], F32, f"sgm{hp}{i}", esC1) for i in range(2)]
                        for hh in range(2):
                            op("pool", lambda e: e.memset(uk[hh][:, 0:4], 0.0), writes=[uk[hh]])
                            op("pool", lambda e: e.memset(uq[hh][:, 0:4], 0.0), writes=[uq[hh]])
                        ukB = [[Buf(None, f"ukB{hp}{hh}{i}") for i in range(8)] for hh in range(2)]
                        uqB = [[Buf(None, f"uqB{hp}{hh}{i}") for i in range(5)] for hh in range(2)]
                        pi_ = [0]

                        def conv_piece(hh, typ, pc):
                            H = 2 * hp + hh
                            ci = typ * 4 + H
                            u = uq[hh] if typ == 0 else uk[hh]
                            if typ == 0:
                                off = 4 + 512 + pc * 512
                                ur = [uqB[hh][pc + 1], uqB[hh][pc]]
                            else:
                                off = 4 + pc * 512
                                ur = [ukB[hh][pc], ukB[hh][pc - 1] if pc > 0 else uk[hh]]
                            y_ = ycv[pi_[0] % 2]
                            s_ = sgm[pi_[0] % 2]
                            pi_[0] += 1
                            op("dve", lambda e: e.tensor_scalar(out=y_[:], in0=u[:, off - 3:off - 3 + 512], scalar1=wcs[:, ci, 0:1], scalar2=bcs[:, ci:ci + 1],
                                                                op0=ALU.mult, op1=ALU.add), reads=ur + [wcs, bcs], writes=[y_])
                            for j in range(1, 4):
                                op("dve", lambda e: e.scalar_tensor_tensor(out=y_[:], in0=u[:, off - 3 + j:off - 3 + j + 512], scalar=wcs[:, ci, j:j + 1], in1=y_[:],
                                                                           op0=ALU.mult, op1=ALU.add), reads=ur + [wcs, y_], writes=[y_])
                            if typ == 0:
                                op("act", lambda e: e.activation(out=qTb[:, H, pc * 512:(pc + 1) * 512], in_=y_[:], func=AF.Silu), reads=[y_], writes=[qTb])
                            else:
                                op("act", lambda e: e.activation(out=s_[:], in_=y_[:], func=AF.Sigmoid), reads=[y_], writes=[s_])
                                op("dve", lambda e: e.scalar_tensor_tensor(out=kTb[:, H, pc * 512:(pc + 1) * 512], in0=y_[:], scalar=128.0 ** -0.5, in1=s_[:],
                                                                           op0=ALU.mult, op1=ALU.mult), reads=[y_, s_], writes=[kTb])

                        def conv_for_block(bdone):
                            for hh in range(2):
                                conv_piece(hh, 1, bdone)
                                if bdone >= 4:
                                    conv_piece(hh, 0, bdone - 4)

                        for blk in range(8):
                            hb = hblk[blk % 2]
                            fw.dma(hb[:], hT_d[:, :, blk * 512:(blk + 1) * 512], reads=hT_tiles[4 * blk:4 * blk + 4], writes=[hb])
                            for hh in range(2):
                                for k in range(8):
                                    mm(PS[hh], PS[hh][:, :], wk[:, k, hh * 128:(hh + 1) * 128], hb[:, k, :], k == 0, k == 7, [wk, hb])
                                op("act", lambda e: e.copy(out=uk[hh][:, 4 + blk * 512:4 + (blk + 1) * 512], in_=PS[hh][:, :]), reads=[PS[hh]], writes=[ukB[hh][blk]])
                            if blk >= 3:
                                for hh in range(2):
                                    for k in range(8):
                                        mm(PS[2 + hh], PS[2 + hh][:, :], wq[:, k, hh * 128:(hh + 1) * 128], hb[:, k, :], k == 0, k == 7, [wq, hb])
                                    op("act", lambda e: e.copy(out=uq[hh][:, 4 + (blk - 3) * 512:4 + (blk - 2) * 512], in_=PS[2 + hh][:, :]),
                                       reads=[PS[2 + hh]], writes=[uqB[hh][blk - 3]])
                            for tl in range(4):
                                t = blk * 4 + tl
                                bv = 4 + tl % 2
                                for k in range(8):
                                    mm(PS[bv], PS[bv][:, 0:256], hb[:, k, tl * 128:(tl + 1) * 128], wv[:, k, :], k == 0, k == 7, [wv, hb])
                                op("dve", lambda e: e.tensor_copy(out=vaug[:, t, 2 * hp:2 * hp + 2, 0:128], in_=PS[bv][:, 0:256].rearrange("p (h d) -> p h d", d=128)),
                                   reads=[PS[bv]], writes=[vaug])
                                if blk >= 4:
                                    bo = 6 + tl % 2
                                    for k in range(8):
                                        mm(PS[bo], PS[bo][:, 0:256], hb[:, k, tl * 128:(tl + 1) * 128], wo[:, k, :], k == 0, k == 7, [wo, hb])
                                    op("act", lambda e: e.activation(out=osig[:, t - NT_OWN, hp * 256:(hp + 1) * 256], in_=PS[bo][:, 0:256], func=AF.Sigmoid),
                                       reads=[PS[bo]], writes=[osig])
                            if blk >= 1:
                                conv_for_block(blk - 1)
                        conv_for_block(7)
                ckpt("C1")
                with fw.scope() as esC3:
                    ktokR = [fw.sb([128, 4, 128], BF16, f"ktokR{i}", esC3) for i in range(3)]
                    CTall = fw.sb([128, NT_OWN, 4, 129], BF16, "CTall", esC3)
                    Xs = [fw.sb([128, 129], F32, f"Xs{H}", esC3) for H in range(4)]
                    Sm = [[fw.sb([128, 128], BF16, f"Sm{H}{i}", esC3) for i in range(2)] for H in range(4)]
                    hm_ = [fw.sb([128, 128], F32, f"hm{H}", esC3) for H in range(4)]
                    yb_ = [fw.sb([128, 128], BF16, f"yb{H}", esC3) for H in range(4)]
                    jk = [fw.sb([128, 128], BF16, f"jk{H}", esC3) for H in range(4)]
                    smc = [fw.sb([128, 8], F32, f"smc{H}", esC3) for H in range(4)]
                    for H in range(4):
                        op("dve", lambda e: e.tensor_tensor(out=vaug[:, :, H, :], in0=vaug[:, :, H, :],
                                                            in1=ee[:, :, H:H + 1].to_broadcast([128, NT_EXT, 129]), op=ALU.mult), reads=[vaug, ee], writes=[vaug])

                    def k_tr(t):
                        bk = t % 2
                        for H in range(4):
                            op("pe", lambda e: e.transpose(out=psbf(bk)[:, H * 128:(H + 1) * 128], in_=kTb[:, H, t * 128:(t + 1) * 128], identity=idb[:]),
                               reads=[kTb, idb], writes=[PS[bk]])
                        op("act", lambda e: e.copy(out=ktokR[t % 3][:], in_=psbf(bk)[:, 0:512].rearrange("p (h d) -> p h d", d=128)), reads=[PS[bk]], writes=[ktokR[t % 3]])

                    k_tr(0)
                    for t in range(NT_EXT - 1):
                        if t + 1 < NT_EXT - 1:
                            k_tr(t + 1)
                        for H in range(4):
                            bU = 2 + H
                            mm(PS[bU], PS[bU][:, 0:129], ktokR[t % 3][:, H, :], vaug[:, t, H, :], True, True, [ktokR[t % 3], vaug])
                            if t == 0:
                                op("dve", lambda e: e.tensor_copy(out=Xs[H][:], in_=PS[bU][:, 0:129]), reads=[PS[bU]], writes=[Xs[H]])
                            else:
                                op("dve", lambda e: e.scalar_tensor_tensor(out=Xs[H][:], in0=Xs[H][:], scalar=fl[:, t - 1, H:H + 1], in1=PS[bU][:, 0:129],
                                                                           op0=ALU.mult, op1=ALU.add), reads=[Xs[H], fl, PS[bU]], writes=[Xs[H]])
                            if t + 1 >= NT_OWN:
                                op("act", lambda e: e.activation(out=CTall[:, t + 1 - NT_OWN, H, :], in_=Xs[H][:], func=AF.Copy, scale=fl[:, t, H:H + 1]),
                                   reads=[Xs[H], fl], writes=[CTall])
                    sc4 = fw.sb([128, 4, 8], F32, "sc4", esC3)

                    def stA(t):
                        tq = t - NT_OWN
                        for H in range(4):
                            sm_ = Sm[H][tq % 2]
                            mm(PS[H], PS[H][:, 0:128], kTb[:, H, t * 128:(t + 1) * 128], qTb[:, H, tq * 128:(tq + 1) * 128], True, True, [kTb, qTb])
                            op("dve", lambda e: e.tensor_tensor(out=sm_[:], in0=PS[H][:, 0:128], in1=caus[:], op=ALU.mult), reads=[PS[H], caus], writes=[sm_])

                    def stRest(t):
                        tq = t - NT_OWN
                        for H in range(4):
                            sm_ = Sm[H][tq % 2]
                            bA = 4 + H
                            mm(PS[bA], PS[bA][:, 0:129], sm_[:], vaug[:, t, H, :], True, False, [sm_, vaug])
                            mm(PS[bA], PS[bA][:, 0:129], qTb[:, H, tq * 128:(tq + 1) * 128], CTall[:, tq, H, :], False, True, [qTb, CTall])
                        for H in range(4):
                            op("act", lambda e: e.activation(out=sc4[:, H, 6:7], in_=PS[4 + H][:, 128:129], func=AF.Abs, scale=ff[:, t, H:H + 1]),
                               reads=[PS[4 + H], ff], writes=[sc4])
                        op("dve", lambda e: e.tensor_scalar(out=sc4[:, :, 0:1], in0=sc4[:, :, 6:7], scalar1=1.0, scalar2=None, op0=ALU.max), reads=[sc4], writes=[sc4])
                        op("dve", lambda e: e.reciprocal(out=sc4[:, :, 1:2], in_=sc4[:, :, 0:1]), reads=[sc4], writes=[sc4])
                        op("dve", lambda e: e.tensor_tensor(out=sc4[:, :, 2:3], in0=sc4[:, :, 1:2], in1=ff[:, t, :].unsqueeze(2), op=ALU.mult), reads=[sc4, ff], writes=[sc4])
                        for H in range(4):
                            op("dve", lambda e: e.scalar_tensor_tensor(out=hm_[H][:], in0=PS[4 + H][:, 0:128], scalar=sc4[:, H, 2:3], in1=osig[:, tq, H * 128:(H + 1) * 128],
                                                                       op0=ALU.mult, op1=ALU.mult), reads=[PS[4 + H], sc4, osig], writes=[hm_[H]])
                        for H in range(4):
                            op("act", lambda e: e.activation(out=jk[H][:], in_=hm_[H][:], func=AF.Square, accum_out=sc4[:, H, 3:4]), reads=[hm_[H]], writes=[jk[H], sc4])
                        op("act", lambda e: e.activation(out=sc4[:, :, 4:5], in_=sc4[:, :, 3:4], func=AF.Sqrt, bias=c_eps[:], scale=1.0 / 128), reads=[sc4, c_eps], writes=[sc4])
                        op("dve", lambda e: e.reciprocal(out=sc4[:, :, 5:6], in_=sc4[:, :, 4:5]), reads=[sc4], writes=[sc4])
                        for H in range(4):
                            op("dve", lambda e: e.scalar_tensor_tensor(out=yb_[H][:], in0=hm_[H][:], scalar=sc4[:, H, 5:6], in1=ghn[:, H * 128:(H + 1) * 128],
                                                                       op0=ALU.mult, op1=ALU.mult), reads=[hm_[H], sc4, ghn], writes=[yb_[H]])
                        for H in range(4):
                            op("pe", lambda e: e.transpose(out=psbf(4 + H)[:, 512:640], in_=yb_[H][:], identity=idb[:]), reads=[yb_[H], idb], writes=[PS[4 + H]])
                        for H in range(4):
                            op("act", lambda e: e.copy(out=YT[:, 4 + H, tq * 128:(tq + 1) * 128], in_=psbf(4 + H)[:, 512:640]), reads=[PS[4 + H]], writes=[YT])

                    stA(NT_OWN)
                    for t in range(NT_OWN, NT_EXT):
                        if t + 1 < NT_EXT:
                            stA(t + 1)
                        stRest(t)
            ckpt("C")
            if "ybT" in dbg:
                o = dbg_t("ybT", [128, 4, S_OWN], BF16)
                fw.dma(o[:, :, :], YT[:, 4:8, :], reads=[YT], is_output=True)


            with fw.scope() as esD:
                x1 = fw.sb([128, NT_OWN, D], F32, "x1", esD)
                with fw.scope() as esD1:
                    mixT = fw.sb([128, 8, S_OWN], BF16, "mixT", esD1)
                    with fw.scope() as esD1a:
                        hTo = fw.sb([128, 8, S_OWN], BF16, "hTo", esD1a)
                        for tb in range(4):
                            fw.dma(hTo[:, :, tb * 512:(tb + 1) * 512], hT_d[:, :, S_OWN + tb * 512:S_OWN + (tb + 1) * 512],
                                   reads=hT_tiles[NT_OWN + 4 * tb:NT_OWN + 4 * tb + 4], writes=[hTo])
                        wga = [fw.sb([128, 8, 128], BF16, f"wga{i}", esD1a) for i in range(2)]
                        wgb = [fw.sb([128, 8, 128], BF16, f"wgb{i}", esD1a) for i in range(2)]
                        wpa = [fw.sb([128, 4, 128], BF16, f"wpa{i}", esD1a) for i in range(2)]
                        wpb = [fw.sb([128, 4, 128], BF16, f"wpb{i}", esD1a) for i in range(2)]
                        sga = [fw.sb([128, 512], BF16, f"sga{i}", esD1a) for i in range(2)]
                        sgb = [fw.sb([128, 512], BF16, f"sgb{i}", esD1a) for i in range(2)]
                        t1 = [fw.sb([128, 512], F32, f"t1_{i}", esD1a) for i in range(2)]
                        t2 = [fw.sb([128, 512], F32, f"t2_{i}", esD1a) for i in range(2)]
                        it = 0
                        for j in range(8):
                            w_ = j % 2
                            fw.dma(wga[w_][:], w_mg_d[:, j * 128:(j + 1) * 128].rearrange("(k p) c -> p k c", p=128), writes=[wga[w_]], q="pool")
                            fw.dma(wgb[w_][:], w_mg_d[:, 1024 + j * 128:1024 + (j + 1) * 128].rearrange("(k p) c -> p k c", p=128), writes=[wgb[w_]], q="pool")
                            fw.dma(wpa[w_][:], w_pa_d[:, j * 128:(j + 1) * 128].rearrange("(k p) c -> p k c", p=128), writes=[wpa[w_]], q="pool")
                            fw.dma(wpb[w_][:], w_pb_d[:, j * 128:(j + 1) * 128].rearrange("(k p) c -> p k c", p=128), writes=[wpb[w_]], q="pool")
                            for tb in range(4):
                                r = it % 2
                                it += 1
                                b0 = 4 * r
                                ts_ = slice(tb * 512, (tb + 1) * 512)
                                for k in range(8):
                                    mm(PS[b0], PS[b0][:, :], wga[w_][:, k, :], hTo[:, k, ts_], k == 0, k == 7, [wga[w_], hTo])
                                op("act", lambda e: e.activation(out=sga[r][:], in_=PS[b0][:, :], func=AF.Sigmoid), reads=[PS[b0]], writes=[sga[r]])
                                for k in range(8):
                                    mm(PS[b0 + 1], PS[b0 + 1][:, :], wgb[w_][:, k, :], hTo[:, k, ts_], k == 0, k == 7, [wgb[w_], hTo])
                                op("act", lambda e: e.activation(out=sgb[r][:], in_=PS[b0 + 1][:, :], func=AF.Sigmoid), reads=[PS[b0 + 1]], writes=[sgb[r]])
                                for k in range(4):
                                    mm(PS[b0 + 2], PS[b0 + 2][:, :], wpa[w_][:, k, :], YT[:, k, ts_], k == 0, k == 3, [wpa[w_], YT])
                                for k in range(4):
                                    mm(PS[b0 + 3], PS[b0 + 3][:, :], wpb[w_][:, k, :], YT[:, 4 + k, ts_], k == 0, k == 3, [wpb[w_], YT])
                                op("dve", lambda e: e.tensor_tensor(out=t1[r][:], in0=PS[b0 + 2][:, :], in1=sga[r][:], op=ALU.mult), reads=[PS[b0 + 2], sga[r]], writes=[t1[r]])
                                op("dve", lambda e: e.tensor_tensor(out=t2[r][:], in0=PS[b0 + 3][:, :], in1=sgb[r][:], op=ALU.mult), reads=[PS[b0 + 3], sgb[r]], writes=[t2[r]])
                                op("pool", lambda e: e.tensor_tensor(out=mixT[:, j, ts_], in0=t1[r][:], in1=t2[r][:], op=ALU.add), reads=[t1[r], t2[r]], writes=[mixT])
                    ckpt("D1a")
                    with fw.scope() as esD1b:
                        w_out = fw.sb([128, 8, D], BF16, "w_out", esD1b)
                        fw.dma(w_out[:], w_out_d.rearrange("(k p) c -> p k c", p=128), writes=[w_out], q="pool")
                        xtl = [fw.sb([128, D], F32, f"xtl{i}", esD1b) for i in range(2)]
                        for t in range(NT_OWN):
                            x_ = xtl[t % 2]
                            fw.dma(x_[:], xe[S_OWN + t * 128:S_OWN + (t + 1) * 128, :], writes=[x_])
                            for half in range(2):
                                b = 2 * (t % 2) + half
                                for j in range(8):
                                    mm(PS[b], PS[b][:, :], mixT[:, j, t * 128:(t + 1) * 128], w_out[:, j, half * 512:(half + 1) * 512], j == 0, j == 7, [mixT, w_out])
                                op("dve", lambda e: e.tensor_tensor(out=x1[:, t, half * 512:(half + 1) * 512], in0=PS[b][:, :], in1=x_[:, half * 512:(half + 1) * 512], op=ALU.add),
                                   reads=[PS[b], x_], writes=[x1])
                ckpt("D1")
                if "x1" in dbg:
                    fw.dma(dbg_t("x1", [128, NT_OWN, D]), x1[:], reads=[x1], is_output=True)
                with fw.scope() as esM:
                    load_gain(1)
                    gateT = fw.sb([16, S_OWN], BF16, "gateT", esM)
                    E16 = fw.sb([16, 16, 128], BF16, "E16", esM)
                    op("pool", lambda e: e.memset(E16[:], 1.0), writes=[E16])
                    op("pool", lambda e: e.affine_select(out=E16[:], in_=E16[:], pattern=[[-1, 16], [0, 128]], compare_op=ALU.is_equal, fill=0.0,
                                                         base=0, channel_multiplier=1), reads=[E16], writes=[E16])
                    with fw.scope() as esR:
                        w_r = fw.sb([128, 8, 20], F32, "w_r", esR)
                        fw.dma(w_r[:], w_r_d.rearrange("(k p) c -> p k c", p=128), writes=[w_r])
                        b_r = fw.sb([128, 20], F32, "b_r", esR)
                        fw.dma(b_r[:], b_r_d[0:1, :].to_broadcast([128, 20]), writes=[b_r])
                        hnf = [fw.sb([128, D], F32, f"hnf{i}", esR) for i in range(2)]
                        hnTf = [fw.sb([128, 8, 128], F32, f"hnTf{i}", esR) for i in range(2)]
                        junkR = fw.sb([128, D], BF16, "junkR", esR)
                        ssr = [fw.sb([128, 1], F32, f"ssr{i}", esR) for i in range(2)]
                        rrr = [fw.sb([128, 1], F32, f"rrr{i}", esR) for i in range(2)]
                        T_ = NT_OWN
                        lgA = fw.sb([128, T_, 20], F32, "lgA", esR)

                        def r_front(t):
                            r = t % 2
                            rs = {"ss": ssr[r], "r": rrr[r]}
                            rms_rstd({"ap": x1[:, t, :], "bufs": [x1]}, rs, D, {"ap": junkR[:], "buf": junkR})
                            op("dve", lambda e: e.scalar_tensor_tensor(out=hnf[r][:], in0=x1[:, t, :], scalar=rs["r"][:], in1=gB[:], op0=ALU.mult, op1=ALU.mult),
                               reads=[x1, rs["r"], gB], writes=[hnf[r]])
                            for k in range(8):
                                b = 2 * r + (0 if k < 4 else 1)
                                op("pe", lambda e: e.transpose(out=PS[b][:, (k % 4) * 128:(k % 4 + 1) * 128], in_=hnf[r][:, k * 128:(k + 1) * 128], identity=idf[:]),
                                   reads=[hnf[r], idf], writes=[PS[b]])
                            for bb in range(2):
                                b = 2 * r + bb
                                op("act", lambda e: e.copy(out=hnTf[r][:, 4 * bb:4 * bb + 4, :], in_=PS[b][:, :].rearrange("p (k t) -> p k t", k=4)), reads=[PS[b]], writes=[hnTf[r]])
                                op("dve", lambda e: e.tensor_copy(out=YT[:, 4 * bb:4 * bb + 4, t * 128:(t + 1) * 128], in_=PS[b][:, :].rearrange("p (k t) -> p k t", k=4)),
                                   reads=[PS[b]], writes=[YT])

                        def r_back(t):
                            r = t % 2
                            bl = 4 + r
                            for k in range(8):
                                mm(PS[bl], PS[bl][:, 0:20], hnTf[r][:, k, :], w_r[:, k, :], k == 0, k == 7, [hnTf[r], w_r])
                            op("dve", lambda e: e.tensor_tensor(out=lgA[:, t, :], in0=PS[bl][:, 0:20], in1=b_r[:], op=ALU.add), reads=[PS[bl], b_r], writes=[lgA])

                        for t in range(T_ + 1):
                            if t < T_:
                                r_front(t)
                            if t >= 1:
                                r_back(t - 1)
                        gl = lgA[:, :, 0:4]
                        el = lgA[:, :, 4:20].rearrange("p t (g e) -> p t g e", g=4)
                        gmax = fw.sb([128, T_], F32, "gmax", esR)
                        g1h = fw.sb([128, T_, 4], F32, "g1h", esR)
                        exg = fw.sb([128, T_, 4], F32, "exg", esR)
                        pgs = fw.sb([128, T_], F32, "pgs", esR)
                        t16 = fw.sb([128, T_, 4, 4], F32, "t16", esR)
                        elg = fw.sb([128, T_, 4], F32, "elg", esR)
                        elg2 = fw.sb([128, T_, 4], F32, "elg2", esR)
                        ev1 = fw.sb([128, T_], F32, "ev1", esR)
                        ev2 = fw.sb([128, T_], F32, "ev2", esR)
                        mk1 = fw.sb([128, T_, 4], F32, "mk1", esR)
                        mk2 = fw.sb([128, T_, 4], F32, "mk2", esR)
                        w12 = fw.sb([128, 2, T_], F32, "w12", esR)
                        gig = fw.sb([128, T_, 4], F32, "gig", esR)
                        gate = fw.sb([128, T_, 4, 4], F32, "gate", esR)
                        B3 = [128, T_, 4]
                        op("dve", lambda e: e.tensor_reduce(out=gmax[:], in_=gl, axis=AX.X, op=ALU.max), reads=[lgA], writes=[gmax])
                        op("dve", lambda e: e.tensor_tensor(out=g1h[:], in0=gl, in1=gmax[:].unsqueeze(2).to_broadcast(B3), op=ALU.is_equal), reads=[lgA, gmax], writes=[g1h])
                        op("dve", lambda e: e.tensor_tensor(out=exg[:], in0=gl, in1=gmax[:].unsqueeze(2).to_broadcast(B3), op=ALU.subtract), reads=[lgA, gmax], writes=[exg])
                        op("act", lambda e: e.activation(out=exg[:], in_=exg[:], func=AF.Exp), reads=[exg], writes=[exg])
                        op("dve", lambda e: e.tensor_reduce(out=pgs[:], in_=exg[:], axis=AX.X, op=ALU.add), reads=[exg], writes=[pgs])
                        op("dve", lambda e: e.reciprocal(out=pgs[:], in_=pgs[:]), reads=[pgs], writes=[pgs])
                        op("dve", lambda e: e.tensor_tensor(out=t16[:], in0=el, in1=g1h[:].unsqueeze(3).to_broadcast([128, T_, 4, 4]), op=ALU.mult), reads=[lgA, g1h], writes=[t16])
                        op("dve", lambda e: e.tensor_reduce(out=elg[:], in_=t16[:].rearrange("p t g e -> p t e g"), axis=AX.X, op=ALU.add), reads=[t16], writes=[elg])
                        op("dve", lambda e: e.tensor_reduce(out=ev1[:], in_=elg[:], axis=AX.X, op=ALU.max), reads=[elg], writes=[ev1])
                        op("dve", lambda e: e.tensor_tensor(out=mk1[:], in0=elg[:], in1=ev1[:].unsqueeze(2).to_broadcast(B3), op=ALU.is_equal), reads=[elg, ev1], writes=[mk1])
                        op("dve", lambda e: e.scalar_tensor_tensor(out=elg2[:], in0=mk1[:], scalar=-1e30, in1=elg[:], op0=ALU.mult, op1=ALU.add), reads=[mk1, elg], writes=[elg2])
                        op("dve", lambda e: e.tensor_reduce(out=ev2[:], in_=elg2[:], axis=AX.X, op=ALU.max), reads=[elg2], writes=[ev2])
                        op("dve", lambda e: e.tensor_tensor(out=mk2[:], in0=elg2[:], in1=ev2[:].unsqueeze(2).to_broadcast(B3), op=ALU.is_equal), reads=[elg2, ev2], writes=[mk2])
                        op("dve", lambda e: e.tensor_tensor(out=w12[:, 0, :], in0=ev1[:], in1=ev2[:], op=ALU.subtract), reads=[ev1, ev2], writes=[w12])
                        op("act", lambda e: e.activation(out=w12[:, 0, :], in_=w12[:, 0, :], func=AF.Sigmoid), reads=[w12], writes=[w12])
                        op("dve", lambda e: e.tensor_scalar(out=w12[:, 1, :], in0=w12[:, 0, :], scalar1=-1.0, scalar2=1.0, op0=ALU.mult, op1=ALU.add), reads=[w12], writes=[w12])
                        op("dve", lambda e: e.tensor_tensor(out=w12[:], in0=w12[:], in1=pgs[:].unsqueeze(1).to_broadcast([128, 2, T_]), op=ALU.mult), reads=[w12, pgs], writes=[w12])
                        op("dve", lambda e: e.tensor_tensor(out=gig[:], in0=mk1[:], in1=w12[:, 0, :].unsqueeze(2).to_broadcast(B3), op=ALU.mult), reads=[mk1, w12], writes=[gig])
                        op("dve", lambda e: e.tensor_tensor(out=mk2[:], in0=mk2[:], in1=w12[:, 1, :].unsqueeze(2).to_broadcast(B3), op=ALU.mult), reads=[mk2, w12], writes=[mk2])
                        op("dve", lambda e: e.tensor_tensor(out=gig[:], in0=gig[:], in1=mk2[:], op=ALU.add), reads=[gig, mk2], writes=[gig])
                        op("dve", lambda e: e.tensor_tensor(out=gate[:], in0=g1h[:].unsqueeze(3).to_broadcast([128, T_, 4, 4]),
                                                            in1=gig[:].unsqueeze(2).to_broadcast([128, T_, 4, 4]), op=ALU.mult), reads=[g1h, gig], writes=[gate])
                        for t4 in range(T_ // 4):
                            bk = 6 + t4 % 2
                            for j in range(4):
                                t = t4 * 4 + j
                                op("pe", lambda e: e.transpose(out=PS[bk][0:16, j * 128:(j + 1) * 128], in_=gate[:, t, :, :].rearrange("p g e -> p (g e)"), identity=idf[:]),
                                   reads=[gate, idf], writes=[PS[bk]])
                            op("act", lambda e: e.copy(out=gateT[:, t4 * 512:(t4 + 1) * 512], in_=PS[bk][0:16, :]), reads=[PS[bk]], writes=[gateT])
                    ckpt("D2r")
                    if "gateT" in dbg:
                        fw.dma(dbg_t("gateT", [16, S_OWN], BF16), gateT[:], reads=[gateT], is_output=True)
                    with fw.scope() as esE:
                        NW = 3
                        w13 = [fw.sb([128, 8, 512], BF16, f"w13_{i}", esE) for i in range(NW)]
                        w2e = [fw.sb([128, 2, D], BF16, f"w2e_{i}", esE) for i in range(NW)]
                        sgE = [fw.sb([128, 512], F32, f"sgE{i}", esE) for i in range(2)]
                        tE = [fw.sb([128, 512], F32, f"tE{i}", esE) for i in range(2)]
                        actT = [[fw.sb([128, 512], BF16, f"actT{i}{fc}", esE) for fc in range(2)] for i in range(2)]
                        x1M = [Buf(x1.t, f"x1m_{t}") for t in range(NT_OWN)]
                        for b_ in x1M:
                            b_.lw = x1.lw
                            b_.rd = dict(x1.rd)
                        ybank = [4, 5, 7]
                        yi = [0]

                        def load_w(ex):
                            wb = ex % NW
                            fw.dma(w13[wb][:], w_e13_d[ex].rearrange("(k p) c -> p k c", p=128), writes=[w13[wb]], q="pool")
                            fw.dma(w2e[wb][:], w_e2_d[ex].rearrange("(k p) c -> p k c", p=128), writes=[w2e[wb]], q="pool")

                        def e_front_pe(it):
                            ex, tb = it // 4, it % 4
                            wb = ex % NW
                            ts_ = slice(tb * 512, (tb + 1) * 512)
                            mm(PS[6], PS[6][:, :], E16[:, ex, :], gateT[:, ts_], True, True, [E16, gateT])
                            for fc in range(2):
                                for k in range(8):
                                    mm(PS[fc], PS[fc][:, :], w13[wb][:, k, fc * 128:(fc + 1) * 128], YT[:, k, ts_], k == 0, k == 7, [w13[wb], YT])
                                for k in range(8):
                                    mm(PS[2 + fc], PS[2 + fc][:, :], w13[wb][:, k, 256 + fc * 128:256 + (fc + 1) * 128], YT[:, k, ts_], k == 0, k == 7, [w13[wb], YT])

                        def e_front_post(it):
                            r = it % 2
                            for fc in range(2):
                                op("act", lambda e: e.activation(out=sgE[fc][:], in_=PS[fc][:, :], func=AF.Silu), reads=[PS[fc]], writes=[sgE[fc]])
                                op("dve", lambda e: e.tensor_tensor(out=tE[fc][:], in0=PS[2 + fc][:, :], in1=sgE[fc][:], op=ALU.mult), reads=[PS[2 + fc], sgE[fc]], writes=[tE[fc]])
                                op("dve", lambda e: e.tensor_tensor(out=actT[r][fc][:], in0=PS[6][:, :], in1=tE[fc][:], op=ALU.mult), reads=[PS[6], tE[fc]], writes=[actT[r][fc]])

                        def e_back(it):
                            ex, tb = it // 4, it % 4
                            wb = ex % NW
                            r = it % 2
                            for tt in range(4):
                                t = tb * 4 + tt
                                for half in range(2):
                                    b = ybank[yi[0] % 3]
                                    yi[0] += 1
                                    for fc in range(2):
                                        mm(PS[b], PS[b][:, :], actT[r][fc][:, tt * 128:(tt + 1) * 128], w2e[wb][:, fc, half * 512:(half + 1) * 512], fc == 0, fc == 1, [actT[r][fc], w2e[wb]])
                                    op("dve", lambda e: e.tensor_tensor(out=x1[:, t, half * 512:(half + 1) * 512], in0=PS[b][:, :], in1=x1[:, t, half * 512:(half + 1) * 512], op=ALU.add),
                                       reads=[PS[b], x1M[t]], writes=[x1M[t]])

                        load_w(0)
                        load_w(1)
                        NIT = 64
                        for it in range(NIT + 1):
                            if it < NIT:
                                e_front_pe(it)
                                e_front_post(it)
                            if it >= 1:
                                e_back(it - 1)
                            if it < NIT and it % 4 == 0 and it // 4 + 2 < 16:
                                load_w(it // 4 + 2)
                        for b_ in x1M:
                            if b_.lw is not None and (x1.lw is None or True):
                                pass
                        x1.lw = None
                        x1.rd = {}
                        fw.barrier()
                ckpt("D2")
                if "x2" in dbg:
                    fw.dma(dbg_t("x2", [128, NT_OWN, D]), x1[:], reads=[x1], is_output=True)
                with fw.scope() as esP:
                    load_gain(2)
                    gB2 = fw.sb([128, D], F32, "gB2", esP)
                    fw.dma(gB2[:], gvec_d[3:4, :].to_broadcast([128, D]), writes=[gB2])
                    w_pg = fw.sb([128, 8, D], BF16, "w_pg", esP)
                    fw.dma(w_pg[:], w_pg_d.rearrange("(k p) c -> p k c", p=128), writes=[w_pg], q="pool")
                    w_pp = fw.sb([128, 2, D], BF16, "w_pp", esP)
                    fw.dma(w_pp[:], w_pp_d.rearrange("(k p) c -> p k c", p=128), writes=[w_pp], q="pool")
                    hpb = [fw.sb([128, D], BF16, f"hpb{i}", esP) for i in range(3)]
                    hpT = [fw.sb([128, 8, 128], BF16, f"hpT{i}", esP) for i in range(3)]
                    plb = [fw.sb([128, 256], BF16, f"plb{i}", esP) for i in range(3)]
                    plT = [fw.sb([128, 2, 128], BF16, f"plT{i}", esP) for i in range(3)]
                    junkP2 = fw.sb([128, D], BF16, "junkP2", esP)
                    sgP = [fw.sb([128, 512], F32, f"sgP{i}", esP) for i in range(2)]
                    tP = [fw.sb([128, 512], F32, f"tP{i}", esP) for i in range(2)]
                    outt = [fw.sb([128, D], F32, f"outt{i}", esP) for i in range(2)]
                    junkP = fw.sb([128, D], BF16, "junkP", esP)
                    ssp = [fw.sb([128, 1], F32, f"ssp{i}", esP) for i in range(5)]
                    rrp = [fw.sb([128, 1], F32, f"rrp{i}", esP) for i in range(5)]
                    x1T = [Buf(x1.t, f"x1_{t}") for t in range(NT_OWN)]
                    for b_ in x1T:
                        b_.lw = x1.lw
                        b_.rd = dict(x1.rd)

                    def p_s1(t):
                        r = t % 3
                        fw.dma(plb[r][:], pl_d[t * 128:(t + 1) * 128, :], writes=[plb[r]], q="pool")
                        rs = {"ss": ssp[r], "r": rrp[r]}
                        rms_rstd({"ap": x1[:, t, :], "bufs": [x1T[t]]}, rs, D, {"ap": junkP[:], "buf": junkP})
                        op("dve", lambda e: e.scalar_tensor_tensor(out=hpb[r][:], in0=x1[:, t, :], scalar=rs["r"][:], in1=gB[:], op0=ALU.mult, op1=ALU.mult),
                           reads=[x1T[t], rs["r"], gB], writes=[hpb[r]])

                    def p_s2(t):
                        r = t % 3
                        b0 = 2 * (t % 2)
                        for k in range(8):
                            op("pe", lambda e: e.transpose(out=psbf(b0)[:, k * 128:(k + 1) * 128], in_=hpb[r][:, k * 128:(k + 1) * 128], identity=idb[:]), reads=[hpb[r], idb], writes=[PS[b0]])
                        op("act", lambda e: e.copy(out=hpT[r][:], in_=psbf(b0).rearrange("p (k t) -> p k t", k=8)), reads=[PS[b0]], writes=[hpT[r]])
                        for k in range(2):
                            op("pe", lambda e: e.transpose(out=psbf(b0 + 1)[:, k * 128:(k + 1) * 128], in_=plb[r][:, k * 128:(k + 1) * 128], identity=idb[:]), reads=[plb[r], idb], writes=[PS[b0 + 1]])
                        op("act", lambda e: e.copy(out=plT[r][:], in_=psbf(b0 + 1)[:, 0:256].rearrange("p (k t) -> p k t", k=2)), reads=[PS[b0 + 1]], writes=[plT[r]])

                    def p_s3(t):
                        r = t % 3
                        for half in range(2):
                            hs = slice(half * 512, (half + 1) * 512)
                            bG = 4 + half
                            bP = 6 + half
                            for k in range(8):
                                mm(PS[bG], PS[bG][:, :], hpT[r][:, k, :], w_pg[:, k, hs], k == 0, k == 7, [hpT[r], w_pg])
                            for k in range(2):
                                mm(PS[bP], PS[bP][:, :], plT[r][:, k, :], w_pp[:, k, hs], k == 0, k == 1, [plT[r], w_pp])
                            op("act", lambda e: e.activation(out=sgP[half][:], in_=PS[bG][:, :], func=AF.Sigmoid), reads=[PS[bG]], writes=[sgP[half]])
                            op("dve", lambda e: e.tensor_tensor(out=tP[half][:], in0=PS[bP][:, :], in1=sgP[half][:], op=ALU.mult), reads=[PS[bP], sgP[half]], writes=[tP[half]])
                            op("dve", lambda e: e.tensor_tensor(out=x1[:, t, hs], in0=x1[:, t, hs], in1=tP[half][:], op=ALU.add), reads=[x1T[t], tP[half]], writes=[x1T[t]])
                        rs2 = {"ss": ssp[3 + t % 2], "r": rrp[3 + t % 2]}
                        rms_rstd({"ap": x1[:, t, :], "bufs": [x1T[t]]}, rs2, D, {"ap": junkP2[:], "buf": junkP2})
                        o_ = outt[t % 2]
                        op("dve", lambda e: e.scalar_tensor_tensor(out=o_[:], in0=x1[:, t, :], scalar=rs2["r"][:], in1=gB2[:], op0=ALU.mult, op1=ALU.mult),
                           reads=[x1T[t], rs2["r"], gB2], writes=[o_])
                        fw.dma(out_d[t * 128:(t + 1) * 128, :], o_[:], reads=[o_], is_output=True)

                    for i in range(NT_OWN + 2):
                        if i < NT_OWN:
                            p_s1(i)
                        if 1 <= i <= NT_OWN:
                            p_s2(i - 1)
                        if i >= 2:
                            p_s3(i - 2)

            if "yaT" in dbg:
                o = dbg_t("yaT", [128, 4, S_OWN], BF16)
                fw.dma(o[:, :, :], YT[:, 0:4, :], reads=[YT], is_output=True)

            if "hT" in dbg:
                o = dbg_t("hT", [128, 8, S_EXT], BF16)
                with fw.scope() as esd:
                    tmp = fw.sb([128, 8, 512], BF16, "dbg_hT", esd)
                    for i in range(8):
                        fw.dma(tmp[:], hT_d[:, :, i * 512:(i + 1) * 512], reads=hT_tiles[4 * i:4 * i + 4], writes=[tmp])
                        fw.dma(o[:, :, i * 512:(i + 1) * 512], tmp[:], reads=[tmp], is_output=True)


        body()
        fw.stopped = False
        fw.finish()
    return nc, dbg_out


_INV = (500000.0 ** (-np.arange(0, 16, 2, dtype=np.float32) / 16.0)).astype(np.float32)


def make_in_maps(inputs):
    f = lambda a: np.ascontiguousarray(np.asarray(a), dtype=np.float32)
    x = f(inputs["x"]); p = f(inputs["p"])
    positions = np.asarray(inputs["positions"]).astype(np.int32)
    w_in = f(inputs["w_in"])[0]
    offs = np.cumsum([0, 512, 128, 128, 128, 128, 128, 128, 24, 1024, 512, 512, 8, 2048])
    seg = {n: (offs[i], offs[i + 1]) for i, n in enumerate(["q", "kc", "vc", "ks", "vs", "kw", "vw", "gate", "qk", "v", "o", "if", "mg"])}
    col = lambda n: w_in[:, seg[n][0]:seg[n][1]]
    w_att = []
    for g in range(2):
        parts = [col("q")[:, g * 256:(g + 1) * 256]]
        for n in ["ks", "kw", "kc", "vc", "vs", "vw"]:
            parts.append(col(n)[:, g * 64:(g + 1) * 64])
        parts.append(col("gate")[:, g * 12:(g + 1) * 12])
        w_att.append(np.concatenate(parts, axis=1))
    w_att = np.ascontiguousarray(np.stack(w_att))
    shared = {
        "invf": np.ascontiguousarray(np.broadcast_to(_INV[None, :], (128, 8))),
        "gvec": np.ascontiguousarray(np.stack([f(inputs["g_mix"])[0], f(inputs["g_ffn"])[0], f(inputs["g_ple"])[0], f(inputs["g_final"])])),
        "w_att": w_att,
        "w_qk": np.ascontiguousarray(col("qk")),
        "w_vo": np.ascontiguousarray(np.concatenate([col("v"), col("o")], axis=1)),
        "w_if": np.ascontiguousarray(col("if")),
        "w_mg": np.ascontiguousarray(col("mg")),
        "b_if": f(inputs["b_if"]).reshape(1, 8),
        "w_c1": np.ascontiguousarray(np.stack([f(inputs["w_ck1"])[0], f(inputs["w_cv1"])[0]])),
        "w_c2": np.ascontiguousarray(np.stack([f(inputs["w_ck2"])[0], f(inputs["w_cv2"])[0]])),
        "pe_c": np.ascontiguousarray(np.stack([f(inputs["pe_ck"])[0], f(inputs["pe_cv"])[0]])),
        "wc": np.ascontiguousarray(f(inputs["w_conv"])[0].reshape(4, 8, 128).transpose(2, 1, 0)),
        "bc": np.ascontiguousarray(f(inputs["b_conv"])[0].reshape(8, 128).T),
        "g_hn": f(inputs["g_hn"]).reshape(1, 512),
        "w_pa": f(inputs["w_pa"])[0], "w_pb": f(inputs["w_pb"])[0], "w_out": f(inputs["w_out"])[0],
        "w_r": np.ascontiguousarray(np.concatenate([f(inputs["w_rg"])[0], f(inputs["w_re"])[0]], axis=1)),
        "b_r": np.ascontiguousarray(np.concatenate([f(inputs["b_rg"])[0], f(inputs["b_re"])[0]])[None, :]),
        "w_e13": f(inputs["w_e13"])[0], "w_e2": f(inputs["w_e2"])[0],
        "w_pg": f(inputs["w_pg"])[0], "w_pp": f(inputs["w_pp"])[0],
    }
    in_maps = []
    for core in range(8):
        b, half = core // 2, core % 2
        if half == 1:
            xe_ = x[b]
            pos_ = positions[b]
        else:
            xe_ = np.concatenate([np.zeros((S_OWN, D), np.float32), x[b, :S_OWN]], axis=0)
            pos_ = np.concatenate([np.zeros(S_OWN, np.int32), positions[b, :S_OWN]])
        m = dict(shared)
        m["xe"] = np.ascontiguousarray(xe_)
        m["pos"] = np.ascontiguousarray(pos_.reshape(NT_EXT, 128).T)
        m["pl"] = np.ascontiguousarray(p[0, b, half * S_OWN:(half + 1) * S_OWN])
        m["hv"] = np.full((128, 1), float(half), np.float32)
        in_maps.append(m)
    return in_maps


def kernel(**inputs):
    nc, _ = build_program()
    in_maps = make_in_maps(inputs)
    res = run_bass_kernel_spmd(nc, in_maps, core_ids=list(range(8)))
    out = np.zeros((4, S_EXT, D), np.float32)
    for core in range(8):
        b, half = core // 2, core % 2
        out[b, half * S_OWN:(half + 1) * S_OWN] = res.results[core]["out"]
    return out
```

```python
import numpy as np
import concourse.bass as bass
import concourse.mybir as mybir
from concourse.bass_utils import run_bass_kernel_spmd
from contextlib import ExitStack

F32 = mybir.dt.float32
BF16 = mybir.dt.bfloat16
I32 = mybir.dt.int32
AF = mybir.ActivationFunctionType
ALU = mybir.AluOpType
AX = mybir.AxisListType

D = 1024
S_OWN = 2048
S_EXT = 4096
NT_OWN = 16
NT_EXT = 32
EPS = 1e-6
NEGB = -30000.0
DBG = []


class Buf:
    __slots__ = ("t", "lw", "rd", "name", "excl")

    def __init__(self, t, name=""):
        self.t = t
        self.excl = False
        self.lw = None
        self.rd = {}
        self.name = name

    def __getitem__(self, k):
        return self.t[k]


class FW:
    NDMA = 24

    def __init__(self, nc, es):
        self.nc = nc
        self.es = es
        self.eng = {"pe": nc.tensor, "act": nc.scalar, "dve": nc.vector, "pool": nc.gpsimd, "sp": nc.sync}
        self.sem = {k: es.enter_context(nc.semaphore("s_" + k)) for k in self.eng}
        self.cnt = {k: 0 for k in self.eng}
        self.known = {k: {} for k in self.eng}
        self.dsem = [es.enter_context(nc.semaphore(f"s_dma{i}")) for i in range(self.NDMA)]
        self.dval = [0] * self.NDMA
        self.dnext = 0
        self.nbuf = 0
        self.out_waits = []
        self.stopped = False

    def sb(self, shape, dt, name=None, es=None):
        self.nbuf += 1
        name = f"sb{self.nbuf}_" + (name or "t")
        return Buf((es or self.es).enter_context(self.nc.sbuf_tensor(name, list(shape), dt)), name)

    def ps(self, shape, dt, name=None):
        self.nbuf += 1
        name = name or f"ps{self.nbuf}"
        b = Buf(self.es.enter_context(self.nc.psum_tensor(name, list(shape), dt)), name)
        b.excl = True
        return b

    def _wait(self, e, src, idx):
        if self.stopped:
            return
        kn = self.known[e]
        if kn.get(src, 0) >= idx:
            return
        s = self.dsem[src[1]] if isinstance(src, tuple) else self.sem[src]
        self.eng[e].wait_ge(s, idx)
        kn[src] = idx

    def _deps(self, e, reads, writes):
        for b in reads:
            if b.lw is not None:
                self._wait(e, b.lw[0], b.lw[1])
            if b.excl:
                for src, idx in b.rd.items():
                    if src != e:
                        self._wait(e, src, idx)
        for b in writes:
            if b.lw is not None and b.lw[0] != e:
                self._wait(e, b.lw[0], b.lw[1])
            for src, idx in b.rd.items():
                if src != e:
                    self._wait(e, src, idx)

    def op(self, e, fn, reads=(), writes=()):
        if self.stopped:
            return None
        self._deps(e, reads, writes)
        inst = fn(self.eng[e])
        self.cnt[e] += 1
        c = self.cnt[e]
        inst.then_inc(self.sem[e], 1)
        for b in reads:
            if b.rd.get(e, 0) < c:
                b.rd[e] = c
        for b in writes:
            b.lw = (e, c)
            b.rd = {}
        return inst

    def dma(self, out, in_, reads=(), writes=(), q="sp", is_output=False):
        if self.stopped and not is_output:
            return None
        self._deps(q, reads, writes)
        slot = self.dnext
        self.dnext = (self.dnext + 1) % self.NDMA
        key = ("d", slot)
        if self.dval[slot] > 0:
            self._wait(q, key, self.dval[slot])
        inst = self.eng[q].dma_start(out=out, in_=in_)
        self.dval[slot] += 16
        inst.then_inc(self.dsem[slot], 16)
        v = self.dval[slot]
        for b in reads:
            if b.rd.get(key, 0) < v:
                b.rd[key] = v
        for b in writes:
            b.lw = (key, v)
            b.rd = {}
        if is_output:
            self.out_waits.append((key, v))
        return inst

    def barrier(self):
        for e in self.eng:
            for src in ("pe", "act", "dve", "pool"):
                if src != e and self.cnt[src] > 0:
                    self._wait(e, src, self.cnt[src])
            for slot in range(self.NDMA):
                if self.dval[slot] > 0:
                    self._wait(e, ("d", slot), self.dval[slot])

    def scope(self):
        fw = self

        class _Scope(ExitStack):
            def __exit__(self, *a):
                fw.barrier()
                return super().__exit__(*a)
        return _Scope()

    def finish(self):
        for key, v in self.out_waits:
            self._wait("sp", key, v)
        for k in ("pe", "act", "dve", "pool"):
            if self.cnt[k] > 0:
                self._wait("sp", k, self.cnt[k])


class _StopBuild(Exception):
    pass


def build_program(dbg=()):
    nc = bass.Bass("TRN2", target_bir_lowering=False)

    def din(name, shape, dt=F32):
        return nc.dram_tensor(name, list(shape), dt, kind="ExternalInput").ap()

    xe = din("xe", [S_EXT, D])
    pos_d = din("pos", [128, NT_EXT], I32)
    pl_d = din("pl", [S_OWN, 256])
    hv_d = din("hv", [128, 1])
    invf_d = din("invf", [128, 8])
    gvec_d = din("gvec", [4, D])
    w_att_d = din("w_att", [2, D, 652])
    w_qk_d = din("w_qk", [D, 1024])
    w_vo_d = din("w_vo", [D, 1024])
    w_if_d = din("w_if", [D, 8])
    w_mg_d = din("w_mg", [D, 2048])
    b_if_d = din("b_if", [1, 8])
    w_c1_d = din("w_c1", [2, 2048, 256])
    w_c2_d = din("w_c2", [2, 256, 64])
    pe_c_d = din("pe_c", [2, 32, 64])
    wc_d = din("wc", [128, 8, 4])
    bc_d = din("bc", [128, 8])
    g_hn_d = din("g_hn", [1, 512])
    w_pa_d = din("w_pa", [512, D])
    w_pb_d = din("w_pb", [512, D])
    w_out_d = din("w_out", [D, D])
    w_r_d = din("w_r", [D, 20])
    b_r_d = din("b_r", [1, 20])
    w_e13_d = din("w_e13", [16, D, 512])
    w_e2_d = din("w_e2", [16, 256, D])
    w_pg_d = din("w_pg", [D, D])
    w_pp_d = din("w_pp", [256, D])
    out_d = nc.dram_tensor("out", [S_OWN, D], F32, kind="ExternalOutput").ap()
    hT_d = nc.dram_tensor("hT_scr", [128, 8, S_EXT], BF16, kind="Internal").ap()
    dbg_out = {}

    def dbg_t(name, shape, dt=F32):
        dbg_out[name] = nc.dram_tensor("dbg_" + name, list(shape), dt, kind="ExternalOutput").ap()
        return dbg_out[name]

    with ExitStack() as es:
        fw = FW(nc, es)
        op = fw.op
        PS = [fw.ps([128, 512], F32, f"psb{i}") for i in range(8)]

        def psbf(i):
            return PS[i][:].bitcast(BF16)

        ones_f = fw.sb([128, 128], F32, "ones_f")
        op("pool", lambda e: e.memset(ones_f[:], 1.0), writes=[ones_f])
        idf = fw.sb([128, 128], F32, "idf")
        op("pool", lambda e: e.affine_select(out=idf[:], in_=ones_f[:], pattern=[[1, 128]], compare_op=ALU.is_equal,
                                             fill=0.0, base=0, channel_multiplier=-1), reads=[ones_f], writes=[idf])
        idb = fw.sb([128, 128], BF16, "idb")
        op("dve", lambda e: e.tensor_copy(out=idb[:], in_=idf[:]), reads=[idf], writes=[idb])
        U_f = fw.sb([128, 128], F32, "U_f")
        op("pool", lambda e: e.affine_select(out=U_f[:], in_=ones_f[:], pattern=[[1, 128]], compare_op=ALU.is_ge,
                                             fill=0.0, base=0, channel_multiplier=-1), reads=[ones_f], writes=[U_f])
        caus = fw.sb([128, 128], BF16, "caus")
        op("dve", lambda e: e.tensor_copy(out=caus[:], in_=U_f[:]), reads=[U_f], writes=[caus])
        wm0_f = fw.sb([128, 128], F32, "wm0_f")
        op("pool", lambda e: e.affine_select(out=wm0_f[:], in_=ones_f[:], pattern=[[-1, 128]], compare_op=ALU.is_ge,
                                             fill=0.0, base=-1, channel_multiplier=1), reads=[ones_f], writes=[wm0_f])
        wm0 = fw.sb([128, 128], BF16, "wm0")
        op("dve", lambda e: e.tensor_copy(out=wm0[:], in_=wm0_f[:]), reads=[wm0_f], writes=[wm0])
        c_eps = fw.sb([128, 1], F32, "c_eps")
        op("pool", lambda e: e.memset(c_eps[:], EPS), writes=[c_eps])
        c_one = fw.sb([128, 1], F32, "c_one")
        op("pool", lambda e: e.memset(c_one[:], 1.0), writes=[c_one])
        c_zero = fw.sb([128, 1], F32, "c_zero")
        op("pool", lambda e: e.memset(c_zero[:], 0.0), writes=[c_zero])
        acc_junk = fw.sb([128, 2], F32, "acc_junk")
        op("act", lambda e: e.activation(out=acc_junk[:, 0:1], in_=c_one[:], func=AF.Square, accum_out=acc_junk[:, 1:2]),
           reads=[c_one], writes=[acc_junk])
        hv = fw.sb([128, 1], F32, "hv")
        fw.dma(hv[:], hv_d[:, :], writes=[hv])
        hbias = fw.sb([128, 1], F32, "hbias")
        op("dve", lambda e: e.tensor_scalar(out=hbias[:], in0=hv[:], scalar1=-1.0, scalar2=-NEGB, op0=ALU.add, op1=ALU.mult),
           reads=[hv], writes=[hbias])
        gB = fw.sb([128, D], F32, "gB")

        def load_gain(i):
            fw.dma(gB[:], gvec_d[i:i + 1, :].to_broadcast([128, D]), writes=[gB])

        cs = fw.sb([128, NT_EXT, 8], F32, "cs")
        sn = fw.sb([128, NT_EXT, 8], F32, "sn")
        with fw.scope() as es1:
            posi = fw.sb([128, NT_EXT], I32, "posi", es1)
            posf = fw.sb([128, NT_EXT], F32, "posf", es1)
            invf = fw.sb([128, 8], F32, "invf", es1)
            ang = fw.sb([128, NT_EXT, 8], F32, "ang", es1)
            kf = fw.sb([128, NT_EXT, 8], F32, "kf", es1)
            ki = fw.sb([128, NT_EXT, 8], I32, "ki", es1)
            r1 = fw.sb([128, NT_EXT, 8], F32, "r1", es1)
            r2 = fw.sb([128, NT_EXT, 8], F32, "r2", es1)
            fw.dma(posi[:], pos_d[:, :], writes=[posi])
            fw.dma(invf[:], invf_d[:, :], writes=[invf])
            op("dve", lambda e: e.tensor_copy(out=posf[:], in_=posi[:]), reads=[posi], writes=[posf])
            op("dve", lambda e: e.tensor_tensor(out=ang[:], in0=posf[:].unsqueeze(2).to_broadcast([128, NT_EXT, 8]),
                                                in1=invf[:].unsqueeze(1).to_broadcast([128, NT_EXT, 8]), op=ALU.mult),
               reads=[posf, invf], writes=[ang])
            TWO_PI = 6.283185307179586
            C1 = 6.28125
            C2 = TWO_PI - C1
            PI_LO = 3.1415925
            op("dve", lambda e: e.tensor_scalar(out=kf[:], in0=ang[:], scalar1=1.0 / TWO_PI, scalar2=None, op0=ALU.mult),
               reads=[ang], writes=[kf])
            op("dve", lambda e: e.tensor_copy(out=ki[:], in_=kf[:]), reads=[kf], writes=[ki])
            op("dve", lambda e: e.tensor_copy(out=kf[:], in_=ki[:]), reads=[ki], writes=[kf])
            op("dve", lambda e: e.scalar_tensor_tensor(out=r1[:], in0=kf[:], scalar=-C1, in1=ang[:], op0=ALU.mult, op1=ALU.add),
               reads=[kf, ang], writes=[r1])
            op("dve", lambda e: e.scalar_tensor_tensor(out=r1[:], in0=kf[:], scalar=-C2, in1=r1[:], op0=ALU.mult, op1=ALU.add),
               reads=[kf, r1], writes=[r1])
            op("dve", lambda e: e.tensor_scalar(out=r1[:], in0=r1[:], scalar1=PI_LO, scalar2=-PI_LO, op0=ALU.min, op1=ALU.max),
               reads=[r1], writes=[r1])
            op("act", lambda e: e.activation(out=sn[:], in_=r1[:], func=AF.Sin), reads=[r1], writes=[sn])
            op("dve", lambda e: e.tensor_scalar(out=r2[:], in0=r1[:], scalar1=PI_LO / 2 + 0.0, scalar2=None, op0=ALU.add),
               reads=[r1], writes=[r2])
            op("dve", lambda e: e.tensor_scalar(out=kf[:], in0=r2[:], scalar1=PI_LO, scalar2=-TWO_PI, op0=ALU.is_gt, op1=ALU.mult),
               reads=[r2], writes=[kf])
            op("dve", lambda e: e.tensor_tensor(out=r2[:], in0=r2[:], in1=kf[:], op=ALU.add), reads=[r2, kf], writes=[r2])
            op("dve", lambda e: e.tensor_scalar(out=r2[:], in0=r2[:], scalar1=PI_LO, scalar2=-PI_LO, op0=ALU.min, op1=ALU.max),
               reads=[r2], writes=[r2])
            op("act", lambda e: e.activation(out=cs[:], in_=r2[:], func=AF.Sin), reads=[r2], writes=[cs])

        def rms_rstd(src, rstd, n, junk):
            ss = rstd["ss"]
            op("act", lambda e: e.activation(out=junk["ap"], in_=src["ap"], func=AF.Square, accum_out=ss[:]),
               reads=src["bufs"], writes=[junk["buf"], ss])
            op("act", lambda e: e.activation(out=ss[:], in_=ss[:], func=AF.Sqrt, bias=c_eps[:], scale=1.0 / n),
               reads=[ss, c_eps], writes=[ss])
            op("dve", lambda e: e.reciprocal(out=rstd["r"][:], in_=ss[:]), reads=[ss], writes=[rstd["r"]])

        load_gain(0)
        hT_tiles = [Buf(None, f"hT_tile{t}") for t in range(NT_EXT)]
        with fw.scope() as esA:
            xt = [fw.sb([128, D], F32, f"xtA{i}", esA) for i in range(6)]
            xn = [fw.sb([128, D], BF16, f"xnA{i}", esA) for i in range(3)]
            junk = fw.sb([128, D], BF16, "junkA", esA)
            hst = [fw.sb([128, 8, 128], BF16, f"hstA{i}", esA) for i in range(4)]
            ssA = [fw.sb([128, 1], F32, f"ssA{i}", esA) for i in range(3)]
            rrA = [fw.sb([128, 1], F32, f"rrA{i}", esA) for i in range(3)]
            def a_s1(t):
                x_ = xt[t % 6]
                if t == 0:
                    for tt in range(5):
                        fw.dma(xt[tt][:], xe[tt * 128:(tt + 1) * 128, :], writes=[xt[tt]])
                if t + 5 < NT_EXT:
                    fw.dma(xt[(t + 5) % 6][:], xe[(t + 5) * 128:(t + 6) * 128, :], writes=[xt[(t + 5) % 6]])
                rs = {"ss": ssA[t % 3], "r": rrA[t % 3]}
                rms_rstd({"ap": x_[:], "bufs": [x_]}, rs, D, {"ap": junk[:], "buf": junk})
                n_ = xn[t % 3]
                op("dve", lambda e: e.scalar_tensor_tensor(out=n_[:], in0=x_[:], scalar=rs["r"][:], in1=gB[:], op0=ALU.mult, op1=ALU.mult),
                   reads=[x_, rs["r"], gB], writes=[n_])

            def a_s2(t):
                n_ = xn[t % 3]
                pb = t % 2
                for k in range(8):
                    op("pe", lambda e: e.transpose(out=psbf(pb)[:, k * 128:(k + 1) * 128], in_=n_[:, k * 128:(k + 1) * 128], identity=idb[:]),
                       reads=[n_, idb], writes=[PS[pb]])
                h_ = hst[t % 4]
                op("act", lambda e: e.copy(out=h_[:], in_=psbf(pb).rearrange("p (k t) -> p k t", k=8)), reads=[PS[pb]], writes=[h_])
                fw.dma(hT_d[:, :, t * 128:(t + 1) * 128], h_[:], reads=[h_], writes=[hT_tiles[t]], q="pool")

            for t in range(NT_EXT + 1):
                if t < NT_EXT:
                    a_s1(t)
                if t >= 1:
                    a_s2(t - 1)

        if "cs" in dbg:
            o = dbg_t("cs", [128, NT_EXT, 8])
            fw.dma(o[:, :, :], cs[:], reads=[cs], is_output=True)
            o = dbg_t("sn", [128, NT_EXT, 8])
            fw.dma(o[:, :, :], sn[:], reads=[sn], is_output=True)

        def ckpt(name):
            if ("stop_" + name) in dbg:
                fw.stopped = True

        def body():
            def mm(bank, out_ap, lhsT, rhs, start, stop, reads):
                op("pe", lambda e: e.matmul(out_ap, lhsT, rhs, start=start, stop=stop), reads=reads, writes=[bank])

            YT = fw.sb([128, 8, S_OWN], BF16, "YT")
            esBc = fw.scope()
            esBc.__enter__()
            cmask = fw.sb([128, 2, S_OWN], BF16, "cmask", esBc)
            op("pool", lambda e: e.memset(cmask[:], 1.0), writes=[cmask])
            op("pool", lambda e: e.affine_select(out=cmask[:, 0, :], in_=cmask[:, 0, :], pattern=[[1, S_OWN]], compare_op=ALU.is_ge, fill=0.0,
                                                 base=2017, channel_multiplier=-16), reads=[cmask], writes=[cmask])
            op("pool", lambda e: e.affine_select(out=cmask[:, 1, :], in_=cmask[:, 1, :], pattern=[[1, S_OWN]], compare_op=ALU.is_ge, fill=0.0,
                                                 base=-31, channel_multiplier=-16), reads=[cmask], writes=[cmask])
            ovl = fw.sb([128, 2, 64], BF16, "ovl", esBc)
            op("pool", lambda e: e.memset(ovl[:], 1.0), writes=[ovl])
            for j in range(2):
                op("pool", lambda e: e.affine_select(out=ovl[:, j, :], in_=ovl[:, j, :], pattern=[[-4, 64]], compare_op=ALU.is_ge, fill=0.0,
                                                     base=128 * j + 1, channel_multiplier=1), reads=[ovl], writes=[ovl])
                op("pool", lambda e: e.affine_select(out=ovl[:, j, :], in_=ovl[:, j, :], pattern=[[4, 64]], compare_op=ALU.is_ge, fill=0.0,
                                                     base=3 - 128 * j, channel_multiplier=-1), reads=[ovl], writes=[ovl])
            maskadd = fw.sb([128, NT_OWN, 64], F32, "maskadd", esBc)
            Mb = fw.sb([128, 64], F32, "Mb", esBc)
            hm1 = fw.sb([128, 2], F32, "hm1", esBc)
            op("dve", lambda e: e.tensor_scalar(out=hm1[:, 0:1], in0=hv[:], scalar1=-1.0, scalar2=1e30, op0=ALU.add, op1=ALU.mult),
               reads=[hv], writes=[hm1])
            op("dve", lambda e: e.tensor_scalar(out=hm1[:, 1:2], in0=hv[:], scalar1=-1.0, scalar2=-1000.0, op0=ALU.add, op1=ALU.mult),
               reads=[hv, hm1], writes=[hm1])
            op("dve", lambda e: e.memset(Mb[:], 0.0), writes=[Mb])
            op("dve", lambda e: e.tensor_copy(out=Mb[:, 0:32], in_=hm1[:, 0:1].to_broadcast([128, 32])), reads=[hm1, Mb], writes=[Mb])
            op("dve", lambda e: e.scalar_tensor_tensor(out=Mb[:, 0:1], in0=hv[:], scalar=1000.0, in1=Mb[:, 0:1], op0=ALU.mult, op1=ALU.add),
               reads=[hv, Mb], writes=[Mb])
            op("dve", lambda e: e.tensor_copy(out=Mb[:, 32:33], in_=hm1[:, 1:2]), reads=[hm1, Mb], writes=[Mb])
            for c in range(NT_OWN):
                op("pool", lambda e: e.tensor_copy(out=maskadd[:, c, :], in_=Mb[:]), reads=[Mb, maskadd], writes=[maskadd])
                for hf in range(2):
                    lo = 32 + 2 * c + hf + 1
                    if lo < 64:
                        op("pool", lambda e: e.memset(maskadd[hf * 64:(hf + 1) * 64, c, lo:64], -1e30), reads=[maskadd], writes=[maskadd])
                    for col in (32 + 2 * c + hf, 32 + 2 * c + hf - 1):
                        op("pool", lambda e: e.tensor_scalar(out=maskadd[hf * 64:(hf + 1) * 64, c, col:col + 1],
                                                             in0=maskadd[hf * 64:(hf + 1) * 64, c, col:col + 1],
                                                             scalar1=1000.0, scalar2=None, op0=ALU.add), reads=[maskadd], writes=[maskadd])

            ckpt("consts")
            for g in range(2):
                with fw.scope() as esG:
                    qT = fw.sb([128, 4, S_OWN], BF16, f"qT{g}", esG)
                    kkT = fw.sb([128, 2, S_EXT], BF16, f"kkT{g}", esG)
                    op("pool", lambda e: e.memset(qT[64:128, :, :], 0.0), writes=[qT])
                    op("pool", lambda e: e.memset(kkT[64:128, 0, :], 1.0), writes=[kkT])
                    op("pool", lambda e: e.memset(kkT[64:128, 1, :], 0.0), writes=[kkT])
                    op("pool", lambda e: e.affine_select(out=kkT[64:128, 0, :], in_=kkT[64:128, 0, :], pattern=[[1, S_EXT]], compare_op=ALU.is_ge, fill=0.0,
                                                         base=0, channel_multiplier=-64), reads=[kkT], writes=[kkT])
                    op("pool", lambda e: e.affine_select(out=kkT[64:128, 0, :], in_=kkT[64:128, 0, :], pattern=[[-1, S_EXT]], compare_op=ALU.is_ge, fill=0.0,
                                                         base=63, channel_multiplier=64), reads=[kkT], writes=[kkT])
                    vv = fw.sb([128, NT_EXT, 2, 65], BF16, f"vv{g}", esG)
                    gsig = fw.sb([128, NT_OWN, 12], F32, f"gsig{g}", esG)
                    kcmpT = fw.sb([128, 256], BF16, f"kcmpT{g}", esG)
                    op("pool", lambda e: e.memset(kcmpT[64:128, :], 0.0), writes=[kcmpT])
                    vca = fw.sb([128, 2, 65], BF16, f"vca{g}", esG)
                    op("pool", lambda e: e.memset(vv[:, :, :, 64:65], 1.0), writes=[vv])
                    op("pool", lambda e: e.memset(vca[:, :, 64:65], 1.0), writes=[vca])
                    ckpt("B0a")
                    with fw.scope() as esC:
                        ccT = fw.sb([64, 2, S_EXT], BF16, f"ccT{g}", esC)
                        with fw.scope() as esB1:
                            w_att = fw.sb([128, 8, 652], BF16, f"w_att{g}", esB1)
                            fw.dma(w_att[:], w_att_d[g].rearrange("(k p) c -> p k c", p=128), writes=[w_att], q="pool")
                            ckpt("B0b")
                            hblk = [fw.sb([128, 8, 512], BF16, f"hblkB{g}{i}", esB1) for i in range(2)]
                            rp = [fw.sb([128, 8, 64], BF16, f"rp{g}{i}", esB1) for i in range(2)]
                            rpf = [fw.sb([128, 8, 64], F32, f"rpf{g}{i}", esB1) for i in range(2)]
                            ta = [fw.sb([128, 7, 8], F32, f"ropa{g}{i}", esB1) for i in range(2)]
                            tb_ = [fw.sb([128, 7, 8], F32, f"ropb{g}{i}", esB1) for i in range(2)]
                            tcx = [fw.sb([128, 7, 8], F32, f"ropc{g}{i}", esB1) for i in range(2)]
                            tdx = [fw.sb([128, 7, 8], F32, f"ropd{g}{i}", esB1) for i in range(2)]
                            def b1_front(t):
                                own = t >= NT_OWN
                                tq = t - NT_OWN
                                hb = hblk[(t // 4) % 2]
                                if t % 4 == 0:
                                    fw.dma(hb[:], hT_d[:, :, t * 128:(t + 4) * 128], reads=hT_tiles[t:t + 4], writes=[hb])
                                tl = t % 4
                                a0 = 0 if own else 256
                                nb = 140 if own else 128
                                bA = 2 + t % 2
                                bB = 4 + t % 2
                                for k in range(8):
                                    mm(PS[bA], PS[bA][:, a0:512], hb[:, k, tl * 128:(tl + 1) * 128], w_att[:, k, a0:512], k == 0, k == 7, [hb, w_att])
                                for k in range(8):
                                    mm(PS[bB], PS[bB][:, 0:nb], hb[:, k, tl * 128:(tl + 1) * 128], w_att[:, k, 512:512 + nb], k == 0, k == 7, [hb, w_att])
                                rp_ = rp[t % 2]
                                h0 = a0 // 64
                                nh = 7 - h0
                                rf = rpf[t % 2]
                                op("act", lambda e: e.copy(out=rf[:, h0:8, :], in_=PS[bA][:, a0:512].rearrange("p (h d) -> p h d", d=64)),
                                   reads=[PS[bA]], writes=[rf])
                                op("pool", lambda e: e.tensor_copy(out=rp_[:, h0:8, :], in_=rf[:, h0:8, :]), reads=[rf], writes=[rp_])
                                t1 = rf[:, h0:7, 0:8]
                                t2 = rf[:, h0:7, 8:16]
                                Cb = cs[:, t, :].unsqueeze(1).to_broadcast([128, nh, 8])
                                Sb_ = sn[:, t, :].unsqueeze(1).to_broadcast([128, nh, 8])
                                ta_, tb2 = ta[t % 2], tb_[t % 2]
                                tc_, td_ = tcx[t % 2], tdx[t % 2]
                                op("dve", lambda e: e.tensor_tensor(out=ta_[:, 0:nh, :], in0=t1, in1=Cb, op=ALU.mult), reads=[rf, cs], writes=[ta_])
                                op("dve", lambda e: e.tensor_tensor(out=tb2[:, 0:nh, :], in0=t2, in1=Sb_, op=ALU.mult), reads=[rf, sn], writes=[tb2])
                                op("dve", lambda e: e.tensor_tensor(out=tc_[:, 0:nh, :], in0=t2, in1=Cb, op=ALU.mult), reads=[rf, cs], writes=[tc_])
                                op("dve", lambda e: e.tensor_tensor(out=td_[:, 0:nh, :], in0=t1, in1=Sb_, op=ALU.mult), reads=[rf, sn], writes=[td_])
                                op("dve", lambda e: e.tensor_tensor(out=rp_[:, h0:7, 0:8], in0=ta_[:, 0:nh, :], in1=tb2[:, 0:nh, :], op=ALU.subtract),
                                   reads=[ta_, tb2, rp_], writes=[rp_])
                                op("dve", lambda e: e.tensor_tensor(out=rp_[:, h0:7, 8:16], in0=tc_[:, 0:nh, :], in1=td_[:, 0:nh, :], op=ALU.add),
                                   reads=[tc_, td_, rp_], writes=[rp_])
                                op("dve", lambda e: e.tensor_copy(out=vv[:, t, :, 0:64], in_=PS[bB][:, 0:128].rearrange("p (h d) -> p h d", d=64)),
                                   reads=[PS[bB]], writes=[vv])
                                if own:
                                    op("act", lambda e: e.activation(out=gsig[:, tq, :], in_=PS[bB][:, 128:140], func=AF.Sigmoid),
                                       reads=[PS[bB]], writes=[gsig])

                            def b1_back(t):
                                own = t >= NT_OWN
                                tq = t - NT_OWN
                                rp_ = rp[t % 2]
                                h0 = 0 if own else 4
                                bT = t % 2
                                psT = psbf(bT)
                                for j, hh in enumerate(range(h0, 8)):
                                    op("pe", lambda e: e.transpose(out=psT[0:64, j * 128:(j + 1) * 128], in_=rp_[:, hh, :], identity=idb[:]),
                                       reads=[rp_, idb], writes=[PS[bT]])
                                if own:
                                    op("act", lambda e: e.copy(out=qT[0:64, :, tq * 128:(tq + 1) * 128], in_=psT[0:64, 0:512].rearrange("p (h t) -> p h t", h=4)),
                                       reads=[PS[bT]], writes=[qT])
                                    o1 = 512
                                else:
                                    o1 = 0
                                op("act", lambda e: e.copy(out=kkT[0:64, :, t * 128:(t + 1) * 128], in_=psT[0:64, o1:o1 + 256].rearrange("p (h t) -> p h t", h=2)),
                                   reads=[PS[bT]], writes=[kkT])
                                op("act", lambda e: e.copy(out=ccT[:, :, t * 128:(t + 1) * 128], in_=psT[0:64, o1 + 256:o1 + 512].rearrange("p (h t) -> p h t", h=2)),
                                   reads=[PS[bT]], writes=[ccT])

                            for t in range(NT_EXT + 1):
                                if t < NT_EXT:
                                    b1_front(t)
                                if t >= 1:
                                    b1_back(t - 1)
                        ckpt("B1")
                        for i in range(2):
                            with fw.scope() as esB2:
                                w1 = fw.sb([64, 32, 256], BF16, f"w1_{g}{i}", esB2)
                                fw.dma(w1[:], w_c1_d[i].rearrange("(l d) h -> d l h", d=64), writes=[w1], q="pool")
                                w2 = fw.sb([128, 2, 64], BF16, f"w2_{g}{i}", esB2)
                                fw.dma(w2[:], w_c2_d[i].rearrange("(c p) d -> p c d", p=128), writes=[w2], q="pool")
                                pe_sb = fw.sb([32, 64], BF16, f"pe_{g}{i}", esB2)
                                fw.dma(pe_sb[:], pe_c_d[i], writes=[pe_sb], q="pool")
                                peT = fw.sb([64, 32], BF16, f"peT_{g}{i}", esB2)
                                op("pe", lambda e: e.transpose(out=psbf(6)[0:64, 0:32], in_=pe_sb[:, :], identity=idb[0:32, 0:32]),
                                   reads=[pe_sb, idb], writes=[PS[6]])
                                op("act", lambda e: e.copy(out=peT[:], in_=psbf(6)[0:64, 0:32]), reads=[PS[6]], writes=[peT])
                                for hc in range(2):
                                    for l in range(32):
                                        mm(PS[7], PS[7][:, hc:hc + 1], w1[:, l, hc * 128:(hc + 1) * 128], peT[:, l:l + 1], l == 0, l == 31, [w1, peT])
                                cbs = fw.sb([128, 2], F32, f"cbs_{g}{i}", esB2)
                                op("act", lambda e: e.copy(out=cbs[:], in_=PS[7][:, 0:2]), reads=[PS[7]], writes=[cbs])
                                G = fw.sb([128, 2, 256], BF16, f"G_{g}{i}", esB2)
                                op("pool", lambda e: e.memset(G[:, :, 255:256], 0.0), writes=[G])
                                u_ = fw.sb([128, 255], F32, f"u_{g}{i}", esB2)
                                u2 = fw.sb([128, 255], F32, f"u2_{g}{i}", esB2)
                                sg_ = fw.sb([128, 255], F32, f"sg_{g}{i}", esB2)
                                for hc in range(2):
                                    for l in range(32):
                                        mm(PS[hc], PS[hc][:, 0:255], w1[:, l, hc * 128:(hc + 1) * 128], ccT[:, i, l:l + 16 * 254 + 1:16], l == 0, l == 31, [w1, ccT])
                                    op("act", lambda e: e.activation(out=u_[:], in_=PS[hc][:, 0:255], func=AF.Identity, bias=cbs[:, hc:hc + 1]),
                                       reads=[PS[hc], cbs], writes=[u_])
                                    op("dve", lambda e: e.tensor_tensor(out=u2[:], in0=u_[:], in1=u_[:], op=ALU.mult), reads=[u_], writes=[u2])
                                    op("dve", lambda e: e.tensor_scalar(out=u2[:], in0=u2[:], scalar1=0.044715, scalar2=1.0, op0=ALU.mult, op1=ALU.add),
                                       reads=[u2], writes=[u2])
                                    op("dve", lambda e: e.tensor_tensor(out=u2[:], in0=u2[:], in1=u_[:], op=ALU.mult), reads=[u2, u_], writes=[u2])
                                    op("act", lambda e: e.activation(out=sg_[:], in_=u2[:], func=AF.Sigmoid, scale=1.5957691216057308),
                                       reads=[u2], writes=[sg_])
                                    op("dve", lambda e: e.tensor_tensor(out=G[:, hc, 0:255], in0=u_[:], in1=sg_[:], op=ALU.mult), reads=[u_, sg_], writes=[G])
                                if i == 0:
                                    for hc in range(2):
                                        mm(PS[6], PS[6][0:64, 0:256], w2[:, hc, :], G[:, hc, :], hc == 0, hc == 1, [w2, G])
                                    op("act", lambda e: e.copy(out=kcmpT[0:64, :], in_=PS[6][0:64, 0:256]), reads=[PS[6]], writes=[kcmpT])
                                else:
                                    for nch in range(2):
                                        for hc in range(2):
                                            mm(PS[6], PS[6][:, nch * 64:(nch + 1) * 64], G[:, hc, nch * 128:(nch + 1) * 128], w2[:, hc, :], hc == 0, hc == 1, [w2, G])
                                    op("act", lambda e: e.copy(out=vca[:, :, 0:64], in_=PS[6][:, 0:128].rearrange("p (n d) -> p n d", d=64)),
                                       reads=[PS[6]], writes=[vca])
                    if g == 0 and "B2dump" in dbg:
                        for nm, bf, shp in (("kkT", kkT, [64, 2, S_EXT]), ("qT", qT, [64, 4, S_OWN]), ("vv", vv, [128, NT_EXT, 2, 65]),
                                            ("kcmpT", kcmpT, [64, 256]), ("vca", vca, [128, 2, 65])):
                            o = dbg_t(nm, shp, BF16)
                            fw.dma(o, bf[0:shp[0]], reads=[bf], is_output=True)
                        o = dbg_t("gsig", [128, NT_OWN, 12])
                        fw.dma(o, gsig[:], reads=[gsig], is_output=True)
                    ckpt("B2")
                    with fw.scope() as esB3:
                        NP = 4
                        LA = 2
                        Pb = [fw.sb([128, 512], BF16, f"Pb{g}{i}", esB3) for i in range(NP)]
                        Sbank = [0, 1, 6, 7]
                        hbS = [fw.sb([128, 4, 132], F32, f"hbS{g}{r}", esB3) for r in range(3)]
                        ya = [fw.sb([128, 4, 64], F32, f"ya{g}{i}", esB3) for i in range(2)]
                        yat = [fw.sb([128, 4, 64], BF16, f"yat{g}{i}", esB3) for i in range(2)]
                        sms = [fw.sb([128, 16], F32, f"sm{g}{i}", esB3) for i in range(3)]
                        rdc = fw.sb([128, 4], F32, f"rdc{g}", esB3)
                        impv = fw.sb([128, 64], F32, f"impv{g}", esB3)
                        wk = fw.sb([128, 64], F32, f"wk{g}", esB3)
                        m8a = fw.sb([128, 8], F32, f"m8a{g}", esB3)
                        m8b = fw.sb([128, 8], F32, f"m8b{g}", esB3)
                        negm2 = fw.sb([128, 128], BF16, f"negm{g}", esB3)
                        op("pool", lambda e: e.memset(negm2[:, 0:64], 0.0), writes=[negm2])
                        rot = [0]
                        REG = {0: (0, 129), 1: (129, 65), 2: (194, 65)}

                        def score(c, lhsT, lreads, extra, bias, mask):
                            r = rot[0] % NP
                            rot[0] += 1
                            sb_i = Sbank[r]
                            P = Pb[r]
                            qrhs = qT[:, :, c * 128:(c + 1) * 128]
                            S3 = PS[sb_i][:, :].rearrange("p (h q) -> p h q", h=4)
                            mm(PS[sb_i], S3, lhsT, qrhs, True, True, lreads + [qT])
                            op("act", lambda e: e.activation(out=P[:], in_=PS[sb_i][:, :], func=AF.Exp, bias=bias[:], scale=0.125),
                               reads=[PS[sb_i], bias], writes=[P])
                            if mask is not None:
                                op("dve", lambda e: e.tensor_tensor(out=P[:].rearrange("p (h q) -> p h q", h=4), in0=P[:].rearrange("p (h q) -> p h q", h=4),
                                                                    in1=mask[0], op=ALU.mult), reads=[P, mask[1]], writes=[P])
                            return P

                        def pv(P, h, reg, vr, vreads, cc, n, first, last):
                            op("pe", lambda e: e.matmul(PS[2 + h][:, cc:cc + n], P[:, h * 128:(h + 1) * 128], vr, start=first, stop=last),
                               reads=[P] + vreads, writes=[PS[2 + h]])

                        def evac_all(c, reg, br, first, final, mid=None):
                            col0, n = REG[reg]
                            hs = hbS[reg]
                            for h in range(4):
                                op("dve", lambda e: e.tensor_copy(out=hs[:, h, 0:n], in_=PS[2 + h][:, col0:col0 + n]), reads=[PS[2 + h]], writes=[hs])
                            sm = sms[reg]
                            yac = ya[c % 2]
                            dn = sm[:, 0:4]
                            rd = sm[:, 4:8] if br != 0 else rdc[:, 0:4]
                            rdb = sm if br != 0 else rdc
                            cf = sm[:, 8:12]
                            op("dve", lambda e: e.tensor_scalar(out=dn.unsqueeze(2), in0=hs[:, :, 64:65], scalar1=1e-30, scalar2=None, op0=ALU.max),
                               reads=[hs], writes=[sm])
                            op("dve", lambda e: e.reciprocal(out=rd, in_=dn), reads=[sm], writes=[rdb])
                            if mid is not None:
                                mid()
                            op("dve", lambda e: e.tensor_tensor(out=cf.unsqueeze(2), in0=rd.unsqueeze(2),
                                                                in1=gsig[:, c, :].rearrange("p (h b) -> p h b", b=3)[:, :, br:br + 1], op=ALU.mult),
                               reads=[sm, rdb, gsig], writes=[sm])
                            cfb = cf.unsqueeze(2).to_broadcast([128, 4, 64])
                            if first:
                                op("dve", lambda e: e.tensor_tensor(out=yac[:], in0=hs[:, :, 0:64], in1=cfb, op=ALU.mult), reads=[hs, sm], writes=[yac])
                            else:
                                op("dve", lambda e: e.tensor_tensor(out=hs[:, :, 0:64], in0=hs[:, :, 0:64], in1=cfb, op=ALU.mult), reads=[hs, sm], writes=[hs])
                                dst = yat[c % 2] if final else yac
                                op("dve", lambda e: e.tensor_tensor(out=dst[:], in0=hs[:, :, 0:64], in1=yac[:], op=ALU.add), reads=[hs, yac], writes=[dst])

                        pend = []

                        def flush():
                            while pend:
                                pend.pop(0)()

                        def pipe(score_fn, pv_fn):
                            P = score_fn()
                            while len(pend) >= LA:
                                pend.pop(0)()
                            pend.append(lambda: pv_fn(P))

                        def tr_slot():
                            r = rot[0] % NP
                            rot[0] += 1
                            return Sbank[r]

                        def cmp_scores_pv(c):
                            Pc = []
                            for nch in range(2):
                                mk = cmask[:, nch, c * 128:(c + 1) * 128].unsqueeze(1).to_broadcast([128, 4, 128])
                                Pc.append(score(c, kcmpT[:, nch * 128:(nch + 1) * 128], [kcmpT], None, hbias if nch == 0 else c_zero, (mk, cmask)))
                            flush()
                            for h in range(4):
                                for nch in range(2):
                                    pv(Pc[nch], h, 0, vca[:, nch, :], [vca], 0, 65, nch == 0, nch == 1)
                                for nch in range(2):
                                    pv(Pc[nch], h, 0, ovl[:, nch, :], [ovl], 65, 64, nch == 0, nch == 1)

                        def cmp_evac_topk(c):
                            def topk_chain():
                                op("dve", lambda e: e.tensor_tensor(out=hbS[0][:, :, 65:129], in0=hbS[0][:, :, 65:129], in1=rdc[:, 0:4].unsqueeze(2).to_broadcast([128, 4, 64]), op=ALU.mult),
                                   reads=[hbS[0], rdc], writes=[hbS[0]])
                                op("dve", lambda e: e.tensor_reduce(out=impv[:], in_=hbS[0][:, :, 65:129].rearrange("p h s -> p s h"), axis=AX.X, op=ALU.add),
                                   reads=[hbS[0]], writes=[impv])
                                op("dve", lambda e: e.tensor_tensor(out=impv[:], in0=impv[:], in1=maskadd[:, c, :], op=ALU.add), reads=[impv, maskadd], writes=[impv])
                                op("dve", lambda e: e.max(out=m8a[:], in_=impv[:]), reads=[impv], writes=[m8a])
                                op("dve", lambda e: e.match_replace(out=wk[:], in_to_replace=m8a[:], in_values=impv[:], imm_value=-3.0e38),
                                   reads=[impv, m8a], writes=[wk])
                                op("dve", lambda e: e.max(out=m8b[:], in_=wk[:]), reads=[wk], writes=[m8b])
                                op("dve", lambda e: e.tensor_scalar(out=negm2[:, 64:128], in0=impv[:], scalar1=m8b[:, 7:8], scalar2=NEGB, op0=ALU.is_lt, op1=ALU.mult),
                                   reads=[impv, m8b, negm2], writes=[negm2])
                            evac_all(c, 0, 0, True, False, mid=topk_chain)

                        def negm_to_q(c):
                            bk = tr_slot()
                            op("pe", lambda e: e.transpose(out=psbf(bk)[:, 0:128], in_=negm2[:, :], identity=idb[:]), reads=[negm2, idb], writes=[PS[bk]])
                            op("act", lambda e: e.copy(out=qT[64:128, :, c * 128:(c + 1) * 128], in_=psbf(bk)[64:128, 0:128].unsqueeze(1).to_broadcast([64, 4, 128])),
                               reads=[PS[bk]], writes=[qT])

                        def finish_tile(cp):
                            evac_all(cp, 1, 1, False, True)
                            bk = tr_slot()
                            for j in range(2):
                                op("pe", lambda e: e.transpose(out=psbf(bk)[:, j * 128:(j + 1) * 128],
                                                               in_=yat[cp % 2][:, 2 * j:2 * j + 2, :].rearrange("p h d -> p (h d)"), identity=idb[:]),
                                   reads=[yat[cp % 2], idb], writes=[PS[bk]])
                            op("act", lambda e: e.copy(out=YT[:, 2 * g:2 * g + 2, cp * 128:(cp + 1) * 128],
                                                       in_=psbf(bk)[:, 0:256].rearrange("p (j t) -> p j t", j=2)), reads=[PS[bk]], writes=[YT])

                        cmp_scores_pv(0)
                        cmp_evac_topk(0)
                        negm_to_q(0)
                        for c in range(NT_OWN):
                            for j in range(5):
                                ch = NT_OWN + c - 4 + j
                                mk = None
                                if j == 0:
                                    mk = (wm0[:].unsqueeze(1).to_broadcast([128, 4, 128]), wm0)
                                elif j == 4:
                                    mk = (caus[:].unsqueeze(1).to_broadcast([128, 4, 128]), caus)

                                def sfn(ch=ch, mk=mk):
                                    return score(c, kkT[:, 1, ch * 128:(ch + 1) * 128], [kkT], None, hbias if ch < NT_OWN else c_zero, mk)

                                def pfn(P, ch=ch, j=j):
                                    for h in range(4):
                                        pv(P, h, 2, vv[:, ch, 1, :], [vv], 194, 65, j == 0, j == 4)
                                pipe(sfn, pfn)
                                if j == 1 and c > 0:
                                    finish_tile(c - 1)
                            flush()
                            evac_all(c, 2, 2, False, False)
                            if c + 1 < NT_OWN:
                                cmp_scores_pv(c + 1)
                            chs = list(range(NT_OWN)) + [NT_OWN + j for j in range(c + 1)]
                            for i, ch in enumerate(chs):
                                mk = None
                                if ch == NT_OWN + c:
                                    mk = (caus[:].unsqueeze(1).to_broadcast([128, 4, 128]), caus)

                                def sfn(ch=ch, mk=mk):
                                    return score(c, kkT[:, 0, ch * 128:(ch + 1) * 128], [kkT], None, hbias if ch < NT_OWN else c_zero, mk)

                                def pfn(P, ch=ch, i=i, n=len(chs)):
                                    for h in range(4):
                                        pv(P, h, 1, vv[:, ch, 0, :], [vv], 129, 65, i == 0, i == n - 1)
                                pipe(sfn, pfn)
                                if c + 1 < NT_OWN:
                                    if i == 2:
                                        cmp_evac_topk(c + 1)
                                    elif i == 10:
                                        negm_to_q(c + 1)
                        flush()
                        finish_tile(NT_OWN - 1)
            esBc.__exit__(None, None, None)
            ckpt("B")
            with fw.scope() as esCg:
                ee = fw.sb([128, NT_EXT, 4], F32, "ee", esCg)
                ff = fw.sb([128, NT_EXT, 4], F32, "ff", esCg)
                fl = fw.sb([128, NT_EXT, 4], F32, "fl", esCg)
                ghn = fw.sb([128, 512], F32, "ghn", esCg)
                fw.dma(ghn[:], g_hn_d[0:1, :].to_broadcast([128, 512]), writes=[ghn])
                wcs = fw.sb([128, 8, 4], F32, "wcs", esCg)
                fw.dma(wcs[:], wc_d[:, :, :], writes=[wcs])
                bcs = fw.sb([128, 8], F32, "bcs", esCg)
                fw.dma(bcs[:], bc_d[:, :], writes=[bcs])
                with fw.scope() as esg:
                    w_if = fw.sb([128, 8, 8], BF16, "w_if", esg)
                    fw.dma(w_if[:], w_if_d.rearrange("(k p) c -> p k c", p=128), writes=[w_if], q="pool")
                    bif = fw.sb([128, 8], F32, "bif", esg)
                    fw.dma(bif[:], b_if_d[0:1, :].to_broadcast([128, 8]), writes=[bif])
                    hblk = [fw.sb([128, 8, 512], BF16, f"hblkG{i}", esg) for i in range(2)]
                    ifp = fw.sb([128, NT_EXT, 8], F32, "ifp", esg)
                    l1 = fw.sb([128, NT_EXT, 4], F32, "l1", esg)
                    tmpg = fw.sb([128, NT_EXT, 4], F32, "tmpg", esg)
                    for t in range(NT_EXT):
                        hb = hblk[(t // 4) % 2]
                        if t % 4 == 0:
                            fw.dma(hb[:], hT_d[:, :, t * 128:(t + 4) * 128], reads=hT_tiles[t:t + 4], writes=[hb])
                        tl = t % 4
                        for k in range(8):
                            mm(PS[0], PS[0][:, t * 8:(t + 1) * 8], hb[:, k, tl * 128:(tl + 1) * 128], w_if[:, k, :], k == 0, k == 7, [hb, w_if])
                    op("act", lambda e: e.copy(out=ifp[:], in_=PS[0][:, 0:256].rearrange("p (t c) -> p t c", c=8)), reads=[PS[0]], writes=[ifp])
                    op("dve", lambda e: e.tensor_tensor(out=ifp[:], in0=ifp[:], in1=bif[:].unsqueeze(1).to_broadcast([128, NT_EXT, 8]), op=ALU.add),
                       reads=[ifp, bif], writes=[ifp])
                    op("act", lambda e: e.activation(out=l1[:], in_=ifp[:, :, 4:8], func=AF.Exp, scale=-1.0), reads=[ifp], writes=[l1])
                    op("act", lambda e: e.activation(out=l1[:], in_=l1[:], func=AF.Ln, bias=c_one[:]), reads=[l1, c_one], writes=[l1])
                    l1f = l1[:].rearrange("p t c -> p (t c)")
                    mm(PS[1], PS[1][:, 0:128], U_f[:], l1f, True, True, [U_f, l1])
                    mm(PS[1], PS[1][:, 128:256], ones_f[:], l1f, True, True, [ones_f, l1])
                    op("act", lambda e: e.copy(out=tmpg[:], in_=PS[1][:, 0:128].rearrange("p (t c) -> p t c", c=4)), reads=[PS[1]], writes=[tmpg])
                    op("act", lambda e: e.activation(out=ff[:], in_=tmpg[:], func=AF.Exp, scale=-1.0), reads=[tmpg], writes=[ff])
                    op("act", lambda e: e.activation(out=fl[:], in_=PS[1][:, 128:256].rearrange("p (t c) -> p t c", c=4), func=AF.Exp, scale=-1.0),
                       reads=[PS[1]], writes=[fl])
                    op("dve", lambda e: e.tensor_tensor(out=tmpg[:], in0=tmpg[:], in1=ifp[:, :, 0:4], op=ALU.add), reads=[tmpg, ifp], writes=[tmpg])
                    op("act", lambda e: e.activation(out=ee[:], in_=tmpg[:], func=AF.Exp), reads=[tmpg], writes=[ee])
                    op("dve", lambda e: e.tensor_scalar(out=ee[:, 0:NT_OWN, :], in0=ee[:, 0:NT_OWN, :], scalar1=hv[:, 0:1], scalar2=None, op0=ALU.mult),
                       reads=[ee, hv], writes=[ee])
                ckpt("Cg")
                qTb = fw.sb([128, 4, S_OWN], BF16, "qTb", esCg)
                kTb = fw.sb([128, 4, S_EXT], BF16, "kTb", esCg)
                vaug = fw.sb([128, NT_EXT, 4, 129], BF16, "vaug", esCg)
                osig = fw.sb([128, NT_OWN, 512], BF16, "osig", esCg)
                op("pool", lambda e: e.memset(vaug[:, :, :, 128:129], 1.0), writes=[vaug])
                for hp in range(2):
                    with fw.scope() as esC1:
                        wq = fw.sb([128, 8, 256], BF16, f"wq{hp}", esC1)
                        wk = fw.sb([128, 8, 256], BF16, f"wk{hp}", esC1)
                        wvo = fw.sb([128, 8, 512], BF16, f"wvo{hp}", esC1)
                        fw.dma(wq[:], w_qk_d[:, hp * 256:(hp + 1) * 256].rearrange("(k p) c -> p k c", p=128), writes=[wq], q="pool")
                        fw.dma(wk[:], w_qk_d[:, 512 + hp * 256:512 + (hp + 1) * 256].rearrange("(k p) c -> p k c", p=128), writes=[wk], q="pool")
                        fw.dma(wvo[:, :, 0:256], w_vo_d[:, hp * 256:(hp + 1) * 256].rearrange("(k p) c -> p k c", p=128), writes=[wvo], q="pool")
                        fw.dma(wvo[:, :, 256:512], w_vo_d[:, 512 + hp * 256:512 + (hp + 1) * 256].rearrange("(k p) c -> p k c", p=128), writes=[wvo], q="pool")
                        hblk = [fw.sb([128, 8, 512], BF16, f"hblkC{hp}{i}", esC1) for i in range(2)]
                        uk = [fw.sb([128, 4 + S_EXT], BF16, f"uk{hp}{i}", esC1) for i in range(2)]
                        uq = [fw.sb([128, 4 + 2560], BF16, f"uq{hp}{i}", esC1) for i in range(2)]
                        ycv = [fw.sb([128, 512], F32, f"ycv{hp}{i}", esC1) for i in range(2)]
                        sgm = [fw.sb([128, 512], F32, f"sgm{hp}{i}", esC1) for i in range(2)]
                        for hh in range(2):
                            op("pool", lambda e: e.memset(uk[hh][:, 0:4], 0.0), writes=[uk[hh]])
                            op("pool", lambda e: e.memset(uq[hh][:, 0:4], 0.0), writes=[uq[hh]])
                        ukB = [[Buf(None, f"ukB{hp}{hh}{i}") for i in range(8)] for hh in range(2)]
                        uqB = [[Buf(None, f"uqB{hp}{hh}{i}") for i in range(5)] for hh in range(2)]
                        pi_ = [0]

                        def conv_piece(hh, typ, pc):
                            H = 2 * hp + hh
                            ci = typ * 4 + H
                            u = uq[hh] if typ == 0 else uk[hh]
                            if typ == 0:
                                off = 4 + 512 + pc * 512
                                ur = [uqB[hh][pc + 1], uqB[hh][pc]]
                            else:
                                off = 4 + pc * 512
                                ur = [ukB[hh][pc], ukB[hh][pc - 1] if pc > 0 else uk[hh]]
                            y_ = ycv[pi_[0] % 2]
                            s_ = sgm[pi_[0] % 2]
                            pi_[0] += 1
                            op("dve", lambda e: e.tensor_scalar(out=y_[:], in0=u[:, off - 3:off - 3 + 512], scalar1=wcs[:, ci, 0:1], scalar2=bcs[:, ci:ci + 1],
                                                                op0=ALU.mult, op1=ALU.add), reads=ur + [wcs, bcs], writes=[y_])
                            for j in range(1, 4):
                                op("dve", lambda e: e.scalar_tensor_tensor(out=y_[:], in0=u[:, off - 3 + j:off - 3 + j + 512], scalar=wcs[:, ci, j:j + 1], in1=y_[:],
                                                                           op0=ALU.mult, op1=ALU.add), reads=ur + [wcs, y_], writes=[y_])
                            if typ == 0:
                                op("act", lambda e: e.activation(out=qTb[:, H, pc * 512:(pc + 1) * 512], in_=y_[:], func=AF.Silu), reads=[y_], writes=[qTb])
                            else:
                                op("act", lambda e: e.activation(out=s_[:], in_=y_[:], func=AF.Sigmoid), reads=[y_], writes=[s_])
                                op("dve", lambda e: e.scalar_tensor_tensor(out=kTb[:, H, pc * 512:(pc + 1) * 512], in0=y_[:], scalar=128.0 ** -0.5, in1=s_[:],
                                                                           op0=ALU.mult, op1=ALU.mult), reads=[y_, s_], writes=[kTb])

                        def conv_for_block(bdone):
                            for hh in range(2):
                                conv_piece(hh, 1, bdone)
                                if bdone >= 4:
                                    conv_piece(hh, 0, bdone - 4)

                        for blk in range(8):
                            hb = hblk[blk % 2]
                            fw.dma(hb[:], hT_d[:, :, blk * 512:(blk + 1) * 512], reads=hT_tiles[4 * blk:4 * blk + 4], writes=[hb])
                            for hh in range(2):
                                for k in range(8):
                                    mm(PS[hh], PS[hh][:, :], wk[:, k, hh * 128:(hh + 1) * 128], hb[:, k, :], k == 0, k == 7, [wk, hb])
                                op("act", lambda e: e.copy(out=uk[hh][:, 4 + blk * 512:4 + (blk + 1) * 512], in_=PS[hh][:, :]), reads=[PS[hh]], writes=[ukB[hh][blk]])
                            if blk >= 3:
                                for hh in range(2):
                                    for k in range(8):
                                        mm(PS[2 + hh], PS[2 + hh][:, :], wq[:, k, hh * 128:(hh + 1) * 128], hb[:, k, :], k == 0, k == 7, [wq, hb])
                                    op("act", lambda e: e.copy(out=uq[hh][:, 4 + (blk - 3) * 512:4 + (blk - 2) * 512], in_=PS[2 + hh][:, :]),
                                       reads=[PS[2 + hh]], writes=[uqB[hh][blk - 3]])
                            for tl in range(4):
                                t = blk * 4 + tl
                                bv = 4 + tl
                                nvo = 512 if blk >= 4 else 256
                                for k in range(8):
                                    mm(PS[bv], PS[bv][:, 0:nvo], hb[:, k, tl * 128:(tl + 1) * 128], wvo[:, k, 0:nvo], k == 0, k == 7, [wvo, hb])
                                op("dve", lambda e: e.tensor_copy(out=vaug[:, t, 2 * hp:2 * hp + 2, 0:128], in_=PS[bv][:, 0:256].rearrange("p (h d) -> p h d", d=128)),
                                   reads=[PS[bv]], writes=[vaug])
                                if blk >= 4:
                                    op("act", lambda e: e.activation(out=osig[:, t - NT_OWN, hp * 256:(hp + 1) * 256], in_=PS[bv][:, 256:512], func=AF.Sigmoid),
                                       reads=[PS[bv]], writes=[osig])
                            if blk >= 1:
                                conv_for_block(blk - 1)
                        conv_for_block(7)
                ckpt("C1")
                with fw.scope() as esC3:
                    ktokR = [fw.sb([128, 4, 128], BF16, f"ktokR{i}", esC3) for i in range(3)]
                    CTall = fw.sb([128, NT_OWN, 4, 129], BF16, "CTall", esC3)
                    Xs = [fw.sb([128, 129], F32, f"Xs{H}", esC3) for H in range(4)]
                    Sm = [[fw.sb([128, 128], BF16, f"Sm{H}{i}", esC3) for i in range(2)] for H in range(4)]
                    hm_ = [fw.sb([128, 128], F32, f"hm{H}", esC3) for H in range(4)]
                    yb_ = [fw.sb([128, 128], BF16, f"yb{H}", esC3) for H in range(4)]
                    jk = [fw.sb([128, 128], BF16, f"jk{H}", esC3) for H in range(4)]
                    smc = [fw.sb([128, 8], F32, f"smc{H}", esC3) for H in range(4)]
                    for H in range(4):
                        op("dve", lambda e: e.tensor_tensor(out=vaug[:, :, H, :], in0=vaug[:, :, H, :],
                                                            in1=ee[:, :, H:H + 1].to_broadcast([128, NT_EXT, 129]), op=ALU.mult), reads=[vaug, ee], writes=[vaug])

                    def k_tr(t):
                        bk = t % 2
                        for H in range(4):
                            op("pe", lambda e: e.transpose(out=psbf(bk)[:, H * 128:(H + 1) * 128], in_=kTb[:, H, t * 128:(t + 1) * 128], identity=idb[:]),
                               reads=[kTb, idb], writes=[PS[bk]])
                        op("act", lambda e: e.copy(out=ktokR[t % 3][:], in_=psbf(bk)[:, 0:512].rearrange("p (h d) -> p h d", d=128)), reads=[PS[bk]], writes=[ktokR[t % 3]])

                    k_tr(0)
                    for t in range(NT_EXT - 1):
                        if t + 1 < NT_EXT - 1:
                            k_tr(t + 1)
                        for H in range(4):
                            bU = 2 + H
                            mm(PS[bU], PS[bU][:, 0:129], ktokR[t % 3][:, H, :], vaug[:, t, H, :], True, True, [ktokR[t % 3], vaug])
                            if t == 0:
                                op("dve", lambda e: e.tensor_copy(out=Xs[H][:], in_=PS[bU][:, 0:129]), reads=[PS[bU]], writes=[Xs[H]])
                            else:
                                op("dve", lambda e: e.scalar_tensor_tensor(out=Xs[H][:], in0=Xs[H][:], scalar=fl[:, t - 1, H:H + 1], in1=PS[bU][:, 0:129],
                                                                           op0=ALU.mult, op1=ALU.add), reads=[Xs[H], fl, PS[bU]], writes=[Xs[H]])
                            if t + 1 >= NT_OWN:
                                op("act", lambda e: e.activation(out=CTall[:, t + 1 - NT_OWN, H, :], in_=Xs[H][:], func=AF.Copy, scale=fl[:, t, H:H + 1]),
                                   reads=[Xs[H], fl], writes=[CTall])
                    sc4 = fw.sb([128, 4, 8], F32, "sc4", esC3)

                    def stA(t):
                        tq = t - NT_OWN
                        for H in range(4):
                            sm_ = Sm[H][tq % 2]
                            mm(PS[H], PS[H][:, 0:128], kTb[:, H, t * 128:(t + 1) * 128], qTb[:, H, tq * 128:(tq + 1) * 128], True, True, [kTb, qTb])
                            op("dve", lambda e: e.tensor_tensor(out=sm_[:], in0=PS[H][:, 0:128], in1=caus[:], op=ALU.mult), reads=[PS[H], caus], writes=[sm_])

                    def stRest(t):
                        tq = t - NT_OWN
                        for H in range(4):
                            sm_ = Sm[H][tq % 2]
                            bA = 4 + H
                            mm(PS[bA], PS[bA][:, 0:129], sm_[:], vaug[:, t, H, :], True, False, [sm_, vaug])
                            mm(PS[bA], PS[bA][:, 0:129], qTb[:, H, tq * 128:(tq + 1) * 128], CTall[:, tq, H, :], False, True, [qTb, CTall])
                        for H in range(4):
                            op("act", lambda e: e.activation(out=sc4[:, H, 6:7], in_=PS[4 + H][:, 128:129], func=AF.Abs, scale=ff[:, t, H:H + 1]),
                               reads=[PS[4 + H], ff], writes=[sc4])
                        op("dve", lambda e: e.tensor_scalar(out=sc4[:, :, 0:1], in0=sc4[:, :, 6:7], scalar1=1.0, scalar2=None, op0=ALU.max), reads=[sc4], writes=[sc4])
                        op("dve", lambda e: e.reciprocal(out=sc4[:, :, 1:2], in_=sc4[:, :, 0:1]), reads=[sc4], writes=[sc4])
                        op("dve", lambda e: e.tensor_tensor(out=sc4[:, :, 2:3], in0=sc4[:, :, 1:2], in1=ff[:, t, :].unsqueeze(2), op=ALU.mult), reads=[sc4, ff], writes=[sc4])
                        for H in range(4):
                            op("dve", lambda e: e.scalar_tensor_tensor(out=hm_[H][:], in0=PS[4 + H][:, 0:128], scalar=sc4[:, H, 2:3], in1=osig[:, tq, H * 128:(H + 1) * 128],
                                                                       op0=ALU.mult, op1=ALU.mult), reads=[PS[4 + H], sc4, osig], writes=[hm_[H]])
                        for H in range(4):
                            op("act", lambda e: e.activation(out=jk[H][:], in_=hm_[H][:], func=AF.Square, accum_out=sc4[:, H, 3:4]), reads=[hm_[H]], writes=[jk[H], sc4])
                        op("act", lambda e: e.activation(out=sc4[:, :, 4:5], in_=sc4[:, :, 3:4], func=AF.Sqrt, bias=c_eps[:], scale=1.0 / 128), reads=[sc4, c_eps], writes=[sc4])
                        op("dve", lambda e: e.reciprocal(out=sc4[:, :, 5:6], in_=sc4[:, :, 4:5]), reads=[sc4], writes=[sc4])
                        for H in range(4):
                            op("dve", lambda e: e.scalar_tensor_tensor(out=yb_[H][:], in0=hm_[H][:], scalar=sc4[:, H, 5:6], in1=ghn[:, H * 128:(H + 1) * 128],
                                                                       op0=ALU.mult, op1=ALU.mult), reads=[hm_[H], sc4, ghn], writes=[yb_[H]])
                        for H in range(4):
                            op("pe", lambda e: e.transpose(out=psbf(4 + H)[:, 512:640], in_=yb_[H][:], identity=idb[:]), reads=[yb_[H], idb], writes=[PS[4 + H]])
                        for H in range(4):
                            op("act", lambda e: e.copy(out=YT[:, 4 + H, tq * 128:(tq + 1) * 128], in_=psbf(4 + H)[:, 512:640]), reads=[PS[4 + H]], writes=[YT])

                    stA(NT_OWN)
                    for t in range(NT_OWN, NT_EXT):
                        if t + 1 < NT_EXT:
                            stA(t + 1)
                        stRest(t)
            ckpt("C")
            if "ybT" in dbg:
                o = dbg_t("ybT", [128, 4, S_OWN], BF16)
                fw.dma(o[:, :, :], YT[:, 4:8, :], reads=[YT], is_output=True)


            with fw.scope() as esD:
                x1 = fw.sb([128, NT_OWN, D], F32, "x1", esD)
                with fw.scope() as esD1:
                    mixT = fw.sb([128, 8, S_OWN], BF16, "mixT", esD1)
                    with fw.scope() as esD1a:
                        hTo = fw.sb([128, 8, S_OWN], BF16, "hTo", esD1a)
                        for tb in range(4):
                            fw.dma(hTo[:, :, tb * 512:(tb + 1) * 512], hT_d[:, :, S_OWN + tb * 512:S_OWN + (tb + 1) * 512],
                                   reads=hT_tiles[NT_OWN + 4 * tb:NT_OWN + 4 * tb + 4], writes=[hTo])
                        wga = [fw.sb([128, 8, 128], BF16, f"wga{i}", esD1a) for i in range(2)]
                        wgb = [fw.sb([128, 8, 128], BF16, f"wgb{i}", esD1a) for i in range(2)]
                        wpa = [fw.sb([128, 4, 128], BF16, f"wpa{i}", esD1a) for i in range(2)]
                        wpb = [fw.sb([128, 4, 128], BF16, f"wpb{i}", esD1a) for i in range(2)]
                        sga = [fw.sb([128, 512], BF16, f"sga{i}", esD1a) for i in range(2)]
                        sgb = [fw.sb([128, 512], BF16, f"sgb{i}", esD1a) for i in range(2)]
                        t1 = [fw.sb([128, 512], F32, f"t1_{i}", esD1a) for i in range(2)]
                        t2 = [fw.sb([128, 512], F32, f"t2_{i}", esD1a) for i in range(2)]
                        it = 0
                        for j in range(8):
                            w_ = j % 2
                            fw.dma(wga[w_][:], w_mg_d[:, j * 128:(j + 1) * 128].rearrange("(k p) c -> p k c", p=128), writes=[wga[w_]], q="pool")
                            fw.dma(wgb[w_][:], w_mg_d[:, 1024 + j * 128:1024 + (j + 1) * 128].rearrange("(k p) c -> p k c", p=128), writes=[wgb[w_]], q="pool")
                            fw.dma(wpa[w_][:], w_pa_d[:, j * 128:(j + 1) * 128].rearrange("(k p) c -> p k c", p=128), writes=[wpa[w_]], q="pool")
                            fw.dma(wpb[w_][:], w_pb_d[:, j * 128:(j + 1) * 128].rearrange("(k p) c -> p k c", p=128), writes=[wpb[w_]], q="pool")
                            for tb in range(4):
                                r = it % 2
                                it += 1
                                b0 = 4 * r
                                ts_ = slice(tb * 512, (tb + 1) * 512)
                                for k in range(8):
                                    mm(PS[b0], PS[b0][:, :], wga[w_][:, k, :], hTo[:, k, ts_], k == 0, k == 7, [wga[w_], hTo])
                                op("act", lambda e: e.activation(out=sga[r][:], in_=PS[b0][:, :], func=AF.Sigmoid), reads=[PS[b0]], writes=[sga[r]])
                                for k in range(8):
                                    mm(PS[b0 + 1], PS[b0 + 1][:, :], wgb[w_][:, k, :], hTo[:, k, ts_], k == 0, k == 7, [wgb[w_], hTo])
                                op("act", lambda e: e.activation(out=sgb[r][:], in_=PS[b0 + 1][:, :], func=AF.Sigmoid), reads=[PS[b0 + 1]], writes=[sgb[r]])
                                for k in range(4):
                                    mm(PS[b0 + 2], PS[b0 + 2][:, :], wpa[w_][:, k, :], YT[:, k, ts_], k == 0, k == 3, [wpa[w_], YT])
                                for k in range(4):
                                    mm(PS[b0 + 3], PS[b0 + 3][:, :], wpb[w_][:, k, :], YT[:, 4 + k, ts_], k == 0, k == 3, [wpb[w_], YT])
                                op("dve", lambda e: e.tensor_tensor(out=t1[r][:], in0=PS[b0 + 2][:, :], in1=sga[r][:], op=ALU.mult), reads=[PS[b0 + 2], sga[r]], writes=[t1[r]])
                                op("dve", lambda e: e.tensor_tensor(out=t2[r][:], in0=PS[b0 + 3][:, :], in1=sgb[r][:], op=ALU.mult), reads=[PS[b0 + 3], sgb[r]], writes=[t2[r]])
                                op("pool", lambda e: e.tensor_tensor(out=mixT[:, j, ts_], in0=t1[r][:], in1=t2[r][:], op=ALU.add), reads=[t1[r], t2[r]], writes=[mixT])
                    ckpt("D1a")
                    with fw.scope() as esD1b:
                        w_out = fw.sb([128, 8, D], BF16, "w_out", esD1b)
                        fw.dma(w_out[:], w_out_d.rearrange("(k p) c -> p k c", p=128), writes=[w_out], q="pool")
                        xtl = [fw.sb([128, D], F32, f"xtl{i}", esD1b) for i in range(2)]
                        for t in range(NT_OWN):
                            x_ = xtl[t % 2]
                            fw.dma(x_[:], xe[S_OWN + t * 128:S_OWN + (t + 1) * 128, :], writes=[x_])
                            for half in range(2):
                                b = 2 * (t % 2) + half
                                for j in range(8):
                                    mm(PS[b], PS[b][:, :], mixT[:, j, t * 128:(t + 1) * 128], w_out[:, j, half * 512:(half + 1) * 512], j == 0, j == 7, [mixT, w_out])
                                op("dve", lambda e: e.tensor_tensor(out=x1[:, t, half * 512:(half + 1) * 512], in0=PS[b][:, :], in1=x_[:, half * 512:(half + 1) * 512], op=ALU.add),
                                   reads=[PS[b], x_], writes=[x1])
                ckpt("D1")
                if "x1" in dbg:
                    fw.dma(dbg_t("x1", [128, NT_OWN, D]), x1[:], reads=[x1], is_output=True)
                with fw.scope() as esM:
                    load_gain(1)
                    gateT = fw.sb([16, S_OWN], BF16, "gateT", esM)
                    E16 = fw.sb([16, 16, 128], BF16, "E16", esM)
                    op("pool", lambda e: e.memset(E16[:], 1.0), writes=[E16])
                    op("pool", lambda e: e.affine_select(out=E16[:], in_=E16[:], pattern=[[-1, 16], [0, 128]], compare_op=ALU.is_equal, fill=0.0,
                                                         base=0, channel_multiplier=1), reads=[E16], writes=[E16])
                    with fw.scope() as esR:
                        w_r = fw.sb([128, 8, 20], F32, "w_r", esR)
                        fw.dma(w_r[:], w_r_d.rearrange("(k p) c -> p k c", p=128), writes=[w_r])
                        b_r = fw.sb([128, 20], F32, "b_r", esR)
                        fw.dma(b_r[:], b_r_d[0:1, :].to_broadcast([128, 20]), writes=[b_r])
                        hnf = [fw.sb([128, D], F32, f"hnf{i}", esR) for i in range(2)]
                        hnTf = [fw.sb([128, 8, 128], F32, f"hnTf{i}", esR) for i in range(2)]
                        junkR = fw.sb([128, D], BF16, "junkR", esR)
                        ssr = [fw.sb([128, 1], F32, f"ssr{i}", esR) for i in range(2)]
                        rrr = [fw.sb([128, 1], F32, f"rrr{i}", esR) for i in range(2)]
                        T_ = NT_OWN
                        lgA = fw.sb([128, T_, 20], F32, "lgA", esR)

                        def r_front(t):
                            r = t % 2
                            rs = {"ss": ssr[r], "r": rrr[r]}
                            rms_rstd({"ap": x1[:, t, :], "bufs": [x1]}, rs, D, {"ap": junkR[:], "buf": junkR})
                            op("dve", lambda e: e.scalar_tensor_tensor(out=hnf[r][:], in0=x1[:, t, :], scalar=rs["r"][:], in1=gB[:], op0=ALU.mult, op1=ALU.mult),
                               reads=[x1, rs["r"], gB], writes=[hnf[r]])
                            for k in range(8):
                                b = 2 * r + (0 if k < 4 else 1)
                                op("pe", lambda e: e.transpose(out=PS[b][:, (k % 4) * 128:(k % 4 + 1) * 128], in_=hnf[r][:, k * 128:(k + 1) * 128], identity=idf[:]),
                                   reads=[hnf[r], idf], writes=[PS[b]])
                            for bb in range(2):
                                b = 2 * r + bb
                                op("act", lambda e: e.copy(out=hnTf[r][:, 4 * bb:4 * bb + 4, :], in_=PS[b][:, :].rearrange("p (k t) -> p k t", k=4)), reads=[PS[b]], writes=[hnTf[r]])
                                op("dve", lambda e: e.tensor_copy(out=YT[:, 4 * bb:4 * bb + 4, t * 128:(t + 1) * 128], in_=PS[b][:, :].rearrange("p (k t) -> p k t", k=4)),
                                   reads=[PS[b]], writes=[YT])

                        def r_back(t):
                            r = t % 2
                            bl = 4 + r
                            for k in range(8):
                                mm(PS[bl], PS[bl][:, 0:20], hnTf[r][:, k, :], w_r[:, k, :], k == 0, k == 7, [hnTf[r], w_r])
                            op("dve", lambda e: e.tensor_tensor(out=lgA[:, t, :], in0=PS[bl][:, 0:20], in1=b_r[:], op=ALU.add), reads=[PS[bl], b_r], writes=[lgA])

                        for t in range(T_ + 1):
                            if t < T_:
                                r_front(t)
                            if t >= 1:
                                r_back(t - 1)
                        gl = lgA[:, :, 0:4]
                        el = lgA[:, :, 4:20].rearrange("p t (g e) -> p t g e", g=4)
                        gmax = fw.sb([128, T_], F32, "gmax", esR)
                        g1h = fw.sb([128, T_, 4], F32, "g1h", esR)
                        exg = fw.sb([128, T_, 4], F32, "exg", esR)
                        pgs = fw.sb([128, T_], F32, "pgs", esR)
                        t16 = fw.sb([128, T_, 4, 4], F32, "t16", esR)
                        elg = fw.sb([128, T_, 4], F32, "elg", esR)
                        elg2 = fw.sb([128, T_, 4], F32, "elg2", esR)
                        ev1 = fw.sb([128, T_], F32, "ev1", esR)
                        ev2 = fw.sb([128, T_], F32, "ev2", esR)
                        mk1 = fw.sb([128, T_, 4], F32, "mk1", esR)
                        mk2 = fw.sb([128, T_, 4], F32, "mk2", esR)
                        w12 = fw.sb([128, 2, T_], F32, "w12", esR)
                        gig = fw.sb([128, T_, 4], F32, "gig", esR)
                        gate = fw.sb([128, T_, 4, 4], F32, "gate", esR)
                        B3 = [128, T_, 4]
                        op("dve", lambda e: e.tensor_reduce(out=gmax[:], in_=gl, axis=AX.X, op=ALU.max), reads=[lgA], writes=[gmax])
                        op("dve", lambda e: e.tensor_tensor(out=g1h[:], in0=gl, in1=gmax[:].unsqueeze(2).to_broadcast(B3), op=ALU.is_equal), reads=[lgA, gmax], writes=[g1h])
                        op("dve", lambda e: e.tensor_tensor(out=exg[:], in0=gl, in1=gmax[:].unsqueeze(2).to_broadcast(B3), op=ALU.subtract), reads=[lgA, gmax], writes=[exg])
                        op("act", lambda e: e.activation(out=exg[:], in_=exg[:], func=AF.Exp), reads=[exg], writes=[exg])
                        op("dve", lambda e: e.tensor_reduce(out=pgs[:], in_=exg[:], axis=AX.X, op=ALU.add), reads=[exg], writes=[pgs])
                        op("dve", lambda e: e.reciprocal(out=pgs[:], in_=pgs[:]), reads=[pgs], writes=[pgs])
                        op("dve", lambda e: e.tensor_tensor(out=t16[:], in0=el, in1=g1h[:].unsqueeze(3).to_broadcast([128, T_, 4, 4]), op=ALU.mult), reads=[lgA, g1h], writes=[t16])
                        op("dve", lambda e: e.tensor_reduce(out=elg[:], in_=t16[:].rearrange("p t g e -> p t e g"), axis=AX.X, op=ALU.add), reads=[t16], writes=[elg])
                        op("dve", lambda e: e.tensor_reduce(out=ev1[:], in_=elg[:], axis=AX.X, op=ALU.max), reads=[elg], writes=[ev1])
                        op("dve", lambda e: e.tensor_tensor(out=mk1[:], in0=elg[:], in1=ev1[:].unsqueeze(2).to_broadcast(B3), op=ALU.is_equal), reads=[elg, ev1], writes=[mk1])
                        op("dve", lambda e: e.scalar_tensor_tensor(out=elg2[:], in0=mk1[:], scalar=-1e30, in1=elg[:], op0=ALU.mult, op1=ALU.add), reads=[mk1, elg], writes=[elg2])
                        op("dve", lambda e: e.tensor_reduce(out=ev2[:], in_=elg2[:], axis=AX.X, op=ALU.max), reads=[elg2], writes=[ev2])
                        op("dve", lambda e: e.tensor_tensor(out=mk2[:], in0=elg2[:], in1=ev2[:].unsqueeze(2).to_broadcast(B3), op=ALU.is_equal), reads=[elg2, ev2], writes=[mk2])
                        op("dve", lambda e: e.tensor_tensor(out=w12[:, 0, :], in0=ev1[:], in1=ev2[:], op=ALU.subtract), reads=[ev1, ev2], writes=[w12])
                        op("act", lambda e: e.activation(out=w12[:, 0, :], in_=w12[:, 0, :], func=AF.Sigmoid), reads=[w12], writes=[w12])
                        op("dve", lambda e: e.tensor_scalar(out=w12[:, 1, :], in0=w12[:, 0, :], scalar1=-1.0, scalar2=1.0, op0=ALU.mult, op1=ALU.add), reads=[w12], writes=[w12])
                        op("dve", lambda e: e.tensor_tensor(out=w12[:], in0=w12[:], in1=pgs[:].unsqueeze(1).to_broadcast([128, 2, T_]), op=ALU.mult), reads=[w12, pgs], writes=[w12])
                        op("dve", lambda e: e.tensor_tensor(out=gig[:], in0=mk1[:], in1=w12[:, 0, :].unsqueeze(2).to_broadcast(B3), op=ALU.mult), reads=[mk1, w12], writes=[gig])
                        op("dve", lambda e: e.tensor_tensor(out=mk2[:], in0=mk2[:], in1=w12[:, 1, :].unsqueeze(2).to_broadcast(B3), op=ALU.mult), reads=[mk2, w12], writes=[mk2])
                        op("dve", lambda e: e.tensor_tensor(out=gig[:], in0=gig[:], in1=mk2[:], op=ALU.add), reads=[gig, mk2], writes=[gig])
                        op("dve", lambda e: e.tensor_tensor(out=gate[:], in0=g1h[:].unsqueeze(3).to_broadcast([128, T_, 4, 4]),
                                                            in1=gig[:].unsqueeze(2).to_broadcast([128, T_, 4, 4]), op=ALU.mult), reads=[g1h, gig], writes=[gate])
                        for t4 in range(T_ // 4):
                            bk = 6 + t4 % 2
                            for j in range(4):
                                t = t4 * 4 + j
                                op("pe", lambda e: e.transpose(out=PS[bk][0:16, j * 128:(j + 1) * 128], in_=gate[:, t, :, :].rearrange("p g e -> p (g e)"), identity=idf[:]),
                                   reads=[gate, idf], writes=[PS[bk]])
                            op("act", lambda e: e.copy(out=gateT[:, t4 * 512:(t4 + 1) * 512], in_=PS[bk][0:16, :]), reads=[PS[bk]], writes=[gateT])
                    ckpt("D2r")
                    if "gateT" in dbg:
                        fw.dma(dbg_t("gateT", [16, S_OWN], BF16), gateT[:], reads=[gateT], is_output=True)
                    with fw.scope() as esE:
                        NW = 3
                        w13 = [fw.sb([128, 8, 512], BF16, f"w13_{i}", esE) for i in range(NW)]
                        w2e = [fw.sb([128, 2, D], BF16, f"w2e_{i}", esE) for i in range(NW)]
                        sgE = [fw.sb([128, 512], F32, f"sgE{i}", esE) for i in range(2)]
                        tE = [fw.sb([128, 512], F32, f"tE{i}", esE) for i in range(2)]
                        actT = [[fw.sb([128, 512], BF16, f"actT{i}{fc}", esE) for fc in range(2)] for i in range(2)]
                        x1M = [Buf(x1.t, f"x1m_{t}") for t in range(NT_OWN)]
                        for b_ in x1M:
                            b_.lw = x1.lw
                            b_.rd = dict(x1.rd)
                        ybank = [4, 5, 7]
                        yi = [0]

                        def load_w(ex):
                            wb = ex % NW
                            fw.dma(w13[wb][:], w_e13_d[ex].rearrange("(k p) c -> p k c", p=128), writes=[w13[wb]], q="pool")
                            fw.dma(w2e[wb][:], w_e2_d[ex].rearrange("(k p) c -> p k c", p=128), writes=[w2e[wb]], q="pool")

                        def e_front_pe(it):
                            ex, tb = it // 4, it % 4
                            wb = ex % NW
                            ts_ = slice(tb * 512, (tb + 1) * 512)
                            mm(PS[6], PS[6][:, :], E16[:, ex, :], gateT[:, ts_], True, True, [E16, gateT])
                            for fc in range(2):
                                for k in range(8):
                                    mm(PS[fc], PS[fc][:, :], w13[wb][:, k, fc * 128:(fc + 1) * 128], YT[:, k, ts_], k == 0, k == 7, [w13[wb], YT])
                                for k in range(8):
                                    mm(PS[2 + fc], PS[2 + fc][:, :], w13[wb][:, k, 256 + fc * 128:256 + (fc + 1) * 128], YT[:, k, ts_], k == 0, k == 7, [w13[wb], YT])

                        def e_front_post(it):
                            r = it % 2
                            for fc in range(2):
                                op("act", lambda e: e.activation(out=sgE[fc][:], in_=PS[fc][:, :], func=AF.Silu), reads=[PS[fc]], writes=[sgE[fc]])
                                op("dve", lambda e: e.tensor_tensor(out=tE[fc][:], in0=PS[2 + fc][:, :], in1=sgE[fc][:], op=ALU.mult), reads=[PS[2 + fc], sgE[fc]], writes=[tE[fc]])
                                op("dve", lambda e: e.tensor_tensor(out=actT[r][fc][:], in0=PS[6][:, :], in1=tE[fc][:], op=ALU.mult), reads=[PS[6], tE[fc]], writes=[actT[r][fc]])

                        def e_back(it):
                            ex, tb = it // 4, it % 4
                            wb = ex % NW
                            r = it % 2
                            for tt in range(4):
                                t = tb * 4 + tt
                                for half in range(2):
                                    b = ybank[yi[0] % 3]
                                    yi[0] += 1
                                    for fc in range(2):
                                        mm(PS[b], PS[b][:, :], actT[r][fc][:, tt * 128:(tt + 1) * 128], w2e[wb][:, fc, half * 512:(half + 1) * 512], fc == 0, fc == 1, [actT[r][fc], w2e[wb]])
                                    op("dve", lambda e: e.tensor_tensor(out=x1[:, t, half * 512:(half + 1) * 512], in0=PS[b][:, :], in1=x1[:, t, half * 512:(half + 1) * 512], op=ALU.add),
                                       reads=[PS[b], x1M[t]], writes=[x1M[t]])

                        load_w(0)
                        load_w(1)
                        NIT = 64
                        for it in range(NIT + 1):
                            if it < NIT:
                                e_front_pe(it)
                                e_front_post(it)
                            if it >= 1:
                                e_back(it - 1)
                            if it < NIT and it % 4 == 0 and it // 4 + 2 < 16:
                                load_w(it // 4 + 2)
                        for b_ in x1M:
                            if b_.lw is not None and (x1.lw is None or True):
                                pass
                        x1.lw = None
                        x1.rd = {}
                        fw.barrier()
                ckpt("D2")
                if "x2" in dbg:
                    fw.dma(dbg_t("x2", [128, NT_OWN, D]), x1[:], reads=[x1], is_output=True)
                with fw.scope() as esP:
                    load_gain(2)
                    gB2 = fw.sb([128, D], F32, "gB2", esP)
                    fw.dma(gB2[:], gvec_d[3:4, :].to_broadcast([128, D]), writes=[gB2])
                    w_pg = fw.sb([128, 8, D], BF16, "w_pg", esP)
                    fw.dma(w_pg[:], w_pg_d.rearrange("(k p) c -> p k c", p=128), writes=[w_pg], q="pool")
                    w_pp = fw.sb([128, 2, D], BF16, "w_pp", esP)
                    fw.dma(w_pp[:], w_pp_d.rearrange("(k p) c -> p k c", p=128), writes=[w_pp], q="pool")
                    hpb = [fw.sb([128, D], BF16, f"hpb{i}", esP) for i in range(3)]
                    hpT = [fw.sb([128, 8, 128], BF16, f"hpT{i}", esP) for i in range(3)]
                    plb = [fw.sb([128, 256], BF16, f"plb{i}", esP) for i in range(3)]
                    plT = [fw.sb([128, 2, 128], BF16, f"plT{i}", esP) for i in range(3)]
                    junkP2 = fw.sb([128, D], BF16, "junkP2", esP)
                    sgP = [fw.sb([128, 512], F32, f"sgP{i}", esP) for i in range(2)]
                    tP = [fw.sb([128, 512], F32, f"tP{i}", esP) for i in range(2)]
                    outt = [fw.sb([128, D], F32, f"outt{i}", esP) for i in range(2)]
                    junkP = fw.sb([128, D], BF16, "junkP", esP)
                    ssp = [fw.sb([128, 1], F32, f"ssp{i}", esP) for i in range(5)]
                    rrp = [fw.sb([128, 1], F32, f"rrp{i}", esP) for i in range(5)]
                    x1T = [Buf(x1.t, f"x1_{t}") for t in range(NT_OWN)]
                    for b_ in x1T:
                        b_.lw = x1.lw
                        b_.rd = dict(x1.rd)

                    def p_s1(t):
                        r = t % 3
                        fw.dma(plb[r][:], pl_d[t * 128:(t + 1) * 128, :], writes=[plb[r]], q="pool")
                        rs = {"ss": ssp[r], "r": rrp[r]}
                        rms_rstd({"ap": x1[:, t, :], "bufs": [x1T[t]]}, rs, D, {"ap": junkP[:], "buf": junkP})
                        op("dve", lambda e: e.scalar_tensor_tensor(out=hpb[r][:], in0=x1[:, t, :], scalar=rs["r"][:], in1=gB[:], op0=ALU.mult, op1=ALU.mult),
                           reads=[x1T[t], rs["r"], gB], writes=[hpb[r]])

                    def p_s2(t):
                        r = t % 3
                        b0 = 2 * (t % 2)
                        for k in range(8):
                            op("pe", lambda e: e.transpose(out=psbf(b0)[:, k * 128:(k + 1) * 128], in_=hpb[r][:, k * 128:(k + 1) * 128], identity=idb[:]), reads=[hpb[r], idb], writes=[PS[b0]])
                        op("act", lambda e: e.copy(out=hpT[r][:], in_=psbf(b0).rearrange("p (k t) -> p k t", k=8)), reads=[PS[b0]], writes=[hpT[r]])
                        for k in range(2):
                            op("pe", lambda e: e.transpose(out=psbf(b0 + 1)[:, k * 128:(k + 1) * 128], in_=plb[r][:, k * 128:(k + 1) * 128], identity=idb[:]), reads=[plb[r], idb], writes=[PS[b0 + 1]])
                        op("act", lambda e: e.copy(out=plT[r][:], in_=psbf(b0 + 1)[:, 0:256].rearrange("p (k t) -> p k t", k=2)), reads=[PS[b0 + 1]], writes=[plT[r]])

                    def p_s3(t):
                        r = t % 3
                        for half in range(2):
                            hs = slice(half * 512, (half + 1) * 512)
                            bG = 4 + half
                            bP = 6 + half
                            for k in range(8):
                                mm(PS[bG], PS[bG][:, :], hpT[r][:, k, :], w_pg[:, k, hs], k == 0, k == 7, [hpT[r], w_pg])
                            for k in range(2):
                                mm(PS[bP], PS[bP][:, :], plT[r][:, k, :], w_pp[:, k, hs], k == 0, k == 1, [plT[r], w_pp])
                            op("act", lambda e: e.activation(out=sgP[half][:], in_=PS[bG][:, :], func=AF.Sigmoid), reads=[PS[bG]], writes=[sgP[half]])
                            op("dve", lambda e: e.tensor_tensor(out=tP[half][:], in0=PS[bP][:, :], in1=sgP[half][:], op=ALU.mult), reads=[PS[bP], sgP[half]], writes=[tP[half]])
                            op("dve", lambda e: e.tensor_tensor(out=x1[:, t, hs], in0=x1[:, t, hs], in1=tP[half][:], op=ALU.add), reads=[x1T[t], tP[half]], writes=[x1T[t]])
                        rs2 = {"ss": ssp[3 + t % 2], "r": rrp[3 + t % 2]}
                        rms_rstd({"ap": x1[:, t, :], "bufs": [x1T[t]]}, rs2, D, {"ap": junkP2[:], "buf": junkP2})
                        o_ = outt[t % 2]
                        op("dve", lambda e: e.scalar_tensor_tensor(out=o_[:], in0=x1[:, t, :], scalar=rs2["r"][:], in1=gB2[:], op0=ALU.mult, op1=ALU.mult),
                           reads=[x1T[t], rs2["r"], gB2], writes=[o_])
                        fw.dma(out_d[t * 128:(t + 1) * 128, :], o_[:], reads=[o_], is_output=True)

                    for i in range(NT_OWN + 2):
                        if i < NT_OWN:
                            p_s1(i)
                        if 1 <= i <= NT_OWN:
                            p_s2(i - 1)
                        if i >= 2:
                            p_s3(i - 2)

            if "yaT" in dbg:
                o = dbg_t("yaT", [128, 4, S_OWN], BF16)
                fw.dma(o[:, :, :], YT[:, 0:4, :], reads=[YT], is_output=True)

            if "hT" in dbg:
                o = dbg_t("hT", [128, 8, S_EXT], BF16)
                with fw.scope() as esd:
                    tmp = fw.sb([128, 8, 512], BF16, "dbg_hT", esd)
                    for i in range(8):
                        fw.dma(tmp[:], hT_d[:, :, i * 512:(i + 1) * 512], reads=hT_tiles[4 * i:4 * i + 4], writes=[tmp])
                        fw.dma(o[:, :, i * 512:(i + 1) * 512], tmp[:], reads=[tmp], is_output=True)


        body()
        fw.stopped = False
        fw.finish()
    return nc, dbg_out


_INV = (500000.0 ** (-np.arange(0, 16, 2, dtype=np.float32) / 16.0)).astype(np.float32)


def make_in_maps(inputs):
    f = lambda a: np.ascontiguousarray(np.asarray(a), dtype=np.float32)
    x = f(inputs["x"]); p = f(inputs["p"])
    positions = np.asarray(inputs["positions"]).astype(np.int32)
    w_in = f(inputs["w_in"])[0]
    offs = np.cumsum([0, 512, 128, 128, 128, 128, 128, 128, 24, 1024, 512, 512, 8, 2048])
    seg = {n: (offs[i], offs[i + 1]) for i, n in enumerate(["q", "kc", "vc", "ks", "vs", "kw", "vw", "gate", "qk", "v", "o", "if", "mg"])}
    col = lambda n: w_in[:, seg[n][0]:seg[n][1]]
    w_att = []
    for g in range(2):
        parts = [col("q")[:, g * 256:(g + 1) * 256]]
        for n in ["ks", "kw", "kc", "vc", "vs", "vw"]:
            parts.append(col(n)[:, g * 64:(g + 1) * 64])
        parts.append(col("gate")[:, g * 12:(g + 1) * 12])
        w_att.append(np.concatenate(parts, axis=1))
    w_att = np.ascontiguousarray(np.stack(w_att))
    shared = {
        "invf": np.ascontiguousarray(np.broadcast_to(_INV[None, :], (128, 8))),
        "gvec": np.ascontiguousarray(np.stack([f(inputs["g_mix"])[0], f(inputs["g_ffn"])[0], f(inputs["g_ple"])[0], f(inputs["g_final"])])),
        "w_att": w_att,
        "w_qk": np.ascontiguousarray(col("qk")),
        "w_vo": np.ascontiguousarray(np.concatenate([col("v"), col("o")], axis=1)),
        "w_if": np.ascontiguousarray(col("if")),
        "w_mg": np.ascontiguousarray(col("mg")),
        "b_if": f(inputs["b_if"]).reshape(1, 8),
        "w_c1": np.ascontiguousarray(np.stack([f(inputs["w_ck1"])[0], f(inputs["w_cv1"])[0]])),
        "w_c2": np.ascontiguousarray(np.stack([f(inputs["w_ck2"])[0], f(inputs["w_cv2"])[0]])),
        "pe_c": np.ascontiguousarray(np.stack([f(inputs["pe_ck"])[0], f(inputs["pe_cv"])[0]])),
        "wc": np.ascontiguousarray(f(inputs["w_conv"])[0].reshape(4, 8, 128).transpose(2, 1, 0)),
        "bc": np.ascontiguousarray(f(inputs["b_conv"])[0].reshape(8, 128).T),
        "g_hn": f(inputs["g_hn"]).reshape(1, 512),
        "w_pa": f(inputs["w_pa"])[0], "w_pb": f(inputs["w_pb"])[0], "w_out": f(inputs["w_out"])[0],
        "w_r": np.ascontiguousarray(np.concatenate([f(inputs["w_rg"])[0], f(inputs["w_re"])[0]], axis=1)),
        "b_r": np.ascontiguousarray(np.concatenate([f(inputs["b_rg"])[0], f(inputs["b_re"])[0]])[None, :]),
        "w_e13": f(inputs["w_e13"])[0], "w_e2": f(inputs["w_e2"])[0],
        "w_pg": f(inputs["w_pg"])[0], "w_pp": f(inputs["w_pp"])[0],
    }
    in_maps = []
    for core in range(8):
        b, half = core // 2, core % 2
        if half == 1:
            xe_ = x[b]
            pos_ = positions[b]
        else:
            xe_ = np.concatenate([np.zeros((S_OWN, D), np.float32), x[b, :S_OWN]], axis=0)
            pos_ = np.concatenate([np.zeros(S_OWN, np.int32), positions[b, :S_OWN]])
        m = dict(shared)
        m["xe"] = np.ascontiguousarray(xe_)
        m["pos"] = np.ascontiguousarray(pos_.reshape(NT_EXT, 128).T)
        m["pl"] = np.ascontiguousarray(p[0, b, half * S_OWN:(half + 1) * S_OWN])
        m["hv"] = np.full((128, 1), float(half), np.float32)
        in_maps.append(m)
    return in_maps


def kernel(**inputs):
    nc, _ = build_program()
    in_maps = make_in_maps(inputs)
    res = run_bass_kernel_spmd(nc, in_maps, core_ids=list(range(8)))
    out = np.zeros((4, S_EXT, D), np.float32)
    for core in range(8):
        b, half = core // 2, core % 2
        out[b, half * S_OWN:(half + 1) * S_OWN] = res.results[core]["out"]
    return out
```

```python
import numpy as np
import concourse.bass as bass
import concourse.mybir as mybir
from concourse.bass_utils import run_bass_kernel_spmd
from contextlib import ExitStack

F32 = mybir.dt.float32
BF16 = mybir.dt.bfloat16
I32 = mybir.dt.int32
AF = mybir.ActivationFunctionType
ALU = mybir.AluOpType
AX = mybir.AxisListType

D = 1024
S_OWN = 2048
S_EXT = 4096
NT_OWN = 16
NT_EXT = 32
EPS = 1e-6
NEGB = -30000.0
DBG = []


class Buf:
    __slots__ = ("t", "lw", "rd", "name", "excl")

    def __init__(self, t, name=""):
        self.t = t
        self.excl = False
        self.lw = None
        self.rd = {}
        self.name = name

    def __getitem__(self, k):
        return self.t[k]


class FW:
    NDMA = 24

    def __init__(self, nc, es):
        self.nc = nc
        self.es = es
        self.eng = {"pe": nc.tensor, "act": nc.scalar, "dve": nc.vector, "pool": nc.gpsimd, "sp": nc.sync}
        self.sem = {k: es.enter_context(nc.semaphore("s_" + k)) for k in self.eng}
        self.cnt = {k: 0 for k in self.eng}
        self.known = {k: {} for k in self.eng}
        self.dsem = [es.enter_context(nc.semaphore(f"s_dma{i}")) for i in range(self.NDMA)]
        self.dval = [0] * self.NDMA
        self.dnext = 0
        self.nbuf = 0
        self.out_waits = []
        self.stopped = False

    def sb(self, shape, dt, name=None, es=None):
        self.nbuf += 1
        name = f"sb{self.nbuf}_" + (name or "t")
        return Buf((es or self.es).enter_context(self.nc.sbuf_tensor(name, list(shape), dt)), name)

    def ps(self, shape, dt, name=None):
        self.nbuf += 1
        name = name or f"ps{self.nbuf}"
        b = Buf(self.es.enter_context(self.nc.psum_tensor(name, list(shape), dt)), name)
        b.excl = True
        return b

    def _wait(self, e, src, idx):
        if self.stopped:
            return
        kn = self.known[e]
        if kn.get(src, 0) >= idx:
            return
        s = self.dsem[src[1]] if isinstance(src, tuple) else self.sem[src]
        self.eng[e].wait_ge(s, idx)
        kn[src] = idx

    def _deps(self, e, reads, writes):
        for b in reads:
            if b.lw is not None:
                self._wait(e, b.lw[0], b.lw[1])
            if b.excl:
                for src, idx in b.rd.items():
                    if src != e:
                        self._wait(e, src, idx)
        for b in writes:
            if b.lw is not None and b.lw[0] != e:
                self._wait(e, b.lw[0], b.lw[1])
            for src, idx in b.rd.items():
                if src != e:
                    self._wait(e, src, idx)

    def op(self, e, fn, reads=(), writes=()):
        if self.stopped:
            return None
        self._deps(e, reads, writes)
        inst = fn(self.eng[e])
        self.cnt[e] += 1
        c = self.cnt[e]
        inst.then_inc(self.sem[e], 1)
        for b in reads:
            if b.rd.get(e, 0) < c:
                b.rd[e] = c
        for b in writes:
            b.lw = (e, c)
            b.rd = {}
        return inst

    def dma(self, out, in_, reads=(), writes=(), q="sp", is_output=False):
        if self.stopped and not is_output:
            return None
        self._deps(q, reads, writes)
        slot = self.dnext
        self.dnext = (self.dnext + 1) % self.NDMA
        key = ("d", slot)
        if self.dval[slot] > 0:
            self._wait(q, key, self.dval[slot])
        inst = self.eng[q].dma_start(out=out, in_=in_)
        self.dval[slot] += 16
        inst.then_inc(self.dsem[slot], 16)
        v = self.dval[slot]
        for b in reads:
            if b.rd.get(key, 0) < v:
                b.rd[key] = v
        for b in writes:
            b.lw = (key, v)
            b.rd = {}
        if is_output:
            self.out_waits.append((key, v))
        return inst

    def barrier(self):
        for e in self.eng:
            for src in ("pe", "act", "dve", "pool"):
                if src != e and self.cnt[src] > 0:
                    self._wait(e, src, self.cnt[src])
            for slot in range(self.NDMA):
                if self.dval[slot] > 0:
                    self._wait(e, ("d", slot), self.dval[slot])

    def scope(self):
        fw = self

        class _Scope(ExitStack):
            def __exit__(self, *a):
                fw.barrier()
                return super().__exit__(*a)
        return _Scope()

    def finish(self):
        for key, v in self.out_waits:
            self._wait("sp", key, v)
        for k in ("pe", "act", "dve", "pool"):
            if self.cnt[k] > 0:
                self._wait("sp", k, self.cnt[k])


class _StopBuild(Exception):
    pass


def build_program(dbg=()):
    nc = bass.Bass("TRN2", target_bir_lowering=False)

    def din(name, shape, dt=F32):
        return nc.dram_tensor(name, list(shape), dt, kind="ExternalInput").ap()

    xe = din("xe", [S_EXT, D])
    pos_d = din("pos", [128, NT_EXT], I32)
    pl_d = din("pl", [S_OWN, 256])
    hv_d = din("hv", [128, 1])
    invf_d = din("invf", [128, 8])
    gvec_d = din("gvec", [4, D])
    w_att_d = din("w_att", [2, D, 652])
    w_qk_d = din("w_qk", [D, 1024])
    w_vo_d = din("w_vo", [D, 1024])
    w_if_d = din("w_if", [D, 8])
    w_mg_d = din("w_mg", [D, 2048])
    b_if_d = din("b_if", [1, 8])
    w_c1_d = din("w_c1", [2, 2048, 256])
    w_c2_d = din("w_c2", [2, 256, 64])
    pe_c_d = din("pe_c", [2, 32, 64])
    wc_d = din("wc", [128, 8, 4])
    bc_d = din("bc", [128, 8])
    g_hn_d = din("g_hn", [1, 512])
    w_pa_d = din("w_pa", [512, D])
    w_pb_d = din("w_pb", [512, D])
    w_out_d = din("w_out", [D, D])
    w_r_d = din("w_r", [D, 20])
    b_r_d = din("b_r", [1, 20])
    w_e13_d = din("w_e13", [16, D, 512])
    w_e2_d = din("w_e2", [16, 256, D])
    w_pg_d = din("w_pg", [D, D])
    w_pp_d = din("w_pp", [256, D])
    out_d = nc.dram_tensor("out", [S_OWN, D], F32, kind="ExternalOutput").ap()
    hT_d = nc.dram_tensor("hT_scr", [128, 8, S_EXT], BF16, kind="Internal").ap()
    dbg_out = {}

    def dbg_t(name, shape, dt=F32):
        dbg_out[name] = nc.dram_tensor("dbg_" + name, list(shape), dt, kind="ExternalOutput").ap()
        return dbg_out[name]

    with ExitStack() as es:
        fw = FW(nc, es)
        op = fw.op
        PS = [fw.ps([128, 512], F32, f"psb{i}") for i in range(8)]

        def psbf(i):
            return PS[i][:].bitcast(BF16)

        ones_f = fw.sb([128, 128], F32, "ones_f")
        op("pool", lambda e: e.memset(ones_f[:], 1.0), writes=[ones_f])
        idf = fw.sb([128, 128], F32, "idf")
        op("pool", lambda e: e.affine_select(out=idf[:], in_=ones_f[:], pattern=[[1, 128]], compare_op=ALU.is_equal,
                                             fill=0.0, base=0, channel_multiplier=-1), reads=[ones_f], writes=[idf])
        idb = fw.sb([128, 128], BF16, "idb")
        op("dve", lambda e: e.tensor_copy(out=idb[:], in_=idf[:]), reads=[idf], writes=[idb])
        U_f = fw.sb([128, 128], F32, "U_f")
        op("pool", lambda e: e.affine_select(out=U_f[:], in_=ones_f[:], pattern=[[1, 128]], compare_op=ALU.is_ge,
                                             fill=0.0, base=0, channel_multiplier=-1), reads=[ones_f], writes=[U_f])
        caus = fw.sb([128, 128], BF16, "caus")
        op("dve", lambda e: e.tensor_copy(out=caus[:], in_=U_f[:]), reads=[U_f], writes=[caus])
        wm0_f = fw.sb([128, 128], F32, "wm0_f")
        op("pool", lambda e: e.affine_select(out=wm0_f[:], in_=ones_f[:], pattern=[[-1, 128]], compare_op=ALU.is_ge,
                                             fill=0.0, base=-1, channel_multiplier=1), reads=[ones_f], writes=[wm0_f])
        wm0 = fw.sb([128, 128], BF16, "wm0")
        op("dve", lambda e: e.tensor_copy(out=wm0[:], in_=wm0_f[:]), reads=[wm0_f], writes=[wm0])
        c_eps = fw.sb([128, 1], F32, "c_eps")
        op("pool", lambda e: e.memset(c_eps[:], EPS), writes=[c_eps])
        c_one = fw.sb([128, 1], F32, "c_one")
        op("pool", lambda e: e.memset(c_one[:], 1.0), writes=[c_one])
        c_zero = fw.sb([128, 1], F32, "c_zero")
        op("pool", lambda e: e.memset(c_zero[:], 0.0), writes=[c_zero])
        acc_junk = fw.sb([128, 2], F32, "acc_junk")
        op("act", lambda e: e.activation(out=acc_junk[:, 0:1], in_=c_one[:], func=AF.Square, accum_out=acc_junk[:, 1:2]),
           reads=[c_one], writes=[acc_junk])
        hv = fw.sb([128, 1], F32, "hv")
        fw.dma(hv[:], hv_d[:, :], writes=[hv])
        hbias = fw.sb([128, 1], F32, "hbias")
        op("dve", lambda e: e.tensor_scalar(out=hbias[:], in0=hv[:], scalar1=-1.0, scalar2=-NEGB, op0=ALU.add, op1=ALU.mult),
           reads=[hv], writes=[hbias])
        gB = fw.sb([128, D], F32, "gB")

        def load_gain(i):
            fw.dma(gB[:], gvec_d[i:i + 1, :].to_broadcast([128, D]), writes=[gB])

        cs = fw.sb([128, NT_EXT, 8], F32, "cs")
        sn = fw.sb([128, NT_EXT, 8], F32, "sn")
        with fw.scope() as es1:
            posi = fw.sb([128, NT_EXT], I32, "posi", es1)
            posf = fw.sb([128, NT_EXT], F32, "posf", es1)
            invf = fw.sb([128, 8], F32, "invf", es1)
            ang = fw.sb([128, NT_EXT, 8], F32, "ang", es1)
            kf = fw.sb([128, NT_EXT, 8], F32, "kf", es1)
            ki = fw.sb([128, NT_EXT, 8], I32, "ki", es1)
            r1 = fw.sb([128, NT_EXT, 8], F32, "r1", es1)
            r2 = fw.sb([128, NT_EXT, 8], F32, "r2", es1)
            fw.dma(posi[:], pos_d[:, :], writes=[posi])
            fw.dma(invf[:], invf_d[:, :], writes=[invf])
            op("dve", lambda e: e.tensor_copy(out=posf[:], in_=posi[:]), reads=[posi], writes=[posf])
            op("dve", lambda e: e.tensor_tensor(out=ang[:], in0=posf[:].unsqueeze(2).to_broadcast([128, NT_EXT, 8]),
                                                in1=invf[:].unsqueeze(1).to_broadcast([128, NT_EXT, 8]), op=ALU.mult),
               reads=[posf, invf], writes=[ang])
            TWO_PI = 6.283185307179586
            C1 = 6.28125
            C2 = TWO_PI - C1
            PI_LO = 3.1415925
            op("dve", lambda e: e.tensor_scalar(out=kf[:], in0=ang[:], scalar1=1.0 / TWO_PI, scalar2=None, op0=ALU.mult),
               reads=[ang], writes=[kf])
            op("dve", lambda e: e.tensor_copy(out=ki[:], in_=kf[:]), reads=[kf], writes=[ki])
            op("dve", lambda e: e.tensor_copy(out=kf[:], in_=ki[:]), reads=[ki], writes=[kf])
            op("dve", lambda e: e.scalar_tensor_tensor(out=r1[:], in0=kf[:], scalar=-C1, in1=ang[:], op0=ALU.mult, op1=ALU.add),
               reads=[kf, ang], writes=[r1])
            op("dve", lambda e: e.scalar_tensor_tensor(out=r1[:], in0=kf[:], scalar=-C2, in1=r1[:], op0=ALU.mult, op1=ALU.add),
               reads=[kf, r1], writes=[r1])
            op("dve", lambda e: e.tensor_scalar(out=r1[:], in0=r1[:], scalar1=PI_LO, scalar2=-PI_LO, op0=ALU.min, op1=ALU.max),
               reads=[r1], writes=[r1])
            op("act", lambda e: e.activation(out=sn[:], in_=r1[:], func=AF.Sin), reads=[r1], writes=[sn])
            op("dve", lambda e: e.tensor_scalar(out=r2[:], in0=r1[:], scalar1=PI_LO / 2 + 0.0, scalar2=None, op0=ALU.add),
               reads=[r1], writes=[r2])
            op("dve", lambda e: e.tensor_scalar(out=kf[:], in0=r2[:], scalar1=PI_LO, scalar2=-TWO_PI, op0=ALU.is_gt, op1=ALU.mult),
               reads=[r2], writes=[kf])
            op("dve", lambda e: e.tensor_tensor(out=r2[:], in0=r2[:], in1=kf[:], op=ALU.add), reads=[r2, kf], writes=[r2])
            op("dve", lambda e: e.tensor_scalar(out=r2[:], in0=r2[:], scalar1=PI_LO, scalar2=-PI_LO, op0=ALU.min, op1=ALU.max),
               reads=[r2], writes=[r2])
            op("act", lambda e: e.activation(out=cs[:], in_=r2[:], func=AF.Sin), reads=[r2], writes=[cs])

        def rms_rstd(src, rstd, n, junk):
            ss = rstd["ss"]
            op("act", lambda e: e.activation(out=junk["ap"], in_=src["ap"], func=AF.Square, accum_out=ss[:]),
               reads=src["bufs"], writes=[junk["buf"], ss])
            op("act", lambda e: e.activation(out=ss[:], in_=ss[:], func=AF.Sqrt, bias=c_eps[:], scale=1.0 / n),
               reads=[ss, c_eps], writes=[ss])
            op("dve", lambda e: e.reciprocal(out=rstd["r"][:], in_=ss[:]), reads=[ss], writes=[rstd["r"]])

        load_gain(0)
        hT_tiles = [Buf(None, f"hT_tile{t}") for t in range(NT_EXT)]
        with fw.scope() as esA:
            xt = [fw.sb([128, D], F32, f"xtA{i}", esA) for i in range(6)]
            xn = [fw.sb([128, D], BF16, f"xnA{i}", esA) for i in range(3)]
            junk = fw.sb([128, D], BF16, "junkA", esA)
            hst = [fw.sb([128, 8, 128], BF16, f"hstA{i}", esA) for i in range(4)]
            ssA = [fw.sb([128, 1], F32, f"ssA{i}", esA) for i in range(3)]
            rrA = [fw.sb([128, 1], F32, f"rrA{i}", esA) for i in range(3)]
            def a_s1(t):
                x_ = xt[t % 6]
                if t == 0:
                    for tt in range(5):
                        fw.dma(xt[tt][:], xe[tt * 128:(tt + 1) * 128, :], writes=[xt[tt]])
                if t + 5 < NT_EXT:
                    fw.dma(xt[(t + 5) % 6][:], xe[(t + 5) * 128:(t + 6) * 128, :], writes=[xt[(t + 5) % 6]])
                rs = {"ss": ssA[t % 3], "r": rrA[t % 3]}
                rms_rstd({"ap": x_[:], "bufs": [x_]}, rs, D, {"ap": junk[:], "buf": junk})
                n_ = xn[t % 3]
                op("dve", lambda e: e.scalar_tensor_tensor(out=n_[:], in0=x_[:], scalar=rs["r"][:], in1=gB[:], op0=ALU.mult, op1=ALU.mult),
                   reads=[x_, rs["r"], gB], writes=[n_])

            def a_s2(t):
                n_ = xn[t % 3]
                pb = t % 2
                for k in range(8):
                    op("pe", lambda e: e.transpose(out=psbf(pb)[:, k * 128:(k + 1) * 128], in_=n_[:, k * 128:(k + 1) * 128], identity=idb[:]),
                       reads=[n_, idb], writes=[PS[pb]])
                h_ = hst[t % 4]
                op("act", lambda e: e.copy(out=h_[:], in_=psbf(pb).rearrange("p (k t) -> p k t", k=8)), reads=[PS[pb]], writes=[h_])
                fw.dma(hT_d[:, :, t * 128:(t + 1) * 128], h_[:], reads=[h_], writes=[hT_tiles[t]], q="pool")

            for t in range(NT_EXT + 1):
                if t < NT_EXT:
                    a_s1(t)
                if t >= 1:
                    a_s2(t - 1)

        if "cs" in dbg:
            o = dbg_t("cs", [128, NT_EXT, 8])
            fw.dma(o[:, :, :], cs[:], reads=[cs], is_output=True)
            o = dbg_t("sn", [128, NT_EXT, 8])
            fw.dma(o[:, :, :], sn[:], reads=[sn], is_output=True)

        def ckpt(name):
            if ("stop_" + name) in dbg:
                fw.stopped = True

        def body():
            def mm(bank, out_ap, lhsT, rhs, start, stop, reads):
                op("pe", lambda e: e.matmul(out_ap, lhsT, rhs, start=start, stop=stop), reads=reads, writes=[bank])

            YT = fw.sb([128, 8, S_OWN], BF16, "YT")
            esBc = fw.scope()
            esBc.__enter__()
            cmask = fw.sb([128, 2, S_OWN], BF16, "cmask", esBc)
            op("pool", lambda e: e.memset(cmask[:], 1.0), writes=[cmask])
            op("pool", lambda e: e.affine_select(out=cmask[:, 0, :], in_=cmask[:, 0, :], pattern=[[1, S_OWN]], compare_op=ALU.is_ge, fill=0.0,
                                                 base=2017, channel_multiplier=-16), reads=[cmask], writes=[cmask])
            op("pool", lambda e: e.affine_select(out=cmask[:, 1, :], in_=cmask[:, 1, :], pattern=[[1, S_OWN]], compare_op=ALU.is_ge, fill=0.0,
                                                 base=-31, channel_multiplier=-16), reads=[cmask], writes=[cmask])
            ovl = fw.sb([128, 2, 64], BF16, "ovl", esBc)
            op("pool", lambda e: e.memset(ovl[:], 1.0), writes=[ovl])
            for j in range(2):
                op("pool", lambda e: e.affine_select(out=ovl[:, j, :], in_=ovl[:, j, :], pattern=[[-4, 64]], compare_op=ALU.is_ge, fill=0.0,
                                                     base=128 * j + 1, channel_multiplier=1), reads=[ovl], writes=[ovl])
                op("pool", lambda e: e.affine_select(out=ovl[:, j, :], in_=ovl[:, j, :], pattern=[[4, 64]], compare_op=ALU.is_ge, fill=0.0,
                                                     base=3 - 128 * j, channel_multiplier=-1), reads=[ovl], writes=[ovl])
            maskadd = fw.sb([128, NT_OWN, 64], F32, "maskadd", esBc)
            Mb = fw.sb([128, 64], F32, "Mb", esBc)
            hm1 = fw.sb([128, 2], F32, "hm1", esBc)
            op("dve", lambda e: e.tensor_scalar(out=hm1[:, 0:1], in0=hv[:], scalar1=-1.0, scalar2=1e30, op0=ALU.add, op1=ALU.mult),
               reads=[hv], writes=[hm1])
            op("dve", lambda e: e.tensor_scalar(out=hm1[:, 1:2], in0=hv[:], scalar1=-1.0, scalar2=-1000.0, op0=ALU.add, op1=ALU.mult),
               reads=[hv, hm1], writes=[hm1])
            op("dve", lambda e: e.memset(Mb[:], 0.0), writes=[Mb])
            op("dve", lambda e: e.tensor_copy(out=Mb[:, 0:32], in_=hm1[:, 0:1].to_broadcast([128, 32])), reads=[hm1, Mb], writes=[Mb])
            op("dve", lambda e: e.scalar_tensor_tensor(out=Mb[:, 0:1], in0=hv[:], scalar=1000.0, in1=Mb[:, 0:1], op0=ALU.mult, op1=ALU.add),
               reads=[hv, Mb], writes=[Mb])
            op("dve", lambda e: e.tensor_copy(out=Mb[:, 32:33], in_=hm1[:, 1:2]), reads=[hm1, Mb], writes=[Mb])
            for c in range(NT_OWN):
                op("pool", lambda e: e.tensor_copy(out=maskadd[:, c, :], in_=Mb[:]), reads=[Mb, maskadd], writes=[maskadd])
                for hf in range(2):
                    lo = 32 + 2 * c + hf + 1
                    if lo < 64:
                        op("pool", lambda e: e.memset(maskadd[hf * 64:(hf + 1) * 64, c, lo:64], -1e30), reads=[maskadd], writes=[maskadd])
                    for col in (32 + 2 * c + hf, 32 + 2 * c + hf - 1):
                        op("pool", lambda e: e.tensor_scalar(out=maskadd[hf * 64:(hf + 1) * 64, c, col:col + 1],
                                                             in0=maskadd[hf * 64:(hf + 1) * 64, c, col:col + 1],
                                                             scalar1=1000.0, scalar2=None, op0=ALU.add), reads=[maskadd], writes=[maskadd])

            ckpt("consts")
            for g in range(2):
                with fw.scope() as esG:
                    qT = fw.sb([128, 4, S_OWN], BF16, f"qT{g}", esG)
                    kkT = fw.sb([128, 2, S_EXT], BF16, f"kkT{g}", esG)
                    op("pool", lambda e: e.memset(qT[64:128, :, :], 0.0), writes=[qT])
                    op("pool", lambda e: e.memset(kkT[64:128, 0, :], 1.0), writes=[kkT])
                    op("pool", lambda e: e.memset(kkT[64:128, 1, :], 0.0), writes=[kkT])
                    op("pool", lambda e: e.affine_select(out=kkT[64:128, 0, :], in_=kkT[64:128, 0, :], pattern=[[1, S_EXT]], compare_op=ALU.is_ge, fill=0.0,
                                                         base=0, channel_multiplier=-64), reads=[kkT], writes=[kkT])
                    op("pool", lambda e: e.affine_select(out=kkT[64:128, 0, :], in_=kkT[64:128, 0, :], pattern=[[-1, S_EXT]], compare_op=ALU.is_ge, fill=0.0,
                                                         base=63, channel_multiplier=64), reads=[kkT], writes=[kkT])
                    vv = fw.sb([128, NT_EXT, 2, 65], BF16, f"vv{g}", esG)
                    gsig = fw.sb([128, NT_OWN, 12], F32, f"gsig{g}", esG)
                    kcmpT = fw.sb([128, 256], BF16, f"kcmpT{g}", esG)
                    op("pool", lambda e: e.memset(kcmpT[64:128, :], 0.0), writes=[kcmpT])
                    vca = fw.sb([128, 2, 65], BF16, f"vca{g}", esG)
                    op("pool", lambda e: e.memset(vv[:, :, :, 64:65], 1.0), writes=[vv])
                    op("pool", lambda e: e.memset(vca[:, :, 64:65], 1.0), writes=[vca])
                    ckpt("B0a")
                    with fw.scope() as esC:
                        ccT = fw.sb([64, 2, S_EXT], BF16, f"ccT{g}", esC)
                        with fw.scope() as esB1:
                            w_att = fw.sb([128, 8, 652], BF16, f"w_att{g}", esB1)
                            fw.dma(w_att[:], w_att_d[g].rearrange("(k p) c -> p k c", p=128), writes=[w_att], q="pool")
                            ckpt("B0b")
                            hblk = [fw.sb([128, 8, 512], BF16, f"hblkB{g}{i}", esB1) for i in range(2)]
                            rp = [fw.sb([128, 8, 64], BF16, f"rp{g}{i}", esB1) for i in range(2)]
                            rpf = [fw.sb([128, 8, 64], F32, f"rpf{g}{i}", esB1) for i in range(2)]
                            ta = [fw.sb([128, 7, 8], F32, f"ropa{g}{i}", esB1) for i in range(2)]
                            tb_ = [fw.sb([128, 7, 8], F32, f"ropb{g}{i}", esB1) for i in range(2)]
                            tcx = [fw.sb([128, 7, 8], F32, f"ropc{g}{i}", esB1) for i in range(2)]
                            tdx = [fw.sb([128, 7, 8], F32, f"ropd{g}{i}", esB1) for i in range(2)]
                            def b1_front(t):
                                own = t >= NT_OWN
                                tq = t - NT_OWN
                                hb = hblk[(t // 4) % 2]
                                if t % 4 == 0:
                                    fw.dma(hb[:], hT_d[:, :, t * 128:(t + 4) * 128], reads=hT_tiles[t:t + 4], writes=[hb])
                                tl = t % 4
                                a0 = 0 if own else 256
                                nb = 140 if own else 128
                                bA = 2 + t % 2
                                bB = 4 + t % 2
                                for k in range(8):
                                    mm(PS[bA], PS[bA][:, a0:512], hb[:, k, tl * 128:(tl + 1) * 128], w_att[:, k, a0:512], k == 0, k == 7, [hb, w_att])
                                for k in range(8):
                                    mm(PS[bB], PS[bB][:, 0:nb], hb[:, k, tl * 128:(tl + 1) * 128], w_att[:, k, 512:512 + nb], k == 0, k == 7, [hb, w_att])
                                rp_ = rp[t % 2]
                                h0 = a0 // 64
                                nh = 7 - h0
                                rf = rpf[t % 2]
                                op("act", lambda e: e.copy(out=rf[:, h0:8, :], in_=PS[bA][:, a0:512].rearrange("p (h d) -> p h d", d=64)),
                                   reads=[PS[bA]], writes=[rf])
                                op("pool", lambda e: e.tensor_copy(out=rp_[:, h0:8, :], in_=rf[:, h0:8, :]), reads=[rf], writes=[rp_])
                                t1 = rf[:, h0:7, 0:8]
                                t2 = rf[:, h0:7, 8:16]
                                Cb = cs[:, t, :].unsqueeze(1).to_broadcast([128, nh, 8])
                                Sb_ = sn[:, t, :].unsqueeze(1).to_broadcast([128, nh, 8])
                                ta_, tb2 = ta[t % 2], tb_[t % 2]
                                tc_, td_ = tcx[t % 2], tdx[t % 2]
                                op("dve", lambda e: e.tensor_tensor(out=ta_[:, 0:nh, :], in0=t1, in1=Cb, op=ALU.mult), reads=[rf, cs], writes=[ta_])
                                op("dve", lambda e: e.tensor_tensor(out=tb2[:, 0:nh, :], in0=t2, in1=Sb_, op=ALU.mult), reads=[rf, sn], writes=[tb2])
                                op("dve", lambda e: e.tensor_tensor(out=tc_[:, 0:nh, :], in0=t2, in1=Cb, op=ALU.mult), reads=[rf, cs], writes=[tc_])
                                op("dve", lambda e: e.tensor_tensor(out=td_[:, 0:nh, :], in0=t1, in1=Sb_, op=ALU.mult), reads=[rf, sn], writes=[td_])
                                op("dve", lambda e: e.tensor_tensor(out=rp_[:, h0:7, 0:8], in0=ta_[:, 0:nh, :], in1=tb2[:, 0:nh, :], op=ALU.subtract),
                                   reads=[ta_, tb2, rp_], writes=[rp_])
                                op("dve", lambda e: e.tensor_tensor(out=rp_[:, h0:7, 8:16], in0=tc_[:, 0:nh, :], in1=td_[:, 0:nh, :], op=ALU.add),
                                   reads=[tc_, td_, rp_], writes=[rp_])
                                op("dve", lambda e: e.tensor_copy(out=vv[:, t, :, 0:64], in_=PS[bB][:, 0:128].rearrange("p (h d) -> p h d", d=64)),
                                   reads=[PS[bB]], writes=[vv])
                                if own:
                                    op("act", lambda e: e.activation(out=gsig[:, tq, :], in_=PS[bB][:, 128:140], func=AF.Sigmoid),
                                       reads=[PS[bB]], writes=[gsig])

                            def b1_back(t):
                                own = t >= NT_OWN
                                tq = t - NT_OWN
                                rp_ = rp[t % 2]
                                h0 = 0 if own else 4
                                bT = t % 2
                                psT = psbf(bT)
                                for j, hh in enumerate(range(h0, 8)):
                                    op("pe", lambda e: e.transpose(out=psT[0:64, j * 128:(j + 1) * 128], in_=rp_[:, hh, :], identity=idb[:]),
                                       reads=[rp_, idb], writes=[PS[bT]])
                                if own:
                                    op("act", lambda e: e.copy(out=qT[0:64, :, tq * 128:(tq + 1) * 128], in_=psT[0:64, 0:512].rearrange("p (h t) -> p h t", h=4)),
                                       reads=[PS[bT]], writes=[qT])
                                    o1 = 512
                                else:
                                    o1 = 0
                                op("act", lambda e: e.copy(out=kkT[0:64, :, t * 128:(t + 1) * 128], in_=psT[0:64, o1:o1 + 256].rearrange("p (h t) -> p h t", h=2)),
                                   reads=[PS[bT]], writes=[kkT])
                                op("act", lambda e: e.copy(out=ccT[:, :, t * 128:(t + 1) * 128], in_=psT[0:64, o1 + 256:o1 + 512].rearrange("p (h t) -> p h t", h=2)),
                                   reads=[PS[bT]], writes=[ccT])

                            for t in range(NT_EXT + 1):
                                if t < NT_EXT:
                                    b1_front(t)
                                if t >= 1:
                                    b1_back(t - 1)
                        ckpt("B1")
                        for i in range(2):
                            with fw.scope() as esB2:
                                w1 = fw.sb([64, 32, 256], BF16, f"w1_{g}{i}", esB2)
                                fw.dma(w1[:], w_c1_d[i].rearrange("(l d) h -> d l h", d=64), writes=[w1], q="pool")
                                w2 = fw.sb([128, 2, 64], BF16, f"w2_{g}{i}", esB2)
                                fw.dma(w2[:], w_c2_d[i].rearrange("(c p) d -> p c d", p=128), writes=[w2], q="pool")
                                pe_sb = fw.sb([32, 64], BF16, f"pe_{g}{i}", esB2)
                                fw.dma(pe_sb[:], pe_c_d[i], writes=[pe_sb], q="pool")
                                peT = fw.sb([64, 32], BF16, f"peT_{g}{i}", esB2)
                                op("pe", lambda e: e.transpose(out=psbf(6)[0:64, 0:32], in_=pe_sb[:, :], identity=idb[0:32, 0:32]),
                                   reads=[pe_sb, idb], writes=[PS[6]])
                                op("act", lambda e: e.copy(out=peT[:], in_=psbf(6)[0:64, 0:32]), reads=[PS[6]], writes=[peT])
                                for hc in range(2):
                                    for l in range(32):
                                        mm(PS[7], PS[7][:, hc:hc + 1], w1[:, l, hc * 128:(hc + 1) * 128], peT[:, l:l + 1], l == 0, l == 31, [w1, peT])
                                cbs = fw.sb([128, 2], F32, f"cbs_{g}{i}", esB2)
                                op("act", lambda e: e.copy(out=cbs[:], in_=PS[7][:, 0:2]), reads=[PS[7]], writes=[cbs])
                                G = fw.sb([128, 2, 256], BF16, f"G_{g}{i}", esB2)
                                op("pool", lambda e: e.memset(G[:, :, 255:256], 0.0), writes=[G])
                                u_ = fw.sb([128, 255], F32, f"u_{g}{i}", esB2)
                                u2 = fw.sb([128, 255], F32, f"u2_{g}{i}", esB2)
                                sg_ = fw.sb([128, 255], F32, f"sg_{g}{i}", esB2)
                                for hc in range(2):
                                    for l in range(32):
                                        mm(PS[hc], PS[hc][:, 0:255], w1[:, l, hc * 128:(hc + 1) * 128], ccT[:, i, l:l + 16 * 254 + 1:16], l == 0, l == 31, [w1, ccT])
                                    op("act", lambda e: e.activation(out=u_[:], in_=PS[hc][:, 0:255], func=AF.Identity, bias=cbs[:, hc:hc + 1]),
                                       reads=[PS[hc], cbs], writes=[u_])
                                    op("dve", lambda e: e.tensor_tensor(out=u2[:], in0=u_[:], in1=u_[:], op=ALU.mult), reads=[u_], writes=[u2])
                                    op("dve", lambda e: e.tensor_scalar(out=u2[:], in0=u2[:], scalar1=0.044715, scalar2=1.0, op0=ALU.mult, op1=ALU.add),
                                       reads=[u2], writes=[u2])
                                    op("dve", lambda e: e.tensor_tensor(out=u2[:], in0=u2[:], in1=u_[:], op=ALU.mult), reads=[u2, u_], writes=[u2])
                                    op("act", lambda e: e.activation(out=sg_[:], in_=u2[:], func=AF.Sigmoid, scale=1.5957691216057308),
                                       reads=[u2], writes=[sg_])
                                    op("dve", lambda e: e.tensor_tensor(out=G[:, hc, 0:255], in0=u_[:], in1=sg_[:], op=ALU.mult), reads=[u_, sg_], writes=[G])
                                if i == 0:
                                    for hc in range(2):
                                        mm(PS[6], PS[6][0:64, 0:256], w2[:, hc, :], G[:, hc, :], hc == 0, hc == 1, [w2, G])
                                    op("act", lambda e: e.copy(out=kcmpT[0:64, :], in_=PS[6][0:64, 0:256]), reads=[PS[6]], writes=[kcmpT])
                                else:
                                    for nch in range(2):
                                        for hc in range(2):
                                            mm(PS[6], PS[6][:, nch * 64:(nch + 1) * 64], G[:, hc, nch * 128:(nch + 1) * 128], w2[:, hc, :], hc == 0, hc == 1, [w2, G])
                                    op("act", lambda e: e.copy(out=vca[:, :, 0:64], in_=PS[6][:, 0:128].rearrange("p (n d) -> p n d", d=64)),
                                       reads=[PS[6]], writes=[vca])
                    if g == 0 and "B2dump" in dbg:
                        for nm, bf, shp in (("kkT", kkT, [64, 2, S_EXT]), ("qT", qT, [64, 4, S_OWN]), ("vv", vv, [128, NT_EXT, 2, 65]),
                                            ("kcmpT", kcmpT, [64, 256]), ("vca", vca, [128, 2, 65])):
                            o = dbg_t(nm, shp, BF16)
                            fw.dma(o, bf[0:shp[0]], reads=[bf], is_output=True)
                        o = dbg_t("gsig", [128, NT_OWN, 12])
                        fw.dma(o, gsig[:], reads=[gsig], is_output=True)
                    ckpt("B2")
                    with fw.scope() as esB3:
                        NP = 4
                        LA = 2
                        Pb = [fw.sb([128, 512], BF16, f"Pb{g}{i}", esB3) for i in range(NP)]
                        Sbank = [0, 1, 6, 7]
                        hbS = [fw.sb([128, 4, 132], F32, f"hbS{g}{r}", esB3) for r in range(3)]
                        ya = [fw.sb([128, 4, 64], F32, f"ya{g}{i}", esB3) for i in range(2)]
                        yat = [fw.sb([128, 4, 64], BF16, f"yat{g}{i}", esB3) for i in range(2)]
                        sms = [fw.sb([128, 16], F32, f"sm{g}{i}", esB3) for i in range(3)]
                        rdc = fw.sb([128, 4], F32, f"rdc{g}", esB3)
                        impv = fw.sb([128, 64], F32, f"impv{g}", esB3)
                        wk = fw.sb([128, 64], F32, f"wk{g}", esB3)
                        m8a = fw.sb([128, 8], F32, f"m8a{g}", esB3)
                        m8b = fw.sb([128, 8], F32, f"m8b{g}", esB3)
                        negm2 = fw.sb([128, 128], BF16, f"negm{g}", esB3)
                        op("pool", lambda e: e.memset(negm2[:, 0:64], 0.0), writes=[negm2])
                        rot = [0]
                        REG = {0: (0, 129), 1: (129, 65), 2: (194, 65)}

                        def score(c, lhsT, lreads, extra, bias, mask):
                            r = rot[0] % NP
                            rot[0] += 1
                            sb_i = Sbank[r]
                            P = Pb[r]
                            qrhs = qT[:, :, c * 128:(c + 1) * 128]
                            S3 = PS[sb_i][:, :].rearrange("p (h q) -> p h q", h=4)
                            mm(PS[sb_i], S3, lhsT, qrhs, True, True, lreads + [qT])
                            op("act", lambda e: e.activation(out=P[:], in_=PS[sb_i][:, :], func=AF.Exp, bias=bias[:], scale=0.125),
                               reads=[PS[sb_i], bias], writes=[P])
                            if mask is not None:
                                op("dve", lambda e: e.tensor_tensor(out=P[:].rearrange("p (h q) -> p h q", h=4), in0=P[:].rearrange("p (h q) -> p h q", h=4),
                                                                    in1=mask[0], op=ALU.mult), reads=[P, mask[1]], writes=[P])
                            return P

                        def pv(P, h, reg, vr, vreads, cc, n, first, last):
                            op("pe", lambda e: e.matmul(PS[2 + h][:, cc:cc + n], P[:, h * 128:(h + 1) * 128], vr, start=first, stop=last),
                               reads=[P] + vreads, writes=[PS[2 + h]])

                        def evac_all(c, reg, br, first, final, mid=None):
                            col0, n = REG[reg]
                            hs = hbS[reg]
                            for h in range(4):
                                op("dve", lambda e: e.tensor_copy(out=hs[:, h, 0:n], in_=PS[2 + h][:, col0:col0 + n]), reads=[PS[2 + h]], writes=[hs])
                            sm = sms[reg]
                            yac = ya[c % 2]
                            dn = sm[:, 0:4]
                            rd = sm[:, 4:8] if br != 0 else rdc[:, 0:4]
                            rdb = sm if br != 0 else rdc
                            cf = sm[:, 8:12]
                            op("dve", lambda e: e.tensor_scalar(out=dn.unsqueeze(2), in0=hs[:, :, 64:65], scalar1=1e-30, scalar2=None, op0=ALU.max),
                               reads=[hs], writes=[sm])
                            op("dve", lambda e: e.reciprocal(out=rd, in_=dn), reads=[sm], writes=[rdb])
                            if mid is not None:
                                mid()
                            op("dve", lambda e: e.tensor_tensor(out=cf.unsqueeze(2), in0=rd.unsqueeze(2),
                                                                in1=gsig[:, c, :].rearrange("p (h b) -> p h b", b=3)[:, :, br:br + 1], op=ALU.mult),
                               reads=[sm, rdb, gsig], writes=[sm])
                            cfb = cf.unsqueeze(2).to_broadcast([128, 4, 64])
                            if first:
                                op("dve", lambda e: e.tensor_tensor(out=yac[:], in0=hs[:, :, 0:64], in1=cfb, op=ALU.mult), reads=[hs, sm], writes=[yac])
                            else:
                                op("dve", lambda e: e.tensor_tensor(out=hs[:, :, 0:64], in0=hs[:, :, 0:64], in1=cfb, op=ALU.mult), reads=[hs, sm], writes=[hs])
                                dst = yat[c % 2] if final else yac
                                op("dve", lambda e: e.tensor_tensor(out=dst[:], in0=hs[:, :, 0:64], in1=yac[:], op=ALU.add), reads=[hs, yac], writes=[dst])

                        pend = []

                        def flush():
                            while pend:
                                pend.pop(0)()

                        def pipe(score_fn, pv_fn):
                            P = score_fn()
                            while len(pend) >= LA:
                                pend.pop(0)()
                            pend.append(lambda: pv_fn(P))

                        def tr_slot():
                            r = rot[0] % NP
                            rot[0] += 1
                            return Sbank[r]

                        def cmp_scores_pv(c):
                            Pc = []
                            for nch in range(2):
                                mk = cmask[:, nch, c * 128:(c + 1) * 128].unsqueeze(1).to_broadcast([128, 4, 128])
                                Pc.append(score(c, kcmpT[:, nch * 128:(nch + 1) * 128], [kcmpT], None, hbias if nch == 0 else c_zero, (mk, cmask)))
                            flush()
                            for h in range(4):
                                for nch in range(2):
                                    pv(Pc[nch], h, 0, vca[:, nch, :], [vca], 0, 65, nch == 0, nch == 1)
                                for nch in range(2):
                                    pv(Pc[nch], h, 0, ovl[:, nch, :], [ovl], 65, 64, nch == 0, nch == 1)

                        def cmp_evac_topk(c):
                            def topk_chain():
                                op("dve", lambda e: e.tensor_tensor(out=hbS[0][:, :, 65:129], in0=hbS[0][:, :, 65:129], in1=rdc[:, 0:4].unsqueeze(2).to_broadcast([128, 4, 64]), op=ALU.mult),
                                   reads=[hbS[0], rdc], writes=[hbS[0]])
                                op("dve", lambda e: e.tensor_reduce(out=impv[:], in_=hbS[0][:, :, 65:129].rearrange("p h s -> p s h"), axis=AX.X, op=ALU.add),
                                   reads=[hbS[0]], writes=[impv])
                                op("dve", lambda e: e.tensor_tensor(out=impv[:], in0=impv[:], in1=maskadd[:, c, :], op=ALU.add), reads=[impv, maskadd], writes=[impv])
                                op("dve", lambda e: e.max(out=m8a[:], in_=impv[:]), reads=[impv], writes=[m8a])
                                op("dve", lambda e: e.match_replace(out=wk[:], in_to_replace=m8a[:], in_values=impv[:], imm_value=-3.0e38),
                                   reads=[impv, m8a], writes=[wk])
                                op("dve", lambda e: e.max(out=m8b[:], in_=wk[:]), reads=[wk], writes=[m8b])
                                op("dve", lambda e: e.tensor_scalar(out=negm2[:, 64:128], in0=impv[:], scalar1=m8b[:, 7:8], scalar2=NEGB, op0=ALU.is_lt, op1=ALU.mult),
                                   reads=[impv, m8b, negm2], writes=[negm2])
                            evac_all(c, 0, 0, True, False, mid=topk_chain)

                        def negm_to_q(c):
                            bk = tr_slot()
                            op("pe", lambda e: e.transpose(out=psbf(bk)[:, 0:128], in_=negm2[:, :], identity=idb[:]), reads=[negm2, idb], writes=[PS[bk]])
                            op("act", lambda e: e.copy(out=qT[64:128, :, c * 128:(c + 1) * 128], in_=psbf(bk)[64:128, 0:128].unsqueeze(1).to_broadcast([64, 4, 128])),
                               reads=[PS[bk]], writes=[qT])

                        def finish_tile(cp):
                            evac_all(cp, 1, 1, False, True)
                            bk = tr_slot()
                            for j in range(2):
                                op("pe", lambda e: e.transpose(out=psbf(bk)[:, j * 128:(j + 1) * 128],
                                                               in_=yat[cp % 2][:, 2 * j:2 * j + 2, :].rearrange("p h d -> p (h d)"), identity=idb[:]),
                                   reads=[yat[cp % 2], idb], writes=[PS[bk]])
                            op("act", lambda e: e.copy(out=YT[:, 2 * g:2 * g + 2, cp * 128:(cp + 1) * 128],
                                                       in_=psbf(bk)[:, 0:256].rearrange("p (j t) -> p j t", j=2)), reads=[PS[bk]], writes=[YT])

                        cmp_scores_pv(0)
                        cmp_evac_topk(0)
                        negm_to_q(0)
                        for c in range(NT_OWN):
                            for j in range(5):
                                ch = NT_OWN + c - 4 + j
                                mk = None
                                if j == 0:
                                    mk = (wm0[:].unsqueeze(1).to_broadcast([128, 4, 128]), wm0)
                                elif j == 4:
                                    mk = (caus[:].unsqueeze(1).to_broadcast([128, 4, 128]), caus)

                                def sfn(ch=ch, mk=mk):
                                    return score(c, kkT[:, 1, ch * 128:(ch + 1) * 128], [kkT], None, hbias if ch < NT_OWN else c_zero, mk)

                                def pfn(P, ch=ch, j=j):
                                    for h in range(4):
                                        pv(P, h, 2, vv[:, ch, 1, :], [vv], 194, 65, j == 0, j == 4)
                                pipe(sfn, pfn)
                                if j == 1 and c > 0:
                                    finish_tile(c - 1)
                            flush()
                            evac_all(c, 2, 2, False, False)
                            if c + 1 < NT_OWN:
                                cmp_scores_pv(c + 1)
                            chs = list(range(NT_OWN)) + [NT_OWN + j for j in range(c + 1)]
                            for i, ch in enumerate(chs):
                                mk = None
                                if ch == NT_OWN + c:
                                    mk = (caus[:].unsqueeze(1).to_broadcast([128, 4, 128]), caus)

                                def sfn(ch=ch, mk=mk):
                                    return score(c, kkT[:, 0, ch * 128:(ch + 1) * 128], [kkT], None, hbias if ch < NT_OWN else c_zero, mk)

                                def pfn(P, ch=ch, i=i, n=len(chs)):
                                    for h in range(4):
                                        pv(P, h, 1, vv[:, ch, 0, :], [vv], 129, 65, i == 0, i == n - 1)
                                pipe(sfn, pfn)
                                if c + 1 < NT_OWN:
                                    if i == 2:
                                        cmp_evac_topk(c + 1)
                                    elif i == 10:
                                        negm_to_q(c + 1)
                        flush()
                        finish_tile(NT_OWN - 1)
            esBc.__exit__(None, None, None)
            ckpt("B")
            with fw.scope() as esCg:
                ee = fw.sb([128, NT_EXT, 4], F32, "ee", esCg)
                ff = fw.sb([128, NT_EXT, 4], F32, "ff", esCg)
                fl = fw.sb([128, NT_EXT, 4], F32, "fl", esCg)
                ghn = fw.sb([128, 512], F32, "ghn", esCg)
                fw.dma(ghn[:], g_hn_d[0:1, :].to_broadcast([128, 512]), writes=[ghn])
                wcs = fw.sb([128, 8, 4], F32, "wcs", esCg)
                fw.dma(wcs[:], wc_d[:, :, :], writes=[wcs])
                bcs = fw.sb([128, 8], F32, "bcs", esCg)
                fw.dma(bcs[:], bc_d[:, :], writes=[bcs])
                with fw.scope() as esg:
                    w_if = fw.sb([128, 8, 8], BF16, "w_if", esg)
                    fw.dma(w_if[:], w_if_d.rearrange("(k p) c -> p k c", p=128), writes=[w_if], q="pool")
                    bif = fw.sb([128, 8], F32, "bif", esg)
                    fw.dma(bif[:], b_if_d[0:1, :].to_broadcast([128, 8]), writes=[bif])
                    hblk = [fw.sb([128, 8, 512], BF16, f"hblkG{i}", esg) for i in range(2)]
                    ifp = fw.sb([128, NT_EXT, 8], F32, "ifp", esg)
                    l1 = fw.sb([128, NT_EXT, 4], F32, "l1", esg)
                    tmpg = fw.sb([128, NT_EXT, 4], F32, "tmpg", esg)
                    for t in range(NT_EXT):
                        hb = hblk[(t // 4) % 2]
                        if t % 4 == 0:
                            fw.dma(hb[:], hT_d[:, :, t * 128:(t + 4) * 128], reads=hT_tiles[t:t + 4], writes=[hb])
                        tl = t % 4
                        for k in range(8):
                            mm(PS[0], PS[0][:, t * 8:(t + 1) * 8], hb[:, k, tl * 128:(tl + 1) * 128], w_if[:, k, :], k == 0, k == 7, [hb, w_if])
                    op("act", lambda e: e.copy(out=ifp[:], in_=PS[0][:, 0:256].rearrange("p (t c) -> p t c", c=8)), reads=[PS[0]], writes=[ifp])
                    op("dve", lambda e: e.tensor_tensor(out=ifp[:], in0=ifp[:], in1=bif[:].unsqueeze(1).to_broadcast([128, NT_EXT, 8]), op=ALU.add),
                       reads=[ifp, bif], writes=[ifp])
                    op("act", lambda e: e.activation(out=l1[:], in_=ifp[:, :, 4:8], func=AF.Exp, scale=-1.0), reads=[ifp], writes=[l1])
                    op("act", lambda e: e.activation(out=l1[:], in_=l1[:], func=AF.Ln, bias=c_one[:]), reads=[l1, c_one], writes=[l1])
                    l1f = l1[:].rearrange("p t c -> p (t c)")
                    mm(PS[1], PS[1][:, 0:128], U_f[:], l1f, True, True, [U_f, l1])
                    mm(PS[1], PS[1][:, 128:256], ones_f[:], l1f, True, True, [ones_f, l1])
                    op("act", lambda e: e.copy(out=tmpg[:], in_=PS[1][:, 0:128].rearrange("p (t c) -> p t c", c=4)), reads=[PS[1]], writes=[tmpg])
                    op("act", lambda e: e.activation(out=ff[:], in_=tmpg[:], func=AF.Exp, scale=-1.0), reads=[tmpg], writes=[ff])
                    op("act", lambda e: e.activation(out=fl[:], in_=PS[1][:, 128:256].rearrange("p (t c) -> p t c", c=4), func=AF.Exp, scale=-1.0),
                       reads=[PS[1]], writes=[fl])
                    op("dve", lambda e: e.tensor_tensor(out=tmpg[:], in0=tmpg[:], in1=ifp[:, :, 0:4], op=ALU.add), reads=[tmpg, ifp], writes=[tmpg])
                    op("act", lambda e: e.activation(out=ee[:], in_=tmpg[:], func=AF.Exp), reads=[tmpg], writes=[ee])
                    op("dve", lambda e: e.tensor_scalar(out=ee[:, 0:NT_OWN, :], in0=ee[:, 0:NT_OWN, :], scalar1=hv[:, 0:1], scalar2=None, op0=ALU.mult),
                       reads=[ee, hv], writes=[ee])
                ckpt("Cg")
                qTb = fw.sb([128, 4, S_OWN], BF16, "qTb", esCg)
                kTb = fw.sb([128, 4, S_EXT], BF16, "kTb", esCg)
                vaug = fw.sb([128, NT_EXT, 4, 129], BF16, "vaug", esCg)
                osig = fw.sb([128, NT_OWN, 512], BF16, "osig", esCg)
                op("pool", lambda e: e.memset(vaug[:, :, :, 128:129], 1.0), writes=[vaug])
                for hp in range(2):
                    with fw.scope() as esC1:
                        wq = fw.sb([128, 8, 256], BF16, f"wq{hp}", esC1)
                        wk = fw.sb([128, 8, 256], BF16, f"wk{hp}", esC1)
                        wvo = fw.sb([128, 8, 512], BF16, f"wvo{hp}", esC1)
                        fw.dma(wq[:], w_qk_d[:, hp * 256:(hp + 1) * 256].rearrange("(k p) c -> p k c", p=128), writes=[wq], q="pool")
                        fw.dma(wk[:], w_qk_d[:, 512 + hp * 256:512 + (hp + 1) * 256].rearrange("(k p) c -> p k c", p=128), writes=[wk], q="pool")
                        fw.dma(wvo[:, :, 0:256], w_vo_d[:, hp * 256:(hp + 1) * 256].rearrange("(k p) c -> p k c", p=128), writes=[wvo], q="pool")
                        fw.dma(wvo[:, :, 256:512], w_vo_d[:, 512 + hp * 256:512 + (hp + 1) * 256].rearrange("(k p) c -> p k c", p=128), writes=[wvo], q="pool")
                        hblk = [fw.sb([128, 8, 512], BF16, f"hblkC{hp}{i}", esC1) for i in range(2)]
                        uk = [fw.sb([128, 4 + S_EXT], BF16, f"uk{hp}{i}", esC1) for i in range(2)]
                        uq = [fw.sb([128, 4 + 2560], BF16, f"uq{hp}{i}", esC1) for i in range(2)]
                        ycv = [fw.sb([128, 512], F32, f"ycv{hp}{i}", esC1) for i in range(2)]
                        sgm = [fw.sb([128, 512], F32, f"sgm{hp}{i}", esC1) for i in range(2)]
                        for hh in range(2):
                            op("pool", lambda e: e.memset(uk[hh][:, 0:4], 0.0), writes=[uk[hh]])
                            op("pool", lambda e: e.memset(uq[hh][:, 0:4], 0.0), writes=[uq[hh]])
                        ukB = [[Buf(None, f"ukB{hp}{hh}{i}") for i in range(8)] for hh in range(2)]
                        uqB = [[Buf(None, f"uqB{hp}{hh}{i}") for i in range(5)] for hh in range(2)]
                        pi_ = [0]

                        def conv_piece(hh, typ, pc):
                            H = 2 * hp + hh
                            ci = typ * 4 + H
                            u = uq[hh] if typ == 0 else uk[hh]
                            if typ == 0:
                                off = 4 + 512 + pc * 512
                                ur = [uqB[hh][pc + 1], uqB[hh][pc]]
                            else:
                                off = 4 + pc * 512
                                ur = [ukB[hh][pc], ukB[hh][pc - 1] if pc > 0 else uk[hh]]
                            y_ = ycv[pi_[0] % 2]
                            s_ = sgm[pi_[0] % 2]
                            pi_[0] += 1
                            op("dve", lambda e: e.tensor_scalar(out=y_[:], in0=u[:, off - 3:off - 3 + 512], scalar1=wcs[:, ci, 0:1], scalar2=bcs[:, ci:ci + 1],
                                                                op0=ALU.mult, op1=ALU.add), reads=ur + [wcs, bcs], writes=[y_])
                            for j in range(1, 4):
                                op("dve", lambda e: e.scalar_tensor_tensor(out=y_[:], in0=u[:, off - 3 + j:off - 3 + j + 512], scalar=wcs[:, ci, j:j + 1], in1=y_[:],
                                                                           op0=ALU.mult, op1=ALU.add), reads=ur + [wcs, y_], writes=[y_])
                            if typ == 0:
                                op("act", lambda e: e.activation(out=qTb[:, H, pc * 512:(pc + 1) * 512], in_=y_[:], func=AF.Silu), reads=[y_], writes=[qTb])
                            else:
                                op("act", lambda e: e.activation(out=s_[:], in_=y_[:], func=AF.Sigmoid), reads=[y_], writes=[s_])
                                op("dve", lambda e: e.scalar_tensor_tensor(out=kTb[:, H, pc * 512:(pc + 1) * 512], in0=y_[:], scalar=128.0 ** -0.5, in1=s_[:],
                                                                           op0=ALU.mult, op1=ALU.mult), reads=[y_, s_], writes=[kTb])

                        def conv_for_block(bdone):
                            for hh in range(2):
                                conv_piece(hh, 1, bdone)
                                if bdone >= 4:
                                    conv_piece(hh, 0, bdone - 4)

                        for blk in range(8):
                            hb = hblk[blk % 2]
                            fw.dma(hb[:], hT_d[:, :, blk * 512:(blk + 1) * 512], reads=hT_tiles[4 * blk:4 * blk + 4], writes=[hb])
                            for hh in range(2):
                                for k in range(8):
                                    mm(PS[hh], PS[hh][:, :], wk[:, k, hh * 128:(hh + 1) * 128], hb[:, k, :], k == 0, k == 7, [wk, hb])
                                op("act", lambda e: e.copy(out=uk[hh][:, 4 + blk * 512:4 + (blk + 1) * 512], in_=PS[hh][:, :]), reads=[PS[hh]], writes=[ukB[hh][blk]])
                            if blk >= 3:
                                for hh in range(2):
                                    for k in range(8):
                                        mm(PS[2 + hh], PS[2 + hh][:, :], wq[:, k, hh * 128:(hh + 1) * 128], hb[:, k, :], k == 0, k == 7, [wq, hb])
                                    op("act", lambda e: e.copy(out=uq[hh][:, 4 + (blk - 3) * 512:4 + (blk - 2) * 512], in_=PS[2 + hh][:, :]),
                                       reads=[PS[2 + hh]], writes=[uqB[hh][blk - 3]])
                            for tl in range(4):
                                t = blk * 4 + tl
                                bv = 4 + tl
                                nvo = 512 if blk >= 4 else 256
                                for k in range(8):
                                    mm(PS[bv], PS[bv][:, 0:nvo], hb[:, k, tl * 128:(tl + 1) * 128], wvo[:, k, 0:nvo], k == 0, k == 7, [wvo, hb])
                                op("dve", lambda e: e.tensor_copy(out=vaug[:, t, 2 * hp:2 * hp + 2, 0:128], in_=PS[bv][:, 0:256].rearrange("p (h d) -> p h d", d=128)),
                                   reads=[PS[bv]], writes=[vaug])
                                if blk >= 4:
                                    op("act", lambda e: e.activation(out=osig[:, t - NT_OWN, hp * 256:(hp + 1) * 256], in_=PS[bv][:, 256:512], func=AF.Sigmoid),
                                       reads=[PS[bv]], writes=[osig])
                            if blk >= 1:
                                conv_for_block(blk - 1)
                        conv_for_block(7)
                ckpt("C1")
                with fw.scope() as esC3:
                    ktokR = [fw.sb([128, 4, 128], BF16, f"ktokR{i}", esC3) for i in range(3)]
                    CTall = fw.sb([128, NT_OWN, 4, 129], BF16, "CTall", esC3)
                    Xs = [fw.sb([128, 129], F32, f"Xs{H}", esC3) for H in range(4)]
                    Sm = [[fw.sb([128, 128], BF16, f"Sm{H}{i}", esC3) for i in range(2)] for H in range(4)]
                    hm_ = [fw.sb([128, 128], F32, f"hm{H}", esC3) for H in range(4)]
                    yb_ = [fw.sb([128, 128], BF16, f"yb{H}", esC3) for H in range(4)]
                    jk = [fw.sb([128, 128], BF16, f"jk{H}", esC3) for H in range(4)]
                    smc = [fw.sb([128, 8], F32, f"smc{H}", esC3) for H in range(4)]
                    for H in range(4):
                        op("dve", lambda e: e.tensor_tensor(out=vaug[:, :, H, :], in0=vaug[:, :, H, :],
                                                            in1=ee[:, :, H:H + 1].to_broadcast([128, NT_EXT, 129]), op=ALU.mult), reads=[vaug, ee], writes=[vaug])

                    def k_tr(t):
                        bk = t % 2
                        for H in range(4):
                            op("pe", lambda e: e.transpose(out=psbf(bk)[:, H * 128:(H + 1) * 128], in_=kTb[:, H, t * 128:(t + 1) * 128], identity=idb[:]),
                               reads=[kTb, idb], writes=[PS[bk]])
                        op("act", lambda e: e.copy(out=ktokR[t % 3][:], in_=psbf(bk)[:, 0:512].rearrange("p (h d) -> p h d", d=128)), reads=[PS[bk]], writes=[ktokR[t % 3]])

                    k_tr(0)
                    for t in range(NT_EXT - 1):
                        if t + 1 < NT_EXT - 1:
                            k_tr(t + 1)
                        for H in range(4):
                            bU = 2 + H
                            mm(PS[bU], PS[bU][:, 0:129], ktokR[t % 3][:, H, :], vaug[:, t, H, :], True, True, [ktokR[t % 3], vaug])
                            if t == 0:
                                op("dve", lambda e: e.tensor_copy(out=Xs[H][:], in_=PS[bU][:, 0:129]), reads=[PS[bU]], writes=[Xs[H]])
                            else:
                                op("dve", lambda e: e.scalar_tensor_tensor(out=Xs[H][:], in0=Xs[H][:], scalar=fl[:, t - 1, H:H + 1], in1=PS[bU][:, 0:129],
                                                                           op0=ALU.mult, op1=ALU.add), reads=[Xs[H], fl, PS[bU]], writes=[Xs[H]])
                            if t + 1 >= NT_OWN:
                                op("act", lambda e: e.activation(out=CTall[:, t + 1 - NT_OWN, H, :], in_=Xs[H][:], func=AF.Copy, scale=fl[:, t, H:H + 1]),
                                   reads=[Xs[H], fl], writes=[CTall])
                    sc4 = fw.sb([128, 4, 8], F32, "sc4", esC3)

                    def stA(t):
                        tq = t - NT_OWN
                        for H in range(4):
                            sm_ = Sm[H][tq % 2]
                            mm(PS[H], PS[H][:, 0:128], kTb[:, H, t * 128:(t + 1) * 128], qTb[:, H, tq * 128:(tq + 1) * 128], True, True, [kTb, qTb])
                            op("dve", lambda e: e.tensor_tensor(out=sm_[:], in0=PS[H][:, 0:128], in1=caus[:], op=ALU.mult), reads=[PS[H], caus], writes=[sm_])

                    def stRest(t):
                        tq = t - NT_OWN
                        for H in range(4):
                            sm_ = Sm[H][tq % 2]
                            bA = 4 + H
                            mm(PS[bA], PS[bA][:, 0:129], sm_[:], vaug[:, t, H, :], True, False, [sm_, vaug])
                            mm(PS[bA], PS[bA][:, 0:129], qTb[:, H, tq * 128:(tq + 1) * 128], CTall[:, tq, H, :], False, True, [qTb, CTall])
                        for H in range(4):
                            op("act", lambda e: e.activation(out=sc4[:, H, 6:7], in_=PS[4 + H][:, 128:129], func=AF.Abs, scale=ff[:, t, H:H + 1]),
                               reads=[PS[4 + H], ff], writes=[sc4])
                        op("dve", lambda e: e.tensor_scalar(out=sc4[:, :, 0:1], in0=sc4[:, :, 6:7], scalar1=1.0, scalar2=None, op0=ALU.max), reads=[sc4], writes=[sc4])
                        op("dve", lambda e: e.reciprocal(out=sc4[:, :, 1:2], in_=sc4[:, :, 0:1]), reads=[sc4], writes=[sc4])
                        op("dve", lambda e: e.tensor_tensor(out=sc4[:, :, 2:3], in0=sc4[:, :, 1:2], in1=ff[:, t, :].unsqueeze(2), op=ALU.mult), reads=[sc4, ff], writes=[sc4])
                        for H in range(4):
                            op("dve", lambda e: e.scalar_tensor_tensor(out=hm_[H][:], in0=PS[4 + H][:, 0:128], scalar=sc4[:, H, 2:3], in1=osig[:, tq, H * 128:(H + 1) * 128],
                                                                       op0=ALU.mult, op1=ALU.mult), reads=[PS[4 + H], sc4, osig], writes=[hm_[H]])
                        for H in range(4):
                            op("act", lambda e: e.activation(out=jk[H][:], in_=hm_[H][:], func=AF.Square, accum_out=sc4[:, H, 3:4]), reads=[hm_[H]], writes=[jk[H], sc4])
                        op("act", lambda e: e.activation(out=sc4[:, :, 4:5], in_=sc4[:, :, 3:4], func=AF.Sqrt, bias=c_eps[:], scale=1.0 / 128), reads=[sc4, c_eps], writes=[sc4])
                        op("dve", lambda e: e.reciprocal(out=sc4[:, :, 5:6], in_=sc4[:, :, 4:5]), reads=[sc4], writes=[sc4])
                        for H in range(4):
                            op("dve", lambda e: e.scalar_tensor_tensor(out=yb_[H][:], in0=hm_[H][:], scalar=sc4[:, H, 5:6], in1=ghn[:, H * 128:(H + 1) * 128],
                                                                       op0=ALU.mult, op1=ALU.mult), reads=[hm_[H], sc4, ghn], writes=[yb_[H]])
                        for H in range(4):
                            op("pe", lambda e: e.transpose(out=psbf(4 + H)[:, 512:640], in_=yb_[H][:], identity=idb[:]), reads=[yb_[H], idb], writes=[PS[4 + H]])
                        for H in range(4):
                            op("act", lambda e: e.copy(out=YT[:, 4 + H, tq * 128:(tq + 1) * 128], in_=psbf(4 + H)[:, 512:640]), reads=[PS[4 + H]], writes=[YT])

                    stA(NT_OWN)
                    for t in range(NT_OWN, NT_EXT):
                        if t + 1 < NT_EXT:
                            stA(t + 1)
                        stRest(t)
            ckpt("C")
            if "ybT" in dbg:
                o = dbg_t("ybT", [128, 4, S_OWN], BF16)
                fw.dma(o[:, :, :], YT[:, 4:8, :], reads=[YT], is_output=True)


            with fw.scope() as esD:
                x1 = fw.sb([128, NT_OWN, D], F32, "x1", esD)
                with fw.scope() as esD1:
                    mixT = fw.sb([128, 8, S_OWN], BF16, "mixT", esD1)
                    with fw.scope() as esD1a:
                        hTo = fw.sb([128, 8, S_OWN], BF16, "hTo", esD1a)
                        for tb in range(4):
                            fw.dma(hTo[:, :, tb * 512:(tb + 1) * 512], hT_d[:, :, S_OWN + tb * 512:S_OWN + (tb + 1) * 512],
                                   reads=hT_tiles[NT_OWN + 4 * tb:NT_OWN + 4 * tb + 4], writes=[hTo])
                        wga = [fw.sb([128, 8, 128], BF16, f"wga{i}", esD1a) for i in range(2)]
                        wgb = [fw.sb([128, 8, 128], BF16, f"wgb{i}", esD1a) for i in range(2)]
                        wpa = [fw.sb([128, 4, 128], BF16, f"wpa{i}", esD1a) for i in range(2)]
                        wpb = [fw.sb([128, 4, 128], BF16, f"wpb{i}", esD1a) for i in range(2)]
                        sga = [fw.sb([128, 512], BF16, f"sga{i}", esD1a) for i in range(2)]
                        sgb = [fw.sb([128, 512], BF16, f"sgb{i}", esD1a) for i in range(2)]
                        t1 = [fw.sb([128, 512], F32, f"t1_{i}", esD1a) for i in range(2)]
                        t2 = [fw.sb([128, 512], F32, f"t2_{i}", esD1a) for i in range(2)]
                        it = 0
                        for j in range(8):
                            w_ = j % 2
                            fw.dma(wga[w_][:], w_mg_d[:, j * 128:(j + 1) * 128].rearrange("(k p) c -> p k c", p=128), writes=[wga[w_]], q="pool")
                            fw.dma(wgb[w_][:], w_mg_d[:, 1024 + j * 128:1024 + (j + 1) * 128].rearrange("(k p) c -> p k c", p=128), writes=[wgb[w_]], q="pool")
                            fw.dma(wpa[w_][:], w_pa_d[:, j * 128:(j + 1) * 128].rearrange("(k p) c -> p k c", p=128), writes=[wpa[w_]], q="pool")
                            fw.dma(wpb[w_][:], w_pb_d[:, j * 128:(j + 1) * 128].rearrange("(k p) c -> p k c", p=128), writes=[wpb[w_]], q="pool")
                            for tb in range(4):
                                r = it % 2
                                it += 1
                                b0 = 4 * r
                                ts_ = slice(tb * 512, (tb + 1) * 512)
                                for k in range(8):
                                    mm(PS[b0], PS[b0][:, :], wga[w_][:, k, :], hTo[:, k, ts_], k == 0, k == 7, [wga[w_], hTo])
                                op("act", lambda e: e.activation(out=sga[r][:], in_=PS[b0][:, :], func=AF.Sigmoid), reads=[PS[b0]], writes=[sga[r]])
                                for k in range(8):
                                    mm(PS[b0 + 1], PS[b0 + 1][:, :], wgb[w_][:, k, :], hTo[:, k, ts_], k == 0, k == 7, [wgb[w_], hTo])
                                op("act", lambda e: e.activation(out=sgb[r][:], in_=PS[b0 + 1][:, :], func=AF.Sigmoid), reads=[PS[b0 + 1]], writes=[sgb[r]])
                                for k in range(4):
                                    mm(PS[b0 + 2], PS[b0 + 2][:, :], wpa[w_][:, k, :], YT[:, k, ts_], k == 0, k == 3, [wpa[w_], YT])
                                for k in range(4):
                                    mm(PS[b0 + 3], PS[b0 + 3][:, :], wpb[w_][:, k, :], YT[:, 4 + k, ts_], k == 0, k == 3, [wpb[w_], YT])
                                op("dve", lambda e: e.tensor_tensor(out=t1[r][:], in0=PS[b0 + 2][:, :], in1=sga[r][:], op=ALU.mult), reads=[PS[b0 + 2], sga[r]], writes=[t1[r]])
                                op("dve", lambda e: e.tensor_tensor(out=t2[r][:], in0=PS[b0 + 3][:, :], in1=sgb[r][:], op=ALU.mult), reads=[PS[b0 + 3], sgb[r]], writes=[t2[r]])
                                op("pool", lambda e: e.tensor_tensor(out=mixT[:, j, ts_], in0=t1[r][:], in1=t2[r][:], op=ALU.add), reads=[t1[r], t2[r]], writes=[mixT])
                    ckpt("D1a")
                    with fw.scope() as esD1b:
                        w_out = fw.sb([128, 8, D], BF16, "w_out", esD1b)
                        fw.dma(w_out[:], w_out_d.rearrange("(k p) c -> p k c", p=128), writes=[w_out], q="pool")
                        xtl = [fw.sb([128, D], F32, f"xtl{i}", esD1b) for i in range(2)]
                        for t in range(NT_OWN):
                            x_ = xtl[t % 2]
                            fw.dma(x_[:], xe[S_OWN + t * 128:S_OWN + (t + 1) * 128, :], writes=[x_])
                            for half in range(2):
                                b = 2 * (t % 2) + half
                                for j in range(8):
                                    mm(PS[b], PS[b][:, :], mixT[:, j, t * 128:(t + 1) * 128], w_out[:, j, half * 512:(half + 1) * 512], j == 0, j == 7, [mixT, w_out])
                                op("dve", lambda e: e.tensor_tensor(out=x1[:, t, half * 512:(half + 1) * 512], in0=PS[b][:, :], in1=x_[:, half * 512:(half + 1) * 512], op=ALU.add),
                                   reads=[PS[b], x_], writes=[x1])
                ckpt("D1")
                if "x1" in dbg:
                    fw.dma(dbg_t("x1", [128, NT_OWN, D]), x1[:], reads=[x1], is_output=True)
                with fw.scope() as esM:
                    load_gain(1)
                    gateT = fw.sb([16, S_OWN], BF16, "gateT", esM)
                    E16 = fw.sb([16, 16, 128], BF16, "E16", esM)
                    op("pool", lambda e: e.memset(E16[:], 1.0), writes=[E16])
                    op("pool", lambda e: e.affine_select(out=E16[:], in_=E16[:], pattern=[[-1, 16], [0, 128]], compare_op=ALU.is_equal, fill=0.0,
                                                         base=0, channel_multiplier=1), reads=[E16], writes=[E16])
                    NW = 3
                    w13 = [fw.sb([128, 8, 512], BF16, f"w13_{i}", esM) for i in range(NW)]
                    w2e = [fw.sb([128, 2, D], BF16, f"w2e_{i}", esM) for i in range(NW)]

                    def load_w(ex):
                        wb = ex % NW
                        fw.dma(w13[wb][:], w_e13_d[ex].rearrange("(k p) c -> p k c", p=128), writes=[w13[wb]], q="pool")
                        fw.dma(w2e[wb][:], w_e2_d[ex].rearrange("(k p) c -> p k c", p=128), writes=[w2e[wb]], q="pool")

                    load_w(0)
                    load_w(1)
                    with fw.scope() as esR:
                        w_r = fw.sb([128, 8, 20], F32, "w_r", esR)
                        fw.dma(w_r[:], w_r_d.rearrange("(k p) c -> p k c", p=128), writes=[w_r])
                        b_r = fw.sb([128, 20], F32, "b_r", esR)
                        fw.dma(b_r[:], b_r_d[0:1, :].to_broadcast([128, 20]), writes=[b_r])
                        hnf = [fw.sb([128, D], F32, f"hnf{i}", esR) for i in range(2)]
                        hnTf = [fw.sb([128, 8, 128], F32, f"hnTf{i}", esR) for i in range(2)]
                        junkR = fw.sb([128, D], BF16, "junkR", esR)
                        ssr = [fw.sb([128, 1], F32, f"ssr{i}", esR) for i in range(2)]
                        rrr = [fw.sb([128, 1], F32, f"rrr{i}", esR) for i in range(2)]
                        T_ = NT_OWN
                        lgA = fw.sb([128, T_, 20], F32, "lgA", esR)

                        def r_front(t):
                            r = t % 2
                            rs = {"ss": ssr[r], "r": rrr[r]}
                            rms_rstd({"ap": x1[:, t, :], "bufs": [x1]}, rs, D, {"ap": junkR[:], "buf": junkR})
                            op("dve", lambda e: e.scalar_tensor_tensor(out=hnf[r][:], in0=x1[:, t, :], scalar=rs["r"][:], in1=gB[:], op0=ALU.mult, op1=ALU.mult),
                               reads=[x1, rs["r"], gB], writes=[hnf[r]])
                            for k in range(8):
                                b = 2 * r + (0 if k < 4 else 1)
                                op("pe", lambda e: e.transpose(out=PS[b][:, (k % 4) * 128:(k % 4 + 1) * 128], in_=hnf[r][:, k * 128:(k + 1) * 128], identity=idf[:]),
                                   reads=[hnf[r], idf], writes=[PS[b]])
                            for bb in range(2):
                                b = 2 * r + bb
                                op("act", lambda e: e.copy(out=hnTf[r][:, 4 * bb:4 * bb + 4, :], in_=PS[b][:, :].rearrange("p (k t) -> p k t", k=4)), reads=[PS[b]], writes=[hnTf[r]])
                                op("dve", lambda e: e.tensor_copy(out=YT[:, 4 * bb:4 * bb + 4, t * 128:(t + 1) * 128], in_=PS[b][:, :].rearrange("p (k t) -> p k t", k=4)),
                                   reads=[PS[b]], writes=[YT])

                        def r_back(t):
                            r = t % 2
                            bl = 4 + r
                            for k in range(8):
                                mm(PS[bl], PS[bl][:, 0:20], hnTf[r][:, k, :], w_r[:, k, :], k == 0, k == 7, [hnTf[r], w_r])
                            op("dve", lambda e: e.tensor_tensor(out=lgA[:, t, :], in0=PS[bl][:, 0:20], in1=b_r[:], op=ALU.add), reads=[PS[bl], b_r], writes=[lgA])

                        for t in range(T_ + 1):
                            if t < T_:
                                r_front(t)
                            if t >= 1:
                                r_back(t - 1)
                        gl = lgA[:, :, 0:4]
                        el = lgA[:, :, 4:20].rearrange("p t (g e) -> p t g e", g=4)
                        gmax = fw.sb([128, T_], F32, "gmax", esR)
                        g1h = fw.sb([128, T_, 4], F32, "g1h", esR)
                        exg = fw.sb([128, T_, 4], F32, "exg", esR)
                        pgs = fw.sb([128, T_], F32, "pgs", esR)
                        t16 = fw.sb([128, T_, 4, 4], F32, "t16", esR)
                        elg = fw.sb([128, T_, 4], F32, "elg", esR)
                        elg2 = fw.sb([128, T_, 4], F32, "elg2", esR)
                        ev1 = fw.sb([128, T_], F32, "ev1", esR)
                        ev2 = fw.sb([128, T_], F32, "ev2", esR)
                        mk1 = fw.sb([128, T_, 4], F32, "mk1", esR)
                        mk2 = fw.sb([128, T_, 4], F32, "mk2", esR)
                        w12 = fw.sb([128, 2, T_], F32, "w12", esR)
                        gig = fw.sb([128, T_, 4], F32, "gig", esR)
                        gate = fw.sb([128, T_, 4, 4], F32, "gate", esR)
                        B3 = [128, T_, 4]
                        op("dve", lambda e: e.tensor_reduce(out=gmax[:], in_=gl, axis=AX.X, op=ALU.max), reads=[lgA], writes=[gmax])
                        op("dve", lambda e: e.tensor_tensor(out=g1h[:], in0=gl, in1=gmax[:].unsqueeze(2).to_broadcast(B3), op=ALU.is_equal), reads=[lgA, gmax], writes=[g1h])
                        op("dve", lambda e: e.tensor_tensor(out=exg[:], in0=gl, in1=gmax[:].unsqueeze(2).to_broadcast(B3), op=ALU.subtract), reads=[lgA, gmax], writes=[exg])
                        op("act", lambda e: e.activation(out=exg[:], in_=exg[:], func=AF.Exp), reads=[exg], writes=[exg])
                        op("dve", lambda e: e.tensor_reduce(out=pgs[:], in_=exg[:], axis=AX.X, op=ALU.add), reads=[exg], writes=[pgs])
                        op("dve", lambda e: e.reciprocal(out=pgs[:], in_=pgs[:]), reads=[pgs], writes=[pgs])
                        op("dve", lambda e: e.tensor_tensor(out=t16[:], in0=el, in1=g1h[:].unsqueeze(3).to_broadcast([128, T_, 4, 4]), op=ALU.mult), reads=[lgA, g1h], writes=[t16])
                        op("dve", lambda e: e.tensor_reduce(out=elg[:], in_=t16[:].rearrange("p t g e -> p t e g"), axis=AX.X, op=ALU.add), reads=[t16], writes=[elg])
                        op("dve", lambda e: e.tensor_reduce(out=ev1[:], in_=elg[:], axis=AX.X, op=ALU.max), reads=[elg], writes=[ev1])
                        op("dve", lambda e: e.tensor_tensor(out=mk1[:], in0=elg[:], in1=ev1[:].unsqueeze(2).to_broadcast(B3), op=ALU.is_equal), reads=[elg, ev1], writes=[mk1])
                        op("dve", lambda e: e.scalar_tensor_tensor(out=elg2[:], in0=mk1[:], scalar=-1e30, in1=elg[:], op0=ALU.mult, op1=ALU.add), reads=[mk1, elg], writes=[elg2])
                        op("dve", lambda e: e.tensor_reduce(out=ev2[:], in_=elg2[:], axis=AX.X, op=ALU.max), reads=[elg2], writes=[ev2])
                        op("dve", lambda e: e.tensor_tensor(out=mk2[:], in0=elg2[:], in1=ev2[:].unsqueeze(2).to_broadcast(B3), op=ALU.is_equal), reads=[elg2, ev2], writes=[mk2])
                        op("dve", lambda e: e.tensor_tensor(out=w12[:, 0, :], in0=ev1[:], in1=ev2[:], op=ALU.subtract), reads=[ev1, ev2], writes=[w12])
                        op("act", lambda e: e.activation(out=w12[:, 0, :], in_=w12[:, 0, :], func=AF.Sigmoid), reads=[w12], writes=[w12])
                        op("dve", lambda e: e.tensor_scalar(out=w12[:, 1, :], in0=w12[:, 0, :], scalar1=-1.0, scalar2=1.0, op0=ALU.mult, op1=ALU.add), reads=[w12], writes=[w12])
                        op("dve", lambda e: e.tensor_tensor(out=w12[:], in0=w12[:], in1=pgs[:].unsqueeze(1).to_broadcast([128, 2, T_]), op=ALU.mult), reads=[w12, pgs], writes=[w12])
                        op("dve", lambda e: e.tensor_tensor(out=gig[:], in0=mk1[:], in1=w12[:, 0, :].unsqueeze(2).to_broadcast(B3), op=ALU.mult), reads=[mk1, w12], writes=[gig])
                        op("dve", lambda e: e.tensor_tensor(out=mk2[:], in0=mk2[:], in1=w12[:, 1, :].unsqueeze(2).to_broadcast(B3), op=ALU.mult), reads=[mk2, w12], writes=[mk2])
                        op("dve", lambda e: e.tensor_tensor(out=gig[:], in0=gig[:], in1=mk2[:], op=ALU.add), reads=[gig, mk2], writes=[gig])
                        op("dve", lambda e: e.tensor_tensor(out=gate[:], in0=g1h[:].unsqueeze(3).to_broadcast([128, T_, 4, 4]),
                                                            in1=gig[:].unsqueeze(2).to_broadcast([128, T_, 4, 4]), op=ALU.mult), reads=[g1h, gig], writes=[gate])
                        for t4 in range(T_ // 4):
                            bk = 6 + t4 % 2
                            for j in range(4):
                                t = t4 * 4 + j
                                op("pe", lambda e: e.transpose(out=PS[bk][0:16, j * 128:(j + 1) * 128], in_=gate[:, t, :, :].rearrange("p g e -> p (g e)"), identity=idf[:]),
                                   reads=[gate, idf], writes=[PS[bk]])
                            op("act", lambda e: e.copy(out=gateT[:, t4 * 512:(t4 + 1) * 512], in_=PS[bk][0:16, :]), reads=[PS[bk]], writes=[gateT])
                    ckpt("D2r")
                    if "gateT" in dbg:
                        fw.dma(dbg_t("gateT", [16, S_OWN], BF16), gateT[:], reads=[gateT], is_output=True)
                    with fw.scope() as esE:
                        sgE = [fw.sb([128, 512], F32, f"sgE{i}", esE) for i in range(2)]
                        tE = [fw.sb([128, 512], F32, f"tE{i}", esE) for i in range(2)]
                        actT = [[fw.sb([128, 512], BF16, f"actT{i}{fc}", esE) for fc in range(2)] for i in range(2)]
                        x1M = [Buf(x1.t, f"x1m_{t}") for t in range(NT_OWN)]
                        for b_ in x1M:
                            b_.lw = x1.lw
                            b_.rd = dict(x1.rd)
                        ybank = [4, 5, 7]
                        yi = [0]

                        def e_front_pe(it):
                            ex, tb = it // 4, it % 4
                            wb = ex % NW
                            ts_ = slice(tb * 512, (tb + 1) * 512)
                            mm(PS[6], PS[6][:, :], E16[:, ex, :], gateT[:, ts_], True, True, [E16, gateT])
                            for fc in range(2):
                                for k in range(8):
                                    mm(PS[fc], PS[fc][:, :], w13[wb][:, k, fc * 128:(fc + 1) * 128], YT[:, k, ts_], k == 0, k == 7, [w13[wb], YT])
                                for k in range(8):
                                    mm(PS[2 + fc], PS[2 + fc][:, :], w13[wb][:, k, 256 + fc * 128:256 + (fc + 1) * 128], YT[:, k, ts_], k == 0, k == 7, [w13[wb], YT])

                        def e_front_post(it):
                            r = it % 2
                            for fc in range(2):
                                op("act", lambda e: e.activation(out=sgE[fc][:], in_=PS[fc][:, :], func=AF.Silu), reads=[PS[fc]], writes=[sgE[fc]])
                                op("dve", lambda e: e.tensor_tensor(out=tE[fc][:], in0=PS[2 + fc][:, :], in1=sgE[fc][:], op=ALU.mult), reads=[PS[2 + fc], sgE[fc]], writes=[tE[fc]])
                                op("dve", lambda e: e.tensor_tensor(out=actT[r][fc][:], in0=PS[6][:, :], in1=tE[fc][:], op=ALU.mult), reads=[PS[6], tE[fc]], writes=[actT[r][fc]])

                        def e_back(it):
                            ex, tb = it // 4, it % 4
                            wb = ex % NW
                            r = it % 2
                            for tt in range(4):
                                t = tb * 4 + tt
                                for half in range(2):
                                    b = ybank[yi[0] % 3]
                                    yi[0] += 1
                                    for fc in range(2):
                                        mm(PS[b], PS[b][:, :], actT[r][fc][:, tt * 128:(tt + 1) * 128], w2e[wb][:, fc, half * 512:(half + 1) * 512], fc == 0, fc == 1, [actT[r][fc], w2e[wb]])
                                    op("dve", lambda e: e.tensor_tensor(out=x1[:, t, half * 512:(half + 1) * 512], in0=PS[b][:, :], in1=x1[:, t, half * 512:(half + 1) * 512], op=ALU.add),
                                       reads=[PS[b], x1M[t]], writes=[x1M[t]])

                        NIT = 64
                        for it in range(NIT + 1):
                            if it < NIT:
                                e_front_pe(it)
                                e_front_post(it)
                            if it >= 1:
                                e_back(it - 1)
                            if it < NIT and it % 4 == 0 and it // 4 + 2 < 16:
                                load_w(it // 4 + 2)
                        for b_ in x1M:
                            if b_.lw is not None and (x1.lw is None or True):
                                pass
                        x1.lw = None
                        x1.rd = {}
                        fw.barrier()
                ckpt("D2")
                if "x2" in dbg:
                    fw.dma(dbg_t("x2", [128, NT_OWN, D]), x1[:], reads=[x1], is_output=True)
                with fw.scope() as esP:
                    load_gain(2)
                    gB2 = fw.sb([128, D], F32, "gB2", esP)
                    fw.dma(gB2[:], gvec_d[3:4, :].to_broadcast([128, D]), writes=[gB2])
                    w_pg = fw.sb([128, 8, D], BF16, "w_pg", esP)
                    fw.dma(w_pg[:], w_pg_d.rearrange("(k p) c -> p k c", p=128), writes=[w_pg], q="pool")
                    w_pp = fw.sb([128, 2, D], BF16, "w_pp", esP)
                    fw.dma(w_pp[:], w_pp_d.rearrange("(k p) c -> p k c", p=128), writes=[w_pp], q="pool")
                    hpb = [fw.sb([128, D], BF16, f"hpb{i}", esP) for i in range(3)]
                    hpT = [fw.sb([128, 8, 128], BF16, f"hpT{i}", esP) for i in range(3)]
                    plb = [fw.sb([128, 256], BF16, f"plb{i}", esP) for i in range(3)]
                    plT = [fw.sb([128, 2, 128], BF16, f"plT{i}", esP) for i in range(3)]
                    junkP2 = fw.sb([128, D], BF16, "junkP2", esP)
                    sgP = [fw.sb([128, 512], F32, f"sgP{i}", esP) for i in range(2)]
                    tP = [fw.sb([128, 512], F32, f"tP{i}", esP) for i in range(2)]
                    outt = [fw.sb([128, D], F32, f"outt{i}", esP) for i in range(2)]
                    junkP = fw.sb([128, D], BF16, "junkP", esP)
                    ssp = [fw.sb([128, 1], F32, f"ssp{i}", esP) for i in range(5)]
                    rrp = [fw.sb([128, 1], F32, f"rrp{i}", esP) for i in range(5)]
                    x1T = [Buf(x1.t, f"x1_{t}") for t in range(NT_OWN)]
                    for b_ in x1T:
                        b_.lw = x1.lw
                        b_.rd = dict(x1.rd)

                    def p_s1(t):
                        r = t % 3
                        fw.dma(plb[r][:], pl_d[t * 128:(t + 1) * 128, :], writes=[plb[r]], q="pool")
                        rs = {"ss": ssp[r], "r": rrp[r]}
                        rms_rstd({"ap": x1[:, t, :], "bufs": [x1T[t]]}, rs, D, {"ap": junkP[:], "buf": junkP})
                        op("dve", lambda e: e.scalar_tensor_tensor(out=hpb[r][:], in0=x1[:, t, :], scalar=rs["r"][:], in1=gB[:], op0=ALU.mult, op1=ALU.mult),
                           reads=[x1T[t], rs["r"], gB], writes=[hpb[r]])

                    def p_s2(t):
                        r = t % 3
                        b0 = 2 * (t % 2)
                        for k in range(8):
                            op("pe", lambda e: e.transpose(out=psbf(b0)[:, k * 128:(k + 1) * 128], in_=hpb[r][:, k * 128:(k + 1) * 128], identity=idb[:]), reads=[hpb[r], idb], writes=[PS[b0]])
                        op("act", lambda e: e.copy(out=hpT[r][:], in_=psbf(b0).rearrange("p (k t) -> p k t", k=8)), reads=[PS[b0]], writes=[hpT[r]])
                        for k in range(2):
                            op("pe", lambda e: e.transpose(out=psbf(b0 + 1)[:, k * 128:(k + 1) * 128], in_=plb[r][:, k * 128:(k + 1) * 128], identity=idb[:]), reads=[plb[r], idb], writes=[PS[b0 + 1]])
                        op("act", lambda e: e.copy(out=plT[r][:], in_=psbf(b0 + 1)[:, 0:256].rearrange("p (k t) -> p k t", k=2)), reads=[PS[b0 + 1]], writes=[plT[r]])

                    def p_s3(t):
                        r = t % 3
                        for half in range(2):
                            hs = slice(half * 512, (half + 1) * 512)
                            bG = 4 + half
                            bP = 6 + half
                            for k in range(8):
                                mm(PS[bG], PS[bG][:, :], hpT[r][:, k, :], w_pg[:, k, hs], k == 0, k == 7, [hpT[r], w_pg])
                            for k in range(2):
                                mm(PS[bP], PS[bP][:, :], plT[r][:, k, :], w_pp[:, k, hs], k == 0, k == 1, [plT[r], w_pp])
                            op("act", lambda e: e.activation(out=sgP[half][:], in_=PS[bG][:, :], func=AF.Sigmoid), reads=[PS[bG]], writes=[sgP[half]])
                            op("dve", lambda e: e.tensor_tensor(out=tP[half][:], in0=PS[bP][:, :], in1=sgP[half][:], op=ALU.mult), reads=[PS[bP], sgP[half]], writes=[tP[half]])
                            op("dve", lambda e: e.tensor_tensor(out=x1[:, t, hs], in0=x1[:, t, hs], in1=tP[half][:], op=ALU.add), reads=[x1T[t], tP[half]], writes=[x1T[t]])
                        rs2 = {"ss": ssp[3 + t % 2], "r": rrp[3 + t % 2]}
                        rms_rstd({"ap": x1[:, t, :], "bufs": [x1T[t]]}, rs2, D, {"ap": junkP2[:], "buf": junkP2})
                        o_ = outt[t % 2]
                        op("dve", lambda e: e.scalar_tensor_tensor(out=o_[:], in0=x1[:, t, :], scalar=rs2["r"][:], in1=gB2[:], op0=ALU.mult, op1=ALU.mult),
                           reads=[x1T[t], rs2["r"], gB2], writes=[o_])
                        fw.dma(out_d[t * 128:(t + 1) * 128, :], o_[:], reads=[o_], is_output=True)

                    for i in range(NT_OWN + 2):
                        if i < NT_OWN:
                            p_s1(i)
                        if 1 <= i <= NT_OWN:
                            p_s2(i - 1)
                        if i >= 2:
                            p_s3(i - 2)

            if "yaT" in dbg:
                o = dbg_t("yaT", [128, 4, S_OWN], BF16)
                fw.dma(o[:, :, :], YT[:, 0:4, :], reads=[YT], is_output=True)

            if "hT" in dbg:
                o = dbg_t("hT", [128, 8, S_EXT], BF16)
                with fw.scope() as esd:
                    tmp = fw.sb([128, 8, 512], BF16, "dbg_hT", esd)
                    for i in range(8):
                        fw.dma(tmp[:], hT_d[:, :, i * 512:(i + 1) * 512], reads=hT_tiles[4 * i:4 * i + 4], writes=[tmp])
                        fw.dma(o[:, :, i * 512:(i + 1) * 512], tmp[:], reads=[tmp], is_output=True)


        body()
        fw.stopped = False
        fw.finish()
    return nc, dbg_out


_INV = (500000.0 ** (-np.arange(0, 16, 2, dtype=np.float32) / 16.0)).astype(np.float32)


def make_in_maps(inputs):
    f = lambda a: np.ascontiguousarray(np.asarray(a), dtype=np.float32)
    x = f(inputs["x"]); p = f(inputs["p"])
    positions = np.asarray(inputs["positions"]).astype(np.int32)
    w_in = f(inputs["w_in"])[0]
    offs = np.cumsum([0, 512, 128, 128, 128, 128, 128, 128, 24, 1024, 512, 512, 8, 2048])
    seg = {n: (offs[i], offs[i + 1]) for i, n in enumerate(["q", "kc", "vc", "ks", "vs", "kw", "vw", "gate", "qk", "v", "o", "if", "mg"])}
    col = lambda n: w_in[:, seg[n][0]:seg[n][1]]
    w_att = []
    for g in range(2):
        parts = [col("q")[:, g * 256:(g + 1) * 256]]
        for n in ["ks", "kw", "kc", "vc", "vs", "vw"]:
            parts.append(col(n)[:, g * 64:(g + 1) * 64])
        parts.append(col("gate")[:, g * 12:(g + 1) * 12])
        w_att.append(np.concatenate(parts, axis=1))
    w_att = np.ascontiguousarray(np.stack(w_att))
    shared = {
        "invf": np.ascontiguousarray(np.broadcast_to(_INV[None, :], (128, 8))),
        "gvec": np.ascontiguousarray(np.stack([f(inputs["g_mix"])[0], f(inputs["g_ffn"])[0], f(inputs["g_ple"])[0], f(inputs["g_final"])])),
        "w_att": w_att,
        "w_qk": np.ascontiguousarray(col("qk")),
        "w_vo": np.ascontiguousarray(np.concatenate([col("v"), col("o")], axis=1)),
        "w_if": np.ascontiguousarray(col("if")),
        "w_mg": np.ascontiguousarray(col("mg")),
        "b_if": f(inputs["b_if"]).reshape(1, 8),
        "w_c1": np.ascontiguousarray(np.stack([f(inputs["w_ck1"])[0], f(inputs["w_cv1"])[0]])),
        "w_c2": np.ascontiguousarray(np.stack([f(inputs["w_ck2"])[0], f(inputs["w_cv2"])[0]])),
        "pe_c": np.ascontiguousarray(np.stack([f(inputs["pe_ck"])[0], f(inputs["pe_cv"])[0]])),
        "wc": np.ascontiguousarray(f(inputs["w_conv"])[0].reshape(4, 8, 128).transpose(2, 1, 0)),
        "bc": np.ascontiguousarray(f(inputs["b_conv"])[0].reshape(8, 128).T),
        "g_hn": f(inputs["g_hn"]).reshape(1, 512),
        "w_pa": f(inputs["w_pa"])[0], "w_pb": f(inputs["w_pb"])[0], "w_out": f(inputs["w_out"])[0],
        "w_r": np.ascontiguousarray(np.concatenate([f(inputs["w_rg"])[0], f(inputs["w_re"])[0]], axis=1)),
        "b_r": np.ascontiguousarray(np.concatenate([f(inputs["b_rg"])[0], f(inputs["b_re"])[0]])[None, :]),
        "w_e13": f(inputs["w_e13"])[0], "w_e2": f(inputs["w_e2"])[0],
        "w_pg": f(inputs["w_pg"])[0], "w_pp": f(inputs["w_pp"])[0],
    }
    in_maps = []
    for core in range(8):
        b, half = core // 2, core % 2
        if half == 1:
            xe_ = x[b]
            pos_ = positions[b]
        else:
            xe_ = np.concatenate([np.zeros((S_OWN, D), np.float32), x[b, :S_OWN]], axis=0)
            pos_ = np.concatenate([np.zeros(S_OWN, np.int32), positions[b, :S_OWN]])
        m = dict(shared)
        m["xe"] = np.ascontiguousarray(xe_)
        m["pos"] = np.ascontiguousarray(pos_.reshape(NT_EXT, 128).T)
        m["pl"] = np.ascontiguousarray(p[0, b, half * S_OWN:(half + 1) * S_OWN])
        m["hv"] = np.full((128, 1), float(half), np.float32)
        in_maps.append(m)
    return in_maps


def kernel(**inputs):
    nc, _ = build_program()
    in_maps = make_in_maps(inputs)
    res = run_bass_kernel_spmd(nc, in_maps, core_ids=list(range(8)))
    out = np.zeros((4, S_EXT, D), np.float32)
    for core in range(8):
        b, half = core // 2, core % 2
        out[b, half * S_OWN:(half + 1) * S_OWN] = res.results[core]["out"]
    return out
```

```python
import numpy as np
import concourse.bass as bass
import concourse.mybir as mybir
from concourse.bass_utils import run_bass_kernel_spmd
from contextlib import ExitStack

F32 = mybir.dt.float32
BF16 = mybir.dt.bfloat16
I32 = mybir.dt.int32
AF = mybir.ActivationFunctionType
ALU = mybir.AluOpType
AX = mybir.AxisListType

D = 1024
S_OWN = 2048
S_EXT = 4096
NT_OWN = 16
NT_EXT = 32
EPS = 1e-6
NEGB = -30000.0
DBG = []


class Buf:
    __slots__ = ("t", "lw", "rd", "name", "excl")

    def __init__(self, t, name=""):
        self.t = t
        self.excl = False
        self.lw = None
        self.rd = {}
        self.name = name

    def __getitem__(self, k):
        return self.t[k]


class FW:
    NDMA = 24

    def __init__(self, nc, es):
        self.nc = nc
        self.es = es
        self.eng = {"pe": nc.tensor, "act": nc.scalar, "dve": nc.vector, "pool": nc.gpsimd, "sp": nc.sync}
        self.sem = {k: es.enter_context(nc.semaphore("s_" + k)) for k in self.eng}
        self.cnt = {k: 0 for k in self.eng}
        self.known = {k: {} for k in self.eng}
        self.dsem = [es.enter_context(nc.semaphore(f"s_dma{i}")) for i in range(self.NDMA)]
        self.dval = [0] * self.NDMA
        self.dnext = 0
        self.nbuf = 0
        self.out_waits = []
        self.stopped = False

    def sb(self, shape, dt, name=None, es=None):
        self.nbuf += 1
        name = f"sb{self.nbuf}_" + (name or "t")
        return Buf((es or self.es).enter_context(self.nc.sbuf_tensor(name, list(shape), dt)), name)

    def ps(self, shape, dt, name=None):
        self.nbuf += 1
        name = name or f"ps{self.nbuf}"
        b = Buf(self.es.enter_context(self.nc.psum_tensor(name, list(shape), dt)), name)
        b.excl = True
        return b

    def _wait(self, e, src, idx):
        if self.stopped:
            return
        kn = self.known[e]
        if kn.get(src, 0) >= idx:
            return
        s = self.dsem[src[1]] if isinstance(src, tuple) else self.sem[src]
        self.eng[e].wait_ge(s, idx)
        kn[src] = idx

    def _deps(self, e, reads, writes):
        for b in reads:
            if b.lw is not None:
                self._wait(e, b.lw[0], b.lw[1])
            if b.excl:
                for src, idx in b.rd.items():
                    if src != e:
                        self._wait(e, src, idx)
        for b in writes:
            if b.lw is not None and b.lw[0] != e:
                self._wait(e, b.lw[0], b.lw[1])
            for src, idx in b.rd.items():
                if src != e:
                    self._wait(e, src, idx)

    def op(self, e, fn, reads=(), writes=()):
        if self.stopped:
            return None
        self._deps(e, reads, writes)
        inst = fn(self.eng[e])
        self.cnt[e] += 1
        c = self.cnt[e]
        inst.then_inc(self.sem[e], 1)
        for b in reads:
            if b.rd.get(e, 0) < c:
                b.rd[e] = c
        for b in writes:
            b.lw = (e, c)
            b.rd = {}
        return inst

    def dma(self, out, in_, reads=(), writes=(), q="sp", is_output=False):
        if self.stopped and not is_output:
            return None
        self._deps(q, reads, writes)
        slot = self.dnext
        self.dnext = (self.dnext + 1) % self.NDMA
        key = ("d", slot)
        if self.dval[slot] > 0:
            self._wait(q, key, self.dval[slot])
        inst = self.eng[q].dma_start(out=out, in_=in_)
        self.dval[slot] += 16
        inst.then_inc(self.dsem[slot], 16)
        v = self.dval[slot]
        for b in reads:
            if b.rd.get(key, 0) < v:
                b.rd[key] = v
        for b in writes:
            b.lw = (key, v)
            b.rd = {}
        if is_output:
            self.out_waits.append((key, v))
        return inst

    def barrier(self):
        for e in self.eng:
            for src in ("pe", "act", "dve", "pool"):
                if src != e and self.cnt[src] > 0:
                    self._wait(e, src, self.cnt[src])
            for slot in range(self.NDMA):
                if self.dval[slot] > 0:
                    self._wait(e, ("d", slot), self.dval[slot])

    def scope(self):
        fw = self

        class _Scope(ExitStack):
            def __exit__(self, *a):
                fw.barrier()
                return super().__exit__(*a)
        return _Scope()

    def finish(self):
        for key, v in self.out_waits:
            self._wait("sp", key, v)
        for k in ("pe", "act", "dve", "pool"):
            if self.cnt[k] > 0:
                self._wait("sp", k, self.cnt[k])


class _StopBuild(Exception):
    pass


def build_program(dbg=()):
    nc = bass.Bass("TRN2", target_bir_lowering=False)

    def din(name, shape, dt=F32):
        return nc.dram_tensor(name, list(shape), dt, kind="ExternalInput").ap()

    xe = din("xe", [S_EXT, D])
    pos_d = din("pos", [128, NT_EXT], I32)
    pl_d = din("pl", [S_OWN, 256])
    hv_d = din("hv", [128, 1])
    invf_d = din("invf", [128, 8])
    gvec_d = din("gvec", [4, D])
    w_att_d = din("w_att", [2, D, 652])
    w_qk_d = din("w_qk", [D, 1024])
    w_vo_d = din("w_vo", [D, 1024])
    w_if_d = din("w_if", [D, 8])
    w_mg_d = din("w_mg", [D, 2048])
    b_if_d = din("b_if", [1, 8])
    w_c1_d = din("w_c1", [2, 2048, 256])
    w_c2_d = din("w_c2", [2, 256, 64])
    pe_c_d = din("pe_c", [2, 32, 64])
    wc_d = din("wc", [128, 8, 4])
    bc_d = din("bc", [128, 8])
    g_hn_d = din("g_hn", [1, 512])
    w_pa_d = din("w_pa", [512, D])
    w_pb_d = din("w_pb", [512, D])
    w_out_d = din("w_out", [D, D])
    w_r_d = din("w_r", [D, 20])
    b_r_d = din("b_r", [1, 20])
    w_e13_d = din("w_e13", [16, D, 512])
    w_e2_d = din("w_e2", [16, 256, D])
    w_pg_d = din("w_pg", [D, D])
    w_pp_d = din("w_pp", [256, D])
    out_d = nc.dram_tensor("out", [S_OWN, D], F32, kind="ExternalOutput").ap()
    hT_d = nc.dram_tensor("hT_scr", [128, 8, S_EXT], BF16, kind="Internal").ap()
    dbg_out = {}

    def dbg_t(name, shape, dt=F32):
        dbg_out[name] = nc.dram_tensor("dbg_" + name, list(shape), dt, kind="ExternalOutput").ap()
        return dbg_out[name]

    with ExitStack() as es:
        fw = FW(nc, es)
        op = fw.op
        PS = [fw.ps([128, 512], F32, f"psb{i}") for i in range(8)]

        def psbf(i):
            return PS[i][:].bitcast(BF16)

        ones_f = fw.sb([128, 128], F32, "ones_f")
        op("pool", lambda e: e.memset(ones_f[:], 1.0), writes=[ones_f])
        idf = fw.sb([128, 128], F32, "idf")
        op("pool", lambda e: e.affine_select(out=idf[:], in_=ones_f[:], pattern=[[1, 128]], compare_op=ALU.is_equal,
                                             fill=0.0, base=0, channel_multiplier=-1), reads=[ones_f], writes=[idf])
        idb = fw.sb([128, 128], BF16, "idb")
        op("dve", lambda e: e.tensor_copy(out=idb[:], in_=idf[:]), reads=[idf], writes=[idb])
        U_f = fw.sb([128, 128], F32, "U_f")
        op("pool", lambda e: e.affine_select(out=U_f[:], in_=ones_f[:], pattern=[[1, 128]], compare_op=ALU.is_ge,
                                             fill=0.0, base=0, channel_multiplier=-1), reads=[ones_f], writes=[U_f])
        caus = fw.sb([128, 128], BF16, "caus")
        op("dve", lambda e: e.tensor_copy(out=caus[:], in_=U_f[:]), reads=[U_f], writes=[caus])
        wm0_f = fw.sb([128, 128], F32, "wm0_f")
        op("pool", lambda e: e.affine_select(out=wm0_f[:], in_=ones_f[:], pattern=[[-1, 128]], compare_op=ALU.is_ge,
                                             fill=0.0, base=-1, channel_multiplier=1), reads=[ones_f], writes=[wm0_f])
        wm0 = fw.sb([128, 128], BF16, "wm0")
        op("dve", lambda e: e.tensor_copy(out=wm0[:], in_=wm0_f[:]), reads=[wm0_f], writes=[wm0])
        c_eps = fw.sb([128, 1], F32, "c_eps")
        op("pool", lambda e: e.memset(c_eps[:], EPS), writes=[c_eps])
        c_one = fw.sb([128, 1], F32, "c_one")
        op("pool", lambda e: e.memset(c_one[:], 1.0), writes=[c_one])
        c_zero = fw.sb([128, 1], F32, "c_zero")
        op("pool", lambda e: e.memset(c_zero[:], 0.0), writes=[c_zero])
        acc_junk = fw.sb([128, 2], F32, "acc_junk")
        op("act", lambda e: e.activation(out=acc_junk[:, 0:1], in_=c_one[:], func=AF.Square, accum_out=acc_junk[:, 1:2]),
           reads=[c_one], writes=[acc_junk])
        hv = fw.sb([128, 1], F32, "hv")
        fw.dma(hv[:], hv_d[:, :], writes=[hv])
        hbias = fw.sb([128, 1], F32, "hbias")
        op("dve", lambda e: e.tensor_scalar(out=hbias[:], in0=hv[:], scalar1=-1.0, scalar2=-NEGB, op0=ALU.add, op1=ALU.mult),
           reads=[hv], writes=[hbias])
        gB = fw.sb([128, D], F32, "gB")

        def load_gain(i):
            fw.dma(gB[:], gvec_d[i:i + 1, :].to_broadcast([128, D]), writes=[gB])

        cs = fw.sb([128, NT_EXT, 8], F32, "cs")
        sn = fw.sb([128, NT_EXT, 8], F32, "sn")
        with fw.scope() as es1:
            posi = fw.sb([128, NT_EXT], I32, "posi", es1)
            posf = fw.sb([128, NT_EXT], F32, "posf", es1)
            invf = fw.sb([128, 8], F32, "invf", es1)
            ang = fw.sb([128, NT_EXT, 8], F32, "ang", es1)
            kf = fw.sb([128, NT_EXT, 8], F32, "kf", es1)
            ki = fw.sb([128, NT_EXT, 8], I32, "ki", es1)
            r1 = fw.sb([128, NT_EXT, 8], F32, "r1", es1)
            r2 = fw.sb([128, NT_EXT, 8], F32, "r2", es1)
            fw.dma(posi[:], pos_d[:, :], writes=[posi])
            fw.dma(invf[:], invf_d[:, :], writes=[invf])
            op("dve", lambda e: e.tensor_copy(out=posf[:], in_=posi[:]), reads=[posi], writes=[posf])
            op("dve", lambda e: e.tensor_tensor(out=ang[:], in0=posf[:].unsqueeze(2).to_broadcast([128, NT_EXT, 8]),
                                                in1=invf[:].unsqueeze(1).to_broadcast([128, NT_EXT, 8]), op=ALU.mult),
               reads=[posf, invf], writes=[ang])
            TWO_PI = 6.283185307179586
            C1 = 6.28125
            C2 = TWO_PI - C1
            PI_LO = 3.1415925
            op("dve", lambda e: e.tensor_scalar(out=kf[:], in0=ang[:], scalar1=1.0 / TWO_PI, scalar2=None, op0=ALU.mult),
               reads=[ang], writes=[kf])
            op("dve", lambda e: e.tensor_copy(out=ki[:], in_=kf[:]), reads=[kf], writes=[ki])
            op("dve", lambda e: e.tensor_copy(out=kf[:], in_=ki[:]), reads=[ki], writes=[kf])
            op("dve", lambda e: e.scalar_tensor_tensor(out=r1[:], in0=kf[:], scalar=-C1, in1=ang[:], op0=ALU.mult, op1=ALU.add),
               reads=[kf, ang], writes=[r1])
            op("dve", lambda e: e.scalar_tensor_tensor(out=r1[:], in0=kf[:], scalar=-C2, in1=r1[:], op0=ALU.mult, op1=ALU.add),
               reads=[kf, r1], writes=[r1])
            op("dve", lambda e: e.tensor_scalar(out=r1[:], in0=r1[:], scalar1=PI_LO, scalar2=-PI_LO, op0=ALU.min, op1=ALU.max),
               reads=[r1], writes=[r1])
            op("act", lambda e: e.activation(out=sn[:], in_=r1[:], func=AF.Sin), reads=[r1], writes=[sn])
            op("dve", lambda e: e.tensor_scalar(out=r2[:], in0=r1[:], scalar1=PI_LO / 2 + 0.0, scalar2=None, op0=ALU.add),
               reads=[r1], writes=[r2])
            op("dve", lambda e: e.tensor_scalar(out=kf[:], in0=r2[:], scalar1=PI_LO, scalar2=-TWO_PI, op0=ALU.is_gt, op1=ALU.mult),
               reads=[r2], writes=[kf])
            op("dve", lambda e: e.tensor_tensor(out=r2[:], in0=r2[:], in1=kf[:], op=ALU.add), reads=[r2, kf], writes=[r2])
            op("dve", lambda e: e.tensor_scalar(out=r2[:], in0=r2[:], scalar1=PI_LO, scalar2=-PI_LO, op0=ALU.min, op1=ALU.max),
               reads=[r2], writes=[r2])
            op("act", lambda e: e.activation(out=cs[:], in_=r2[:], func=AF.Sin), reads=[r2], writes=[cs])

        def rms_rstd(src, rstd, n, junk):
            ss = rstd["ss"]
            op("act", lambda e: e.activation(out=junk["ap"], in_=src["ap"], func=AF.Square, accum_out=ss[:]),
               reads=src["bufs"], writes=[junk["buf"], ss])
            op("act", lambda e: e.activation(out=ss[:], in_=ss[:], func=AF.Sqrt, bias=c_eps[:], scale=1.0 / n),
               reads=[ss, c_eps], writes=[ss])
            op("dve", lambda e: e.reciprocal(out=rstd["r"][:], in_=ss[:]), reads=[ss], writes=[rstd["r"]])

        load_gain(0)
        hT_tiles = [Buf(None, f"hT_tile{t}") for t in range(NT_EXT)]
        with fw.scope() as esA:
            xt = [fw.sb([128, D], F32, f"xtA{i}", esA) for i in range(6)]
            xn = [fw.sb([128, D], BF16, f"xnA{i}", esA) for i in range(3)]
            junk = fw.sb([128, D], BF16, "junkA", esA)
            hst = [fw.sb([128, 8, 128], BF16, f"hstA{i}", esA) for i in range(4)]
            ssA = [fw.sb([128, 1], F32, f"ssA{i}", esA) for i in range(3)]
            rrA = [fw.sb([128, 1], F32, f"rrA{i}", esA) for i in range(3)]
            def a_s1(t):
                x_ = xt[t % 6]
                if t == 0:
                    for tt in range(5):
                        fw.dma(xt[tt][:], xe[tt * 128:(tt + 1) * 128, :], writes=[xt[tt]])
                if t + 5 < NT_EXT:
                    fw.dma(xt[(t + 5) % 6][:], xe[(t + 5) * 128:(t + 6) * 128, :], writes=[xt[(t + 5) % 6]])
                rs = {"ss": ssA[t % 3], "r": rrA[t % 3]}
                rms_rstd({"ap": x_[:], "bufs": [x_]}, rs, D, {"ap": junk[:], "buf": junk})
                n_ = xn[t % 3]
                op("dve", lambda e: e.scalar_tensor_tensor(out=n_[:], in0=x_[:], scalar=rs["r"][:], in1=gB[:], op0=ALU.mult, op1=ALU.mult),
                   reads=[x_, rs["r"], gB], writes=[n_])

            def a_s2(t):
                n_ = xn[t % 3]
                pb = t % 2
                for k in range(8):
                    op("pe", lambda e: e.transpose(out=psbf(pb)[:, k * 128:(k + 1) * 128], in_=n_[:, k * 128:(k + 1) * 128], identity=idb[:]),
                       reads=[n_, idb], writes=[PS[pb]])
                h_ = hst[t % 4]
                op("act", lambda e: e.copy(out=h_[:], in_=psbf(pb).rearrange("p (k t) -> p k t", k=8)), reads=[PS[pb]], writes=[h_])
                fw.dma(hT_d[:, :, t * 128:(t + 1) * 128], h_[:], reads=[h_], writes=[hT_tiles[t]], q="pool")

            for t in range(NT_EXT + 1):
                if t < NT_EXT:
                    a_s1(t)
                if t >= 1:
                    a_s2(t - 1)

        if "cs" in dbg:
            o = dbg_t("cs", [128, NT_EXT, 8])
            fw.dma(o[:, :, :], cs[:], reads=[cs], is_output=True)
            o = dbg_t("sn", [128, NT_EXT, 8])
            fw.dma(o[:, :, :], sn[:], reads=[sn], is_output=True)

        def ckpt(name):
            if ("stop_" + name) in dbg:
                fw.stopped = True

        def body():
            def mm(bank, out_ap, lhsT, rhs, start, stop, reads):
                op("pe", lambda e: e.matmul(out_ap, lhsT, rhs, start=start, stop=stop), reads=reads, writes=[bank])

            YT = fw.sb([128, 8, S_OWN], BF16, "YT")
            esBc = fw.scope()
            esBc.__enter__()
            cmask = fw.sb([128, 2, S_OWN], BF16, "cmask", esBc)
            op("pool", lambda e: e.memset(cmask[:], 1.0), writes=[cmask])
            op("pool", lambda e: e.affine_select(out=cmask[:, 0, :], in_=cmask[:, 0, :], pattern=[[1, S_OWN]], compare_op=ALU.is_ge, fill=0.0,
                                                 base=2017, channel_multiplier=-16), reads=[cmask], writes=[cmask])
            op("pool", lambda e: e.affine_select(out=cmask[:, 1, :], in_=cmask[:, 1, :], pattern=[[1, S_OWN]], compare_op=ALU.is_ge, fill=0.0,
                                                 base=-31, channel_multiplier=-16), reads=[cmask], writes=[cmask])
            ovl = fw.sb([128, 2, 64], BF16, "ovl", esBc)
            op("pool", lambda e: e.memset(ovl[:], 1.0), writes=[ovl])
            for j in range(2):
                op("pool", lambda e: e.affine_select(out=ovl[:, j, :], in_=ovl[:, j, :], pattern=[[-4, 64]], compare_op=ALU.is_ge, fill=0.0,
                                                     base=128 * j + 1, channel_multiplier=1), reads=[ovl], writes=[ovl])
                op("pool", lambda e: e.affine_select(out=ovl[:, j, :], in_=ovl[:, j, :], pattern=[[4, 64]], compare_op=ALU.is_ge, fill=0.0,
                                                     base=3 - 128 * j, channel_multiplier=-1), reads=[ovl], writes=[ovl])
            maskadd = fw.sb([128, NT_OWN, 64], F32, "maskadd", esBc)
            Mb = fw.sb([128, 64], F32, "Mb", esBc)
            hm1 = fw.sb([128, 2], F32, "hm1", esBc)
            op("dve", lambda e: e.tensor_scalar(out=hm1[:, 0:1], in0=hv[:], scalar1=-1.0, scalar2=1e30, op0=ALU.add, op1=ALU.mult),
               reads=[hv], writes=[hm1])
            op("dve", lambda e: e.tensor_scalar(out=hm1[:, 1:2], in0=hv[:], scalar1=-1.0, scalar2=-1000.0, op0=ALU.add, op1=ALU.mult),
               reads=[hv, hm1], writes=[hm1])
            op("dve", lambda e: e.memset(Mb[:], 0.0), writes=[Mb])
            op("dve", lambda e: e.tensor_copy(out=Mb[:, 0:32], in_=hm1[:, 0:1].to_broadcast([128, 32])), reads=[hm1, Mb], writes=[Mb])
            op("dve", lambda e: e.scalar_tensor_tensor(out=Mb[:, 0:1], in0=hv[:], scalar=1000.0, in1=Mb[:, 0:1], op0=ALU.mult, op1=ALU.add),
               reads=[hv, Mb], writes=[Mb])
            op("dve", lambda e: e.tensor_copy(out=Mb[:, 32:33], in_=hm1[:, 1:2]), reads=[hm1, Mb], writes=[Mb])
            for c in range(NT_OWN):
                op("pool", lambda e: e.tensor_copy(out=maskadd[:, c, :], in_=Mb[:]), reads=[Mb, maskadd], writes=[maskadd])
                for hf in range(2):
                    lo = 32 + 2 * c + hf + 1
                    if lo < 64:
                        op("pool", lambda e: e.memset(maskadd[hf * 64:(hf + 1) * 64, c, lo:64], -1e30), reads=[maskadd], writes=[maskadd])
                    for col in (32 + 2 * c + hf, 32 + 2 * c + hf - 1):
                        op("pool", lambda e: e.tensor_scalar(out=maskadd[hf * 64:(hf + 1) * 64, c, col:col + 1],
                                                             in0=maskadd[hf * 64:(hf + 1) * 64, c, col:col + 1],
                                                             scalar1=1000.0, scalar2=None, op0=ALU.add), reads=[maskadd], writes=[maskadd])

            ckpt("consts")
            for g in range(2):
                with fw.scope() as esG:
                    qT = fw.sb([128, 4, S_OWN], BF16, f"qT{g}", esG)
                    kkT = fw.sb([128, 2, S_EXT], BF16, f"kkT{g}", esG)
                    op("pool", lambda e: e.memset(qT[64:128, :, :], 0.0), writes=[qT])
                    op("pool", lambda e: e.memset(kkT[64:128, 0, :], 1.0), writes=[kkT])
                    op("pool", lambda e: e.memset(kkT[64:128, 1, :], 0.0), writes=[kkT])
                    op("pool", lambda e: e.affine_select(out=kkT[64:128, 0, :], in_=kkT[64:128, 0, :], pattern=[[1, S_EXT]], compare_op=ALU.is_ge, fill=0.0,
                                                         base=0, channel_multiplier=-64), reads=[kkT], writes=[kkT])
                    op("pool", lambda e: e.affine_select(out=kkT[64:128, 0, :], in_=kkT[64:128, 0, :], pattern=[[-1, S_EXT]], compare_op=ALU.is_ge, fill=0.0,
                                                         base=63, channel_multiplier=64), reads=[kkT], writes=[kkT])
                    vv = fw.sb([128, NT_EXT, 2, 65], BF16, f"vv{g}", esG)
                    gsig = fw.sb([128, NT_OWN, 12], F32, f"gsig{g}", esG)
                    kcmpT = fw.sb([128, 256], BF16, f"kcmpT{g}", esG)
                    op("pool", lambda e: e.memset(kcmpT[64:128, :], 0.0), writes=[kcmpT])
                    vca = fw.sb([128, 2, 65], BF16, f"vca{g}", esG)
                    op("pool", lambda e: e.memset(vv[:, :, :, 64:65], 1.0), writes=[vv])
                    op("pool", lambda e: e.memset(vca[:, :, 64:65], 1.0), writes=[vca])
                    ckpt("B0a")
                    with fw.scope() as esC:
                        ccT = fw.sb([64, 2, S_EXT], BF16, f"ccT{g}", esC)
                        with fw.scope() as esB1:
                            w_att = fw.sb([128, 8, 652], BF16, f"w_att{g}", esB1)
                            fw.dma(w_att[:], w_att_d[g].rearrange("(k p) c -> p k c", p=128), writes=[w_att], q="pool")
                            ckpt("B0b")
                            hblk = [fw.sb([128, 8, 512], BF16, f"hblkB{g}{i}", esB1) for i in range(2)]
                            rp = [fw.sb([128, 8, 64], BF16, f"rp{g}{i}", esB1) for i in range(2)]
                            rpf = [fw.sb([128, 8, 64], F32, f"rpf{g}{i}", esB1) for i in range(2)]
                            ta = [fw.sb([128, 7, 8], F32, f"ropa{g}{i}", esB1) for i in range(2)]
                            tb_ = [fw.sb([128, 7, 8], F32, f"ropb{g}{i}", esB1) for i in range(2)]
                            tcx = [fw.sb([128, 7, 8], F32, f"ropc{g}{i}", esB1) for i in range(2)]
                            tdx = [fw.sb([128, 7, 8], F32, f"ropd{g}{i}", esB1) for i in range(2)]
                            def b1_front(t):
                                own = t >= NT_OWN
                                tq = t - NT_OWN
                                hb = hblk[(t // 4) % 2]
                                if t % 4 == 0:
                                    fw.dma(hb[:], hT_d[:, :, t * 128:(t + 4) * 128], reads=hT_tiles[t:t + 4], writes=[hb])
                                tl = t % 4
                                a0 = 0 if own else 256
                                nb = 140 if own else 128
                                bA = 2 + t % 2
                                bB = 4 + t % 2
                                for k in range(8):
                                    mm(PS[bA], PS[bA][:, a0:512], hb[:, k, tl * 128:(tl + 1) * 128], w_att[:, k, a0:512], k == 0, k == 7, [hb, w_att])
                                for k in range(8):
                                    mm(PS[bB], PS[bB][:, 0:nb], hb[:, k, tl * 128:(tl + 1) * 128], w_att[:, k, 512:512 + nb], k == 0, k == 7, [hb, w_att])
                                rp_ = rp[t % 2]
                                h0 = a0 // 64
                                nh = 7 - h0
                                rf = rpf[t % 2]
                                op("act", lambda e: e.copy(out=rf[:, h0:8, :], in_=PS[bA][:, a0:512].rearrange("p (h d) -> p h d", d=64)),
                                   reads=[PS[bA]], writes=[rf])
                                op("pool", lambda e: e.tensor_copy(out=rp_[:, h0:8, :], in_=rf[:, h0:8, :]), reads=[rf], writes=[rp_])
                                t1 = rf[:, h0:7, 0:8]
                                t2 = rf[:, h0:7, 8:16]
                                Cb = cs[:, t, :].unsqueeze(1).to_broadcast([128, nh, 8])
                                Sb_ = sn[:, t, :].unsqueeze(1).to_broadcast([128, nh, 8])
                                ta_, tb2 = ta[t % 2], tb_[t % 2]
                                tc_, td_ = tcx[t % 2], tdx[t % 2]
                                op("dve", lambda e: e.tensor_tensor(out=ta_[:, 0:nh, :], in0=t1, in1=Cb, op=ALU.mult), reads=[rf, cs], writes=[ta_])
                                op("dve", lambda e: e.tensor_tensor(out=tb2[:, 0:nh, :], in0=t2, in1=Sb_, op=ALU.mult), reads=[rf, sn], writes=[tb2])
                                op("dve", lambda e: e.tensor_tensor(out=tc_[:, 0:nh, :], in0=t2, in1=Cb, op=ALU.mult), reads=[rf, cs], writes=[tc_])
                                op("dve", lambda e: e.tensor_tensor(out=td_[:, 0:nh, :], in0=t1, in1=Sb_, op=ALU.mult), reads=[rf, sn], writes=[td_])
                                op("dve", lambda e: e.tensor_tensor(out=rp_[:, h0:7, 0:8], in0=ta_[:, 0:nh, :], in1=tb2[:, 0:nh, :], op=ALU.subtract),
                                   reads=[ta_, tb2, rp_], writes=[rp_])
                                op("dve", lambda e: e.tensor_tensor(out=rp_[:, h0:7, 8:16], in0=tc_[:, 0:nh, :], in1=td_[:, 0:nh, :], op=ALU.add),
                                   reads=[tc_, td_, rp_], writes=[rp_])
                                op("dve", lambda e: e.tensor_copy(out=vv[:, t, :, 0:64], in_=PS[bB][:, 0:128].rearrange("p (h d) -> p h d", d=64)),
                                   reads=[PS[bB]], writes=[vv])
                                if own:
                                    op("act", lambda e: e.activation(out=gsig[:, tq, :], in_=PS[bB][:, 128:140], func=AF.Sigmoid),
                                       reads=[PS[bB]], writes=[gsig])

                            def b1_back(t):
                                own = t >= NT_OWN
                                tq = t - NT_OWN
                                rp_ = rp[t % 2]
                                h0 = 0 if own else 4
                                bT = t % 2
                                psT = psbf(bT)
                                for j, hh in enumerate(range(h0, 8)):
                                    op("pe", lambda e: e.transpose(out=psT[0:64, j * 128:(j + 1) * 128], in_=rp_[:, hh, :], identity=idb[:]),
                                       reads=[rp_, idb], writes=[PS[bT]])
                                if own:
                                    op("act", lambda e: e.copy(out=qT[0:64, :, tq * 128:(tq + 1) * 128], in_=psT[0:64, 0:512].rearrange("p (h t) -> p h t", h=4)),
                                       reads=[PS[bT]], writes=[qT])
                                    o1 = 512
                                else:
                                    o1 = 0
                                op("act", lambda e: e.copy(out=kkT[0:64, :, t * 128:(t + 1) * 128], in_=psT[0:64, o1:o1 + 256].rearrange("p (h t) -> p h t", h=2)),
                                   reads=[PS[bT]], writes=[kkT])
                                op("act", lambda e: e.copy(out=ccT[:, :, t * 128:(t + 1) * 128], in_=psT[0:64, o1 + 256:o1 + 512].rearrange("p (h t) -> p h t", h=2)),
                                   reads=[PS[bT]], writes=[ccT])

                            for t in range(NT_EXT + 1):
                                if t < NT_EXT:
                                    b1_front(t)
                                if t >= 1:
                                    b1_back(t - 1)
                        ckpt("B1")
                        for i in range(2):
                            with fw.scope() as esB2:
                                w1 = fw.sb([64, 32, 256], BF16, f"w1_{g}{i}", esB2)
                                fw.dma(w1[:], w_c1_d[i].rearrange("(l d) h -> d l h", d=64), writes=[w1], q="pool")
                                w2 = fw.sb([128, 2, 64], BF16, f"w2_{g}{i}", esB2)
                                fw.dma(w2[:], w_c2_d[i].rearrange("(c p) d -> p c d", p=128), writes=[w2], q="pool")
                                pe_sb = fw.sb([32, 64], BF16, f"pe_{g}{i}", esB2)
                                fw.dma(pe_sb[:], pe_c_d[i], writes=[pe_sb], q="pool")
                                peT = fw.sb([64, 32], BF16, f"peT_{g}{i}", esB2)
                                op("pe", lambda e: e.transpose(out=psbf(6)[0:64, 0:32], in_=pe_sb[:, :], identity=idb[0:32, 0:32]),
                                   reads=[pe_sb, idb], writes=[PS[6]])
                                op("act", lambda e: e.copy(out=peT[:], in_=psbf(6)[0:64, 0:32]), reads=[PS[6]], writes=[peT])
                                for hc in range(2):
                                    for l in range(32):
                                        mm(PS[7], PS[7][:, hc:hc + 1], w1[:, l, hc * 128:(hc + 1) * 128], peT[:, l:l + 1], l == 0, l == 31, [w1, peT])
                                cbs = fw.sb([128, 2], F32, f"cbs_{g}{i}", esB2)
                                op("act", lambda e: e.copy(out=cbs[:], in_=PS[7][:, 0:2]), reads=[PS[7]], writes=[cbs])
                                G = fw.sb([128, 2, 256], BF16, f"G_{g}{i}", esB2)
                                op("pool", lambda e: e.memset(G[:, :, 255:256], 0.0), writes=[G])
                                u_ = fw.sb([128, 255], F32, f"u_{g}{i}", esB2)
                                u2 = fw.sb([128, 255], F32, f"u2_{g}{i}", esB2)
                                sg_ = fw.sb([128, 255], F32, f"sg_{g}{i}", esB2)
                                for hc in range(2):
                                    for l in range(32):
                                        mm(PS[hc], PS[hc][:, 0:255], w1[:, l, hc * 128:(hc + 1) * 128], ccT[:, i, l:l + 16 * 254 + 1:16], l == 0, l == 31, [w1, ccT])
                                    op("act", lambda e: e.activation(out=u_[:], in_=PS[hc][:, 0:255], func=AF.Identity, bias=cbs[:, hc:hc + 1]),
                                       reads=[PS[hc], cbs], writes=[u_])
                                    op("dve", lambda e: e.tensor_tensor(out=u2[:], in0=u_[:], in1=u_[:], op=ALU.mult), reads=[u_], writes=[u2])
                                    op("dve", lambda e: e.tensor_scalar(out=u2[:], in0=u2[:], scalar1=0.044715, scalar2=1.0, op0=ALU.mult, op1=ALU.add),
                                       reads=[u2], writes=[u2])
                                    op("dve", lambda e: e.tensor_tensor(out=u2[:], in0=u2[:], in1=u_[:], op=ALU.mult), reads=[u2, u_], writes=[u2])
                                    op("act", lambda e: e.activation(out=sg_[:], in_=u2[:], func=AF.Sigmoid, scale=1.5957691216057308),
                                       reads=[u2], writes=[sg_])
                                    op("dve", lambda e: e.tensor_tensor(out=G[:, hc, 0:255], in0=u_[:], in1=sg_[:], op=ALU.mult), reads=[u_, sg_], writes=[G])
                                if i == 0:
                                    for hc in range(2):
                                        mm(PS[6], PS[6][0:64, 0:256], w2[:, hc, :], G[:, hc, :], hc == 0, hc == 1, [w2, G])
                                    op("act", lambda e: e.copy(out=kcmpT[0:64, :], in_=PS[6][0:64, 0:256]), reads=[PS[6]], writes=[kcmpT])
                                else:
                                    for nch in range(2):
                                        for hc in range(2):
                                            mm(PS[6], PS[6][:, nch * 64:(nch + 1) * 64], G[:, hc, nch * 128:(nch + 1) * 128], w2[:, hc, :], hc == 0, hc == 1, [w2, G])
                                    op("act", lambda e: e.copy(out=vca[:, :, 0:64], in_=PS[6][:, 0:128].rearrange("p (n d) -> p n d", d=64)),
                                       reads=[PS[6]], writes=[vca])
                    if g == 0 and "B2dump" in dbg:
                        for nm, bf, shp in (("kkT", kkT, [64, 2, S_EXT]), ("qT", qT, [64, 4, S_OWN]), ("vv", vv, [128, NT_EXT, 2, 65]),
                                            ("kcmpT", kcmpT, [64, 256]), ("vca", vca, [128, 2, 65])):
                            o = dbg_t(nm, shp, BF16)
                            fw.dma(o, bf[0:shp[0]], reads=[bf], is_output=True)
                        o = dbg_t("gsig", [128, NT_OWN, 12])
                        fw.dma(o, gsig[:], reads=[gsig], is_output=True)
                    ckpt("B2")
                    with fw.scope() as esB3:
                        NP = 4
                        LA = 2
                        Pb = [fw.sb([128, 512], BF16, f"Pb{g}{i}", esB3) for i in range(NP)]
                        Sbank = [0, 1, 6, 7]
                        hbS = [fw.sb([128, 4, 132], F32, f"hbS{g}{r}", esB3) for r in range(3)]
                        ya = [fw.sb([128, 4, 64], F32, f"ya{g}{i}", esB3) for i in range(2)]
                        yat = [fw.sb([128, 4, 64], BF16, f"yat{g}{i}", esB3) for i in range(2)]
                        sms = [fw.sb([128, 16], F32, f"sm{g}{i}", esB3) for i in range(3)]
                        rdc = fw.sb([128, 4], F32, f"rdc{g}", esB3)
                        impv = fw.sb([128, 64], F32, f"impv{g}", esB3)
                        wk = fw.sb([128, 64], F32, f"wk{g}", esB3)
                        m8a = fw.sb([128, 8], F32, f"m8a{g}", esB3)
                        m8b = fw.sb([128, 8], F32, f"m8b{g}", esB3)
                        negm2 = fw.sb([128, 128], BF16, f"negm{g}", esB3)
                        op("pool", lambda e: e.memset(negm2[:, 0:64], 0.0), writes=[negm2])
                        rot = [0]
                        REG = {0: (0, 129), 1: (129, 65), 2: (194, 65)}

                        def score(c, lhsT, lreads, extra, bias, mask):
                            r = rot[0] % NP
                            rot[0] += 1
                            sb_i = Sbank[r]
                            P = Pb[r]
                            qrhs = qT[:, :, c * 128:(c + 1) * 128]
                            S3 = PS[sb_i][:, :].rearrange("p (h q) -> p h q", h=4)
                            mm(PS[sb_i], S3, lhsT, qrhs, True, True, lreads + [qT])
                            op("act", lambda e: e.activation(out=P[:], in_=PS[sb_i][:, :], func=AF.Exp, bias=bias[:], scale=0.125),
                               reads=[PS[sb_i], bias], writes=[P])
                            if mask is not None:
                                op("dve", lambda e: e.tensor_tensor(out=P[:].rearrange("p (h q) -> p h q", h=4), in0=P[:].rearrange("p (h q) -> p h q", h=4),
                                                                    in1=mask[0], op=ALU.mult), reads=[P, mask[1]], writes=[P])
                            return P

                        def pv(P, h, reg, vr, vreads, cc, n, first, last):
                            op("pe", lambda e: e.matmul(PS[2 + h][:, cc:cc + n], P[:, h * 128:(h + 1) * 128], vr, start=first, stop=last),
                               reads=[P] + vreads, writes=[PS[2 + h]])

                        def evac_all(c, reg, br, first, final, mid=None):
                            col0, n = REG[reg]
                            hs = hbS[reg]
                            for h in range(4):
                                op("dve", lambda e: e.tensor_copy(out=hs[:, h, 0:n], in_=PS[2 + h][:, col0:col0 + n]), reads=[PS[2 + h]], writes=[hs])
                            sm = sms[reg]
                            yac = ya[c % 2]
                            dn = sm[:, 0:4]
                            rd = sm[:, 4:8] if br != 0 else rdc[:, 0:4]
                            rdb = sm if br != 0 else rdc
                            cf = sm[:, 8:12]
                            op("dve", lambda e: e.tensor_scalar(out=dn.unsqueeze(2), in0=hs[:, :, 64:65], scalar1=1e-30, scalar2=None, op0=ALU.max),
                               reads=[hs], writes=[sm])
                            op("dve", lambda e: e.reciprocal(out=rd, in_=dn), reads=[sm], writes=[rdb])
                            if mid is not None:
                                mid()
                            op("dve", lambda e: e.tensor_tensor(out=cf.unsqueeze(2), in0=rd.unsqueeze(2),
                                                                in1=gsig[:, c, :].rearrange("p (h b) -> p h b", b=3)[:, :, br:br + 1], op=ALU.mult),
                               reads=[sm, rdb, gsig], writes=[sm])
                            cfb = cf.unsqueeze(2).to_broadcast([128, 4, 64])
                            if first:
                                op("dve", lambda e: e.tensor_tensor(out=yac[:], in0=hs[:, :, 0:64], in1=cfb, op=ALU.mult), reads=[hs, sm], writes=[yac])
                            else:
                                op("dve", lambda e: e.tensor_tensor(out=hs[:, :, 0:64], in0=hs[:, :, 0:64], in1=cfb, op=ALU.mult), reads=[hs, sm], writes=[hs])
                                dst = yat[c % 2] if final else yac
                                op("dve", lambda e: e.tensor_tensor(out=dst[:], in0=hs[:, :, 0:64], in1=yac[:], op=ALU.add), reads=[hs, yac], writes=[dst])

                        pend = []

                        def flush():
                            while pend:
                                pend.pop(0)()

                        def pipe(score_fn, pv_fn):
                            P = score_fn()
                            while len(pend) >= LA:
                                pend.pop(0)()
                            pend.append(lambda: pv_fn(P))

                        def tr_slot():
                            r = rot[0] % NP
                            rot[0] += 1
                            return Sbank[r]

                        def cmp_scores_pv(c):
                            Pc = []
                            for nch in range(2):
                                mk = cmask[:, nch, c * 128:(c + 1) * 128].unsqueeze(1).to_broadcast([128, 4, 128])
                                Pc.append(score(c, kcmpT[:, nch * 128:(nch + 1) * 128], [kcmpT], None, hbias if nch == 0 else c_zero, (mk, cmask)))
                            flush()
                            for h in range(4):
                                for nch in range(2):
                                    pv(Pc[nch], h, 0, vca[:, nch, :], [vca], 0, 65, nch == 0, nch == 1)
                                for nch in range(2):
                                    pv(Pc[nch], h, 0, ovl[:, nch, :], [ovl], 65, 64, nch == 0, nch == 1)

                        def cmp_evac_topk(c):
                            def topk_chain():
                                op("dve", lambda e: e.tensor_tensor(out=hbS[0][:, :, 65:129], in0=hbS[0][:, :, 65:129], in1=rdc[:, 0:4].unsqueeze(2).to_broadcast([128, 4, 64]), op=ALU.mult),
                                   reads=[hbS[0], rdc], writes=[hbS[0]])
                                op("dve", lambda e: e.tensor_reduce(out=impv[:], in_=hbS[0][:, :, 65:129].rearrange("p h s -> p s h"), axis=AX.X, op=ALU.add),
                                   reads=[hbS[0]], writes=[impv])
                                op("dve", lambda e: e.tensor_tensor(out=impv[:], in0=impv[:], in1=maskadd[:, c, :], op=ALU.add), reads=[impv, maskadd], writes=[impv])
                                op("dve", lambda e: e.max(out=m8a[:], in_=impv[:]), reads=[impv], writes=[m8a])
                                op("dve", lambda e: e.match_replace(out=wk[:], in_to_replace=m8a[:], in_values=impv[:], imm_value=-3.0e38),
                                   reads=[impv, m8a], writes=[wk])
                                op("dve", lambda e: e.max(out=m8b[:], in_=wk[:]), reads=[wk], writes=[m8b])
                                op("dve", lambda e: e.tensor_scalar(out=negm2[:, 64:128], in0=impv[:], scalar1=m8b[:, 7:8], scalar2=NEGB, op0=ALU.is_lt, op1=ALU.mult),
                                   reads=[impv, m8b, negm2], writes=[negm2])
                            evac_all(c, 0, 0, True, False, mid=topk_chain)

                        def negm_to_q(c):
                            bk = tr_slot()
                            op("pe", lambda e: e.transpose(out=psbf(bk)[:, 0:128], in_=negm2[:, :], identity=idb[:]), reads=[negm2, idb], writes=[PS[bk]])
                            op("act", lambda e: e.copy(out=qT[64:128, :, c * 128:(c + 1) * 128], in_=psbf(bk)[64:128, 0:128].unsqueeze(1).to_broadcast([64, 4, 128])),
                               reads=[PS[bk]], writes=[qT])

                        def finish_tile(cp):
                            evac_all(cp, 1, 1, False, True)
                            bk = tr_slot()
                            for j in range(2):
                                op("pe", lambda e: e.transpose(out=psbf(bk)[:, j * 128:(j + 1) * 128],
                                                               in_=yat[cp % 2][:, 2 * j:2 * j + 2, :].rearrange("p h d -> p (h d)"), identity=idb[:]),
                                   reads=[yat[cp % 2], idb], writes=[PS[bk]])
                            op("act", lambda e: e.copy(out=YT[:, 2 * g:2 * g + 2, cp * 128:(cp + 1) * 128],
                                                       in_=psbf(bk)[:, 0:256].rearrange("p (j t) -> p j t", j=2)), reads=[PS[bk]], writes=[YT])

                        cmp_scores_pv(0)
                        cmp_evac_topk(0)
                        negm_to_q(0)
                        for c in range(NT_OWN):
                            for j in range(5):
                                ch = NT_OWN + c - 4 + j
                                mk = None
                                if j == 0:
                                    mk = (wm0[:].unsqueeze(1).to_broadcast([128, 4, 128]), wm0)
                                elif j == 4:
                                    mk = (caus[:].unsqueeze(1).to_broadcast([128, 4, 128]), caus)

                                def sfn(ch=ch, mk=mk):
                                    return score(c, kkT[:, 1, ch * 128:(ch + 1) * 128], [kkT], None, hbias if ch < NT_OWN else c_zero, mk)

                                def pfn(P, ch=ch, j=j):
                                    for h in range(4):
                                        pv(P, h, 2, vv[:, ch, 1, :], [vv], 194, 65, j == 0, j == 4)
                                pipe(sfn, pfn)
                                if j == 1 and c > 0:
                                    finish_tile(c - 1)
                            flush()
                            evac_all(c, 2, 2, False, False)
                            if c + 1 < NT_OWN:
                                cmp_scores_pv(c + 1)
                            chs = list(range(NT_OWN)) + [NT_OWN + j for j in range(c + 1)]
                            for i, ch in enumerate(chs):
                                mk = None
                                if ch == NT_OWN + c:
                                    mk = (caus[:].unsqueeze(1).to_broadcast([128, 4, 128]), caus)

                                def sfn(ch=ch, mk=mk):
                                    return score(c, kkT[:, 0, ch * 128:(ch + 1) * 128], [kkT], None, hbias if ch < NT_OWN else c_zero, mk)

                                def pfn(P, ch=ch, i=i, n=len(chs)):
                                    for h in range(4):
                                        pv(P, h, 1, vv[:, ch, 0, :], [vv], 129, 65, i == 0, i == n - 1)
                                pipe(sfn, pfn)
                                if c + 1 < NT_OWN:
                                    if i == 2:
                                        cmp_evac_topk(c + 1)
                                    elif i == 10:
                                        negm_to_q(c + 1)
                        flush()
                        finish_tile(NT_OWN - 1)
            esBc.__exit__(None, None, None)
            ckpt("B")
            with fw.scope() as esCg:
                ee = fw.sb([128, NT_EXT, 4], F32, "ee", esCg)
                ff = fw.sb([128, NT_EXT, 4], F32, "ff", esCg)
                fl = fw.sb([128, NT_EXT, 4], F32, "fl", esCg)
                ghn = fw.sb([128, 512], F32, "ghn", esCg)
                fw.dma(ghn[:], g_hn_d[0:1, :].to_broadcast([128, 512]), writes=[ghn])
                wcs = fw.sb([128, 8, 4], F32, "wcs", esCg)
                fw.dma(wcs[:], wc_d[:, :, :], writes=[wcs])
                bcs = fw.sb([128, 8], F32, "bcs", esCg)
                fw.dma(bcs[:], bc_d[:, :], writes=[bcs])
                with fw.scope() as esg:
                    w_if = fw.sb([128, 8, 8], BF16, "w_if", esg)
                    fw.dma(w_if[:], w_if_d.rearrange("(k p) c -> p k c", p=128), writes=[w_if], q="pool")
                    bif = fw.sb([128, 8], F32, "bif", esg)
                    fw.dma(bif[:], b_if_d[0:1, :].to_broadcast([128, 8]), writes=[bif])
                    hblk = [fw.sb([128, 8, 512], BF16, f"hblkG{i}", esg) for i in range(2)]
                    ifp = fw.sb([128, NT_EXT, 8], F32, "ifp", esg)
                    l1 = fw.sb([128, NT_EXT, 4], F32, "l1", esg)
                    tmpg = fw.sb([128, NT_EXT, 4], F32, "tmpg", esg)
                    for t in range(NT_EXT):
                        hb = hblk[(t // 4) % 2]
                        if t % 4 == 0:
                            fw.dma(hb[:], hT_d[:, :, t * 128:(t + 4) * 128], reads=hT_tiles[t:t + 4], writes=[hb])
                        tl = t % 4
                        for k in range(8):
                            mm(PS[0], PS[0][:, t * 8:(t + 1) * 8], hb[:, k, tl * 128:(tl + 1) * 128], w_if[:, k, :], k == 0, k == 7, [hb, w_if])
                    op("act", lambda e: e.copy(out=ifp[:], in_=PS[0][:, 0:256].rearrange("p (t c) -> p t c", c=8)), reads=[PS[0]], writes=[ifp])
                    op("dve", lambda e: e.tensor_tensor(out=ifp[:], in0=ifp[:], in1=bif[:].unsqueeze(1).to_broadcast([128, NT_EXT, 8]), op=ALU.add),
                       reads=[ifp, bif], writes=[ifp])
                    op("act", lambda e: e.activation(out=l1[:], in_=ifp[:, :, 4:8], func=AF.Exp, scale=-1.0), reads=[ifp], writes=[l1])
                    op("act", lambda e: e.activation(out=l1[:], in_=l1[:], func=AF.Ln, bias=c_one[:]), reads=[l1, c_one], writes=[l1])
                    l1f = l1[:].rearrange("p t c -> p (t c)")
                    mm(PS[1], PS[1][:, 0:128], U_f[:], l1f, True, True, [U_f, l1])
                    mm(PS[1], PS[1][:, 128:256], ones_f[:], l1f, True, True, [ones_f, l1])
                    op("act", lambda e: e.copy(out=tmpg[:], in_=PS[1][:, 0:128].rearrange("p (t c) -> p t c", c=4)), reads=[PS[1]], writes=[tmpg])
                    op("act", lambda e: e.activation(out=ff[:], in_=tmpg[:], func=AF.Exp, scale=-1.0), reads=[tmpg], writes=[ff])
                    op("act", lambda e: e.activation(out=fl[:], in_=PS[1][:, 128:256].rearrange("p (t c) -> p t c", c=4), func=AF.Exp, scale=-1.0),
                       reads=[PS[1]], writes=[fl])
                    op("dve", lambda e: e.tensor_tensor(out=tmpg[:], in0=tmpg[:], in1=ifp[:, :, 0:4], op=ALU.add), reads=[tmpg, ifp], writes=[tmpg])
                    op("act", lambda e: e.activation(out=ee[:], in_=tmpg[:], func=AF.Exp), reads=[tmpg], writes=[ee])
                    op("dve", lambda e: e.tensor_scalar(out=ee[:, 0:NT_OWN, :], in0=ee[:, 0:NT_OWN, :], scalar1=hv[:, 0:1], scalar2=None, op0=ALU.mult),
                       reads=[ee, hv], writes=[ee])
                ckpt("Cg")
                qTb = fw.sb([128, 4, S_OWN], BF16, "qTb", esCg)
                kTb = fw.sb([128, 4, S_EXT], BF16, "kTb", esCg)
                vaug = fw.sb([128, NT_EXT, 4, 129], BF16, "vaug", esCg)
                osig = fw.sb([128, NT_OWN, 512], BF16, "osig", esCg)
                op("pool", lambda e: e.memset(vaug[:, :, :, 128:129], 1.0), writes=[vaug])
                for hp in range(2):
                    with fw.scope() as esC1:
                        wq = fw.sb([128, 8, 256], BF16, f"wq{hp}", esC1)
                        wk = fw.sb([128, 8, 256], BF16, f"wk{hp}", esC1)
                        wvo = fw.sb([128, 8, 512], BF16, f"wvo{hp}", esC1)
                        fw.dma(wq[:], w_qk_d[:, hp * 256:(hp + 1) * 256].rearrange("(k p) c -> p k c", p=128), writes=[wq], q="pool")
                        fw.dma(wk[:], w_qk_d[:, 512 + hp * 256:512 + (hp + 1) * 256].rearrange("(k p) c -> p k c", p=128), writes=[wk], q="pool")
                        fw.dma(wvo[:, :, 0:256], w_vo_d[:, hp * 256:(hp + 1) * 256].rearrange("(k p) c -> p k c", p=128), writes=[wvo], q="pool")
                        fw.dma(wvo[:, :, 256:512], w_vo_d[:, 512 + hp * 256:512 + (hp + 1) * 256].rearrange("(k p) c -> p k c", p=128), writes=[wvo], q="pool")
                        hblk = [fw.sb([128, 8, 512], BF16, f"hblkC{hp}{i}", esC1) for i in range(2)]
                        uk = [fw.sb([128, 4 + S_EXT], BF16, f"uk{hp}{i}", esC1) for i in range(2)]
                        uq = [fw.sb([128, 4 + 2560], BF16, f"uq{hp}{i}", esC1) for i in range(2)]
                        ycv = [fw.sb([128, 512], F32, f"ycv{hp}{i}", esC1) for i in range(2)]
                        sgm = [fw.sb([128, 512], F32, f"sgm{hp}{i}", esC1) for i in range(2)]
                        for hh in range(2):
                            op("pool", lambda e: e.memset(uk[hh][:, 0:4], 0.0), writes=[uk[hh]])
                            op("pool", lambda e: e.memset(uq[hh][:, 0:4], 0.0), writes=[uq[hh]])
                        ukB = [[Buf(None, f"ukB{hp}{hh}{i}") for i in range(8)] for hh in range(2)]
                        uqB = [[Buf(None, f"uqB{hp}{hh}{i}") for i in range(5)] for hh in range(2)]
                        pi_ = [0]

                        def conv_piece(hh, typ, pc):
                            H = 2 * hp + hh
                            ci = typ * 4 + H
                            u = uq[hh] if typ == 0 else uk[hh]
                            if typ == 0:
                                off = 4 + 512 + pc * 512
                                ur = [uqB[hh][pc + 1], uqB[hh][pc]]
                            else:
                                off = 4 + pc * 512
                                ur = [ukB[hh][pc], ukB[hh][pc - 1] if pc > 0 else uk[hh]]
                            y_ = ycv[pi_[0] % 2]
                            s_ = sgm[pi_[0] % 2]
                            pi_[0] += 1
                            op("dve", lambda e: e.tensor_scalar(out=y_[:], in0=u[:, off - 3:off - 3 + 512], scalar1=wcs[:, ci, 0:1], scalar2=bcs[:, ci:ci + 1],
                                                                op0=ALU.mult, op1=ALU.add), reads=ur + [wcs, bcs], writes=[y_])
                            for j in range(1, 4):
                                op("dve", lambda e: e.scalar_tensor_tensor(out=y_[:], in0=u[:, off - 3 + j:off - 3 + j + 512], scalar=wcs[:, ci, j:j + 1], in1=y_[:],
                                                                           op0=ALU.mult, op1=ALU.add), reads=ur + [wcs, y_], writes=[y_])
                            if typ == 0:
                                op("act", lambda e: e.activation(out=qTb[:, H, pc * 512:(pc + 1) * 512], in_=y_[:], func=AF.Silu), reads=[y_], writes=[qTb])
                            else:
                                op("act", lambda e: e.activation(out=s_[:], in_=y_[:], func=AF.Sigmoid), reads=[y_], writes=[s_])
                                op("dve", lambda e: e.scalar_tensor_tensor(out=kTb[:, H, pc * 512:(pc + 1) * 512], in0=y_[:], scalar=128.0 ** -0.5, in1=s_[:],
                                                                           op0=ALU.mult, op1=ALU.mult), reads=[y_, s_], writes=[kTb])

                        def conv_for_block(bdone):
                            for hh in range(2):
                                conv_piece(hh, 1, bdone)
                                if bdone >= 4:
                                    conv_piece(hh, 0, bdone - 4)

                        for blk in range(8):
                            hb = hblk[blk % 2]
                            fw.dma(hb[:], hT_d[:, :, blk * 512:(blk + 1) * 512], reads=hT_tiles[4 * blk:4 * blk + 4], writes=[hb])
                            for hh in range(2):
                                for k in range(8):
                                    mm(PS[hh], PS[hh][:, :], wk[:, k, hh * 128:(hh + 1) * 128], hb[:, k, :], k == 0, k == 7, [wk, hb])
                                op("act", lambda e: e.copy(out=uk[hh][:, 4 + blk * 512:4 + (blk + 1) * 512], in_=PS[hh][:, :]), reads=[PS[hh]], writes=[ukB[hh][blk]])
                            if blk >= 3:
                                for hh in range(2):
                                    for k in range(8):
                                        mm(PS[2 + hh], PS[2 + hh][:, :], wq[:, k, hh * 128:(hh + 1) * 128], hb[:, k, :], k == 0, k == 7, [wq, hb])
                                    op("act", lambda e: e.copy(out=uq[hh][:, 4 + (blk - 3) * 512:4 + (blk - 2) * 512], in_=PS[2 + hh][:, :]),
                                       reads=[PS[2 + hh]], writes=[uqB[hh][blk - 3]])
                            for tl in range(4):
                                t = blk * 4 + tl
                                bv = 4 + tl
                                nvo = 512 if blk >= 4 else 256
                                for k in range(8):
                                    mm(PS[bv], PS[bv][:, 0:nvo], hb[:, k, tl * 128:(tl + 1) * 128], wvo[:, k, 0:nvo], k == 0, k == 7, [wvo, hb])
                                op("dve", lambda e: e.tensor_copy(out=vaug[:, t, 2 * hp:2 * hp + 2, 0:128], in_=PS[bv][:, 0:256].rearrange("p (h d) -> p h d", d=128)),
                                   reads=[PS[bv]], writes=[vaug])
                                if blk >= 4:
                                    op("act", lambda e: e.activation(out=osig[:, t - NT_OWN, hp * 256:(hp + 1) * 256], in_=PS[bv][:, 256:512], func=AF.Sigmoid),
                                       reads=[PS[bv]], writes=[osig])
                            if blk >= 1:
                                conv_for_block(blk - 1)
                        conv_for_block(7)
                ckpt("C1")
                with fw.scope() as esC3:
                    ktokR = [fw.sb([128, 4, 128], BF16, f"ktokR{i}", esC3) for i in range(3)]
                    CTall = fw.sb([128, NT_OWN, 4, 129], BF16, "CTall", esC3)
                    Xs = [fw.sb([128, 129], F32, f"Xs{H}", esC3) for H in range(4)]
                    Sm = [[fw.sb([128, 128], BF16, f"Sm{H}{i}", esC3) for i in range(2)] for H in range(4)]
                    hm_ = [fw.sb([128, 128], F32, f"hm{H}", esC3) for H in range(4)]
                    yb_ = [fw.sb([128, 128], BF16, f"yb{H}", esC3) for H in range(4)]
                    jk = [fw.sb([128, 128], BF16, f"jk{H}", esC3) for H in range(4)]
                    smc = [fw.sb([128, 8], F32, f"smc{H}", esC3) for H in range(4)]
                    for H in range(4):
                        op("dve", lambda e: e.tensor_tensor(out=vaug[:, :, H, :], in0=vaug[:, :, H, :],
                                                            in1=ee[:, :, H:H + 1].to_broadcast([128, NT_EXT, 129]), op=ALU.mult), reads=[vaug, ee], writes=[vaug])

                    def k_tr(t):
                        bk = t % 2
                        for H in range(4):
                            op("pe", lambda e: e.transpose(out=psbf(bk)[:, H * 128:(H + 1) * 128], in_=kTb[:, H, t * 128:(t + 1) * 128], identity=idb[:]),
                               reads=[kTb, idb], writes=[PS[bk]])
                        op("act", lambda e: e.copy(out=ktokR[t % 3][:], in_=psbf(bk)[:, 0:512].rearrange("p (h d) -> p h d", d=128)), reads=[PS[bk]], writes=[ktokR[t % 3]])

                    k_tr(0)
                    for t in range(NT_EXT - 1):
                        if t + 1 < NT_EXT - 1:
                            k_tr(t + 1)
                        for H in range(4):
                            bU = 2 + H
                            mm(PS[bU], PS[bU][:, 0:129], ktokR[t % 3][:, H, :], vaug[:, t, H, :], True, True, [ktokR[t % 3], vaug])
                            if t == 0:
                                op("dve", lambda e: e.tensor_copy(out=Xs[H][:], in_=PS[bU][:, 0:129]), reads=[PS[bU]], writes=[Xs[H]])
                            else:
                                op("dve", lambda e: e.scalar_tensor_tensor(out=Xs[H][:], in0=Xs[H][:], scalar=fl[:, t - 1, H:H + 1], in1=PS[bU][:, 0:129],
                                                                           op0=ALU.mult, op1=ALU.add), reads=[Xs[H], fl, PS[bU]], writes=[Xs[H]])
                            if t + 1 >= NT_OWN:
                                op("act", lambda e: e.activation(out=CTall[:, t + 1 - NT_OWN, H, :], in_=Xs[H][:], func=AF.Copy, scale=fl[:, t, H:H + 1]),
                                   reads=[Xs[H], fl], writes=[CTall])
                    sc4 = fw.sb([128, 4, 8], F32, "sc4", esC3)

                    def stA(t):
                        tq = t - NT_OWN
                        for H in range(4):
                            sm_ = Sm[H][tq % 2]
                            mm(PS[H], PS[H][:, 0:128], kTb[:, H, t * 128:(t + 1) * 128], qTb[:, H, tq * 128:(tq + 1) * 128], True, True, [kTb, qTb])
                            op("dve", lambda e: e.tensor_tensor(out=sm_[:], in0=PS[H][:, 0:128], in1=caus[:], op=ALU.mult), reads=[PS[H], caus], writes=[sm_])

                    def stRest(t):
                        tq = t - NT_OWN
                        for H in range(4):
                            sm_ = Sm[H][tq % 2]
                            bA = 4 + H
                            mm(PS[bA], PS[bA][:, 0:129], sm_[:], vaug[:, t, H, :], True, False, [sm_, vaug])
                            mm(PS[bA], PS[bA][:, 0:129], qTb[:, H, tq * 128:(tq + 1) * 128], CTall[:, tq, H, :], False, True, [qTb, CTall])
                        for H in range(4):
                            op("act", lambda e: e.activation(out=sc4[:, H, 6:7], in_=PS[4 + H][:, 128:129], func=AF.Abs, scale=ff[:, t, H:H + 1]),
                               reads=[PS[4 + H], ff], writes=[sc4])
                        op("dve", lambda e: e.tensor_scalar(out=sc4[:, :, 0:1], in0=sc4[:, :, 6:7], scalar1=1.0, scalar2=None, op0=ALU.max), reads=[sc4], writes=[sc4])
                        op("dve", lambda e: e.reciprocal(out=sc4[:, :, 1:2], in_=sc4[:, :, 0:1]), reads=[sc4], writes=[sc4])
                        op("dve", lambda e: e.tensor_tensor(out=sc4[:, :, 2:3], in0=sc4[:, :, 1:2], in1=ff[:, t, :].unsqueeze(2), op=ALU.mult), reads=[sc4, ff], writes=[sc4])
                        for H in range(4):
                            op("dve", lambda e: e.scalar_tensor_tensor(out=hm_[H][:], in0=PS[4 + H][:, 0:128], scalar=sc4[:, H, 2:3], in1=osig[:, tq, H * 128:(H + 1) * 128],
                                                                       op0=ALU.mult, op1=ALU.mult), reads=[PS[4 + H], sc4, osig], writes=[hm_[H]])
                        for H in range(4):
                            op("act", lambda e: e.activation(out=jk[H][:], in_=hm_[H][:], func=AF.Square, accum_out=sc4[:, H, 3:4]), reads=[hm_[H]], writes=[jk[H], sc4])
                        op("act", lambda e: e.activation(out=sc4[:, :, 4:5], in_=sc4[:, :, 3:4], func=AF.Sqrt, bias=c_eps[:], scale=1.0 / 128), reads=[sc4, c_eps], writes=[sc4])
                        op("dve", lambda e: e.reciprocal(out=sc4[:, :, 5:6], in_=sc4[:, :, 4:5]), reads=[sc4], writes=[sc4])
                        for H in range(4):
                            op("dve", lambda e: e.scalar_tensor_tensor(out=yb_[H][:], in0=hm_[H][:], scalar=sc4[:, H, 5:6], in1=ghn[:, H * 128:(H + 1) * 128],
                                                                       op0=ALU.mult, op1=ALU.mult), reads=[hm_[H], sc4, ghn], writes=[yb_[H]])
                        for H in range(4):
                            op("pe", lambda e: e.transpose(out=psbf(4 + H)[:, 512:640], in_=yb_[H][:], identity=idb[:]), reads=[yb_[H], idb], writes=[PS[4 + H]])
                        for H in range(4):
                            op("act", lambda e: e.copy(out=YT[:, 4 + H, tq * 128:(tq + 1) * 128], in_=psbf(4 + H)[:, 512:640]), reads=[PS[4 + H]], writes=[YT])

                    stA(NT_OWN)
                    for t in range(NT_OWN, NT_EXT):
                        if t + 1 < NT_EXT:
                            stA(t + 1)
                        stRest(t)
            ckpt("C")
            if "ybT" in dbg:
                o = dbg_t("ybT", [128, 4, S_OWN], BF16)
                fw.dma(o[:, :, :], YT[:, 4:8, :], reads=[YT], is_output=True)


            with fw.scope() as esD:
                x1 = fw.sb([128, NT_OWN, D], F32, "x1", esD)
                with fw.scope() as esD1:
                    mixT = fw.sb([128, 8, S_OWN], BF16, "mixT", esD1)
                    with fw.scope() as esD1a:
                        hTo = fw.sb([128, 8, S_OWN], BF16, "hTo", esD1a)
                        for tb in range(4):
                            fw.dma(hTo[:, :, tb * 512:(tb + 1) * 512], hT_d[:, :, S_OWN + tb * 512:S_OWN + (tb + 1) * 512],
                                   reads=hT_tiles[NT_OWN + 4 * tb:NT_OWN + 4 * tb + 4], writes=[hTo])
                        wga = [fw.sb([128, 8, 128], BF16, f"wga{i}", esD1a) for i in range(2)]
                        wgb = [fw.sb([128, 8, 128], BF16, f"wgb{i}", esD1a) for i in range(2)]
                        wpa = [fw.sb([128, 4, 128], BF16, f"wpa{i}", esD1a) for i in range(2)]
                        wpb = [fw.sb([128, 4, 128], BF16, f"wpb{i}", esD1a) for i in range(2)]
                        sga = [fw.sb([128, 512], BF16, f"sga{i}", esD1a) for i in range(2)]
                        sgb = [fw.sb([128, 512], BF16, f"sgb{i}", esD1a) for i in range(2)]
                        t1 = [fw.sb([128, 512], F32, f"t1_{i}", esD1a) for i in range(2)]
                        t2 = [fw.sb([128, 512], F32, f"t2_{i}", esD1a) for i in range(2)]
                        it = 0
                        for j in range(8):
                            w_ = j % 2
                            fw.dma(wga[w_][:], w_mg_d[:, j * 128:(j + 1) * 128].rearrange("(k p) c -> p k c", p=128), writes=[wga[w_]], q="pool")
                            fw.dma(wgb[w_][:], w_mg_d[:, 1024 + j * 128:1024 + (j + 1) * 128].rearrange("(k p) c -> p k c", p=128), writes=[wgb[w_]], q="pool")
                            fw.dma(wpa[w_][:], w_pa_d[:, j * 128:(j + 1) * 128].rearrange("(k p) c -> p k c", p=128), writes=[wpa[w_]], q="pool")
                            fw.dma(wpb[w_][:], w_pb_d[:, j * 128:(j + 1) * 128].rearrange("(k p) c -> p k c", p=128), writes=[wpb[w_]], q="pool")
                            for tb in range(4):
                                r = it % 2
                                it += 1
                                b0 = 4 * r
                                ts_ = slice(tb * 512, (tb + 1) * 512)
                                for k in range(8):
                                    mm(PS[b0], PS[b0][:, :], wga[w_][:, k, :], hTo[:, k, ts_], k == 0, k == 7, [wga[w_], hTo])
                                op("act", lambda e: e.activation(out=sga[r][:], in_=PS[b0][:, :], func=AF.Sigmoid), reads=[PS[b0]], writes=[sga[r]])
                                for k in range(8):
                                    mm(PS[b0 + 1], PS[b0 + 1][:, :], wgb[w_][:, k, :], hTo[:, k, ts_], k == 0, k == 7, [wgb[w_], hTo])
                                op("act", lambda e: e.activation(out=sgb[r][:], in_=PS[b0 + 1][:, :], func=AF.Sigmoid), reads=[PS[b0 + 1]], writes=[sgb[r]])
                                for k in range(4):
                                    mm(PS[b0 + 2], PS[b0 + 2][:, :], wpa[w_][:, k, :], YT[:, k, ts_], k == 0, k == 3, [wpa[w_], YT])
                                for k in range(4):
                                    mm(PS[b0 + 3], PS[b0 + 3][:, :], wpb[w_][:, k, :], YT[:, 4 + k, ts_], k == 0, k == 3, [wpb[w_], YT])
                                op("dve", lambda e: e.tensor_tensor(out=t1[r][:], in0=PS[b0 + 2][:, :], in1=sga[r][:], op=ALU.mult), reads=[PS[b0 + 2], sga[r]], writes=[t1[r]])
                                op("dve", lambda e: e.tensor_tensor(out=t2[r][:], in0=PS[b0 + 3][:, :], in1=sgb[r][:], op=ALU.mult), reads=[PS[b0 + 3], sgb[r]], writes=[t2[r]])
                                op("pool", lambda e: e.tensor_tensor(out=mixT[:, j, ts_], in0=t1[r][:], in1=t2[r][:], op=ALU.add), reads=[t1[r], t2[r]], writes=[mixT])
                    ckpt("D1a")
                    with fw.scope() as esD1b:
                        w_out = fw.sb([128, 8, D], BF16, "w_out", esD1b)
                        fw.dma(w_out[:], w_out_d.rearrange("(k p) c -> p k c", p=128), writes=[w_out], q="pool")
                        xtl = [fw.sb([128, D], F32, f"xtl{i}", esD1b) for i in range(2)]
                        for t in range(NT_OWN):
                            x_ = xtl[t % 2]
                            fw.dma(x_[:], xe[S_OWN + t * 128:S_OWN + (t + 1) * 128, :], writes=[x_])
                            for half in range(2):
                                b = 2 * (t % 2) + half
                                for j in range(8):
                                    mm(PS[b], PS[b][:, :], mixT[:, j, t * 128:(t + 1) * 128], w_out[:, j, half * 512:(half + 1) * 512], j == 0, j == 7, [mixT, w_out])
                                op("dve", lambda e: e.tensor_tensor(out=x1[:, t, half * 512:(half + 1) * 512], in0=PS[b][:, :], in1=x_[:, half * 512:(half + 1) * 512], op=ALU.add),
                                   reads=[PS[b], x_], writes=[x1])
                ckpt("D1")
                if "x1" in dbg:
                    fw.dma(dbg_t("x1", [128, NT_OWN, D]), x1[:], reads=[x1], is_output=True)
                w_pg = fw.sb([128, 8, D], BF16, "w_pg", esD)
                fw.dma(w_pg[:], w_pg_d.rearrange("(k p) c -> p k c", p=128), writes=[w_pg], q="pool")
                w_pp = fw.sb([128, 2, D], BF16, "w_pp", esD)
                fw.dma(w_pp[:], w_pp_d.rearrange("(k p) c -> p k c", p=128), writes=[w_pp], q="pool")
                with fw.scope() as esM:
                    load_gain(1)
                    gateT = fw.sb([16, S_OWN], BF16, "gateT", esM)
                    E16 = fw.sb([16, 16, 128], BF16, "E16", esM)
                    op("pool", lambda e: e.memset(E16[:], 1.0), writes=[E16])
                    op("pool", lambda e: e.affine_select(out=E16[:], in_=E16[:], pattern=[[-1, 16], [0, 128]], compare_op=ALU.is_equal, fill=0.0,
                                                         base=0, channel_multiplier=1), reads=[E16], writes=[E16])
                    NW = 3
                    w13 = [fw.sb([128, 8, 512], BF16, f"w13_{i}", esM) for i in range(NW)]
                    w2e = [fw.sb([128, 2, D], BF16, f"w2e_{i}", esM) for i in range(NW)]

                    def load_w(ex):
                        wb = ex % NW
                        fw.dma(w13[wb][:], w_e13_d[ex].rearrange("(k p) c -> p k c", p=128), writes=[w13[wb]], q="pool")
                        fw.dma(w2e[wb][:], w_e2_d[ex].rearrange("(k p) c -> p k c", p=128), writes=[w2e[wb]], q="pool")

                    load_w(0)
                    load_w(1)
                    with fw.scope() as esR:
                        w_r = fw.sb([128, 8, 20], F32, "w_r", esR)
                        fw.dma(w_r[:], w_r_d.rearrange("(k p) c -> p k c", p=128), writes=[w_r])
                        b_r = fw.sb([128, 20], F32, "b_r", esR)
                        fw.dma(b_r[:], b_r_d[0:1, :].to_broadcast([128, 20]), writes=[b_r])
                        hnf = [fw.sb([128, D], F32, f"hnf{i}", esR) for i in range(2)]
                        hnTf = [fw.sb([128, 8, 128], F32, f"hnTf{i}", esR) for i in range(2)]
                        junkR = fw.sb([128, D], BF16, "junkR", esR)
                        ssr = [fw.sb([128, 1], F32, f"ssr{i}", esR) for i in range(2)]
                        rrr = [fw.sb([128, 1], F32, f"rrr{i}", esR) for i in range(2)]
                        T_ = NT_OWN
                        lgA = fw.sb([128, T_, 20], F32, "lgA", esR)

                        def r_front(t):
                            r = t % 2
                            rs = {"ss": ssr[r], "r": rrr[r]}
                            rms_rstd({"ap": x1[:, t, :], "bufs": [x1]}, rs, D, {"ap": junkR[:], "buf": junkR})
                            op("dve", lambda e: e.scalar_tensor_tensor(out=hnf[r][:], in0=x1[:, t, :], scalar=rs["r"][:], in1=gB[:], op0=ALU.mult, op1=ALU.mult),
                               reads=[x1, rs["r"], gB], writes=[hnf[r]])
                            for k in range(8):
                                b = 2 * r + (0 if k < 4 else 1)
                                op("pe", lambda e: e.transpose(out=PS[b][:, (k % 4) * 128:(k % 4 + 1) * 128], in_=hnf[r][:, k * 128:(k + 1) * 128], identity=idf[:]),
                                   reads=[hnf[r], idf], writes=[PS[b]])
                            for bb in range(2):
                                b = 2 * r + bb
                                op("act", lambda e: e.copy(out=hnTf[r][:, 4 * bb:4 * bb + 4, :], in_=PS[b][:, :].rearrange("p (k t) -> p k t", k=4)), reads=[PS[b]], writes=[hnTf[r]])
                                op("dve", lambda e: e.tensor_copy(out=YT[:, 4 * bb:4 * bb + 4, t * 128:(t + 1) * 128], in_=PS[b][:, :].rearrange("p (k t) -> p k t", k=4)),
                                   reads=[PS[b]], writes=[YT])

                        def r_back(t):
                            r = t % 2
                            bl = 4 + r
                            for k in range(8):
                                mm(PS[bl], PS[bl][:, 0:20], hnTf[r][:, k, :], w_r[:, k, :], k == 0, k == 7, [hnTf[r], w_r])
                            op("dve", lambda e: e.tensor_tensor(out=lgA[:, t, :], in0=PS[bl][:, 0:20], in1=b_r[:], op=ALU.add), reads=[PS[bl], b_r], writes=[lgA])

                        for t in range(T_ + 1):
                            if t < T_:
                                r_front(t)
                            if t >= 1:
                                r_back(t - 1)
                        gl = lgA[:, :, 0:4]
                        el = lgA[:, :, 4:20].rearrange("p t (g e) -> p t g e", g=4)
                        gmax = fw.sb([128, T_], F32, "gmax", esR)
                        g1h = fw.sb([128, T_, 4], F32, "g1h", esR)
                        exg = fw.sb([128, T_, 4], F32, "exg", esR)
                        pgs = fw.sb([128, T_], F32, "pgs", esR)
                        t16 = fw.sb([128, T_, 4, 4], F32, "t16", esR)
                        elg = fw.sb([128, T_, 4], F32, "elg", esR)
                        elg2 = fw.sb([128, T_, 4], F32, "elg2", esR)
                        ev1 = fw.sb([128, T_], F32, "ev1", esR)
                        ev2 = fw.sb([128, T_], F32, "ev2", esR)
                        mk1 = fw.sb([128, T_, 4], F32, "mk1", esR)
                        mk2 = fw.sb([128, T_, 4], F32, "mk2", esR)
                        w12 = fw.sb([128, 2, T_], F32, "w12", esR)
                        gig = fw.sb([128, T_, 4], F32, "gig", esR)
                        gate = fw.sb([128, T_, 4, 4], F32, "gate", esR)
                        B3 = [128, T_, 4]
                        op("dve", lambda e: e.tensor_reduce(out=gmax[:], in_=gl, axis=AX.X, op=ALU.max), reads=[lgA], writes=[gmax])
                        op("dve", lambda e: e.tensor_tensor(out=g1h[:], in0=gl, in1=gmax[:].unsqueeze(2).to_broadcast(B3), op=ALU.is_equal), reads=[lgA, gmax], writes=[g1h])
                        op("dve", lambda e: e.tensor_tensor(out=exg[:], in0=gl, in1=gmax[:].unsqueeze(2).to_broadcast(B3), op=ALU.subtract), reads=[lgA, gmax], writes=[exg])
                        op("act", lambda e: e.activation(out=exg[:], in_=exg[:], func=AF.Exp), reads=[exg], writes=[exg])
                        op("dve", lambda e: e.tensor_reduce(out=pgs[:], in_=exg[:], axis=AX.X, op=ALU.add), reads=[exg], writes=[pgs])
                        op("dve", lambda e: e.reciprocal(out=pgs[:], in_=pgs[:]), reads=[pgs], writes=[pgs])
                        op("dve", lambda e: e.tensor_tensor(out=t16[:], in0=el, in1=g1h[:].unsqueeze(3).to_broadcast([128, T_, 4, 4]), op=ALU.mult), reads=[lgA, g1h], writes=[t16])
                        op("dve", lambda e: e.tensor_reduce(out=elg[:], in_=t16[:].rearrange("p t g e -> p t e g"), axis=AX.X, op=ALU.add), reads=[t16], writes=[elg])
                        op("dve", lambda e: e.tensor_reduce(out=ev1[:], in_=elg[:], axis=AX.X, op=ALU.max), reads=[elg], writes=[ev1])
                        op("dve", lambda e: e.tensor_tensor(out=mk1[:], in0=elg[:], in1=ev1[:].unsqueeze(2).to_broadcast(B3), op=ALU.is_equal), reads=[elg, ev1], writes=[mk1])
                        op("dve", lambda e: e.scalar_tensor_tensor(out=elg2[:], in0=mk1[:], scalar=-1e30, in1=elg[:], op0=ALU.mult, op1=ALU.add), reads=[mk1, elg], writes=[elg2])
                        op("dve", lambda e: e.tensor_reduce(out=ev2[:], in_=elg2[:], axis=AX.X, op=ALU.max), reads=[elg2], writes=[ev2])
                        op("dve", lambda e: e.tensor_tensor(out=mk2[:], in0=elg2[:], in1=ev2[:].unsqueeze(2).to_broadcast(B3), op=ALU.is_equal), reads=[elg2, ev2], writes=[mk2])
                        op("dve", lambda e: e.tensor_tensor(out=w12[:, 0, :], in0=ev1[:], in1=ev2[:], op=ALU.subtract), reads=[ev1, ev2], writes=[w12])
                        op("act", lambda e: e.activation(out=w12[:, 0, :], in_=w12[:, 0, :], func=AF.Sigmoid), reads=[w12], writes=[w12])
                        op("dve", lambda e: e.tensor_scalar(out=w12[:, 1, :], in0=w12[:, 0, :], scalar1=-1.0, scalar2=1.0, op0=ALU.mult, op1=ALU.add), reads=[w12], writes=[w12])
                        op("dve", lambda e: e.tensor_tensor(out=w12[:], in0=w12[:], in1=pgs[:].unsqueeze(1).to_broadcast([128, 2, T_]), op=ALU.mult), reads=[w12, pgs], writes=[w12])
                        op("dve", lambda e: e.tensor_tensor(out=gig[:], in0=mk1[:], in1=w12[:, 0, :].unsqueeze(2).to_broadcast(B3), op=ALU.mult), reads=[mk1, w12], writes=[gig])
                        op("dve", lambda e: e.tensor_tensor(out=mk2[:], in0=mk2[:], in1=w12[:, 1, :].unsqueeze(2).to_broadcast(B3), op=ALU.mult), reads=[mk2, w12], writes=[mk2])
                        op("dve", lambda e: e.tensor_tensor(out=gig[:], in0=gig[:], in1=mk2[:], op=ALU.add), reads=[gig, mk2], writes=[gig])
                        op("dve", lambda e: e.tensor_tensor(out=gate[:], in0=g1h[:].unsqueeze(3).to_broadcast([128, T_, 4, 4]),
                                                            in1=gig[:].unsqueeze(2).to_broadcast([128, T_, 4, 4]), op=ALU.mult), reads=[g1h, gig], writes=[gate])
                        for t4 in range(T_ // 4):
                            bk = 6 + t4 % 2
                            for j in range(4):
                                t = t4 * 4 + j
                                op("pe", lambda e: e.transpose(out=PS[bk][0:16, j * 128:(j + 1) * 128], in_=gate[:, t, :, :].rearrange("p g e -> p (g e)"), identity=idf[:]),
                                   reads=[gate, idf], writes=[PS[bk]])
                            op("act", lambda e: e.copy(out=gateT[:, t4 * 512:(t4 + 1) * 512], in_=PS[bk][0:16, :]), reads=[PS[bk]], writes=[gateT])
                    ckpt("D2r")
                    if "gateT" in dbg:
                        fw.dma(dbg_t("gateT", [16, S_OWN], BF16), gateT[:], reads=[gateT], is_output=True)
                    with fw.scope() as esE:
                        sgE = [fw.sb([128, 512], F32, f"sgE{i}", esE) for i in range(2)]
                        tE = [fw.sb([128, 512], F32, f"tE{i}", esE) for i in range(2)]
                        actT = [[fw.sb([128, 512], BF16, f"actT{i}{fc}", esE) for fc in range(2)] for i in range(2)]
                        x1M = [Buf(x1.t, f"x1m_{t}") for t in range(NT_OWN)]
                        for b_ in x1M:
                            b_.lw = x1.lw
                            b_.rd = dict(x1.rd)
                        ybank = [4, 5, 7]
                        yi = [0]

                        def e_front_pe(it):
                            ex, tb = it // 4, it % 4
                            wb = ex % NW
                            ts_ = slice(tb * 512, (tb + 1) * 512)
                            mm(PS[6], PS[6][:, :], E16[:, ex, :], gateT[:, ts_], True, True, [E16, gateT])
                            for fc in range(2):
                                for k in range(8):
                                    mm(PS[fc], PS[fc][:, :], w13[wb][:, k, fc * 128:(fc + 1) * 128], YT[:, k, ts_], k == 0, k == 7, [w13[wb], YT])
                                for k in range(8):
                                    mm(PS[2 + fc], PS[2 + fc][:, :], w13[wb][:, k, 256 + fc * 128:256 + (fc + 1) * 128], YT[:, k, ts_], k == 0, k == 7, [w13[wb], YT])

                        def e_front_post(it):
                            r = it % 2
                            for fc in range(2):
                                op("act", lambda e: e.activation(out=sgE[fc][:], in_=PS[fc][:, :], func=AF.Silu), reads=[PS[fc]], writes=[sgE[fc]])
                                op("dve", lambda e: e.tensor_tensor(out=tE[fc][:], in0=PS[2 + fc][:, :], in1=sgE[fc][:], op=ALU.mult), reads=[PS[2 + fc], sgE[fc]], writes=[tE[fc]])
                                op("dve", lambda e: e.tensor_tensor(out=actT[r][fc][:], in0=PS[6][:, :], in1=tE[fc][:], op=ALU.mult), reads=[PS[6], tE[fc]], writes=[actT[r][fc]])

                        def e_back(it):
                            ex, tb = it // 4, it % 4
                            wb = ex % NW
                            r = it % 2
                            for tt in range(4):
                                t = tb * 4 + tt
                                for half in range(2):
                                    b = ybank[yi[0] % 3]
                                    yi[0] += 1
                                    for fc in range(2):
                                        mm(PS[b], PS[b][:, :], actT[r][fc][:, tt * 128:(tt + 1) * 128], w2e[wb][:, fc, half * 512:(half + 1) * 512], fc == 0, fc == 1, [actT[r][fc], w2e[wb]])
                                    op("dve", lambda e: e.tensor_tensor(out=x1[:, t, half * 512:(half + 1) * 512], in0=PS[b][:, :], in1=x1[:, t, half * 512:(half + 1) * 512], op=ALU.add),
                                       reads=[PS[b], x1M[t]], writes=[x1M[t]])

                        NIT = 64
                        for it in range(NIT + 1):
                            if it < NIT:
                                e_front_pe(it)
                                e_front_post(it)
                            if it >= 1:
                                e_back(it - 1)
                            if it < NIT and it % 4 == 0 and it // 4 + 2 < 16:
                                load_w(it // 4 + 2)
                        for b_ in x1M:
                            if b_.lw is not None and (x1.lw is None or True):
                                pass
                        x1.lw = None
                        x1.rd = {}
                        fw.barrier()
                ckpt("D2")
                if "x2" in dbg:
                    fw.dma(dbg_t("x2", [128, NT_OWN, D]), x1[:], reads=[x1], is_output=True)
                with fw.scope() as esP:
                    load_gain(2)
                    gB2 = fw.sb([128, D], F32, "gB2", esP)
                    fw.dma(gB2[:], gvec_d[3:4, :].to_broadcast([128, D]), writes=[gB2])
                    hpb = [fw.sb([128, D], BF16, f"hpb{i}", esP) for i in range(3)]
                    hpT = [fw.sb([128, 8, 128], BF16, f"hpT{i}", esP) for i in range(3)]
                    plb = [fw.sb([128, 256], BF16, f"plb{i}", esP) for i in range(3)]
                    plT = [fw.sb([128, 2, 128], BF16, f"plT{i}", esP) for i in range(3)]
                    junkP2 = fw.sb([128, D], BF16, "junkP2", esP)
                    sgP = [fw.sb([128, 512], F32, f"sgP{i}", esP) for i in range(2)]
                    tP = [fw.sb([128, 512], F32, f"tP{i}", esP) for i in range(2)]
                    outt = [fw.sb([128, D], F32, f"outt{i}", esP) for i in range(2)]
                    junkP = fw.sb([128, D], BF16, "junkP", esP)
                    ssp = [fw.sb([128, 1], F32, f"ssp{i}", esP) for i in range(5)]
                    rrp = [fw.sb([128, 1], F32, f"rrp{i}", esP) for i in range(5)]
                    x1T = [Buf(x1.t, f"x1_{t}") for t in range(NT_OWN)]
                    for b_ in x1T:
                        b_.lw = x1.lw
                        b_.rd = dict(x1.rd)

                    def p_s1(t):
                        r = t % 3
                        fw.dma(plb[r][:], pl_d[t * 128:(t + 1) * 128, :], writes=[plb[r]], q="pool")
                        rs = {"ss": ssp[r], "r": rrp[r]}
                        rms_rstd({"ap": x1[:, t, :], "bufs": [x1T[t]]}, rs, D, {"ap": junkP[:], "buf": junkP})
                        op("dve", lambda e: e.scalar_tensor_tensor(out=hpb[r][:], in0=x1[:, t, :], scalar=rs["r"][:], in1=gB[:], op0=ALU.mult, op1=ALU.mult),
                           reads=[x1T[t], rs["r"], gB], writes=[hpb[r]])

                    def p_s2(t):
                        r = t % 3
                        b0 = 2 * (t % 2)
                        for k in range(8):
                            op("pe", lambda e: e.transpose(out=psbf(b0)[:, k * 128:(k + 1) * 128], in_=hpb[r][:, k * 128:(k + 1) * 128], identity=idb[:]), reads=[hpb[r], idb], writes=[PS[b0]])
                        op("act", lambda e: e.copy(out=hpT[r][:], in_=psbf(b0).rearrange("p (k t) -> p k t", k=8)), reads=[PS[b0]], writes=[hpT[r]])
                        for k in range(2):
                            op("pe", lambda e: e.transpose(out=psbf(b0 + 1)[:, k * 128:(k + 1) * 128], in_=plb[r][:, k * 128:(k + 1) * 128], identity=idb[:]), reads=[plb[r], idb], writes=[PS[b0 + 1]])
                        op("act", lambda e: e.copy(out=plT[r][:], in_=psbf(b0 + 1)[:, 0:256].rearrange("p (k t) -> p k t", k=2)), reads=[PS[b0 + 1]], writes=[plT[r]])

                    def p_s3(t):
                        r = t % 3
                        for half in range(2):
                            hs = slice(half * 512, (half + 1) * 512)
                            bG = 4 + half
                            bP = 6 + half
                            for k in range(8):
                                mm(PS[bG], PS[bG][:, :], hpT[r][:, k, :], w_pg[:, k, hs], k == 0, k == 7, [hpT[r], w_pg])
                            for k in range(2):
                                mm(PS[bP], PS[bP][:, :], plT[r][:, k, :], w_pp[:, k, hs], k == 0, k == 1, [plT[r], w_pp])
                            op("act", lambda e: e.activation(out=sgP[half][:], in_=PS[bG][:, :], func=AF.Sigmoid), reads=[PS[bG]], writes=[sgP[half]])
                            op("dve", lambda e: e.tensor_tensor(out=tP[half][:], in0=PS[bP][:, :], in1=sgP[half][:], op=ALU.mult), reads=[PS[bP], sgP[half]], writes=[tP[half]])
                            op("dve", lambda e: e.tensor_tensor(out=x1[:, t, hs], in0=x1[:, t, hs], in1=tP[half][:], op=ALU.add), reads=[x1T[t], tP[half]], writes=[x1T[t]])
                        rs2 = {"ss": ssp[3 + t % 2], "r": rrp[3 + t % 2]}
                        rms_rstd({"ap": x1[:, t, :], "bufs": [x1T[t]]}, rs2, D, {"ap": junkP2[:], "buf": junkP2})
                        o_ = outt[t % 2]
                        op("dve", lambda e: e.scalar_tensor_tensor(out=o_[:], in0=x1[:, t, :], scalar=rs2["r"][:], in1=gB2[:], op0=ALU.mult, op1=ALU.mult),
                           reads=[x1T[t], rs2["r"], gB2], writes=[o_])
                        fw.dma(out_d[t * 128:(t + 1) * 128, :], o_[:], reads=[o_], is_output=True)

                    for i in range(NT_OWN + 2):
                        if i < NT_OWN:
                            p_s1(i)
                        if 1 <= i <= NT_OWN:
                            p_s2(i - 1)
                        if i >= 2:
                            p_s3(i - 2)

            if "yaT" in dbg:
                o = dbg_t("yaT", [128, 4, S_OWN], BF16)
                fw.dma(o[:, :, :], YT[:, 0:4, :], reads=[YT], is_output=True)

            if "hT" in dbg:
                o = dbg_t("hT", [128, 8, S_EXT], BF16)
                with fw.scope() as esd:
                    tmp = fw.sb([128, 8, 512], BF16, "dbg_hT", esd)
                    for i in range(8):
                        fw.dma(tmp[:], hT_d[:, :, i * 512:(i + 1) * 512], reads=hT_tiles[4 * i:4 * i + 4], writes=[tmp])
                        fw.dma(o[:, :, i * 512:(i + 1) * 512], tmp[:], reads=[tmp], is_output=True)


        body()
        fw.stopped = False
        fw.finish()
    return nc, dbg_out


_INV = (500000.0 ** (-np.arange(0, 16, 2, dtype=np.float32) / 16.0)).astype(np.float32)


def make_in_maps(inputs):
    f = lambda a: np.ascontiguousarray(np.asarray(a), dtype=np.float32)
    x = f(inputs["x"]); p = f(inputs["p"])
    positions = np.asarray(inputs["positions"]).astype(np.int32)
    w_in = f(inputs["w_in"])[0]
    offs = np.cumsum([0, 512, 128, 128, 128, 128, 128, 128, 24, 1024, 512, 512, 8, 2048])
    seg = {n: (offs[i], offs[i + 1]) for i, n in enumerate(["q", "kc", "vc", "ks", "vs", "kw", "vw", "gate", "qk", "v", "o", "if", "mg"])}
    col = lambda n: w_in[:, seg[n][0]:seg[n][1]]
    w_att = []
    for g in range(2):
        parts = [col("q")[:, g * 256:(g + 1) * 256]]
        for n in ["ks", "kw", "kc", "vc", "vs", "vw"]:
            parts.append(col(n)[:, g * 64:(g + 1) * 64])
        parts.append(col("gate")[:, g * 12:(g + 1) * 12])
        w_att.append(np.concatenate(parts, axis=1))
    w_att = np.ascontiguousarray(np.stack(w_att))
    shared = {
        "invf": np.ascontiguousarray(np.broadcast_to(_INV[None, :], (128, 8))),
        "gvec": np.ascontiguousarray(np.stack([f(inputs["g_mix"])[0], f(inputs["g_ffn"])[0], f(inputs["g_ple"])[0], f(inputs["g_final"])])),
        "w_att": w_att,
        "w_qk": np.ascontiguousarray(col("qk")),
        "w_vo": np.ascontiguousarray(np.concatenate([col("v"), col("o")], axis=1)),
        "w_if": np.ascontiguousarray(col("if")),
        "w_mg": np.ascontiguousarray(col("mg")),
        "b_if": f(inputs["b_if"]).reshape(1, 8),
        "w_c1": np.ascontiguousarray(np.stack([f(inputs["w_ck1"])[0], f(inputs["w_cv1"])[0]])),
        "w_c2": np.ascontiguousarray(np.stack([f(inputs["w_ck2"])[0], f(inputs["w_cv2"])[0]])),
        "pe_c": np.ascontiguousarray(np.stack([f(inputs["pe_ck"])[0], f(inputs["pe_cv"])[0]])),
        "wc": np.ascontiguousarray(f(inputs["w_conv"])[0].reshape(4, 8, 128).transpose(2, 1, 0)),
        "bc": np.ascontiguousarray(f(inputs["b_conv"])[0].reshape(8, 128).T),
        "g_hn": f(inputs["g_hn"]).reshape(1, 512),
        "w_pa": f(inputs["w_pa"])[0], "w_pb": f(inputs["w_pb"])[0], "w_out": f(inputs["w_out"])[0],
        "w_r": np.ascontiguousarray(np.concatenate([f(inputs["w_rg"])[0], f(inputs["w_re"])[0]], axis=1)),
        "b_r": np.ascontiguousarray(np.concatenate([f(inputs["b_rg"])[0], f(inputs["b_re"])[0]])[None, :]),
        "w_e13": f(inputs["w_e13"])[0], "w_e2": f(inputs["w_e2"])[0],
        "w_pg": f(inputs["w_pg"])[0], "w_pp": f(inputs["w_pp"])[0],
    }
    in_maps = []
    for core in range(8):
        b, half = core // 2, core % 2
        if half == 1:
            xe_ = x[b]
            pos_ = positions[b]
        else:
            xe_ = np.concatenate([np.zeros((S_OWN, D), np.float32), x[b, :S_OWN]], axis=0)
            pos_ = np.concatenate([np.zeros(S_OWN, np.int32), positions[b, :S_OWN]])
        m = dict(shared)
        m["xe"] = np.ascontiguousarray(xe_)
        m["pos"] = np.ascontiguousarray(pos_.reshape(NT_EXT, 128).T)
        m["pl"] = np.ascontiguousarray(p[0, b, half * S_OWN:(half + 1) * S_OWN])
        m["hv"] = np.full((128, 1), float(half), np.float32)
        in_maps.append(m)
    return in_maps


def kernel(**inputs):
    nc, _ = build_program()
    in_maps = make_in_maps(inputs)
    res = run_bass_kernel_spmd(nc, in_maps, core_ids=list(range(8)))
    out = np.zeros((4, S_EXT, D), np.float32)
    for core in range(8):
        b, half = core // 2, core % 2
        out[b, half * S_OWN:(half + 1) * S_OWN] = res.results[core]["out"]
    return out
```
